# Optimizing a Trainium2 kernel written in Bass

```python
import jax, jax.numpy as jnp
from jax import lax
import numpy as np

D_MODEL = 1024
BATCH = 8
SEQ = 8192
DEPTH = 2

GRID_W = 64
CTX_LEN = 256
NAT_HEADS = 8
NAT_HEAD_DIM = 64
NAT_WIDTH = NAT_HEADS * NAT_HEAD_DIM
NAT_KH = 8
NAT_KW = 16
HGRN_HEADS = 4
HGRN_KEY_DIM = 128
HGRN_VAL_DIM = 128
HGRN_QK_WIDTH = HGRN_HEADS * HGRN_KEY_DIM
HGRN_V_WIDTH = HGRN_HEADS * HGRN_VAL_DIM
HGRN_CHUNK = 64
AB_IN_WIDTH = 3 * NAT_WIDTH + 3 * HGRN_QK_WIDTH + 2 * HGRN_V_WIDTH
AB_OUT_WIDTH = NAT_WIDTH + HGRN_V_WIDTH
CONV_WIDTH = 31
N_EXPERTS = 32
TOP_K = 4
D_FF_EXPERT = D_MODEL
SWIGLU_LIMIT = 7.0
SWIGLU_ALPHA = 1.702
ROPE_BASE = 10000.0
NORM_EPS = 1e-6
MASK_VALUE = -1e30

kernel_name = "hybrid_nat_hgrn2_conformer_moe_dit"


def _rmsnorm(x, g):
    xf = x.astype(jnp.float32)
    y = xf * lax.rsqrt(jnp.mean(xf * xf, axis=-1, keepdims=True) + NORM_EPS)
    return (y * g.astype(jnp.float32)).astype(x.dtype)


def _layernorm(x, g, b):
    xf = x.astype(jnp.float32)
    mu = jnp.mean(xf, axis=-1, keepdims=True)
    var = jnp.mean(jnp.square(xf - mu), axis=-1, keepdims=True)
    y = (xf - mu) * lax.rsqrt(var + NORM_EPS)
    return (y * g.astype(jnp.float32) + b.astype(jnp.float32)).astype(x.dtype)


def _modulate(x, shift, scale):
    return x * (1 + scale) + shift


def _heads(a, n_heads):
    return a.reshape(a.shape[:-1] + (n_heads, a.shape[-1] // n_heads))


def _rotate(xh, pos):
    n = xh.shape[-1] // 2
    inv_freq = ROPE_BASE ** (-jnp.arange(n, dtype=jnp.float32) / n)
    ang = pos.astype(jnp.float32)[:, None] * inv_freq[None, :]
    cos = jnp.cos(ang)[None, :, None, :]
    sin = jnp.sin(ang)[None, :, None, :]
    x1 = xh[..., :n].astype(jnp.float32)
    x2 = xh[..., n:].astype(jnp.float32)
    return jnp.concatenate([x1 * cos - x2 * sin, x2 * cos + x1 * sin], axis=-1).astype(xh.dtype)


def _axial_rope(x, row_pos, col_pos):
    half = x.shape[-1] // 2
    return jnp.concatenate([_rotate(x[..., :half], row_pos), _rotate(x[..., half:], col_pos)], axis=-1)


def _neighbourhood_attention(q, k, v, q_free, k_ctx, v_ctx, rpb, rows):
    B, S, H, dh = q.shape
    kh = min(NAT_KH, rows)
    n_cb = GRID_W // NAT_KW
    band = 2 * NAT_KW
    q_cols = np.arange(GRID_W).reshape(n_cb, NAT_KW)
    win_c = np.clip(q_cols - NAT_KW // 2, 0, GRID_W - NAT_KW)
    band_c = np.clip(np.arange(n_cb) * NAT_KW - NAT_KW // 2, 0, GRID_W - band)
    key_cols = band_c[:, None] + np.arange(band)[None, :]
    kc = key_cols[:, None, :]
    col_mask = (kc >= win_c[..., None]) & (kc < win_c[..., None] + NAT_KW)
    col_bias_idx = np.clip(kc - q_cols[..., None] + NAT_KW - 1, 0, 2 * NAT_KW - 2)
    kg = k.reshape(B, rows, GRID_W, H, dh)
    vg = v.reshape(B, rows, GRID_W, H, dh)
    n_loc = kh * band

    def row_block(args):
        r, q_r, qf_r = args
        rs = jnp.clip(r - kh // 2, 0, rows - kh)
        k_blk = lax.dynamic_slice_in_dim(kg, rs, kh, axis=1)[:, :, key_cols]
        v_blk = lax.dynamic_slice_in_dim(vg, rs, kh, axis=1)[:, :, key_cols]
        bias = jnp.take(rpb, rs + jnp.arange(kh) - r + NAT_KH - 1, axis=1)[:, :, col_bias_idx]
        bias = bias.transpose(0, 2, 3, 1, 4).astype(jnp.float32)
        q_blk = q_r.reshape(B, n_cb, NAT_KW, H, dh)
        s_loc = jnp.einsum('bcqhd,bicjhd->bhcqij', q_blk, k_blk).astype(jnp.float32) + bias[None]
        s_loc = jnp.where(col_mask[None, None, :, :, None, :], s_loc, MASK_VALUE)
        s_ctx = jnp.einsum('bcqhd,blhd->bhcql', qf_r.reshape(B, n_cb, NAT_KW, H, dh), k_ctx).astype(jnp.float32)
        s_all = jnp.concatenate([s_loc.reshape(B, H, n_cb, NAT_KW, n_loc), s_ctx], axis=-1)
        p = jax.nn.softmax(s_all, axis=-1).astype(v.dtype)
        out = (jnp.einsum('bhcqij,bicjhd->bcqhd', p[..., :n_loc].reshape(B, H, n_cb, NAT_KW, kh, band), v_blk)
               + jnp.einsum('bhcql,blhd->bcqhd', p[..., n_loc:], v_ctx))
        return out.reshape(B, GRID_W, H, dh)

    def to_rows(a):
        return a.reshape(B, rows, GRID_W, H, dh).transpose(1, 0, 2, 3, 4)

    out = lax.map(row_block, (jnp.arange(rows, dtype=jnp.int32), to_rows(q), to_rows(q_free)))
    return out.transpose(1, 0, 2, 3, 4).reshape(B, S, H, dh)


def _context_attention(q, k, v):
    s = jnp.einsum('blhd,bmhd->bhlm', q, k).astype(jnp.float32)
    p = jax.nn.softmax(s, axis=-1).astype(v.dtype)
    return jnp.einsum('bhlm,bmhd->blhd', p, v)


def _hgrn_query(q_raw):
    return _heads(jax.nn.silu(q_raw), HGRN_HEADS) * (HGRN_KEY_DIM ** -0.5)


def _hgrn_gates(f_raw, lb):
    f = lb + (1 - lb) * jax.nn.sigmoid(f_raw.astype(jnp.float32))
    f = _heads(f, HGRN_HEADS)
    return 1 - f, jnp.log(f)


def _flip_if(a, rev):
    return jnp.flip(a, axis=1) if rev else a


def _gla_chunked(q, k, v, log_f, s0):
    B, T, H, dk = k.shape
    dv = v.shape[-1]
    nc = T // HGRN_CHUNK

    def to_chunks(a):
        return a.reshape(B, nc, HGRN_CHUNK, H, a.shape[-1]).transpose(1, 0, 3, 2, 4)

    with_out = q is not None
    xs = (to_chunks(k), to_chunks(v), to_chunks(log_f)) + ((to_chunks(q),) if with_out else ())
    lower = np.tril(np.ones((HGRN_CHUNK, HGRN_CHUNK), dtype=bool))[None, None, :, :, None]

    def step(state, chunk):
        k_c, v_c, lf_c = chunk[0], chunk[1], chunk[2]
        b = jnp.cumsum(lf_c, axis=2)
        b_end = b[:, :, -1:, :]
        state_new = (jnp.exp(b_end)[:, :, 0, :, None] * state
                     + jnp.einsum('bhsk,bhsv->bhkv', k_c * jnp.exp(b_end - b), v_c))
        if not with_out:
            return state_new, None
        q_c = chunk[3]
        decay = jnp.exp(jnp.where(lower, b[:, :, :, None, :] - b[:, :, None, :, :], -jnp.inf))
        scores = jnp.einsum('bhtk,bhsk,bhtsk->bhts', q_c, k_c, decay)
        o_c = (jnp.einsum('bhts,bhsv->bhtv', scores, v_c)
               + jnp.einsum('bhtk,bhkv->bhtv', q_c * jnp.exp(b), state))
        return state_new, o_c

    s_final, o = lax.scan(step, s0, xs)
    if not with_out:
        return None, s_final
    return o.transpose(1, 0, 3, 2, 4).reshape(B, T, H, dv), s_final


def _ab_mixer(xm, xm_ctx, w_in, w_out, q_norm_g, k_norm_g, rpb, lb, o_norm_g, row_pos, col_pos, rows, ctx_out):
    B, S, _ = xm.shape
    L = xm_ctx.shape[1]
    splits = np.cumsum([NAT_WIDTH] * 3 + [HGRN_QK_WIDTH] * 3 + [HGRN_V_WIDTH] * 2)[:-1].tolist()
    nq, nk, nv, hq, hf_fwd, hf_bwd, hi, hg = jnp.split(xm @ w_in, splits, axis=-1)
    cq, ck, cv, chq, chf_fwd, chf_bwd, chi, chg = jnp.split(xm_ctx @ w_in, splits, axis=-1)

    scale = NAT_HEAD_DIM ** -0.5
    q = _rmsnorm(_heads(nq, NAT_HEADS), q_norm_g) * scale
    k = _rmsnorm(_heads(nk, NAT_HEADS), k_norm_g)
    v = _heads(nv, NAT_HEADS)
    k_c = _rmsnorm(_heads(ck, NAT_HEADS), k_norm_g)
    v_c = _heads(cv, NAT_HEADS)
    nat = _neighbourhood_attention(_axial_rope(q, row_pos, col_pos), _axial_rope(k, row_pos, col_pos), v,
                                   q, k_c, v_c, rpb, rows)

    s0 = jnp.zeros((B, HGRN_HEADS, HGRN_KEY_DIM, HGRN_VAL_DIM), jnp.float32)
    q_l = _hgrn_query(hq)
    i_l = _heads(hi, HGRN_HEADS)
    i_c = _heads(chi, HGRN_HEADS)
    q_ch = _hgrn_query(chq) if ctx_out else None
    o_l = jnp.zeros((B, S, HGRN_HEADS, HGRN_VAL_DIM), jnp.float32)
    o_c = jnp.zeros((B, L, HGRN_HEADS, HGRN_VAL_DIM), jnp.float32)
    for d, (f_lat, f_ctx) in enumerate(((hf_fwd, chf_fwd), (hf_bwd, chf_bwd))):
        rev = d == 1
        k_lat, lf_lat = _hgrn_gates(f_lat, lb[d])
        k_cx, lf_cx = _hgrn_gates(f_ctx, lb[d])
        oc, s_ctx = _gla_chunked(_flip_if(q_ch, rev) if ctx_out else None, _flip_if(k_cx, rev),
                                 _flip_if(i_c, rev), _flip_if(lf_cx, rev), s0)
        ol, _ = _gla_chunked(_flip_if(q_l, rev), _flip_if(k_lat, rev), _flip_if(i_l, rev),
                             _flip_if(lf_lat, rev), s_ctx)
        o_l = o_l + _flip_if(ol, rev)
        if ctx_out:
            o_c = o_c + _flip_if(oc, rev)

    def gated(o, g):
        y = _rmsnorm(o.astype(xm.dtype), o_norm_g) * jax.nn.silu(_heads(g, HGRN_HEADS))
        return y.reshape(o.shape[0], o.shape[1], HGRN_V_WIDTH)

    y = jnp.concatenate([nat.reshape(B, S, NAT_WIDTH), gated(o_l, hg)], axis=-1) @ w_out
    if not ctx_out:
        return y, None
    q_c = _rmsnorm(_heads(cq, NAT_HEADS), q_norm_g) * scale
    nat_c = _context_attention(q_c, k_c, v_c)
    y_ctx = jnp.concatenate([nat_c.reshape(B, L, NAT_WIDTH), gated(o_c, chg)], axis=-1) @ w_out
    return y, y_ctx


def _depthwise_conv(x, w, b):
    C = x.shape[-1]
    pad = CONV_WIDTH // 2
    y = lax.conv_general_dilated(x, w[:, None, :].astype(x.dtype), window_strides=(1,), padding=[(pad, pad)],
                                 dimension_numbers=('NWC', 'WIO', 'NWC'), feature_group_count=C)
    return y + b


def _conformer_conv(xm, w1, b1, w_dw, b_dw, ln_g, ln_b, w2, b2):
    h = xm @ w1 + b1
    a, gate = jnp.split(h, 2, axis=-1)
    h = a * jax.nn.sigmoid(gate)
    h = _depthwise_conv(h, w_dw, b_dw)
    h = jax.nn.silu(_layernorm(h, ln_g, ln_b))
    return h @ w2 + b2


def _moe(x2d, router_w, router_b, w1, b1, w2, b2):
    logits = (x2d @ router_w + router_b).astype(jnp.float32)
    top_vals, top_idx = lax.top_k(logits, TOP_K)
    top_w = jax.nn.softmax(top_vals, axis=-1)
    combine = jnp.sum(jax.nn.one_hot(top_idx, N_EXPERTS, dtype=jnp.float32) * top_w[..., None], axis=1)

    def expert(acc, p):
        w1e, b1e, w2e, b2e, g_e = p
        h = x2d @ w1e + b1e
        x_glu, x_lin = jnp.split(h, 2, axis=-1)
        x_glu = jnp.minimum(x_glu, SWIGLU_LIMIT)
        x_lin = jnp.clip(x_lin, -SWIGLU_LIMIT, SWIGLU_LIMIT)
        act = x_glu * jax.nn.sigmoid(SWIGLU_ALPHA * x_glu) * (x_lin + 1)
        return acc + g_e[:, None] * (act @ w2e + b2e), None

    out, _ = lax.scan(expert, jnp.zeros_like(x2d), (w1, b1, w2, b2, combine.T.astype(x2d.dtype)))
    return out


def setup_inputs(seed: int = 0) -> dict:
    key = jax.random.key(seed)
    ks = iter(jax.random.split(key, 32))
    n_ab = (DEPTH + 1) // 2
    n_c = DEPTH // 2
    D = D_MODEL

    def nrm(shape, scale):
        return jax.random.normal(next(ks), shape, jnp.float32) * scale

    return {
        'x': nrm((BATCH, SEQ, D), 1.0),
        'c': nrm((BATCH, D), 1.0),
        'ctx': nrm((BATCH, CTX_LEN, D), 1.0),
        'c_ctx': nrm((D,), 1.0),
        'ada_w': nrm((DEPTH, D, 6 * D), 0.5 * D ** -0.5),
        'ada_b': nrm((DEPTH, 6 * D), 0.02),
        'norm1_g': 1.0 + nrm((DEPTH, D), 0.02),
        'norm2_g': 1.0 + nrm((DEPTH, D), 0.02),
        'ab_w_in': nrm((n_ab, D, AB_IN_WIDTH), D ** -0.5),
        'ab_w_out': nrm((n_ab, AB_OUT_WIDTH, D), AB_OUT_WIDTH ** -0.5),
        'nat_q_norm': 1.0 + nrm((n_ab, NAT_HEAD_DIM), 0.02),
        'nat_k_norm': 1.0 + nrm((n_ab, NAT_HEAD_DIM), 0.02),
        'nat_rpb': nrm((n_ab, NAT_HEADS, 2 * NAT_KH - 1, 2 * NAT_KW - 1), 0.5),
        'hgrn_lb': nrm((2, n_ab + 1, HGRN_QK_WIDTH), 0.5),
        'hgrn_o_norm': 1.0 + nrm((n_ab, HGRN_VAL_DIM), 0.02),
        'conv_w1': nrm((n_c, D, 2 * D), D ** -0.5),
        'conv_b1': nrm((n_c, 2 * D), 0.02),
        'conv_dw': nrm((n_c, CONV_WIDTH, D), CONV_WIDTH ** -0.5),
        'conv_dw_b': nrm((n_c, D), 0.02),
        'conv_ln_g': 1.0 + nrm((n_c, D), 0.02),
        'conv_ln_b': nrm((n_c, D), 0.02),
        'conv_w2': nrm((n_c, D, D), D ** -0.5),
        'conv_b2': nrm((n_c, D), 0.02),
        'router_w': nrm((DEPTH, D, N_EXPERTS), D ** -0.5),
        'router_b': nrm((DEPTH, N_EXPERTS), 0.01),
        'moe_w1': nrm((DEPTH, N_EXPERTS, D, 2 * D_FF_EXPERT), D ** -0.5),
        'moe_b1': nrm((DEPTH, N_EXPERTS, 2 * D_FF_EXPERT), 0.02),
        'moe_w2': nrm((DEPTH, N_EXPERTS, D_FF_EXPERT, D), D_FF_EXPERT ** -0.5),
        'moe_b2': nrm((DEPTH, N_EXPERTS, D), 0.02),
    }


def reference(x, c, ctx, c_ctx, ada_w, ada_b, norm1_g, norm2_g, ab_w_in, ab_w_out, nat_q_norm, nat_k_norm,
              nat_rpb, hgrn_lb, hgrn_o_norm, conv_w1, conv_b1, conv_dw, conv_dw_b, conv_ln_g, conv_ln_b,
              conv_w2, conv_b2, router_w, router_b, moe_w1, moe_b1, moe_w2, moe_b2):
    B, S, D = x.shape
    L = ctx.shape[1]
    rows = S // GRID_W
    pos = jnp.arange(S, dtype=jnp.int32)
    row_pos, col_pos = pos // GRID_W, pos % GRID_W
    lb_all = jnp.cumsum(jax.nn.softmax(hgrn_lb.astype(jnp.float32), axis=1), axis=1)
    silu_c = jax.nn.silu(c)
    silu_cc = jax.nn.silu(c_ctx)
    h, h_ctx = x, ctx
    for layer in range(DEPTH):
        j = layer // 2
        is_ab = layer % 2 == 0
        ctx_after = any(l % 2 == 0 for l in range(layer + 1, DEPTH))
        mod = jnp.split((silu_c @ ada_w[layer] + ada_b[layer])[:, None, :], 6, axis=-1)
        xm = _modulate(_rmsnorm(h, norm1_g[layer]), mod[0], mod[1])
        if is_ab or ctx_after:
            cmod = jnp.split(silu_cc @ ada_w[layer] + ada_b[layer], 6, axis=-1)
            xm_ctx = _modulate(_rmsnorm(h_ctx, norm1_g[layer]), cmod[0], cmod[1])
        if is_ab:
            y, y_ctx = _ab_mixer(xm, xm_ctx, ab_w_in[j], ab_w_out[j], nat_q_norm[j], nat_k_norm[j], nat_rpb[j],
                                 lb_all[:, j], hgrn_o_norm[j], row_pos, col_pos, rows, ctx_after)
        else:
            conv_p = (conv_w1[j], conv_b1[j], conv_dw[j], conv_dw_b[j], conv_ln_g[j], conv_ln_b[j], conv_w2[j], conv_b2[j])
            y = _conformer_conv(xm, *conv_p)
            y_ctx = _conformer_conv(xm_ctx, *conv_p) if ctx_after else None
        h = h + mod[2] * y
        moe_p = (router_w[layer], router_b[layer], moe_w1[layer], moe_b1[layer], moe_w2[layer], moe_b2[layer])
        if ctx_after:
            h_ctx = h_ctx + cmod[2] * y_ctx
            xm2 = jnp.concatenate([_modulate(_rmsnorm(h, norm2_g[layer]), mod[3], mod[4]),
                                   _modulate(_rmsnorm(h_ctx, norm2_g[layer]), cmod[3], cmod[4])], axis=1)
            out2 = _moe(xm2.reshape(-1, D), *moe_p).reshape(B, S + L, D)
            h = h + mod[5] * out2[:, :S]
            h_ctx = h_ctx + cmod[5] * out2[:, S:]
        else:
            xm2 = _modulate(_rmsnorm(h, norm2_g[layer]), mod[3], mod[4])
            h = h + mod[5] * _moe(xm2.reshape(-1, D), *moe_p).reshape(B, S, D)
    return h
```

```python
from contextlib import ExitStack
import numpy as np
import ml_dtypes
import concourse.bass as bass
import concourse.mybir as mybir
from concourse.bass_utils import run_bass_kernel_spmd

F32 = mybir.dt.float32
BF16 = mybir.dt.bfloat16
AF = mybir.ActivationFunctionType
ALU = mybir.AluOpType
AX = mybir.AxisListType

COMPUTE = ("pe", "act", "dve", "pool")
ALLENG = ("pe", "act", "dve", "pool", "sp")
NDSEM = 72


class Buf:
    __slots__ = ("name", "writers", "readers")

    def __init__(self, name):
        self.name = name
        self.writers = []
        self.readers = []


class Op:
    __slots__ = ("eng", "fn", "raw", "oth", "signal", "tok_sem", "tok_val", "is_dma", "key", "phase")

    def __init__(self, eng, fn, phase):
        self.eng = eng
        self.fn = fn
        self.raw = []
        self.oth = []
        self.signal = False
        self.tok_sem = None
        self.tok_val = 0
        self.is_dma = False
        self.key = None
        self.phase = phase


class Prog:
    def __init__(self, nc, es):
        self.nc = nc
        self.phase = 0
        self.esem = {e: es.enter_context(nc.semaphore("s_" + e)) for e in COMPUTE}
        self.ecnt = {e: 0 for e in COMPUTE}
        self.dsem = [es.enter_context(nc.semaphore("d%d" % i)) for i in range(NDSEM)]
        self.dcnt = [0] * NDSEM
        self.seen = {e: {} for e in ALLENG}
        self._reset()
        self.nops = 0

    def _reset(self):
        self.ops = {e: [] for e in ALLENG}
        self.order = []
        self.keymap = {}
        self.last = {}

    def buf(self, name="b"):
        return Buf(name)

    def _deps(self, op, reads, writes, wpart):
        ph = self.phase
        for b in reads:
            for w in b.writers:
                if w.phase == ph:
                    op.raw.append(w)
        for b in list(writes) + list(wpart):
            for r in b.readers:
                if r.phase == ph:
                    op.oth.append(r)
        for b in writes:
            for w in b.writers:
                if w.phase == ph:
                    op.oth.append(w)
        for b in reads:
            b.readers.append(op)
        for b in writes:
            b.writers = [op]
            b.readers = []
        for b in wpart:
            if b.readers:
                b.writers = [op]
                b.readers = []
            else:
                b.writers.append(op)

    def op(self, eng, fn, reads=(), writes=(), wpart=()):
        o = Op(eng, fn, self.phase)
        self._deps(o, reads, writes, wpart)
        self.ops[eng].append(o)
        self.order.append(o)
        self.last[eng] = o
        return o

    def dma(self, q, out, in_, reads=(), writes=(), wpart=(), key=None, **kw):
        def fn(e, out=out, in_=in_, kw=kw):
            return e.dma_start(out=out, in_=in_, **kw)
        o = Op(q, fn, self.phase)
        o.is_dma = True
        if key is None:
            ws = list(writes) + list(wpart)
            key = ws[0]
        if key not in self.keymap:
            self.keymap[key] = len(self.keymap)
            assert len(self.keymap) <= NDSEM, "too many DMA keys in phase"
        o.key = self.keymap[key]
        self._deps(o, reads, writes, wpart)
        self.ops[q].append(o)
        self.order.append(o)
        return o

    def end_phase(self):
        nc = self.nc
        lasts = [self.last[e] for e in COMPUTE if e in self.last]
        lastd = {}
        for o in self.order:
            if o.is_dma:
                lastd[o.key] = o
        for e in ALLENG:
            o = Op(e, (lambda eng: eng.nop()), self.phase)
            o.raw = list(lasts) + list(lastd.values())
            self.ops[e].append(o)
            self.order.append(o)
        for o in self.order:
            for d in o.raw:
                if d.is_dma:
                    continue
                if d.eng == o.eng and o.eng == "pe" and not o.is_dma:
                    continue
                d.signal = True
            for d in o.oth:
                if d.is_dma:
                    continue
                if d.eng == o.eng and not o.is_dma:
                    continue
                d.signal = True
        for e in COMPUTE:
            for o in self.ops[e]:
                if o.is_dma:
                    continue
                if o.signal:
                    self.ecnt[e] += 1
                    o.tok_sem = self.esem[e]
                    o.tok_val = self.ecnt[e]
        for o in self.order:
            if o.is_dma:
                self.dcnt[o.key] += 16
                o.tok_sem = self.dsem[o.key]
                o.tok_val = self.dcnt[o.key]
        self.nops += len(self.order)

        with nc.Block() as block:
            def run(ename):
                def body(eng):
                    seen = self.seen[ename]
                    for o in self.ops[ename]:
                        need = {}
                        for d in o.raw:
                            if d.tok_sem is None:
                                continue
                            if (not d.is_dma) and d.eng == ename and ename == "pe" and not o.is_dma:
                                continue
                            s = d.tok_sem
                            if need.get(s.num, (None, 0))[1] < d.tok_val:
                                need[s.num] = (s, d.tok_val)
                        for d in o.oth:
                            if d.tok_sem is None:
                                continue
                            if (not d.is_dma) and d.eng == ename and not o.is_dma:
                                continue
                            s = d.tok_sem
                            if need.get(s.num, (None, 0))[1] < d.tok_val:
                                need[s.num] = (s, d.tok_val)
                        for s, v in need.values():
                            if seen.get(s.num, 0) < v:
                                eng.wait_ge(s, v)
                                seen[s.num] = v
                        ins = o.fn(eng)
                        if o.is_dma:
                            ins.then_inc(o.tok_sem, 16)
                        elif o.signal:
                            ins.then_inc(o.tok_sem, 1)
                return body

            block.tensor(run("pe"))
            block.scalar(run("act"))
            block.vector(run("dve"))
            block.gpsimd(run("pool"))
            block.sync(run("sp"))
        self.phase += 1
        self._reset()


class Slot:
    __slots__ = ("t", "b")

    def __init__(self, t, b):
        self.t = t
        self.b = b


class Ctx:
    pass


class K:
    def __init__(self, P):
        self.P = P

    def ts(self, eng, out, in0, s1, op0, s2=None, op1=None, r=(), w=(), wp=(), accum=None):
        if op1 is None:
            if accum is None:
                f = lambda e: e.tensor_scalar(out=out, in0=in0, scalar1=s1, scalar2=None, op0=op0)
            else:
                f = lambda e: e.tensor_scalar(out=out, in0=in0, scalar1=s1, scalar2=None, op0=op0, accum_out=accum)
        else:
            f = lambda e: e.tensor_scalar(out=out, in0=in0, scalar1=s1, scalar2=s2, op0=op0, op1=op1)
        return self.P.op(eng, f, reads=r, writes=w, wpart=wp)

    def tt(self, eng, out, in0, in1, op, r=(), w=(), wp=()):
        return self.P.op(eng, lambda e: e.tensor_tensor(out=out, in0=in0, in1=in1, op=op), reads=r, writes=w, wpart=wp)

    def stt(self, out, in0, scalar, in1, op0, op1, r=(), w=(), wp=(), accum=None):
        if accum is None:
            f = lambda e: e.scalar_tensor_tensor(out=out, in0=in0, scalar=scalar, in1=in1, op0=op0, op1=op1)
        else:
            f = lambda e: e.scalar_tensor_tensor(out=out, in0=in0, scalar=scalar, in1=in1, op0=op0, op1=op1, accum_out=accum)
        return self.P.op("dve", f, reads=r, writes=w, wpart=wp)

    def act(self, out, in_, func, r=(), w=(), wp=(), bias=None, scale=None, accum=None):
        kw = {}
        if bias is not None:
            kw["bias"] = bias
        if scale is not None:
            kw["scale"] = scale
        if accum is not None:
            kw["accum_out"] = accum
        return self.P.op("act", lambda e: e.activation(out=out, in_=in_, func=func, **kw), reads=r, writes=w, wpart=wp)

    def cp(self, eng, out, in_, r=(), w=(), wp=()):
        if eng == "act":
            return self.P.op("act", lambda e: e.copy(out=out, in_=in_), reads=r, writes=w, wpart=wp)
        return self.P.op(eng, lambda e: e.tensor_copy(out=out, in_=in_), reads=r, writes=w, wpart=wp)

    def memset(self, eng, ap, val, w=(), wp=()):
        return self.P.op(eng, lambda e: e.memset(ap, val), writes=w, wpart=wp)

    def mm(self, out, lhsT, rhs, start, stop, r=(), w=(), wp=()):
        return self.P.op("pe", lambda e: e.matmul(out, lhsT, rhs, start=start, stop=stop), reads=r, writes=w, wpart=wp)

    def tr(self, out, in_, ident, r=(), w=(), wp=()):
        return self.P.op("pe", lambda e: e.transpose(out, in_, ident), reads=r, writes=w, wpart=wp)

    def red(self, out, in_, r=(), w=(), wp=()):
        return self.P.op("dve", lambda e: e.tensor_reduce(out=out, in_=in_, axis=AX.X, op=ALU.add), reads=r, writes=w, wpart=wp)

    def recip(self, out, in_, r=(), w=(), wp=()):
        return self.P.op("dve", lambda e: e.reciprocal(out=out, in_=in_), reads=r, writes=w, wpart=wp)

    def dma(self, q, out, in_, r=(), w=(), wp=(), key=None):
        return self.P.dma(q, out, in_, reads=r, writes=w, wpart=wp, key=key)


class Cfg:
    def __init__(self, S=8192, L=256, E=32, debug=False, stop_after=None):
        self.S, self.L, self.E = S, L, E
        self.D = 1024
        self.NT = S // 128
        self.ROWS = S // 64
        self.NCH = S // 64
        self.MB = min(1024, S)
        self.NTILE = (4 * S) // 512 + E
        self.NSLOT = self.NTILE * 512
        self.debug = debug
        self.dense = False
        self.stop_after = stop_after


EPS = 1e-6
NEG = -30000.0


def host_consts(cfg):
    S = cfg.S
    NT = cfg.NT
    cs = {}
    cs["ident_f"] = np.eye(128, dtype=np.float32)
    p = np.arange(128)
    t = np.arange(64)
    s = p % 64
    cs["trif"] = (s[:, None] <= t[None, :]).astype(np.float32)
    cs["trib"] = (s[:, None] >= t[None, :]).astype(np.float32)
    r = np.ones((128, 512), np.float32)
    r[:, ::64] = 0.0
    cs["reset"] = r
    inv = (10000.0 ** (-np.arange(16, dtype=np.float32) / 16.0)).astype(np.float32)
    tt = np.arange(NT)
    row = (2 * tt[None, :] + (p[:, None] // 64)).astype(np.float32)
    col = np.broadcast_to((p % 64).astype(np.float32)[:, None], (128, NT))
    ang = np.stack([row[:, :, None] * inv[None, None, :], col[:, :, None] * inv[None, None, :]], axis=2)
    ang = ang.astype(np.float32)
    E, NTILE = cfg.E, cfg.NTILE
    cs["ut"] = (p[:, None] < p[None, :]).astype(np.float32)
    cs["iota"] = p.astype(np.float32).reshape(128, 1)
    kp = np.zeros((128, 9), np.float32)
    kp[:, :8] = np.arange(8)[None, :] * 128 + p[:, None]
    kp[:, 8] = p % 16
    cs["kp"] = kp
    cs["th"] = np.broadcast_to((512.0 * np.arange(16, dtype=np.float32))[None, None, :], (1, E, 16)).reshape(1, E * 16).copy()
    cs["j512"] = np.broadcast_to((512.0 * np.arange(NTILE, dtype=np.float32))[None, :, None], (1, NTILE, E)).reshape(1, NTILE * E).copy()
    si = np.zeros((cfg.NSLOT, 2), np.float32)
    si[:, 0] = S + (np.arange(cfg.NSLOT) % 128)
    cs["slotinit"] = si
    cs["cos"] = np.cos(ang).astype(np.float32).reshape(128, NT * 32)
    cs["sin"] = np.sin(ang).astype(np.float32).reshape(128, NT * 32)
    return cs


def layout_rpb(rpb):
    H = rpb.shape[0]
    c = np.arange(64)[:, None]
    kc = np.arange(64)[None, :]
    win = np.clip(c - 8, 0, 48)
    valid = (kc >= win) & (kc < win + 16)
    idx = np.clip(kc - c + 15, 0, 30)
    g = rpb[:, :, idx]
    g = np.where(valid[None, None], g, np.float32(NEG)).astype(np.float32)
    return np.ascontiguousarray(g.transpose(2, 0, 1, 3)).reshape(64, H * 15 * 64)


_uid = [0]


def sb(es, nc, shape, dt, n=1, name="t"):
    out = []
    for i in range(n):
        _uid[0] += 1
        t = es.enter_context(nc.sbuf_tensor("%s_%d" % (name, _uid[0]), list(shape), dt))
        out.append(Slot(t, Buf(name)))
    return out if n > 1 else out[0]


def ps(es, nc, shape, dt, n=1, name="p"):
    out = []
    for i in range(n):
        _uid[0] += 1
        t = es.enter_context(nc.psum_tensor("%s_%d" % (name, _uid[0]), list(shape), dt))
        out.append(Slot(t, Buf(name)))
    return out if n > 1 else out[0]


def declare_io(nc, cfg):
    S, L, E = cfg.S, cfg.L, cfg.E
    d = {}

    inputs = set()
    d["_inputs"] = inputs

    def inp(name, shape, dt=F32):
        d[name] = nc.dram_tensor(name, list(shape), dt, kind="ExternalInput").ap()
        inputs.add(name)

    inp("x", [S, 1024]); inp("c", [8, 128]); inp("ctx", [L, 1024]); inp("c_ctx", [8, 128])
    inp("ada_w", [2, 1024, 6144]); inp("ada_b", [2, 6144]); inp("norm1_g", [2, 1024]); inp("norm2_g", [2, 1024])
    inp("ab_w_in", [1024, 4096]); inp("ab_w_out", [1024, 1024]); inp("nat_q_norm", [1, 64]); inp("nat_k_norm", [1, 64])
    inp("rpb_full", [64, 8 * 15 * 64]); inp("hgrn_lb", [16, 128]); inp("hgrn_o_norm", [1, 128])
    inp("conv_w1", [1024, 2048]); inp("conv_b1", [16, 128]); inp("conv_dw", [31, 1024]); inp("conv_dw_b", [8, 128])
    inp("conv_ln_g", [8, 128]); inp("conv_ln_b", [8, 128]); inp("conv_w2", [1024, 1024]); inp("conv_b2", [1, 1024])
    inp("router_w", [2, 1024, E]); inp("router_b", [2, E])
    inp("moe_w1", [2, E, 1024, 2048]); inp("moe_b1", [2, E * 16, 128]); inp("moe_w2", [2, E, 1024, 1024]); inp("moe_b2", [2, E, 1024])
    inp("k_ident_f", [128, 128]); inp("k_trif", [128, 64]); inp("k_trib", [128, 64]); inp("k_reset", [128, 512])
    inp("k_cos", [128, cfg.NT * 32]); inp("k_sin", [128, cfg.NT * 32])
    inp("k_ut", [128, 128]); inp("k_iota", [128, 1]); inp("k_th", [1, E * 16]); inp("k_j512", [1, cfg.NTILE * E])
    inp("k_slotinit", [cfg.NSLOT, 2]); inp("k_kp", [128, 9])
    d["out"] = nc.dram_tensor("out", [S, 1024], F32, kind="ExternalOutput").ap()
    kind = "ExternalOutput" if cfg.debug else "Internal"

    def scr(name, shape, dt):
        d[name] = nc.dram_tensor(name, list(shape), dt, kind=kind).ap()

    scr("XT", [128, 8, S], BF16); scr("XTc", [128, 8, L], BF16)
    scr("QTr", [128, 4, S], BF16); scr("QTf", [128, 4, S], BF16); scr("KTr", [128, 4, S], BF16)
    scr("KcT", [128, 4, L], BF16)
    scr("VA", [S, 520], BF16); scr("VcA", [L, 520], BF16)
    scr("VH", [S, 512], BF16); scr("VHc", [L, 512], BF16)
    scr("G", [S, 512], F32)
    scr("HQ", [2, 128, 4, S], BF16); scr("HK", [2, 128, 4, S], BF16)
    scr("KH", [2, S, 512], BF16); scr("KHc", [2, L, 512], BF16)
    scr("DEC", [2, 128, 4, S // 64], F32); scr("DECc", [2, 128, 4, L // 64], F32)
    scr("OF", [S, 512], F32)
    scr("CAT", [S, 1024], BF16)
    scr("H1", [S, 1024], F32); scr("H2", [S, 1024], F32); scr("H3", [S, 1024], F32)
    scr("XT2", [128, 8, S], BF16)
    scr("COMB", [S, E], F32)
    scr("ACC0", [S, 1024], F32)
    scr("GT", [128, 8, S + 32], BF16)
    scr("YD", [128, 8, S], F32)
    scr("XM2", [S + 128, 1024], BF16)
    scr("MK", [S, E], F32)
    scr("ACCd", [S + 128, 1024], F32)
    scr("SLOT", [cfg.NSLOT, 2], F32)
    return d


def build_program(cfg):
    nc = bass.Bass("TRN2", target_bir_lowering=False)
    c = Ctx()
    c.nc, c.cfg = nc, cfg
    c.d = declare_io(nc, cfg)
    c.inputs = c.d.pop("_inputs")
    with ExitStack() as ges:
        P = Prog(nc, ges)
        c.P = P
        c.k = K(P)
        c.ident_f = sb(ges, nc, [128, 128], F32, name="identf")
        c.ident_b = sb(ges, nc, [128, 128], BF16, name="identb")
        c.ones_f = sb(ges, nc, [128, 128], F32, name="onesf")
        c.k.dma("sp", c.ident_f.t[:], c.d["k_ident_f"], w=[c.ident_f.b])
        c.k.cp("dve", c.ident_b.t[:], c.ident_f.t[:], r=[c.ident_f.b], w=[c.ident_b.b])
        c.k.memset("dve", c.ones_f.t[:], 1.0, w=[c.ones_f.b])
        touch = sb(ges, nc, [1, 64], F32, name="touch")
        for i, nm in enumerate(sorted(c.inputs)):
            ap = c.d[nm]
            idx = tuple([0] * (len(ap.shape) - 2) + [slice(0, 1), slice(0, 1)])
            c.k.dma("sp", touch.t[0:1, i:i + 1], ap[idx], wp=[touch.b])
        if cfg.debug:
            tb = sb(ges, nc, [1, 2], BF16, name="touchb")
            c.k.memset("dve", touch.t[0:1, 62:64], 0.0, wp=[touch.b])
            c.k.memset("dve", tb.t[:], 0.0, w=[tb.b])
            for i, (nm, ap) in enumerate(c.d.items()):
                if nm not in c.inputs:
                    idx = tuple([0] * (len(ap.shape) - 2) + [slice(0, 1), slice(0, 1)])
                    src = tb.t[0:1, 0:1] if ap.dtype == BF16 else touch.t[0:1, 63:64]
                    c.k.dma("sp", ap[idx], src, r=[tb.b, touch.b], w=[Buf("o")])
        P.end_phase()
        def dbg_stop(name):
            return cfg.stop_after == name

        done = False
        for layer in (0, 1):
            with ExitStack() as lay:
                c.MOD = sb(lay, nc, [128, 6144], F32, name="MOD")
                if layer == 0:
                    c.CMOD = sb(lay, nc, [128, 2048], F32, name="CMOD")
                    seq = [("mods0", lambda: phase_mods(c, 0)), ("a1", lambda: phase_a1(c)), ("a2", lambda: phase_a2(c)),
                           ("nat", lambda: phase_nat(c)), ("hgrn", lambda: phase_hgrn(c)), ("post0", lambda: phase_post(c, 0))]
                else:
                    seq = [("mods1", lambda: phase_mods(c, 1)), ("f", lambda: phase_f(c)), ("g1", lambda: phase_g1(c)),
                           ("post1", lambda: phase_post(c, 1))]
                for name, fn in seq:
                    fn()
                    if dbg_stop(name):
                        done = True
                        break
            if done:
                break
            with ExitStack() as m5:
                MOD5 = sb(m5, nc, [128, 1024], F32, name="MOD5")
                with ExitStack() as ph:
                    emit_mods(c, ph, layer, [10, 11], lambda ct: MOD5.t[:, (ct - 10) * 512:(ct - 9) * 512], MOD5.b, False)
                    P.end_phase()
                if cfg.dense:
                    phase_moe(c, layer, MOD5)
                else:
                    TEi = (sb(m5, nc, [128, cfg.NTILE, 8], mybir.dt.int32, name="IDXW"), sb(m5, nc, [128, cfg.NTILE], mybir.dt.int32, name="IDXB"))
                    phase_route(c, layer, TEi)
                    if dbg_stop("route%d" % layer):
                        break
                    phase_smoe(c, layer, MOD5, TEi)
            if dbg_stop("moe%d" % layer):
                break
    return nc, c


def emit_mods(c, ph, layer, cts, dst_fn, dst_buf, with_ctx):
    nc, P, k, d = c.nc, c.P, c.k, c.d
    crow = sb(ph, nc, [16, 128], F32, name="crow")
    crow2 = sb(ph, nc, [16, 128], F32, name="crow2")
    ccol = sb(ph, nc, [128, 16], F32, name="ccol")
    CB = sb(ph, nc, [128, 16, 128], F32, name="CB")
    AW = sb(ph, nc, [128, 8, 512], F32, n=2, name="AW")
    ABr = sb(ph, nc, [1, 512], F32, n=2, name="ABr")
    pT = ps(ph, nc, [128, 16], F32, name="pT")
    pM = ps(ph, nc, [128, 512], F32, n=2, name="pM")
    pC = ps(ph, nc, [128, 512], F32, n=2, name="pC")
    k.dma("sp", crow.t[0:8, :], d["c"], wp=[crow.b])
    k.dma("sp", crow.t[8:16, :], d["c_ctx"], wp=[crow.b])
    k.act(crow2.t[:], crow.t[:], AF.Silu, r=[crow.b], w=[crow2.b])
    k.tr(pT.t[:], crow2.t[:], c.ident_f.t[0:16, 0:16], r=[crow2.b], w=[pT.b])
    k.cp("dve", ccol.t[:], pT.t[:], r=[pT.b], w=[ccol.b])
    for j in range(16):
        k.cp("dve" if j % 2 else "pool", CB.t[:, j, :], ccol.t[:, j:j + 1].to_broadcast([128, 128]), r=[ccol.b], wp=[CB.b])
    awv = d["ada_w"][layer].rearrange("(k p) n -> p k n", p=128)
    for i, ct in enumerate(cts):
        aw, ab = AW[i % 2], ABr[i % 2]
        k.dma("sp", aw.t[:], awv[:, :, ct * 512:(ct + 1) * 512], w=[aw.b])
        k.dma("sp", ab.t[:], d["ada_b"][layer:layer + 1, ct * 512:(ct + 1) * 512], w=[ab.b])
        pm = pM[i % 2]
        for kk in range(8):
            k.mm(pm.t[:], CB.t[:, kk, :], aw.t[:, kk, :], kk == 0, False, r=[CB.b, aw.b], w=[pm.b] if kk == 0 else (), wp=() if kk == 0 else [pm.b])
        k.mm(pm.t[:], c.ones_f.t[0:1, :], ab.t[:], False, True, r=[ab.b], wp=[pm.b])
        k.cp("act", dst_fn(ct), pm.t[:], r=[pm.b], wp=[dst_buf])
        if with_ctx and ct < 4:
            pc = pC[i % 2]
            for kk in range(8):
                k.mm(pc.t[:], CB.t[:, 8 + kk, :], aw.t[:, kk, :], kk == 0, False, r=[CB.b, aw.b], w=[pc.b] if kk == 0 else (), wp=() if kk == 0 else [pc.b])
            k.mm(pc.t[:], c.ones_f.t[0:1, :], ab.t[:], False, True, r=[ab.b], wp=[pc.b])
            k.cp("dve", c.CMOD.t[:, ct * 512:(ct + 1) * 512], pc.t[:], r=[pc.b], wp=[c.CMOD.b])


def phase_mods(c, layer):
    with ExitStack() as ph:
        emit_mods(c, ph, layer, list(range(10)), lambda ct: c.MOD.t[:, ct * 512:(ct + 1) * 512], c.MOD.b, layer == 0)
        c.P.end_phase()


def make_A(c, ph, gname, layer, modcols, modt):
    nc, k, d = c.nc, c.k, c.d
    g = sb(ph, nc, [128, 1024], F32, name="gbc")
    A = sb(ph, nc, [128, 1024], F32, name="A")
    k.dma("sp", g.t[:], d[gname][layer:layer + 1, :].partition_broadcast(128), w=[g.b])
    k.stt(A.t[:], modt.t[:, modcols:modcols + 1024], 1.0, g.t[:], ALU.add, ALU.mult, r=[modt.b, g.b], w=[A.b])
    return A


def norm_mod(c, xt, A, SH, shb, outs, tmp, j):
    k = c.k
    ss, t1 = tmp["ss"], tmp["t1"]
    k.stt(t1.t[:], xt.t[:], 1.0, xt.t[:], ALU.mult, ALU.mult, r=[xt.b], w=[t1.b], wp=[ss.b], accum=ss.t[:, 4 * j:4 * j + 1])
    k.ts("dve", ss.t[:, 4 * j + 1:4 * j + 2], ss.t[:, 4 * j:4 * j + 1], 1.0 / 1024, ALU.mult, EPS, ALU.add, r=[ss.b], wp=[ss.b])
    k.act(ss.t[:, 4 * j + 2:4 * j + 3], ss.t[:, 4 * j + 1:4 * j + 2], AF.Sqrt, r=[ss.b], wp=[ss.b])
    k.recip(ss.t[:, 4 * j + 3:4 * j + 4], ss.t[:, 4 * j + 2:4 * j + 3], r=[ss.b], wp=[ss.b])
    k.stt(t1.t[:], xt.t[:], ss.t[:, 4 * j + 3:4 * j + 4], A.t[:], ALU.mult, ALU.mult, r=[xt.b, ss.b, A.b], w=[t1.b])
    for (ap, eng, slot) in outs:
        k.tt(eng, ap, t1.t[:], SH, ALU.add, r=[t1.b, shb], wp=[slot.b])


def phase_a1(c):
    nc, P, k, d, cfg = c.nc, c.P, c.k, c.d, c.cfg
    S, L, NT = cfg.S, cfg.L, cfg.NT
    with ExitStack() as ph:
        W = sb(ph, nc, [128, 8, 2560], BF16, name="Wtok")
        wv = d["ab_w_in"].rearrange("(k p) n -> p k n", p=128)
        for i, c0 in enumerate((0, 512, 1024, 3072, 3584)):
            k.dma("pool", W.t[:, :, i * 512:(i + 1) * 512], wv[:, :, c0:c0 + 512], wp=[W.b])
        A = make_A(c, ph, "norm1_g", 0, 1024, c.MOD)
        Ac = make_A(c, ph, "norm1_g", 0, 1024, c.CMOD)
        g64 = sb(ph, nc, [128, 128], F32, name="g64")
        GQ = sb(ph, nc, [128, 512], F32, name="GQ")
        GK = sb(ph, nc, [128, 512], F32, name="GK")
        k.dma("sp", g64.t[:, 0:64], d["nat_q_norm"].partition_broadcast(128), wp=[g64.b])
        k.dma("sp", g64.t[:, 64:128], d["nat_k_norm"].partition_broadcast(128), wp=[g64.b])
        k.ts("dve", GQ.t[:].rearrange("p (h e) -> p h e", h=8), g64.t[:, 0:64].unsqueeze(1).to_broadcast([128, 8, 64]), 0.125, ALU.mult, r=[g64.b], w=[GQ.b])
        k.ts("dve", GK.t[:].rearrange("p (h e) -> p h e", h=8), g64.t[:, 64:128].unsqueeze(1).to_broadcast([128, 8, 64]), 1.0, ALU.mult, r=[g64.b], w=[GK.b])
        COSG = sb(ph, nc, [128, 128], F32, n=2, name="COS")
        SING = sb(ph, nc, [128, 128], F32, n=2, name="SIN")
        XIN = sb(ph, nc, [128, 1024], F32, n=2, name="xin")
        tmps = [{"ss": sb(ph, nc, [128, 16], F32, name="ss"), "t1": sb(ph, nc, [128, 1024], F32, name="t1")} for _ in range(2)]
        XM = sb(ph, nc, [128, 1024], BF16, n=2, name="xm")
        XTG = sb(ph, nc, [128, 8, 512], BF16, n=2, name="xtg")
        pT = ps(ph, nc, [128, 1024], BF16, name="pT")
        pS = ps(ph, nc, [128, 512], F32, n=5, name="pS")
        pO = ps(ph, nc, [128, 1024], BF16, n=2, name="pO")
        SQs = sb(ph, nc, [128, 1024], F32, n=2, name="sq")
        STs = sb(ph, nc, [128, 48], F32, n=2, name="st")
        QNs = sb(ph, nc, [128, 1024], F32, n=2, name="qn")
        R1s = sb(ph, nc, [128, 1024], F32, n=2, name="r1")
        R2s = sb(ph, nc, [128, 1024], F32, n=2, name="r2")
        OB = sb(ph, nc, [128, 1536], BF16, n=2, name="ob")
        OTG = sb(ph, nc, [128, 3, 4, 512], BF16, n=2, name="otg")
        VAs = sb(ph, nc, [128, 8, 65], BF16, n=2, name="vas")
        VHs = sb(ph, nc, [128, 512], BF16, n=2, name="vhs")
        Gs = sb(ph, nc, [128, 512], F32, n=2, name="gs")
        for v in VAs:
            k.memset("pool", v.t[:, :, 64:65], 1.0, wp=[v.b])
        dXT, dXTc = Buf("dXT"), Buf("dXTc")
        dQ, dV, dVH, dG = Buf("dQ"), Buf("dV"), Buf("dVH"), Buf("dG")

        def run(src, ntile, Asl, modt, is_ctx):
            ngrp = (ntile + 3) // 4
            it = 0
            for g in range(ngrp):
                nj = min(4, ntile - 4 * g)
                xtg = XTG[g % 2]
                otg = OTG[g % 2]
                COS, SIN = COSG[g % 2], SING[g % 2]
                if not is_ctx:
                    k.dma("sp", COS.t[:, 0:nj * 32], d["k_cos"][:, g * 128:g * 128 + nj * 32], w=[COS.b])
                    k.dma("sp", SIN.t[:, 0:nj * 32], d["k_sin"][:, g * 128:g * 128 + nj * 32], w=[SIN.b])
                for j in range(nj):
                    t = 4 * g + j
                    xin, xm = XIN[it % 2], XM[it % 2]
                    ob, vas, vhs, gs = OB[it % 2], VAs[it % 2], VHs[it % 2], Gs[it % 2]
                    SQ, ST, QN, R1, R2 = SQs[it % 2], STs[it % 2], QNs[it % 2], R1s[it % 2], R2s[it % 2]
                    it += 1
                    k.dma("sp", xin.t[:], src[t * 128:(t + 1) * 128, :], w=[xin.b])
                    norm_mod(c, xin, Asl, modt.t[:, 0:1024], modt.b, [(xm.t[:], "pool", xm)], tmps[it % 2], j)
                    for kk in range(8):
                        k.tr(pT.t[:, kk * 128:(kk + 1) * 128], xm.t[:, kk * 128:(kk + 1) * 128], c.ident_b.t[:], r=[xm.b],
                             w=[pT.b] if kk == 0 else (), wp=() if kk == 0 else [pT.b])
                    k.cp("act", xtg.t[:, :, j * 128:(j + 1) * 128], pT.t[:].rearrange("p (k t) -> p k t", k=8), r=[pT.b], wp=[xtg.b])
                    cols = (1, 2, 3) if is_ctx else (0, 1, 2, 3, 4)
                    for ci in cols:
                        for kk in range(8):
                            k.mm(pS[ci].t[:], xtg.t[:, kk, j * 128:(j + 1) * 128], W.t[:, kk, ci * 512:(ci + 1) * 512], kk == 0, kk == 7,
                                 r=[xtg.b, W.b], w=[pS[ci].b] if kk == 0 else (), wp=() if kk == 0 else [pS[ci].b])
                    srcs = ((1, 1),) if is_ctx else ((0, 0), (1, 1))
                    for (ci, slot_i) in srcs:
                        k.act(SQ.t[:, slot_i * 512:(slot_i + 1) * 512], pS[ci].t[:], AF.Square, r=[pS[ci].b], wp=[SQ.b])
                        k.red(ST.t[:, slot_i * 8:(slot_i + 1) * 8], SQ.t[:, slot_i * 512:(slot_i + 1) * 512].rearrange("p (h e) -> p h e", h=8), r=[SQ.b], wp=[ST.b])
                    k.ts("dve", ST.t[:, 16:32], ST.t[:, 0:16], 1.0 / 64, ALU.mult, EPS, ALU.add, r=[ST.b], wp=[ST.b])
                    k.act(ST.t[:, 32:48], ST.t[:, 16:32], AF.Sqrt, r=[ST.b], wp=[ST.b])
                    k.recip(ST.t[:, 16:32], ST.t[:, 32:48], r=[ST.b], wp=[ST.b])
                    for (ci, slot_i) in srcs:
                        qn = QN.t[:, slot_i * 512:(slot_i + 1) * 512]
                        k.tt("dve", qn.rearrange("p (h e) -> p h e", h=8), pS[ci].t[:].rearrange("p (h e) -> p h e", h=8),
                             ST.t[:, 16 + slot_i * 8:16 + slot_i * 8 + 8].unsqueeze(2).to_broadcast([128, 8, 64]), ALU.mult,
                             r=[pS[ci].b, ST.b], wp=[QN.b])
                        Gt = GQ if slot_i == 0 else GK
                        k.tt("pool", qn, qn, Gt.t[:], ALU.mult, r=[QN.b, Gt.b], wp=[QN.b])
                    if is_ctx:
                        k.cp("act", ob.t[:, 1024:1536], QN.t[:, 512:1024], r=[QN.b], wp=[ob.b])
                    else:
                        k.cp("act", ob.t[:, 512:1024], QN.t[:, 0:512], r=[QN.b], wp=[ob.b])
                        qv = QN.t[:].rearrange("p (h a b i) -> p h a b i", h=16, a=2, b=2)
                        cosb = COS.t[:, j * 32:(j + 1) * 32].rearrange("p (a i) -> p a i", a=2).unsqueeze(1).to_broadcast([128, 16, 2, 16])
                        sinb = SIN.t[:, j * 32:(j + 1) * 32].rearrange("p (a i) -> p a i", a=2).unsqueeze(1).to_broadcast([128, 16, 2, 16])
                        r1v = R1.t[:].rearrange("p (h a b i) -> p h a b i", h=16, a=2, b=2)
                        r2v = R2.t[:].rearrange("p (h a b i) -> p h a b i", h=16, a=2, b=2)
                        x1, x2 = qv[:, :, :, 0, :], qv[:, :, :, 1, :]
                        k.tt("dve", r1v[:, :, :, 0, :], x1, cosb, ALU.mult, r=[QN.b, COS.b], wp=[R1.b])
                        k.tt("pool", r2v[:, :, :, 0, :], x2, sinb, ALU.mult, r=[QN.b, SIN.b], wp=[R2.b])
                        k.tt("dve", r1v[:, :, :, 1, :], x2, cosb, ALU.mult, r=[QN.b, COS.b], wp=[R1.b])
                        k.tt("pool", r2v[:, :, :, 1, :], x1, sinb, ALU.mult, r=[QN.b, SIN.b], wp=[R2.b])
                        for slot_i, o0 in ((0, 0), (1, 1024)):
                            ov = ob.t[:, o0:o0 + 512].rearrange("p (h a b i) -> p h a b i", h=8, a=2, b=2)
                            a1 = r1v[:, slot_i * 8:(slot_i + 1) * 8]
                            a2 = r2v[:, slot_i * 8:(slot_i + 1) * 8]
                            k.tt("dve", ov[:, :, :, 0, :], a1[:, :, :, 0, :], a2[:, :, :, 0, :], ALU.subtract, r=[R1.b, R2.b], wp=[ob.b])
                            k.tt("dve", ov[:, :, :, 1, :], a1[:, :, :, 1, :], a2[:, :, :, 1, :], ALU.add, r=[R1.b, R2.b], wp=[ob.b])
                    which = (2,) if is_ctx else (0, 1, 2)
                    for wi in which:
                        po = pO[0] if wi < 2 else pO[1]
                        for hp in range(4):
                            col = ((wi % 2) * 4 + hp) * 128
                            first = (hp == 0 and wi in (0, 2))
                            k.tr(po.t[:, col:col + 128], ob.t[:, wi * 512 + hp * 128: wi * 512 + (hp + 1) * 128], c.ident_b.t[:], r=[ob.b],
                                 w=[po.b] if first else (), wp=() if first else [po.b])
                    if not is_ctx:
                        k.cp("act", otg.t[:, 0:2, :, j * 128:(j + 1) * 128], pO[0].t[:].rearrange("p (w h t) -> p w h t", w=2, h=4), r=[pO[0].b], wp=[otg.b])
                    k.cp("dve", otg.t[:, 2, :, j * 128:(j + 1) * 128], pO[1].t[:, 0:512].rearrange("p (h t) -> p h t", h=4), r=[pO[1].b], wp=[otg.b])
                    k.cp("act", vas.t[:, :, 0:64], pS[2].t[:].rearrange("p (h e) -> p h e", h=8), r=[pS[2].b], wp=[vas.b])
                    k.cp("dve", vhs.t[:], pS[3].t[:], r=[pS[3].b], w=[vhs.b])
                    rows = slice(t * 128, (t + 1) * 128)
                    if is_ctx:
                        k.dma("sp", d["VcA"][rows, :], vas.t[:].rearrange("p h e -> p (h e)"), r=[vas.b], wp=[dV])
                        k.dma("sp", d["VHc"][rows, :], vhs.t[:], r=[vhs.b], wp=[dVH])
                    else:
                        k.act(gs.t[:], pS[4].t[:], AF.Silu, r=[pS[4].b], w=[gs.b])
                        k.dma("sp", d["VA"][rows, :], vas.t[:].rearrange("p h e -> p (h e)"), r=[vas.b], wp=[dV])
                        k.dma("sp", d["VH"][rows, :], vhs.t[:], r=[vhs.b], wp=[dVH])
                        k.dma("sp", d["G"][rows, :], gs.t[:], r=[gs.b], wp=[dG])
                tok = slice(g * 512, g * 512 + nj * 128)
                w_ = nj * 128
                if is_ctx:
                    k.dma("sp", d["XTc"][:, :, tok], xtg.t[:, :, 0:w_], r=[xtg.b], wp=[dXTc])
                    k.dma("sp", d["KcT"][:, :, tok], otg.t[:, 2, :, 0:w_], r=[otg.b], wp=[dQ])
                else:
                    k.dma("sp", d["XT"][:, :, tok], xtg.t[:, :, 0:w_], r=[xtg.b], wp=[dXT])
                    k.dma("sp", d["QTr"][:, :, tok], otg.t[:, 0, :, 0:w_], r=[otg.b], wp=[dQ])
                    k.dma("sp", d["QTf"][:, :, tok], otg.t[:, 1, :, 0:w_], r=[otg.b], wp=[dQ])
                    k.dma("sp", d["KTr"][:, :, tok], otg.t[:, 2, :, 0:w_], r=[otg.b], wp=[dQ])

        run(d["ctx"], L // 128, Ac, c.CMOD, True)
        run(d["x"], NT, A, c.MOD, False)
        P.end_phase()


def core_inputs(inp, b, cfg, consts):
    f = lambda a: np.ascontiguousarray(np.asarray(a, dtype=np.float32))
    m = {
        "x": f(inp["x"][b]), "c": f(inp["c"][b]).reshape(8, 128), "ctx": f(inp["ctx"][b]),
        "c_ctx": f(inp["c_ctx"]).reshape(8, 128),
        "ada_w": f(inp["ada_w"]), "ada_b": f(inp["ada_b"]), "norm1_g": f(inp["norm1_g"]), "norm2_g": f(inp["norm2_g"]),
        "ab_w_in": f(inp["ab_w_in"][0]), "ab_w_out": f(inp["ab_w_out"][0]),
        "nat_q_norm": f(inp["nat_q_norm"][0]).reshape(1, 64), "nat_k_norm": f(inp["nat_k_norm"][0]).reshape(1, 64),
        "rpb_full": layout_rpb(f(inp["nat_rpb"][0])),
        "hgrn_lb": f(inp["hgrn_lb"]).reshape(16, 128), "hgrn_o_norm": f(inp["hgrn_o_norm"][0]).reshape(1, 128),
        "conv_w1": f(inp["conv_w1"][0]), "conv_b1": f(inp["conv_b1"][0]).reshape(16, 128), "conv_dw": f(inp["conv_dw"][0]),
        "conv_dw_b": f(inp["conv_dw_b"][0]).reshape(8, 128), "conv_ln_g": f(inp["conv_ln_g"][0]).reshape(8, 128),
        "conv_ln_b": f(inp["conv_ln_b"][0]).reshape(8, 128), "conv_w2": f(inp["conv_w2"][0]), "conv_b2": f(inp["conv_b2"][0]).reshape(1, 1024),
        "router_w": f(inp["router_w"]), "router_b": f(inp["router_b"]),
        "moe_w1": f(inp["moe_w1"]), "moe_b1": f(inp["moe_b1"]).reshape(2, cfg.E * 16, 128),
        "moe_w2": f(inp["moe_w2"]), "moe_b2": f(inp["moe_b2"]),
    }
    for kname, v in consts.items():
        m["k_" + kname] = v
    return m


_cache = {}


def kernel(**inputs):
    B = inputs["x"].shape[0]
    S = inputs["x"].shape[1]
    cfg = Cfg(S=S, L=inputs["ctx"].shape[1], E=inputs["moe_w1"].shape[1])
    key = (cfg.S, cfg.L, cfg.E)
    if key not in _cache:
        _cache[key] = build_program(cfg)[0]
    nc = _cache[key]
    consts = host_consts(cfg)
    in_maps = [core_inputs(inputs, b, cfg, consts) for b in range(B)]
    res = run_bass_kernel_spmd(nc, in_maps, core_ids=list(range(B)))
    return np.stack([np.asarray(r["out"], dtype=np.float32) for r in res.results], axis=0)


def phase_a2(c):
    nc, P, k, d, cfg = c.nc, c.P, c.k, c.d, c.cfg
    S, L = cfg.S, cfg.L
    with ExitStack() as ph:
        W = sb(ph, nc, [128, 8, 1536], BF16, name="Wfm")
        wv = d["ab_w_in"].rearrange("(k p) n -> p k n", p=128)
        for i in range(3):
            k.dma("pool", W.t[:, :, i * 512:(i + 1) * 512], wv[:, :, 1536 + i * 512:1536 + (i + 1) * 512], wp=[W.b])
        RESET = sb(ph, nc, [128, 512], F32, name="reset")
        k.dma("sp", RESET.t[:], d["k_reset"], w=[RESET.b])
        lbr = sb(ph, nc, [16, 128], F32, name="lbr")
        Ee = sb(ph, nc, [128, 16], F32, name="Ee")
        LB = sb(ph, nc, [128, 24], F32, name="LB")
        pL = ps(ph, nc, [128, 16], F32, name="pL")
        k.dma("sp", lbr.t[:], d["hgrn_lb"], w=[lbr.b])
        k.tr(pL.t[:], lbr.t[:], c.ident_f.t[0:16, 0:16], r=[lbr.b], w=[pL.b])
        k.act(Ee.t[:], pL.t[:], AF.Exp, r=[pL.b], w=[Ee.b])
        ev = Ee.t[:].rearrange("p (d j h) -> p d j h", d=2, j=2)
        k.tt("dve", LB.t[:, 16:24].rearrange("p (d h) -> p d h", d=2), ev[:, :, 0, :], ev[:, :, 1, :], ALU.add, r=[Ee.b], wp=[LB.b])
        k.recip(LB.t[:, 16:24], LB.t[:, 16:24], r=[LB.b], wp=[LB.b])
        k.tt("dve", LB.t[:, 0:8].rearrange("p (d h) -> p d h", d=2), ev[:, :, 0, :], LB.t[:, 16:24].rearrange("p (d h) -> p d h", d=2), ALU.mult, r=[Ee.b, LB.b], wp=[LB.b])
        k.ts("dve", LB.t[:, 8:16], LB.t[:, 0:8], -1.0, ALU.mult, 1.0, ALU.add, r=[LB.b], wp=[LB.b])

        XTG = sb(ph, nc, [128, 8, 512], BF16, n=2, name="xtg")
        names = ("q32", "sg", "f", "lf", "kk", "B", "e1", "e2", "t1", "r", "e3")
        TM = {n_: sb(ph, nc, [128, 512], F32, n=2, name=n_) for n_ in names}
        HQs = sb(ph, nc, [128, 2, 4, 512], BF16, n=2, name="hqs")
        HKs = sb(ph, nc, [128, 2, 4, 512], BF16, n=2, name="hks")
        KHT = sb(ph, nc, [128, 512], BF16, n=2, name="kht")
        KHs = sb(ph, nc, [128, 4, 2, 512], BF16, n=2, name="khs")
        DECs = sb(ph, nc, [128, 2, 4, 8], F32, n=2, name="decs")
        pQ = ps(ph, nc, [128, 512], F32, n=2, name="pQ")
        pF = ps(ph, nc, [128, 512], F32, n=3, name="pF")
        pK = ps(ph, nc, [128, 512], BF16, n=2, name="pK")
        dHQ, dHK, dKH, dDEC = Buf("dHQ"), Buf("dHK"), Buf("dKH"), Buf("dDEC")
        QS = 128.0 ** -0.5

        def run(src, ntok, is_ctx):
            ngrp = (ntok + 511) // 512
            it = 0
            for g in range(ngrp):
                n = min(512, ntok - g * 512)
                nch = n // 64
                nsub = n // 128
                xtg, hqs, hks, khs, decs = XTG[g % 2], HQs[g % 2], HKs[g % 2], KHs[g % 2], DECs[g % 2]
                k.dma("sp", xtg.t[:, :, 0:n], src[:, :, g * 512:g * 512 + n], w=[xtg.b])
                for h in range(4):
                    pq = pQ[h % 2]
                    if not is_ctx:
                        for kk_ in range(8):
                            k.mm(pq.t[:, 0:n], W.t[:, kk_, h * 128:(h + 1) * 128], xtg.t[:, kk_, 0:n], kk_ == 0, kk_ == 7,
                                 r=[W.b, xtg.b], w=[pq.b] if kk_ == 0 else (), wp=() if kk_ == 0 else [pq.b])
                        q32 = TM["q32"][h % 2]
                        k.act(q32.t[:, 0:n], pq.t[:, 0:n], AF.Silu, r=[pq.b], w=[q32.b])
                    for dd in range(2):
                        pf = pF[(2 * h + dd) % 3]
                        c0 = 512 + dd * 512 + h * 128
                        for kk_ in range(8):
                            k.mm(pf.t[:, 0:n], W.t[:, kk_, c0:c0 + 128], xtg.t[:, kk_, 0:n], kk_ == 0, kk_ == 7,
                                 r=[W.b, xtg.b], w=[pf.b] if kk_ == 0 else (), wp=() if kk_ == 0 else [pf.b])
                        tm = {n_: TM[n_][it % 2] for n_ in names}
                        kht = KHT[it % 2]
                        pk = pK[it % 2]
                        it += 1
                        sg, f, lf, kk, Bc, e1, e2, t1, rr, e3 = (tm[x] for x in ("sg", "f", "lf", "kk", "B", "e1", "e2", "t1", "r", "e3"))
                        li = dd * 4 + h
                        k.act(sg.t[:, 0:n], pf.t[:, 0:n], AF.Sigmoid, r=[pf.b], w=[sg.b])
                        k.ts("dve", f.t[:, 0:n], sg.t[:, 0:n], LB.t[:, 8 + li:9 + li], ALU.mult, LB.t[:, li:li + 1], ALU.add, r=[sg.b, LB.b], w=[f.b])
                        k.act(lf.t[:, 0:n], f.t[:, 0:n], AF.Ln, r=[f.b], w=[lf.b])
                        k.ts("pool", kk.t[:, 0:n], f.t[:, 0:n], -1.0, ALU.mult, 1.0, ALU.add, r=[f.b], w=[kk.b])
                        P.op("dve", (lambda e, o=Bc.t[:, 0:n], a=RESET.t[:, 0:n], b_=lf.t[:, 0:n]:
                                     e.tensor_tensor_scan(out=o, data0=a, data1=b_, initial=0.0, op0=ALU.mult, op1=ALU.add)),
                             reads=[RESET.b, lf.b], writes=[Bc.b])
                        Bv = Bc.t[:, 0:n].rearrange("p (c t) -> p c t", t=64)
                        Bend = Bv[:, :, 63:64].to_broadcast([128, nch, 64])
                        v3 = lambda s_: s_.t[:, 0:n].rearrange("p (c t) -> p c t", t=64)
                        if dd == 0:
                            k.act(e1.t[:, 0:n], Bc.t[:, 0:n], AF.Exp, r=[Bc.b], w=[e1.b])
                            k.act(e2.t[:, 0:n], Bc.t[:, 0:n], AF.Exp, r=[Bc.b], w=[e2.b], scale=-1.0)
                            k.tt("dve", v3(t1), Bend, Bv, ALU.subtract, r=[Bc.b], w=[t1.b])
                            k.act(e3.t[:, 0:n], t1.t[:, 0:n], AF.Exp, r=[t1.b], w=[e3.b])
                        else:
                            k.tt("dve", t1.t[:, 0:n], lf.t[:, 0:n], Bc.t[:, 0:n], ALU.subtract, r=[lf.b, Bc.b], w=[t1.b])
                            k.tt("dve", v3(rr), v3(t1), Bend, ALU.add, r=[t1.b, Bc.b], w=[rr.b])
                            k.act(e1.t[:, 0:n], rr.t[:, 0:n], AF.Exp, r=[rr.b], w=[e1.b])
                            k.act(e2.t[:, 0:n], rr.t[:, 0:n], AF.Exp, r=[rr.b], w=[e2.b], scale=-1.0)
                            k.act(e3.t[:, 0:n], t1.t[:, 0:n], AF.Exp, r=[t1.b], w=[e3.b], scale=-1.0)
                        k.act(decs.t[:, dd, h, 0:nch], Bv[:, :, 63], AF.Exp, r=[Bc.b], wp=[decs.b])
                        if not is_ctx:
                            q32 = TM["q32"][h % 2]
                            k.stt(hqs.t[:, dd, h, 0:n], q32.t[:, 0:n], QS, e1.t[:, 0:n], ALU.mult, ALU.mult, r=[q32.b, e1.b], wp=[hqs.b])
                            k.tt("pool", hks.t[:, dd, h, 0:n], kk.t[:, 0:n], e2.t[:, 0:n], ALU.mult, r=[kk.b, e2.b], wp=[hks.b])
                        k.tt("pool", kht.t[:, 0:n], kk.t[:, 0:n], e3.t[:, 0:n], ALU.mult, r=[kk.b, e3.b], w=[kht.b])
                        for sub in range(nsub):
                            k.tr(pk.t[:, sub * 128:(sub + 1) * 128], kht.t[:, sub * 128:(sub + 1) * 128], c.ident_b.t[:], r=[kht.b],
                                 w=[pk.b] if sub == 0 else (), wp=() if sub == 0 else [pk.b])
                        k.cp("act", khs.t[:, 0:nsub, dd, h * 128:(h + 1) * 128], pk.t[:, 0:n].rearrange("p (s e) -> p s e", e=128), r=[pk.b], wp=[khs.b])
                tok = slice(g * 512, g * 512 + n)
                for dd in range(2):
                    if is_ctx:
                        k.dma("sp", d["KHc"][dd, tok, :].rearrange("(s p) e -> p s e", p=128), khs.t[:, 0:nsub, dd, :], r=[khs.b], wp=[dKH])
                        k.dma("sp", d["DECc"][dd, :, :, g * 8:g * 8 + nch], decs.t[:, dd, :, 0:nch], r=[decs.b], wp=[dDEC])
                    else:
                        k.dma("sp", d["HQ"][dd, :, :, tok], hqs.t[:, dd, :, 0:n], r=[hqs.b], wp=[dHQ])
                        k.dma("sp", d["HK"][dd, :, :, tok], hks.t[:, dd, :, 0:n], r=[hks.b], wp=[dHK])
                        k.dma("sp", d["KH"][dd, tok, :].rearrange("(s p) e -> p s e", p=128), khs.t[:, 0:nsub, dd, :], r=[khs.b], wp=[dKH])
                        k.dma("sp", d["DEC"][dd, :, :, g * 8:g * 8 + nch], decs.t[:, dd, :, 0:nch], r=[decs.b], wp=[dDEC])

        run(d["XTc"], L, True)
        run(d["XT"], S, False)
        P.end_phase()


def phase_nat(c):
    nc, P, k, d, cfg = c.nc, c.P, c.k, c.d, c.cfg
    S, L, ROWS = cfg.S, cfg.L, cfg.ROWS
    NCC = L // 128
    NCH = 4 + NCC
    with ExitStack() as ph:
        BFf = sb(ph, nc, [128, 7680], F32, name="bff")
        BFb = sb(ph, nc, [128, 8, 960], BF16, name="bfb")
        k.dma("sp", BFf.t[0:64, :], d["rpb_full"], wp=[BFf.b])
        k.dma("sp", BFf.t[64:128, :], d["rpb_full"], wp=[BFf.b])
        k.cp("dve", BFb.t[:].rearrange("p h e -> p (h e)"), BFf.t[:], r=[BFf.b], w=[BFb.b])
        KcT = sb(ph, nc, [128, 4, L], BF16, name="kct")
        VcA = sb(ph, nc, [128, NCC, 520], BF16, name="vca")
        k.dma("sp", KcT.t[:], d["KcT"], w=[KcT.b])
        k.dma("sp", VcA.t[:], d["VcA"].rearrange("(c p) f -> p c f", p=128), w=[VcA.b])
        QR = sb(ph, nc, [128, 4, 512], BF16, n=2, name="qr")
        QF = sb(ph, nc, [128, 4, 512], BF16, n=2, name="qf")
        KW = sb(ph, nc, [128, 4, 512], BF16, n=3, name="kw")
        VW = sb(ph, nc, [128, 4, 520], BF16, n=3, name="vw")
        PT = sb(ph, nc, [128, NCH * 64], BF16, n=3, name="pt")
        NS = sb(ph, nc, [64, 512], BF16, n=2, name="ns")
        RD = sb(ph, nc, [64, 8], F32, n=2, name="rd")
        pS = ps(ph, nc, [128, 512], F32, n=4, name="pS")
        pO = ps(ph, nc, [64, 4, 65], F32, n=4, name="pO")
        dCAT = Buf("dCATn")
        it = 0
        for r in range(ROWS):
            g8, ro = r // 8, (r % 8) * 64
            qr, qf = QR[g8 % 2], QF[g8 % 2]
            if r % 8 == 0:
                k.dma("sp", qr.t[:], d["QTr"][:, :, g8 * 512:(g8 + 1) * 512], w=[qr.b])
                k.dma("sp", qf.t[:], d["QTf"][:, :, g8 * 512:(g8 + 1) * 512], w=[qf.b])
            rs = min(max(r - 4, 0), ROWS - 8)
            dr0 = rs - r + 7
            kw, vw = KW[r % 3], VW[r % 3]
            k.dma("sp", kw.t[:], d["KTr"][:, :, rs * 64:rs * 64 + 512], w=[kw.b])
            k.dma("sp", vw.t[:], d["VA"][rs * 64:rs * 64 + 512, :].rearrange("(c p) f -> p c f", p=128), w=[vw.b])
            ns, rd = NS[r % 2], RD[r % 2]
            po2 = (pO[(2 * r) % 4], pO[(2 * r + 1) % 4])
            def scores(h):
                nonlocal it
                hp, pb = h // 2, (h % 2) * 64
                psx, pt = pS[it % 4], PT[it % 3]
                it += 1
                first = True
                for kc in range(4):
                    k.mm(psx.t[:, kc * 64:(kc + 1) * 64], kw.t[pb:pb + 64, hp, kc * 128:(kc + 1) * 128], qr.t[pb:pb + 64, hp, ro:ro + 64], True, False,
                         r=[kw.b, qr.b], w=[psx.b] if first else (), wp=() if first else [psx.b])
                    first = False
                    k.mm(psx.t[:, kc * 64:(kc + 1) * 64], BFb.t[pb:pb + 64, h, (dr0 + 2 * kc) * 64:(dr0 + 2 * kc) * 64 + 128], c.ident_b.t[pb:pb + 64, pb:pb + 64], False, True,
                         r=[BFb.b], wp=[psx.b])
                for cc in range(NCC):
                    k.mm(psx.t[:, (4 + cc) * 64:(5 + cc) * 64], KcT.t[pb:pb + 64, hp, cc * 128:(cc + 1) * 128], qf.t[pb:pb + 64, hp, ro:ro + 64], True, True,
                         r=[KcT.b, qf.b], wp=[psx.b])
                k.act(pt.t[:], psx.t[:, 0:NCH * 64], AF.Exp, r=[psx.b], w=[pt.b])
                return pt

            def pv(h, pt):
                po = po2[h // 4]
                hh = h % 4
                for ch in range(NCH):
                    rhs = vw.t[:, ch, h * 65:(h + 1) * 65] if ch < 4 else VcA.t[:, ch - 4, h * 65:(h + 1) * 65]
                    k.mm(po.t[:, hh, :], pt.t[:, ch * 64:(ch + 1) * 64], rhs, ch == 0, ch == NCH - 1,
                         r=[pt.b, vw.b, VcA.b], w=[po.b] if (ch == 0 and hh == 0) else (), wp=() if (ch == 0 and hh == 0) else [po.b])

            pts = [scores(0)]
            for h in range(8):
                if h + 1 < 8:
                    pts.append(scores(h + 1))
                pv(h, pts[h])
            for half in range(2):
                po = po2[half]
                k.recip(rd.t[:, half * 4:(half + 1) * 4], po.t[:, :, 64], r=[po.b], wp=[rd.b])
                k.tt("dve", ns.t[:, half * 256:(half + 1) * 256].rearrange("p (h e) -> p h e", h=4), po.t[:, :, 0:64],
                     rd.t[:, half * 4:(half + 1) * 4].unsqueeze(2).to_broadcast([64, 4, 64]), ALU.mult, r=[po.b, rd.b], wp=[ns.b])
            k.dma("sp", d["CAT"][r * 64:(r + 1) * 64, 0:512], ns.t[:], r=[ns.b], wp=[dCAT])
        P.end_phase()


def phase_hgrn(c):
    nc, P, k, d, cfg = c.nc, c.P, c.k, c.d, c.cfg
    S, L = cfg.S, cfg.L
    NG = S // 512
    NCHT = S // 64
    LC = L // 64
    with ExitStack() as ph:
        TRI = sb(ph, nc, [128, 2, 64], F32, name="tri")
        k.dma("sp", TRI.t[:, 0, :], d["k_trif"], wp=[TRI.b])
        k.dma("sp", TRI.t[:, 1, :], d["k_trib"], wp=[TRI.b])
        og = sb(ph, nc, [128, 128], F32, name="og")
        ONG = sb(ph, nc, [128, 512], F32, name="ong")
        k.dma("sp", og.t[:], d["hgrn_o_norm"].partition_broadcast(128), w=[og.b])
        k.ts("dve", ONG.t[:].rearrange("p (h e) -> p h e", h=4), og.t[:].unsqueeze(1).to_broadcast([128, 4, 128]), 1.0, ALU.mult, r=[og.b], w=[ONG.b])
        S32 = sb(ph, nc, [128, 4, 128], F32, name="s32")
        Sbf = sb(ph, nc, [128, 4, 128], BF16, name="sbf")
        DECt = sb(ph, nc, [128, 4, NCHT], F32, name="dect")
        DECc = sb(ph, nc, [128, 4, LC], F32, name="decc")
        KHc = sb(ph, nc, [128, L // 128, 512], BF16, name="khc")
        VHc = sb(ph, nc, [128, L // 128, 512], BF16, name="vhc")
        HQg = sb(ph, nc, [128, 4, 512], BF16, n=2, name="hqg")
        HKg = sb(ph, nc, [128, 4, 512], BF16, n=2, name="hkg")
        KHg = sb(ph, nc, [128, 4, 512], BF16, n=2, name="khg")
        VHg = sb(ph, nc, [128, 4, 512], BF16, n=2, name="vhg")
        SC = [sb(ph, nc, [128, 256], BF16, n=2, name="sc%d" % i) for i in range(2)]
        for i in range(2):
            for s_ in SC[i]:
                k.memset("pool", s_.t[:], 0.0, w=[s_.b])
        OFs = sb(ph, nc, [64, 512], F32, n=2, name="ofs")
        OFc = sb(ph, nc, [64, 512], F32, n=2, name="ofc")
        Gc = sb(ph, nc, [64, 512], F32, n=2, name="gc")
        O32 = sb(ph, nc, [64, 512], F32, n=2, name="o32")
        SQ = sb(ph, nc, [64, 512], F32, n=2, name="sq")
        ST = sb(ph, nc, [64, 16], F32, n=2, name="st")
        Y1 = sb(ph, nc, [64, 512], F32, n=2, name="y1")
        YB = sb(ph, nc, [64, 512], BF16, n=2, name="yb")
        pSs = ps(ph, nc, [128, 512], F32, n=2, name="pSs")
        pSo = ps(ph, nc, [128, 512], F32, n=2, name="pSo")
        pSt = ps(ph, nc, [128, 512], F32, n=2, name="pSt")
        dOF, dCAT = Buf("dOF"), Buf("dCATh")
        k.dma("sp", VHc.t[:], d["VHc"].rearrange("(s p) e -> p s e", p=128), w=[VHc.b])
        it = 0
        for dd in range(2):
            k.memset("dve", S32.t[:], 0.0, w=[S32.b])
            k.dma("sp", DECt.t[:], d["DEC"][dd], w=[DECt.b])
            k.dma("sp", DECc.t[:], d["DECc"][dd], w=[DECc.b])
            k.dma("sp", KHc.t[:], d["KHc"][dd].rearrange("(s p) e -> p s e", p=128), w=[KHc.b])

            def state_update(khs, vhs, tl, pb, dec_ap_fn):
                nonlocal it
                pst = pSt[it % 2]
                for h in range(4):
                    k.mm(pst.t[:, h * 128:(h + 1) * 128], khs.t[pb:pb + 64, tl, h * 128:(h + 1) * 128], vhs.t[pb:pb + 64, tl, h * 128:(h + 1) * 128], True, True,
                         r=[khs.b, vhs.b], w=[pst.b] if h == 0 else (), wp=() if h == 0 else [pst.b])
                for h in range(4):
                    k.stt(S32.t[:, h, :], S32.t[:, h, :], dec_ap_fn(h), pst.t[:, h * 128:(h + 1) * 128], ALU.mult, ALU.add,
                          r=[S32.b, pst.b, DECt.b, DECc.b], wp=[S32.b])
                k.cp("act", Sbf.t[:], S32.t[:], r=[S32.b], w=[Sbf.b])

            chs = range(LC) if dd == 0 else range(LC - 1, -1, -1)
            for ch in chs:
                state_update(KHc, VHc, ch // 2, (ch % 2) * 64, lambda h, ch=ch: DECc.t[:, h, ch:ch + 1])
                it += 1
            groups = list(range(NG)) if dd == 0 else list(range(NG - 1, -1, -1))
            order = []
            for gi, g in enumerate(groups):
                for tl in (range(4) if dd == 0 else range(3, -1, -1)):
                    for cc in ((0, 1) if dd == 0 else (1, 0)):
                        order.append((gi, g, tl, cc))
            loaded = set()

            def ensure(gi, g):
                if gi in loaded:
                    return
                loaded.add(gi)
                hq, hk, kh, vh = HQg[gi % 2], HKg[gi % 2], KHg[gi % 2], VHg[gi % 2]
                tok = slice(g * 512, (g + 1) * 512)
                k.dma("sp", hq.t[:], d["HQ"][dd, :, :, tok], w=[hq.b])
                k.dma("sp", hk.t[:], d["HK"][dd, :, :, tok], w=[hk.b])
                k.dma("sp", kh.t[:], d["KH"][dd, tok, :].rearrange("(s p) e -> p s e", p=128), w=[kh.b])
                k.dma("sp", vh.t[:], d["VH"][tok, :].rearrange("(s p) e -> p s e", p=128), w=[vh.b])

            def scores(n):
                gi, g, tl, cc = order[n]
                ensure(gi, g)
                hq, hk = HQg[gi % 2], HKg[gi % 2]
                pb = cc * 64
                toff, qoff = tl * 128, tl * 128 + cc * 64
                pss = pSs[n % 2]
                sc = SC[cc][(n // 2) % 2]
                for h in range(4):
                    k.mm(pss.t[:, h * 64:(h + 1) * 64], hk.t[:, h, toff:toff + 128], hq.t[:, h, qoff:qoff + 64], True, True,
                         r=[hk.b, hq.b], w=[pss.b] if h == 0 else (), wp=() if h == 0 else [pss.b])
                k.tt("dve", sc.t[pb:pb + 64, :].rearrange("p (h t) -> p h t", h=4), pss.t[pb:pb + 64, 0:256].rearrange("p (h t) -> p h t", h=4),
                     TRI.t[pb:pb + 64, dd, :].unsqueeze(1).to_broadcast([64, 4, 64]), ALU.mult, r=[pss.b, TRI.b], wp=[sc.b])
                return sc

            def rest(n, sc):
                nonlocal it
                gi, g, tl, cc = order[n]
                hq, hk, kh, vh = HQg[gi % 2], HKg[gi % 2], KHg[gi % 2], VHg[gi % 2]
                ch = g * 8 + tl * 2 + cc
                pb = cc * 64
                qoff = tl * 128 + cc * 64
                pso = pSo[n % 2]
                for h in range(4):
                    k.mm(pso.t[0:64, h * 128:(h + 1) * 128], sc.t[:, h * 64:(h + 1) * 64], vh.t[:, tl, h * 128:(h + 1) * 128], True, False,
                         r=[sc.b, vh.b], w=[pso.b] if h == 0 else (), wp=() if h == 0 else [pso.b])
                    k.mm(pso.t[0:64, h * 128:(h + 1) * 128], hq.t[:, h, qoff:qoff + 64], Sbf.t[:, h, :], False, True,
                         r=[hq.b, Sbf.b], wp=[pso.b])
                rows = slice(ch * 64, (ch + 1) * 64)
                if dd == 0:
                    ofs = OFs[n % 2]
                    k.cp("act", ofs.t[:], pso.t[0:64, :], r=[pso.b], w=[ofs.b])
                    k.dma("sp", d["OF"][rows, :], ofs.t[:], r=[ofs.b], wp=[dOF])
                else:
                    ofc, gc, o32, sq, st, y1, yb = (X[n % 2] for X in (OFc, Gc, O32, SQ, ST, Y1, YB))
                    k.dma("sp", ofc.t[:], d["OF"][rows, :], r=[dOF], w=[ofc.b])
                    k.dma("sp", gc.t[:], d["G"][rows, :], w=[gc.b])
                    k.tt("dve", o32.t[:], pso.t[0:64, :], ofc.t[:], ALU.add, r=[pso.b, ofc.b], w=[o32.b])
                    k.act(sq.t[:], o32.t[:], AF.Square, r=[o32.b], w=[sq.b])
                    k.red(st.t[:, 0:4], sq.t[:].rearrange("p (h e) -> p h e", h=4), r=[sq.b], wp=[st.b])
                    k.ts("dve", st.t[:, 4:8], st.t[:, 0:4], 1.0 / 128, ALU.mult, EPS, ALU.add, r=[st.b], wp=[st.b])
                    k.act(st.t[:, 8:12], st.t[:, 4:8], AF.Sqrt, r=[st.b], wp=[st.b])
                    k.recip(st.t[:, 12:16], st.t[:, 8:12], r=[st.b], wp=[st.b])
                    k.tt("dve", y1.t[:].rearrange("p (h e) -> p h e", h=4), o32.t[:].rearrange("p (h e) -> p h e", h=4),
                         st.t[:, 12:16].unsqueeze(2).to_broadcast([64, 4, 128]), ALU.mult, r=[o32.b, st.b], w=[y1.b])
                    k.tt("pool", y1.t[:], y1.t[:], ONG.t[0:64, :], ALU.mult, r=[y1.b, ONG.b], w=[y1.b])
                    k.tt("pool", yb.t[:], y1.t[:], gc.t[:], ALU.mult, r=[y1.b, gc.b], w=[yb.b])
                    k.dma("sp", d["CAT"][rows, 512:1024], yb.t[:], r=[yb.b], wp=[dCAT])
                state_update(kh, vh, tl, pb, lambda h, ch=ch: DECt.t[:, h, ch:ch + 1])
                it += 1

            N = len(order)
            cur = scores(0)
            for n in range(N):
                nxt = scores(n + 1) if n + 1 < N else None
                rest(n, cur)
                cur = nxt
        P.end_phase()


def phase_post(c, layer):
    nc, P, k, d, cfg = c.nc, c.P, c.k, c.d, c.cfg
    S, E, NT = cfg.S, cfg.E, cfg.NT
    NG = S // 512
    hin = d["x"] if layer == 0 else d["H2"]
    hout = d["H1"] if layer == 0 else d["H3"]
    with ExitStack() as ph:
        Wo = sb(ph, nc, [128, 8, 1024], BF16, name="Wo")
        wsrc = d["ab_w_out"] if layer == 0 else d["conv_w2"]
        k.dma("pool", Wo.t[:], wsrc.rearrange("(k p) n -> p k n", p=128), w=[Wo.b])
        RW = sb(ph, nc, [128, 8, E], F32, name="RW")
        k.dma("sp", RW.t[:], d["router_w"][layer].rearrange("(k p) e -> p k e", p=128), w=[RW.b])
        RB = sb(ph, nc, [128, E], F32, name="RB")
        k.dma("sp", RB.t[:], d["router_b"][layer:layer + 1, :].partition_broadcast(128), w=[RB.b])
        B2t = sb(ph, nc, [E, 1024], F32, name="B2t")
        k.dma("sp", B2t.t[:], d["moe_b2"][layer], w=[B2t.b])
        A2 = make_A(c, ph, "norm2_g", layer, 4096, c.MOD)
        XIN = sb(ph, nc, [128, 1024], F32, n=2, name="xin")
        Hs = sb(ph, nc, [128, 1024], F32, n=2, name="hs")
        V1s = sb(ph, nc, [128, 1024], F32, n=2, name="v1")
        tmps = [{"ss": sb(ph, nc, [128, 16], F32, name="ss"), "t1": sb(ph, nc, [128, 1024], F32, name="t1")} for _ in range(2)]
        XFs = sb(ph, nc, [128, 1024], F32, n=2, name="xf")
        XB = sb(ph, nc, [128, 1024], BF16, n=2, name="xb")
        XT2g = sb(ph, nc, [128, 8, 512], BF16, n=2, name="xt2g")
        XFT = sb(ph, nc, [128, 8, 128], F32, name="xft")
        LG = sb(ph, nc, [128, 4 * E + 32], F32, n=2, name="lg")
        CT = sb(ph, nc, [E, 128], F32, name="ct")
        ACs = sb(ph, nc, [128, 1024], F32, name="acs")
        pT = ps(ph, nc, [128, 1024], BF16, name="pT")
        pY = ps(ph, nc, [128, 512], F32, n=2, name="pY")
        pTf = ps(ph, nc, [128, 512], F32, n=2, name="pTf")
        pLg = ps(ph, nc, [128, E], F32, name="pLg")
        pCT = ps(ph, nc, [E, 128], F32, name="pCT")
        dH, dXT2, dCOMB, dACC = Buf("dH"), Buf("dXT2"), Buf("dCOMB"), Buf("dACC")
        if layer == 0:
            CATt = sb(ph, nc, [128, 1024], BF16, n=2, name="catt")
            catT = sb(ph, nc, [128, 8, 128], BF16, n=2, name="catT")
        else:
            YG = sb(ph, nc, [128, 8, 512], F32, name="yg")
            YSQ = sb(ph, nc, [128, 512], F32, n=2, name="ysq")
            Mm = sb(ph, nc, [128, 512], F32, name="mm_")
            MSQ = sb(ph, nc, [128, 512], F32, name="msq")
            RS = sb(ph, nc, [128, 512], F32, name="rs")
            TA = sb(ph, nc, [128, 512], F32, n=2, name="ta")
            TB = sb(ph, nc, [128, 512], F32, n=2, name="tb")
            HN = sb(ph, nc, [128, 8, 512], BF16, name="hn")
            pSum = pTf[0]
            pSq = pTf[1]
            lnr = sb(ph, nc, [16, 128], F32, name="lnr")
            LNP = sb(ph, nc, [128, 16], F32, name="lnp")
            k.dma("sp", lnr.t[0:8, :], d["conv_ln_g"], wp=[lnr.b])
            k.dma("sp", lnr.t[8:16, :], d["conv_ln_b"], wp=[lnr.b])
            k.tr(pLg.t[:, 0:16] if E >= 16 else pTf[0].t[:, 0:16], lnr.t[:], c.ident_f.t[0:16, 0:16], r=[lnr.b], w=[pLg.b if E >= 16 else pTf[0].b])
            k.cp("dve", LNP.t[:], pLg.t[:, 0:16] if E >= 16 else pTf[0].t[:, 0:16], r=[pLg.b if E >= 16 else pTf[0].b], w=[LNP.b])
            b2bc = sb(ph, nc, [128, 1024], F32, name="b2bc")
            B2M = sb(ph, nc, [128, 1024], F32, name="b2m")
            k.dma("sp", b2bc.t[:], d["conv_b2"].partition_broadcast(128), w=[b2bc.b])
            k.tt("dve", B2M.t[:], b2bc.t[:], c.MOD.t[:, 2048:3072], ALU.mult, r=[b2bc.b, c.MOD.b], w=[B2M.b])

        it = 0
        for g in range(NG):
            xt2g = XT2g[g % 2]
            if layer == 1:
                k.dma("sp", YG.t[:], d["YD"][:, :, g * 512:(g + 1) * 512], w=[YG.b])
                for cch in range(8):
                    ysq = YSQ[cch % 2]
                    k.act(ysq.t[:], YG.t[:, cch, :], AF.Square, r=[YG.b], w=[ysq.b])
                    k.mm(pSum.t[:], c.ones_f.t[:], YG.t[:, cch, :], cch == 0, cch == 7, r=[YG.b, c.ones_f.b], w=[pSum.b] if cch == 0 else (), wp=() if cch == 0 else [pSum.b])
                    k.mm(pSq.t[:], c.ones_f.t[:], ysq.t[:], cch == 0, cch == 7, r=[ysq.b, c.ones_f.b], w=[pSq.b] if cch == 0 else (), wp=() if cch == 0 else [pSq.b])
                k.act(Mm.t[:], pSum.t[:], AF.Copy, r=[pSum.b], w=[Mm.b], scale=1.0 / 1024)
                k.tt("pool", MSQ.t[:], Mm.t[:], Mm.t[:], ALU.mult, r=[Mm.b], w=[MSQ.b])
                k.stt(RS.t[:], pSq.t[:], 1.0 / 1024, MSQ.t[:], ALU.mult, ALU.subtract, r=[pSq.b, MSQ.b], w=[RS.b])
                k.ts("dve", RS.t[:], RS.t[:], EPS, ALU.add, r=[RS.b], w=[RS.b])
                k.act(RS.t[:], RS.t[:], AF.Sqrt, r=[RS.b], w=[RS.b])
                k.recip(RS.t[:], RS.t[:], r=[RS.b], w=[RS.b])
                for cch in range(8):
                    ta, tb = TA[cch % 2], TB[cch % 2]
                    k.tt("dve", ta.t[:], YG.t[:, cch, :], Mm.t[:], ALU.subtract, r=[YG.b, Mm.b], w=[ta.b])
                    k.tt("pool", tb.t[:], ta.t[:], RS.t[:], ALU.mult, r=[ta.b, RS.b], w=[tb.b])
                    k.act(HN.t[:, cch, :], tb.t[:], AF.Silu, r=[tb.b, LNP.b], wp=[HN.b], scale=LNP.t[:, cch:cch + 1], bias=LNP.t[:, 8 + cch:9 + cch])
            for j in range(4):
                t = g * 4 + j
                rows = slice(t * 128, (t + 1) * 128)
                xin, hs, xb = XIN[it % 2], Hs[it % 2], XB[it % 2]
                lg = LG[it % 2]
                V1, XF = V1s[it % 2], XFs[it % 2]
                k.dma("sp", xin.t[:], hin[rows, :], w=[xin.b])
                if layer == 0:
                    cat, ctT = CATt[it % 2], catT[it % 2]
                    k.dma("sp", cat.t[:], d["CAT"][rows, :], w=[cat.b])
                    for kk in range(8):
                        k.tr(pT.t[:, kk * 128:(kk + 1) * 128], cat.t[:, kk * 128:(kk + 1) * 128], c.ident_b.t[:], r=[cat.b],
                             w=[pT.b] if kk == 0 else (), wp=() if kk == 0 else [pT.b])
                    k.cp("act", ctT.t[:].rearrange("p k t -> p (k t)"), pT.t[:], r=[pT.b], w=[ctT.b])
                    lhs = lambda kk: ctT.t[:, kk, :]
                    lb_ = ctT.b
                else:
                    lhs = lambda kk: HN.t[:, kk, j * 128:(j + 1) * 128]
                    lb_ = HN.b
                it += 1
                for half in range(2):
                    for kk in range(8):
                        k.mm(pY[half].t[:], lhs(kk), Wo.t[:, kk, half * 512:(half + 1) * 512], kk == 0, kk == 7,
                             r=[lb_, Wo.b], w=[pY[half].b] if kk == 0 else (), wp=() if kk == 0 else [pY[half].b])
                    k.tt("dve", V1.t[:, half * 512:(half + 1) * 512], pY[half].t[:], c.MOD.t[:, 2048 + half * 512:2048 + (half + 1) * 512], ALU.mult,
                         r=[pY[half].b, c.MOD.b], wp=[V1.b])
                if layer == 1:
                    k.tt("pool", xin.t[:], xin.t[:], B2M.t[:], ALU.add, r=[xin.b, B2M.b], w=[xin.b])
                k.tt("pool", hs.t[:], V1.t[:], xin.t[:], ALU.add, r=[V1.b, xin.b], w=[hs.b])
                k.dma("sp", hout[rows, :], hs.t[:], r=[hs.b], wp=[dH])
                norm_mod(c, hs, A2, c.MOD.t[:, 3072:4096], c.MOD.b, [(XF.t[:], "dve", XF), (xb.t[:], "pool", xb)], tmps[it % 2], j)
                for kk in range(8):
                    k.tr(pT.t[:, kk * 128:(kk + 1) * 128], xb.t[:, kk * 128:(kk + 1) * 128], c.ident_b.t[:], r=[xb.b],
                         w=[pT.b] if kk == 0 else (), wp=() if kk == 0 else [pT.b])
                k.cp("act", xt2g.t[:, :, j * 128:(j + 1) * 128], pT.t[:].rearrange("p (k t) -> p k t", k=8), r=[pT.b], wp=[xt2g.b])
                for kk in range(8):
                    pf = pTf[kk // 4]
                    k.tr(pf.t[:, (kk % 4) * 128:(kk % 4 + 1) * 128], XF.t[:, kk * 128:(kk + 1) * 128], c.ident_f.t[:], r=[XF.b],
                         w=[pf.b] if kk % 4 == 0 else (), wp=() if kk % 4 == 0 else [pf.b])
                for hf in range(2):
                    k.cp("act" if hf else "dve", XFT.t[:, hf * 4:(hf + 1) * 4, :], pTf[hf].t[:].rearrange("p (k t) -> p k t", k=4), r=[pTf[hf].b], wp=[XFT.b])
                for kk in range(8):
                    k.mm(pLg.t[:], XFT.t[:, kk, :], RW.t[:, kk, :], kk == 0, kk == 7, r=[XFT.b, RW.b], w=[pLg.b] if kk == 0 else (), wp=() if kk == 0 else [pLg.b])
                L0, MK, EX, EXM, MS = (lg.t[:, 0:E], lg.t[:, E:2 * E], lg.t[:, 2 * E:3 * E], lg.t[:, 3 * E:4 * E], lg.t[:, 4 * E:4 * E + 32])
                k.tt("dve", L0, pLg.t[:], RB.t[:], ALU.add, r=[pLg.b, RB.b], wp=[lg.b])
                P.op("dve", (lambda e, o=MS[:, 0:8], i_=L0: e.max(out=o, in_=i_)), reads=[lg.b], wpart=[lg.b])
                k.ts("dve", MK, L0, MS[:, 3:4], ALU.is_ge, r=[lg.b], wp=[lg.b])
                k.ts("dve", MS[:, 8:9], MS[:, 0:1], -1.0, ALU.mult, r=[lg.b], wp=[lg.b])
                k.act(EX, L0, AF.Exp, r=[lg.b], wp=[lg.b], bias=MS[:, 8:9])
                k.stt(EXM, EX, 1.0, MK, ALU.mult, ALU.mult, r=[lg.b], wp=[lg.b], accum=MS[:, 9:10])
                k.recip(MS[:, 10:11], MS[:, 9:10], r=[lg.b], wp=[lg.b])
                k.ts("dve", EXM, EXM, MS[:, 10:11], ALU.mult, r=[lg.b], wp=[lg.b])
                k.dma("sp", d["COMB"][rows, :], EXM, r=[lg.b], wp=[dCOMB])
                k.dma("sp", d["MK"][rows, :], MK, r=[lg.b], wp=[dCOMB])
                k.dma("sp", d["XM2"][rows, :], xb.t[:], r=[xb.b], wp=[dXT2])
                k.tr(pCT.t[:], EXM, c.ident_f.t[:], r=[lg.b], w=[pCT.b])
                k.cp("act", CT.t[:], pCT.t[:], r=[pCT.b], w=[CT.b])
                for half in range(2):
                    k.mm(pY[half].t[:], CT.t[:], B2t.t[:, half * 512:(half + 1) * 512], True, True, r=[CT.b, B2t.b], w=[pY[half].b])
                    k.cp("act" if half else "dve", ACs.t[:, half * 512:(half + 1) * 512], pY[half].t[:], r=[pY[half].b], wp=[ACs.b])
                k.dma("sp", d["ACCd"][rows, :], ACs.t[:], r=[ACs.b], wp=[dACC])
            k.dma("sp", d["XT2"][:, :, g * 512:(g + 1) * 512], xt2g.t[:], r=[xt2g.b], wp=[dXT2])
        P.end_phase()


def phase_moe(c, layer, MOD5):
    nc, P, k, d, cfg = c.nc, c.P, c.k, c.d, c.cfg
    S, E, MB = cfg.S, cfg.E, cfg.MB
    NTB = MB // 128
    NH = MB // 512
    hin = d["H1"] if layer == 0 else d["H3"]
    hout = d["H2"] if layer == 0 else d["out"]
    with ExitStack() as ph:
        nb1 = (E * 16) // 128
        B1T = sb(ph, nc, [128, E * 16], F32, name="b1t")
        b1r = sb(ph, nc, [128, 128], F32, n=2, name="b1r")
        ACC = sb(ph, nc, [128, NTB, 1024], F32, name="acc")
        XT2b = sb(ph, nc, [128, 8, MB], BF16, name="xt2b")
        CMB = sb(ph, nc, [128, NTB, E], F32, name="cmb")
        W1 = sb(ph, nc, [128, 8, 2048], BF16, n=2, name="w1")
        W2 = sb(ph, nc, [128, 8, 1024], BF16, name="w2")
        ACTT = sb(ph, nc, [128, 8, 512], BF16, n=2, name="actt")
        G1 = sb(ph, nc, [128, 512], F32, n=2, name="g1")
        S1 = sb(ph, nc, [128, 512], F32, n=2, name="s1")
        L1 = sb(ph, nc, [128, 512], F32, n=2, name="l1")
        L2 = sb(ph, nc, [128, 512], F32, n=2, name="l2")
        GS = sb(ph, nc, [128, 512], F32, n=2, name="gs")
        HT = sb(ph, nc, [128, 1024], F32, n=2, name="ht")
        pG = ps(ph, nc, [128, 512], F32, n=2, name="pG")
        pL = ps(ph, nc, [128, 512], F32, n=2, name="pL")
        pO = ps(ph, nc, [128, 512], F32, n=4, name="pO")
        dOUT = Buf("dOUT")
        for i in range(nb1):
            br = b1r[i % 2]
            k.dma("sp", br.t[:], d["moe_b1"][layer, i * 128:(i + 1) * 128, :], w=[br.b])
            k.tr(pG[i % 2].t[:, 0:128], br.t[:], c.ident_f.t[:], r=[br.b], w=[pG[i % 2].b])
            k.cp("dve", B1T.t[:, i * 128:(i + 1) * 128], pG[i % 2].t[:, 0:128], r=[pG[i % 2].b], wp=[B1T.b])
        wi = 0
        io = 0
        for blk in range(S // MB):
            t0 = blk * NTB
            rows = slice(blk * MB, (blk + 1) * MB)
            k.dma("sp", ACC.t[:], d["ACCd"][rows, :].rearrange("(t p) n -> p t n", p=128), w=[ACC.b])
            k.dma("sp", XT2b.t[:], d["XT2"][:, :, rows], w=[XT2b.b])
            k.dma("sp", CMB.t[:], d["COMB"][rows, :].rearrange("(t p) e -> p t e", p=128), w=[CMB.b])
            for e in range(E):
                w1 = W1[wi % 2]
                wi += 1
                w1v = d["moe_w1"][layer, e].rearrange("(k p) n -> p k n", p=128)
                k.dma("pool", w1.t[:, 0:4, :], w1v[:, 0:4, :], wp=[w1.b])
                k.dma("pool", w1.t[:, 4:8, :], w1v[:, 4:8, :], wp=[w1.b])
                w2_loaded = False
                for ht in range(NH):
                    actt = ACTT[io % 2]
                    for pr in range(8):
                        pg, pl = pG[pr % 2], pL[pr % 2]
                        g1, s1, l1, l2, gs = (X[pr % 2] for X in (G1, S1, L1, L2, GS))
                        for kk in range(8):
                            k.mm(pg.t[:], w1.t[:, kk, pr * 128:(pr + 1) * 128], XT2b.t[:, kk, ht * 512:(ht + 1) * 512], kk == 0, kk == 7,
                                 r=[w1.b, XT2b.b], w=[pg.b] if kk == 0 else (), wp=() if kk == 0 else [pg.b])
                        for kk in range(8):
                            k.mm(pl.t[:], w1.t[:, kk, 1024 + pr * 128:1024 + (pr + 1) * 128], XT2b.t[:, kk, ht * 512:(ht + 1) * 512], kk == 0, kk == 7,
                                 r=[w1.b, XT2b.b], w=[pl.b] if kk == 0 else (), wp=() if kk == 0 else [pl.b])
                        bg = B1T.t[:, e * 16 + pr:e * 16 + pr + 1]
                        bl = B1T.t[:, e * 16 + 8 + pr:e * 16 + 8 + pr + 1]
                        k.ts("dve", g1.t[:], pg.t[:], bg, ALU.add, 7.0, ALU.min, r=[pg.b, B1T.b], w=[g1.b])
                        k.act(s1.t[:], g1.t[:], AF.Sigmoid, r=[g1.b], w=[s1.b], scale=1.702)
                        k.act(l1.t[:], pl.t[:], AF.Identity, r=[pl.b, B1T.b], w=[l1.b], bias=bl)
                        k.ts("dve", l2.t[:], l1.t[:], 7.0, ALU.min, -7.0, ALU.max, r=[l1.b], w=[l2.b])
                        k.tt("pool", gs.t[:], g1.t[:], s1.t[:], ALU.mult, r=[g1.b, s1.b], w=[gs.b])
                        k.stt(actt.t[:, pr, :], l2.t[:], 1.0, gs.t[:], ALU.add, ALU.mult, r=[l2.b, gs.b], wp=[actt.b])
                    if not w2_loaded:
                        k.dma("pool", W2.t[:], d["moe_w2"][layer, e].rearrange("(k p) n -> p k n", p=128), w=[W2.b])
                        w2_loaded = True
                    for sub in range(4):
                        tl = ht * 4 + sub
                        for half in range(2):
                            po = pO[io % 4]
                            io += 1
                            for jj in range(8):
                                k.mm(po.t[:], actt.t[:, jj, sub * 128:(sub + 1) * 128], W2.t[:, jj, half * 512:(half + 1) * 512], jj == 0, jj == 7,
                                     r=[actt.b, W2.b], w=[po.b] if jj == 0 else (), wp=() if jj == 0 else [po.b])
                            acc = ACC.t[:, tl, half * 512:(half + 1) * 512]
                            k.stt(acc, po.t[:], CMB.t[:, tl, e:e + 1], acc, ALU.mult, ALU.add, r=[po.b, CMB.b, ACC.b], wp=[ACC.b])
            for tl in range(NTB):
                t = t0 + tl
                ht_ = HT[tl % 2]
                k.dma("sp", ht_.t[:], hin[t * 128:(t + 1) * 128, :], w=[ht_.b])
                k.tt("dve", ACC.t[:, tl, :], ACC.t[:, tl, :], MOD5.t[:], ALU.mult, r=[ACC.b, MOD5.b], wp=[ACC.b])
                k.tt("pool", ht_.t[:], ht_.t[:], ACC.t[:, tl, :], ALU.add, r=[ht_.b, ACC.b], w=[ht_.b])
                k.dma("sp", hout[t * 128:(t + 1) * 128, :], ht_.t[:], r=[ht_.b], wp=[dOUT])
        P.end_phase()


def phase_f(c):
    nc, P, k, d, cfg = c.nc, c.P, c.k, c.d, c.cfg
    S = cfg.S
    NG = S // 512
    with ExitStack() as ph:
        W = sb(ph, nc, [128, 8, 2048], BF16, name="Wc1")
        wv = d["conv_w1"].rearrange("(k p) n -> p k n", p=128)
        k.dma("pool", W.t[:, 0:4, :], wv[:, 0:4, :], wp=[W.b])
        k.dma("pool", W.t[:, 4:8, :], wv[:, 4:8, :], wp=[W.b])
        A = make_A(c, ph, "norm1_g", 1, 1024, c.MOD)
        b1r = sb(ph, nc, [16, 128], F32, name="b1r")
        CB1 = sb(ph, nc, [128, 16], F32, name="cb1")
        pB = ps(ph, nc, [128, 16], F32, name="pB")
        k.dma("sp", b1r.t[:], d["conv_b1"], w=[b1r.b])
        k.tr(pB.t[:], b1r.t[:], c.ident_f.t[0:16, 0:16], r=[b1r.b], w=[pB.b])
        k.cp("dve", CB1.t[:], pB.t[:], r=[pB.b], w=[CB1.b])
        Z = sb(ph, nc, [128, 8, 16], BF16, name="z")
        k.memset("dve", Z.t[:], 0.0, w=[Z.b])
        dGT = Buf("dGT")
        k.dma("sp", d["GT"][:, :, 0:16], Z.t[:], r=[Z.b], wp=[dGT])
        k.dma("sp", d["GT"][:, :, S + 16:S + 32], Z.t[:], r=[Z.b], wp=[dGT])
        XIN = sb(ph, nc, [128, 1024], F32, n=2, name="xin")
        tmps = [{"ss": sb(ph, nc, [128, 16], F32, name="ss"), "t1": sb(ph, nc, [128, 1024], F32, name="t1")} for _ in range(2)]
        XM = sb(ph, nc, [128, 1024], BF16, n=2, name="xm")
        XTG = sb(ph, nc, [128, 8, 512], BF16, n=2, name="xtg")
        SG = sb(ph, nc, [128, 512], F32, n=2, name="sg")
        GTs = sb(ph, nc, [128, 8, 512], BF16, n=2, name="gts")
        pT = ps(ph, nc, [128, 1024], BF16, name="pT")
        pA = ps(ph, nc, [128, 512], F32, n=2, name="pA")
        pGt = ps(ph, nc, [128, 512], F32, n=2, name="pGt")
        it = 0
        for g in range(NG):
            xtg, gts = XTG[g % 2], GTs[g % 2]
            for j in range(4):
                t = 4 * g + j
                xin, xm = XIN[it % 2], XM[it % 2]
                it += 1
                k.dma("sp", xin.t[:], d["H2"][t * 128:(t + 1) * 128, :], w=[xin.b])
                norm_mod(c, xin, A, c.MOD.t[:, 0:1024], c.MOD.b, [(xm.t[:], "pool", xm)], tmps[it % 2], j)
                for kk in range(8):
                    k.tr(pT.t[:, kk * 128:(kk + 1) * 128], xm.t[:, kk * 128:(kk + 1) * 128], c.ident_b.t[:], r=[xm.b],
                         w=[pT.b] if kk == 0 else (), wp=() if kk == 0 else [pT.b])
                k.cp("act", xtg.t[:, :, j * 128:(j + 1) * 128], pT.t[:].rearrange("p (k t) -> p k t", k=8), r=[pT.b], wp=[xtg.b])
            for cp_ in range(8):
                pa, pg, sg = pA[cp_ % 2], pGt[cp_ % 2], SG[cp_ % 2]
                for kk in range(8):
                    k.mm(pa.t[:], W.t[:, kk, cp_ * 128:(cp_ + 1) * 128], xtg.t[:, kk, :], kk == 0, kk == 7, r=[W.b, xtg.b],
                         w=[pa.b] if kk == 0 else (), wp=() if kk == 0 else [pa.b])
                for kk in range(8):
                    k.mm(pg.t[:], W.t[:, kk, 1024 + cp_ * 128:1024 + (cp_ + 1) * 128], xtg.t[:, kk, :], kk == 0, kk == 7, r=[W.b, xtg.b],
                         w=[pg.b] if kk == 0 else (), wp=() if kk == 0 else [pg.b])
                k.act(sg.t[:], pg.t[:], AF.Sigmoid, r=[pg.b, CB1.b], w=[sg.b], bias=CB1.t[:, 8 + cp_:9 + cp_])
                k.stt(gts.t[:, cp_, :], pa.t[:], CB1.t[:, cp_:cp_ + 1], sg.t[:], ALU.add, ALU.mult, r=[pa.b, sg.b, CB1.b], wp=[gts.b])
            k.dma("sp", d["GT"][:, :, 16 + g * 512:16 + (g + 1) * 512], gts.t[:], r=[gts.b], wp=[dGT])
        P.end_phase()


def phase_g1(c):
    nc, P, k, d, cfg = c.nc, c.P, c.k, c.d, c.cfg
    S = cfg.S
    NG = S // 512
    with ExitStack() as ph:
        dwr = sb(ph, nc, [32, 1024], F32, name="dwr")
        DWT = sb(ph, nc, [128, 8, 31], F32, name="dwt")
        dbr = sb(ph, nc, [8, 128], F32, name="dbr")
        DWB = sb(ph, nc, [128, 8], F32, name="dwb")
        DG = sb(ph, nc, [128, 8, 31, 128], BF16, name="dg")
        pD = ps(ph, nc, [128, 8, 32], F32, name="pD")
        pB = ps(ph, nc, [128, 8], F32, name="pB")
        k.dma("sp", dwr.t[0:31, :], d["conv_dw"], w=[dwr.b])
        for cch in range(8):
            k.tr(pD.t[:, cch, 0:31], dwr.t[0:31, cch * 128:(cch + 1) * 128], c.ident_f.t[0:31, 0:31], r=[dwr.b],
                 w=[pD.b] if cch == 0 else (), wp=() if cch == 0 else [pD.b])
        k.cp("dve", DWT.t[:], pD.t[:, :, 0:31], r=[pD.b], w=[DWT.b])
        k.dma("sp", dbr.t[:], d["conv_dw_b"], w=[dbr.b])
        k.tr(pB.t[:], dbr.t[:], c.ident_f.t[0:8, 0:8], r=[dbr.b], w=[pB.b])
        k.cp("dve", DWB.t[:], pB.t[:], r=[pB.b], w=[DWB.b])
        n_ = 0
        for cch in range(8):
            for j in range(31):
                k.ts("dve" if n_ % 2 else "pool", DG.t[:, cch, j, :], c.ident_f.t[:], DWT.t[:, cch, j:j + 1], ALU.mult, r=[DWT.b, c.ident_f.b], wp=[DG.b])
                n_ += 1
        GTw = sb(ph, nc, [128, 8, 544], BF16, n=2, name="gtw")
        Ys = sb(ph, nc, [128, 8, 512], F32, n=2, name="ys")
        pC = ps(ph, nc, [128, 512], F32, n=4, name="pC")
        dYD = Buf("dYD")
        for g in range(NG):
            gtw, ys = GTw[g % 2], Ys[g % 2]
            k.dma("sp", gtw.t[:], d["GT"][:, :, g * 512:g * 512 + 544], w=[gtw.b])
            for cch in range(8):
                pc = pC[cch % 4]
                for j in range(31):
                    k.mm(pc.t[:], DG.t[:, cch, j, :], gtw.t[:, cch, j + 1:j + 513], j == 0, j == 30, r=[DG.b, gtw.b],
                         w=[pc.b] if j == 0 else (), wp=() if j == 0 else [pc.b])
                k.act(ys.t[:, cch, :], pc.t[:], AF.Identity, r=[pc.b, DWB.b], wp=[ys.b], bias=DWB.t[:, cch:cch + 1])
            k.dma("sp", d["YD"][:, :, g * 512:(g + 1) * 512], ys.t[:], r=[ys.b], wp=[dYD])
        P.end_phase()


I32 = mybir.dt.int32


def pool_dma_op(P, fn, reads=(), writes=(), wpart=(), key=None):
    o = Op("pool", fn, P.phase)
    o.is_dma = True
    if key is None:
        key = (list(writes) + list(wpart))[0]
    if key not in P.keymap:
        P.keymap[key] = len(P.keymap)
        assert len(P.keymap) <= NDSEM
    o.key = P.keymap[key]
    P._deps(o, reads, writes, wpart)
    P.ops["pool"].append(o)
    P.order.append(o)
    return o


def phase_route(c, layer, TEi):
    nc, P, k, d, cfg = c.nc, c.P, c.k, c.d, c.cfg
    S, E, NT, NTILE, NSLOT = cfg.S, cfg.E, cfg.NT, cfg.NTILE, cfg.NSLOT
    with ExitStack() as ph:
        MKf = sb(ph, nc, [128, NT, E], F32, name="mkf")
        MKb = sb(ph, nc, [128, NT, E], BF16, name="mkb")
        CMB = sb(ph, nc, [128, NT, E], F32, name="cmb")
        UTf = sb(ph, nc, [128, 128], F32, name="utf")
        UT = sb(ph, nc, [128, 128], BF16, name="ut")
        ONb = sb(ph, nc, [128, 128], BF16, name="onb")
        IOTA = sb(ph, nc, [128, 1], F32, name="iota")
        TH = sb(ph, nc, [1, E * 16], F32, name="th")
        J5 = sb(ph, nc, [1, NTILE * E], F32, name="j5")
        dSLOT, dInit = Buf("dSLOT"), Buf("dInit")
        k.dma("sp", d["SLOT"], d["k_slotinit"], w=[dSLOT, dInit])
        k.dma("sp", MKf.t[:], d["MK"].rearrange("(t p) e -> p t e", p=128), w=[MKf.b])
        k.dma("sp", CMB.t[:], d["COMB"].rearrange("(t p) e -> p t e", p=128), w=[CMB.b])
        k.dma("sp", UTf.t[:], d["k_ut"], w=[UTf.b])
        k.dma("sp", IOTA.t[:], d["k_iota"], w=[IOTA.b])
        k.dma("sp", TH.t[:], d["k_th"], w=[TH.b])
        k.dma("sp", J5.t[:], d["k_j512"], w=[J5.b])
        k.cp("dve", UT.t[:], UTf.t[:], r=[UTf.b], w=[UT.b])
        k.cp("pool", MKb.t[:], MKf.t[:], r=[MKf.b], w=[MKb.b])
        k.memset("dve", ONb.t[:], 1.0, w=[ONb.b])
        pC = ps(ph, nc, [1, E], F32, name="pC")
        pSB = ps(ph, nc, [128, E], F32, name="pSB")
        pR = ps(ph, nc, [128, E], F32, n=2, name="pR")
        V = sb(ph, nc, [1, 8 * E], F32, name="v")
        C16 = sb(ph, nc, [1, E * 16], F32, name="c16")
        CJ = sb(ph, nc, [1, NTILE * E], F32, name="cj")
        TEf = sb(ph, nc, [1, NTILE], F32, name="tef")
        SEGB = sb(ph, nc, [128, E], F32, name="segb")
        for t in range(NT):
            k.mm(pC.t[:], ONb.t[:, 0:1], MKb.t[:, t, :], t == 0, t == NT - 1, r=[ONb.b, MKb.b], w=[pC.b] if t == 0 else (), wp=() if t == 0 else [pC.b])
        cnt, ntl, c512, inc, segs, one = (V.t[:, i * E:(i + 1) * E] for i in range(6))
        k.cp("dve", cnt, pC.t[:], r=[pC.b], wp=[V.b])
        k.tt("dve", C16.t[:].rearrange("o (e m) -> o e m", m=16), cnt.unsqueeze(2).to_broadcast([1, E, 16]), TH.t[:].rearrange("o (e m) -> o e m", m=16), ALU.is_gt,
             r=[V.b, TH.b], w=[C16.b])
        k.red(ntl, C16.t[:].rearrange("o (e m) -> o e m", m=16), r=[C16.b], wp=[V.b])
        k.ts("dve", c512, ntl, 512.0, ALU.mult, r=[V.b], wp=[V.b])
        k.memset("dve", one, 1.0, wp=[V.b])
        P.op("dve", (lambda e_, o=inc, a=one, b_=c512: e_.tensor_tensor_scan(out=o, data0=a, data1=b_, initial=0.0, op0=ALU.mult, op1=ALU.add)),
             reads=[V.b], wpart=[V.b])
        k.tt("dve", segs, inc, c512, ALU.subtract, r=[V.b], wp=[V.b])
        k.tt("dve", CJ.t[:].rearrange("o (j e) -> o j e", e=E), segs.unsqueeze(1).to_broadcast([1, NTILE, E]), J5.t[:].rearrange("o (j e) -> o j e", e=E), ALU.is_le,
             r=[V.b, J5.b], w=[CJ.b])
        k.red(TEf.t[:], CJ.t[:].rearrange("o (j e) -> o j e", e=E), r=[CJ.b], w=[TEf.b])
        k.ts("dve", TEf.t[:], TEf.t[:], -1.0, ALU.add, 0.0, ALU.max, r=[TEf.b], w=[TEf.b])
        IDXW, IDXB = TEi
        KP = sb(ph, nc, [128, 9], F32, name="kp")
        k.dma("sp", KP.t[:], d["k_kp"], w=[KP.b])
        pTE = ps(ph, nc, [128, NTILE], F32, name="pTE")
        TEb = sb(ph, nc, [128, NTILE], F32, name="teb")
        XW = sb(ph, nc, [128, NTILE, 8], F32, name="xw")
        k.mm(pTE.t[:], c.ones_f.t[0:1, :], TEf.t[:], True, True, r=[TEf.b, c.ones_f.b], w=[pTE.b])
        k.cp("dve", TEb.t[:], pTE.t[:], r=[pTE.b], w=[TEb.b])
        for kk in range(8):
            k.ts("dve", XW.t[:, :, kk], TEb.t[:], 1024.0, ALU.mult, KP.t[:, kk:kk + 1], ALU.add, r=[TEb.b, KP.b], wp=[XW.b])
        if layer:
            k.ts("dve", XW.t[:], XW.t[:], float(layer * E * 1024), ALU.add, r=[XW.b], w=[XW.b])
        k.cp("dve", IDXW.t[:], XW.t[:], r=[XW.b], w=[IDXW.b])
        k.ts("dve", TEb.t[:], TEb.t[:], 16.0, ALU.mult, KP.t[:, 8:9], ALU.add, r=[TEb.b, KP.b], w=[TEb.b])
        if layer:
            k.ts("dve", TEb.t[:], TEb.t[:], float(layer * E * 16), ALU.add, r=[TEb.b], w=[TEb.b])
        k.cp("dve", IDXB.t[:], TEb.t[:], r=[TEb.b], w=[IDXB.b])
        k.mm(pSB.t[:], c.ones_f.t[0:1, :], segs, True, True, r=[V.b, c.ones_f.b], w=[pSB.b])
        k.cp("dve", SEGB.t[:], pSB.t[:], r=[pSB.b], w=[SEGB.b])
        POS = sb(ph, nc, [128, E], F32, n=2, name="pos")
        T8 = sb(ph, nc, [128, 8], F32, n=2, name="t8")
        OH = sb(ph, nc, [128, E], F32, n=2, name="oh")
        JK = sb(ph, nc, [128, E], F32, n=2, name="jk")
        P4 = sb(ph, nc, [128, 4], F32, n=2, name="p4")
        P4i = sb(ph, nc, [128, 4], I32, n=2, name="p4i")
        SR = sb(ph, nc, [128, 4, 2], F32, n=2, name="sr")
        for i in range(NT):
            pr = pR[i % 2]
            pos, t8, p4, p4i, sr = POS[i % 2], T8[i % 2], P4[i % 2], P4i[i % 2], SR[i % 2]
            for ip in range(i):
                k.mm(pr.t[:], ONb.t[:], MKb.t[:, ip, :], ip == 0, False, r=[ONb.b, MKb.b], w=[pr.b] if ip == 0 else (), wp=() if ip == 0 else [pr.b])
            k.mm(pr.t[:], UT.t[:], MKb.t[:, i, :], i == 0, True, r=[UT.b, MKb.b], w=[pr.b] if i == 0 else (), wp=() if i == 0 else [pr.b])
            k.tt("dve", pos.t[:], pr.t[:], SEGB.t[:], ALU.add, r=[pr.b, SEGB.b], w=[pos.b])
            P.op("dve", (lambda e_, o=t8.t[:], a=CMB.t[:, i, :]: e_.max(out=o, in_=a)), reads=[CMB.b], writes=[t8.b])
            for kq in range(4):
                oh, jk = OH[kq % 2], JK[kq % 2]
                k.ts("dve", oh.t[:], CMB.t[:, i, :], t8.t[:, kq:kq + 1], ALU.is_equal, r=[CMB.b, t8.b], w=[oh.b])
                k.stt(jk.t[:], oh.t[:], 1.0, pos.t[:], ALU.mult, ALU.mult, r=[oh.b, pos.b], w=[jk.b], wp=[p4.b], accum=p4.t[:, kq:kq + 1])
                k.ts("pool", sr.t[:, kq, 0:1], IOTA.t[:], float(i * 128), ALU.add, r=[IOTA.b], wp=[sr.b])
                k.cp("pool", sr.t[:, kq, 1:2], t8.t[:, kq:kq + 1], r=[t8.b], wp=[sr.b])
            k.ts("dve", p4.t[:], p4.t[:], float(NSLOT - 1), ALU.min, r=[p4.b], w=[p4.b])
            k.cp("dve", p4i.t[:], p4.t[:], r=[p4.b], w=[p4i.b])
            for kq in range(4):
                def sca(e_, off=p4i.t[:, kq:kq + 1], src=sr.t[:, kq, :]):
                    return e_.indirect_dma_start(out=d["SLOT"], out_offset=bass.IndirectOffsetOnAxis(ap=off, axis=0), in_=src, in_offset=None)
                pool_dma_op(P, sca, reads=[p4i.b, sr.b, dInit], wpart=[dSLOT])
        P.end_phase()


def phase_smoe(c, layer, MOD5, TEi):
    nc, P, k, d, cfg = c.nc, c.P, c.k, c.d, c.cfg
    S, E, NTILE = cfg.S, cfg.E, cfg.NTILE
    IDXW, IDXB = TEi
    hin = d["H1"] if layer == 0 else d["H3"]
    hout = d["H2"] if layer == 0 else d["out"]
    w1tab = d["moe_w1"].rearrange("l e k n -> (l e k) n")
    w2tab = d["moe_w2"].rearrange("l e k n -> (l e k) n")
    b1tab = d["moe_b1"].rearrange("l r f -> (l r) f")
    with ExitStack() as ph:
        Z = sb(ph, nc, [128, 1024], BF16, name="z")
        W1 = sb(ph, nc, [128, 8, 2048], BF16, n=2, name="w1")
        W2 = sb(ph, nc, [128, 8, 1024], BF16, name="w2")
        B1r = sb(ph, nc, [128, 128], F32, n=2, name="b1r")
        B1c = sb(ph, nc, [128, 16], F32, n=2, name="b1c")
        SLt = sb(ph, nc, [128, 4, 2], F32, n=2, name="slt")
        TKi = sb(ph, nc, [128, 4], I32, n=2, name="tki")
        XG = sb(ph, nc, [128, 4, 1024], BF16, n=2, name="xg")
        XT = sb(ph, nc, [128, 8, 512], BF16, n=2, name="xt")
        ACTT = sb(ph, nc, [128, 8, 512], BF16, n=2, name="actt")
        G1 = sb(ph, nc, [128, 512], F32, n=2, name="g1")
        S1 = sb(ph, nc, [128, 512], F32, n=2, name="s1")
        L1 = sb(ph, nc, [128, 512], F32, n=2, name="l1")
        L2 = sb(ph, nc, [128, 512], F32, n=2, name="l2")
        GS = sb(ph, nc, [128, 512], F32, n=2, name="gs")
        OS = sb(ph, nc, [128, 1024], F32, n=4, name="os")
        HT = sb(ph, nc, [128, 1024], F32, n=2, name="ht")
        AT = sb(ph, nc, [128, 1024], F32, n=2, name="at")
        pT = ps(ph, nc, [128, 1024], BF16, n=2, name="pT")
        pG = ps(ph, nc, [128, 512], F32, n=2, name="pG")
        pL = ps(ph, nc, [128, 512], F32, n=2, name="pL")
        pO = ps(ph, nc, [128, 512], F32, n=2, name="pO")
        dACC, dXM2z, dOUT = Buf("dACCs"), Buf("dXM2z"), Buf("dOUT")
        k.memset("dve", Z.t[:], 0.0, w=[Z.b])
        k.dma("sp", d["XM2"][S:S + 128, :], Z.t[:], r=[Z.b], w=[dXM2z])

        def gather(out_ap, tab, idx_ap, reads, wslot, part=False):
            def g(e_):
                return e_.indirect_dma_start(out=out_ap, out_offset=None, in_=tab, in_offset=bass.IndirectOffsetOnAxis(ap=idx_ap, axis=0))
            return pool_dma_op(P, g, reads=reads, writes=() if part else [wslot], wpart=[wslot] if part else ())

        def loads(j):
            w1, b1r, slt, tki, xg = W1[j % 2], B1r[j % 2], SLt[j % 2], TKi[j % 2], XG[j % 2]
            k.dma("sp", slt.t[:], d["SLOT"][j * 512:(j + 1) * 512, :].rearrange("(s p) c -> p s c", p=128), w=[slt.b])
            k.cp("dve", tki.t[:], slt.t[:, :, 0], r=[slt.b], w=[tki.b])
            for kk in range(8):
                gather(w1.t[:, kk, :], w1tab, IDXW.t[:, j, kk:kk + 1], [IDXW.b], w1.b, part=True)
            gather(b1r.t[:], b1tab, IDXB.t[:, j:j + 1], [IDXB.b], b1r.b)
            for sub in range(4):
                gather(xg.t[:, sub, :], d["XM2"], tki.t[:, sub:sub + 1], [tki.b, dXM2z], xg.b, part=True)

        io = 0
        loads(0)
        for j in range(NTILE):
            if j + 1 < NTILE:
                loads(j + 1)
            w1, b1r, b1c, slt, tki, xg, xt, actt = (X[j % 2] for X in (W1, B1r, B1c, SLt, TKi, XG, XT, ACTT))
            k.tr(pG[0].t[:, 0:16], b1r.t[0:16, :], c.ident_f.t[0:16, 0:16], r=[b1r.b], w=[pG[0].b])
            k.cp("dve", b1c.t[:], pG[0].t[:, 0:16], r=[pG[0].b], w=[b1c.b])
            for sub in range(4):
                pt = pT[sub % 2]
                for kk in range(8):
                    k.tr(pt.t[:, kk * 128:(kk + 1) * 128], xg.t[:, sub, kk * 128:(kk + 1) * 128], c.ident_b.t[:], r=[xg.b],
                         w=[pt.b] if kk == 0 else (), wp=() if kk == 0 else [pt.b])
                k.cp("act" if sub % 2 else "dve", xt.t[:, :, sub * 128:(sub + 1) * 128], pt.t[:].rearrange("p (k t) -> p k t", k=8), r=[pt.b], wp=[xt.b])
            for pr in range(8):
                pg, pl = pG[pr % 2], pL[pr % 2]
                g1, s1, l1, l2, gs = (X[pr % 2] for X in (G1, S1, L1, L2, GS))
                for kk in range(8):
                    k.mm(pg.t[:], w1.t[:, kk, pr * 128:(pr + 1) * 128], xt.t[:, kk, :], kk == 0, kk == 7,
                         r=[w1.b, xt.b], w=[pg.b] if kk == 0 else (), wp=() if kk == 0 else [pg.b])
                for kk in range(8):
                    k.mm(pl.t[:], w1.t[:, kk, 1024 + pr * 128:1024 + (pr + 1) * 128], xt.t[:, kk, :], kk == 0, kk == 7,
                         r=[w1.b, xt.b], w=[pl.b] if kk == 0 else (), wp=() if kk == 0 else [pl.b])
                k.ts("dve", g1.t[:], pg.t[:], b1c.t[:, pr:pr + 1], ALU.add, 7.0, ALU.min, r=[pg.b, b1c.b], w=[g1.b])
                k.act(s1.t[:], g1.t[:], AF.Sigmoid, r=[g1.b], w=[s1.b], scale=1.702)
                k.act(l1.t[:], pl.t[:], AF.Identity, r=[pl.b, b1c.b], w=[l1.b], bias=b1c.t[:, 8 + pr:9 + pr])
                k.ts("dve", l2.t[:], l1.t[:], 7.0, ALU.min, -7.0, ALU.max, r=[l1.b], w=[l2.b])
                k.tt("dve", gs.t[:], g1.t[:], s1.t[:], ALU.mult, r=[g1.b, s1.b], w=[gs.b])
                k.stt(actt.t[:, pr, :], l2.t[:], 1.0, gs.t[:], ALU.add, ALU.mult, r=[l2.b, gs.b], wp=[actt.b])
            for kk in range(8):
                gather(W2.t[:, kk, :], w2tab, IDXW.t[:, j, kk:kk + 1], [IDXW.b], W2.b, part=True)
            for sub in range(4):
                os_ = OS[(4 * j + sub) % 4]
                for half in range(2):
                    po = pO[io % 2]
                    io += 1
                    for jj in range(8):
                        k.mm(po.t[:], actt.t[:, jj, sub * 128:(sub + 1) * 128], W2.t[:, jj, half * 512:(half + 1) * 512], jj == 0, jj == 7,
                             r=[actt.b, W2.b], w=[po.b] if jj == 0 else (), wp=() if jj == 0 else [po.b])
                    k.act(os_.t[:, half * 512:(half + 1) * 512], po.t[:], AF.Copy, r=[po.b, slt.b], wp=[os_.b], scale=slt.t[:, sub, 1:2])

                def sca(e_, off=tki.t[:, sub:sub + 1], src=os_.t[:]):
                    return e_.indirect_dma_start(out=d["ACCd"], out_offset=bass.IndirectOffsetOnAxis(ap=off, axis=0), in_=src, in_offset=None,
                                                 compute_op=ALU.add)
                pool_dma_op(P, sca, reads=[tki.b, os_.b], writes=[dACC])
        for t in range(S // 128):
            ht_, at_ = HT[t % 2], AT[t % 2]
            rows = slice(t * 128, (t + 1) * 128)
            k.dma("sp", ht_.t[:], hin[rows, :], w=[ht_.b])
            k.dma("sp", at_.t[:], d["ACCd"][rows, :], r=[dACC], w=[at_.b])
            k.tt("dve", at_.t[:], at_.t[:], MOD5.t[:], ALU.mult, r=[at_.b, MOD5.b], w=[at_.b])
            k.tt("pool", ht_.t[:], ht_.t[:], at_.t[:], ALU.add, r=[ht_.b, at_.b], w=[ht_.b])
            k.dma("sp", hout[rows, :], ht_.t[:], r=[ht_.b], wp=[dOUT])
        P.end_phase()
```

```python
from contextlib import ExitStack
import numpy as np
import ml_dtypes
import concourse.bass as bass
import concourse.mybir as mybir
from concourse.bass_utils import run_bass_kernel_spmd

F32 = mybir.dt.float32
BF16 = mybir.dt.bfloat16
AF = mybir.ActivationFunctionType
ALU = mybir.AluOpType
AX = mybir.AxisListType

COMPUTE = ("pe", "act", "dve", "pool")
ALLENG = ("pe", "act", "dve", "pool", "sp")
NDSEM = 72


class Buf:
    __slots__ = ("name", "writers", "readers")

    def __init__(self, name):
        self.name = name
        self.writers = []
        self.readers = []


class Op:
    __slots__ = ("eng", "fn", "raw", "oth", "signal", "tok_sem", "tok_val", "is_dma", "key", "phase")

    def __init__(self, eng, fn, phase):
        self.eng = eng
        self.fn = fn
        self.raw = []
        self.oth = []
        self.signal = False
        self.tok_sem = None
        self.tok_val = 0
        self.is_dma = False
        self.key = None
        self.phase = phase


class Prog:
    def __init__(self, nc, es):
        self.nc = nc
        self.phase = 0
        self.esem = {e: es.enter_context(nc.semaphore("s_" + e)) for e in COMPUTE}
        self.ecnt = {e: 0 for e in COMPUTE}
        self.dsem = [es.enter_context(nc.semaphore("d%d" % i)) for i in range(NDSEM)]
        self.dcnt = [0] * NDSEM
        self.seen = {e: {} for e in ALLENG}
        self._reset()
        self.nops = 0

    def _reset(self):
        self.ops = {e: [] for e in ALLENG}
        self.order = []
        self.keymap = {}
        self.last = {}

    def buf(self, name="b"):
        return Buf(name)

    def _deps(self, op, reads, writes, wpart):
        ph = self.phase
        for b in reads:
            for w in b.writers:
                if w.phase == ph:
                    op.raw.append(w)
        for b in list(writes) + list(wpart):
            for r in b.readers:
                if r.phase == ph:
                    op.oth.append(r)
        for b in writes:
            for w in b.writers:
                if w.phase == ph:
                    op.oth.append(w)
        for b in reads:
            b.readers.append(op)
        for b in writes:
            b.writers = [op]
            b.readers = []
        for b in wpart:
            if b.readers:
                b.writers = [op]
                b.readers = []
            else:
                b.writers.append(op)

    def op(self, eng, fn, reads=(), writes=(), wpart=()):
        o = Op(eng, fn, self.phase)
        self._deps(o, reads, writes, wpart)
        self.ops[eng].append(o)
        self.order.append(o)
        self.last[eng] = o
        return o

    def dma(self, q, out, in_, reads=(), writes=(), wpart=(), key=None, **kw):
        def fn(e, out=out, in_=in_, kw=kw):
            return e.dma_start(out=out, in_=in_, **kw)
        o = Op(q, fn, self.phase)
        o.is_dma = True
        if key is None:
            ws = list(writes) + list(wpart)
            key = ws[0]
        if key not in self.keymap:
            self.keymap[key] = len(self.keymap)
            assert len(self.keymap) <= NDSEM, "too many DMA keys in phase"
        o.key = self.keymap[key]
        self._deps(o, reads, writes, wpart)
        self.ops[q].append(o)
        self.order.append(o)
        return o

    def end_phase(self):
        nc = self.nc
        lasts = [self.last[e] for e in COMPUTE if e in self.last]
        lastd = {}
        for o in self.order:
            if o.is_dma:
                lastd[o.key] = o
        for e in ALLENG:
            o = Op(e, (lambda eng: eng.nop()), self.phase)
            o.raw = list(lasts) + list(lastd.values())
            self.ops[e].append(o)
            self.order.append(o)
        for o in self.order:
            for d in o.raw:
                if d.is_dma:
                    continue
                if d.eng == o.eng and o.eng == "pe" and not o.is_dma:
                    continue
                d.signal = True
            for d in o.oth:
                if d.is_dma:
                    continue
                if d.eng == o.eng and not o.is_dma:
                    continue
                d.signal = True
        for e in COMPUTE:
            for o in self.ops[e]:
                if o.is_dma:
                    continue
                if o.signal:
                    self.ecnt[e] += 1
                    o.tok_sem = self.esem[e]
                    o.tok_val = self.ecnt[e]
        for o in self.order:
            if o.is_dma:
                self.dcnt[o.key] += 16
                o.tok_sem = self.dsem[o.key]
                o.tok_val = self.dcnt[o.key]
        self.nops += len(self.order)

        with nc.Block() as block:
            def run(ename):
                def body(eng):
                    seen = self.seen[ename]
                    for o in self.ops[ename]:
                        need = {}
                        for d in o.raw:
                            if d.tok_sem is None:
                                continue
                            if (not d.is_dma) and d.eng == ename and ename == "pe" and not o.is_dma:
                                continue
                            s = d.tok_sem
                            if need.get(s.num, (None, 0))[1] < d.tok_val:
                                need[s.num] = (s, d.tok_val)
                        for d in o.oth:
                            if d.tok_sem is None:
                                continue
                            if (not d.is_dma) and d.eng == ename and not o.is_dma:
                                continue
                            s = d.tok_sem
                            if need.get(s.num, (None, 0))[1] < d.tok_val:
                                need[s.num] = (s, d.tok_val)
                        for s, v in need.values():
                            if seen.get(s.num, 0) < v:
                                eng.wait_ge(s, v)
                                seen[s.num] = v
                        ins = o.fn(eng)
                        if o.is_dma:
                            ins.then_inc(o.tok_sem, 16)
                        elif o.signal:
                            ins.then_inc(o.tok_sem, 1)
                return body

            block.tensor(run("pe"))
            block.scalar(run("act"))
            block.vector(run("dve"))
            block.gpsimd(run("pool"))
            block.sync(run("sp"))
        self.phase += 1
        self._reset()


class Slot:
    __slots__ = ("t", "b")

    def __init__(self, t, b):
        self.t = t
        self.b = b


class Ctx:
    pass


class K:
    def __init__(self, P):
        self.P = P

    def ts(self, eng, out, in0, s1, op0, s2=None, op1=None, r=(), w=(), wp=(), accum=None):
        if op1 is None:
            if accum is None:
                f = lambda e: e.tensor_scalar(out=out, in0=in0, scalar1=s1, scalar2=None, op0=op0)
            else:
                f = lambda e: e.tensor_scalar(out=out, in0=in0, scalar1=s1, scalar2=None, op0=op0, accum_out=accum)
        else:
            f = lambda e: e.tensor_scalar(out=out, in0=in0, scalar1=s1, scalar2=s2, op0=op0, op1=op1)
        return self.P.op(eng, f, reads=r, writes=w, wpart=wp)

    def tt(self, eng, out, in0, in1, op, r=(), w=(), wp=()):
        return self.P.op(eng, lambda e: e.tensor_tensor(out=out, in0=in0, in1=in1, op=op), reads=r, writes=w, wpart=wp)

    def stt(self, out, in0, scalar, in1, op0, op1, r=(), w=(), wp=(), accum=None):
        if accum is None:
            f = lambda e: e.scalar_tensor_tensor(out=out, in0=in0, scalar=scalar, in1=in1, op0=op0, op1=op1)
        else:
            f = lambda e: e.scalar_tensor_tensor(out=out, in0=in0, scalar=scalar, in1=in1, op0=op0, op1=op1, accum_out=accum)
        return self.P.op("dve", f, reads=r, writes=w, wpart=wp)

    def act(self, out, in_, func, r=(), w=(), wp=(), bias=None, scale=None, accum=None):
        kw = {}
        if bias is not None:
            kw["bias"] = bias
        if scale is not None:
            kw["scale"] = scale
        if accum is not None:
            kw["accum_out"] = accum
        return self.P.op("act", lambda e: e.activation(out=out, in_=in_, func=func, **kw), reads=r, writes=w, wpart=wp)

    def cp(self, eng, out, in_, r=(), w=(), wp=()):
        if eng == "act":
            return self.P.op("act", lambda e: e.copy(out=out, in_=in_), reads=r, writes=w, wpart=wp)
        return self.P.op(eng, lambda e: e.tensor_copy(out=out, in_=in_), reads=r, writes=w, wpart=wp)

    def memset(self, eng, ap, val, w=(), wp=()):
        return self.P.op(eng, lambda e: e.memset(ap, val), writes=w, wpart=wp)

    def mm(self, out, lhsT, rhs, start, stop, r=(), w=(), wp=()):
        return self.P.op("pe", lambda e: e.matmul(out, lhsT, rhs, start=start, stop=stop), reads=r, writes=w, wpart=wp)

    def tr(self, out, in_, ident, r=(), w=(), wp=()):
        return self.P.op("pe", lambda e: e.transpose(out, in_, ident), reads=r, writes=w, wpart=wp)

    def red(self, out, in_, r=(), w=(), wp=()):
        return self.P.op("dve", lambda e: e.tensor_reduce(out=out, in_=in_, axis=AX.X, op=ALU.add), reads=r, writes=w, wpart=wp)

    def recip(self, out, in_, r=(), w=(), wp=()):
        return self.P.op("dve", lambda e: e.reciprocal(out=out, in_=in_), reads=r, writes=w, wpart=wp)

    def dma(self, q, out, in_, r=(), w=(), wp=(), key=None):
        return self.P.dma(q, out, in_, reads=r, writes=w, wpart=wp, key=key)


class Cfg:
    def __init__(self, S=8192, L=256, E=32, debug=False, stop_after=None):
        self.S, self.L, self.E = S, L, E
        self.D = 1024
        self.NT = S // 128
        self.ROWS = S // 64
        self.NCH = S // 64
        self.MB = min(1024, S)
        self.NTILE = (4 * S) // 512 + E
        self.NSLOT = self.NTILE * 512
        self.debug = debug
        self.dense = False
        self.stop_after = stop_after


EPS = 1e-6
NEG = -30000.0


def host_consts(cfg):
    S = cfg.S
    NT = cfg.NT
    cs = {}
    cs["ident_f"] = np.eye(128, dtype=np.float32)
    p = np.arange(128)
    t = np.arange(64)
    s = p % 64
    cs["trif"] = (s[:, None] <= t[None, :]).astype(np.float32)
    cs["trib"] = (s[:, None] >= t[None, :]).astype(np.float32)
    r = np.ones((128, 512), np.float32)
    r[:, ::64] = 0.0
    cs["reset"] = r
    inv = (10000.0 ** (-np.arange(16, dtype=np.float32) / 16.0)).astype(np.float32)
    tt = np.arange(NT)
    row = (2 * tt[None, :] + (p[:, None] // 64)).astype(np.float32)
    col = np.broadcast_to((p % 64).astype(np.float32)[:, None], (128, NT))
    ang = np.stack([row[:, :, None] * inv[None, None, :], col[:, :, None] * inv[None, None, :]], axis=2)
    ang = ang.astype(np.float32)
    E, NTILE = cfg.E, cfg.NTILE
    cs["ut"] = (p[:, None] < p[None, :]).astype(np.float32)
    cs["iota"] = p.astype(np.float32).reshape(128, 1)
    kp = np.zeros((128, 9), np.float32)
    kp[:, :8] = np.arange(8)[None, :] * 128 + p[:, None]
    kp[:, 8] = p % 16
    cs["kp"] = kp
    cs["th"] = np.broadcast_to((512.0 * np.arange(16, dtype=np.float32))[None, None, :], (1, E, 16)).reshape(1, E * 16).copy()
    cs["j512"] = np.broadcast_to((512.0 * np.arange(NTILE, dtype=np.float32))[None, :, None], (1, NTILE, E)).reshape(1, NTILE * E).copy()
    si = np.zeros((cfg.NSLOT, 2), np.float32)
    si[:, 0] = S + (np.arange(cfg.NSLOT) % 128)
    cs["slotinit"] = si
    cs["cos"] = np.cos(ang).astype(np.float32).reshape(128, NT * 32)
    cs["sin"] = np.sin(ang).astype(np.float32).reshape(128, NT * 32)
    return cs


def layout_rpb(rpb):
    H = rpb.shape[0]
    c = np.arange(64)[:, None]
    kc = np.arange(64)[None, :]
    win = np.clip(c - 8, 0, 48)
    valid = (kc >= win) & (kc < win + 16)
    idx = np.clip(kc - c + 15, 0, 30)
    g = rpb[:, :, idx]
    g = np.where(valid[None, None], g, np.float32(NEG)).astype(np.float32)
    return np.ascontiguousarray(g.transpose(2, 0, 1, 3)).reshape(64, H * 15 * 64)


_uid = [0]


def sb(es, nc, shape, dt, n=1, name="t"):
    out = []
    for i in range(n):
        _uid[0] += 1
        t = es.enter_context(nc.sbuf_tensor("%s_%d" % (name, _uid[0]), list(shape), dt))
        out.append(Slot(t, Buf(name)))
    return out if n > 1 else out[0]


def ps(es, nc, shape, dt, n=1, name="p"):
    out = []
    for i in range(n):
        _uid[0] += 1
        t = es.enter_context(nc.psum_tensor("%s_%d" % (name, _uid[0]), list(shape), dt))
        out.append(Slot(t, Buf(name)))
    return out if n > 1 else out[0]


def declare_io(nc, cfg):
    S, L, E = cfg.S, cfg.L, cfg.E
    d = {}

    inputs = set()
    d["_inputs"] = inputs

    def inp(name, shape, dt=F32):
        d[name] = nc.dram_tensor(name, list(shape), dt, kind="ExternalInput").ap()
        inputs.add(name)

    inp("x", [S, 1024]); inp("c", [8, 128]); inp("ctx", [L, 1024]); inp("c_ctx", [8, 128])
    inp("ada_w", [2, 1024, 6144]); inp("ada_b", [2, 6144]); inp("norm1_g", [2, 1024]); inp("norm2_g", [2, 1024])
    inp("ab_w_in", [1024, 4096]); inp("ab_w_out", [1024, 1024]); inp("nat_q_norm", [1, 64]); inp("nat_k_norm", [1, 64])
    inp("rpb_full", [64, 8 * 15 * 64]); inp("hgrn_lb", [16, 128]); inp("hgrn_o_norm", [1, 128])
    inp("conv_w1", [1024, 2048]); inp("conv_b1", [16, 128]); inp("conv_dw", [31, 1024]); inp("conv_dw_b", [8, 128])
    inp("conv_ln_g", [8, 128]); inp("conv_ln_b", [8, 128]); inp("conv_w2", [1024, 1024]); inp("conv_b2", [1, 1024])
    inp("router_w", [2, 1024, E]); inp("router_b", [2, E])
    inp("moe_w1", [2, E, 1024, 2048]); inp("moe_b1", [2, E * 16, 128]); inp("moe_w2", [2, E, 1024, 1024]); inp("moe_b2", [2, E, 1024])
    inp("k_ident_f", [128, 128]); inp("k_trif", [128, 64]); inp("k_trib", [128, 64]); inp("k_reset", [128, 512])
    inp("k_cos", [128, cfg.NT * 32]); inp("k_sin", [128, cfg.NT * 32])
    inp("k_ut", [128, 128]); inp("k_iota", [128, 1]); inp("k_th", [1, E * 16]); inp("k_j512", [1, cfg.NTILE * E])
    inp("k_slotinit", [cfg.NSLOT, 2]); inp("k_kp", [128, 9])
    d["out"] = nc.dram_tensor("out", [S, 1024], F32, kind="ExternalOutput").ap()
    kind = "ExternalOutput" if cfg.debug else "Internal"

    def scr(name, shape, dt):
        d[name] = nc.dram_tensor(name, list(shape), dt, kind=kind).ap()

    scr("XT", [128, 8, S], BF16); scr("XTc", [128, 8, L], BF16)
    scr("QTr", [128, 4, S], BF16); scr("QTf", [128, 4, S], BF16); scr("KTr", [128, 4, S], BF16)
    scr("KcT", [128, 4, L], BF16)
    scr("VA", [S, 520], BF16); scr("VcA", [L, 520], BF16)
    scr("VH", [S, 512], BF16); scr("VHc", [L, 512], BF16)
    scr("G", [S, 512], F32)
    scr("HQ", [2, 128, 4, S], BF16); scr("HK", [2, 128, 4, S], BF16)
    scr("KH", [2, S, 512], BF16); scr("KHc", [2, L, 512], BF16)
    scr("DEC", [2, 128, 4, S // 64], F32); scr("DECc", [2, 128, 4, L // 64], F32)
    scr("OF", [S, 512], F32)
    scr("CAT", [S, 1024], BF16)
    scr("H1", [S, 1024], F32); scr("H2", [S, 1024], F32); scr("H3", [S, 1024], F32)
    scr("XT2", [128, 8, S], BF16)
    scr("COMB", [S, E], F32)
    scr("ACC0", [S, 1024], F32)
    scr("GT", [128, 8, S + 32], BF16)
    scr("YD", [128, 8, S], F32)
    scr("XM2", [S + 128, 1024], BF16)
    scr("MK", [S, E], F32)
    scr("ACCd", [S + 128, 1024], F32)
    scr("SLOT", [cfg.NSLOT, 2], F32)
    return d


def build_program(cfg):
    nc = bass.Bass("TRN2", target_bir_lowering=False)
    c = Ctx()
    c.nc, c.cfg = nc, cfg
    c.d = declare_io(nc, cfg)
    c.inputs = c.d.pop("_inputs")
    with ExitStack() as ges:
        P = Prog(nc, ges)
        c.P = P
        c.k = K(P)
        c.ident_f = sb(ges, nc, [128, 128], F32, name="identf")
        c.ident_b = sb(ges, nc, [128, 128], BF16, name="identb")
        c.ones_f = sb(ges, nc, [128, 128], F32, name="onesf")
        c.k.dma("sp", c.ident_f.t[:], c.d["k_ident_f"], w=[c.ident_f.b])
        c.k.cp("dve", c.ident_b.t[:], c.ident_f.t[:], r=[c.ident_f.b], w=[c.ident_b.b])
        c.k.memset("dve", c.ones_f.t[:], 1.0, w=[c.ones_f.b])
        touch = sb(ges, nc, [1, 64], F32, name="touch")
        for i, nm in enumerate(sorted(c.inputs)):
            ap = c.d[nm]
            idx = tuple([0] * (len(ap.shape) - 2) + [slice(0, 1), slice(0, 1)])
            c.k.dma("sp", touch.t[0:1, i:i + 1], ap[idx], wp=[touch.b])
        if cfg.debug:
            tb = sb(ges, nc, [1, 2], BF16, name="touchb")
            c.k.memset("dve", touch.t[0:1, 62:64], 0.0, wp=[touch.b])
            c.k.memset("dve", tb.t[:], 0.0, w=[tb.b])
            for i, (nm, ap) in enumerate(c.d.items()):
                if nm not in c.inputs:
                    idx = tuple([0] * (len(ap.shape) - 2) + [slice(0, 1), slice(0, 1)])
                    src = tb.t[0:1, 0:1] if ap.dtype == BF16 else touch.t[0:1, 63:64]
                    c.k.dma("sp", ap[idx], src, r=[tb.b, touch.b], w=[Buf("o")])
        P.end_phase()
        def dbg_stop(name):
            return cfg.stop_after == name

        done = False
        for layer in (0, 1):
            with ExitStack() as lay:
                c.MOD = sb(lay, nc, [128, 6144], F32, name="MOD")
                if layer == 0:
                    c.CMOD = sb(lay, nc, [128, 2048], F32, name="CMOD")
                    seq = [("mods0", lambda: phase_mods(c, 0)), ("a1", lambda: phase_a1(c)), ("a2", lambda: phase_a2(c)),
                           ("nat", lambda: phase_nat(c)), ("hgrn", lambda: phase_hgrn(c)), ("post0", lambda: phase_post(c, 0))]
                else:
                    seq = [("mods1", lambda: phase_mods(c, 1)), ("f", lambda: phase_f(c)), ("g1", lambda: phase_g1(c)),
                           ("post1", lambda: phase_post(c, 1))]
                for name, fn in seq:
                    fn()
                    if dbg_stop(name):
                        done = True
                        break
            if done:
                break
            with ExitStack() as m5:
                MOD5 = sb(m5, nc, [128, 1024], F32, name="MOD5")
                with ExitStack() as ph:
                    emit_mods(c, ph, layer, [10, 11], lambda ct: MOD5.t[:, (ct - 10) * 512:(ct - 9) * 512], MOD5.b, False)
                    P.end_phase()
                if cfg.dense:
                    phase_moe(c, layer, MOD5)
                else:
                    TEi = (sb(m5, nc, [128, cfg.NTILE, 8], mybir.dt.int32, name="IDXW"), sb(m5, nc, [128, cfg.NTILE], mybir.dt.int32, name="IDXB"))
                    phase_route(c, layer, TEi)
                    if dbg_stop("route%d" % layer):
                        break
                    phase_smoe(c, layer, MOD5, TEi)
            if dbg_stop("moe%d" % layer):
                break
    return nc, c


def emit_mods(c, ph, layer, cts, dst_fn, dst_buf, with_ctx):
    nc, P, k, d = c.nc, c.P, c.k, c.d
    crow = sb(ph, nc, [16, 128], F32, name="crow")
    crow2 = sb(ph, nc, [16, 128], F32, name="crow2")
    ccol = sb(ph, nc, [128, 16], F32, name="ccol")
    CB = sb(ph, nc, [128, 16, 128], F32, name="CB")
    AW = sb(ph, nc, [128, 8, 512], F32, n=2, name="AW")
    ABr = sb(ph, nc, [1, 512], F32, n=2, name="ABr")
    pT = ps(ph, nc, [128, 16], F32, name="pT")
    pM = ps(ph, nc, [128, 512], F32, n=2, name="pM")
    pC = ps(ph, nc, [128, 512], F32, n=2, name="pC")
    k.dma("sp", crow.t[0:8, :], d["c"], wp=[crow.b])
    k.dma("sp", crow.t[8:16, :], d["c_ctx"], wp=[crow.b])
    k.act(crow2.t[:], crow.t[:], AF.Silu, r=[crow.b], w=[crow2.b])
    k.tr(pT.t[:], crow2.t[:], c.ident_f.t[0:16, 0:16], r=[crow2.b], w=[pT.b])
    k.cp("dve", ccol.t[:], pT.t[:], r=[pT.b], w=[ccol.b])
    for j in range(16):
        k.cp("dve" if j % 2 else "pool", CB.t[:, j, :], ccol.t[:, j:j + 1].to_broadcast([128, 128]), r=[ccol.b], wp=[CB.b])
    awv = d["ada_w"][layer].rearrange("(k p) n -> p k n", p=128)
    for i, ct in enumerate(cts):
        aw, ab = AW[i % 2], ABr[i % 2]
        k.dma("sp", aw.t[:], awv[:, :, ct * 512:(ct + 1) * 512], w=[aw.b])
        k.dma("sp", ab.t[:], d["ada_b"][layer:layer + 1, ct * 512:(ct + 1) * 512], w=[ab.b])
        pm = pM[i % 2]
        for kk in range(8):
            k.mm(pm.t[:], CB.t[:, kk, :], aw.t[:, kk, :], kk == 0, False, r=[CB.b, aw.b], w=[pm.b] if kk == 0 else (), wp=() if kk == 0 else [pm.b])
        k.mm(pm.t[:], c.ones_f.t[0:1, :], ab.t[:], False, True, r=[ab.b], wp=[pm.b])
        k.cp("act", dst_fn(ct), pm.t[:], r=[pm.b], wp=[dst_buf])
        if with_ctx and ct < 4:
            pc = pC[i % 2]
            for kk in range(8):
                k.mm(pc.t[:], CB.t[:, 8 + kk, :], aw.t[:, kk, :], kk == 0, False, r=[CB.b, aw.b], w=[pc.b] if kk == 0 else (), wp=() if kk == 0 else [pc.b])
            k.mm(pc.t[:], c.ones_f.t[0:1, :], ab.t[:], False, True, r=[ab.b], wp=[pc.b])
            k.cp("dve", c.CMOD.t[:, ct * 512:(ct + 1) * 512], pc.t[:], r=[pc.b], wp=[c.CMOD.b])


def phase_mods(c, layer):
    with ExitStack() as ph:
        emit_mods(c, ph, layer, list(range(10)), lambda ct: c.MOD.t[:, ct * 512:(ct + 1) * 512], c.MOD.b, layer == 0)
        c.P.end_phase()


def make_A(c, ph, gname, layer, modcols, modt):
    nc, k, d = c.nc, c.k, c.d
    g = sb(ph, nc, [128, 1024], F32, name="gbc")
    A = sb(ph, nc, [128, 1024], F32, name="A")
    k.dma("sp", g.t[:], d[gname][layer:layer + 1, :].partition_broadcast(128), w=[g.b])
    k.stt(A.t[:], modt.t[:, modcols:modcols + 1024], 1.0, g.t[:], ALU.add, ALU.mult, r=[modt.b, g.b], w=[A.b])
    return A


def norm_mod(c, xt, A, SH, shb, outs, tmp, j):
    k = c.k
    ss, t1 = tmp["ss"], tmp["t1"]
    k.stt(t1.t[:], xt.t[:], 1.0, xt.t[:], ALU.mult, ALU.mult, r=[xt.b], w=[t1.b], wp=[ss.b], accum=ss.t[:, 4 * j:4 * j + 1])
    k.ts("dve", ss.t[:, 4 * j + 1:4 * j + 2], ss.t[:, 4 * j:4 * j + 1], 1.0 / 1024, ALU.mult, EPS, ALU.add, r=[ss.b], wp=[ss.b])
    k.act(ss.t[:, 4 * j + 2:4 * j + 3], ss.t[:, 4 * j + 1:4 * j + 2], AF.Sqrt, r=[ss.b], wp=[ss.b])
    k.recip(ss.t[:, 4 * j + 3:4 * j + 4], ss.t[:, 4 * j + 2:4 * j + 3], r=[ss.b], wp=[ss.b])
    k.stt(t1.t[:], xt.t[:], ss.t[:, 4 * j + 3:4 * j + 4], A.t[:], ALU.mult, ALU.mult, r=[xt.b, ss.b, A.b], w=[t1.b])
    for (ap, eng, slot) in outs:
        k.tt(eng, ap, t1.t[:], SH, ALU.add, r=[t1.b, shb], wp=[slot.b])


def phase_a1(c):
    nc, P, k, d, cfg = c.nc, c.P, c.k, c.d, c.cfg
    S, L, NT = cfg.S, cfg.L, cfg.NT
    with ExitStack() as ph:
        W = sb(ph, nc, [128, 8, 2560], BF16, name="Wtok")
        wv = d["ab_w_in"].rearrange("(k p) n -> p k n", p=128)
        for i, c0 in enumerate((0, 512, 1024, 3072, 3584)):
            k.dma("pool", W.t[:, :, i * 512:(i + 1) * 512], wv[:, :, c0:c0 + 512], wp=[W.b])
        A = make_A(c, ph, "norm1_g", 0, 1024, c.MOD)
        Ac = make_A(c, ph, "norm1_g", 0, 1024, c.CMOD)
        g64 = sb(ph, nc, [128, 128], F32, name="g64")
        GQ = sb(ph, nc, [128, 512], F32, name="GQ")
        GK = sb(ph, nc, [128, 512], F32, name="GK")
        k.dma("sp", g64.t[:, 0:64], d["nat_q_norm"].partition_broadcast(128), wp=[g64.b])
        k.dma("sp", g64.t[:, 64:128], d["nat_k_norm"].partition_broadcast(128), wp=[g64.b])
        k.ts("dve", GQ.t[:].rearrange("p (h e) -> p h e", h=8), g64.t[:, 0:64].unsqueeze(1).to_broadcast([128, 8, 64]), 0.125, ALU.mult, r=[g64.b], w=[GQ.b])
        k.ts("dve", GK.t[:].rearrange("p (h e) -> p h e", h=8), g64.t[:, 64:128].unsqueeze(1).to_broadcast([128, 8, 64]), 1.0, ALU.mult, r=[g64.b], w=[GK.b])
        COSG = sb(ph, nc, [128, 128], F32, n=2, name="COS")
        SING = sb(ph, nc, [128, 128], F32, n=2, name="SIN")
        XIN = sb(ph, nc, [128, 1024], F32, n=2, name="xin")
        tmps = [{"ss": sb(ph, nc, [128, 16], F32, name="ss"), "t1": sb(ph, nc, [128, 1024], F32, name="t1")} for _ in range(2)]
        XM = sb(ph, nc, [128, 1024], BF16, n=2, name="xm")
        XTG = sb(ph, nc, [128, 8, 512], BF16, n=2, name="xtg")
        pT = ps(ph, nc, [128, 1024], BF16, name="pT")
        pS = ps(ph, nc, [128, 512], F32, n=5, name="pS")
        pO = ps(ph, nc, [128, 1024], BF16, n=2, name="pO")
        SQs = sb(ph, nc, [128, 1024], F32, n=2, name="sq")
        STs = sb(ph, nc, [128, 48], F32, n=2, name="st")
        QNs = sb(ph, nc, [128, 1024], F32, n=2, name="qn")
        R1s = sb(ph, nc, [128, 1024], F32, n=2, name="r1")
        R2s = sb(ph, nc, [128, 1024], F32, n=2, name="r2")
        OB = sb(ph, nc, [128, 1536], BF16, n=2, name="ob")
        OTG = sb(ph, nc, [128, 3, 4, 512], BF16, n=2, name="otg")
        VAs = sb(ph, nc, [128, 8, 65], BF16, n=2, name="vas")
        VHs = sb(ph, nc, [128, 512], BF16, n=2, name="vhs")
        Gs = sb(ph, nc, [128, 512], F32, n=2, name="gs")
        for v in VAs:
            k.memset("pool", v.t[:, :, 64:65], 1.0, wp=[v.b])
        dXT, dXTc = Buf("dXT"), Buf("dXTc")
        dQ, dV, dVH, dG = Buf("dQ"), Buf("dV"), Buf("dVH"), Buf("dG")

        def run(src, ntile, Asl, modt, is_ctx):
            ngrp = (ntile + 3) // 4
            it = 0
            for g in range(ngrp):
                nj = min(4, ntile - 4 * g)
                xtg = XTG[g % 2]
                otg = OTG[g % 2]
                COS, SIN = COSG[g % 2], SING[g % 2]
                if not is_ctx:
                    k.dma("sp", COS.t[:, 0:nj * 32], d["k_cos"][:, g * 128:g * 128 + nj * 32], w=[COS.b])
                    k.dma("sp", SIN.t[:, 0:nj * 32], d["k_sin"][:, g * 128:g * 128 + nj * 32], w=[SIN.b])
                for j in range(nj):
                    t = 4 * g + j
                    xin, xm = XIN[it % 2], XM[it % 2]
                    ob, vas, vhs, gs = OB[it % 2], VAs[it % 2], VHs[it % 2], Gs[it % 2]
                    SQ, ST, QN, R1, R2 = SQs[it % 2], STs[it % 2], QNs[it % 2], R1s[it % 2], R2s[it % 2]
                    it += 1
                    k.dma("sp", xin.t[:], src[t * 128:(t + 1) * 128, :], w=[xin.b])
                    norm_mod(c, xin, Asl, modt.t[:, 0:1024], modt.b, [(xm.t[:], "pool", xm)], tmps[it % 2], j)
                    for kk in range(8):
                        k.tr(pT.t[:, kk * 128:(kk + 1) * 128], xm.t[:, kk * 128:(kk + 1) * 128], c.ident_b.t[:], r=[xm.b],
                             w=[pT.b] if kk == 0 else (), wp=() if kk == 0 else [pT.b])
                    k.cp("act", xtg.t[:, :, j * 128:(j + 1) * 128], pT.t[:].rearrange("p (k t) -> p k t", k=8), r=[pT.b], wp=[xtg.b])
                    cols = (1, 2, 3) if is_ctx else (0, 1, 2, 3, 4)
                    for ci in cols:
                        for kk in range(8):
                            k.mm(pS[ci].t[:], xtg.t[:, kk, j * 128:(j + 1) * 128], W.t[:, kk, ci * 512:(ci + 1) * 512], kk == 0, kk == 7,
                                 r=[xtg.b, W.b], w=[pS[ci].b] if kk == 0 else (), wp=() if kk == 0 else [pS[ci].b])
                    srcs = ((1, 1),) if is_ctx else ((0, 0), (1, 1))
                    for (ci, slot_i) in srcs:
                        k.act(SQ.t[:, slot_i * 512:(slot_i + 1) * 512], pS[ci].t[:], AF.Square, r=[pS[ci].b], wp=[SQ.b])
                        k.red(ST.t[:, slot_i * 8:(slot_i + 1) * 8], SQ.t[:, slot_i * 512:(slot_i + 1) * 512].rearrange("p (h e) -> p h e", h=8), r=[SQ.b], wp=[ST.b])
                    k.ts("dve", ST.t[:, 16:32], ST.t[:, 0:16], 1.0 / 64, ALU.mult, EPS, ALU.add, r=[ST.b], wp=[ST.b])
                    k.act(ST.t[:, 32:48], ST.t[:, 16:32], AF.Sqrt, r=[ST.b], wp=[ST.b])
                    k.recip(ST.t[:, 16:32], ST.t[:, 32:48], r=[ST.b], wp=[ST.b])
                    for (ci, slot_i) in srcs:
                        qn = QN.t[:, slot_i * 512:(slot_i + 1) * 512]
                        k.tt("dve", qn.rearrange("p (h e) -> p h e", h=8), pS[ci].t[:].rearrange("p (h e) -> p h e", h=8),
                             ST.t[:, 16 + slot_i * 8:16 + slot_i * 8 + 8].unsqueeze(2).to_broadcast([128, 8, 64]), ALU.mult,
                             r=[pS[ci].b, ST.b], wp=[QN.b])
                        Gt = GQ if slot_i == 0 else GK
                        k.tt("pool", qn, qn, Gt.t[:], ALU.mult, r=[QN.b, Gt.b], wp=[QN.b])
                    if is_ctx:
                        k.cp("act", ob.t[:, 1024:1536], QN.t[:, 512:1024], r=[QN.b], wp=[ob.b])
                    else:
                        k.cp("act", ob.t[:, 512:1024], QN.t[:, 0:512], r=[QN.b], wp=[ob.b])
                        qv = QN.t[:].rearrange("p (h a b i) -> p h a b i", h=16, a=2, b=2)
                        cosb = COS.t[:, j * 32:(j + 1) * 32].rearrange("p (a i) -> p a i", a=2).unsqueeze(1).to_broadcast([128, 16, 2, 16])
                        sinb = SIN.t[:, j * 32:(j + 1) * 32].rearrange("p (a i) -> p a i", a=2).unsqueeze(1).to_broadcast([128, 16, 2, 16])
                        r1v = R1.t[:].rearrange("p (h a b i) -> p h a b i", h=16, a=2, b=2)
                        r2v = R2.t[:].rearrange("p (h a b i) -> p h a b i", h=16, a=2, b=2)
                        x1, x2 = qv[:, :, :, 0, :], qv[:, :, :, 1, :]
                        k.tt("dve", r1v[:, :, :, 0, :], x1, cosb, ALU.mult, r=[QN.b, COS.b], wp=[R1.b])
                        k.tt("pool", r2v[:, :, :, 0, :], x2, sinb, ALU.mult, r=[QN.b, SIN.b], wp=[R2.b])
                        k.tt("dve", r1v[:, :, :, 1, :], x2, cosb, ALU.mult, r=[QN.b, COS.b], wp=[R1.b])
                        k.tt("pool", r2v[:, :, :, 1, :], x1, sinb, ALU.mult, r=[QN.b, SIN.b], wp=[R2.b])
                        for slot_i, o0 in ((0, 0), (1, 1024)):
                            ov = ob.t[:, o0:o0 + 512].rearrange("p (h a b i) -> p h a b i", h=8, a=2, b=2)
                            a1 = r1v[:, slot_i * 8:(slot_i + 1) * 8]
                            a2 = r2v[:, slot_i * 8:(slot_i + 1) * 8]
                            k.tt("dve", ov[:, :, :, 0, :], a1[:, :, :, 0, :], a2[:, :, :, 0, :], ALU.subtract, r=[R1.b, R2.b], wp=[ob.b])
                            k.tt("dve", ov[:, :, :, 1, :], a1[:, :, :, 1, :], a2[:, :, :, 1, :], ALU.add, r=[R1.b, R2.b], wp=[ob.b])
                    which = (2,) if is_ctx else (0, 1, 2)
                    for wi in which:
                        po = pO[0] if wi < 2 else pO[1]
                        for hp in range(4):
                            col = ((wi % 2) * 4 + hp) * 128
                            first = (hp == 0 and wi in (0, 2))
                            k.tr(po.t[:, col:col + 128], ob.t[:, wi * 512 + hp * 128: wi * 512 + (hp + 1) * 128], c.ident_b.t[:], r=[ob.b],
                                 w=[po.b] if first else (), wp=() if first else [po.b])
                    if not is_ctx:
                        k.cp("act", otg.t[:, 0:2, :, j * 128:(j + 1) * 128], pO[0].t[:].rearrange("p (w h t) -> p w h t", w=2, h=4), r=[pO[0].b], wp=[otg.b])
                    k.cp("dve", otg.t[:, 2, :, j * 128:(j + 1) * 128], pO[1].t[:, 0:512].rearrange("p (h t) -> p h t", h=4), r=[pO[1].b], wp=[otg.b])
                    k.cp("act", vas.t[:, :, 0:64], pS[2].t[:].rearrange("p (h e) -> p h e", h=8), r=[pS[2].b], wp=[vas.b])
                    k.cp("dve", vhs.t[:], pS[3].t[:], r=[pS[3].b], w=[vhs.b])
                    rows = slice(t * 128, (t + 1) * 128)
                    if is_ctx:
                        k.dma("sp", d["VcA"][rows, :], vas.t[:].rearrange("p h e -> p (h e)"), r=[vas.b], wp=[dV])
                        k.dma("sp", d["VHc"][rows, :], vhs.t[:], r=[vhs.b], wp=[dVH])
                    else:
                        k.act(gs.t[:], pS[4].t[:], AF.Silu, r=[pS[4].b], w=[gs.b])
                        k.dma("sp", d["VA"][rows, :], vas.t[:].rearrange("p h e -> p (h e)"), r=[vas.b], wp=[dV])
                        k.dma("sp", d["VH"][rows, :], vhs.t[:], r=[vhs.b], wp=[dVH])
                        k.dma("sp", d["G"][rows, :], gs.t[:], r=[gs.b], wp=[dG])
                tok = slice(g * 512, g * 512 + nj * 128)
                w_ = nj * 128
                if is_ctx:
                    k.dma("sp", d["XTc"][:, :, tok], xtg.t[:, :, 0:w_], r=[xtg.b], wp=[dXTc])
                    k.dma("sp", d["KcT"][:, :, tok], otg.t[:, 2, :, 0:w_], r=[otg.b], wp=[dQ])
                else:
                    k.dma("sp", d["XT"][:, :, tok], xtg.t[:, :, 0:w_], r=[xtg.b], wp=[dXT])
                    k.dma("sp", d["QTr"][:, :, tok], otg.t[:, 0, :, 0:w_], r=[otg.b], wp=[dQ])
                    k.dma("sp", d["QTf"][:, :, tok], otg.t[:, 1, :, 0:w_], r=[otg.b], wp=[dQ])
                    k.dma("sp", d["KTr"][:, :, tok], otg.t[:, 2, :, 0:w_], r=[otg.b], wp=[dQ])

        run(d["ctx"], L // 128, Ac, c.CMOD, True)
        run(d["x"], NT, A, c.MOD, False)
        P.end_phase()


def core_inputs(inp, b, cfg, consts):
    f = lambda a: np.ascontiguousarray(np.asarray(a, dtype=np.float32))
    m = {
        "x": f(inp["x"][b]), "c": f(inp["c"][b]).reshape(8, 128), "ctx": f(inp["ctx"][b]),
        "c_ctx": f(inp["c_ctx"]).reshape(8, 128),
        "ada_w": f(inp["ada_w"]), "ada_b": f(inp["ada_b"]), "norm1_g": f(inp["norm1_g"]), "norm2_g": f(inp["norm2_g"]),
        "ab_w_in": f(inp["ab_w_in"][0]), "ab_w_out": f(inp["ab_w_out"][0]),
        "nat_q_norm": f(inp["nat_q_norm"][0]).reshape(1, 64), "nat_k_norm": f(inp["nat_k_norm"][0]).reshape(1, 64),
        "rpb_full": layout_rpb(f(inp["nat_rpb"][0])),
        "hgrn_lb": f(inp["hgrn_lb"]).reshape(16, 128), "hgrn_o_norm": f(inp["hgrn_o_norm"][0]).reshape(1, 128),
        "conv_w1": f(inp["conv_w1"][0]), "conv_b1": f(inp["conv_b1"][0]).reshape(16, 128), "conv_dw": f(inp["conv_dw"][0]),
        "conv_dw_b": f(inp["conv_dw_b"][0]).reshape(8, 128), "conv_ln_g": f(inp["conv_ln_g"][0]).reshape(8, 128),
        "conv_ln_b": f(inp["conv_ln_b"][0]).reshape(8, 128), "conv_w2": f(inp["conv_w2"][0]), "conv_b2": f(inp["conv_b2"][0]).reshape(1, 1024),
        "router_w": f(inp["router_w"]), "router_b": f(inp["router_b"]),
        "moe_w1": f(inp["moe_w1"]), "moe_b1": f(inp["moe_b1"]).reshape(2, cfg.E * 16, 128),
        "moe_w2": f(inp["moe_w2"]), "moe_b2": f(inp["moe_b2"]),
    }
    for kname, v in consts.items():
        m["k_" + kname] = v
    return m


_cache = {}


def kernel(**inputs):
    B = inputs["x"].shape[0]
    S = inputs["x"].shape[1]
    cfg = Cfg(S=S, L=inputs["ctx"].shape[1], E=inputs["moe_w1"].shape[1])
    key = (cfg.S, cfg.L, cfg.E)
    if key not in _cache:
        _cache[key] = build_program(cfg)[0]
    nc = _cache[key]
    consts = host_consts(cfg)
    in_maps = [core_inputs(inputs, b, cfg, consts) for b in range(B)]
    res = run_bass_kernel_spmd(nc, in_maps, core_ids=list(range(B)))
    return np.stack([np.asarray(r["out"], dtype=np.float32) for r in res.results], axis=0)


def phase_a2(c):
    nc, P, k, d, cfg = c.nc, c.P, c.k, c.d, c.cfg
    S, L = cfg.S, cfg.L
    with ExitStack() as ph:
        W = sb(ph, nc, [128, 8, 1536], BF16, name="Wfm")
        wv = d["ab_w_in"].rearrange("(k p) n -> p k n", p=128)
        for i in range(3):
            k.dma("pool", W.t[:, :, i * 512:(i + 1) * 512], wv[:, :, 1536 + i * 512:1536 + (i + 1) * 512], wp=[W.b])
        RESET = sb(ph, nc, [128, 512], F32, name="reset")
        k.dma("sp", RESET.t[:], d["k_reset"], w=[RESET.b])
        lbr = sb(ph, nc, [16, 128], F32, name="lbr")
        Ee = sb(ph, nc, [128, 16], F32, name="Ee")
        LB = sb(ph, nc, [128, 24], F32, name="LB")
        pL = ps(ph, nc, [128, 16], F32, name="pL")
        k.dma("sp", lbr.t[:], d["hgrn_lb"], w=[lbr.b])
        k.tr(pL.t[:], lbr.t[:], c.ident_f.t[0:16, 0:16], r=[lbr.b], w=[pL.b])
        k.act(Ee.t[:], pL.t[:], AF.Exp, r=[pL.b], w=[Ee.b])
        ev = Ee.t[:].rearrange("p (d j h) -> p d j h", d=2, j=2)
        k.tt("dve", LB.t[:, 16:24].rearrange("p (d h) -> p d h", d=2), ev[:, :, 0, :], ev[:, :, 1, :], ALU.add, r=[Ee.b], wp=[LB.b])
        k.recip(LB.t[:, 16:24], LB.t[:, 16:24], r=[LB.b], wp=[LB.b])
        k.tt("dve", LB.t[:, 0:8].rearrange("p (d h) -> p d h", d=2), ev[:, :, 0, :], LB.t[:, 16:24].rearrange("p (d h) -> p d h", d=2), ALU.mult, r=[Ee.b, LB.b], wp=[LB.b])
        k.ts("dve", LB.t[:, 8:16], LB.t[:, 0:8], -1.0, ALU.mult, 1.0, ALU.add, r=[LB.b], wp=[LB.b])

        XTG = sb(ph, nc, [128, 8, 512], BF16, n=2, name="xtg")
        names = ("q32", "sg", "f", "lf", "kk", "B", "e1", "e2", "t1", "r", "e3")
        TM = {n_: sb(ph, nc, [128, 512], F32, n=2, name=n_) for n_ in names}
        HQs = sb(ph, nc, [128, 2, 4, 512], BF16, n=2, name="hqs")
        HKs = sb(ph, nc, [128, 2, 4, 512], BF16, n=2, name="hks")
        KHT = sb(ph, nc, [128, 512], BF16, n=2, name="kht")
        KHs = sb(ph, nc, [128, 4, 2, 512], BF16, n=2, name="khs")
        DECs = sb(ph, nc, [128, 2, 4, 8], F32, n=2, name="decs")
        pQ = ps(ph, nc, [128, 512], F32, n=2, name="pQ")
        pF = ps(ph, nc, [128, 512], F32, n=3, name="pF")
        pK = ps(ph, nc, [128, 512], BF16, n=2, name="pK")
        dHQ, dHK, dKH, dDEC = Buf("dHQ"), Buf("dHK"), Buf("dKH"), Buf("dDEC")
        QS = 128.0 ** -0.5

        def run(src, ntok, is_ctx):
            ngrp = (ntok + 511) // 512
            it = 0
            for g in range(ngrp):
                n = min(512, ntok - g * 512)
                nch = n // 64
                nsub = n // 128
                xtg, hqs, hks, khs, decs = XTG[g % 2], HQs[g % 2], HKs[g % 2], KHs[g % 2], DECs[g % 2]
                k.dma("sp", xtg.t[:, :, 0:n], src[:, :, g * 512:g * 512 + n], w=[xtg.b])
                for h in range(4):
                    pq = pQ[h % 2]
                    if not is_ctx:
                        for kk_ in range(8):
                            k.mm(pq.t[:, 0:n], W.t[:, kk_, h * 128:(h + 1) * 128], xtg.t[:, kk_, 0:n], kk_ == 0, kk_ == 7,
                                 r=[W.b, xtg.b], w=[pq.b] if kk_ == 0 else (), wp=() if kk_ == 0 else [pq.b])
                        q32 = TM["q32"][h % 2]
                        k.act(q32.t[:, 0:n], pq.t[:, 0:n], AF.Silu, r=[pq.b], w=[q32.b])
                    for dd in range(2):
                        pf = pF[(2 * h + dd) % 3]
                        c0 = 512 + dd * 512 + h * 128
                        for kk_ in range(8):
                            k.mm(pf.t[:, 0:n], W.t[:, kk_, c0:c0 + 128], xtg.t[:, kk_, 0:n], kk_ == 0, kk_ == 7,
                                 r=[W.b, xtg.b], w=[pf.b] if kk_ == 0 else (), wp=() if kk_ == 0 else [pf.b])
                        tm = {n_: TM[n_][it % 2] for n_ in names}
                        kht = KHT[it % 2]
                        pk = pK[it % 2]
                        it += 1
                        sg, f, lf, kk, Bc, e1, e2, t1, rr, e3 = (tm[x] for x in ("sg", "f", "lf", "kk", "B", "e1", "e2", "t1", "r", "e3"))
                        li = dd * 4 + h
                        k.act(sg.t[:, 0:n], pf.t[:, 0:n], AF.Sigmoid, r=[pf.b], w=[sg.b])
                        k.ts("dve", f.t[:, 0:n], sg.t[:, 0:n], LB.t[:, 8 + li:9 + li], ALU.mult, LB.t[:, li:li + 1], ALU.add, r=[sg.b, LB.b], w=[f.b])
                        k.act(lf.t[:, 0:n], f.t[:, 0:n], AF.Ln, r=[f.b], w=[lf.b])
                        k.ts("pool", kk.t[:, 0:n], f.t[:, 0:n], -1.0, ALU.mult, 1.0, ALU.add, r=[f.b], w=[kk.b])
                        P.op("dve", (lambda e, o=Bc.t[:, 0:n], a=RESET.t[:, 0:n], b_=lf.t[:, 0:n]:
                                     e.tensor_tensor_scan(out=o, data0=a, data1=b_, initial=0.0, op0=ALU.mult, op1=ALU.add)),
                             reads=[RESET.b, lf.b], writes=[Bc.b])
                        Bv = Bc.t[:, 0:n].rearrange("p (c t) -> p c t", t=64)
                        Bend = Bv[:, :, 63:64].to_broadcast([128, nch, 64])
                        v3 = lambda s_: s_.t[:, 0:n].rearrange("p (c t) -> p c t", t=64)
                        if dd == 0:
                            k.act(e1.t[:, 0:n], Bc.t[:, 0:n], AF.Exp, r=[Bc.b], w=[e1.b])
                            k.act(e2.t[:, 0:n], Bc.t[:, 0:n], AF.Exp, r=[Bc.b], w=[e2.b], scale=-1.0)
                            k.tt("dve", v3(t1), Bend, Bv, ALU.subtract, r=[Bc.b], w=[t1.b])
                            k.act(e3.t[:, 0:n], t1.t[:, 0:n], AF.Exp, r=[t1.b], w=[e3.b])
                        else:
                            k.tt("dve", t1.t[:, 0:n], lf.t[:, 0:n], Bc.t[:, 0:n], ALU.subtract, r=[lf.b, Bc.b], w=[t1.b])
                            k.tt("dve", v3(rr), v3(t1), Bend, ALU.add, r=[t1.b, Bc.b], w=[rr.b])
                            k.act(e1.t[:, 0:n], rr.t[:, 0:n], AF.Exp, r=[rr.b], w=[e1.b])
                            k.act(e2.t[:, 0:n], rr.t[:, 0:n], AF.Exp, r=[rr.b], w=[e2.b], scale=-1.0)
                            k.act(e3.t[:, 0:n], t1.t[:, 0:n], AF.Exp, r=[t1.b], w=[e3.b], scale=-1.0)
                        k.act(decs.t[:, dd, h, 0:nch], Bv[:, :, 63], AF.Exp, r=[Bc.b], wp=[decs.b])
                        if not is_ctx:
                            q32 = TM["q32"][h % 2]
                            k.stt(hqs.t[:, dd, h, 0:n], q32.t[:, 0:n], QS, e1.t[:, 0:n], ALU.mult, ALU.mult, r=[q32.b, e1.b], wp=[hqs.b])
                            k.tt("pool", hks.t[:, dd, h, 0:n], kk.t[:, 0:n], e2.t[:, 0:n], ALU.mult, r=[kk.b, e2.b], wp=[hks.b])
                        k.tt("pool", kht.t[:, 0:n], kk.t[:, 0:n], e3.t[:, 0:n], ALU.mult, r=[kk.b, e3.b], w=[kht.b])
                        for sub in range(nsub):
                            k.tr(pk.t[:, sub * 128:(sub + 1) * 128], kht.t[:, sub * 128:(sub + 1) * 128], c.ident_b.t[:], r=[kht.b],
                                 w=[pk.b] if sub == 0 else (), wp=() if sub == 0 else [pk.b])
                        k.cp("act", khs.t[:, 0:nsub, dd, h * 128:(h + 1) * 128], pk.t[:, 0:n].rearrange("p (s e) -> p s e", e=128), r=[pk.b], wp=[khs.b])
                tok = slice(g * 512, g * 512 + n)
                for dd in range(2):
                    if is_ctx:
                        k.dma("sp", d["KHc"][dd, tok, :].rearrange("(s p) e -> p s e", p=128), khs.t[:, 0:nsub, dd, :], r=[khs.b], wp=[dKH])
                        k.dma("sp", d["DECc"][dd, :, :, g * 8:g * 8 + nch], decs.t[:, dd, :, 0:nch], r=[decs.b], wp=[dDEC])
                    else:
                        k.dma("sp", d["HQ"][dd, :, :, tok], hqs.t[:, dd, :, 0:n], r=[hqs.b], wp=[dHQ])
                        k.dma("sp", d["HK"][dd, :, :, tok], hks.t[:, dd, :, 0:n], r=[hks.b], wp=[dHK])
                        k.dma("sp", d["KH"][dd, tok, :].rearrange("(s p) e -> p s e", p=128), khs.t[:, 0:nsub, dd, :], r=[khs.b], wp=[dKH])
                        k.dma("sp", d["DEC"][dd, :, :, g * 8:g * 8 + nch], decs.t[:, dd, :, 0:nch], r=[decs.b], wp=[dDEC])

        run(d["XTc"], L, True)
        run(d["XT"], S, False)
        P.end_phase()


def phase_nat(c):
    nc, P, k, d, cfg = c.nc, c.P, c.k, c.d, c.cfg
    S, L, ROWS = cfg.S, cfg.L, cfg.ROWS
    NCC = L // 128
    NCH = 4 + NCC
    with ExitStack() as ph:
        BFf = sb(ph, nc, [128, 7680], F32, name="bff")
        BFb = sb(ph, nc, [128, 8, 960], BF16, name="bfb")
        k.dma("sp", BFf.t[0:64, :], d["rpb_full"], wp=[BFf.b])
        k.dma("sp", BFf.t[64:128, :], d["rpb_full"], wp=[BFf.b])
        k.cp("dve", BFb.t[:].rearrange("p h e -> p (h e)"), BFf.t[:], r=[BFf.b], w=[BFb.b])
        KcT = sb(ph, nc, [128, 4, L], BF16, name="kct")
        VcA = sb(ph, nc, [128, NCC, 520], BF16, name="vca")
        k.dma("sp", KcT.t[:], d["KcT"], w=[KcT.b])
        k.dma("sp", VcA.t[:], d["VcA"].rearrange("(c p) f -> p c f", p=128), w=[VcA.b])
        QR = sb(ph, nc, [128, 4, 512], BF16, n=2, name="qr")
        QF = sb(ph, nc, [128, 4, 512], BF16, n=2, name="qf")
        KW = sb(ph, nc, [128, 4, 512], BF16, n=3, name="kw")
        VW = sb(ph, nc, [128, 4, 520], BF16, n=3, name="vw")
        PT = sb(ph, nc, [128, NCH * 64], BF16, n=3, name="pt")
        NS = sb(ph, nc, [64, 512], BF16, n=2, name="ns")
        RD = sb(ph, nc, [64, 8], F32, n=2, name="rd")
        pS = ps(ph, nc, [128, 512], F32, n=4, name="pS")
        pO = ps(ph, nc, [64, 4, 65], F32, n=4, name="pO")
        dCAT = Buf("dCATn")
        it = 0
        for r in range(ROWS):
            g8, ro = r // 8, (r % 8) * 64
            qr, qf = QR[g8 % 2], QF[g8 % 2]
            if r % 8 == 0:
                k.dma("sp", qr.t[:], d["QTr"][:, :, g8 * 512:(g8 + 1) * 512], w=[qr.b])
                k.dma("sp", qf.t[:], d["QTf"][:, :, g8 * 512:(g8 + 1) * 512], w=[qf.b])
            rs = min(max(r - 4, 0), ROWS - 8)
            dr0 = rs - r + 7
            kw, vw = KW[r % 3], VW[r % 3]
            k.dma("sp", kw.t[:], d["KTr"][:, :, rs * 64:rs * 64 + 512], w=[kw.b])
            k.dma("sp", vw.t[:], d["VA"][rs * 64:rs * 64 + 512, :].rearrange("(c p) f -> p c f", p=128), w=[vw.b])
            ns, rd = NS[r % 2], RD[r % 2]
            po2 = (pO[(2 * r) % 4], pO[(2 * r + 1) % 4])
            def scores(h):
                nonlocal it
                hp, pb = h // 2, (h % 2) * 64
                psx, pt = pS[it % 4], PT[it % 3]
                it += 1
                first = True
                for kc in range(4):
                    k.mm(psx.t[:, kc * 64:(kc + 1) * 64], kw.t[pb:pb + 64, hp, kc * 128:(kc + 1) * 128], qr.t[pb:pb + 64, hp, ro:ro + 64], True, False,
                         r=[kw.b, qr.b], w=[psx.b] if first else (), wp=() if first else [psx.b])
                    first = False
                    k.mm(psx.t[:, kc * 64:(kc + 1) * 64], BFb.t[pb:pb + 64, h, (dr0 + 2 * kc) * 64:(dr0 + 2 * kc) * 64 + 128], c.ident_b.t[pb:pb + 64, pb:pb + 64], False, True,
                         r=[BFb.b], wp=[psx.b])
                for cc in range(NCC):
                    k.mm(psx.t[:, (4 + cc) * 64:(5 + cc) * 64], KcT.t[pb:pb + 64, hp, cc * 128:(cc + 1) * 128], qf.t[pb:pb + 64, hp, ro:ro + 64], True, True,
                         r=[KcT.b, qf.b], wp=[psx.b])
                k.act(pt.t[:], psx.t[:, 0:NCH * 64], AF.Exp, r=[psx.b], w=[pt.b])
                return pt

            def pv(h, pt):
                po = po2[h // 4]
                hh = h % 4
                for ch in range(NCH):
                    rhs = vw.t[:, ch, h * 65:(h + 1) * 65] if ch < 4 else VcA.t[:, ch - 4, h * 65:(h + 1) * 65]
                    k.mm(po.t[:, hh, :], pt.t[:, ch * 64:(ch + 1) * 64], rhs, ch == 0, ch == NCH - 1,
                         r=[pt.b, vw.b, VcA.b], w=[po.b] if (ch == 0 and hh == 0) else (), wp=() if (ch == 0 and hh == 0) else [po.b])

            pts = [scores(0)]
            for h in range(8):
                if h + 1 < 8:
                    pts.append(scores(h + 1))
                pv(h, pts[h])
            for half in range(2):
                po = po2[half]
                k.recip(rd.t[:, half * 4:(half + 1) * 4], po.t[:, :, 64], r=[po.b], wp=[rd.b])
                k.tt("dve", ns.t[:, half * 256:(half + 1) * 256].rearrange("p (h e) -> p h e", h=4), po.t[:, :, 0:64],
                     rd.t[:, half * 4:(half + 1) * 4].unsqueeze(2).to_broadcast([64, 4, 64]), ALU.mult, r=[po.b, rd.b], wp=[ns.b])
            k.dma("sp", d["CAT"][r * 64:(r + 1) * 64, 0:512], ns.t[:], r=[ns.b], wp=[dCAT])
        P.end_phase()


def phase_hgrn(c):
    nc, P, k, d, cfg = c.nc, c.P, c.k, c.d, c.cfg
    S, L = cfg.S, cfg.L
    NG = S // 512
    NCHT = S // 64
    LC = L // 64
    with ExitStack() as ph:
        TRI = sb(ph, nc, [128, 2, 64], F32, name="tri")
        k.dma("sp", TRI.t[:, 0, :], d["k_trif"], wp=[TRI.b])
        k.dma("sp", TRI.t[:, 1, :], d["k_trib"], wp=[TRI.b])
        og = sb(ph, nc, [128, 128], F32, name="og")
        ONG = sb(ph, nc, [128, 512], F32, name="ong")
        k.dma("sp", og.t[:], d["hgrn_o_norm"].partition_broadcast(128), w=[og.b])
        k.ts("dve", ONG.t[:].rearrange("p (h e) -> p h e", h=4), og.t[:].unsqueeze(1).to_broadcast([128, 4, 128]), 1.0, ALU.mult, r=[og.b], w=[ONG.b])
        S32s = sb(ph, nc, [128, 4, 128], F32, n=2, name="s32")
        Sbfs = sb(ph, nc, [128, 4, 128], BF16, n=2, name="sbf")
        DECt = sb(ph, nc, [128, 4, NCHT], F32, name="dect")
        DECc = sb(ph, nc, [128, 4, LC], F32, name="decc")
        KHc = sb(ph, nc, [128, L // 128, 512], BF16, name="khc")
        VHc = sb(ph, nc, [128, L // 128, 512], BF16, name="vhc")
        HQg = sb(ph, nc, [128, 4, 512], BF16, n=2, name="hqg")
        HKg = sb(ph, nc, [128, 4, 512], BF16, n=2, name="hkg")
        KHg = sb(ph, nc, [128, 4, 512], BF16, n=2, name="khg")
        VHg = sb(ph, nc, [128, 4, 512], BF16, n=2, name="vhg")
        SC = [sb(ph, nc, [128, 256], BF16, n=2, name="sc%d" % i) for i in range(2)]
        for i in range(2):
            for s_ in SC[i]:
                k.memset("pool", s_.t[:], 0.0, w=[s_.b])
        OFs = sb(ph, nc, [64, 512], F32, n=2, name="ofs")
        OFc = sb(ph, nc, [64, 512], F32, n=2, name="ofc")
        Gc = sb(ph, nc, [64, 512], F32, n=2, name="gc")
        O32 = sb(ph, nc, [64, 512], F32, n=2, name="o32")
        SQ = sb(ph, nc, [64, 512], F32, n=2, name="sq")
        ST = sb(ph, nc, [64, 16], F32, n=2, name="st")
        Y1 = sb(ph, nc, [64, 512], F32, n=2, name="y1")
        YB = sb(ph, nc, [64, 512], BF16, n=2, name="yb")
        pSs = ps(ph, nc, [128, 512], F32, n=2, name="pSs")
        pSo = ps(ph, nc, [128, 512], F32, n=2, name="pSo")
        pSt = ps(ph, nc, [128, 512], F32, n=2, name="pSt")
        dOF, dCAT = Buf("dOF"), Buf("dCATh")
        k.dma("sp", VHc.t[:], d["VHc"].rearrange("(s p) e -> p s e", p=128), w=[VHc.b])
        it = 0
        for dd in range(2):
            k.memset("dve", S32s[0].t[:], 0.0, w=[S32s[0].b])
            k.dma("sp", DECt.t[:], d["DEC"][dd], w=[DECt.b])
            k.dma("sp", DECc.t[:], d["DECc"][dd], w=[DECc.b])
            k.dma("sp", KHc.t[:], d["KHc"][dd].rearrange("(s p) e -> p s e", p=128), w=[KHc.b])

            sn = 0

            def state_update(khs, vhs, tl, pb, dec_ap_fn):
                nonlocal it, sn
                pst = pSt[it % 2]
                for h in range(4):
                    k.mm(pst.t[:, h * 128:(h + 1) * 128], khs.t[pb:pb + 64, tl, h * 128:(h + 1) * 128], vhs.t[pb:pb + 64, tl, h * 128:(h + 1) * 128], True, True,
                         r=[khs.b, vhs.b], w=[pst.b] if h == 0 else (), wp=() if h == 0 else [pst.b])
                so, sw = S32s[sn % 2], S32s[(sn + 1) % 2]
                for h in range(4):
                    k.stt(sw.t[:, h, :], so.t[:, h, :], dec_ap_fn(h), pst.t[:, h * 128:(h + 1) * 128], ALU.mult, ALU.add,
                          r=[so.b, pst.b, DECt.b, DECc.b], wp=[sw.b])
                nb = Sbfs[(sn + 1) % 2]
                k.cp("act", nb.t[:], sw.t[:], r=[sw.b], w=[nb.b])
                sn += 1

            chs = range(LC) if dd == 0 else range(LC - 1, -1, -1)
            for ch in chs:
                state_update(KHc, VHc, ch // 2, (ch % 2) * 64, lambda h, ch=ch: DECc.t[:, h, ch:ch + 1])
                it += 1
            groups = list(range(NG)) if dd == 0 else list(range(NG - 1, -1, -1))
            order = []
            for gi, g in enumerate(groups):
                for tl in (range(4) if dd == 0 else range(3, -1, -1)):
                    for cc in ((0, 1) if dd == 0 else (1, 0)):
                        order.append((gi, g, tl, cc))
            loaded = set()

            def ensure(gi, g):
                if gi in loaded:
                    return
                loaded.add(gi)
                hq, hk, kh, vh = HQg[gi % 2], HKg[gi % 2], KHg[gi % 2], VHg[gi % 2]
                tok = slice(g * 512, (g + 1) * 512)
                k.dma("sp", hq.t[:], d["HQ"][dd, :, :, tok], w=[hq.b])
                k.dma("sp", hk.t[:], d["HK"][dd, :, :, tok], w=[hk.b])
                k.dma("sp", kh.t[:], d["KH"][dd, tok, :].rearrange("(s p) e -> p s e", p=128), w=[kh.b])
                k.dma("sp", vh.t[:], d["VH"][tok, :].rearrange("(s p) e -> p s e", p=128), w=[vh.b])

            def scores(n):
                gi, g, tl, cc = order[n]
                ensure(gi, g)
                hq, hk = HQg[gi % 2], HKg[gi % 2]
                pb = cc * 64
                toff, qoff = tl * 128, tl * 128 + cc * 64
                pss = pSs[n % 2]
                sc = SC[cc][(n // 2) % 2]
                for h in range(4):
                    k.mm(pss.t[:, h * 64:(h + 1) * 64], hk.t[:, h, toff:toff + 128], hq.t[:, h, qoff:qoff + 64], True, True,
                         r=[hk.b, hq.b], w=[pss.b] if h == 0 else (), wp=() if h == 0 else [pss.b])
                k.tt("dve", sc.t[pb:pb + 64, :].rearrange("p (h t) -> p h t", h=4), pss.t[pb:pb + 64, 0:256].rearrange("p (h t) -> p h t", h=4),
                     TRI.t[pb:pb + 64, dd, :].unsqueeze(1).to_broadcast([64, 4, 64]), ALU.mult, r=[pss.b, TRI.b], wp=[sc.b])
                return sc

            def rest(n, sc):
                nonlocal it
                gi, g, tl, cc = order[n]
                hq, hk, kh, vh = HQg[gi % 2], HKg[gi % 2], KHg[gi % 2], VHg[gi % 2]
                ch = g * 8 + tl * 2 + cc
                pb = cc * 64
                qoff = tl * 128 + cc * 64
                pso = pSo[n % 2]
                Sbf = Sbfs[sn % 2]
                state_update(kh, vh, tl, pb, lambda h, ch=ch: DECt.t[:, h, ch:ch + 1])
                it += 1
                for h in range(4):
                    k.mm(pso.t[0:64, h * 128:(h + 1) * 128], sc.t[:, h * 64:(h + 1) * 64], vh.t[:, tl, h * 128:(h + 1) * 128], True, False,
                         r=[sc.b, vh.b], w=[pso.b] if h == 0 else (), wp=() if h == 0 else [pso.b])
                    k.mm(pso.t[0:64, h * 128:(h + 1) * 128], hq.t[:, h, qoff:qoff + 64], Sbf.t[:, h, :], False, True,
                         r=[hq.b, Sbf.b], wp=[pso.b])
                rows = slice(ch * 64, (ch + 1) * 64)
                if dd == 0:
                    ofs = OFs[n % 2]
                    k.cp("act", ofs.t[:], pso.t[0:64, :], r=[pso.b], w=[ofs.b])
                    k.dma("sp", d["OF"][rows, :], ofs.t[:], r=[ofs.b], wp=[dOF])
                else:
                    ofc, gc, o32, sq, st, y1, yb = (X[n % 2] for X in (OFc, Gc, O32, SQ, ST, Y1, YB))
                    k.dma("sp", ofc.t[:], d["OF"][rows, :], r=[dOF], w=[ofc.b])
                    k.dma("sp", gc.t[:], d["G"][rows, :], w=[gc.b])
                    k.tt("dve", o32.t[:], pso.t[0:64, :], ofc.t[:], ALU.add, r=[pso.b, ofc.b], w=[o32.b])
                    k.act(sq.t[:], o32.t[:], AF.Square, r=[o32.b], w=[sq.b])
                    k.red(st.t[:, 0:4], sq.t[:].rearrange("p (h e) -> p h e", h=4), r=[sq.b], wp=[st.b])
                    k.ts("dve", st.t[:, 4:8], st.t[:, 0:4], 1.0 / 128, ALU.mult, EPS, ALU.add, r=[st.b], wp=[st.b])
                    k.act(st.t[:, 8:12], st.t[:, 4:8], AF.Sqrt, r=[st.b], wp=[st.b])
                    k.recip(st.t[:, 12:16], st.t[:, 8:12], r=[st.b], wp=[st.b])
                    k.tt("dve", y1.t[:].rearrange("p (h e) -> p h e", h=4), o32.t[:].rearrange("p (h e) -> p h e", h=4),
                         st.t[:, 12:16].unsqueeze(2).to_broadcast([64, 4, 128]), ALU.mult, r=[o32.b, st.b], w=[y1.b])
                    k.tt("pool", y1.t[:], y1.t[:], ONG.t[0:64, :], ALU.mult, r=[y1.b, ONG.b], w=[y1.b])
                    k.tt("pool", yb.t[:], y1.t[:], gc.t[:], ALU.mult, r=[y1.b, gc.b], w=[yb.b])
                    k.dma("sp", d["CAT"][rows, 512:1024], yb.t[:], r=[yb.b], wp=[dCAT])

            N = len(order)
            cur = scores(0)
            for n in range(N):
                nxt = scores(n + 1) if n + 1 < N else None
                rest(n, cur)
                cur = nxt
        P.end_phase()


def phase_post(c, layer):
    nc, P, k, d, cfg = c.nc, c.P, c.k, c.d, c.cfg
    S, E, NT = cfg.S, cfg.E, cfg.NT
    NG = S // 512
    hin = d["x"] if layer == 0 else d["H2"]
    hout = d["H1"] if layer == 0 else d["H3"]
    with ExitStack() as ph:
        Wo = sb(ph, nc, [128, 8, 1024], BF16, name="Wo")
        wsrc = d["ab_w_out"] if layer == 0 else d["conv_w2"]
        k.dma("pool", Wo.t[:], wsrc.rearrange("(k p) n -> p k n", p=128), w=[Wo.b])
        RW = sb(ph, nc, [128, 8, E], F32, name="RW")
        k.dma("sp", RW.t[:], d["router_w"][layer].rearrange("(k p) e -> p k e", p=128), w=[RW.b])
        RB = sb(ph, nc, [128, E], F32, name="RB")
        k.dma("sp", RB.t[:], d["router_b"][layer:layer + 1, :].partition_broadcast(128), w=[RB.b])
        B2t = sb(ph, nc, [E, 1024], F32, name="B2t")
        k.dma("sp", B2t.t[:], d["moe_b2"][layer], w=[B2t.b])
        A2 = make_A(c, ph, "norm2_g", layer, 4096, c.MOD)
        XIN = sb(ph, nc, [128, 1024], F32, n=2, name="xin")
        Hs = sb(ph, nc, [128, 1024], F32, n=2, name="hs")
        V1s = sb(ph, nc, [128, 1024], F32, n=2, name="v1")
        tmps = [{"ss": sb(ph, nc, [128, 16], F32, name="ss"), "t1": sb(ph, nc, [128, 1024], F32, name="t1")} for _ in range(2)]
        XFs = sb(ph, nc, [128, 1024], F32, n=2, name="xf")
        XB = sb(ph, nc, [128, 1024], BF16, n=2, name="xb")
        XT2g = sb(ph, nc, [128, 8, 512], BF16, n=2, name="xt2g")
        XFT = sb(ph, nc, [128, 8, 128], F32, name="xft")
        LG = sb(ph, nc, [128, 4 * E + 32], F32, n=2, name="lg")
        CT = sb(ph, nc, [E, 128], F32, name="ct")
        ACs = sb(ph, nc, [128, 1024], F32, name="acs")
        pT = ps(ph, nc, [128, 1024], BF16, name="pT")
        pY = ps(ph, nc, [128, 512], F32, n=2, name="pY")
        pTf = ps(ph, nc, [128, 512], F32, n=2, name="pTf")
        pLg = ps(ph, nc, [128, E], F32, name="pLg")
        pCT = ps(ph, nc, [E, 128], F32, name="pCT")
        dH, dXT2, dCOMB, dACC = Buf("dH"), Buf("dXT2"), Buf("dCOMB"), Buf("dACC")
        if layer == 0:
            CATt = sb(ph, nc, [128, 1024], BF16, n=2, name="catt")
            catT = sb(ph, nc, [128, 8, 128], BF16, n=2, name="catT")
        else:
            YG = sb(ph, nc, [128, 8, 512], F32, name="yg")
            YSQ = sb(ph, nc, [128, 512], F32, n=2, name="ysq")
            Mm = sb(ph, nc, [128, 512], F32, name="mm_")
            MSQ = sb(ph, nc, [128, 512], F32, name="msq")
            RS = sb(ph, nc, [128, 512], F32, name="rs")
            TA = sb(ph, nc, [128, 512], F32, n=2, name="ta")
            TB = sb(ph, nc, [128, 512], F32, n=2, name="tb")
            HN = sb(ph, nc, [128, 8, 512], BF16, name="hn")
            pSum = pTf[0]
            pSq = pTf[1]
            lnr = sb(ph, nc, [16, 128], F32, name="lnr")
            LNP = sb(ph, nc, [128, 16], F32, name="lnp")
            k.dma("sp", lnr.t[0:8, :], d["conv_ln_g"], wp=[lnr.b])
            k.dma("sp", lnr.t[8:16, :], d["conv_ln_b"], wp=[lnr.b])
            k.tr(pLg.t[:, 0:16] if E >= 16 else pTf[0].t[:, 0:16], lnr.t[:], c.ident_f.t[0:16, 0:16], r=[lnr.b], w=[pLg.b if E >= 16 else pTf[0].b])
            k.cp("dve", LNP.t[:], pLg.t[:, 0:16] if E >= 16 else pTf[0].t[:, 0:16], r=[pLg.b if E >= 16 else pTf[0].b], w=[LNP.b])
            b2bc = sb(ph, nc, [128, 1024], F32, name="b2bc")
            B2M = sb(ph, nc, [128, 1024], F32, name="b2m")
            k.dma("sp", b2bc.t[:], d["conv_b2"].partition_broadcast(128), w=[b2bc.b])
            k.tt("dve", B2M.t[:], b2bc.t[:], c.MOD.t[:, 2048:3072], ALU.mult, r=[b2bc.b, c.MOD.b], w=[B2M.b])

        it = 0
        for g in range(NG):
            xt2g = XT2g[g % 2]
            if layer == 1:
                k.dma("sp", YG.t[:], d["YD"][:, :, g * 512:(g + 1) * 512], w=[YG.b])
                for cch in range(8):
                    ysq = YSQ[cch % 2]
                    k.act(ysq.t[:], YG.t[:, cch, :], AF.Square, r=[YG.b], w=[ysq.b])
                    k.mm(pSum.t[:], c.ones_f.t[:], YG.t[:, cch, :], cch == 0, cch == 7, r=[YG.b, c.ones_f.b], w=[pSum.b] if cch == 0 else (), wp=() if cch == 0 else [pSum.b])
                    k.mm(pSq.t[:], c.ones_f.t[:], ysq.t[:], cch == 0, cch == 7, r=[ysq.b, c.ones_f.b], w=[pSq.b] if cch == 0 else (), wp=() if cch == 0 else [pSq.b])
                k.act(Mm.t[:], pSum.t[:], AF.Copy, r=[pSum.b], w=[Mm.b], scale=1.0 / 1024)
                k.tt("pool", MSQ.t[:], Mm.t[:], Mm.t[:], ALU.mult, r=[Mm.b], w=[MSQ.b])
                k.stt(RS.t[:], pSq.t[:], 1.0 / 1024, MSQ.t[:], ALU.mult, ALU.subtract, r=[pSq.b, MSQ.b], w=[RS.b])
                k.ts("dve", RS.t[:], RS.t[:], EPS, ALU.add, r=[RS.b], w=[RS.b])
                k.act(RS.t[:], RS.t[:], AF.Sqrt, r=[RS.b], w=[RS.b])
                k.recip(RS.t[:], RS.t[:], r=[RS.b], w=[RS.b])
                for cch in range(8):
                    ta, tb = TA[cch % 2], TB[cch % 2]
                    k.tt("dve", ta.t[:], YG.t[:, cch, :], Mm.t[:], ALU.subtract, r=[YG.b, Mm.b], w=[ta.b])
                    k.tt("pool", tb.t[:], ta.t[:], RS.t[:], ALU.mult, r=[ta.b, RS.b], w=[tb.b])
                    k.act(HN.t[:, cch, :], tb.t[:], AF.Silu, r=[tb.b, LNP.b], wp=[HN.b], scale=LNP.t[:, cch:cch + 1], bias=LNP.t[:, 8 + cch:9 + cch])
            for j in range(4):
                t = g * 4 + j
                rows = slice(t * 128, (t + 1) * 128)
                xin, hs, xb = XIN[it % 2], Hs[it % 2], XB[it % 2]
                lg = LG[it % 2]
                V1, XF = V1s[it % 2], XFs[it % 2]
                k.dma("sp", xin.t[:], hin[rows, :], w=[xin.b])
                if layer == 0:
                    cat, ctT = CATt[it % 2], catT[it % 2]
                    k.dma("sp", cat.t[:], d["CAT"][rows, :], w=[cat.b])
                    for kk in range(8):
                        k.tr(pT.t[:, kk * 128:(kk + 1) * 128], cat.t[:, kk * 128:(kk + 1) * 128], c.ident_b.t[:], r=[cat.b],
                             w=[pT.b] if kk == 0 else (), wp=() if kk == 0 else [pT.b])
                    k.cp("act", ctT.t[:].rearrange("p k t -> p (k t)"), pT.t[:], r=[pT.b], w=[ctT.b])
                    lhs = lambda kk: ctT.t[:, kk, :]
                    lb_ = ctT.b
                else:
                    lhs = lambda kk: HN.t[:, kk, j * 128:(j + 1) * 128]
                    lb_ = HN.b
                it += 1
                for half in range(2):
                    for kk in range(8):
                        k.mm(pY[half].t[:], lhs(kk), Wo.t[:, kk, half * 512:(half + 1) * 512], kk == 0, kk == 7,
                             r=[lb_, Wo.b], w=[pY[half].b] if kk == 0 else (), wp=() if kk == 0 else [pY[half].b])
                    k.tt("dve", V1.t[:, half * 512:(half + 1) * 512], pY[half].t[:], c.MOD.t[:, 2048 + half * 512:2048 + (half + 1) * 512], ALU.mult,
                         r=[pY[half].b, c.MOD.b], wp=[V1.b])
                if layer == 1:
                    k.tt("pool", xin.t[:], xin.t[:], B2M.t[:], ALU.add, r=[xin.b, B2M.b], w=[xin.b])
                k.tt("pool", hs.t[:], V1.t[:], xin.t[:], ALU.add, r=[V1.b, xin.b], w=[hs.b])
                k.dma("sp", hout[rows, :], hs.t[:], r=[hs.b], wp=[dH])
                norm_mod(c, hs, A2, c.MOD.t[:, 3072:4096], c.MOD.b, [(XF.t[:], "dve", XF), (xb.t[:], "pool", xb)], tmps[it % 2], j)
                for kk in range(8):
                    k.tr(pT.t[:, kk * 128:(kk + 1) * 128], xb.t[:, kk * 128:(kk + 1) * 128], c.ident_b.t[:], r=[xb.b],
                         w=[pT.b] if kk == 0 else (), wp=() if kk == 0 else [pT.b])
                k.cp("act", xt2g.t[:, :, j * 128:(j + 1) * 128], pT.t[:].rearrange("p (k t) -> p k t", k=8), r=[pT.b], wp=[xt2g.b])
                for kk in range(8):
                    pf = pTf[kk // 4]
                    k.tr(pf.t[:, (kk % 4) * 128:(kk % 4 + 1) * 128], XF.t[:, kk * 128:(kk + 1) * 128], c.ident_f.t[:], r=[XF.b],
                         w=[pf.b] if kk % 4 == 0 else (), wp=() if kk % 4 == 0 else [pf.b])
                for hf in range(2):
                    k.cp("act" if hf else "dve", XFT.t[:, hf * 4:(hf + 1) * 4, :], pTf[hf].t[:].rearrange("p (k t) -> p k t", k=4), r=[pTf[hf].b], wp=[XFT.b])
                for kk in range(8):
                    k.mm(pLg.t[:], XFT.t[:, kk, :], RW.t[:, kk, :], kk == 0, kk == 7, r=[XFT.b, RW.b], w=[pLg.b] if kk == 0 else (), wp=() if kk == 0 else [pLg.b])
                L0, MK, EX, EXM, MS = (lg.t[:, 0:E], lg.t[:, E:2 * E], lg.t[:, 2 * E:3 * E], lg.t[:, 3 * E:4 * E], lg.t[:, 4 * E:4 * E + 32])
                k.tt("dve", L0, pLg.t[:], RB.t[:], ALU.add, r=[pLg.b, RB.b], wp=[lg.b])
                P.op("dve", (lambda e, o=MS[:, 0:8], i_=L0: e.max(out=o, in_=i_)), reads=[lg.b], wpart=[lg.b])
                k.ts("dve", MK, L0, MS[:, 3:4], ALU.is_ge, r=[lg.b], wp=[lg.b])
                k.ts("dve", MS[:, 8:9], MS[:, 0:1], -1.0, ALU.mult, r=[lg.b], wp=[lg.b])
                k.act(EX, L0, AF.Exp, r=[lg.b], wp=[lg.b], bias=MS[:, 8:9])
                k.stt(EXM, EX, 1.0, MK, ALU.mult, ALU.mult, r=[lg.b], wp=[lg.b], accum=MS[:, 9:10])
                k.recip(MS[:, 10:11], MS[:, 9:10], r=[lg.b], wp=[lg.b])
                k.ts("dve", EXM, EXM, MS[:, 10:11], ALU.mult, r=[lg.b], wp=[lg.b])
                k.dma("sp", d["COMB"][rows, :], EXM, r=[lg.b], wp=[dCOMB])
                k.dma("sp", d["MK"][rows, :], MK, r=[lg.b], wp=[dCOMB])
                k.dma("sp", d["XM2"][rows, :], xb.t[:], r=[xb.b], wp=[dXT2])
                k.tr(pCT.t[:], EXM, c.ident_f.t[:], r=[lg.b], w=[pCT.b])
                k.cp("act", CT.t[:], pCT.t[:], r=[pCT.b], w=[CT.b])
                for half in range(2):
                    k.mm(pY[half].t[:], CT.t[:], B2t.t[:, half * 512:(half + 1) * 512], True, True, r=[CT.b, B2t.b], w=[pY[half].b])
                    k.cp("act" if half else "dve", ACs.t[:, half * 512:(half + 1) * 512], pY[half].t[:], r=[pY[half].b], wp=[ACs.b])
                k.dma("sp", d["ACCd"][rows, :], ACs.t[:], r=[ACs.b], wp=[dACC])
            k.dma("sp", d["XT2"][:, :, g * 512:(g + 1) * 512], xt2g.t[:], r=[xt2g.b], wp=[dXT2])
        P.end_phase()


def phase_moe(c, layer, MOD5):
    nc, P, k, d, cfg = c.nc, c.P, c.k, c.d, c.cfg
    S, E, MB = cfg.S, cfg.E, cfg.MB
    NTB = MB // 128
    NH = MB // 512
    hin = d["H1"] if layer == 0 else d["H3"]
    hout = d["H2"] if layer == 0 else d["out"]
    with ExitStack() as ph:
        nb1 = (E * 16) // 128
        B1T = sb(ph, nc, [128, E * 16], F32, name="b1t")
        b1r = sb(ph, nc, [128, 128], F32, n=2, name="b1r")
        ACC = sb(ph, nc, [128, NTB, 1024], F32, name="acc")
        XT2b = sb(ph, nc, [128, 8, MB], BF16, name="xt2b")
        CMB = sb(ph, nc, [128, NTB, E], F32, name="cmb")
        W1 = sb(ph, nc, [128, 8, 2048], BF16, n=2, name="w1")
        W2 = sb(ph, nc, [128, 8, 1024], BF16, name="w2")
        ACTT = sb(ph, nc, [128, 8, 512], BF16, n=2, name="actt")
        G1 = sb(ph, nc, [128, 512], F32, n=2, name="g1")
        S1 = sb(ph, nc, [128, 512], F32, n=2, name="s1")
        L1 = sb(ph, nc, [128, 512], F32, n=2, name="l1")
        L2 = sb(ph, nc, [128, 512], F32, n=2, name="l2")
        GS = sb(ph, nc, [128, 512], F32, n=2, name="gs")
        HT = sb(ph, nc, [128, 1024], F32, n=2, name="ht")
        pG = ps(ph, nc, [128, 512], F32, n=2, name="pG")
        pL = ps(ph, nc, [128, 512], F32, n=2, name="pL")
        pO = ps(ph, nc, [128, 512], F32, n=4, name="pO")
        dOUT = Buf("dOUT")
        for i in range(nb1):
            br = b1r[i % 2]
            k.dma("sp", br.t[:], d["moe_b1"][layer, i * 128:(i + 1) * 128, :], w=[br.b])
            k.tr(pG[i % 2].t[:, 0:128], br.t[:], c.ident_f.t[:], r=[br.b], w=[pG[i % 2].b])
            k.cp("dve", B1T.t[:, i * 128:(i + 1) * 128], pG[i % 2].t[:, 0:128], r=[pG[i % 2].b], wp=[B1T.b])
        wi = 0
        io = 0
        for blk in range(S // MB):
            t0 = blk * NTB
            rows = slice(blk * MB, (blk + 1) * MB)
            k.dma("sp", ACC.t[:], d["ACCd"][rows, :].rearrange("(t p) n -> p t n", p=128), w=[ACC.b])
            k.dma("sp", XT2b.t[:], d["XT2"][:, :, rows], w=[XT2b.b])
            k.dma("sp", CMB.t[:], d["COMB"][rows, :].rearrange("(t p) e -> p t e", p=128), w=[CMB.b])
            for e in range(E):
                w1 = W1[wi % 2]
                wi += 1
                w1v = d["moe_w1"][layer, e].rearrange("(k p) n -> p k n", p=128)
                k.dma("pool", w1.t[:, 0:4, :], w1v[:, 0:4, :], wp=[w1.b])
                k.dma("pool", w1.t[:, 4:8, :], w1v[:, 4:8, :], wp=[w1.b])
                w2_loaded = False
                for ht in range(NH):
                    actt = ACTT[io % 2]
                    for pr in range(8):
                        pg, pl = pG[pr % 2], pL[pr % 2]
                        g1, s1, l1, l2, gs = (X[pr % 2] for X in (G1, S1, L1, L2, GS))
                        for kk in range(8):
                            k.mm(pg.t[:], w1.t[:, kk, pr * 128:(pr + 1) * 128], XT2b.t[:, kk, ht * 512:(ht + 1) * 512], kk == 0, kk == 7,
                                 r=[w1.b, XT2b.b], w=[pg.b] if kk == 0 else (), wp=() if kk == 0 else [pg.b])
                        for kk in range(8):
                            k.mm(pl.t[:], w1.t[:, kk, 1024 + pr * 128:1024 + (pr + 1) * 128], XT2b.t[:, kk, ht * 512:(ht + 1) * 512], kk == 0, kk == 7,
                                 r=[w1.b, XT2b.b], w=[pl.b] if kk == 0 else (), wp=() if kk == 0 else [pl.b])
                        bg = B1T.t[:, e * 16 + pr:e * 16 + pr + 1]
                        bl = B1T.t[:, e * 16 + 8 + pr:e * 16 + 8 + pr + 1]
                        k.ts("dve", g1.t[:], pg.t[:], bg, ALU.add, 7.0, ALU.min, r=[pg.b, B1T.b], w=[g1.b])
                        k.act(s1.t[:], g1.t[:], AF.Sigmoid, r=[g1.b], w=[s1.b], scale=1.702)
                        k.act(l1.t[:], pl.t[:], AF.Identity, r=[pl.b, B1T.b], w=[l1.b], bias=bl)
                        k.ts("dve", l2.t[:], l1.t[:], 7.0, ALU.min, -7.0, ALU.max, r=[l1.b], w=[l2.b])
                        k.tt("pool", gs.t[:], g1.t[:], s1.t[:], ALU.mult, r=[g1.b, s1.b], w=[gs.b])
                        k.stt(actt.t[:, pr, :], l2.t[:], 1.0, gs.t[:], ALU.add, ALU.mult, r=[l2.b, gs.b], wp=[actt.b])
                    if not w2_loaded:
                        k.dma("pool", W2.t[:], d["moe_w2"][layer, e].rearrange("(k p) n -> p k n", p=128), w=[W2.b])
                        w2_loaded = True
                    for sub in range(4):
                        tl = ht * 4 + sub
                        for half in range(2):
                            po = pO[io % 4]
                            io += 1
                            for jj in range(8):
                                k.mm(po.t[:], actt.t[:, jj, sub * 128:(sub + 1) * 128], W2.t[:, jj, half * 512:(half + 1) * 512], jj == 0, jj == 7,
                                     r=[actt.b, W2.b], w=[po.b] if jj == 0 else (), wp=() if jj == 0 else [po.b])
                            acc = ACC.t[:, tl, half * 512:(half + 1) * 512]
                            k.stt(acc, po.t[:], CMB.t[:, tl, e:e + 1], acc, ALU.mult, ALU.add, r=[po.b, CMB.b, ACC.b], wp=[ACC.b])
            for tl in range(NTB):
                t = t0 + tl
                ht_ = HT[tl % 2]
                k.dma("sp", ht_.t[:], hin[t * 128:(t + 1) * 128, :], w=[ht_.b])
                k.tt("dve", ACC.t[:, tl, :], ACC.t[:, tl, :], MOD5.t[:], ALU.mult, r=[ACC.b, MOD5.b], wp=[ACC.b])
                k.tt("pool", ht_.t[:], ht_.t[:], ACC.t[:, tl, :], ALU.add, r=[ht_.b, ACC.b], w=[ht_.b])
                k.dma("sp", hout[t * 128:(t + 1) * 128, :], ht_.t[:], r=[ht_.b], wp=[dOUT])
        P.end_phase()


def phase_f(c):
    nc, P, k, d, cfg = c.nc, c.P, c.k, c.d, c.cfg
    S = cfg.S
    NG = S // 512
    with ExitStack() as ph:
        W = sb(ph, nc, [128, 8, 2048], BF16, name="Wc1")
        wv = d["conv_w1"].rearrange("(k p) n -> p k n", p=128)
        k.dma("pool", W.t[:, 0:4, :], wv[:, 0:4, :], wp=[W.b])
        k.dma("pool", W.t[:, 4:8, :], wv[:, 4:8, :], wp=[W.b])
        A = make_A(c, ph, "norm1_g", 1, 1024, c.MOD)
        b1r = sb(ph, nc, [16, 128], F32, name="b1r")
        CB1 = sb(ph, nc, [128, 16], F32, name="cb1")
        pB = ps(ph, nc, [128, 16], F32, name="pB")
        k.dma("sp", b1r.t[:], d["conv_b1"], w=[b1r.b])
        k.tr(pB.t[:], b1r.t[:], c.ident_f.t[0:16, 0:16], r=[b1r.b], w=[pB.b])
        k.cp("dve", CB1.t[:], pB.t[:], r=[pB.b], w=[CB1.b])
        Z = sb(ph, nc, [128, 8, 16], BF16, name="z")
        k.memset("dve", Z.t[:], 0.0, w=[Z.b])
        dGT = Buf("dGT")
        k.dma("sp", d["GT"][:, :, 0:16], Z.t[:], r=[Z.b], wp=[dGT])
        k.dma("sp", d["GT"][:, :, S + 16:S + 32], Z.t[:], r=[Z.b], wp=[dGT])
        XIN = sb(ph, nc, [128, 1024], F32, n=2, name="xin")
        tmps = [{"ss": sb(ph, nc, [128, 16], F32, name="ss"), "t1": sb(ph, nc, [128, 1024], F32, name="t1")} for _ in range(2)]
        XM = sb(ph, nc, [128, 1024], BF16, n=2, name="xm")
        XTG = sb(ph, nc, [128, 8, 512], BF16, n=2, name="xtg")
        SG = sb(ph, nc, [128, 512], F32, n=2, name="sg")
        GTs = sb(ph, nc, [128, 8, 512], BF16, n=2, name="gts")
        pT = ps(ph, nc, [128, 1024], BF16, name="pT")
        pA = ps(ph, nc, [128, 512], F32, n=2, name="pA")
        pGt = ps(ph, nc, [128, 512], F32, n=2, name="pGt")
        it = 0
        for g in range(NG):
            xtg, gts = XTG[g % 2], GTs[g % 2]
            for j in range(4):
                t = 4 * g + j
                xin, xm = XIN[it % 2], XM[it % 2]
                it += 1
                k.dma("sp", xin.t[:], d["H2"][t * 128:(t + 1) * 128, :], w=[xin.b])
                norm_mod(c, xin, A, c.MOD.t[:, 0:1024], c.MOD.b, [(xm.t[:], "pool", xm)], tmps[it % 2], j)
                for kk in range(8):
                    k.tr(pT.t[:, kk * 128:(kk + 1) * 128], xm.t[:, kk * 128:(kk + 1) * 128], c.ident_b.t[:], r=[xm.b],
                         w=[pT.b] if kk == 0 else (), wp=() if kk == 0 else [pT.b])
                k.cp("act", xtg.t[:, :, j * 128:(j + 1) * 128], pT.t[:].rearrange("p (k t) -> p k t", k=8), r=[pT.b], wp=[xtg.b])
            for cp_ in range(8):
                pa, pg, sg = pA[cp_ % 2], pGt[cp_ % 2], SG[cp_ % 2]
                for kk in range(8):
                    k.mm(pa.t[:], W.t[:, kk, cp_ * 128:(cp_ + 1) * 128], xtg.t[:, kk, :], kk == 0, kk == 7, r=[W.b, xtg.b],
                         w=[pa.b] if kk == 0 else (), wp=() if kk == 0 else [pa.b])
                for kk in range(8):
                    k.mm(pg.t[:], W.t[:, kk, 1024 + cp_ * 128:1024 + (cp_ + 1) * 128], xtg.t[:, kk, :], kk == 0, kk == 7, r=[W.b, xtg.b],
                         w=[pg.b] if kk == 0 else (), wp=() if kk == 0 else [pg.b])
                k.act(sg.t[:], pg.t[:], AF.Sigmoid, r=[pg.b, CB1.b], w=[sg.b], bias=CB1.t[:, 8 + cp_:9 + cp_])
                k.stt(gts.t[:, cp_, :], pa.t[:], CB1.t[:, cp_:cp_ + 1], sg.t[:], ALU.add, ALU.mult, r=[pa.b, sg.b, CB1.b], wp=[gts.b])
            k.dma("sp", d["GT"][:, :, 16 + g * 512:16 + (g + 1) * 512], gts.t[:], r=[gts.b], wp=[dGT])
        P.end_phase()


def phase_g1(c):
    nc, P, k, d, cfg = c.nc, c.P, c.k, c.d, c.cfg
    S = cfg.S
    NG = S // 512
    with ExitStack() as ph:
        dwr = sb(ph, nc, [32, 1024], F32, name="dwr")
        DWT = sb(ph, nc, [128, 8, 31], F32, name="dwt")
        dbr = sb(ph, nc, [8, 128], F32, name="dbr")
        DWB = sb(ph, nc, [128, 8], F32, name="dwb")
        DG = sb(ph, nc, [128, 8, 31, 128], BF16, name="dg")
        pD = ps(ph, nc, [128, 8, 32], F32, name="pD")
        pB = ps(ph, nc, [128, 8], F32, name="pB")
        k.dma("sp", dwr.t[0:31, :], d["conv_dw"], w=[dwr.b])
        for cch in range(8):
            k.tr(pD.t[:, cch, 0:31], dwr.t[0:31, cch * 128:(cch + 1) * 128], c.ident_f.t[0:31, 0:31], r=[dwr.b],
                 w=[pD.b] if cch == 0 else (), wp=() if cch == 0 else [pD.b])
        k.cp("dve", DWT.t[:], pD.t[:, :, 0:31], r=[pD.b], w=[DWT.b])
        k.dma("sp", dbr.t[:], d["conv_dw_b"], w=[dbr.b])
        k.tr(pB.t[:], dbr.t[:], c.ident_f.t[0:8, 0:8], r=[dbr.b], w=[pB.b])
        k.cp("dve", DWB.t[:], pB.t[:], r=[pB.b], w=[DWB.b])
        n_ = 0
        for cch in range(8):
            for j in range(31):
                k.ts("dve" if n_ % 2 else "pool", DG.t[:, cch, j, :], c.ident_f.t[:], DWT.t[:, cch, j:j + 1], ALU.mult, r=[DWT.b, c.ident_f.b], wp=[DG.b])
                n_ += 1
        GTw = sb(ph, nc, [128, 8, 544], BF16, n=2, name="gtw")
        Ys = sb(ph, nc, [128, 8, 512], F32, n=2, name="ys")
        pC = ps(ph, nc, [128, 512], F32, n=4, name="pC")
        dYD = Buf("dYD")
        for g in range(NG):
            gtw, ys = GTw[g % 2], Ys[g % 2]
            k.dma("sp", gtw.t[:], d["GT"][:, :, g * 512:g * 512 + 544], w=[gtw.b])
            for cch in range(8):
                pc = pC[cch % 4]
                for j in range(31):
                    k.mm(pc.t[:], DG.t[:, cch, j, :], gtw.t[:, cch, j + 1:j + 513], j == 0, j == 30, r=[DG.b, gtw.b],
                         w=[pc.b] if j == 0 else (), wp=() if j == 0 else [pc.b])
                k.act(ys.t[:, cch, :], pc.t[:], AF.Identity, r=[pc.b, DWB.b], wp=[ys.b], bias=DWB.t[:, cch:cch + 1])
            k.dma("sp", d["YD"][:, :, g * 512:(g + 1) * 512], ys.t[:], r=[ys.b], wp=[dYD])
        P.end_phase()


I32 = mybir.dt.int32


def pool_dma_op(P, fn, reads=(), writes=(), wpart=(), key=None):
    o = Op("pool", fn, P.phase)
    o.is_dma = True
    if key is None:
        key = (list(writes) + list(wpart))[0]
    if key not in P.keymap:
        P.keymap[key] = len(P.keymap)
        assert len(P.keymap) <= NDSEM
    o.key = P.keymap[key]
    P._deps(o, reads, writes, wpart)
    P.ops["pool"].append(o)
    P.order.append(o)
    return o


def phase_route(c, layer, TEi):
    nc, P, k, d, cfg = c.nc, c.P, c.k, c.d, c.cfg
    S, E, NT, NTILE, NSLOT = cfg.S, cfg.E, cfg.NT, cfg.NTILE, cfg.NSLOT
    with ExitStack() as ph:
        MKf = sb(ph, nc, [128, NT, E], F32, name="mkf")
        MKb = sb(ph, nc, [128, NT, E], BF16, name="mkb")
        CMB = sb(ph, nc, [128, NT, E], F32, name="cmb")
        UTf = sb(ph, nc, [128, 128], F32, name="utf")
        UT = sb(ph, nc, [128, 128], BF16, name="ut")
        ONb = sb(ph, nc, [128, 128], BF16, name="onb")
        IOTA = sb(ph, nc, [128, 1], F32, name="iota")
        TH = sb(ph, nc, [1, E * 16], F32, name="th")
        J5 = sb(ph, nc, [1, NTILE * E], F32, name="j5")
        dSLOT, dInit = Buf("dSLOT"), Buf("dInit")
        k.dma("sp", d["SLOT"], d["k_slotinit"], w=[dSLOT, dInit])
        k.dma("sp", MKf.t[:], d["MK"].rearrange("(t p) e -> p t e", p=128), w=[MKf.b])
        k.dma("sp", CMB.t[:], d["COMB"].rearrange("(t p) e -> p t e", p=128), w=[CMB.b])
        k.dma("sp", UTf.t[:], d["k_ut"], w=[UTf.b])
        k.dma("sp", IOTA.t[:], d["k_iota"], w=[IOTA.b])
        k.dma("sp", TH.t[:], d["k_th"], w=[TH.b])
        k.dma("sp", J5.t[:], d["k_j512"], w=[J5.b])
        k.cp("dve", UT.t[:], UTf.t[:], r=[UTf.b], w=[UT.b])
        k.cp("pool", MKb.t[:], MKf.t[:], r=[MKf.b], w=[MKb.b])
        k.memset("dve", ONb.t[:], 1.0, w=[ONb.b])
        pC = ps(ph, nc, [1, E], F32, name="pC")
        pSB = ps(ph, nc, [128, E], F32, name="pSB")
        pR = ps(ph, nc, [128, E], F32, n=2, name="pR")
        V = sb(ph, nc, [1, 8 * E], F32, name="v")
        C16 = sb(ph, nc, [1, E * 16], F32, name="c16")
        CJ = sb(ph, nc, [1, NTILE * E], F32, name="cj")
        TEf = sb(ph, nc, [1, NTILE], F32, name="tef")
        SEGB = sb(ph, nc, [128, E], F32, name="segb")
        for t in range(NT):
            k.mm(pC.t[:], ONb.t[:, 0:1], MKb.t[:, t, :], t == 0, t == NT - 1, r=[ONb.b, MKb.b], w=[pC.b] if t == 0 else (), wp=() if t == 0 else [pC.b])
        cnt, ntl, c512, inc, segs, one = (V.t[:, i * E:(i + 1) * E] for i in range(6))
        k.cp("dve", cnt, pC.t[:], r=[pC.b], wp=[V.b])
        k.tt("dve", C16.t[:].rearrange("o (e m) -> o e m", m=16), cnt.unsqueeze(2).to_broadcast([1, E, 16]), TH.t[:].rearrange("o (e m) -> o e m", m=16), ALU.is_gt,
             r=[V.b, TH.b], w=[C16.b])
        k.red(ntl, C16.t[:].rearrange("o (e m) -> o e m", m=16), r=[C16.b], wp=[V.b])
        k.ts("dve", c512, ntl, 512.0, ALU.mult, r=[V.b], wp=[V.b])
        k.memset("dve", one, 1.0, wp=[V.b])
        P.op("dve", (lambda e_, o=inc, a=one, b_=c512: e_.tensor_tensor_scan(out=o, data0=a, data1=b_, initial=0.0, op0=ALU.mult, op1=ALU.add)),
             reads=[V.b], wpart=[V.b])
        k.tt("dve", segs, inc, c512, ALU.subtract, r=[V.b], wp=[V.b])
        k.tt("dve", CJ.t[:].rearrange("o (j e) -> o j e", e=E), segs.unsqueeze(1).to_broadcast([1, NTILE, E]), J5.t[:].rearrange("o (j e) -> o j e", e=E), ALU.is_le,
             r=[V.b, J5.b], w=[CJ.b])
        k.red(TEf.t[:], CJ.t[:].rearrange("o (j e) -> o j e", e=E), r=[CJ.b], w=[TEf.b])
        k.ts("dve", TEf.t[:], TEf.t[:], -1.0, ALU.add, 0.0, ALU.max, r=[TEf.b], w=[TEf.b])
        IDXW, IDXB = TEi
        KP = sb(ph, nc, [128, 9], F32, name="kp")
        k.dma("sp", KP.t[:], d["k_kp"], w=[KP.b])
        pTE = ps(ph, nc, [128, NTILE], F32, name="pTE")
        TEb = sb(ph, nc, [128, NTILE], F32, name="teb")
        XW = sb(ph, nc, [128, NTILE, 8], F32, name="xw")
        k.mm(pTE.t[:], c.ones_f.t[0:1, :], TEf.t[:], True, True, r=[TEf.b, c.ones_f.b], w=[pTE.b])
        k.cp("dve", TEb.t[:], pTE.t[:], r=[pTE.b], w=[TEb.b])
        for kk in range(8):
            k.ts("dve", XW.t[:, :, kk], TEb.t[:], 1024.0, ALU.mult, KP.t[:, kk:kk + 1], ALU.add, r=[TEb.b, KP.b], wp=[XW.b])
        if layer:
            k.ts("dve", XW.t[:], XW.t[:], float(layer * E * 1024), ALU.add, r=[XW.b], w=[XW.b])
        k.cp("dve", IDXW.t[:], XW.t[:], r=[XW.b], w=[IDXW.b])
        k.ts("dve", TEb.t[:], TEb.t[:], 16.0, ALU.mult, KP.t[:, 8:9], ALU.add, r=[TEb.b, KP.b], w=[TEb.b])
        if layer:
            k.ts("dve", TEb.t[:], TEb.t[:], float(layer * E * 16), ALU.add, r=[TEb.b], w=[TEb.b])
        k.cp("dve", IDXB.t[:], TEb.t[:], r=[TEb.b], w=[IDXB.b])
        k.mm(pSB.t[:], c.ones_f.t[0:1, :], segs, True, True, r=[V.b, c.ones_f.b], w=[pSB.b])
        k.cp("dve", SEGB.t[:], pSB.t[:], r=[pSB.b], w=[SEGB.b])
        POS = sb(ph, nc, [128, E], F32, n=2, name="pos")
        T8 = sb(ph, nc, [128, 8], F32, n=2, name="t8")
        OH = sb(ph, nc, [128, E], F32, n=2, name="oh")
        JK = sb(ph, nc, [128, E], F32, n=2, name="jk")
        P4 = sb(ph, nc, [128, 4], F32, n=2, name="p4")
        P4i = sb(ph, nc, [128, 4], I32, n=2, name="p4i")
        SR = sb(ph, nc, [128, 4, 2], F32, n=2, name="sr")
        for i in range(NT):
            pr = pR[i % 2]
            pos, t8, p4, p4i, sr = POS[i % 2], T8[i % 2], P4[i % 2], P4i[i % 2], SR[i % 2]
            for ip in range(i):
                k.mm(pr.t[:], ONb.t[:], MKb.t[:, ip, :], ip == 0, False, r=[ONb.b, MKb.b], w=[pr.b] if ip == 0 else (), wp=() if ip == 0 else [pr.b])
            k.mm(pr.t[:], UT.t[:], MKb.t[:, i, :], i == 0, True, r=[UT.b, MKb.b], w=[pr.b] if i == 0 else (), wp=() if i == 0 else [pr.b])
            k.tt("dve", pos.t[:], pr.t[:], SEGB.t[:], ALU.add, r=[pr.b, SEGB.b], w=[pos.b])
            P.op("dve", (lambda e_, o=t8.t[:], a=CMB.t[:, i, :]: e_.max(out=o, in_=a)), reads=[CMB.b], writes=[t8.b])
            for kq in range(4):
                oh, jk = OH[kq % 2], JK[kq % 2]
                k.ts("dve", oh.t[:], CMB.t[:, i, :], t8.t[:, kq:kq + 1], ALU.is_equal, r=[CMB.b, t8.b], w=[oh.b])
                k.stt(jk.t[:], oh.t[:], 1.0, pos.t[:], ALU.mult, ALU.mult, r=[oh.b, pos.b], w=[jk.b], wp=[p4.b], accum=p4.t[:, kq:kq + 1])
                k.ts("pool", sr.t[:, kq, 0:1], IOTA.t[:], float(i * 128), ALU.add, r=[IOTA.b], wp=[sr.b])
                k.cp("pool", sr.t[:, kq, 1:2], t8.t[:, kq:kq + 1], r=[t8.b], wp=[sr.b])
            k.ts("dve", p4.t[:], p4.t[:], float(NSLOT - 1), ALU.min, r=[p4.b], w=[p4.b])
            k.cp("dve", p4i.t[:], p4.t[:], r=[p4.b], w=[p4i.b])
            for kq in range(4):
                def sca(e_, off=p4i.t[:, kq:kq + 1], src=sr.t[:, kq, :]):
                    return e_.indirect_dma_start(out=d["SLOT"], out_offset=bass.IndirectOffsetOnAxis(ap=off, axis=0), in_=src, in_offset=None)
                pool_dma_op(P, sca, reads=[p4i.b, sr.b, dInit], wpart=[dSLOT])
        P.end_phase()


def phase_smoe(c, layer, MOD5, TEi):
    nc, P, k, d, cfg = c.nc, c.P, c.k, c.d, c.cfg
    S, E, NTILE = cfg.S, cfg.E, cfg.NTILE
    IDXW, IDXB = TEi
    hin = d["H1"] if layer == 0 else d["H3"]
    hout = d["H2"] if layer == 0 else d["out"]
    w1tab = d["moe_w1"].rearrange("l e k n -> (l e k) n")
    w2tab = d["moe_w2"].rearrange("l e k n -> (l e k) n")
    b1tab = d["moe_b1"].rearrange("l r f -> (l r) f")
    with ExitStack() as ph:
        Z = sb(ph, nc, [128, 1024], BF16, name="z")
        W1 = sb(ph, nc, [128, 8, 2048], BF16, n=2, name="w1")
        W2 = sb(ph, nc, [128, 8, 1024], BF16, name="w2")
        B1r = sb(ph, nc, [128, 128], F32, n=2, name="b1r")
        B1c = sb(ph, nc, [128, 16], F32, n=2, name="b1c")
        SLt = sb(ph, nc, [128, 4, 2], F32, n=2, name="slt")
        TKi = sb(ph, nc, [128, 4], I32, n=2, name="tki")
        XG = sb(ph, nc, [128, 4, 1024], BF16, n=2, name="xg")
        XT = sb(ph, nc, [128, 8, 512], BF16, n=2, name="xt")
        ACTT = sb(ph, nc, [128, 8, 512], BF16, n=2, name="actt")
        G1 = sb(ph, nc, [128, 512], F32, n=2, name="g1")
        S1 = sb(ph, nc, [128, 512], F32, n=2, name="s1")
        L1 = sb(ph, nc, [128, 512], F32, n=2, name="l1")
        L2 = sb(ph, nc, [128, 512], F32, n=2, name="l2")
        GS = sb(ph, nc, [128, 512], F32, n=2, name="gs")
        OS = sb(ph, nc, [128, 1024], F32, n=4, name="os")
        HT = sb(ph, nc, [128, 1024], F32, n=2, name="ht")
        AT = sb(ph, nc, [128, 1024], F32, n=2, name="at")
        pT = ps(ph, nc, [128, 1024], BF16, n=2, name="pT")
        pG = ps(ph, nc, [128, 512], F32, n=2, name="pG")
        pL = ps(ph, nc, [128, 512], F32, n=2, name="pL")
        pO = ps(ph, nc, [128, 512], F32, n=2, name="pO")
        dACC, dXM2z, dOUT = Buf("dACCs"), Buf("dXM2z"), Buf("dOUT")
        k.memset("dve", Z.t[:], 0.0, w=[Z.b])
        k.dma("sp", d["XM2"][S:S + 128, :], Z.t[:], r=[Z.b], w=[dXM2z])

        def gather(out_ap, tab, idx_ap, reads, wslot, part=False):
            def g(e_):
                return e_.indirect_dma_start(out=out_ap, out_offset=None, in_=tab, in_offset=bass.IndirectOffsetOnAxis(ap=idx_ap, axis=0))
            return pool_dma_op(P, g, reads=reads, writes=() if part else [wslot], wpart=[wslot] if part else ())

        def loads(j):
            w1, b1r, slt, tki, xg = W1[j % 2], B1r[j % 2], SLt[j % 2], TKi[j % 2], XG[j % 2]
            k.dma("sp", slt.t[:], d["SLOT"][j * 512:(j + 1) * 512, :].rearrange("(s p) c -> p s c", p=128), w=[slt.b])
            k.cp("dve", tki.t[:], slt.t[:, :, 0], r=[slt.b], w=[tki.b])
            for kk in range(8):
                gather(w1.t[:, kk, :], w1tab, IDXW.t[:, j, kk:kk + 1], [IDXW.b], w1.b, part=True)
            gather(b1r.t[:], b1tab, IDXB.t[:, j:j + 1], [IDXB.b], b1r.b)
            for sub in range(4):
                gather(xg.t[:, sub, :], d["XM2"], tki.t[:, sub:sub + 1], [tki.b, dXM2z], xg.b, part=True)

        io = 0
        loads(0)
        for j in range(NTILE):
            if j + 1 < NTILE:
                loads(j + 1)
            w1, b1r, b1c, slt, tki, xg, xt, actt = (X[j % 2] for X in (W1, B1r, B1c, SLt, TKi, XG, XT, ACTT))
            k.tr(pG[0].t[:, 0:16], b1r.t[0:16, :], c.ident_f.t[0:16, 0:16], r=[b1r.b], w=[pG[0].b])
            k.cp("dve", b1c.t[:], pG[0].t[:, 0:16], r=[pG[0].b], w=[b1c.b])
            for sub in range(4):
                pt = pT[sub % 2]
                for kk in range(8):
                    k.tr(pt.t[:, kk * 128:(kk + 1) * 128], xg.t[:, sub, kk * 128:(kk + 1) * 128], c.ident_b.t[:], r=[xg.b],
                         w=[pt.b] if kk == 0 else (), wp=() if kk == 0 else [pt.b])
                k.cp("act" if sub % 2 else "dve", xt.t[:, :, sub * 128:(sub + 1) * 128], pt.t[:].rearrange("p (k t) -> p k t", k=8), r=[pt.b], wp=[xt.b])
            for pr in range(8):
                pg, pl = pG[pr % 2], pL[pr % 2]
                g1, s1, l1, l2, gs = (X[pr % 2] for X in (G1, S1, L1, L2, GS))
                for kk in range(8):
                    k.mm(pg.t[:], w1.t[:, kk, pr * 128:(pr + 1) * 128], xt.t[:, kk, :], kk == 0, kk == 7,
                         r=[w1.b, xt.b], w=[pg.b] if kk == 0 else (), wp=() if kk == 0 else [pg.b])
                for kk in range(8):
                    k.mm(pl.t[:], w1.t[:, kk, 1024 + pr * 128:1024 + (pr + 1) * 128], xt.t[:, kk, :], kk == 0, kk == 7,
                         r=[w1.b, xt.b], w=[pl.b] if kk == 0 else (), wp=() if kk == 0 else [pl.b])
                k.ts("dve", g1.t[:], pg.t[:], b1c.t[:, pr:pr + 1], ALU.add, 7.0, ALU.min, r=[pg.b, b1c.b], w=[g1.b])
                k.act(s1.t[:], g1.t[:], AF.Sigmoid, r=[g1.b], w=[s1.b], scale=1.702)
                k.act(l1.t[:], pl.t[:], AF.Identity, r=[pl.b, b1c.b], w=[l1.b], bias=b1c.t[:, 8 + pr:9 + pr])
                k.ts("dve", l2.t[:], l1.t[:], 7.0, ALU.min, -7.0, ALU.max, r=[l1.b], w=[l2.b])
                k.tt("dve", gs.t[:], g1.t[:], s1.t[:], ALU.mult, r=[g1.b, s1.b], w=[gs.b])
                k.stt(actt.t[:, pr, :], l2.t[:], 1.0, gs.t[:], ALU.add, ALU.mult, r=[l2.b, gs.b], wp=[actt.b])
            for kk in range(8):
                gather(W2.t[:, kk, :], w2tab, IDXW.t[:, j, kk:kk + 1], [IDXW.b], W2.b, part=True)
            for sub in range(4):
                os_ = OS[(4 * j + sub) % 4]
                for half in range(2):
                    po = pO[io % 2]
                    io += 1
                    for jj in range(8):
                        k.mm(po.t[:], actt.t[:, jj, sub * 128:(sub + 1) * 128], W2.t[:, jj, half * 512:(half + 1) * 512], jj == 0, jj == 7,
                             r=[actt.b, W2.b], w=[po.b] if jj == 0 else (), wp=() if jj == 0 else [po.b])
                    k.act(os_.t[:, half * 512:(half + 1) * 512], po.t[:], AF.Copy, r=[po.b, slt.b], wp=[os_.b], scale=slt.t[:, sub, 1:2])

                def sca(e_, off=tki.t[:, sub:sub + 1], src=os_.t[:]):
                    return e_.indirect_dma_start(out=d["ACCd"], out_offset=bass.IndirectOffsetOnAxis(ap=off, axis=0), in_=src, in_offset=None,
                                                 compute_op=ALU.add)
                pool_dma_op(P, sca, reads=[tki.b, os_.b], writes=[dACC])
        for t in range(S // 128):
            ht_, at_ = HT[t % 2], AT[t % 2]
            rows = slice(t * 128, (t + 1) * 128)
            k.dma("sp", ht_.t[:], hin[rows, :], w=[ht_.b])
            k.dma("sp", at_.t[:], d["ACCd"][rows, :], r=[dACC], w=[at_.b])
            k.tt("dve", at_.t[:], at_.t[:], MOD5.t[:], ALU.mult, r=[at_.b, MOD5.b], w=[at_.b])
            k.tt("pool", ht_.t[:], ht_.t[:], at_.t[:], ALU.add, r=[ht_.b, at_.b], w=[ht_.b])
            k.dma("sp", hout[rows, :], ht_.t[:], r=[ht_.b], wp=[dOUT])
        P.end_phase()
```

```python
from contextlib import ExitStack
import numpy as np
import ml_dtypes
import concourse.bass as bass
import concourse.mybir as mybir
from concourse.bass_utils import run_bass_kernel_spmd

F32 = mybir.dt.float32
BF16 = mybir.dt.bfloat16
AF = mybir.ActivationFunctionType
ALU = mybir.AluOpType
AX = mybir.AxisListType

COMPUTE = ("pe", "act", "dve", "pool")
ALLENG = ("pe", "act", "dve", "pool", "sp")
NDSEM = 72


class Buf:
    __slots__ = ("name", "writers", "readers")

    def __init__(self, name):
        self.name = name
        self.writers = []
        self.readers = []


class Op:
    __slots__ = ("eng", "fn", "raw", "oth", "signal", "tok_sem", "tok_val", "is_dma", "key", "phase")

    def __init__(self, eng, fn, phase):
        self.eng = eng
        self.fn = fn
        self.raw = []
        self.oth = []
        self.signal = False
        self.tok_sem = None
        self.tok_val = 0
        self.is_dma = False
        self.key = None
        self.phase = phase


class Prog:
    def __init__(self, nc, es):
        self.nc = nc
        self.phase = 0
        self.esem = {e: es.enter_context(nc.semaphore("s_" + e)) for e in COMPUTE}
        self.ecnt = {e: 0 for e in COMPUTE}
        self.dsem = [es.enter_context(nc.semaphore("d%d" % i)) for i in range(NDSEM)]
        self.dcnt = [0] * NDSEM
        self.seen = {e: {} for e in ALLENG}
        self._reset()
        self.nops = 0

    def _reset(self):
        self.ops = {e: [] for e in ALLENG}
        self.order = []
        self.keymap = {}
        self.last = {}

    def buf(self, name="b"):
        return Buf(name)

    def _deps(self, op, reads, writes, wpart):
        ph = self.phase
        for b in reads:
            for w in b.writers:
                if w.phase == ph:
                    op.raw.append(w)
        for b in list(writes) + list(wpart):
            for r in b.readers:
                if r.phase == ph:
                    op.oth.append(r)
        for b in writes:
            for w in b.writers:
                if w.phase == ph:
                    op.oth.append(w)
        for b in reads:
            b.readers.append(op)
        for b in writes:
            b.writers = [op]
            b.readers = []
        for b in wpart:
            if b.readers:
                b.writers = [op]
                b.readers = []
            else:
                b.writers.append(op)

    def op(self, eng, fn, reads=(), writes=(), wpart=()):
        o = Op(eng, fn, self.phase)
        self._deps(o, reads, writes, wpart)
        self.ops[eng].append(o)
        self.order.append(o)
        self.last[eng] = o
        return o

    def dma(self, q, out, in_, reads=(), writes=(), wpart=(), key=None, **kw):
        def fn(e, out=out, in_=in_, kw=kw):
            return e.dma_start(out=out, in_=in_, **kw)
        o = Op(q, fn, self.phase)
        o.is_dma = True
        if key is None:
            ws = list(writes) + list(wpart)
            key = ws[0]
        if key not in self.keymap:
            self.keymap[key] = len(self.keymap)
            assert len(self.keymap) <= NDSEM, "too many DMA keys in phase"
        o.key = self.keymap[key]
        self._deps(o, reads, writes, wpart)
        self.ops[q].append(o)
        self.order.append(o)
        return o

    def end_phase(self):
        nc = self.nc
        lasts = [self.last[e] for e in COMPUTE if e in self.last]
        lastd = {}
        for o in self.order:
            if o.is_dma:
                lastd[o.key] = o
        for e in ALLENG:
            o = Op(e, (lambda eng: eng.nop()), self.phase)
            o.raw = list(lasts) + list(lastd.values())
            self.ops[e].append(o)
            self.order.append(o)
        for o in self.order:
            for d in o.raw:
                if d.is_dma:
                    continue
                if d.eng == o.eng and o.eng == "pe" and not o.is_dma:
                    continue
                d.signal = True
            for d in o.oth:
                if d.is_dma:
                    continue
                if d.eng == o.eng and not o.is_dma:
                    continue
                d.signal = True
        for e in COMPUTE:
            for o in self.ops[e]:
                if o.is_dma:
                    continue
                if o.signal:
                    self.ecnt[e] += 1
                    o.tok_sem = self.esem[e]
                    o.tok_val = self.ecnt[e]
        for o in self.order:
            if o.is_dma:
                self.dcnt[o.key] += 16
                o.tok_sem = self.dsem[o.key]
                o.tok_val = self.dcnt[o.key]
        self.nops += len(self.order)

        with nc.Block() as block:
            def run(ename):
                def body(eng):
                    seen = self.seen[ename]
                    for o in self.ops[ename]:
                        need = {}
                        for d in o.raw:
                            if d.tok_sem is None:
                                continue
                            if (not d.is_dma) and d.eng == ename and ename == "pe" and not o.is_dma:
                                continue
                            s = d.tok_sem
                            if need.get(s.num, (None, 0))[1] < d.tok_val:
                                need[s.num] = (s, d.tok_val)
                        for d in o.oth:
                            if d.tok_sem is None:
                                continue
                            if (not d.is_dma) and d.eng == ename and not o.is_dma:
                                continue
                            s = d.tok_sem
                            if need.get(s.num, (None, 0))[1] < d.tok_val:
                                need[s.num] = (s, d.tok_val)
                        for s, v in need.values():
                            if seen.get(s.num, 0) < v:
                                eng.wait_ge(s, v)
                                seen[s.num] = v
                        ins = o.fn(eng)
                        if o.is_dma:
                            ins.then_inc(o.tok_sem, 16)
                        elif o.signal:
                            ins.then_inc(o.tok_sem, 1)
                return body

            block.tensor(run("pe"))
            block.scalar(run("act"))
            block.vector(run("dve"))
            block.gpsimd(run("pool"))
            block.sync(run("sp"))
        self.phase += 1
        self._reset()


class Slot:
    __slots__ = ("t", "b")

    def __init__(self, t, b):
        self.t = t
        self.b = b


class Ctx:
    pass


class K:
    def __init__(self, P):
        self.P = P

    def ts(self, eng, out, in0, s1, op0, s2=None, op1=None, r=(), w=(), wp=(), accum=None):
        if op1 is None:
            if accum is None:
                f = lambda e: e.tensor_scalar(out=out, in0=in0, scalar1=s1, scalar2=None, op0=op0)
            else:
                f = lambda e: e.tensor_scalar(out=out, in0=in0, scalar1=s1, scalar2=None, op0=op0, accum_out=accum)
        else:
            f = lambda e: e.tensor_scalar(out=out, in0=in0, scalar1=s1, scalar2=s2, op0=op0, op1=op1)
        return self.P.op(eng, f, reads=r, writes=w, wpart=wp)

    def tt(self, eng, out, in0, in1, op, r=(), w=(), wp=()):
        return self.P.op(eng, lambda e: e.tensor_tensor(out=out, in0=in0, in1=in1, op=op), reads=r, writes=w, wpart=wp)

    def stt(self, out, in0, scalar, in1, op0, op1, r=(), w=(), wp=(), accum=None):
        if accum is None:
            f = lambda e: e.scalar_tensor_tensor(out=out, in0=in0, scalar=scalar, in1=in1, op0=op0, op1=op1)
        else:
            f = lambda e: e.scalar_tensor_tensor(out=out, in0=in0, scalar=scalar, in1=in1, op0=op0, op1=op1, accum_out=accum)
        return self.P.op("dve", f, reads=r, writes=w, wpart=wp)

    def act(self, out, in_, func, r=(), w=(), wp=(), bias=None, scale=None, accum=None):
        kw = {}
        if bias is not None:
            kw["bias"] = bias
        if scale is not None:
            kw["scale"] = scale
        if accum is not None:
            kw["accum_out"] = accum
        return self.P.op("act", lambda e: e.activation(out=out, in_=in_, func=func, **kw), reads=r, writes=w, wpart=wp)

    def cp(self, eng, out, in_, r=(), w=(), wp=()):
        if eng == "act":
            return self.P.op("act", lambda e: e.copy(out=out, in_=in_), reads=r, writes=w, wpart=wp)
        return self.P.op(eng, lambda e: e.tensor_copy(out=out, in_=in_), reads=r, writes=w, wpart=wp)

    def memset(self, eng, ap, val, w=(), wp=()):
        return self.P.op(eng, lambda e: e.memset(ap, val), writes=w, wpart=wp)

    def mm(self, out, lhsT, rhs, start, stop, r=(), w=(), wp=()):
        return self.P.op("pe", lambda e: e.matmul(out, lhsT, rhs, start=start, stop=stop), reads=r, writes=w, wpart=wp)

    def tr(self, out, in_, ident, r=(), w=(), wp=()):
        return self.P.op("pe", lambda e: e.transpose(out, in_, ident), reads=r, writes=w, wpart=wp)

    def red(self, out, in_, r=(), w=(), wp=()):
        return self.P.op("dve", lambda e: e.tensor_reduce(out=out, in_=in_, axis=AX.X, op=ALU.add), reads=r, writes=w, wpart=wp)

    def recip(self, out, in_, r=(), w=(), wp=()):
        return self.P.op("dve", lambda e: e.reciprocal(out=out, in_=in_), reads=r, writes=w, wpart=wp)

    def dma(self, q, out, in_, r=(), w=(), wp=(), key=None):
        return self.P.dma(q, out, in_, reads=r, writes=w, wpart=wp, key=key)


class Cfg:
    def __init__(self, S=8192, L=256, E=32, debug=False, stop_after=None):
        self.S, self.L, self.E = S, L, E
        self.D = 1024
        self.NT = S // 128
        self.ROWS = S // 64
        self.NCH = S // 64
        self.MB = min(1024, S)
        self.NTILE = (4 * S) // 512 + E
        self.NSLOT = self.NTILE * 512
        self.debug = debug
        self.dense = False
        self.stop_after = stop_after


EPS = 1e-6
NEG = -30000.0


def host_consts(cfg):
    S = cfg.S
    NT = cfg.NT
    cs = {}
    cs["ident_f"] = np.eye(128, dtype=np.float32)
    p = np.arange(128)
    t = np.arange(64)
    s = p % 64
    cs["trif"] = (s[:, None] <= t[None, :]).astype(np.float32)
    cs["trib"] = (s[:, None] >= t[None, :]).astype(np.float32)
    r = np.ones((128, 512), np.float32)
    r[:, ::64] = 0.0
    cs["reset"] = r
    inv = (10000.0 ** (-np.arange(16, dtype=np.float32) / 16.0)).astype(np.float32)
    tt = np.arange(NT)
    row = (2 * tt[None, :] + (p[:, None] // 64)).astype(np.float32)
    col = np.broadcast_to((p % 64).astype(np.float32)[:, None], (128, NT))
    ang = np.stack([row[:, :, None] * inv[None, None, :], col[:, :, None] * inv[None, None, :]], axis=2)
    ang = ang.astype(np.float32)
    E, NTILE = cfg.E, cfg.NTILE
    cs["ut"] = (p[:, None] < p[None, :]).astype(np.float32)
    cs["iota"] = p.astype(np.float32).reshape(128, 1)
    kp = np.zeros((128, 9), np.float32)
    kp[:, :8] = np.arange(8)[None, :] * 128 + p[:, None]
    kp[:, 8] = p % 16
    cs["kp"] = kp
    cs["th"] = np.broadcast_to((512.0 * np.arange(16, dtype=np.float32))[None, None, :], (1, E, 16)).reshape(1, E * 16).copy()
    cs["j512"] = np.broadcast_to((512.0 * np.arange(NTILE, dtype=np.float32))[None, :, None], (1, NTILE, E)).reshape(1, NTILE * E).copy()
    si = np.zeros((cfg.NSLOT, 2), np.float32)
    si[:, 0] = S + (np.arange(cfg.NSLOT) % 128)
    cs["slotinit"] = si
    cs["cos"] = np.cos(ang).astype(np.float32).reshape(128, NT * 32)
    cs["sin"] = np.sin(ang).astype(np.float32).reshape(128, NT * 32)
    return cs


def layout_rpb(rpb):
    H = rpb.shape[0]
    c = np.arange(64)[:, None]
    kc = np.arange(64)[None, :]
    win = np.clip(c - 8, 0, 48)
    valid = (kc >= win) & (kc < win + 16)
    idx = np.clip(kc - c + 15, 0, 30)
    g = rpb[:, :, idx]
    g = np.where(valid[None, None], g, np.float32(NEG)).astype(np.float32)
    return np.ascontiguousarray(g.transpose(2, 0, 1, 3)).reshape(64, H * 15 * 64)


_uid = [0]


def sb(es, nc, shape, dt, n=1, name="t"):
    out = []
    for i in range(n):
        _uid[0] += 1
        t = es.enter_context(nc.sbuf_tensor("%s_%d" % (name, _uid[0]), list(shape), dt))
        out.append(Slot(t, Buf(name)))
    return out if n > 1 else out[0]


def ps(es, nc, shape, dt, n=1, name="p"):
    out = []
    for i in range(n):
        _uid[0] += 1
        t = es.enter_context(nc.psum_tensor("%s_%d" % (name, _uid[0]), list(shape), dt))
        out.append(Slot(t, Buf(name)))
    return out if n > 1 else out[0]


def declare_io(nc, cfg):
    S, L, E = cfg.S, cfg.L, cfg.E
    d = {}

    inputs = set()
    d["_inputs"] = inputs

    def inp(name, shape, dt=F32):
        d[name] = nc.dram_tensor(name, list(shape), dt, kind="ExternalInput").ap()
        inputs.add(name)

    inp("x", [S, 1024]); inp("c", [8, 128]); inp("ctx", [L, 1024]); inp("c_ctx", [8, 128])
    inp("ada_w", [2, 1024, 6144]); inp("ada_b", [2, 6144]); inp("norm1_g", [2, 1024]); inp("norm2_g", [2, 1024])
    inp("ab_w_in", [1024, 4096]); inp("ab_w_out", [1024, 1024]); inp("nat_q_norm", [1, 64]); inp("nat_k_norm", [1, 64])
    inp("rpb_full", [64, 8 * 15 * 64]); inp("hgrn_lb", [16, 128]); inp("hgrn_o_norm", [1, 128])
    inp("conv_w1", [1024, 2048]); inp("conv_b1", [16, 128]); inp("conv_dw", [31, 1024]); inp("conv_dw_b", [8, 128])
    inp("conv_ln_g", [8, 128]); inp("conv_ln_b", [8, 128]); inp("conv_w2", [1024, 1024]); inp("conv_b2", [1, 1024])
    inp("router_w", [2, 1024, E]); inp("router_b", [2, E])
    inp("moe_w1", [2, E, 1024, 2048]); inp("moe_b1", [2, E * 16, 128]); inp("moe_w2", [2, E, 1024, 1024]); inp("moe_b2", [2, E, 1024])
    inp("k_ident_f", [128, 128]); inp("k_trif", [128, 64]); inp("k_trib", [128, 64]); inp("k_reset", [128, 512])
    inp("k_cos", [128, cfg.NT * 32]); inp("k_sin", [128, cfg.NT * 32])
    inp("k_ut", [128, 128]); inp("k_iota", [128, 1]); inp("k_th", [1, E * 16]); inp("k_j512", [1, cfg.NTILE * E])
    inp("k_slotinit", [cfg.NSLOT, 2]); inp("k_kp", [128, 9])
    d["out"] = nc.dram_tensor("out", [S, 1024], F32, kind="ExternalOutput").ap()
    kind = "ExternalOutput" if cfg.debug else "Internal"

    def scr(name, shape, dt):
        d[name] = nc.dram_tensor(name, list(shape), dt, kind=kind).ap()

    scr("XT", [128, 8, S], BF16); scr("XTc", [128, 8, L], BF16)
    scr("QTr", [128, 4, S], BF16); scr("QTf", [128, 4, S], BF16); scr("KTr", [128, 4, S], BF16)
    scr("KcT", [128, 4, L], BF16)
    scr("VA", [S, 520], BF16); scr("VcA", [L, 520], BF16)
    scr("VH", [S, 512], BF16); scr("VHc", [L, 512], BF16)
    scr("G", [S, 512], F32)
    scr("HQ", [2, 128, 4, S], BF16); scr("HK", [2, 128, 4, S], BF16)
    scr("KH", [2, S, 512], BF16); scr("KHc", [2, L, 512], BF16)
    scr("DEC", [2, 128, 4, S // 64], F32); scr("DECc", [2, 128, 4, L // 64], F32)
    scr("OF", [S, 512], F32)
    scr("CAT", [S, 1024], BF16)
    scr("H1", [S, 1024], F32); scr("H2", [S, 1024], F32); scr("H3", [S, 1024], F32)
    scr("XT2", [128, 8, S], BF16)
    scr("COMB", [S, E], F32)
    scr("ACC0", [S, 1024], F32)
    scr("GT", [128, 8, S + 32], BF16)
    scr("YD", [128, 8, S], F32)
    scr("XM2", [S + 128, 1024], BF16)
    scr("MK", [S, E], F32)
    scr("ACCd", [S + 128, 1024], F32)
    scr("SLOT", [cfg.NSLOT, 2], F32)
    return d


def build_program(cfg):
    nc = bass.Bass("TRN2", target_bir_lowering=False)
    c = Ctx()
    c.nc, c.cfg = nc, cfg
    c.d = declare_io(nc, cfg)
    c.inputs = c.d.pop("_inputs")
    with ExitStack() as ges:
        P = Prog(nc, ges)
        c.P = P
        c.k = K(P)
        c.ident_f = sb(ges, nc, [128, 128], F32, name="identf")
        c.ident_b = sb(ges, nc, [128, 128], BF16, name="identb")
        c.ones_f = sb(ges, nc, [128, 128], F32, name="onesf")
        c.k.dma("sp", c.ident_f.t[:], c.d["k_ident_f"], w=[c.ident_f.b])
        c.k.cp("dve", c.ident_b.t[:], c.ident_f.t[:], r=[c.ident_f.b], w=[c.ident_b.b])
        c.k.memset("dve", c.ones_f.t[:], 1.0, w=[c.ones_f.b])
        touch = sb(ges, nc, [1, 64], F32, name="touch")
        for i, nm in enumerate(sorted(c.inputs)):
            ap = c.d[nm]
            idx = tuple([0] * (len(ap.shape) - 2) + [slice(0, 1), slice(0, 1)])
            c.k.dma("sp", touch.t[0:1, i:i + 1], ap[idx], wp=[touch.b])
        if cfg.debug:
            tb = sb(ges, nc, [1, 2], BF16, name="touchb")
            c.k.memset("dve", touch.t[0:1, 62:64], 0.0, wp=[touch.b])
            c.k.memset("dve", tb.t[:], 0.0, w=[tb.b])
            for i, (nm, ap) in enumerate(c.d.items()):
                if nm not in c.inputs:
                    idx = tuple([0] * (len(ap.shape) - 2) + [slice(0, 1), slice(0, 1)])
                    src = tb.t[0:1, 0:1] if ap.dtype == BF16 else touch.t[0:1, 63:64]
                    c.k.dma("sp", ap[idx], src, r=[tb.b, touch.b], w=[Buf("o")])
        P.end_phase()
        def dbg_stop(name):
            return cfg.stop_after == name

        done = False
        for layer in (0, 1):
            with ExitStack() as lay:
                c.MOD = sb(lay, nc, [128, 6144], F32, name="MOD")
                if layer == 0:
                    c.CMOD = sb(lay, nc, [128, 2048], F32, name="CMOD")
                    seq = [("mods0", lambda: phase_mods(c, 0)), ("a1", lambda: phase_a1(c)), ("a2", lambda: phase_a2(c)),
                           ("nat", lambda: phase_nat(c)), ("hgrn", lambda: phase_hgrn(c)), ("post0", lambda: phase_post(c, 0))]
                else:
                    seq = [("mods1", lambda: phase_mods(c, 1)), ("f", lambda: phase_f(c)), ("g1", lambda: phase_g1(c)),
                           ("post1", lambda: phase_post(c, 1))]
                for name, fn in seq:
                    fn()
                    if dbg_stop(name):
                        done = True
                        break
            if done:
                break
            with ExitStack() as m5:
                MOD5 = sb(m5, nc, [128, 1024], F32, name="MOD5")
                with ExitStack() as ph:
                    emit_mods(c, ph, layer, [10, 11], lambda ct: MOD5.t[:, (ct - 10) * 512:(ct - 9) * 512], MOD5.b, False)
                    P.end_phase()
                if cfg.dense:
                    phase_moe(c, layer, MOD5)
                else:
                    TEi = (sb(m5, nc, [128, cfg.NTILE, 8], mybir.dt.int32, name="IDXW"), sb(m5, nc, [128, cfg.NTILE], mybir.dt.int32, name="IDXB"))
                    phase_route(c, layer, TEi)
                    if dbg_stop("route%d" % layer):
                        break
                    phase_smoe(c, layer, MOD5, TEi)
            if dbg_stop("moe%d" % layer):
                break
    return nc, c


def emit_mods(c, ph, layer, cts, dst_fn, dst_buf, with_ctx):
    nc, P, k, d = c.nc, c.P, c.k, c.d
    crow = sb(ph, nc, [16, 128], F32, name="crow")
    crow2 = sb(ph, nc, [16, 128], F32, name="crow2")
    ccol = sb(ph, nc, [128, 16], F32, name="ccol")
    CB = sb(ph, nc, [128, 16, 128], F32, name="CB")
    AW = sb(ph, nc, [128, 8, 512], F32, n=2, name="AW")
    ABr = sb(ph, nc, [1, 512], F32, n=2, name="ABr")
    pT = ps(ph, nc, [128, 16], F32, name="pT")
    pM = ps(ph, nc, [128, 512], F32, n=2, name="pM")
    pC = ps(ph, nc, [128, 512], F32, n=2, name="pC")
    k.dma("sp", crow.t[0:8, :], d["c"], wp=[crow.b])
    k.dma("sp", crow.t[8:16, :], d["c_ctx"], wp=[crow.b])
    k.act(crow2.t[:], crow.t[:], AF.Silu, r=[crow.b], w=[crow2.b])
    k.tr(pT.t[:], crow2.t[:], c.ident_f.t[0:16, 0:16], r=[crow2.b], w=[pT.b])
    k.cp("dve", ccol.t[:], pT.t[:], r=[pT.b], w=[ccol.b])
    for j in range(16):
        k.cp("dve" if j % 2 else "pool", CB.t[:, j, :], ccol.t[:, j:j + 1].to_broadcast([128, 128]), r=[ccol.b], wp=[CB.b])
    awv = d["ada_w"][layer].rearrange("(k p) n -> p k n", p=128)
    for i, ct in enumerate(cts):
        aw, ab = AW[i % 2], ABr[i % 2]
        k.dma("sp", aw.t[:], awv[:, :, ct * 512:(ct + 1) * 512], w=[aw.b])
        k.dma("sp", ab.t[:], d["ada_b"][layer:layer + 1, ct * 512:(ct + 1) * 512], w=[ab.b])
        pm = pM[i % 2]
        for kk in range(8):
            k.mm(pm.t[:], CB.t[:, kk, :], aw.t[:, kk, :], kk == 0, False, r=[CB.b, aw.b], w=[pm.b] if kk == 0 else (), wp=() if kk == 0 else [pm.b])
        k.mm(pm.t[:], c.ones_f.t[0:1, :], ab.t[:], False, True, r=[ab.b], wp=[pm.b])
        k.cp("act", dst_fn(ct), pm.t[:], r=[pm.b], wp=[dst_buf])
        if with_ctx and ct < 4:
            pc = pC[i % 2]
            for kk in range(8):
                k.mm(pc.t[:], CB.t[:, 8 + kk, :], aw.t[:, kk, :], kk == 0, False, r=[CB.b, aw.b], w=[pc.b] if kk == 0 else (), wp=() if kk == 0 else [pc.b])
            k.mm(pc.t[:], c.ones_f.t[0:1, :], ab.t[:], False, True, r=[ab.b], wp=[pc.b])
            k.cp("dve", c.CMOD.t[:, ct * 512:(ct + 1) * 512], pc.t[:], r=[pc.b], wp=[c.CMOD.b])


def phase_mods(c, layer):
    with ExitStack() as ph:
        emit_mods(c, ph, layer, list(range(10)), lambda ct: c.MOD.t[:, ct * 512:(ct + 1) * 512], c.MOD.b, layer == 0)
        c.P.end_phase()


def make_A(c, ph, gname, layer, modcols, modt):
    nc, k, d = c.nc, c.k, c.d
    g = sb(ph, nc, [128, 1024], F32, name="gbc")
    A = sb(ph, nc, [128, 1024], F32, name="A")
    k.dma("sp", g.t[:], d[gname][layer:layer + 1, :].partition_broadcast(128), w=[g.b])
    k.stt(A.t[:], modt.t[:, modcols:modcols + 1024], 1.0, g.t[:], ALU.add, ALU.mult, r=[modt.b, g.b], w=[A.b])
    return A


def norm_mod(c, xt, A, SH, shb, outs, tmp, j):
    k = c.k
    ss, t1 = tmp["ss"], tmp["t1"]
    k.stt(t1.t[:], xt.t[:], 1.0, xt.t[:], ALU.mult, ALU.mult, r=[xt.b], w=[t1.b], wp=[ss.b], accum=ss.t[:, 4 * j:4 * j + 1])
    k.ts("dve", ss.t[:, 4 * j + 1:4 * j + 2], ss.t[:, 4 * j:4 * j + 1], 1.0 / 1024, ALU.mult, EPS, ALU.add, r=[ss.b], wp=[ss.b])
    k.act(ss.t[:, 4 * j + 2:4 * j + 3], ss.t[:, 4 * j + 1:4 * j + 2], AF.Sqrt, r=[ss.b], wp=[ss.b])
    k.recip(ss.t[:, 4 * j + 3:4 * j + 4], ss.t[:, 4 * j + 2:4 * j + 3], r=[ss.b], wp=[ss.b])
    k.stt(t1.t[:], xt.t[:], ss.t[:, 4 * j + 3:4 * j + 4], A.t[:], ALU.mult, ALU.mult, r=[xt.b, ss.b, A.b], w=[t1.b])
    for (ap, eng, slot) in outs:
        k.tt(eng, ap, t1.t[:], SH, ALU.add, r=[t1.b, shb], wp=[slot.b])


def phase_a1(c):
    nc, P, k, d, cfg = c.nc, c.P, c.k, c.d, c.cfg
    S, L, NT = cfg.S, cfg.L, cfg.NT
    with ExitStack() as ph:
        W = sb(ph, nc, [128, 8, 2560], BF16, name="Wtok")
        wv = d["ab_w_in"].rearrange("(k p) n -> p k n", p=128)
        for i, c0 in enumerate((0, 512, 1024, 3072, 3584)):
            k.dma("pool", W.t[:, :, i * 512:(i + 1) * 512], wv[:, :, c0:c0 + 512], wp=[W.b])
        A = make_A(c, ph, "norm1_g", 0, 1024, c.MOD)
        Ac = make_A(c, ph, "norm1_g", 0, 1024, c.CMOD)
        g64 = sb(ph, nc, [128, 128], F32, name="g64")
        GQ = sb(ph, nc, [128, 512], F32, name="GQ")
        GK = sb(ph, nc, [128, 512], F32, name="GK")
        k.dma("sp", g64.t[:, 0:64], d["nat_q_norm"].partition_broadcast(128), wp=[g64.b])
        k.dma("sp", g64.t[:, 64:128], d["nat_k_norm"].partition_broadcast(128), wp=[g64.b])
        k.ts("dve", GQ.t[:].rearrange("p (h e) -> p h e", h=8), g64.t[:, 0:64].unsqueeze(1).to_broadcast([128, 8, 64]), 0.125, ALU.mult, r=[g64.b], w=[GQ.b])
        k.ts("dve", GK.t[:].rearrange("p (h e) -> p h e", h=8), g64.t[:, 64:128].unsqueeze(1).to_broadcast([128, 8, 64]), 1.0, ALU.mult, r=[g64.b], w=[GK.b])
        COSG = sb(ph, nc, [128, 128], F32, n=2, name="COS")
        SING = sb(ph, nc, [128, 128], F32, n=2, name="SIN")
        XIN = sb(ph, nc, [128, 1024], F32, n=2, name="xin")
        tmps = [{"ss": sb(ph, nc, [128, 16], F32, name="ss"), "t1": sb(ph, nc, [128, 1024], F32, name="t1")} for _ in range(2)]
        XM = sb(ph, nc, [128, 1024], BF16, n=2, name="xm")
        XTG = sb(ph, nc, [128, 8, 512], BF16, n=2, name="xtg")
        pT = ps(ph, nc, [128, 1024], BF16, name="pT")
        pS = ps(ph, nc, [128, 512], F32, n=5, name="pS")
        pO = ps(ph, nc, [128, 1024], BF16, n=2, name="pO")
        SQs = sb(ph, nc, [128, 1024], F32, n=2, name="sq")
        STs = sb(ph, nc, [128, 48], F32, n=2, name="st")
        QNs = sb(ph, nc, [128, 1024], F32, n=2, name="qn")
        R1s = sb(ph, nc, [128, 1024], F32, n=2, name="r1")
        R2s = sb(ph, nc, [128, 1024], F32, n=2, name="r2")
        OB = sb(ph, nc, [128, 1536], BF16, n=2, name="ob")
        OTG = sb(ph, nc, [128, 3, 4, 512], BF16, n=2, name="otg")
        VAs = sb(ph, nc, [128, 8, 65], BF16, n=2, name="vas")
        VHs = sb(ph, nc, [128, 512], BF16, n=2, name="vhs")
        Gs = sb(ph, nc, [128, 512], F32, n=2, name="gs")
        for v in VAs:
            k.memset("pool", v.t[:, :, 64:65], 1.0, wp=[v.b])
        dXT, dXTc = Buf("dXT"), Buf("dXTc")
        dQ, dV, dVH, dG = Buf("dQ"), Buf("dV"), Buf("dVH"), Buf("dG")

        def run(src, ntile, Asl, modt, is_ctx):
            ngrp = (ntile + 3) // 4
            it = 0
            for g in range(ngrp):
                nj = min(4, ntile - 4 * g)
                xtg = XTG[g % 2]
                otg = OTG[g % 2]
                COS, SIN = COSG[g % 2], SING[g % 2]
                if not is_ctx:
                    k.dma("sp", COS.t[:, 0:nj * 32], d["k_cos"][:, g * 128:g * 128 + nj * 32], w=[COS.b])
                    k.dma("sp", SIN.t[:, 0:nj * 32], d["k_sin"][:, g * 128:g * 128 + nj * 32], w=[SIN.b])
                for j in range(nj):
                    t = 4 * g + j
                    xin, xm = XIN[it % 2], XM[it % 2]
                    ob, vas, vhs, gs = OB[it % 2], VAs[it % 2], VHs[it % 2], Gs[it % 2]
                    SQ, ST, QN, R1, R2 = SQs[it % 2], STs[it % 2], QNs[it % 2], R1s[it % 2], R2s[it % 2]
                    it += 1
                    k.dma("sp", xin.t[:], src[t * 128:(t + 1) * 128, :], w=[xin.b])
                    norm_mod(c, xin, Asl, modt.t[:, 0:1024], modt.b, [(xm.t[:], "pool", xm)], tmps[it % 2], j)
                    for kk in range(8):
                        k.tr(pT.t[:, kk * 128:(kk + 1) * 128], xm.t[:, kk * 128:(kk + 1) * 128], c.ident_b.t[:], r=[xm.b],
                             w=[pT.b] if kk == 0 else (), wp=() if kk == 0 else [pT.b])
                    k.cp("act", xtg.t[:, :, j * 128:(j + 1) * 128], pT.t[:].rearrange("p (k t) -> p k t", k=8), r=[pT.b], wp=[xtg.b])
                    cols = (1, 2, 3) if is_ctx else (0, 1, 2, 3, 4)
                    for ci in cols:
                        for kk in range(8):
                            k.mm(pS[ci].t[:], xtg.t[:, kk, j * 128:(j + 1) * 128], W.t[:, kk, ci * 512:(ci + 1) * 512], kk == 0, kk == 7,
                                 r=[xtg.b, W.b], w=[pS[ci].b] if kk == 0 else (), wp=() if kk == 0 else [pS[ci].b])
                    srcs = ((1, 1),) if is_ctx else ((0, 0), (1, 1))
                    for (ci, slot_i) in srcs:
                        k.act(SQ.t[:, slot_i * 512:(slot_i + 1) * 512], pS[ci].t[:], AF.Square, r=[pS[ci].b], wp=[SQ.b])
                        k.red(ST.t[:, slot_i * 8:(slot_i + 1) * 8], SQ.t[:, slot_i * 512:(slot_i + 1) * 512].rearrange("p (h e) -> p h e", h=8), r=[SQ.b], wp=[ST.b])
                    k.ts("dve", ST.t[:, 16:32], ST.t[:, 0:16], 1.0 / 64, ALU.mult, EPS, ALU.add, r=[ST.b], wp=[ST.b])
                    k.act(ST.t[:, 32:48], ST.t[:, 16:32], AF.Sqrt, r=[ST.b], wp=[ST.b])
                    k.recip(ST.t[:, 16:32], ST.t[:, 32:48], r=[ST.b], wp=[ST.b])
                    for (ci, slot_i) in srcs:
                        qn = QN.t[:, slot_i * 512:(slot_i + 1) * 512]
                        k.tt("dve", qn.rearrange("p (h e) -> p h e", h=8), pS[ci].t[:].rearrange("p (h e) -> p h e", h=8),
                             ST.t[:, 16 + slot_i * 8:16 + slot_i * 8 + 8].unsqueeze(2).to_broadcast([128, 8, 64]), ALU.mult,
                             r=[pS[ci].b, ST.b], wp=[QN.b])
                        Gt = GQ if slot_i == 0 else GK
                        k.tt("pool", qn, qn, Gt.t[:], ALU.mult, r=[QN.b, Gt.b], wp=[QN.b])
                    if is_ctx:
                        k.cp("act", ob.t[:, 1024:1536], QN.t[:, 512:1024], r=[QN.b], wp=[ob.b])
                    else:
                        k.cp("act", ob.t[:, 512:1024], QN.t[:, 0:512], r=[QN.b], wp=[ob.b])
                        qv = QN.t[:].rearrange("p (h a b i) -> p h a b i", h=16, a=2, b=2)
                        cosb = COS.t[:, j * 32:(j + 1) * 32].rearrange("p (a i) -> p a i", a=2).unsqueeze(1).to_broadcast([128, 16, 2, 16])
                        sinb = SIN.t[:, j * 32:(j + 1) * 32].rearrange("p (a i) -> p a i", a=2).unsqueeze(1).to_broadcast([128, 16, 2, 16])
                        r1v = R1.t[:].rearrange("p (h a b i) -> p h a b i", h=16, a=2, b=2)
                        r2v = R2.t[:].rearrange("p (h a b i) -> p h a b i", h=16, a=2, b=2)
                        x1, x2 = qv[:, :, :, 0, :], qv[:, :, :, 1, :]
                        k.tt("dve", r1v[:, :, :, 0, :], x1, cosb, ALU.mult, r=[QN.b, COS.b], wp=[R1.b])
                        k.tt("pool", r2v[:, :, :, 0, :], x2, sinb, ALU.mult, r=[QN.b, SIN.b], wp=[R2.b])
                        k.tt("dve", r1v[:, :, :, 1, :], x2, cosb, ALU.mult, r=[QN.b, COS.b], wp=[R1.b])
                        k.tt("pool", r2v[:, :, :, 1, :], x1, sinb, ALU.mult, r=[QN.b, SIN.b], wp=[R2.b])
                        for slot_i, o0 in ((0, 0), (1, 1024)):
                            ov = ob.t[:, o0:o0 + 512].rearrange("p (h a b i) -> p h a b i", h=8, a=2, b=2)
                            a1 = r1v[:, slot_i * 8:(slot_i + 1) * 8]
                            a2 = r2v[:, slot_i * 8:(slot_i + 1) * 8]
                            k.tt("dve", ov[:, :, :, 0, :], a1[:, :, :, 0, :], a2[:, :, :, 0, :], ALU.subtract, r=[R1.b, R2.b], wp=[ob.b])
                            k.tt("dve", ov[:, :, :, 1, :], a1[:, :, :, 1, :], a2[:, :, :, 1, :], ALU.add, r=[R1.b, R2.b], wp=[ob.b])
                    which = (2,) if is_ctx else (0, 1, 2)
                    for wi in which:
                        po = pO[0] if wi < 2 else pO[1]
                        for hp in range(4):
                            col = ((wi % 2) * 4 + hp) * 128
                            first = (hp == 0 and wi in (0, 2))
                            k.tr(po.t[:, col:col + 128], ob.t[:, wi * 512 + hp * 128: wi * 512 + (hp + 1) * 128], c.ident_b.t[:], r=[ob.b],
                                 w=[po.b] if first else (), wp=() if first else [po.b])
                    if not is_ctx:
                        k.cp("act", otg.t[:, 0:2, :, j * 128:(j + 1) * 128], pO[0].t[:].rearrange("p (w h t) -> p w h t", w=2, h=4), r=[pO[0].b], wp=[otg.b])
                    k.cp("dve", otg.t[:, 2, :, j * 128:(j + 1) * 128], pO[1].t[:, 0:512].rearrange("p (h t) -> p h t", h=4), r=[pO[1].b], wp=[otg.b])
                    k.cp("act", vas.t[:, :, 0:64], pS[2].t[:].rearrange("p (h e) -> p h e", h=8), r=[pS[2].b], wp=[vas.b])
                    k.cp("dve", vhs.t[:], pS[3].t[:], r=[pS[3].b], w=[vhs.b])
                    rows = slice(t * 128, (t + 1) * 128)
                    if is_ctx:
                        k.dma("sp", d["VcA"][rows, :], vas.t[:].rearrange("p h e -> p (h e)"), r=[vas.b], wp=[dV])
                        k.dma("sp", d["VHc"][rows, :], vhs.t[:], r=[vhs.b], wp=[dVH])
                    else:
                        k.act(gs.t[:], pS[4].t[:], AF.Silu, r=[pS[4].b], w=[gs.b])
                        k.dma("sp", d["VA"][rows, :], vas.t[:].rearrange("p h e -> p (h e)"), r=[vas.b], wp=[dV])
                        k.dma("sp", d["VH"][rows, :], vhs.t[:], r=[vhs.b], wp=[dVH])
                        k.dma("sp", d["G"][rows, :], gs.t[:], r=[gs.b], wp=[dG])
                tok = slice(g * 512, g * 512 + nj * 128)
                w_ = nj * 128
                if is_ctx:
                    k.dma("sp", d["XTc"][:, :, tok], xtg.t[:, :, 0:w_], r=[xtg.b], wp=[dXTc])
                    k.dma("sp", d["KcT"][:, :, tok], otg.t[:, 2, :, 0:w_], r=[otg.b], wp=[dQ])
                else:
                    k.dma("sp", d["XT"][:, :, tok], xtg.t[:, :, 0:w_], r=[xtg.b], wp=[dXT])
                    k.dma("sp", d["QTr"][:, :, tok], otg.t[:, 0, :, 0:w_], r=[otg.b], wp=[dQ])
                    k.dma("sp", d["QTf"][:, :, tok], otg.t[:, 1, :, 0:w_], r=[otg.b], wp=[dQ])
                    k.dma("sp", d["KTr"][:, :, tok], otg.t[:, 2, :, 0:w_], r=[otg.b], wp=[dQ])

        run(d["ctx"], L // 128, Ac, c.CMOD, True)
        run(d["x"], NT, A, c.MOD, False)
        P.end_phase()


def core_inputs(inp, b, cfg, consts):
    f = lambda a: np.ascontiguousarray(np.asarray(a, dtype=np.float32))
    m = {
        "x": f(inp["x"][b]), "c": f(inp["c"][b]).reshape(8, 128), "ctx": f(inp["ctx"][b]),
        "c_ctx": f(inp["c_ctx"]).reshape(8, 128),
        "ada_w": f(inp["ada_w"]), "ada_b": f(inp["ada_b"]), "norm1_g": f(inp["norm1_g"]), "norm2_g": f(inp["norm2_g"]),
        "ab_w_in": f(inp["ab_w_in"][0]), "ab_w_out": f(inp["ab_w_out"][0]),
        "nat_q_norm": f(inp["nat_q_norm"][0]).reshape(1, 64), "nat_k_norm": f(inp["nat_k_norm"][0]).reshape(1, 64),
        "rpb_full": layout_rpb(f(inp["nat_rpb"][0])),
        "hgrn_lb": f(inp["hgrn_lb"]).reshape(16, 128), "hgrn_o_norm": f(inp["hgrn_o_norm"][0]).reshape(1, 128),
        "conv_w1": f(inp["conv_w1"][0]), "conv_b1": f(inp["conv_b1"][0]).reshape(16, 128), "conv_dw": f(inp["conv_dw"][0]),
        "conv_dw_b": f(inp["conv_dw_b"][0]).reshape(8, 128), "conv_ln_g": f(inp["conv_ln_g"][0]).reshape(8, 128),
        "conv_ln_b": f(inp["conv_ln_b"][0]).reshape(8, 128), "conv_w2": f(inp["conv_w2"][0]), "conv_b2": f(inp["conv_b2"][0]).reshape(1, 1024),
        "router_w": f(inp["router_w"]), "router_b": f(inp["router_b"]),
        "moe_w1": f(inp["moe_w1"]), "moe_b1": f(inp["moe_b1"]).reshape(2, cfg.E * 16, 128),
        "moe_w2": f(inp["moe_w2"]), "moe_b2": f(inp["moe_b2"]),
    }
    for kname, v in consts.items():
        m["k_" + kname] = v
    return m


_cache = {}


def kernel(**inputs):
    B = inputs["x"].shape[0]
    S = inputs["x"].shape[1]
    cfg = Cfg(S=S, L=inputs["ctx"].shape[1], E=inputs["moe_w1"].shape[1])
    key = (cfg.S, cfg.L, cfg.E)
    if key not in _cache:
        _cache[key] = build_program(cfg)[0]
    nc = _cache[key]
    consts = host_consts(cfg)
    in_maps = [core_inputs(inputs, b, cfg, consts) for b in range(B)]
    res = run_bass_kernel_spmd(nc, in_maps, core_ids=list(range(B)))
    return np.stack([np.asarray(r["out"], dtype=np.float32) for r in res.results], axis=0)


def phase_a2(c):
    nc, P, k, d, cfg = c.nc, c.P, c.k, c.d, c.cfg
    S, L = cfg.S, cfg.L
    with ExitStack() as ph:
        W = sb(ph, nc, [128, 8, 1536], BF16, name="Wfm")
        wv = d["ab_w_in"].rearrange("(k p) n -> p k n", p=128)
        for i in range(3):
            k.dma("pool", W.t[:, :, i * 512:(i + 1) * 512], wv[:, :, 1536 + i * 512:1536 + (i + 1) * 512], wp=[W.b])
        RESET = sb(ph, nc, [128, 512], F32, name="reset")
        k.dma("sp", RESET.t[:], d["k_reset"], w=[RESET.b])
        lbr = sb(ph, nc, [16, 128], F32, name="lbr")
        Ee = sb(ph, nc, [128, 16], F32, name="Ee")
        LB = sb(ph, nc, [128, 24], F32, name="LB")
        pL = ps(ph, nc, [128, 16], F32, name="pL")
        k.dma("sp", lbr.t[:], d["hgrn_lb"], w=[lbr.b])
        k.tr(pL.t[:], lbr.t[:], c.ident_f.t[0:16, 0:16], r=[lbr.b], w=[pL.b])
        k.act(Ee.t[:], pL.t[:], AF.Exp, r=[pL.b], w=[Ee.b])
        ev = Ee.t[:].rearrange("p (d j h) -> p d j h", d=2, j=2)
        k.tt("dve", LB.t[:, 16:24].rearrange("p (d h) -> p d h", d=2), ev[:, :, 0, :], ev[:, :, 1, :], ALU.add, r=[Ee.b], wp=[LB.b])
        k.recip(LB.t[:, 16:24], LB.t[:, 16:24], r=[LB.b], wp=[LB.b])
        k.tt("dve", LB.t[:, 0:8].rearrange("p (d h) -> p d h", d=2), ev[:, :, 0, :], LB.t[:, 16:24].rearrange("p (d h) -> p d h", d=2), ALU.mult, r=[Ee.b, LB.b], wp=[LB.b])
        k.ts("dve", LB.t[:, 8:16], LB.t[:, 0:8], -1.0, ALU.mult, 1.0, ALU.add, r=[LB.b], wp=[LB.b])

        XTG = sb(ph, nc, [128, 8, 512], BF16, n=2, name="xtg")
        names = ("q32", "sg", "f", "lf", "kk", "B", "e1", "e2", "t1", "r", "e3")
        TM = {n_: sb(ph, nc, [128, 512], F32, n=2, name=n_) for n_ in names}
        HQs = sb(ph, nc, [128, 2, 4, 512], BF16, n=2, name="hqs")
        HKs = sb(ph, nc, [128, 2, 4, 512], BF16, n=2, name="hks")
        KHT = sb(ph, nc, [128, 512], BF16, n=2, name="kht")
        KHs = sb(ph, nc, [128, 4, 2, 512], BF16, n=2, name="khs")
        DECs = sb(ph, nc, [128, 2, 4, 8], F32, n=2, name="decs")
        pQ = ps(ph, nc, [128, 512], F32, n=2, name="pQ")
        pF = ps(ph, nc, [128, 512], F32, n=3, name="pF")
        pK = ps(ph, nc, [128, 512], BF16, n=2, name="pK")
        dHQ, dHK, dKH, dDEC = Buf("dHQ"), Buf("dHK"), Buf("dKH"), Buf("dDEC")
        QS = 128.0 ** -0.5

        def run(src, ntok, is_ctx):
            ngrp = (ntok + 511) // 512
            it = 0
            for g in range(ngrp):
                n = min(512, ntok - g * 512)
                nch = n // 64
                nsub = n // 128
                xtg, hqs, hks, khs, decs = XTG[g % 2], HQs[g % 2], HKs[g % 2], KHs[g % 2], DECs[g % 2]
                k.dma("sp", xtg.t[:, :, 0:n], src[:, :, g * 512:g * 512 + n], w=[xtg.b])
                for h in range(4):
                    pq = pQ[h % 2]
                    if not is_ctx:
                        for kk_ in range(8):
                            k.mm(pq.t[:, 0:n], W.t[:, kk_, h * 128:(h + 1) * 128], xtg.t[:, kk_, 0:n], kk_ == 0, kk_ == 7,
                                 r=[W.b, xtg.b], w=[pq.b] if kk_ == 0 else (), wp=() if kk_ == 0 else [pq.b])
                        q32 = TM["q32"][h % 2]
                        k.act(q32.t[:, 0:n], pq.t[:, 0:n], AF.Silu, r=[pq.b], w=[q32.b])
                    for dd in range(2):
                        pf = pF[(2 * h + dd) % 3]
                        c0 = 512 + dd * 512 + h * 128
                        for kk_ in range(8):
                            k.mm(pf.t[:, 0:n], W.t[:, kk_, c0:c0 + 128], xtg.t[:, kk_, 0:n], kk_ == 0, kk_ == 7,
                                 r=[W.b, xtg.b], w=[pf.b] if kk_ == 0 else (), wp=() if kk_ == 0 else [pf.b])
                        tm = {n_: TM[n_][it % 2] for n_ in names}
                        kht = KHT[it % 2]
                        pk = pK[it % 2]
                        it += 1
                        sg, f, lf, kk, Bc, e1, e2, t1, rr, e3 = (tm[x] for x in ("sg", "f", "lf", "kk", "B", "e1", "e2", "t1", "r", "e3"))
                        li = dd * 4 + h
                        k.act(sg.t[:, 0:n], pf.t[:, 0:n], AF.Sigmoid, r=[pf.b], w=[sg.b])
                        k.ts("dve", f.t[:, 0:n], sg.t[:, 0:n], LB.t[:, 8 + li:9 + li], ALU.mult, LB.t[:, li:li + 1], ALU.add, r=[sg.b, LB.b], w=[f.b])
                        k.act(lf.t[:, 0:n], f.t[:, 0:n], AF.Ln, r=[f.b], w=[lf.b])
                        k.ts("pool", kk.t[:, 0:n], f.t[:, 0:n], -1.0, ALU.mult, 1.0, ALU.add, r=[f.b], w=[kk.b])
                        P.op("dve", (lambda e, o=Bc.t[:, 0:n], a=RESET.t[:, 0:n], b_=lf.t[:, 0:n]:
                                     e.tensor_tensor_scan(out=o, data0=a, data1=b_, initial=0.0, op0=ALU.mult, op1=ALU.add)),
                             reads=[RESET.b, lf.b], writes=[Bc.b])
                        Bv = Bc.t[:, 0:n].rearrange("p (c t) -> p c t", t=64)
                        Bend = Bv[:, :, 63:64].to_broadcast([128, nch, 64])
                        v3 = lambda s_: s_.t[:, 0:n].rearrange("p (c t) -> p c t", t=64)
                        if dd == 0:
                            k.act(e1.t[:, 0:n], Bc.t[:, 0:n], AF.Exp, r=[Bc.b], w=[e1.b])
                            k.act(e2.t[:, 0:n], Bc.t[:, 0:n], AF.Exp, r=[Bc.b], w=[e2.b], scale=-1.0)
                            k.tt("dve", v3(t1), Bend, Bv, ALU.subtract, r=[Bc.b], w=[t1.b])
                            k.act(e3.t[:, 0:n], t1.t[:, 0:n], AF.Exp, r=[t1.b], w=[e3.b])
                        else:
                            k.tt("dve", t1.t[:, 0:n], lf.t[:, 0:n], Bc.t[:, 0:n], ALU.subtract, r=[lf.b, Bc.b], w=[t1.b])
                            k.tt("dve", v3(rr), v3(t1), Bend, ALU.add, r=[t1.b, Bc.b], w=[rr.b])
                            k.act(e1.t[:, 0:n], rr.t[:, 0:n], AF.Exp, r=[rr.b], w=[e1.b])
                            k.act(e2.t[:, 0:n], rr.t[:, 0:n], AF.Exp, r=[rr.b], w=[e2.b], scale=-1.0)
                            k.act(e3.t[:, 0:n], t1.t[:, 0:n], AF.Exp, r=[t1.b], w=[e3.b], scale=-1.0)
                        k.act(decs.t[:, dd, h, 0:nch], Bv[:, :, 63], AF.Exp, r=[Bc.b], wp=[decs.b])
                        if not is_ctx:
                            q32 = TM["q32"][h % 2]
                            k.stt(hqs.t[:, dd, h, 0:n], q32.t[:, 0:n], QS, e1.t[:, 0:n], ALU.mult, ALU.mult, r=[q32.b, e1.b], wp=[hqs.b])
                            k.tt("pool", hks.t[:, dd, h, 0:n], kk.t[:, 0:n], e2.t[:, 0:n], ALU.mult, r=[kk.b, e2.b], wp=[hks.b])
                        k.tt("pool", kht.t[:, 0:n], kk.t[:, 0:n], e3.t[:, 0:n], ALU.mult, r=[kk.b, e3.b], w=[kht.b])
                        for sub in range(nsub):
                            k.tr(pk.t[:, sub * 128:(sub + 1) * 128], kht.t[:, sub * 128:(sub + 1) * 128], c.ident_b.t[:], r=[kht.b],
                                 w=[pk.b] if sub == 0 else (), wp=() if sub == 0 else [pk.b])
                        k.cp("act", khs.t[:, 0:nsub, dd, h * 128:(h + 1) * 128], pk.t[:, 0:n].rearrange("p (s e) -> p s e", e=128), r=[pk.b], wp=[khs.b])
                tok = slice(g * 512, g * 512 + n)
                for dd in range(2):
                    if is_ctx:
                        k.dma("sp", d["KHc"][dd, tok, :].rearrange("(s p) e -> p s e", p=128), khs.t[:, 0:nsub, dd, :], r=[khs.b], wp=[dKH])
                        k.dma("sp", d["DECc"][dd, :, :, g * 8:g * 8 + nch], decs.t[:, dd, :, 0:nch], r=[decs.b], wp=[dDEC])
                    else:
                        k.dma("sp", d["HQ"][dd, :, :, tok], hqs.t[:, dd, :, 0:n], r=[hqs.b], wp=[dHQ])
                        k.dma("sp", d["HK"][dd, :, :, tok], hks.t[:, dd, :, 0:n], r=[hks.b], wp=[dHK])
                        k.dma("sp", d["KH"][dd, tok, :].rearrange("(s p) e -> p s e", p=128), khs.t[:, 0:nsub, dd, :], r=[khs.b], wp=[dKH])
                        k.dma("sp", d["DEC"][dd, :, :, g * 8:g * 8 + nch], decs.t[:, dd, :, 0:nch], r=[decs.b], wp=[dDEC])

        run(d["XTc"], L, True)
        run(d["XT"], S, False)
        P.end_phase()


def phase_nat(c):
    nc, P, k, d, cfg = c.nc, c.P, c.k, c.d, c.cfg
    S, L, ROWS = cfg.S, cfg.L, cfg.ROWS
    NCC = L // 128
    NCH = 4 + NCC
    with ExitStack() as ph:
        BFf = sb(ph, nc, [128, 7680], F32, name="bff")
        BFb = sb(ph, nc, [128, 8, 960], BF16, name="bfb")
        k.dma("sp", BFf.t[0:64, :], d["rpb_full"], wp=[BFf.b])
        k.dma("sp", BFf.t[64:128, :], d["rpb_full"], wp=[BFf.b])
        k.cp("dve", BFb.t[:].rearrange("p h e -> p (h e)"), BFf.t[:], r=[BFf.b], w=[BFb.b])
        KcT = sb(ph, nc, [128, 4, L], BF16, name="kct")
        VcA = sb(ph, nc, [128, NCC, 520], BF16, name="vca")
        k.dma("sp", KcT.t[:], d["KcT"], w=[KcT.b])
        k.dma("sp", VcA.t[:], d["VcA"].rearrange("(c p) f -> p c f", p=128), w=[VcA.b])
        QR = sb(ph, nc, [128, 4, 512], BF16, n=2, name="qr")
        QF = sb(ph, nc, [128, 4, 512], BF16, n=2, name="qf")
        KW = sb(ph, nc, [128, 4, 512], BF16, n=3, name="kw")
        VW = sb(ph, nc, [128, 4, 520], BF16, n=3, name="vw")
        PT = sb(ph, nc, [128, NCH * 64], BF16, n=3, name="pt")
        NS = sb(ph, nc, [64, 512], BF16, n=2, name="ns")
        RD = sb(ph, nc, [64, 8], F32, n=2, name="rd")
        pS = ps(ph, nc, [128, 512], F32, n=4, name="pS")
        pO = ps(ph, nc, [64, 4, 65], F32, n=4, name="pO")
        dCAT = Buf("dCATn")
        it = 0
        for r in range(ROWS):
            g8, ro = r // 8, (r % 8) * 64
            qr, qf = QR[g8 % 2], QF[g8 % 2]
            if r % 8 == 0:
                k.dma("sp", qr.t[:], d["QTr"][:, :, g8 * 512:(g8 + 1) * 512], w=[qr.b])
                k.dma("sp", qf.t[:], d["QTf"][:, :, g8 * 512:(g8 + 1) * 512], w=[qf.b])
            rs = min(max(r - 4, 0), ROWS - 8)
            dr0 = rs - r + 7
            kw, vw = KW[r % 3], VW[r % 3]
            k.dma("sp", kw.t[:], d["KTr"][:, :, rs * 64:rs * 64 + 512], w=[kw.b])
            k.dma("sp", vw.t[:], d["VA"][rs * 64:rs * 64 + 512, :].rearrange("(c p) f -> p c f", p=128), w=[vw.b])
            ns, rd = NS[r % 2], RD[r % 2]
            po2 = (pO[(2 * r) % 4], pO[(2 * r + 1) % 4])
            def scores(h):
                nonlocal it
                hp, pb = h // 2, (h % 2) * 64
                psx, pt = pS[it % 4], PT[it % 3]
                it += 1
                first = True
                for kc in range(4):
                    k.mm(psx.t[:, kc * 64:(kc + 1) * 64], kw.t[pb:pb + 64, hp, kc * 128:(kc + 1) * 128], qr.t[pb:pb + 64, hp, ro:ro + 64], True, False,
                         r=[kw.b, qr.b], w=[psx.b] if first else (), wp=() if first else [psx.b])
                    first = False
                    k.mm(psx.t[:, kc * 64:(kc + 1) * 64], BFb.t[pb:pb + 64, h, (dr0 + 2 * kc) * 64:(dr0 + 2 * kc) * 64 + 128], c.ident_b.t[pb:pb + 64, pb:pb + 64], False, True,
                         r=[BFb.b], wp=[psx.b])
                for cc in range(NCC):
                    k.mm(psx.t[:, (4 + cc) * 64:(5 + cc) * 64], KcT.t[pb:pb + 64, hp, cc * 128:(cc + 1) * 128], qf.t[pb:pb + 64, hp, ro:ro + 64], True, True,
                         r=[KcT.b, qf.b], wp=[psx.b])
                k.act(pt.t[:], psx.t[:, 0:NCH * 64], AF.Exp, r=[psx.b], w=[pt.b])
                return pt

            def pv(h, pt):
                po = po2[h // 4]
                hh = h % 4
                for ch in range(NCH):
                    rhs = vw.t[:, ch, h * 65:(h + 1) * 65] if ch < 4 else VcA.t[:, ch - 4, h * 65:(h + 1) * 65]
                    k.mm(po.t[:, hh, :], pt.t[:, ch * 64:(ch + 1) * 64], rhs, ch == 0, ch == NCH - 1,
                         r=[pt.b, vw.b, VcA.b], w=[po.b] if (ch == 0 and hh == 0) else (), wp=() if (ch == 0 and hh == 0) else [po.b])

            pts = [scores(0)]
            for h in range(8):
                if h + 1 < 8:
                    pts.append(scores(h + 1))
                pv(h, pts[h])
            for half in range(2):
                po = po2[half]
                k.recip(rd.t[:, half * 4:(half + 1) * 4], po.t[:, :, 64], r=[po.b], wp=[rd.b])
                k.tt("dve", ns.t[:, half * 256:(half + 1) * 256].rearrange("p (h e) -> p h e", h=4), po.t[:, :, 0:64],
                     rd.t[:, half * 4:(half + 1) * 4].unsqueeze(2).to_broadcast([64, 4, 64]), ALU.mult, r=[po.b, rd.b], wp=[ns.b])
            k.dma("sp", d["CAT"][r * 64:(r + 1) * 64, 0:512], ns.t[:], r=[ns.b], wp=[dCAT])
        P.end_phase()


def phase_hgrn(c):
    nc, P, k, d, cfg = c.nc, c.P, c.k, c.d, c.cfg
    S, L = cfg.S, cfg.L
    NG = S // 512
    NCHT = S // 64
    LC = L // 64
    with ExitStack() as ph:
        TRI = sb(ph, nc, [128, 2, 64], F32, name="tri")
        k.dma("sp", TRI.t[:, 0, :], d["k_trif"], wp=[TRI.b])
        k.dma("sp", TRI.t[:, 1, :], d["k_trib"], wp=[TRI.b])
        og = sb(ph, nc, [128, 128], F32, name="og")
        ONG = sb(ph, nc, [128, 512], F32, name="ong")
        k.dma("sp", og.t[:], d["hgrn_o_norm"].partition_broadcast(128), w=[og.b])
        k.ts("dve", ONG.t[:].rearrange("p (h e) -> p h e", h=4), og.t[:].unsqueeze(1).to_broadcast([128, 4, 128]), 1.0, ALU.mult, r=[og.b], w=[ONG.b])
        S32s = sb(ph, nc, [128, 4, 128], F32, n=2, name="s32")
        Sbfs = sb(ph, nc, [128, 4, 128], BF16, n=2, name="sbf")
        DECt = sb(ph, nc, [128, 4, NCHT], F32, name="dect")
        DECc = sb(ph, nc, [128, 4, LC], F32, name="decc")
        KHc = sb(ph, nc, [128, L // 128, 512], BF16, name="khc")
        VHc = sb(ph, nc, [128, L // 128, 512], BF16, name="vhc")
        HQg = sb(ph, nc, [128, 4, 512], BF16, n=2, name="hqg")
        HKg = sb(ph, nc, [128, 4, 512], BF16, n=2, name="hkg")
        KHg = sb(ph, nc, [128, 4, 512], BF16, n=2, name="khg")
        VHg = sb(ph, nc, [128, 4, 512], BF16, n=2, name="vhg")
        SC = [sb(ph, nc, [128, 256], BF16, n=2, name="sc%d" % i) for i in range(2)]
        for i in range(2):
            for s_ in SC[i]:
                k.memset("pool", s_.t[:], 0.0, w=[s_.b])
        OFs = sb(ph, nc, [64, 512], F32, n=2, name="ofs")
        OFc = sb(ph, nc, [64, 512], F32, n=2, name="ofc")
        Gc = sb(ph, nc, [64, 512], F32, n=2, name="gc")
        O32 = sb(ph, nc, [64, 512], F32, n=2, name="o32")
        SQ = sb(ph, nc, [64, 512], F32, n=2, name="sq")
        ST = sb(ph, nc, [64, 16], F32, n=2, name="st")
        Y1 = sb(ph, nc, [64, 512], F32, n=2, name="y1")
        YB = sb(ph, nc, [64, 512], BF16, n=2, name="yb")
        pSs = ps(ph, nc, [128, 512], F32, n=2, name="pSs")
        pSo = ps(ph, nc, [128, 512], F32, n=2, name="pSo")
        pSt = ps(ph, nc, [128, 512], F32, n=2, name="pSt")
        dOF, dCAT = Buf("dOF"), Buf("dCATh")
        k.dma("sp", VHc.t[:], d["VHc"].rearrange("(s p) e -> p s e", p=128), w=[VHc.b])
        it = 0
        for dd in range(2):
            k.memset("dve", S32s[0].t[:], 0.0, w=[S32s[0].b])
            k.dma("sp", DECt.t[:], d["DEC"][dd], w=[DECt.b])
            k.dma("sp", DECc.t[:], d["DECc"][dd], w=[DECc.b])
            k.dma("sp", KHc.t[:], d["KHc"][dd].rearrange("(s p) e -> p s e", p=128), w=[KHc.b])

            sn = 0

            def state_update(khs, vhs, tl, pb, dec_ap_fn):
                nonlocal it, sn
                pst = pSt[it % 2]
                for h in range(4):
                    k.mm(pst.t[:, h * 128:(h + 1) * 128], khs.t[pb:pb + 64, tl, h * 128:(h + 1) * 128], vhs.t[pb:pb + 64, tl, h * 128:(h + 1) * 128], True, True,
                         r=[khs.b, vhs.b], w=[pst.b] if h == 0 else (), wp=() if h == 0 else [pst.b])
                so, sw = S32s[sn % 2], S32s[(sn + 1) % 2]
                for h in range(4):
                    k.stt(sw.t[:, h, :], so.t[:, h, :], dec_ap_fn(h), pst.t[:, h * 128:(h + 1) * 128], ALU.mult, ALU.add,
                          r=[so.b, pst.b, DECt.b, DECc.b], wp=[sw.b])
                nb = Sbfs[(sn + 1) % 2]
                k.cp("act", nb.t[:], sw.t[:], r=[sw.b], w=[nb.b])
                sn += 1

            chs = range(LC) if dd == 0 else range(LC - 1, -1, -1)
            for ch in chs:
                state_update(KHc, VHc, ch // 2, (ch % 2) * 64, lambda h, ch=ch: DECc.t[:, h, ch:ch + 1])
                it += 1
            groups = list(range(NG)) if dd == 0 else list(range(NG - 1, -1, -1))
            order = []
            for gi, g in enumerate(groups):
                for tl in (range(4) if dd == 0 else range(3, -1, -1)):
                    for cc in ((0, 1) if dd == 0 else (1, 0)):
                        order.append((gi, g, tl, cc))
            loaded = set()

            def ensure(gi, g):
                if gi in loaded:
                    return
                loaded.add(gi)
                hq, hk, kh, vh = HQg[gi % 2], HKg[gi % 2], KHg[gi % 2], VHg[gi % 2]
                tok = slice(g * 512, (g + 1) * 512)
                k.dma("sp", hq.t[:], d["HQ"][dd, :, :, tok], w=[hq.b])
                k.dma("sp", hk.t[:], d["HK"][dd, :, :, tok], w=[hk.b])
                k.dma("sp", kh.t[:], d["KH"][dd, tok, :].rearrange("(s p) e -> p s e", p=128), w=[kh.b])
                k.dma("sp", vh.t[:], d["VH"][tok, :].rearrange("(s p) e -> p s e", p=128), w=[vh.b])

            def scores(n):
                gi, g, tl, cc = order[n]
                ensure(gi, g)
                hq, hk = HQg[gi % 2], HKg[gi % 2]
                pb = cc * 64
                toff, qoff = tl * 128, tl * 128 + cc * 64
                pss = pSs[n % 2]
                sc = SC[cc][(n // 2) % 2]
                for h in range(4):
                    k.mm(pss.t[:, h * 64:(h + 1) * 64], hk.t[:, h, toff:toff + 128], hq.t[:, h, qoff:qoff + 64], True, True,
                         r=[hk.b, hq.b], w=[pss.b] if h == 0 else (), wp=() if h == 0 else [pss.b])
                k.tt("dve", sc.t[pb:pb + 64, :].rearrange("p (h t) -> p h t", h=4), pss.t[pb:pb + 64, 0:256].rearrange("p (h t) -> p h t", h=4),
                     TRI.t[pb:pb + 64, dd, :].unsqueeze(1).to_broadcast([64, 4, 64]), ALU.mult, r=[pss.b, TRI.b], wp=[sc.b])
                return sc

            def rest(n, sc):
                nonlocal it
                gi, g, tl, cc = order[n]
                hq, hk, kh, vh = HQg[gi % 2], HKg[gi % 2], KHg[gi % 2], VHg[gi % 2]
                ch = g * 8 + tl * 2 + cc
                pb = cc * 64
                qoff = tl * 128 + cc * 64
                pso = pSo[n % 2]
                Sbf = Sbfs[sn % 2]
                state_update(kh, vh, tl, pb, lambda h, ch=ch: DECt.t[:, h, ch:ch + 1])
                it += 1
                for h in range(4):
                    k.mm(pso.t[0:64, h * 128:(h + 1) * 128], sc.t[:, h * 64:(h + 1) * 64], vh.t[:, tl, h * 128:(h + 1) * 128], True, False,
                         r=[sc.b, vh.b], w=[pso.b] if h == 0 else (), wp=() if h == 0 else [pso.b])
                    k.mm(pso.t[0:64, h * 128:(h + 1) * 128], hq.t[:, h, qoff:qoff + 64], Sbf.t[:, h, :], False, True,
                         r=[hq.b, Sbf.b], wp=[pso.b])
                rows = slice(ch * 64, (ch + 1) * 64)
                if dd == 0:
                    ofs = OFs[n % 2]
                    k.cp("act", ofs.t[:], pso.t[0:64, :], r=[pso.b], w=[ofs.b])
                    k.dma("sp", d["OF"][rows, :], ofs.t[:], r=[ofs.b], wp=[dOF])
                else:
                    ofc, gc, o32, sq, st, y1, yb = (X[n % 2] for X in (OFc, Gc, O32, SQ, ST, Y1, YB))
                    k.dma("sp", ofc.t[:], d["OF"][rows, :], r=[dOF], w=[ofc.b])
                    k.dma("sp", gc.t[:], d["G"][rows, :], w=[gc.b])
                    k.tt("dve", o32.t[:], pso.t[0:64, :], ofc.t[:], ALU.add, r=[pso.b, ofc.b], w=[o32.b])
                    k.act(sq.t[:], o32.t[:], AF.Square, r=[o32.b], w=[sq.b])
                    k.red(st.t[:, 0:4], sq.t[:].rearrange("p (h e) -> p h e", h=4), r=[sq.b], wp=[st.b])
                    k.ts("dve", st.t[:, 4:8], st.t[:, 0:4], 1.0 / 128, ALU.mult, EPS, ALU.add, r=[st.b], wp=[st.b])
                    k.act(st.t[:, 8:12], st.t[:, 4:8], AF.Sqrt, r=[st.b], wp=[st.b])
                    k.recip(st.t[:, 12:16], st.t[:, 8:12], r=[st.b], wp=[st.b])
                    k.tt("dve", y1.t[:].rearrange("p (h e) -> p h e", h=4), o32.t[:].rearrange("p (h e) -> p h e", h=4),
                         st.t[:, 12:16].unsqueeze(2).to_broadcast([64, 4, 128]), ALU.mult, r=[o32.b, st.b], w=[y1.b])
                    k.tt("pool", y1.t[:], y1.t[:], ONG.t[0:64, :], ALU.mult, r=[y1.b, ONG.b], w=[y1.b])
                    k.tt("pool", yb.t[:], y1.t[:], gc.t[:], ALU.mult, r=[y1.b, gc.b], w=[yb.b])
                    k.dma("sp", d["CAT"][rows, 512:1024], yb.t[:], r=[yb.b], wp=[dCAT])

            N = len(order)
            cur = scores(0)
            for n in range(N):
                nxt = scores(n + 1) if n + 1 < N else None
                rest(n, cur)
                cur = nxt
        P.end_phase()


def phase_post(c, layer):
    nc, P, k, d, cfg = c.nc, c.P, c.k, c.d, c.cfg
    S, E, NT = cfg.S, cfg.E, cfg.NT
    NG = S // 512
    hin = d["x"] if layer == 0 else d["H2"]
    hout = d["H1"] if layer == 0 else d["H3"]
    with ExitStack() as ph:
        Wo = sb(ph, nc, [128, 8, 1024], BF16, name="Wo")
        wsrc = d["ab_w_out"] if layer == 0 else d["conv_w2"]
        k.dma("pool", Wo.t[:], wsrc.rearrange("(k p) n -> p k n", p=128), w=[Wo.b])
        RW = sb(ph, nc, [128, 8, E], F32, name="RW")
        k.dma("sp", RW.t[:], d["router_w"][layer].rearrange("(k p) e -> p k e", p=128), w=[RW.b])
        RB = sb(ph, nc, [128, E], F32, name="RB")
        k.dma("sp", RB.t[:], d["router_b"][layer:layer + 1, :].partition_broadcast(128), w=[RB.b])
        B2t = sb(ph, nc, [E, 1024], F32, name="B2t")
        k.dma("sp", B2t.t[:], d["moe_b2"][layer], w=[B2t.b])
        A2 = make_A(c, ph, "norm2_g", layer, 4096, c.MOD)
        XIN = sb(ph, nc, [128, 1024], F32, n=2, name="xin")
        Hs = sb(ph, nc, [128, 1024], F32, n=2, name="hs")
        V1s = sb(ph, nc, [128, 1024], F32, n=2, name="v1")
        tmps = [{"ss": sb(ph, nc, [128, 16], F32, name="ss"), "t1": sb(ph, nc, [128, 1024], F32, name="t1")} for _ in range(2)]
        XFs = sb(ph, nc, [128, 1024], F32, n=2, name="xf")
        XB = sb(ph, nc, [128, 1024], BF16, n=2, name="xb")
        XT2g = sb(ph, nc, [128, 8, 512], BF16, n=2, name="xt2g")
        XFTs = sb(ph, nc, [128, 8, 128], F32, n=2, name="xft")
        LG = sb(ph, nc, [128, 4 * E + 32], F32, n=2, name="lg")
        CTs = sb(ph, nc, [E, 128], F32, n=2, name="ct")
        ACss = sb(ph, nc, [128, 1024], F32, n=2, name="acs")
        pT = ps(ph, nc, [128, 1024], BF16, name="pT")
        pT2 = ps(ph, nc, [128, 1024], BF16, name="pT2")
        pY = ps(ph, nc, [128, 512], F32, n=2, name="pY")
        pTf = ps(ph, nc, [128, 512], F32, n=2, name="pTf")
        pLg = ps(ph, nc, [128, E], F32, name="pLg")
        pCT = ps(ph, nc, [E, 128], F32, name="pCT")
        dH, dXT2, dCOMB, dACC = Buf("dH"), Buf("dXT2"), Buf("dCOMB"), Buf("dACC")
        if layer == 0:
            CATt = sb(ph, nc, [128, 1024], BF16, n=2, name="catt")
            catT = sb(ph, nc, [128, 8, 128], BF16, n=2, name="catT")
        else:
            YG = sb(ph, nc, [128, 8, 512], F32, name="yg")
            YSQ = sb(ph, nc, [128, 512], F32, n=2, name="ysq")
            Mm = sb(ph, nc, [128, 512], F32, name="mm_")
            MSQ = sb(ph, nc, [128, 512], F32, name="msq")
            RS = sb(ph, nc, [128, 512], F32, name="rs")
            TA = sb(ph, nc, [128, 512], F32, n=2, name="ta")
            TB = sb(ph, nc, [128, 512], F32, n=2, name="tb")
            HN = sb(ph, nc, [128, 8, 512], BF16, name="hn")
            pSum = pTf[0]
            pSq = pTf[1]
            lnr = sb(ph, nc, [16, 128], F32, name="lnr")
            LNP = sb(ph, nc, [128, 16], F32, name="lnp")
            k.dma("sp", lnr.t[0:8, :], d["conv_ln_g"], wp=[lnr.b])
            k.dma("sp", lnr.t[8:16, :], d["conv_ln_b"], wp=[lnr.b])
            k.tr(pLg.t[:, 0:16] if E >= 16 else pTf[0].t[:, 0:16], lnr.t[:], c.ident_f.t[0:16, 0:16], r=[lnr.b], w=[pLg.b if E >= 16 else pTf[0].b])
            k.cp("dve", LNP.t[:], pLg.t[:, 0:16] if E >= 16 else pTf[0].t[:, 0:16], r=[pLg.b if E >= 16 else pTf[0].b], w=[LNP.b])
            b2bc = sb(ph, nc, [128, 1024], F32, name="b2bc")
            B2M = sb(ph, nc, [128, 1024], F32, name="b2m")
            k.dma("sp", b2bc.t[:], d["conv_b2"].partition_broadcast(128), w=[b2bc.b])
            k.tt("dve", B2M.t[:], b2bc.t[:], c.MOD.t[:, 2048:3072], ALU.mult, r=[b2bc.b, c.MOD.b], w=[B2M.b])

        it = 0
        for g in range(NG):
            xt2g = XT2g[g % 2]
            if layer == 1:
                k.dma("sp", YG.t[:], d["YD"][:, :, g * 512:(g + 1) * 512], w=[YG.b])
                for cch in range(8):
                    ysq = YSQ[cch % 2]
                    k.act(ysq.t[:], YG.t[:, cch, :], AF.Square, r=[YG.b], w=[ysq.b])
                    k.mm(pSum.t[:], c.ones_f.t[:], YG.t[:, cch, :], cch == 0, cch == 7, r=[YG.b, c.ones_f.b], w=[pSum.b] if cch == 0 else (), wp=() if cch == 0 else [pSum.b])
                    k.mm(pSq.t[:], c.ones_f.t[:], ysq.t[:], cch == 0, cch == 7, r=[ysq.b, c.ones_f.b], w=[pSq.b] if cch == 0 else (), wp=() if cch == 0 else [pSq.b])
                k.act(Mm.t[:], pSum.t[:], AF.Copy, r=[pSum.b], w=[Mm.b], scale=1.0 / 1024)
                k.tt("pool", MSQ.t[:], Mm.t[:], Mm.t[:], ALU.mult, r=[Mm.b], w=[MSQ.b])
                k.stt(RS.t[:], pSq.t[:], 1.0 / 1024, MSQ.t[:], ALU.mult, ALU.subtract, r=[pSq.b, MSQ.b], w=[RS.b])
                k.ts("dve", RS.t[:], RS.t[:], EPS, ALU.add, r=[RS.b], w=[RS.b])
                k.act(RS.t[:], RS.t[:], AF.Sqrt, r=[RS.b], w=[RS.b])
                k.recip(RS.t[:], RS.t[:], r=[RS.b], w=[RS.b])
                for cch in range(8):
                    ta, tb = TA[cch % 2], TB[cch % 2]
                    k.tt("dve", ta.t[:], YG.t[:, cch, :], Mm.t[:], ALU.subtract, r=[YG.b, Mm.b], w=[ta.b])
                    k.tt("pool", tb.t[:], ta.t[:], RS.t[:], ALU.mult, r=[ta.b, RS.b], w=[tb.b])
                    k.act(HN.t[:, cch, :], tb.t[:], AF.Silu, r=[tb.b, LNP.b], wp=[HN.b], scale=LNP.t[:, cch:cch + 1], bias=LNP.t[:, 8 + cch:9 + cch])
            states = {}

            def front(j):
                nonlocal it
                t = g * 4 + j
                rows = slice(t * 128, (t + 1) * 128)
                xin, hs, xb = XIN[it % 2], Hs[it % 2], XB[it % 2]
                lg = LG[it % 2]
                V1, XF = V1s[it % 2], XFs[it % 2]
                k.dma("sp", xin.t[:], hin[rows, :], w=[xin.b])
                if layer == 0:
                    cat, ctT = CATt[it % 2], catT[it % 2]
                    k.dma("sp", cat.t[:], d["CAT"][rows, :], w=[cat.b])
                    for kk in range(8):
                        k.tr(pT.t[:, kk * 128:(kk + 1) * 128], cat.t[:, kk * 128:(kk + 1) * 128], c.ident_b.t[:], r=[cat.b],
                             w=[pT.b] if kk == 0 else (), wp=() if kk == 0 else [pT.b])
                    k.cp("act", ctT.t[:].rearrange("p k t -> p (k t)"), pT.t[:], r=[pT.b], w=[ctT.b])
                    lhs = lambda kk: ctT.t[:, kk, :]
                    lb_ = ctT.b
                else:
                    lhs = lambda kk: HN.t[:, kk, j * 128:(j + 1) * 128]
                    lb_ = HN.b
                it += 1
                for half in range(2):
                    for kk in range(8):
                        k.mm(pY[half].t[:], lhs(kk), Wo.t[:, kk, half * 512:(half + 1) * 512], kk == 0, kk == 7,
                             r=[lb_, Wo.b], w=[pY[half].b] if kk == 0 else (), wp=() if kk == 0 else [pY[half].b])
                    k.tt("dve", V1.t[:, half * 512:(half + 1) * 512], pY[half].t[:], c.MOD.t[:, 2048 + half * 512:2048 + (half + 1) * 512], ALU.mult,
                         r=[pY[half].b, c.MOD.b], wp=[V1.b])
                if layer == 1:
                    k.tt("pool", xin.t[:], xin.t[:], B2M.t[:], ALU.add, r=[xin.b, B2M.b], w=[xin.b])
                k.tt("pool", hs.t[:], V1.t[:], xin.t[:], ALU.add, r=[V1.b, xin.b], w=[hs.b])
                k.dma("sp", hout[rows, :], hs.t[:], r=[hs.b], wp=[dH])
                norm_mod(c, hs, A2, c.MOD.t[:, 3072:4096], c.MOD.b, [(XF.t[:], "dve", XF), (xb.t[:], "pool", xb)], tmps[it % 2], j)

                states[j] = (t, rows, hs, xb, lg, XF)

            def back(j):
                t, rows, hs, xb, lg, XF = states[j]
                XFT_, CT_, ACs_ = XFTs[t % 2], CTs[t % 2], ACss[t % 2]
                for kk in range(8):
                    k.tr(pT2.t[:, kk * 128:(kk + 1) * 128], xb.t[:, kk * 128:(kk + 1) * 128], c.ident_b.t[:], r=[xb.b],
                         w=[pT2.b] if kk == 0 else (), wp=() if kk == 0 else [pT2.b])
                k.cp("act", xt2g.t[:, :, j * 128:(j + 1) * 128], pT2.t[:].rearrange("p (k t) -> p k t", k=8), r=[pT2.b], wp=[xt2g.b])
                for kk in range(8):
                    pf = pTf[kk // 4]
                    k.tr(pf.t[:, (kk % 4) * 128:(kk % 4 + 1) * 128], XF.t[:, kk * 128:(kk + 1) * 128], c.ident_f.t[:], r=[XF.b],
                         w=[pf.b] if kk % 4 == 0 else (), wp=() if kk % 4 == 0 else [pf.b])
                for hf in range(2):
                    k.cp("act" if hf else "dve", XFT_.t[:, hf * 4:(hf + 1) * 4, :], pTf[hf].t[:].rearrange("p (k t) -> p k t", k=4), r=[pTf[hf].b], wp=[XFT_.b])
                for kk in range(8):
                    k.mm(pLg.t[:], XFT_.t[:, kk, :], RW.t[:, kk, :], kk == 0, kk == 7, r=[XFT_.b, RW.b], w=[pLg.b] if kk == 0 else (), wp=() if kk == 0 else [pLg.b])
                L0, MK, EX, EXM, MS = (lg.t[:, 0:E], lg.t[:, E:2 * E], lg.t[:, 2 * E:3 * E], lg.t[:, 3 * E:4 * E], lg.t[:, 4 * E:4 * E + 32])
                k.tt("dve", L0, pLg.t[:], RB.t[:], ALU.add, r=[pLg.b, RB.b], wp=[lg.b])
                P.op("dve", (lambda e, o=MS[:, 0:8], i_=L0: e.max(out=o, in_=i_)), reads=[lg.b], wpart=[lg.b])
                k.ts("dve", MK, L0, MS[:, 3:4], ALU.is_ge, r=[lg.b], wp=[lg.b])
                k.ts("dve", MS[:, 8:9], MS[:, 0:1], -1.0, ALU.mult, r=[lg.b], wp=[lg.b])
                k.act(EX, L0, AF.Exp, r=[lg.b], wp=[lg.b], bias=MS[:, 8:9])
                k.stt(EXM, EX, 1.0, MK, ALU.mult, ALU.mult, r=[lg.b], wp=[lg.b], accum=MS[:, 9:10])
                k.recip(MS[:, 10:11], MS[:, 9:10], r=[lg.b], wp=[lg.b])
                k.ts("dve", EXM, EXM, MS[:, 10:11], ALU.mult, r=[lg.b], wp=[lg.b])
                k.dma("sp", d["COMB"][rows, :], EXM, r=[lg.b], wp=[dCOMB])
                k.dma("sp", d["MK"][rows, :], MK, r=[lg.b], wp=[dCOMB])
                k.dma("sp", d["XM2"][rows, :], xb.t[:], r=[xb.b], wp=[dXT2])
                k.tr(pCT.t[:], EXM, c.ident_f.t[:], r=[lg.b], w=[pCT.b])
                k.cp("act", CT_.t[:], pCT.t[:], r=[pCT.b], w=[CT_.b])
                for half in range(2):
                    k.mm(pTf[half].t[:], CT_.t[:], B2t.t[:, half * 512:(half + 1) * 512], True, True, r=[CT_.b, B2t.b], w=[pTf[half].b])
                    k.cp("act" if half else "dve", ACs_.t[:, half * 512:(half + 1) * 512], pTf[half].t[:], r=[pTf[half].b], wp=[ACs_.b])
                k.dma("sp", d["ACCd"][rows, :], ACs_.t[:], r=[ACs_.b], wp=[dACC])

            front(0)
            for j in range(4):
                if j + 1 < 4:
                    front(j + 1)
                back(j)
            k.dma("sp", d["XT2"][:, :, g * 512:(g + 1) * 512], xt2g.t[:], r=[xt2g.b], wp=[dXT2])
        P.end_phase()


def phase_moe(c, layer, MOD5):
    nc, P, k, d, cfg = c.nc, c.P, c.k, c.d, c.cfg
    S, E, MB = cfg.S, cfg.E, cfg.MB
    NTB = MB // 128
    NH = MB // 512
    hin = d["H1"] if layer == 0 else d["H3"]
    hout = d["H2"] if layer == 0 else d["out"]
    with ExitStack() as ph:
        nb1 = (E * 16) // 128
        B1T = sb(ph, nc, [128, E * 16], F32, name="b1t")
        b1r = sb(ph, nc, [128, 128], F32, n=2, name="b1r")
        ACC = sb(ph, nc, [128, NTB, 1024], F32, name="acc")
        XT2b = sb(ph, nc, [128, 8, MB], BF16, name="xt2b")
        CMB = sb(ph, nc, [128, NTB, E], F32, name="cmb")
        W1 = sb(ph, nc, [128, 8, 2048], BF16, n=2, name="w1")
        W2 = sb(ph, nc, [128, 8, 1024], BF16, name="w2")
        ACTT = sb(ph, nc, [128, 8, 512], BF16, n=2, name="actt")
        G1 = sb(ph, nc, [128, 512], F32, n=2, name="g1")
        S1 = sb(ph, nc, [128, 512], F32, n=2, name="s1")
        L1 = sb(ph, nc, [128, 512], F32, n=2, name="l1")
        L2 = sb(ph, nc, [128, 512], F32, n=2, name="l2")
        GS = sb(ph, nc, [128, 512], F32, n=2, name="gs")
        HT = sb(ph, nc, [128, 1024], F32, n=2, name="ht")
        pG = ps(ph, nc, [128, 512], F32, n=2, name="pG")
        pL = ps(ph, nc, [128, 512], F32, n=2, name="pL")
        pO = ps(ph, nc, [128, 512], F32, n=4, name="pO")
        dOUT = Buf("dOUT")
        for i in range(nb1):
            br = b1r[i % 2]
            k.dma("sp", br.t[:], d["moe_b1"][layer, i * 128:(i + 1) * 128, :], w=[br.b])
            k.tr(pG[i % 2].t[:, 0:128], br.t[:], c.ident_f.t[:], r=[br.b], w=[pG[i % 2].b])
            k.cp("dve", B1T.t[:, i * 128:(i + 1) * 128], pG[i % 2].t[:, 0:128], r=[pG[i % 2].b], wp=[B1T.b])
        wi = 0
        io = 0
        for blk in range(S // MB):
            t0 = blk * NTB
            rows = slice(blk * MB, (blk + 1) * MB)
            k.dma("sp", ACC.t[:], d["ACCd"][rows, :].rearrange("(t p) n -> p t n", p=128), w=[ACC.b])
            k.dma("sp", XT2b.t[:], d["XT2"][:, :, rows], w=[XT2b.b])
            k.dma("sp", CMB.t[:], d["COMB"][rows, :].rearrange("(t p) e -> p t e", p=128), w=[CMB.b])
            for e in range(E):
                w1 = W1[wi % 2]
                wi += 1
                w1v = d["moe_w1"][layer, e].rearrange("(k p) n -> p k n", p=128)
                k.dma("pool", w1.t[:, 0:4, :], w1v[:, 0:4, :], wp=[w1.b])
                k.dma("pool", w1.t[:, 4:8, :], w1v[:, 4:8, :], wp=[w1.b])
                w2_loaded = False
                for ht in range(NH):
                    actt = ACTT[io % 2]
                    for pr in range(8):
                        pg, pl = pG[pr % 2], pL[pr % 2]
                        g1, s1, l1, l2, gs = (X[pr % 2] for X in (G1, S1, L1, L2, GS))
                        for kk in range(8):
                            k.mm(pg.t[:], w1.t[:, kk, pr * 128:(pr + 1) * 128], XT2b.t[:, kk, ht * 512:(ht + 1) * 512], kk == 0, kk == 7,
                                 r=[w1.b, XT2b.b], w=[pg.b] if kk == 0 else (), wp=() if kk == 0 else [pg.b])
                        for kk in range(8):
                            k.mm(pl.t[:], w1.t[:, kk, 1024 + pr * 128:1024 + (pr + 1) * 128], XT2b.t[:, kk, ht * 512:(ht + 1) * 512], kk == 0, kk == 7,
                                 r=[w1.b, XT2b.b], w=[pl.b] if kk == 0 else (), wp=() if kk == 0 else [pl.b])
                        bg = B1T.t[:, e * 16 + pr:e * 16 + pr + 1]
                        bl = B1T.t[:, e * 16 + 8 + pr:e * 16 + 8 + pr + 1]
                        k.ts("dve", g1.t[:], pg.t[:], bg, ALU.add, 7.0, ALU.min, r=[pg.b, B1T.b], w=[g1.b])
                        k.act(s1.t[:], g1.t[:], AF.Sigmoid, r=[g1.b], w=[s1.b], scale=1.702)
                        k.act(l1.t[:], pl.t[:], AF.Identity, r=[pl.b, B1T.b], w=[l1.b], bias=bl)
                        k.ts("dve", l2.t[:], l1.t[:], 7.0, ALU.min, -7.0, ALU.max, r=[l1.b], w=[l2.b])
                        k.tt("pool", gs.t[:], g1.t[:], s1.t[:], ALU.mult, r=[g1.b, s1.b], w=[gs.b])
                        k.stt(actt.t[:, pr, :], l2.t[:], 1.0, gs.t[:], ALU.add, ALU.mult, r=[l2.b, gs.b], wp=[actt.b])
                    if not w2_loaded:
                        k.dma("pool", W2.t[:], d["moe_w2"][layer, e].rearrange("(k p) n -> p k n", p=128), w=[W2.b])
                        w2_loaded = True
                    for sub in range(4):
                        tl = ht * 4 + sub
                        for half in range(2):
                            po = pO[io % 4]
                            io += 1
                            for jj in range(8):
                                k.mm(po.t[:], actt.t[:, jj, sub * 128:(sub + 1) * 128], W2.t[:, jj, half * 512:(half + 1) * 512], jj == 0, jj == 7,
                                     r=[actt.b, W2.b], w=[po.b] if jj == 0 else (), wp=() if jj == 0 else [po.b])
                            acc = ACC.t[:, tl, half * 512:(half + 1) * 512]
                            k.stt(acc, po.t[:], CMB.t[:, tl, e:e + 1], acc, ALU.mult, ALU.add, r=[po.b, CMB.b, ACC.b], wp=[ACC.b])
            for tl in range(NTB):
                t = t0 + tl
                ht_ = HT[tl % 2]
                k.dma("sp", ht_.t[:], hin[t * 128:(t + 1) * 128, :], w=[ht_.b])
                k.tt("dve", ACC.t[:, tl, :], ACC.t[:, tl, :], MOD5.t[:], ALU.mult, r=[ACC.b, MOD5.b], wp=[ACC.b])
                k.tt("pool", ht_.t[:], ht_.t[:], ACC.t[:, tl, :], ALU.add, r=[ht_.b, ACC.b], w=[ht_.b])
                k.dma("sp", hout[t * 128:(t + 1) * 128, :], ht_.t[:], r=[ht_.b], wp=[dOUT])
        P.end_phase()


def phase_f(c):
    nc, P, k, d, cfg = c.nc, c.P, c.k, c.d, c.cfg
    S = cfg.S
    NG = S // 512
    with ExitStack() as ph:
        W = sb(ph, nc, [128, 8, 2048], BF16, name="Wc1")
        wv = d["conv_w1"].rearrange("(k p) n -> p k n", p=128)
        k.dma("pool", W.t[:, 0:4, :], wv[:, 0:4, :], wp=[W.b])
        k.dma("pool", W.t[:, 4:8, :], wv[:, 4:8, :], wp=[W.b])
        A = make_A(c, ph, "norm1_g", 1, 1024, c.MOD)
        b1r = sb(ph, nc, [16, 128], F32, name="b1r")
        CB1 = sb(ph, nc, [128, 16], F32, name="cb1")
        pB = ps(ph, nc, [128, 16], F32, name="pB")
        k.dma("sp", b1r.t[:], d["conv_b1"], w=[b1r.b])
        k.tr(pB.t[:], b1r.t[:], c.ident_f.t[0:16, 0:16], r=[b1r.b], w=[pB.b])
        k.cp("dve", CB1.t[:], pB.t[:], r=[pB.b], w=[CB1.b])
        Z = sb(ph, nc, [128, 8, 16], BF16, name="z")
        k.memset("dve", Z.t[:], 0.0, w=[Z.b])
        dGT = Buf("dGT")
        k.dma("sp", d["GT"][:, :, 0:16], Z.t[:], r=[Z.b], wp=[dGT])
        k.dma("sp", d["GT"][:, :, S + 16:S + 32], Z.t[:], r=[Z.b], wp=[dGT])
        XIN = sb(ph, nc, [128, 1024], F32, n=2, name="xin")
        tmps = [{"ss": sb(ph, nc, [128, 16], F32, name="ss"), "t1": sb(ph, nc, [128, 1024], F32, name="t1")} for _ in range(2)]
        XM = sb(ph, nc, [128, 1024], BF16, n=2, name="xm")
        XTG = sb(ph, nc, [128, 8, 512], BF16, n=2, name="xtg")
        SG = sb(ph, nc, [128, 512], F32, n=2, name="sg")
        GTs = sb(ph, nc, [128, 8, 512], BF16, n=2, name="gts")
        pT = ps(ph, nc, [128, 1024], BF16, name="pT")
        pA = ps(ph, nc, [128, 512], F32, n=2, name="pA")
        pGt = ps(ph, nc, [128, 512], F32, n=2, name="pGt")
        it = 0
        for g in range(NG):
            xtg, gts = XTG[g % 2], GTs[g % 2]
            for j in range(4):
                t = 4 * g + j
                xin, xm = XIN[it % 2], XM[it % 2]
                it += 1
                k.dma("sp", xin.t[:], d["H2"][t * 128:(t + 1) * 128, :], w=[xin.b])
                norm_mod(c, xin, A, c.MOD.t[:, 0:1024], c.MOD.b, [(xm.t[:], "pool", xm)], tmps[it % 2], j)
                for kk in range(8):
                    k.tr(pT.t[:, kk * 128:(kk + 1) * 128], xm.t[:, kk * 128:(kk + 1) * 128], c.ident_b.t[:], r=[xm.b],
                         w=[pT.b] if kk == 0 else (), wp=() if kk == 0 else [pT.b])
                k.cp("act", xtg.t[:, :, j * 128:(j + 1) * 128], pT.t[:].rearrange("p (k t) -> p k t", k=8), r=[pT.b], wp=[xtg.b])
            for cp_ in range(8):
                pa, pg, sg = pA[cp_ % 2], pGt[cp_ % 2], SG[cp_ % 2]
                for kk in range(8):
                    k.mm(pa.t[:], W.t[:, kk, cp_ * 128:(cp_ + 1) * 128], xtg.t[:, kk, :], kk == 0, kk == 7, r=[W.b, xtg.b],
                         w=[pa.b] if kk == 0 else (), wp=() if kk == 0 else [pa.b])
                for kk in range(8):
                    k.mm(pg.t[:], W.t[:, kk, 1024 + cp_ * 128:1024 + (cp_ + 1) * 128], xtg.t[:, kk, :], kk == 0, kk == 7, r=[W.b, xtg.b],
                         w=[pg.b] if kk == 0 else (), wp=() if kk == 0 else [pg.b])
                k.act(sg.t[:], pg.t[:], AF.Sigmoid, r=[pg.b, CB1.b], w=[sg.b], bias=CB1.t[:, 8 + cp_:9 + cp_])
                k.stt(gts.t[:, cp_, :], pa.t[:], CB1.t[:, cp_:cp_ + 1], sg.t[:], ALU.add, ALU.mult, r=[pa.b, sg.b, CB1.b], wp=[gts.b])
            k.dma("sp", d["GT"][:, :, 16 + g * 512:16 + (g + 1) * 512], gts.t[:], r=[gts.b], wp=[dGT])
        P.end_phase()


def phase_g1(c):
    nc, P, k, d, cfg = c.nc, c.P, c.k, c.d, c.cfg
    S = cfg.S
    NG = S // 512
    with ExitStack() as ph:
        dwr = sb(ph, nc, [32, 1024], F32, name="dwr")
        DWT = sb(ph, nc, [128, 8, 31], F32, name="dwt")
        dbr = sb(ph, nc, [8, 128], F32, name="dbr")
        DWB = sb(ph, nc, [128, 8], F32, name="dwb")
        DG = sb(ph, nc, [128, 8, 31, 128], BF16, name="dg")
        pD = ps(ph, nc, [128, 8, 32], F32, name="pD")
        pB = ps(ph, nc, [128, 8], F32, name="pB")
        k.dma("sp", dwr.t[0:31, :], d["conv_dw"], w=[dwr.b])
        for cch in range(8):
            k.tr(pD.t[:, cch, 0:31], dwr.t[0:31, cch * 128:(cch + 1) * 128], c.ident_f.t[0:31, 0:31], r=[dwr.b],
                 w=[pD.b] if cch == 0 else (), wp=() if cch == 0 else [pD.b])
        k.cp("dve", DWT.t[:], pD.t[:, :, 0:31], r=[pD.b], w=[DWT.b])
        k.dma("sp", dbr.t[:], d["conv_dw_b"], w=[dbr.b])
        k.tr(pB.t[:], dbr.t[:], c.ident_f.t[0:8, 0:8], r=[dbr.b], w=[pB.b])
        k.cp("dve", DWB.t[:], pB.t[:], r=[pB.b], w=[DWB.b])
        n_ = 0
        for cch in range(8):
            for j in range(31):
                k.ts("dve" if n_ % 2 else "pool", DG.t[:, cch, j, :], c.ident_f.t[:], DWT.t[:, cch, j:j + 1], ALU.mult, r=[DWT.b, c.ident_f.b], wp=[DG.b])
                n_ += 1
        GTw = sb(ph, nc, [128, 8, 544], BF16, n=2, name="gtw")
        Ys = sb(ph, nc, [128, 8, 512], F32, n=2, name="ys")
        pC = ps(ph, nc, [128, 512], F32, n=4, name="pC")
        dYD = Buf("dYD")
        for g in range(NG):
            gtw, ys = GTw[g % 2], Ys[g % 2]
            k.dma("sp", gtw.t[:], d["GT"][:, :, g * 512:g * 512 + 544], w=[gtw.b])
            for cch in range(8):
                pc = pC[cch % 4]
                for j in range(31):
                    k.mm(pc.t[:], DG.t[:, cch, j, :], gtw.t[:, cch, j + 1:j + 513], j == 0, j == 30, r=[DG.b, gtw.b],
                         w=[pc.b] if j == 0 else (), wp=() if j == 0 else [pc.b])
                k.act(ys.t[:, cch, :], pc.t[:], AF.Identity, r=[pc.b, DWB.b], wp=[ys.b], bias=DWB.t[:, cch:cch + 1])
            k.dma("sp", d["YD"][:, :, g * 512:(g + 1) * 512], ys.t[:], r=[ys.b], wp=[dYD])
        P.end_phase()


I32 = mybir.dt.int32


def pool_dma_op(P, fn, reads=(), writes=(), wpart=(), key=None):
    o = Op("pool", fn, P.phase)
    o.is_dma = True
    if key is None:
        key = (list(writes) + list(wpart))[0]
    if key not in P.keymap:
        P.keymap[key] = len(P.keymap)
        assert len(P.keymap) <= NDSEM
    o.key = P.keymap[key]
    P._deps(o, reads, writes, wpart)
    P.ops["pool"].append(o)
    P.order.append(o)
    return o


def phase_route(c, layer, TEi):
    nc, P, k, d, cfg = c.nc, c.P, c.k, c.d, c.cfg
    S, E, NT, NTILE, NSLOT = cfg.S, cfg.E, cfg.NT, cfg.NTILE, cfg.NSLOT
    with ExitStack() as ph:
        MKf = sb(ph, nc, [128, NT, E], F32, name="mkf")
        MKb = sb(ph, nc, [128, NT, E], BF16, name="mkb")
        CMB = sb(ph, nc, [128, NT, E], F32, name="cmb")
        UTf = sb(ph, nc, [128, 128], F32, name="utf")
        UT = sb(ph, nc, [128, 128], BF16, name="ut")
        ONb = sb(ph, nc, [128, 128], BF16, name="onb")
        IOTA = sb(ph, nc, [128, 1], F32, name="iota")
        TH = sb(ph, nc, [1, E * 16], F32, name="th")
        J5 = sb(ph, nc, [1, NTILE * E], F32, name="j5")
        dSLOT, dInit = Buf("dSLOT"), Buf("dInit")
        k.dma("sp", d["SLOT"], d["k_slotinit"], w=[dSLOT, dInit])
        k.dma("sp", MKf.t[:], d["MK"].rearrange("(t p) e -> p t e", p=128), w=[MKf.b])
        k.dma("sp", CMB.t[:], d["COMB"].rearrange("(t p) e -> p t e", p=128), w=[CMB.b])
        k.dma("sp", UTf.t[:], d["k_ut"], w=[UTf.b])
        k.dma("sp", IOTA.t[:], d["k_iota"], w=[IOTA.b])
        k.dma("sp", TH.t[:], d["k_th"], w=[TH.b])
        k.dma("sp", J5.t[:], d["k_j512"], w=[J5.b])
        k.cp("dve", UT.t[:], UTf.t[:], r=[UTf.b], w=[UT.b])
        k.cp("pool", MKb.t[:], MKf.t[:], r=[MKf.b], w=[MKb.b])
        k.memset("dve", ONb.t[:], 1.0, w=[ONb.b])
        pC = ps(ph, nc, [1, E], F32, name="pC")
        pSB = ps(ph, nc, [128, E], F32, name="pSB")
        pR = ps(ph, nc, [128, E], F32, n=2, name="pR")
        V = sb(ph, nc, [1, 8 * E], F32, name="v")
        C16 = sb(ph, nc, [1, E * 16], F32, name="c16")
        CJ = sb(ph, nc, [1, NTILE * E], F32, name="cj")
        TEf = sb(ph, nc, [1, NTILE], F32, name="tef")
        SEGB = sb(ph, nc, [128, E], F32, name="segb")
        for t in range(NT):
            k.mm(pC.t[:], ONb.t[:, 0:1], MKb.t[:, t, :], t == 0, t == NT - 1, r=[ONb.b, MKb.b], w=[pC.b] if t == 0 else (), wp=() if t == 0 else [pC.b])
        cnt, ntl, c512, inc, segs, one = (V.t[:, i * E:(i + 1) * E] for i in range(6))
        k.cp("dve", cnt, pC.t[:], r=[pC.b], wp=[V.b])
        k.tt("dve", C16.t[:].rearrange("o (e m) -> o e m", m=16), cnt.unsqueeze(2).to_broadcast([1, E, 16]), TH.t[:].rearrange("o (e m) -> o e m", m=16), ALU.is_gt,
             r=[V.b, TH.b], w=[C16.b])
        k.red(ntl, C16.t[:].rearrange("o (e m) -> o e m", m=16), r=[C16.b], wp=[V.b])
        k.ts("dve", c512, ntl, 512.0, ALU.mult, r=[V.b], wp=[V.b])
        k.memset("dve", one, 1.0, wp=[V.b])
        P.op("dve", (lambda e_, o=inc, a=one, b_=c512: e_.tensor_tensor_scan(out=o, data0=a, data1=b_, initial=0.0, op0=ALU.mult, op1=ALU.add)),
             reads=[V.b], wpart=[V.b])
        k.tt("dve", segs, inc, c512, ALU.subtract, r=[V.b], wp=[V.b])
        k.tt("dve", CJ.t[:].rearrange("o (j e) -> o j e", e=E), segs.unsqueeze(1).to_broadcast([1, NTILE, E]), J5.t[:].rearrange("o (j e) -> o j e", e=E), ALU.is_le,
             r=[V.b, J5.b], w=[CJ.b])
        k.red(TEf.t[:], CJ.t[:].rearrange("o (j e) -> o j e", e=E), r=[CJ.b], w=[TEf.b])
        k.ts("dve", TEf.t[:], TEf.t[:], -1.0, ALU.add, 0.0, ALU.max, r=[TEf.b], w=[TEf.b])
        IDXW, IDXB = TEi
        KP = sb(ph, nc, [128, 9], F32, name="kp")
        k.dma("sp", KP.t[:], d["k_kp"], w=[KP.b])
        pTE = ps(ph, nc, [128, NTILE], F32, name="pTE")
        TEb = sb(ph, nc, [128, NTILE], F32, name="teb")
        XW = sb(ph, nc, [128, NTILE, 8], F32, name="xw")
        k.mm(pTE.t[:], c.ones_f.t[0:1, :], TEf.t[:], True, True, r=[TEf.b, c.ones_f.b], w=[pTE.b])
        k.cp("dve", TEb.t[:], pTE.t[:], r=[pTE.b], w=[TEb.b])
        for kk in range(8):
            k.ts("dve", XW.t[:, :, kk], TEb.t[:], 1024.0, ALU.mult, KP.t[:, kk:kk + 1], ALU.add, r=[TEb.b, KP.b], wp=[XW.b])
        if layer:
            k.ts("dve", XW.t[:], XW.t[:], float(layer * E * 1024), ALU.add, r=[XW.b], w=[XW.b])
        k.cp("dve", IDXW.t[:], XW.t[:], r=[XW.b], w=[IDXW.b])
        k.ts("dve", TEb.t[:], TEb.t[:], 16.0, ALU.mult, KP.t[:, 8:9], ALU.add, r=[TEb.b, KP.b], w=[TEb.b])
        if layer:
            k.ts("dve", TEb.t[:], TEb.t[:], float(layer * E * 16), ALU.add, r=[TEb.b], w=[TEb.b])
        k.cp("dve", IDXB.t[:], TEb.t[:], r=[TEb.b], w=[IDXB.b])
        k.mm(pSB.t[:], c.ones_f.t[0:1, :], segs, True, True, r=[V.b, c.ones_f.b], w=[pSB.b])
        k.cp("dve", SEGB.t[:], pSB.t[:], r=[pSB.b], w=[SEGB.b])
        POS = sb(ph, nc, [128, E], F32, n=2, name="pos")
        T8 = sb(ph, nc, [128, 8], F32, n=2, name="t8")
        OH = sb(ph, nc, [128, E], F32, n=2, name="oh")
        JK = sb(ph, nc, [128, E], F32, n=2, name="jk")
        P4 = sb(ph, nc, [128, 4], F32, n=2, name="p4")
        P4i = sb(ph, nc, [128, 4], I32, n=2, name="p4i")
        SR = sb(ph, nc, [128, 4, 2], F32, n=2, name="sr")
        for i in range(NT):
            pr = pR[i % 2]
            pos, t8, p4, p4i, sr = POS[i % 2], T8[i % 2], P4[i % 2], P4i[i % 2], SR[i % 2]
            for ip in range(i):
                k.mm(pr.t[:], ONb.t[:], MKb.t[:, ip, :], ip == 0, False, r=[ONb.b, MKb.b], w=[pr.b] if ip == 0 else (), wp=() if ip == 0 else [pr.b])
            k.mm(pr.t[:], UT.t[:], MKb.t[:, i, :], i == 0, True, r=[UT.b, MKb.b], w=[pr.b] if i == 0 else (), wp=() if i == 0 else [pr.b])
            k.tt("dve", pos.t[:], pr.t[:], SEGB.t[:], ALU.add, r=[pr.b, SEGB.b], w=[pos.b])
            P.op("dve", (lambda e_, o=t8.t[:], a=CMB.t[:, i, :]: e_.max(out=o, in_=a)), reads=[CMB.b], writes=[t8.b])
            for kq in range(4):
                oh, jk = OH[kq % 2], JK[kq % 2]
                k.ts("dve", oh.t[:], CMB.t[:, i, :], t8.t[:, kq:kq + 1], ALU.is_equal, r=[CMB.b, t8.b], w=[oh.b])
                k.stt(jk.t[:], oh.t[:], 1.0, pos.t[:], ALU.mult, ALU.mult, r=[oh.b, pos.b], w=[jk.b], wp=[p4.b], accum=p4.t[:, kq:kq + 1])
                k.ts("pool", sr.t[:, kq, 0:1], IOTA.t[:], float(i * 128), ALU.add, r=[IOTA.b], wp=[sr.b])
                k.cp("pool", sr.t[:, kq, 1:2], t8.t[:, kq:kq + 1], r=[t8.b], wp=[sr.b])
            k.ts("dve", p4.t[:], p4.t[:], float(NSLOT - 1), ALU.min, r=[p4.b], w=[p4.b])
            k.cp("dve", p4i.t[:], p4.t[:], r=[p4.b], w=[p4i.b])
            for kq in range(4):
                def sca(e_, off=p4i.t[:, kq:kq + 1], src=sr.t[:, kq, :]):
                    return e_.indirect_dma_start(out=d["SLOT"], out_offset=bass.IndirectOffsetOnAxis(ap=off, axis=0), in_=src, in_offset=None)
                pool_dma_op(P, sca, reads=[p4i.b, sr.b, dInit], wpart=[dSLOT])
        P.end_phase()


def phase_smoe(c, layer, MOD5, TEi):
    nc, P, k, d, cfg = c.nc, c.P, c.k, c.d, c.cfg
    S, E, NTILE = cfg.S, cfg.E, cfg.NTILE
    IDXW, IDXB = TEi
    hin = d["H1"] if layer == 0 else d["H3"]
    hout = d["H2"] if layer == 0 else d["out"]
    w1tab = d["moe_w1"].rearrange("l e k n -> (l e k) n")
    w2tab = d["moe_w2"].rearrange("l e k n -> (l e k) n")
    b1tab = d["moe_b1"].rearrange("l r f -> (l r) f")
    with ExitStack() as ph:
        Z = sb(ph, nc, [128, 1024], BF16, name="z")
        W1 = sb(ph, nc, [128, 8, 2048], BF16, n=2, name="w1")
        W2 = sb(ph, nc, [128, 8, 1024], BF16, name="w2")
        B1r = sb(ph, nc, [128, 128], F32, n=2, name="b1r")
        B1c = sb(ph, nc, [128, 16], F32, n=2, name="b1c")
        SLt = sb(ph, nc, [128, 4, 2], F32, n=2, name="slt")
        TKi = sb(ph, nc, [128, 4], I32, n=2, name="tki")
        XG = sb(ph, nc, [128, 4, 1024], BF16, n=2, name="xg")
        XT = sb(ph, nc, [128, 8, 512], BF16, n=2, name="xt")
        ACTT = sb(ph, nc, [128, 8, 512], BF16, n=2, name="actt")
        G1 = sb(ph, nc, [128, 512], F32, n=2, name="g1")
        S1 = sb(ph, nc, [128, 512], F32, n=2, name="s1")
        L1 = sb(ph, nc, [128, 512], F32, n=2, name="l1")
        L2 = sb(ph, nc, [128, 512], F32, n=2, name="l2")
        GS = sb(ph, nc, [128, 512], F32, n=2, name="gs")
        OS = sb(ph, nc, [128, 1024], F32, n=4, name="os")
        HT = sb(ph, nc, [128, 1024], F32, n=2, name="ht")
        AT = sb(ph, nc, [128, 1024], F32, n=2, name="at")
        pT = ps(ph, nc, [128, 1024], BF16, n=2, name="pT")
        pG = ps(ph, nc, [128, 512], F32, n=2, name="pG")
        pL = ps(ph, nc, [128, 512], F32, n=2, name="pL")
        pO = ps(ph, nc, [128, 512], F32, n=2, name="pO")
        dACC, dXM2z, dOUT = Buf("dACCs"), Buf("dXM2z"), Buf("dOUT")
        k.memset("dve", Z.t[:], 0.0, w=[Z.b])
        k.dma("sp", d["XM2"][S:S + 128, :], Z.t[:], r=[Z.b], w=[dXM2z])

        def gather(out_ap, tab, idx_ap, reads, wslot, part=False):
            def g(e_):
                return e_.indirect_dma_start(out=out_ap, out_offset=None, in_=tab, in_offset=bass.IndirectOffsetOnAxis(ap=idx_ap, axis=0))
            return pool_dma_op(P, g, reads=reads, writes=() if part else [wslot], wpart=[wslot] if part else ())

        def loads(j):
            w1, b1r, slt, tki, xg = W1[j % 2], B1r[j % 2], SLt[j % 2], TKi[j % 2], XG[j % 2]
            k.dma("sp", slt.t[:], d["SLOT"][j * 512:(j + 1) * 512, :].rearrange("(s p) c -> p s c", p=128), w=[slt.b])
            k.cp("dve", tki.t[:], slt.t[:, :, 0], r=[slt.b], w=[tki.b])
            for kk in range(8):
                gather(w1.t[:, kk, :], w1tab, IDXW.t[:, j, kk:kk + 1], [IDXW.b], w1.b, part=True)
            gather(b1r.t[:], b1tab, IDXB.t[:, j:j + 1], [IDXB.b], b1r.b)
            for sub in range(4):
                gather(xg.t[:, sub, :], d["XM2"], tki.t[:, sub:sub + 1], [tki.b, dXM2z], xg.b, part=True)

        io = 0
        loads(0)
        for j in range(NTILE):
            if j + 1 < NTILE:
                loads(j + 1)
            w1, b1r, b1c, slt, tki, xg, xt, actt = (X[j % 2] for X in (W1, B1r, B1c, SLt, TKi, XG, XT, ACTT))
            k.tr(pG[0].t[:, 0:16], b1r.t[0:16, :], c.ident_f.t[0:16, 0:16], r=[b1r.b], w=[pG[0].b])
            k.cp("dve", b1c.t[:], pG[0].t[:, 0:16], r=[pG[0].b], w=[b1c.b])
            for sub in range(4):
                pt = pT[sub % 2]
                for kk in range(8):
                    k.tr(pt.t[:, kk * 128:(kk + 1) * 128], xg.t[:, sub, kk * 128:(kk + 1) * 128], c.ident_b.t[:], r=[xg.b],
                         w=[pt.b] if kk == 0 else (), wp=() if kk == 0 else [pt.b])
                k.cp("act" if sub % 2 else "dve", xt.t[:, :, sub * 128:(sub + 1) * 128], pt.t[:].rearrange("p (k t) -> p k t", k=8), r=[pt.b], wp=[xt.b])
            for pr in range(8):
                pg, pl = pG[pr % 2], pL[pr % 2]
                g1, s1, l1, l2, gs = (X[pr % 2] for X in (G1, S1, L1, L2, GS))
                for kk in range(8):
                    k.mm(pg.t[:], w1.t[:, kk, pr * 128:(pr + 1) * 128], xt.t[:, kk, :], kk == 0, kk == 7,
                         r=[w1.b, xt.b], w=[pg.b] if kk == 0 else (), wp=() if kk == 0 else [pg.b])
                for kk in range(8):
                    k.mm(pl.t[:], w1.t[:, kk, 1024 + pr * 128:1024 + (pr + 1) * 128], xt.t[:, kk, :], kk == 0, kk == 7,
                         r=[w1.b, xt.b], w=[pl.b] if kk == 0 else (), wp=() if kk == 0 else [pl.b])
                k.ts("dve", g1.t[:], pg.t[:], b1c.t[:, pr:pr + 1], ALU.add, 7.0, ALU.min, r=[pg.b, b1c.b], w=[g1.b])
                k.act(s1.t[:], g1.t[:], AF.Sigmoid, r=[g1.b], w=[s1.b], scale=1.702)
                k.act(l1.t[:], pl.t[:], AF.Identity, r=[pl.b, b1c.b], w=[l1.b], bias=b1c.t[:, 8 + pr:9 + pr])
                k.ts("dve", l2.t[:], l1.t[:], 7.0, ALU.min, -7.0, ALU.max, r=[l1.b], w=[l2.b])
                k.tt("dve", gs.t[:], g1.t[:], s1.t[:], ALU.mult, r=[g1.b, s1.b], w=[gs.b])
                k.stt(actt.t[:, pr, :], l2.t[:], 1.0, gs.t[:], ALU.add, ALU.mult, r=[l2.b, gs.b], wp=[actt.b])
            for kk in range(8):
                gather(W2.t[:, kk, :], w2tab, IDXW.t[:, j, kk:kk + 1], [IDXW.b], W2.b, part=True)
            for sub in range(4):
                os_ = OS[(4 * j + sub) % 4]
                for half in range(2):
                    po = pO[io % 2]
                    io += 1
                    for jj in range(8):
                        k.mm(po.t[:], actt.t[:, jj, sub * 128:(sub + 1) * 128], W2.t[:, jj, half * 512:(half + 1) * 512], jj == 0, jj == 7,
                             r=[actt.b, W2.b], w=[po.b] if jj == 0 else (), wp=() if jj == 0 else [po.b])
                    k.act(os_.t[:, half * 512:(half + 1) * 512], po.t[:], AF.Copy, r=[po.b, slt.b], wp=[os_.b], scale=slt.t[:, sub, 1:2])

                def sca(e_, off=tki.t[:, sub:sub + 1], src=os_.t[:]):
                    return e_.indirect_dma_start(out=d["ACCd"], out_offset=bass.IndirectOffsetOnAxis(ap=off, axis=0), in_=src, in_offset=None,
                                                 compute_op=ALU.add)
                pool_dma_op(P, sca, reads=[tki.b, os_.b], writes=[dACC])
        for t in range(S // 128):
            ht_, at_ = HT[t % 2], AT[t % 2]
            rows = slice(t * 128, (t + 1) * 128)
            k.dma("sp", ht_.t[:], hin[rows, :], w=[ht_.b])
            k.dma("sp", at_.t[:], d["ACCd"][rows, :], r=[dACC], w=[at_.b])
            k.tt("dve", at_.t[:], at_.t[:], MOD5.t[:], ALU.mult, r=[at_.b, MOD5.b], w=[at_.b])
            k.tt("pool", ht_.t[:], ht_.t[:], at_.t[:], ALU.add, r=[ht_.b, at_.b], w=[ht_.b])
            k.dma("sp", hout[rows, :], ht_.t[:], r=[ht_.b], wp=[dOUT])
        P.end_phase()
```

```python
from contextlib import ExitStack
import numpy as np
import ml_dtypes
import concourse.bass as bass
import concourse.mybir as mybir
from concourse.bass_utils import run_bass_kernel_spmd

F32 = mybir.dt.float32
BF16 = mybir.dt.bfloat16
AF = mybir.ActivationFunctionType
ALU = mybir.AluOpType
AX = mybir.AxisListType

COMPUTE = ("pe", "act", "dve", "pool")
ALLENG = ("pe", "act", "dve", "pool", "sp")
NDSEM = 72


class Buf:
    __slots__ = ("name", "writers", "readers")

    def __init__(self, name):
        self.name = name
        self.writers = []
        self.readers = []


class Op:
    __slots__ = ("eng", "fn", "raw", "oth", "signal", "tok_sem", "tok_val", "is_dma", "key", "phase")

    def __init__(self, eng, fn, phase):
        self.eng = eng
        self.fn = fn
        self.raw = []
        self.oth = []
        self.signal = False
        self.tok_sem = None
        self.tok_val = 0
        self.is_dma = False
        self.key = None
        self.phase = phase


class Prog:
    def __init__(self, nc, es):
        self.nc = nc
        self.phase = 0
        self.esem = {e: es.enter_context(nc.semaphore("s_" + e)) for e in COMPUTE}
        self.ecnt = {e: 0 for e in COMPUTE}
        self.dsem = [es.enter_context(nc.semaphore("d%d" % i)) for i in range(NDSEM)]
        self.dcnt = [0] * NDSEM
        self.seen = {e: {} for e in ALLENG}
        self._reset()
        self.nops = 0

    def _reset(self):
        self.ops = {e: [] for e in ALLENG}
        self.order = []
        self.keymap = {}
        self.last = {}

    def buf(self, name="b"):
        return Buf(name)

    def _deps(self, op, reads, writes, wpart):
        ph = self.phase
        for b in reads:
            for w in b.writers:
                if w.phase == ph:
                    op.raw.append(w)
        for b in list(writes) + list(wpart):
            for r in b.readers:
                if r.phase == ph:
                    op.oth.append(r)
        for b in writes:
            for w in b.writers:
                if w.phase == ph:
                    op.oth.append(w)
        for b in reads:
            b.readers.append(op)
        for b in writes:
            b.writers = [op]
            b.readers = []
        for b in wpart:
            if b.readers:
                b.writers = [op]
                b.readers = []
            else:
                b.writers.append(op)

    def op(self, eng, fn, reads=(), writes=(), wpart=()):
        o = Op(eng, fn, self.phase)
        self._deps(o, reads, writes, wpart)
        self.ops[eng].append(o)
        self.order.append(o)
        self.last[eng] = o
        return o

    def dma(self, q, out, in_, reads=(), writes=(), wpart=(), key=None, **kw):
        def fn(e, out=out, in_=in_, kw=kw):
            return e.dma_start(out=out, in_=in_, **kw)
        o = Op(q, fn, self.phase)
        o.is_dma = True
        if key is None:
            ws = list(writes) + list(wpart)
            key = ws[0]
        if key not in self.keymap:
            self.keymap[key] = len(self.keymap)
            assert len(self.keymap) <= NDSEM, "too many DMA keys in phase"
        o.key = self.keymap[key]
        self._deps(o, reads, writes, wpart)
        self.ops[q].append(o)
        self.order.append(o)
        return o

    def end_phase(self):
        nc = self.nc
        lasts = [self.last[e] for e in COMPUTE if e in self.last]
        lastd = {}
        for o in self.order:
            if o.is_dma:
                lastd[o.key] = o
        for e in ALLENG:
            o = Op(e, (lambda eng: eng.nop()), self.phase)
            o.raw = list(lasts) + list(lastd.values())
            self.ops[e].append(o)
            self.order.append(o)
        for o in self.order:
            for d in o.raw:
                if d.is_dma:
                    continue
                if d.eng == o.eng and o.eng == "pe" and not o.is_dma:
                    continue
                d.signal = True
            for d in o.oth:
                if d.is_dma:
                    continue
                if d.eng == o.eng and not o.is_dma:
                    continue
                d.signal = True
        for e in COMPUTE:
            for o in self.ops[e]:
                if o.is_dma:
                    continue
                if o.signal:
                    self.ecnt[e] += 1
                    o.tok_sem = self.esem[e]
                    o.tok_val = self.ecnt[e]
        for o in self.order:
            if o.is_dma:
                self.dcnt[o.key] += 16
                o.tok_sem = self.dsem[o.key]
                o.tok_val = self.dcnt[o.key]
        self.nops += len(self.order)

        with nc.Block() as block:
            def run(ename):
                def body(eng):
                    seen = self.seen[ename]
                    for o in self.ops[ename]:
                        need = {}
                        for d in o.raw:
                            if d.tok_sem is None:
                                continue
                            if (not d.is_dma) and d.eng == ename and ename == "pe" and not o.is_dma:
                                continue
                            s = d.tok_sem
                            if need.get(s.num, (None, 0))[1] < d.tok_val:
                                need[s.num] = (s, d.tok_val)
                        for d in o.oth:
                            if d.tok_sem is None:
                                continue
                            if (not d.is_dma) and d.eng == ename and not o.is_dma:
                                continue
                            s = d.tok_sem
                            if need.get(s.num, (None, 0))[1] < d.tok_val:
                                need[s.num] = (s, d.tok_val)
                        for s, v in need.values():
                            if seen.get(s.num, 0) < v:
                                eng.wait_ge(s, v)
                                seen[s.num] = v
                        ins = o.fn(eng)
                        if o.is_dma:
                            ins.then_inc(o.tok_sem, 16)
                        elif o.signal:
                            ins.then_inc(o.tok_sem, 1)
                return body

            block.tensor(run("pe"))
            block.scalar(run("act"))
            block.vector(run("dve"))
            block.gpsimd(run("pool"))
            block.sync(run("sp"))
        self.phase += 1
        self._reset()


class Slot:
    __slots__ = ("t", "b")

    def __init__(self, t, b):
        self.t = t
        self.b = b


class Ctx:
    pass


class K:
    def __init__(self, P):
        self.P = P

    def ts(self, eng, out, in0, s1, op0, s2=None, op1=None, r=(), w=(), wp=(), accum=None):
        if op1 is None:
            if accum is None:
                f = lambda e: e.tensor_scalar(out=out, in0=in0, scalar1=s1, scalar2=None, op0=op0)
            else:
                f = lambda e: e.tensor_scalar(out=out, in0=in0, scalar1=s1, scalar2=None, op0=op0, accum_out=accum)
        else:
            f = lambda e: e.tensor_scalar(out=out, in0=in0, scalar1=s1, scalar2=s2, op0=op0, op1=op1)
        return self.P.op(eng, f, reads=r, writes=w, wpart=wp)

    def tt(self, eng, out, in0, in1, op, r=(), w=(), wp=()):
        return self.P.op(eng, lambda e: e.tensor_tensor(out=out, in0=in0, in1=in1, op=op), reads=r, writes=w, wpart=wp)

    def stt(self, out, in0, scalar, in1, op0, op1, r=(), w=(), wp=(), accum=None):
        if accum is None:
            f = lambda e: e.scalar_tensor_tensor(out=out, in0=in0, scalar=scalar, in1=in1, op0=op0, op1=op1)
        else:
            f = lambda e: e.scalar_tensor_tensor(out=out, in0=in0, scalar=scalar, in1=in1, op0=op0, op1=op1, accum_out=accum)
        return self.P.op("dve", f, reads=r, writes=w, wpart=wp)

    def act(self, out, in_, func, r=(), w=(), wp=(), bias=None, scale=None, accum=None):
        kw = {}
        if bias is not None:
            kw["bias"] = bias
        if scale is not None:
            kw["scale"] = scale
        if accum is not None:
            kw["accum_out"] = accum
        return self.P.op("act", lambda e: e.activation(out=out, in_=in_, func=func, **kw), reads=r, writes=w, wpart=wp)

    def cp(self, eng, out, in_, r=(), w=(), wp=()):
        if eng == "act":
            return self.P.op("act", lambda e: e.copy(out=out, in_=in_), reads=r, writes=w, wpart=wp)
        return self.P.op(eng, lambda e: e.tensor_copy(out=out, in_=in_), reads=r, writes=w, wpart=wp)

    def memset(self, eng, ap, val, w=(), wp=()):
        return self.P.op(eng, lambda e: e.memset(ap, val), writes=w, wpart=wp)

    def mm(self, out, lhsT, rhs, start, stop, r=(), w=(), wp=()):
        return self.P.op("pe", lambda e: e.matmul(out, lhsT, rhs, start=start, stop=stop), reads=r, writes=w, wpart=wp)

    def tr(self, out, in_, ident, r=(), w=(), wp=()):
        return self.P.op("pe", lambda e: e.transpose(out, in_, ident), reads=r, writes=w, wpart=wp)

    def red(self, out, in_, r=(), w=(), wp=()):
        return self.P.op("dve", lambda e: e.tensor_reduce(out=out, in_=in_, axis=AX.X, op=ALU.add), reads=r, writes=w, wpart=wp)

    def recip(self, out, in_, r=(), w=(), wp=()):
        return self.P.op("dve", lambda e: e.reciprocal(out=out, in_=in_), reads=r, writes=w, wpart=wp)

    def dma(self, q, out, in_, r=(), w=(), wp=(), key=None):
        return self.P.dma(q, out, in_, reads=r, writes=w, wpart=wp, key=key)


class Cfg:
    def __init__(self, S=8192, L=256, E=32, debug=False, stop_after=None):
        self.S, self.L, self.E = S, L, E
        self.D = 1024
        self.NT = S // 128
        self.ROWS = S // 64
        self.NCH = S // 64
        self.MB = min(1024, S)
        self.NTILE = (4 * S) // 512 + E
        self.NSLOT = self.NTILE * 512
        self.debug = debug
        self.dense = False
        self.stop_after = stop_after


EPS = 1e-6
NEG = -30000.0


def host_consts(cfg):
    S = cfg.S
    NT = cfg.NT
    cs = {}
    cs["ident_f"] = np.eye(128, dtype=np.float32)
    p = np.arange(128)
    t = np.arange(64)
    s = p % 64
    cs["trif"] = (s[:, None] <= t[None, :]).astype(np.float32)
    cs["trib"] = (s[:, None] >= t[None, :]).astype(np.float32)
    r = np.ones((128, 512), np.float32)
    r[:, ::64] = 0.0
    cs["reset"] = r
    inv = (10000.0 ** (-np.arange(16, dtype=np.float32) / 16.0)).astype(np.float32)
    tt = np.arange(NT)
    row = (2 * tt[None, :] + (p[:, None] // 64)).astype(np.float32)
    col = np.broadcast_to((p % 64).astype(np.float32)[:, None], (128, NT))
    ang = np.stack([row[:, :, None] * inv[None, None, :], col[:, :, None] * inv[None, None, :]], axis=2)
    ang = ang.astype(np.float32)
    E, NTILE = cfg.E, cfg.NTILE
    cs["ut"] = (p[:, None] < p[None, :]).astype(np.float32)
    cs["iota"] = p.astype(np.float32).reshape(128, 1)
    kp = np.zeros((128, 9), np.float32)
    kp[:, :8] = np.arange(8)[None, :] * 128 + p[:, None]
    kp[:, 8] = p % 16
    cs["kp"] = kp
    cs["th"] = np.broadcast_to((512.0 * np.arange(16, dtype=np.float32))[None, None, :], (1, E, 16)).reshape(1, E * 16).copy()
    cs["j512"] = np.broadcast_to((512.0 * np.arange(NTILE, dtype=np.float32))[None, :, None], (1, NTILE, E)).reshape(1, NTILE * E).copy()
    si = np.zeros((cfg.NSLOT, 2), np.float32)
    si[:, 0] = S + (np.arange(cfg.NSLOT) % 128)
    cs["slotinit"] = si
    cs["cos"] = np.cos(ang).astype(np.float32).reshape(128, NT * 32)
    cs["sin"] = np.sin(ang).astype(np.float32).reshape(128, NT * 32)
    return cs


def layout_rpb(rpb):
    H = rpb.shape[0]
    c = np.arange(64)[:, None]
    kc = np.arange(64)[None, :]
    win = np.clip(c - 8, 0, 48)
    valid = (kc >= win) & (kc < win + 16)
    idx = np.clip(kc - c + 15, 0, 30)
    g = rpb[:, :, idx]
    g = np.where(valid[None, None], g, np.float32(NEG)).astype(np.float32)
    return np.ascontiguousarray(g.transpose(2, 0, 1, 3)).reshape(64, H * 15 * 64)


_uid = [0]


def sb(es, nc, shape, dt, n=1, name="t"):
    out = []
    for i in range(n):
        _uid[0] += 1
        t = es.enter_context(nc.sbuf_tensor("%s_%d" % (name, _uid[0]), list(shape), dt))
        out.append(Slot(t, Buf(name)))
    return out if n > 1 else out[0]


def ps(es, nc, shape, dt, n=1, name="p"):
    out = []
    for i in range(n):
        _uid[0] += 1
        t = es.enter_context(nc.psum_tensor("%s_%d" % (name, _uid[0]), list(shape), dt))
        out.append(Slot(t, Buf(name)))
    return out if n > 1 else out[0]


def declare_io(nc, cfg):
    S, L, E = cfg.S, cfg.L, cfg.E
    d = {}

    inputs = set()
    d["_inputs"] = inputs

    def inp(name, shape, dt=F32):
        d[name] = nc.dram_tensor(name, list(shape), dt, kind="ExternalInput").ap()
        inputs.add(name)

    inp("x", [S, 1024]); inp("c", [8, 128]); inp("ctx", [L, 1024]); inp("c_ctx", [8, 128])
    inp("ada_w", [2, 1024, 6144]); inp("ada_b", [2, 6144]); inp("norm1_g", [2, 1024]); inp("norm2_g", [2, 1024])
    inp("ab_w_in", [1024, 4096]); inp("ab_w_out", [1024, 1024]); inp("nat_q_norm", [1, 64]); inp("nat_k_norm", [1, 64])
    inp("rpb_full", [64, 8 * 15 * 64]); inp("hgrn_lb", [16, 128]); inp("hgrn_o_norm", [1, 128])
    inp("conv_w1", [1024, 2048]); inp("conv_b1", [16, 128]); inp("conv_dw", [31, 1024]); inp("conv_dw_b", [8, 128])
    inp("conv_ln_g", [8, 128]); inp("conv_ln_b", [8, 128]); inp("conv_w2", [1024, 1024]); inp("conv_b2", [1, 1024])
    inp("router_w", [2, 1024, E]); inp("router_b", [2, E])
    inp("moe_w1", [2, E, 1024, 2048]); inp("moe_b1", [2, E * 16, 128]); inp("moe_w2", [2, E, 1024, 1024]); inp("moe_b2", [2, E, 1024])
    inp("k_ident_f", [128, 128]); inp("k_trif", [128, 64]); inp("k_trib", [128, 64]); inp("k_reset", [128, 512])
    inp("k_cos", [128, cfg.NT * 32]); inp("k_sin", [128, cfg.NT * 32])
    inp("k_ut", [128, 128]); inp("k_iota", [128, 1]); inp("k_th", [1, E * 16]); inp("k_j512", [1, cfg.NTILE * E])
    inp("k_slotinit", [cfg.NSLOT, 2]); inp("k_kp", [128, 9])
    d["out"] = nc.dram_tensor("out", [S, 1024], F32, kind="ExternalOutput").ap()
    kind = "ExternalOutput" if cfg.debug else "Internal"

    def scr(name, shape, dt):
        d[name] = nc.dram_tensor(name, list(shape), dt, kind=kind).ap()

    scr("XT", [128, 8, S], BF16); scr("XTc", [128, 8, L], BF16)
    scr("QTr", [128, 4, S], BF16); scr("QTf", [128, 4, S], BF16); scr("KTr", [128, 4, S], BF16)
    scr("KcT", [128, 4, L], BF16)
    scr("VA", [S, 520], BF16); scr("VcA", [L, 520], BF16)
    scr("VH", [S, 512], BF16); scr("VHc", [L, 512], BF16)
    scr("G", [S, 512], F32)
    scr("HQ", [2, 128, 4, S], BF16); scr("HK", [2, 128, 4, S], BF16)
    scr("KH", [2, S, 512], BF16); scr("KHc", [2, L, 512], BF16)
    scr("DEC", [2, 128, 4, S // 64], F32); scr("DECc", [2, 128, 4, L // 64], F32)
    scr("OF", [S, 512], F32)
    scr("CAT", [S, 1024], BF16)
    scr("H1", [S, 1024], F32); scr("H2", [S, 1024], F32); scr("H3", [S, 1024], F32)
    scr("XT2", [128, 8, S], BF16)
    scr("COMB", [S, E], F32)
    scr("ACC0", [S, 1024], F32)
    scr("GT", [128, 8, S + 32], BF16)
    scr("YD", [128, 8, S], F32)
    scr("XM2", [S + 128, 1024], BF16)
    scr("MK", [S, E], F32)
    scr("ACCd", [S + 128, 1024], F32)
    scr("SLOT", [cfg.NSLOT, 2], F32)
    return d


def build_program(cfg):
    nc = bass.Bass("TRN2", target_bir_lowering=False)
    c = Ctx()
    c.nc, c.cfg = nc, cfg
    c.d = declare_io(nc, cfg)
    c.inputs = c.d.pop("_inputs")
    with ExitStack() as ges:
        P = Prog(nc, ges)
        c.P = P
        c.k = K(P)
        c.ident_f = sb(ges, nc, [128, 128], F32, name="identf")
        c.ident_b = sb(ges, nc, [128, 128], BF16, name="identb")
        c.ones_f = sb(ges, nc, [128, 128], F32, name="onesf")
        c.k.dma("sp", c.ident_f.t[:], c.d["k_ident_f"], w=[c.ident_f.b])
        c.k.cp("dve", c.ident_b.t[:], c.ident_f.t[:], r=[c.ident_f.b], w=[c.ident_b.b])
        c.k.memset("dve", c.ones_f.t[:], 1.0, w=[c.ones_f.b])
        touch = sb(ges, nc, [1, 64], F32, name="touch")
        for i, nm in enumerate(sorted(c.inputs)):
            ap = c.d[nm]
            idx = tuple([0] * (len(ap.shape) - 2) + [slice(0, 1), slice(0, 1)])
            c.k.dma("sp", touch.t[0:1, i:i + 1], ap[idx], wp=[touch.b])
        if cfg.debug:
            tb = sb(ges, nc, [1, 2], BF16, name="touchb")
            c.k.memset("dve", touch.t[0:1, 62:64], 0.0, wp=[touch.b])
            c.k.memset("dve", tb.t[:], 0.0, w=[tb.b])
            for i, (nm, ap) in enumerate(c.d.items()):
                if nm not in c.inputs:
                    idx = tuple([0] * (len(ap.shape) - 2) + [slice(0, 1), slice(0, 1)])
                    src = tb.t[0:1, 0:1] if ap.dtype == BF16 else touch.t[0:1, 63:64]
                    c.k.dma("sp", ap[idx], src, r=[tb.b, touch.b], w=[Buf("o")])
        P.end_phase()
        def dbg_stop(name):
            return cfg.stop_after == name

        done = False
        for layer in (0, 1):
            with ExitStack() as lay:
                c.MOD = sb(lay, nc, [128, 6144], F32, name="MOD")
                if layer == 0:
                    c.CMOD = sb(lay, nc, [128, 2048], F32, name="CMOD")
                    seq = [("mods0", lambda: phase_mods(c, 0)), ("a1", lambda: phase_a1(c)), ("a2", lambda: phase_a2(c)),
                           ("nat", lambda: phase_nat(c)), ("hgrn", lambda: phase_hgrn(c)), ("post0", lambda: phase_post(c, 0))]
                else:
                    seq = [("mods1", lambda: phase_mods(c, 1)), ("f", lambda: phase_f(c)), ("g1", lambda: phase_g1(c)),
                           ("post1", lambda: phase_post(c, 1))]
                for name, fn in seq:
                    fn()
                    if dbg_stop(name):
                        done = True
                        break
            if done:
                break
            with ExitStack() as m5:
                MOD5 = sb(m5, nc, [128, 1024], F32, name="MOD5")
                with ExitStack() as ph:
                    emit_mods(c, ph, layer, [10, 11], lambda ct: MOD5.t[:, (ct - 10) * 512:(ct - 9) * 512], MOD5.b, False)
                    P.end_phase()
                if cfg.dense:
                    phase_moe(c, layer, MOD5)
                else:
                    TEi = (sb(m5, nc, [128, cfg.NTILE, 8], mybir.dt.int32, name="IDXW"), sb(m5, nc, [128, cfg.NTILE], mybir.dt.int32, name="IDXB"))
                    phase_route(c, layer, TEi)
                    if dbg_stop("route%d" % layer):
                        break
                    phase_smoe(c, layer, MOD5, TEi)
            if dbg_stop("moe%d" % layer):
                break
    return nc, c


def emit_mods(c, ph, layer, cts, dst_fn, dst_buf, with_ctx):
    nc, P, k, d = c.nc, c.P, c.k, c.d
    crow = sb(ph, nc, [16, 128], F32, name="crow")
    crow2 = sb(ph, nc, [16, 128], F32, name="crow2")
    ccol = sb(ph, nc, [128, 16], F32, name="ccol")
    CB = sb(ph, nc, [128, 16, 128], F32, name="CB")
    AW = sb(ph, nc, [128, 8, 512], F32, n=2, name="AW")
    ABr = sb(ph, nc, [1, 512], F32, n=2, name="ABr")
    pT = ps(ph, nc, [128, 16], F32, name="pT")
    pM = ps(ph, nc, [128, 512], F32, n=2, name="pM")
    pC = ps(ph, nc, [128, 512], F32, n=2, name="pC")
    k.dma("sp", crow.t[0:8, :], d["c"], wp=[crow.b])
    k.dma("sp", crow.t[8:16, :], d["c_ctx"], wp=[crow.b])
    k.act(crow2.t[:], crow.t[:], AF.Silu, r=[crow.b], w=[crow2.b])
    k.tr(pT.t[:], crow2.t[:], c.ident_f.t[0:16, 0:16], r=[crow2.b], w=[pT.b])
    k.cp("dve", ccol.t[:], pT.t[:], r=[pT.b], w=[ccol.b])
    for j in range(16):
        k.cp("dve" if j % 2 else "pool", CB.t[:, j, :], ccol.t[:, j:j + 1].to_broadcast([128, 128]), r=[ccol.b], wp=[CB.b])
    awv = d["ada_w"][layer].rearrange("(k p) n -> p k n", p=128)
    for i, ct in enumerate(cts):
        aw, ab = AW[i % 2], ABr[i % 2]
        k.dma("sp", aw.t[:], awv[:, :, ct * 512:(ct + 1) * 512], w=[aw.b])
        k.dma("sp", ab.t[:], d["ada_b"][layer:layer + 1, ct * 512:(ct + 1) * 512], w=[ab.b])
        pm = pM[i % 2]
        for kk in range(8):
            k.mm(pm.t[:], CB.t[:, kk, :], aw.t[:, kk, :], kk == 0, False, r=[CB.b, aw.b], w=[pm.b] if kk == 0 else (), wp=() if kk == 0 else [pm.b])
        k.mm(pm.t[:], c.ones_f.t[0:1, :], ab.t[:], False, True, r=[ab.b], wp=[pm.b])
        k.cp("act", dst_fn(ct), pm.t[:], r=[pm.b], wp=[dst_buf])
        if with_ctx and ct < 4:
            pc = pC[i % 2]
            for kk in range(8):
                k.mm(pc.t[:], CB.t[:, 8 + kk, :], aw.t[:, kk, :], kk == 0, False, r=[CB.b, aw.b], w=[pc.b] if kk == 0 else (), wp=() if kk == 0 else [pc.b])
            k.mm(pc.t[:], c.ones_f.t[0:1, :], ab.t[:], False, True, r=[ab.b], wp=[pc.b])
            k.cp("dve", c.CMOD.t[:, ct * 512:(ct + 1) * 512], pc.t[:], r=[pc.b], wp=[c.CMOD.b])


def phase_mods(c, layer):
    with ExitStack() as ph:
        emit_mods(c, ph, layer, list(range(10)), lambda ct: c.MOD.t[:, ct * 512:(ct + 1) * 512], c.MOD.b, layer == 0)
        c.P.end_phase()


def make_A(c, ph, gname, layer, modcols, modt):
    nc, k, d = c.nc, c.k, c.d
    g = sb(ph, nc, [128, 1024], F32, name="gbc")
    A = sb(ph, nc, [128, 1024], F32, name="A")
    k.dma("sp", g.t[:], d[gname][layer:layer + 1, :].partition_broadcast(128), w=[g.b])
    k.stt(A.t[:], modt.t[:, modcols:modcols + 1024], 1.0, g.t[:], ALU.add, ALU.mult, r=[modt.b, g.b], w=[A.b])
    return A


def norm_mod(c, xt, A, SH, shb, outs, tmp, j):
    k = c.k
    ss, t1 = tmp["ss"], tmp["t1"]
    k.stt(t1.t[:], xt.t[:], 1.0, xt.t[:], ALU.mult, ALU.mult, r=[xt.b], w=[t1.b], wp=[ss.b], accum=ss.t[:, 4 * j:4 * j + 1])
    k.ts("dve", ss.t[:, 4 * j + 1:4 * j + 2], ss.t[:, 4 * j:4 * j + 1], 1.0 / 1024, ALU.mult, EPS, ALU.add, r=[ss.b], wp=[ss.b])
    k.act(ss.t[:, 4 * j + 2:4 * j + 3], ss.t[:, 4 * j + 1:4 * j + 2], AF.Sqrt, r=[ss.b], wp=[ss.b])
    k.recip(ss.t[:, 4 * j + 3:4 * j + 4], ss.t[:, 4 * j + 2:4 * j + 3], r=[ss.b], wp=[ss.b])
    k.stt(t1.t[:], xt.t[:], ss.t[:, 4 * j + 3:4 * j + 4], A.t[:], ALU.mult, ALU.mult, r=[xt.b, ss.b, A.b], w=[t1.b])
    for (ap, eng, slot) in outs:
        k.tt(eng, ap, t1.t[:], SH, ALU.add, r=[t1.b, shb], wp=[slot.b])


def phase_a1(c):
    nc, P, k, d, cfg = c.nc, c.P, c.k, c.d, c.cfg
    S, L, NT = cfg.S, cfg.L, cfg.NT
    with ExitStack() as ph:
        W = sb(ph, nc, [128, 8, 2560], BF16, name="Wtok")
        wv = d["ab_w_in"].rearrange("(k p) n -> p k n", p=128)
        for i, c0 in enumerate((0, 512, 1024, 3072, 3584)):
            k.dma("pool", W.t[:, :, i * 512:(i + 1) * 512], wv[:, :, c0:c0 + 512], wp=[W.b])
        A = make_A(c, ph, "norm1_g", 0, 1024, c.MOD)
        Ac = make_A(c, ph, "norm1_g", 0, 1024, c.CMOD)
        g64 = sb(ph, nc, [128, 128], F32, name="g64")
        GQ = sb(ph, nc, [128, 512], F32, name="GQ")
        GK = sb(ph, nc, [128, 512], F32, name="GK")
        k.dma("sp", g64.t[:, 0:64], d["nat_q_norm"].partition_broadcast(128), wp=[g64.b])
        k.dma("sp", g64.t[:, 64:128], d["nat_k_norm"].partition_broadcast(128), wp=[g64.b])
        k.ts("dve", GQ.t[:].rearrange("p (h e) -> p h e", h=8), g64.t[:, 0:64].unsqueeze(1).to_broadcast([128, 8, 64]), 0.125, ALU.mult, r=[g64.b], w=[GQ.b])
        k.ts("dve", GK.t[:].rearrange("p (h e) -> p h e", h=8), g64.t[:, 64:128].unsqueeze(1).to_broadcast([128, 8, 64]), 1.0, ALU.mult, r=[g64.b], w=[GK.b])
        COSG = sb(ph, nc, [128, 128], F32, n=2, name="COS")
        SING = sb(ph, nc, [128, 128], F32, n=2, name="SIN")
        XIN = sb(ph, nc, [128, 1024], F32, n=2, name="xin")
        tmps = [{"ss": sb(ph, nc, [128, 16], F32, name="ss"), "t1": sb(ph, nc, [128, 1024], F32, name="t1")} for _ in range(2)]
        XM = sb(ph, nc, [128, 1024], BF16, n=2, name="xm")
        XTG = sb(ph, nc, [128, 8, 512], BF16, n=2, name="xtg")
        pT = ps(ph, nc, [128, 1024], BF16, name="pT")
        pS = ps(ph, nc, [128, 512], F32, n=5, name="pS")
        pO = ps(ph, nc, [128, 1024], BF16, n=2, name="pO")
        SQs = sb(ph, nc, [128, 1024], F32, n=2, name="sq")
        STs = sb(ph, nc, [128, 48], F32, n=2, name="st")
        QNs = sb(ph, nc, [128, 1024], F32, n=2, name="qn")
        R1s = sb(ph, nc, [128, 1024], F32, n=2, name="r1")
        R2s = sb(ph, nc, [128, 1024], F32, n=2, name="r2")
        OB = sb(ph, nc, [128, 1536], BF16, n=2, name="ob")
        OTG = sb(ph, nc, [128, 3, 4, 512], BF16, n=2, name="otg")
        VAs = sb(ph, nc, [128, 8, 65], BF16, n=2, name="vas")
        VHs = sb(ph, nc, [128, 512], BF16, n=2, name="vhs")
        Gs = sb(ph, nc, [128, 512], F32, n=2, name="gs")
        for v in VAs:
            k.memset("pool", v.t[:, :, 64:65], 1.0, wp=[v.b])
        dXT, dXTc = Buf("dXT"), Buf("dXTc")
        dQ, dV, dVH, dG = Buf("dQ"), Buf("dV"), Buf("dVH"), Buf("dG")

        def run(src, ntile, Asl, modt, is_ctx):
            ngrp = (ntile + 3) // 4
            it = 0
            for g in range(ngrp):
                nj = min(4, ntile - 4 * g)
                xtg = XTG[g % 2]
                otg = OTG[g % 2]
                COS, SIN = COSG[g % 2], SING[g % 2]
                if not is_ctx:
                    k.dma("sp", COS.t[:, 0:nj * 32], d["k_cos"][:, g * 128:g * 128 + nj * 32], w=[COS.b])
                    k.dma("sp", SIN.t[:, 0:nj * 32], d["k_sin"][:, g * 128:g * 128 + nj * 32], w=[SIN.b])
                states = {}

                def head(j):
                    nonlocal it
                    t = 4 * g + j
                    xin, xm = XIN[it % 2], XM[it % 2]
                    ob, vas, vhs, gs = OB[it % 2], VAs[it % 2], VHs[it % 2], Gs[it % 2]
                    SQ, ST, QN, R1, R2 = SQs[it % 2], STs[it % 2], QNs[it % 2], R1s[it % 2], R2s[it % 2]
                    it += 1
                    k.dma("sp", xin.t[:], src[t * 128:(t + 1) * 128, :], w=[xin.b])
                    norm_mod(c, xin, Asl, modt.t[:, 0:1024], modt.b, [(xm.t[:], "pool", xm)], tmps[it % 2], j)
                    for kk in range(8):
                        k.tr(pT.t[:, kk * 128:(kk + 1) * 128], xm.t[:, kk * 128:(kk + 1) * 128], c.ident_b.t[:], r=[xm.b],
                             w=[pT.b] if kk == 0 else (), wp=() if kk == 0 else [pT.b])
                    k.cp("act", xtg.t[:, :, j * 128:(j + 1) * 128], pT.t[:].rearrange("p (k t) -> p k t", k=8), r=[pT.b], wp=[xtg.b])
                    cols = (1, 2, 3) if is_ctx else (0, 1, 2, 3, 4)
                    for ci in cols:
                        for kk in range(8):
                            k.mm(pS[ci].t[:], xtg.t[:, kk, j * 128:(j + 1) * 128], W.t[:, kk, ci * 512:(ci + 1) * 512], kk == 0, kk == 7,
                                 r=[xtg.b, W.b], w=[pS[ci].b] if kk == 0 else (), wp=() if kk == 0 else [pS[ci].b])
                    k.cp("act", vas.t[:, :, 0:64], pS[2].t[:].rearrange("p (h e) -> p h e", h=8), r=[pS[2].b], wp=[vas.b])
                    k.cp("dve", vhs.t[:], pS[3].t[:], r=[pS[3].b], w=[vhs.b])
                    rows = slice(t * 128, (t + 1) * 128)
                    if is_ctx:
                        k.dma("sp", d["VcA"][rows, :], vas.t[:].rearrange("p h e -> p (h e)"), r=[vas.b], wp=[dV])
                        k.dma("sp", d["VHc"][rows, :], vhs.t[:], r=[vhs.b], wp=[dVH])
                    else:
                        k.act(gs.t[:], pS[4].t[:], AF.Silu, r=[pS[4].b], w=[gs.b])
                        k.dma("sp", d["VA"][rows, :], vas.t[:].rearrange("p h e -> p (h e)"), r=[vas.b], wp=[dV])
                        k.dma("sp", d["VH"][rows, :], vhs.t[:], r=[vhs.b], wp=[dVH])
                        k.dma("sp", d["G"][rows, :], gs.t[:], r=[gs.b], wp=[dG])
                    srcs = ((1, 1),) if is_ctx else ((0, 0), (1, 1))
                    for (ci, slot_i) in srcs:
                        k.act(SQ.t[:, slot_i * 512:(slot_i + 1) * 512], pS[ci].t[:], AF.Square, r=[pS[ci].b], wp=[SQ.b])
                        k.red(ST.t[:, slot_i * 8:(slot_i + 1) * 8], SQ.t[:, slot_i * 512:(slot_i + 1) * 512].rearrange("p (h e) -> p h e", h=8), r=[SQ.b], wp=[ST.b])
                    k.ts("dve", ST.t[:, 16:32], ST.t[:, 0:16], 1.0 / 64, ALU.mult, EPS, ALU.add, r=[ST.b], wp=[ST.b])
                    k.act(ST.t[:, 32:48], ST.t[:, 16:32], AF.Sqrt, r=[ST.b], wp=[ST.b])
                    k.recip(ST.t[:, 16:32], ST.t[:, 32:48], r=[ST.b], wp=[ST.b])
                    for (ci, slot_i) in srcs:
                        qn = QN.t[:, slot_i * 512:(slot_i + 1) * 512]
                        k.tt("dve", qn.rearrange("p (h e) -> p h e", h=8), pS[ci].t[:].rearrange("p (h e) -> p h e", h=8),
                             ST.t[:, 16 + slot_i * 8:16 + slot_i * 8 + 8].unsqueeze(2).to_broadcast([128, 8, 64]), ALU.mult,
                             r=[pS[ci].b, ST.b], wp=[QN.b])
                        Gt = GQ if slot_i == 0 else GK
                        k.tt("pool", qn, qn, Gt.t[:], ALU.mult, r=[QN.b, Gt.b], wp=[QN.b])
                    if is_ctx:
                        k.cp("act", ob.t[:, 1024:1536], QN.t[:, 512:1024], r=[QN.b], wp=[ob.b])
                    else:
                        k.cp("act", ob.t[:, 512:1024], QN.t[:, 0:512], r=[QN.b], wp=[ob.b])
                        qv = QN.t[:].rearrange("p (h a b i) -> p h a b i", h=16, a=2, b=2)
                        cosb = COS.t[:, j * 32:(j + 1) * 32].rearrange("p (a i) -> p a i", a=2).unsqueeze(1).to_broadcast([128, 16, 2, 16])
                        sinb = SIN.t[:, j * 32:(j + 1) * 32].rearrange("p (a i) -> p a i", a=2).unsqueeze(1).to_broadcast([128, 16, 2, 16])
                        r1v = R1.t[:].rearrange("p (h a b i) -> p h a b i", h=16, a=2, b=2)
                        r2v = R2.t[:].rearrange("p (h a b i) -> p h a b i", h=16, a=2, b=2)
                        x1, x2 = qv[:, :, :, 0, :], qv[:, :, :, 1, :]
                        k.tt("dve", r1v[:, :, :, 0, :], x1, cosb, ALU.mult, r=[QN.b, COS.b], wp=[R1.b])
                        k.tt("pool", r2v[:, :, :, 0, :], x2, sinb, ALU.mult, r=[QN.b, SIN.b], wp=[R2.b])
                        k.tt("dve", r1v[:, :, :, 1, :], x2, cosb, ALU.mult, r=[QN.b, COS.b], wp=[R1.b])
                        k.tt("pool", r2v[:, :, :, 1, :], x1, sinb, ALU.mult, r=[QN.b, SIN.b], wp=[R2.b])
                        for slot_i, o0 in ((0, 0), (1, 1024)):
                            ov = ob.t[:, o0:o0 + 512].rearrange("p (h a b i) -> p h a b i", h=8, a=2, b=2)
                            a1 = r1v[:, slot_i * 8:(slot_i + 1) * 8]
                            a2 = r2v[:, slot_i * 8:(slot_i + 1) * 8]
                            k.tt("dve", ov[:, :, :, 0, :], a1[:, :, :, 0, :], a2[:, :, :, 0, :], ALU.subtract, r=[R1.b, R2.b], wp=[ob.b])
                            k.tt("dve", ov[:, :, :, 1, :], a1[:, :, :, 1, :], a2[:, :, :, 1, :], ALU.add, r=[R1.b, R2.b], wp=[ob.b])
                    states[j] = (ob,)

                def tail(j):
                    (ob,) = states[j]
                    which = (2,) if is_ctx else (0, 1, 2)
                    for wi in which:
                        po = pO[0] if wi < 2 else pO[1]
                        for hp in range(4):
                            col = ((wi % 2) * 4 + hp) * 128
                            first = (hp == 0 and wi in (0, 2))
                            k.tr(po.t[:, col:col + 128], ob.t[:, wi * 512 + hp * 128: wi * 512 + (hp + 1) * 128], c.ident_b.t[:], r=[ob.b],
                                 w=[po.b] if first else (), wp=() if first else [po.b])
                    if not is_ctx:
                        k.cp("act", otg.t[:, 0:2, :, j * 128:(j + 1) * 128], pO[0].t[:].rearrange("p (w h t) -> p w h t", w=2, h=4), r=[pO[0].b], wp=[otg.b])
                    k.cp("dve", otg.t[:, 2, :, j * 128:(j + 1) * 128], pO[1].t[:, 0:512].rearrange("p (h t) -> p h t", h=4), r=[pO[1].b], wp=[otg.b])

                head(0)
                for j in range(nj):
                    if j + 1 < nj:
                        head(j + 1)
                    tail(j)
                tok = slice(g * 512, g * 512 + nj * 128)
                w_ = nj * 128
                if is_ctx:
                    k.dma("sp", d["XTc"][:, :, tok], xtg.t[:, :, 0:w_], r=[xtg.b], wp=[dXTc])
                    k.dma("sp", d["KcT"][:, :, tok], otg.t[:, 2, :, 0:w_], r=[otg.b], wp=[dQ])
                else:
                    k.dma("sp", d["XT"][:, :, tok], xtg.t[:, :, 0:w_], r=[xtg.b], wp=[dXT])
                    k.dma("sp", d["QTr"][:, :, tok], otg.t[:, 0, :, 0:w_], r=[otg.b], wp=[dQ])
                    k.dma("sp", d["QTf"][:, :, tok], otg.t[:, 1, :, 0:w_], r=[otg.b], wp=[dQ])
                    k.dma("sp", d["KTr"][:, :, tok], otg.t[:, 2, :, 0:w_], r=[otg.b], wp=[dQ])

        run(d["ctx"], L // 128, Ac, c.CMOD, True)
        run(d["x"], NT, A, c.MOD, False)
        P.end_phase()


def core_inputs(inp, b, cfg, consts):
    f = lambda a: np.ascontiguousarray(np.asarray(a, dtype=np.float32))
    m = {
        "x": f(inp["x"][b]), "c": f(inp["c"][b]).reshape(8, 128), "ctx": f(inp["ctx"][b]),
        "c_ctx": f(inp["c_ctx"]).reshape(8, 128),
        "ada_w": f(inp["ada_w"]), "ada_b": f(inp["ada_b"]), "norm1_g": f(inp["norm1_g"]), "norm2_g": f(inp["norm2_g"]),
        "ab_w_in": f(inp["ab_w_in"][0]), "ab_w_out": f(inp["ab_w_out"][0]),
        "nat_q_norm": f(inp["nat_q_norm"][0]).reshape(1, 64), "nat_k_norm": f(inp["nat_k_norm"][0]).reshape(1, 64),
        "rpb_full": layout_rpb(f(inp["nat_rpb"][0])),
        "hgrn_lb": f(inp["hgrn_lb"]).reshape(16, 128), "hgrn_o_norm": f(inp["hgrn_o_norm"][0]).reshape(1, 128),
        "conv_w1": f(inp["conv_w1"][0]), "conv_b1": f(inp["conv_b1"][0]).reshape(16, 128), "conv_dw": f(inp["conv_dw"][0]),
        "conv_dw_b": f(inp["conv_dw_b"][0]).reshape(8, 128), "conv_ln_g": f(inp["conv_ln_g"][0]).reshape(8, 128),
        "conv_ln_b": f(inp["conv_ln_b"][0]).reshape(8, 128), "conv_w2": f(inp["conv_w2"][0]), "conv_b2": f(inp["conv_b2"][0]).reshape(1, 1024),
        "router_w": f(inp["router_w"]), "router_b": f(inp["router_b"]),
        "moe_w1": f(inp["moe_w1"]), "moe_b1": f(inp["moe_b1"]).reshape(2, cfg.E * 16, 128),
        "moe_w2": f(inp["moe_w2"]), "moe_b2": f(inp["moe_b2"]),
    }
    for kname, v in consts.items():
        m["k_" + kname] = v
    return m


_cache = {}


def kernel(**inputs):
    B = inputs["x"].shape[0]
    S = inputs["x"].shape[1]
    cfg = Cfg(S=S, L=inputs["ctx"].shape[1], E=inputs["moe_w1"].shape[1])
    key = (cfg.S, cfg.L, cfg.E)
    if key not in _cache:
        _cache[key] = build_program(cfg)[0]
    nc = _cache[key]
    consts = host_consts(cfg)
    in_maps = [core_inputs(inputs, b, cfg, consts) for b in range(B)]
    res = run_bass_kernel_spmd(nc, in_maps, core_ids=list(range(B)))
    return np.stack([np.asarray(r["out"], dtype=np.float32) for r in res.results], axis=0)


def phase_a2(c):
    nc, P, k, d, cfg = c.nc, c.P, c.k, c.d, c.cfg
    S, L = cfg.S, cfg.L
    with ExitStack() as ph:
        W = sb(ph, nc, [128, 8, 1536], BF16, name="Wfm")
        wv = d["ab_w_in"].rearrange("(k p) n -> p k n", p=128)
        for i in range(3):
            k.dma("pool", W.t[:, :, i * 512:(i + 1) * 512], wv[:, :, 1536 + i * 512:1536 + (i + 1) * 512], wp=[W.b])
        RESET = sb(ph, nc, [128, 512], F32, name="reset")
        k.dma("sp", RESET.t[:], d["k_reset"], w=[RESET.b])
        lbr = sb(ph, nc, [16, 128], F32, name="lbr")
        Ee = sb(ph, nc, [128, 16], F32, name="Ee")
        LB = sb(ph, nc, [128, 24], F32, name="LB")
        pL = ps(ph, nc, [128, 16], F32, name="pL")
        k.dma("sp", lbr.t[:], d["hgrn_lb"], w=[lbr.b])
        k.tr(pL.t[:], lbr.t[:], c.ident_f.t[0:16, 0:16], r=[lbr.b], w=[pL.b])
        k.act(Ee.t[:], pL.t[:], AF.Exp, r=[pL.b], w=[Ee.b])
        ev = Ee.t[:].rearrange("p (d j h) -> p d j h", d=2, j=2)
        k.tt("dve", LB.t[:, 16:24].rearrange("p (d h) -> p d h", d=2), ev[:, :, 0, :], ev[:, :, 1, :], ALU.add, r=[Ee.b], wp=[LB.b])
        k.recip(LB.t[:, 16:24], LB.t[:, 16:24], r=[LB.b], wp=[LB.b])
        k.tt("dve", LB.t[:, 0:8].rearrange("p (d h) -> p d h", d=2), ev[:, :, 0, :], LB.t[:, 16:24].rearrange("p (d h) -> p d h", d=2), ALU.mult, r=[Ee.b, LB.b], wp=[LB.b])
        k.ts("dve", LB.t[:, 8:16], LB.t[:, 0:8], -1.0, ALU.mult, 1.0, ALU.add, r=[LB.b], wp=[LB.b])

        XTG = sb(ph, nc, [128, 8, 512], BF16, n=2, name="xtg")
        names = ("q32", "sg", "f", "lf", "kk", "B", "e1", "e2", "t1", "r", "e3")
        TM = {n_: sb(ph, nc, [128, 512], F32, n=2, name=n_) for n_ in names}
        HQs = sb(ph, nc, [128, 2, 4, 512], BF16, n=2, name="hqs")
        HKs = sb(ph, nc, [128, 2, 4, 512], BF16, n=2, name="hks")
        KHT = sb(ph, nc, [128, 512], BF16, n=2, name="kht")
        KHs = sb(ph, nc, [128, 4, 2, 512], BF16, n=2, name="khs")
        DECs = sb(ph, nc, [128, 2, 4, 8], F32, n=2, name="decs")
        pQ = ps(ph, nc, [128, 512], F32, n=2, name="pQ")
        pF = ps(ph, nc, [128, 512], F32, n=3, name="pF")
        pK = ps(ph, nc, [128, 512], BF16, n=2, name="pK")
        dHQ, dHK, dKH, dDEC = Buf("dHQ"), Buf("dHK"), Buf("dKH"), Buf("dDEC")
        QS = 128.0 ** -0.5

        def run(src, ntok, is_ctx):
            ngrp = (ntok + 511) // 512
            it = 0
            for g in range(ngrp):
                n = min(512, ntok - g * 512)
                nch = n // 64
                nsub = n // 128
                xtg, hqs, hks, khs, decs = XTG[g % 2], HQs[g % 2], HKs[g % 2], KHs[g % 2], DECs[g % 2]
                k.dma("sp", xtg.t[:, :, 0:n], src[:, :, g * 512:g * 512 + n], w=[xtg.b])
                for h in range(4):
                    pq = pQ[h % 2]
                    if not is_ctx:
                        for kk_ in range(8):
                            k.mm(pq.t[:, 0:n], W.t[:, kk_, h * 128:(h + 1) * 128], xtg.t[:, kk_, 0:n], kk_ == 0, kk_ == 7,
                                 r=[W.b, xtg.b], w=[pq.b] if kk_ == 0 else (), wp=() if kk_ == 0 else [pq.b])
                        q32 = TM["q32"][h % 2]
                        k.act(q32.t[:, 0:n], pq.t[:, 0:n], AF.Silu, r=[pq.b], w=[q32.b])
                    for dd in range(2):
                        pf = pF[(2 * h + dd) % 3]
                        c0 = 512 + dd * 512 + h * 128
                        for kk_ in range(8):
                            k.mm(pf.t[:, 0:n], W.t[:, kk_, c0:c0 + 128], xtg.t[:, kk_, 0:n], kk_ == 0, kk_ == 7,
                                 r=[W.b, xtg.b], w=[pf.b] if kk_ == 0 else (), wp=() if kk_ == 0 else [pf.b])
                        tm = {n_: TM[n_][it % 2] for n_ in names}
                        kht = KHT[it % 2]
                        pk = pK[it % 2]
                        it += 1
                        sg, f, lf, kk, Bc, e1, e2, t1, rr, e3 = (tm[x] for x in ("sg", "f", "lf", "kk", "B", "e1", "e2", "t1", "r", "e3"))
                        li = dd * 4 + h
                        k.act(sg.t[:, 0:n], pf.t[:, 0:n], AF.Sigmoid, r=[pf.b], w=[sg.b])
                        k.ts("dve", f.t[:, 0:n], sg.t[:, 0:n], LB.t[:, 8 + li:9 + li], ALU.mult, LB.t[:, li:li + 1], ALU.add, r=[sg.b, LB.b], w=[f.b])
                        k.act(lf.t[:, 0:n], f.t[:, 0:n], AF.Ln, r=[f.b], w=[lf.b])
                        k.ts("pool", kk.t[:, 0:n], f.t[:, 0:n], -1.0, ALU.mult, 1.0, ALU.add, r=[f.b], w=[kk.b])
                        P.op("dve", (lambda e, o=Bc.t[:, 0:n], a=RESET.t[:, 0:n], b_=lf.t[:, 0:n]:
                                     e.tensor_tensor_scan(out=o, data0=a, data1=b_, initial=0.0, op0=ALU.mult, op1=ALU.add)),
                             reads=[RESET.b, lf.b], writes=[Bc.b])
                        Bv = Bc.t[:, 0:n].rearrange("p (c t) -> p c t", t=64)
                        Bend = Bv[:, :, 63:64].to_broadcast([128, nch, 64])
                        v3 = lambda s_: s_.t[:, 0:n].rearrange("p (c t) -> p c t", t=64)
                        if dd == 0:
                            k.act(e1.t[:, 0:n], Bc.t[:, 0:n], AF.Exp, r=[Bc.b], w=[e1.b])
                            k.act(e2.t[:, 0:n], Bc.t[:, 0:n], AF.Exp, r=[Bc.b], w=[e2.b], scale=-1.0)
                            k.tt("dve", v3(t1), Bend, Bv, ALU.subtract, r=[Bc.b], w=[t1.b])
                            k.act(e3.t[:, 0:n], t1.t[:, 0:n], AF.Exp, r=[t1.b], w=[e3.b])
                        else:
                            k.tt("dve", t1.t[:, 0:n], lf.t[:, 0:n], Bc.t[:, 0:n], ALU.subtract, r=[lf.b, Bc.b], w=[t1.b])
                            k.tt("dve", v3(rr), v3(t1), Bend, ALU.add, r=[t1.b, Bc.b], w=[rr.b])
                            k.act(e1.t[:, 0:n], rr.t[:, 0:n], AF.Exp, r=[rr.b], w=[e1.b])
                            k.act(e2.t[:, 0:n], rr.t[:, 0:n], AF.Exp, r=[rr.b], w=[e2.b], scale=-1.0)
                            k.act(e3.t[:, 0:n], t1.t[:, 0:n], AF.Exp, r=[t1.b], w=[e3.b], scale=-1.0)
                        k.act(decs.t[:, dd, h, 0:nch], Bv[:, :, 63], AF.Exp, r=[Bc.b], wp=[decs.b])
                        if not is_ctx:
                            q32 = TM["q32"][h % 2]
                            k.stt(hqs.t[:, dd, h, 0:n], q32.t[:, 0:n], QS, e1.t[:, 0:n], ALU.mult, ALU.mult, r=[q32.b, e1.b], wp=[hqs.b])
                            k.tt("pool", hks.t[:, dd, h, 0:n], kk.t[:, 0:n], e2.t[:, 0:n], ALU.mult, r=[kk.b, e2.b], wp=[hks.b])
                        k.tt("pool", kht.t[:, 0:n], kk.t[:, 0:n], e3.t[:, 0:n], ALU.mult, r=[kk.b, e3.b], w=[kht.b])
                        for sub in range(nsub):
                            k.tr(pk.t[:, sub * 128:(sub + 1) * 128], kht.t[:, sub * 128:(sub + 1) * 128], c.ident_b.t[:], r=[kht.b],
                                 w=[pk.b] if sub == 0 else (), wp=() if sub == 0 else [pk.b])
                        k.cp("act", khs.t[:, 0:nsub, dd, h * 128:(h + 1) * 128], pk.t[:, 0:n].rearrange("p (s e) -> p s e", e=128), r=[pk.b], wp=[khs.b])
                tok = slice(g * 512, g * 512 + n)
                for dd in range(2):
                    if is_ctx:
                        k.dma("sp", d["KHc"][dd, tok, :].rearrange("(s p) e -> p s e", p=128), khs.t[:, 0:nsub, dd, :], r=[khs.b], wp=[dKH])
                        k.dma("sp", d["DECc"][dd, :, :, g * 8:g * 8 + nch], decs.t[:, dd, :, 0:nch], r=[decs.b], wp=[dDEC])
                    else:
                        k.dma("sp", d["HQ"][dd, :, :, tok], hqs.t[:, dd, :, 0:n], r=[hqs.b], wp=[dHQ])
                        k.dma("sp", d["HK"][dd, :, :, tok], hks.t[:, dd, :, 0:n], r=[hks.b], wp=[dHK])
                        k.dma("sp", d["KH"][dd, tok, :].rearrange("(s p) e -> p s e", p=128), khs.t[:, 0:nsub, dd, :], r=[khs.b], wp=[dKH])
                        k.dma("sp", d["DEC"][dd, :, :, g * 8:g * 8 + nch], decs.t[:, dd, :, 0:nch], r=[decs.b], wp=[dDEC])

        run(d["XTc"], L, True)
        run(d["XT"], S, False)
        P.end_phase()


def phase_nat(c):
    nc, P, k, d, cfg = c.nc, c.P, c.k, c.d, c.cfg
    S, L, ROWS = cfg.S, cfg.L, cfg.ROWS
    NCC = L // 128
    NCH = 4 + NCC
    with ExitStack() as ph:
        BFf = sb(ph, nc, [128, 7680], F32, name="bff")
        BFb = sb(ph, nc, [128, 8, 960], BF16, name="bfb")
        k.dma("sp", BFf.t[0:64, :], d["rpb_full"], wp=[BFf.b])
        k.dma("sp", BFf.t[64:128, :], d["rpb_full"], wp=[BFf.b])
        k.cp("dve", BFb.t[:].rearrange("p h e -> p (h e)"), BFf.t[:], r=[BFf.b], w=[BFb.b])
        KcT = sb(ph, nc, [128, 4, L], BF16, name="kct")
        VcA = sb(ph, nc, [128, NCC, 520], BF16, name="vca")
        k.dma("sp", KcT.t[:], d["KcT"], w=[KcT.b])
        k.dma("sp", VcA.t[:], d["VcA"].rearrange("(c p) f -> p c f", p=128), w=[VcA.b])
        QR = sb(ph, nc, [128, 4, 512], BF16, n=2, name="qr")
        QF = sb(ph, nc, [128, 4, 512], BF16, n=2, name="qf")
        KW = sb(ph, nc, [128, 4, 512], BF16, n=3, name="kw")
        VW = sb(ph, nc, [128, 4, 520], BF16, n=3, name="vw")
        PT = sb(ph, nc, [128, NCH * 64], BF16, n=3, name="pt")
        NS = sb(ph, nc, [64, 512], BF16, n=2, name="ns")
        RD = sb(ph, nc, [64, 8], F32, n=2, name="rd")
        pS = ps(ph, nc, [128, 512], F32, n=4, name="pS")
        pO = ps(ph, nc, [64, 4, 65], F32, n=4, name="pO")
        dCAT = Buf("dCATn")
        it = 0
        for r in range(ROWS):
            g8, ro = r // 8, (r % 8) * 64
            qr, qf = QR[g8 % 2], QF[g8 % 2]
            if r % 8 == 0:
                k.dma("sp", qr.t[:], d["QTr"][:, :, g8 * 512:(g8 + 1) * 512], w=[qr.b])
                k.dma("sp", qf.t[:], d["QTf"][:, :, g8 * 512:(g8 + 1) * 512], w=[qf.b])
            rs = min(max(r - 4, 0), ROWS - 8)
            dr0 = rs - r + 7
            kw, vw = KW[r % 3], VW[r % 3]
            k.dma("sp", kw.t[:], d["KTr"][:, :, rs * 64:rs * 64 + 512], w=[kw.b])
            k.dma("sp", vw.t[:], d["VA"][rs * 64:rs * 64 + 512, :].rearrange("(c p) f -> p c f", p=128), w=[vw.b])
            ns, rd = NS[r % 2], RD[r % 2]
            po2 = (pO[(2 * r) % 4], pO[(2 * r + 1) % 4])
            def scores(h):
                nonlocal it
                hp, pb = h // 2, (h % 2) * 64
                psx, pt = pS[it % 4], PT[it % 3]
                it += 1
                first = True
                for kc in range(4):
                    k.mm(psx.t[:, kc * 64:(kc + 1) * 64], kw.t[pb:pb + 64, hp, kc * 128:(kc + 1) * 128], qr.t[pb:pb + 64, hp, ro:ro + 64], True, False,
                         r=[kw.b, qr.b], w=[psx.b] if first else (), wp=() if first else [psx.b])
                    first = False
                    k.mm(psx.t[:, kc * 64:(kc + 1) * 64], BFb.t[pb:pb + 64, h, (dr0 + 2 * kc) * 64:(dr0 + 2 * kc) * 64 + 128], c.ident_b.t[pb:pb + 64, pb:pb + 64], False, True,
                         r=[BFb.b], wp=[psx.b])
                for cc in range(NCC):
                    k.mm(psx.t[:, (4 + cc) * 64:(5 + cc) * 64], KcT.t[pb:pb + 64, hp, cc * 128:(cc + 1) * 128], qf.t[pb:pb + 64, hp, ro:ro + 64], True, True,
                         r=[KcT.b, qf.b], wp=[psx.b])
                k.act(pt.t[:], psx.t[:, 0:NCH * 64], AF.Exp, r=[psx.b], w=[pt.b])
                return pt

            def pv(h, pt):
                po = po2[h // 4]
                hh = h % 4
                for ch in range(NCH):
                    rhs = vw.t[:, ch, h * 65:(h + 1) * 65] if ch < 4 else VcA.t[:, ch - 4, h * 65:(h + 1) * 65]
                    k.mm(po.t[:, hh, :], pt.t[:, ch * 64:(ch + 1) * 64], rhs, ch == 0, ch == NCH - 1,
                         r=[pt.b, vw.b, VcA.b], w=[po.b] if (ch == 0 and hh == 0) else (), wp=() if (ch == 0 and hh == 0) else [po.b])

            pts = [scores(0)]
            for h in range(8):
                if h + 1 < 8:
                    pts.append(scores(h + 1))
                pv(h, pts[h])
            for half in range(2):
                po = po2[half]
                k.recip(rd.t[:, half * 4:(half + 1) * 4], po.t[:, :, 64], r=[po.b], wp=[rd.b])
                k.tt("dve", ns.t[:, half * 256:(half + 1) * 256].rearrange("p (h e) -> p h e", h=4), po.t[:, :, 0:64],
                     rd.t[:, half * 4:(half + 1) * 4].unsqueeze(2).to_broadcast([64, 4, 64]), ALU.mult, r=[po.b, rd.b], wp=[ns.b])
            k.dma("sp", d["CAT"][r * 64:(r + 1) * 64, 0:512], ns.t[:], r=[ns.b], wp=[dCAT])
        P.end_phase()


def phase_hgrn(c):
    nc, P, k, d, cfg = c.nc, c.P, c.k, c.d, c.cfg
    S, L = cfg.S, cfg.L
    NG = S // 512
    NCHT = S // 64
    LC = L // 64
    with ExitStack() as ph:
        TRI = sb(ph, nc, [128, 2, 64], F32, name="tri")
        k.dma("sp", TRI.t[:, 0, :], d["k_trif"], wp=[TRI.b])
        k.dma("sp", TRI.t[:, 1, :], d["k_trib"], wp=[TRI.b])
        og = sb(ph, nc, [128, 128], F32, name="og")
        ONG = sb(ph, nc, [128, 512], F32, name="ong")
        k.dma("sp", og.t[:], d["hgrn_o_norm"].partition_broadcast(128), w=[og.b])
        k.ts("dve", ONG.t[:].rearrange("p (h e) -> p h e", h=4), og.t[:].unsqueeze(1).to_broadcast([128, 4, 128]), 1.0, ALU.mult, r=[og.b], w=[ONG.b])
        S32s = sb(ph, nc, [128, 4, 128], F32, n=2, name="s32")
        Sbfs = sb(ph, nc, [128, 4, 128], BF16, n=2, name="sbf")
        DECt = sb(ph, nc, [128, 4, NCHT], F32, name="dect")
        DECc = sb(ph, nc, [128, 4, LC], F32, name="decc")
        KHc = sb(ph, nc, [128, L // 128, 512], BF16, name="khc")
        VHc = sb(ph, nc, [128, L // 128, 512], BF16, name="vhc")
        HQg = sb(ph, nc, [128, 4, 512], BF16, n=2, name="hqg")
        HKg = sb(ph, nc, [128, 4, 512], BF16, n=2, name="hkg")
        KHg = sb(ph, nc, [128, 4, 512], BF16, n=2, name="khg")
        VHg = sb(ph, nc, [128, 4, 512], BF16, n=2, name="vhg")
        SC = [sb(ph, nc, [128, 256], BF16, n=2, name="sc%d" % i) for i in range(2)]
        for i in range(2):
            for s_ in SC[i]:
                k.memset("pool", s_.t[:], 0.0, w=[s_.b])
        OFs = sb(ph, nc, [64, 512], F32, n=2, name="ofs")
        OFc = sb(ph, nc, [64, 512], F32, n=2, name="ofc")
        Gc = sb(ph, nc, [64, 512], F32, n=2, name="gc")
        O32 = sb(ph, nc, [64, 512], F32, n=2, name="o32")
        SQ = sb(ph, nc, [64, 512], F32, n=2, name="sq")
        ST = sb(ph, nc, [64, 16], F32, n=2, name="st")
        Y1 = sb(ph, nc, [64, 512], F32, n=2, name="y1")
        YB = sb(ph, nc, [64, 512], BF16, n=2, name="yb")
        pSs = ps(ph, nc, [128, 512], F32, n=2, name="pSs")
        pSo = ps(ph, nc, [128, 512], F32, n=2, name="pSo")
        pSt = ps(ph, nc, [128, 512], F32, n=2, name="pSt")
        dOF, dCAT = Buf("dOF"), Buf("dCATh")
        k.dma("sp", VHc.t[:], d["VHc"].rearrange("(s p) e -> p s e", p=128), w=[VHc.b])
        it = 0
        for dd in range(2):
            k.memset("dve", S32s[0].t[:], 0.0, w=[S32s[0].b])
            k.dma("sp", DECt.t[:], d["DEC"][dd], w=[DECt.b])
            k.dma("sp", DECc.t[:], d["DECc"][dd], w=[DECc.b])
            k.dma("sp", KHc.t[:], d["KHc"][dd].rearrange("(s p) e -> p s e", p=128), w=[KHc.b])

            sn = 0

            def state_update(khs, vhs, tl, pb, dec_ap_fn):
                nonlocal it, sn
                pst = pSt[it % 2]
                for h in range(4):
                    k.mm(pst.t[:, h * 128:(h + 1) * 128], khs.t[pb:pb + 64, tl, h * 128:(h + 1) * 128], vhs.t[pb:pb + 64, tl, h * 128:(h + 1) * 128], True, True,
                         r=[khs.b, vhs.b], w=[pst.b] if h == 0 else (), wp=() if h == 0 else [pst.b])
                so, sw = S32s[sn % 2], S32s[(sn + 1) % 2]
                for h in range(4):
                    k.stt(sw.t[:, h, :], so.t[:, h, :], dec_ap_fn(h), pst.t[:, h * 128:(h + 1) * 128], ALU.mult, ALU.add,
                          r=[so.b, pst.b, DECt.b, DECc.b], wp=[sw.b])
                nb = Sbfs[(sn + 1) % 2]
                k.cp("act", nb.t[:], sw.t[:], r=[sw.b], w=[nb.b])
                sn += 1

            chs = range(LC) if dd == 0 else range(LC - 1, -1, -1)
            for ch in chs:
                state_update(KHc, VHc, ch // 2, (ch % 2) * 64, lambda h, ch=ch: DECc.t[:, h, ch:ch + 1])
                it += 1
            groups = list(range(NG)) if dd == 0 else list(range(NG - 1, -1, -1))
            order = []
            for gi, g in enumerate(groups):
                for tl in (range(4) if dd == 0 else range(3, -1, -1)):
                    for cc in ((0, 1) if dd == 0 else (1, 0)):
                        order.append((gi, g, tl, cc))
            loaded = set()

            def ensure(gi, g):
                if gi in loaded:
                    return
                loaded.add(gi)
                hq, hk, kh, vh = HQg[gi % 2], HKg[gi % 2], KHg[gi % 2], VHg[gi % 2]
                tok = slice(g * 512, (g + 1) * 512)
                k.dma("sp", hq.t[:], d["HQ"][dd, :, :, tok], w=[hq.b])
                k.dma("sp", hk.t[:], d["HK"][dd, :, :, tok], w=[hk.b])
                k.dma("sp", kh.t[:], d["KH"][dd, tok, :].rearrange("(s p) e -> p s e", p=128), w=[kh.b])
                k.dma("sp", vh.t[:], d["VH"][tok, :].rearrange("(s p) e -> p s e", p=128), w=[vh.b])

            def scores(n):
                gi, g, tl, cc = order[n]
                ensure(gi, g)
                hq, hk = HQg[gi % 2], HKg[gi % 2]
                pb = cc * 64
                toff, qoff = tl * 128, tl * 128 + cc * 64
                pss = pSs[n % 2]
                sc = SC[cc][(n // 2) % 2]
                for h in range(4):
                    k.mm(pss.t[:, h * 64:(h + 1) * 64], hk.t[:, h, toff:toff + 128], hq.t[:, h, qoff:qoff + 64], True, True,
                         r=[hk.b, hq.b], w=[pss.b] if h == 0 else (), wp=() if h == 0 else [pss.b])
                k.tt("dve", sc.t[pb:pb + 64, :].rearrange("p (h t) -> p h t", h=4), pss.t[pb:pb + 64, 0:256].rearrange("p (h t) -> p h t", h=4),
                     TRI.t[pb:pb + 64, dd, :].unsqueeze(1).to_broadcast([64, 4, 64]), ALU.mult, r=[pss.b, TRI.b], wp=[sc.b])
                return sc

            def rest(n, sc):
                nonlocal it
                gi, g, tl, cc = order[n]
                hq, hk, kh, vh = HQg[gi % 2], HKg[gi % 2], KHg[gi % 2], VHg[gi % 2]
                ch = g * 8 + tl * 2 + cc
                pb = cc * 64
                qoff = tl * 128 + cc * 64
                pso = pSo[n % 2]
                Sbf = Sbfs[sn % 2]
                state_update(kh, vh, tl, pb, lambda h, ch=ch: DECt.t[:, h, ch:ch + 1])
                it += 1
                for h in range(4):
                    k.mm(pso.t[0:64, h * 128:(h + 1) * 128], sc.t[:, h * 64:(h + 1) * 64], vh.t[:, tl, h * 128:(h + 1) * 128], True, False,
                         r=[sc.b, vh.b], w=[pso.b] if h == 0 else (), wp=() if h == 0 else [pso.b])
                    k.mm(pso.t[0:64, h * 128:(h + 1) * 128], hq.t[:, h, qoff:qoff + 64], Sbf.t[:, h, :], False, True,
                         r=[hq.b, Sbf.b], wp=[pso.b])
                rows = slice(ch * 64, (ch + 1) * 64)
                if dd == 0:
                    ofs = OFs[n % 2]
                    k.cp("act", ofs.t[:], pso.t[0:64, :], r=[pso.b], w=[ofs.b])
                    k.dma("sp", d["OF"][rows, :], ofs.t[:], r=[ofs.b], wp=[dOF])
                else:
                    ofc, gc, o32, sq, st, y1, yb = (X[n % 2] for X in (OFc, Gc, O32, SQ, ST, Y1, YB))
                    k.dma("sp", ofc.t[:], d["OF"][rows, :], r=[dOF], w=[ofc.b])
                    k.dma("sp", gc.t[:], d["G"][rows, :], w=[gc.b])
                    k.tt("dve", o32.t[:], pso.t[0:64, :], ofc.t[:], ALU.add, r=[pso.b, ofc.b], w=[o32.b])
                    k.act(sq.t[:], o32.t[:], AF.Square, r=[o32.b], w=[sq.b])
                    k.red(st.t[:, 0:4], sq.t[:].rearrange("p (h e) -> p h e", h=4), r=[sq.b], wp=[st.b])
                    k.ts("dve", st.t[:, 4:8], st.t[:, 0:4], 1.0 / 128, ALU.mult, EPS, ALU.add, r=[st.b], wp=[st.b])
                    k.act(st.t[:, 8:12], st.t[:, 4:8], AF.Sqrt, r=[st.b], wp=[st.b])
                    k.recip(st.t[:, 12:16], st.t[:, 8:12], r=[st.b], wp=[st.b])
                    k.tt("dve", y1.t[:].rearrange("p (h e) -> p h e", h=4), o32.t[:].rearrange("p (h e) -> p h e", h=4),
                         st.t[:, 12:16].unsqueeze(2).to_broadcast([64, 4, 128]), ALU.mult, r=[o32.b, st.b], w=[y1.b])
                    k.tt("pool", y1.t[:], y1.t[:], ONG.t[0:64, :], ALU.mult, r=[y1.b, ONG.b], w=[y1.b])
                    k.tt("pool", yb.t[:], y1.t[:], gc.t[:], ALU.mult, r=[y1.b, gc.b], w=[yb.b])
                    k.dma("sp", d["CAT"][rows, 512:1024], yb.t[:], r=[yb.b], wp=[dCAT])

            N = len(order)
            cur = scores(0)
            for n in range(N):
                nxt = scores(n + 1) if n + 1 < N else None
                rest(n, cur)
                cur = nxt
        P.end_phase()


def phase_post(c, layer):
    nc, P, k, d, cfg = c.nc, c.P, c.k, c.d, c.cfg
    S, E, NT = cfg.S, cfg.E, cfg.NT
    NG = S // 512
    hin = d["x"] if layer == 0 else d["H2"]
    hout = d["H1"] if layer == 0 else d["H3"]
    with ExitStack() as ph:
        Wo = sb(ph, nc, [128, 8, 1024], BF16, name="Wo")
        wsrc = d["ab_w_out"] if layer == 0 else d["conv_w2"]
        k.dma("pool", Wo.t[:], wsrc.rearrange("(k p) n -> p k n", p=128), w=[Wo.b])
        RW = sb(ph, nc, [128, 8, E], F32, name="RW")
        k.dma("sp", RW.t[:], d["router_w"][layer].rearrange("(k p) e -> p k e", p=128), w=[RW.b])
        RB = sb(ph, nc, [128, E], F32, name="RB")
        k.dma("sp", RB.t[:], d["router_b"][layer:layer + 1, :].partition_broadcast(128), w=[RB.b])
        B2t = sb(ph, nc, [E, 1024], F32, name="B2t")
        k.dma("sp", B2t.t[:], d["moe_b2"][layer], w=[B2t.b])
        A2 = make_A(c, ph, "norm2_g", layer, 4096, c.MOD)
        XIN = sb(ph, nc, [128, 1024], F32, n=2, name="xin")
        Hs = sb(ph, nc, [128, 1024], F32, n=2, name="hs")
        V1s = sb(ph, nc, [128, 1024], F32, n=2, name="v1")
        tmps = [{"ss": sb(ph, nc, [128, 16], F32, name="ss"), "t1": sb(ph, nc, [128, 1024], F32, name="t1")} for _ in range(2)]
        XFs = sb(ph, nc, [128, 1024], F32, n=2, name="xf")
        XB = sb(ph, nc, [128, 1024], BF16, n=2, name="xb")
        XT2g = sb(ph, nc, [128, 8, 512], BF16, n=2, name="xt2g")
        XFTs = sb(ph, nc, [128, 8, 128], F32, n=2, name="xft")
        LG = sb(ph, nc, [128, 4 * E + 32], F32, n=2, name="lg")
        CTs = sb(ph, nc, [E, 128], F32, n=2, name="ct")
        ACss = sb(ph, nc, [128, 1024], F32, n=2, name="acs")
        pT = ps(ph, nc, [128, 1024], BF16, name="pT")
        pT2 = ps(ph, nc, [128, 1024], BF16, name="pT2")
        pY = ps(ph, nc, [128, 512], F32, n=2, name="pY")
        pTf = ps(ph, nc, [128, 512], F32, n=2, name="pTf")
        pLg = ps(ph, nc, [128, E], F32, name="pLg")
        pCT = ps(ph, nc, [E, 128], F32, name="pCT")
        dH, dXT2, dCOMB, dACC = Buf("dH"), Buf("dXT2"), Buf("dCOMB"), Buf("dACC")
        if layer == 0:
            CATt = sb(ph, nc, [128, 1024], BF16, n=2, name="catt")
            catT = sb(ph, nc, [128, 8, 128], BF16, n=2, name="catT")
        else:
            YG = sb(ph, nc, [128, 8, 512], F32, name="yg")
            YSQ = sb(ph, nc, [128, 512], F32, n=2, name="ysq")
            Mm = sb(ph, nc, [128, 512], F32, name="mm_")
            MSQ = sb(ph, nc, [128, 512], F32, name="msq")
            RS = sb(ph, nc, [128, 512], F32, name="rs")
            TA = sb(ph, nc, [128, 512], F32, n=2, name="ta")
            TB = sb(ph, nc, [128, 512], F32, n=2, name="tb")
            HN = sb(ph, nc, [128, 8, 512], BF16, name="hn")
            pSum = pTf[0]
            pSq = pTf[1]
            lnr = sb(ph, nc, [16, 128], F32, name="lnr")
            LNP = sb(ph, nc, [128, 16], F32, name="lnp")
            k.dma("sp", lnr.t[0:8, :], d["conv_ln_g"], wp=[lnr.b])
            k.dma("sp", lnr.t[8:16, :], d["conv_ln_b"], wp=[lnr.b])
            k.tr(pLg.t[:, 0:16] if E >= 16 else pTf[0].t[:, 0:16], lnr.t[:], c.ident_f.t[0:16, 0:16], r=[lnr.b], w=[pLg.b if E >= 16 else pTf[0].b])
            k.cp("dve", LNP.t[:], pLg.t[:, 0:16] if E >= 16 else pTf[0].t[:, 0:16], r=[pLg.b if E >= 16 else pTf[0].b], w=[LNP.b])
            b2bc = sb(ph, nc, [128, 1024], F32, name="b2bc")
            B2M = sb(ph, nc, [128, 1024], F32, name="b2m")
            k.dma("sp", b2bc.t[:], d["conv_b2"].partition_broadcast(128), w=[b2bc.b])
            k.tt("dve", B2M.t[:], b2bc.t[:], c.MOD.t[:, 2048:3072], ALU.mult, r=[b2bc.b, c.MOD.b], w=[B2M.b])

        it = 0
        for g in range(NG):
            xt2g = XT2g[g % 2]
            if layer == 1:
                k.dma("sp", YG.t[:], d["YD"][:, :, g * 512:(g + 1) * 512], w=[YG.b])
                for cch in range(8):
                    ysq = YSQ[cch % 2]
                    k.act(ysq.t[:], YG.t[:, cch, :], AF.Square, r=[YG.b], w=[ysq.b])
                    k.mm(pSum.t[:], c.ones_f.t[:], YG.t[:, cch, :], cch == 0, cch == 7, r=[YG.b, c.ones_f.b], w=[pSum.b] if cch == 0 else (), wp=() if cch == 0 else [pSum.b])
                    k.mm(pSq.t[:], c.ones_f.t[:], ysq.t[:], cch == 0, cch == 7, r=[ysq.b, c.ones_f.b], w=[pSq.b] if cch == 0 else (), wp=() if cch == 0 else [pSq.b])
                k.act(Mm.t[:], pSum.t[:], AF.Copy, r=[pSum.b], w=[Mm.b], scale=1.0 / 1024)
                k.tt("pool", MSQ.t[:], Mm.t[:], Mm.t[:], ALU.mult, r=[Mm.b], w=[MSQ.b])
                k.stt(RS.t[:], pSq.t[:], 1.0 / 1024, MSQ.t[:], ALU.mult, ALU.subtract, r=[pSq.b, MSQ.b], w=[RS.b])
                k.ts("dve", RS.t[:], RS.t[:], EPS, ALU.add, r=[RS.b], w=[RS.b])
                k.act(RS.t[:], RS.t[:], AF.Sqrt, r=[RS.b], w=[RS.b])
                k.recip(RS.t[:], RS.t[:], r=[RS.b], w=[RS.b])
                for cch in range(8):
                    ta, tb = TA[cch % 2], TB[cch % 2]
                    k.tt("dve", ta.t[:], YG.t[:, cch, :], Mm.t[:], ALU.subtract, r=[YG.b, Mm.b], w=[ta.b])
                    k.tt("pool", tb.t[:], ta.t[:], RS.t[:], ALU.mult, r=[ta.b, RS.b], w=[tb.b])
                    k.act(HN.t[:, cch, :], tb.t[:], AF.Silu, r=[tb.b, LNP.b], wp=[HN.b], scale=LNP.t[:, cch:cch + 1], bias=LNP.t[:, 8 + cch:9 + cch])
            states = {}

            def front(j):
                nonlocal it
                t = g * 4 + j
                rows = slice(t * 128, (t + 1) * 128)
                xin, hs, xb = XIN[it % 2], Hs[it % 2], XB[it % 2]
                lg = LG[it % 2]
                V1, XF = V1s[it % 2], XFs[it % 2]
                k.dma("sp", xin.t[:], hin[rows, :], w=[xin.b])
                if layer == 0:
                    cat, ctT = CATt[it % 2], catT[it % 2]
                    k.dma("sp", cat.t[:], d["CAT"][rows, :], w=[cat.b])
                    for kk in range(8):
                        k.tr(pT.t[:, kk * 128:(kk + 1) * 128], cat.t[:, kk * 128:(kk + 1) * 128], c.ident_b.t[:], r=[cat.b],
                             w=[pT.b] if kk == 0 else (), wp=() if kk == 0 else [pT.b])
                    k.cp("act", ctT.t[:].rearrange("p k t -> p (k t)"), pT.t[:], r=[pT.b], w=[ctT.b])
                    lhs = lambda kk: ctT.t[:, kk, :]
                    lb_ = ctT.b
                else:
                    lhs = lambda kk: HN.t[:, kk, j * 128:(j + 1) * 128]
                    lb_ = HN.b
                it += 1
                for half in range(2):
                    for kk in range(8):
                        k.mm(pY[half].t[:], lhs(kk), Wo.t[:, kk, half * 512:(half + 1) * 512], kk == 0, kk == 7,
                             r=[lb_, Wo.b], w=[pY[half].b] if kk == 0 else (), wp=() if kk == 0 else [pY[half].b])
                    k.tt("dve", V1.t[:, half * 512:(half + 1) * 512], pY[half].t[:], c.MOD.t[:, 2048 + half * 512:2048 + (half + 1) * 512], ALU.mult,
                         r=[pY[half].b, c.MOD.b], wp=[V1.b])
                if layer == 1:
                    k.tt("pool", xin.t[:], xin.t[:], B2M.t[:], ALU.add, r=[xin.b, B2M.b], w=[xin.b])
                k.tt("pool", hs.t[:], V1.t[:], xin.t[:], ALU.add, r=[V1.b, xin.b], w=[hs.b])
                k.dma("sp", hout[rows, :], hs.t[:], r=[hs.b], wp=[dH])
                norm_mod(c, hs, A2, c.MOD.t[:, 3072:4096], c.MOD.b, [(XF.t[:], "dve", XF), (xb.t[:], "pool", xb)], tmps[it % 2], j)

                states[j] = (t, rows, hs, xb, lg, XF)

            def back(j):
                t, rows, hs, xb, lg, XF = states[j]
                XFT_, CT_, ACs_ = XFTs[t % 2], CTs[t % 2], ACss[t % 2]
                for kk in range(8):
                    k.tr(pT2.t[:, kk * 128:(kk + 1) * 128], xb.t[:, kk * 128:(kk + 1) * 128], c.ident_b.t[:], r=[xb.b],
                         w=[pT2.b] if kk == 0 else (), wp=() if kk == 0 else [pT2.b])
                k.cp("act", xt2g.t[:, :, j * 128:(j + 1) * 128], pT2.t[:].rearrange("p (k t) -> p k t", k=8), r=[pT2.b], wp=[xt2g.b])
                for kk in range(8):
                    pf = pTf[kk // 4]
                    k.tr(pf.t[:, (kk % 4) * 128:(kk % 4 + 1) * 128], XF.t[:, kk * 128:(kk + 1) * 128], c.ident_f.t[:], r=[XF.b],
                         w=[pf.b] if kk % 4 == 0 else (), wp=() if kk % 4 == 0 else [pf.b])
                for hf in range(2):
                    k.cp("act" if hf else "dve", XFT_.t[:, hf * 4:(hf + 1) * 4, :], pTf[hf].t[:].rearrange("p (k t) -> p k t", k=4), r=[pTf[hf].b], wp=[XFT_.b])
                for kk in range(8):
                    k.mm(pLg.t[:], XFT_.t[:, kk, :], RW.t[:, kk, :], kk == 0, kk == 7, r=[XFT_.b, RW.b], w=[pLg.b] if kk == 0 else (), wp=() if kk == 0 else [pLg.b])
                L0, MK, EX, EXM, MS = (lg.t[:, 0:E], lg.t[:, E:2 * E], lg.t[:, 2 * E:3 * E], lg.t[:, 3 * E:4 * E], lg.t[:, 4 * E:4 * E + 32])
                k.tt("dve", L0, pLg.t[:], RB.t[:], ALU.add, r=[pLg.b, RB.b], wp=[lg.b])
                P.op("dve", (lambda e, o=MS[:, 0:8], i_=L0: e.max(out=o, in_=i_)), reads=[lg.b], wpart=[lg.b])
                k.ts("dve", MK, L0, MS[:, 3:4], ALU.is_ge, r=[lg.b], wp=[lg.b])
                k.ts("dve", MS[:, 8:9], MS[:, 0:1], -1.0, ALU.mult, r=[lg.b], wp=[lg.b])
                k.act(EX, L0, AF.Exp, r=[lg.b], wp=[lg.b], bias=MS[:, 8:9])
                k.stt(EXM, EX, 1.0, MK, ALU.mult, ALU.mult, r=[lg.b], wp=[lg.b], accum=MS[:, 9:10])
                k.recip(MS[:, 10:11], MS[:, 9:10], r=[lg.b], wp=[lg.b])
                k.ts("dve", EXM, EXM, MS[:, 10:11], ALU.mult, r=[lg.b], wp=[lg.b])
                k.dma("sp", d["COMB"][rows, :], EXM, r=[lg.b], wp=[dCOMB])
                k.dma("sp", d["MK"][rows, :], MK, r=[lg.b], wp=[dCOMB])
                k.dma("sp", d["XM2"][rows, :], xb.t[:], r=[xb.b], wp=[dXT2])
                k.tr(pCT.t[:], EXM, c.ident_f.t[:], r=[lg.b], w=[pCT.b])
                k.cp("act", CT_.t[:], pCT.t[:], r=[pCT.b], w=[CT_.b])
                for half in range(2):
                    k.mm(pTf[half].t[:], CT_.t[:], B2t.t[:, half * 512:(half + 1) * 512], True, True, r=[CT_.b, B2t.b], w=[pTf[half].b])
                    k.cp("act" if half else "dve", ACs_.t[:, half * 512:(half + 1) * 512], pTf[half].t[:], r=[pTf[half].b], wp=[ACs_.b])
                k.dma("sp", d["ACCd"][rows, :], ACs_.t[:], r=[ACs_.b], wp=[dACC])

            front(0)
            for j in range(4):
                if j + 1 < 4:
                    front(j + 1)
                back(j)
            k.dma("sp", d["XT2"][:, :, g * 512:(g + 1) * 512], xt2g.t[:], r=[xt2g.b], wp=[dXT2])
        P.end_phase()


def phase_moe(c, layer, MOD5):
    nc, P, k, d, cfg = c.nc, c.P, c.k, c.d, c.cfg
    S, E, MB = cfg.S, cfg.E, cfg.MB
    NTB = MB // 128
    NH = MB // 512
    hin = d["H1"] if layer == 0 else d["H3"]
    hout = d["H2"] if layer == 0 else d["out"]
    with ExitStack() as ph:
        nb1 = (E * 16) // 128
        B1T = sb(ph, nc, [128, E * 16], F32, name="b1t")
        b1r = sb(ph, nc, [128, 128], F32, n=2, name="b1r")
        ACC = sb(ph, nc, [128, NTB, 1024], F32, name="acc")
        XT2b = sb(ph, nc, [128, 8, MB], BF16, name="xt2b")
        CMB = sb(ph, nc, [128, NTB, E], F32, name="cmb")
        W1 = sb(ph, nc, [128, 8, 2048], BF16, n=2, name="w1")
        W2 = sb(ph, nc, [128, 8, 1024], BF16, name="w2")
        ACTT = sb(ph, nc, [128, 8, 512], BF16, n=2, name="actt")
        G1 = sb(ph, nc, [128, 512], F32, n=2, name="g1")
        S1 = sb(ph, nc, [128, 512], F32, n=2, name="s1")
        L1 = sb(ph, nc, [128, 512], F32, n=2, name="l1")
        L2 = sb(ph, nc, [128, 512], F32, n=2, name="l2")
        GS = sb(ph, nc, [128, 512], F32, n=2, name="gs")
        HT = sb(ph, nc, [128, 1024], F32, n=2, name="ht")
        pG = ps(ph, nc, [128, 512], F32, n=2, name="pG")
        pL = ps(ph, nc, [128, 512], F32, n=2, name="pL")
        pO = ps(ph, nc, [128, 512], F32, n=4, name="pO")
        dOUT = Buf("dOUT")
        for i in range(nb1):
            br = b1r[i % 2]
            k.dma("sp", br.t[:], d["moe_b1"][layer, i * 128:(i + 1) * 128, :], w=[br.b])
            k.tr(pG[i % 2].t[:, 0:128], br.t[:], c.ident_f.t[:], r=[br.b], w=[pG[i % 2].b])
            k.cp("dve", B1T.t[:, i * 128:(i + 1) * 128], pG[i % 2].t[:, 0:128], r=[pG[i % 2].b], wp=[B1T.b])
        wi = 0
        io = 0
        for blk in range(S // MB):
            t0 = blk * NTB
            rows = slice(blk * MB, (blk + 1) * MB)
            k.dma("sp", ACC.t[:], d["ACCd"][rows, :].rearrange("(t p) n -> p t n", p=128), w=[ACC.b])
            k.dma("sp", XT2b.t[:], d["XT2"][:, :, rows], w=[XT2b.b])
            k.dma("sp", CMB.t[:], d["COMB"][rows, :].rearrange("(t p) e -> p t e", p=128), w=[CMB.b])
            for e in range(E):
                w1 = W1[wi % 2]
                wi += 1
                w1v = d["moe_w1"][layer, e].rearrange("(k p) n -> p k n", p=128)
                k.dma("pool", w1.t[:, 0:4, :], w1v[:, 0:4, :], wp=[w1.b])
                k.dma("pool", w1.t[:, 4:8, :], w1v[:, 4:8, :], wp=[w1.b])
                w2_loaded = False
                for ht in range(NH):
                    actt = ACTT[io % 2]
                    for pr in range(8):
                        pg, pl = pG[pr % 2], pL[pr % 2]
                        g1, s1, l1, l2, gs = (X[pr % 2] for X in (G1, S1, L1, L2, GS))
                        for kk in range(8):
                            k.mm(pg.t[:], w1.t[:, kk, pr * 128:(pr + 1) * 128], XT2b.t[:, kk, ht * 512:(ht + 1) * 512], kk == 0, kk == 7,
                                 r=[w1.b, XT2b.b], w=[pg.b] if kk == 0 else (), wp=() if kk == 0 else [pg.b])
                        for kk in range(8):
                            k.mm(pl.t[:], w1.t[:, kk, 1024 + pr * 128:1024 + (pr + 1) * 128], XT2b.t[:, kk, ht * 512:(ht + 1) * 512], kk == 0, kk == 7,
                                 r=[w1.b, XT2b.b], w=[pl.b] if kk == 0 else (), wp=() if kk == 0 else [pl.b])
                        bg = B1T.t[:, e * 16 + pr:e * 16 + pr + 1]
                        bl = B1T.t[:, e * 16 + 8 + pr:e * 16 + 8 + pr + 1]
                        k.ts("dve", g1.t[:], pg.t[:], bg, ALU.add, 7.0, ALU.min, r=[pg.b, B1T.b], w=[g1.b])
                        k.act(s1.t[:], g1.t[:], AF.Sigmoid, r=[g1.b], w=[s1.b], scale=1.702)
                        k.act(l1.t[:], pl.t[:], AF.Identity, r=[pl.b, B1T.b], w=[l1.b], bias=bl)
                        k.ts("dve", l2.t[:], l1.t[:], 7.0, ALU.min, -7.0, ALU.max, r=[l1.b], w=[l2.b])
                        k.tt("pool", gs.t[:], g1.t[:], s1.t[:], ALU.mult, r=[g1.b, s1.b], w=[gs.b])
                        k.stt(actt.t[:, pr, :], l2.t[:], 1.0, gs.t[:], ALU.add, ALU.mult, r=[l2.b, gs.b], wp=[actt.b])
                    if not w2_loaded:
                        k.dma("pool", W2.t[:], d["moe_w2"][layer, e].rearrange("(k p) n -> p k n", p=128), w=[W2.b])
                        w2_loaded = True
                    for sub in range(4):
                        tl = ht * 4 + sub
                        for half in range(2):
                            po = pO[io % 4]
                            io += 1
                            for jj in range(8):
                                k.mm(po.t[:], actt.t[:, jj, sub * 128:(sub + 1) * 128], W2.t[:, jj, half * 512:(half + 1) * 512], jj == 0, jj == 7,
                                     r=[actt.b, W2.b], w=[po.b] if jj == 0 else (), wp=() if jj == 0 else [po.b])
                            acc = ACC.t[:, tl, half * 512:(half + 1) * 512]
                            k.stt(acc, po.t[:], CMB.t[:, tl, e:e + 1], acc, ALU.mult, ALU.add, r=[po.b, CMB.b, ACC.b], wp=[ACC.b])
            for tl in range(NTB):
                t = t0 + tl
                ht_ = HT[tl % 2]
                k.dma("sp", ht_.t[:], hin[t * 128:(t + 1) * 128, :], w=[ht_.b])
                k.tt("dve", ACC.t[:, tl, :], ACC.t[:, tl, :], MOD5.t[:], ALU.mult, r=[ACC.b, MOD5.b], wp=[ACC.b])
                k.tt("pool", ht_.t[:], ht_.t[:], ACC.t[:, tl, :], ALU.add, r=[ht_.b, ACC.b], w=[ht_.b])
                k.dma("sp", hout[t * 128:(t + 1) * 128, :], ht_.t[:], r=[ht_.b], wp=[dOUT])
        P.end_phase()


def phase_f(c):
    nc, P, k, d, cfg = c.nc, c.P, c.k, c.d, c.cfg
    S = cfg.S
    NG = S // 512
    with ExitStack() as ph:
        W = sb(ph, nc, [128, 8, 2048], BF16, name="Wc1")
        wv = d["conv_w1"].rearrange("(k p) n -> p k n", p=128)
        k.dma("pool", W.t[:, 0:4, :], wv[:, 0:4, :], wp=[W.b])
        k.dma("pool", W.t[:, 4:8, :], wv[:, 4:8, :], wp=[W.b])
        A = make_A(c, ph, "norm1_g", 1, 1024, c.MOD)
        b1r = sb(ph, nc, [16, 128], F32, name="b1r")
        CB1 = sb(ph, nc, [128, 16], F32, name="cb1")
        pB = ps(ph, nc, [128, 16], F32, name="pB")
        k.dma("sp", b1r.t[:], d["conv_b1"], w=[b1r.b])
        k.tr(pB.t[:], b1r.t[:], c.ident_f.t[0:16, 0:16], r=[b1r.b], w=[pB.b])
        k.cp("dve", CB1.t[:], pB.t[:], r=[pB.b], w=[CB1.b])
        Z = sb(ph, nc, [128, 8, 16], BF16, name="z")
        k.memset("dve", Z.t[:], 0.0, w=[Z.b])
        dGT = Buf("dGT")
        k.dma("sp", d["GT"][:, :, 0:16], Z.t[:], r=[Z.b], wp=[dGT])
        k.dma("sp", d["GT"][:, :, S + 16:S + 32], Z.t[:], r=[Z.b], wp=[dGT])
        XIN = sb(ph, nc, [128, 1024], F32, n=2, name="xin")
        tmps = [{"ss": sb(ph, nc, [128, 16], F32, name="ss"), "t1": sb(ph, nc, [128, 1024], F32, name="t1")} for _ in range(2)]
        XM = sb(ph, nc, [128, 1024], BF16, n=2, name="xm")
        XTG = sb(ph, nc, [128, 8, 512], BF16, n=2, name="xtg")
        SG = sb(ph, nc, [128, 512], F32, n=2, name="sg")
        GTs = sb(ph, nc, [128, 8, 512], BF16, n=2, name="gts")
        pT = ps(ph, nc, [128, 1024], BF16, name="pT")
        pA = ps(ph, nc, [128, 512], F32, n=2, name="pA")
        pGt = ps(ph, nc, [128, 512], F32, n=2, name="pGt")
        it = 0
        for g in range(NG):
            xtg, gts = XTG[g % 2], GTs[g % 2]
            for j in range(4):
                t = 4 * g + j
                xin, xm = XIN[it % 2], XM[it % 2]
                it += 1
                k.dma("sp", xin.t[:], d["H2"][t * 128:(t + 1) * 128, :], w=[xin.b])
                norm_mod(c, xin, A, c.MOD.t[:, 0:1024], c.MOD.b, [(xm.t[:], "pool", xm)], tmps[it % 2], j)
                for kk in range(8):
                    k.tr(pT.t[:, kk * 128:(kk + 1) * 128], xm.t[:, kk * 128:(kk + 1) * 128], c.ident_b.t[:], r=[xm.b],
                         w=[pT.b] if kk == 0 else (), wp=() if kk == 0 else [pT.b])
                k.cp("act", xtg.t[:, :, j * 128:(j + 1) * 128], pT.t[:].rearrange("p (k t) -> p k t", k=8), r=[pT.b], wp=[xtg.b])
            for cp_ in range(8):
                pa, pg, sg = pA[cp_ % 2], pGt[cp_ % 2], SG[cp_ % 2]
                for kk in range(8):
                    k.mm(pa.t[:], W.t[:, kk, cp_ * 128:(cp_ + 1) * 128], xtg.t[:, kk, :], kk == 0, kk == 7, r=[W.b, xtg.b],
                         w=[pa.b] if kk == 0 else (), wp=() if kk == 0 else [pa.b])
                for kk in range(8):
                    k.mm(pg.t[:], W.t[:, kk, 1024 + cp_ * 128:1024 + (cp_ + 1) * 128], xtg.t[:, kk, :], kk == 0, kk == 7, r=[W.b, xtg.b],
                         w=[pg.b] if kk == 0 else (), wp=() if kk == 0 else [pg.b])
                k.act(sg.t[:], pg.t[:], AF.Sigmoid, r=[pg.b, CB1.b], w=[sg.b], bias=CB1.t[:, 8 + cp_:9 + cp_])
                k.stt(gts.t[:, cp_, :], pa.t[:], CB1.t[:, cp_:cp_ + 1], sg.t[:], ALU.add, ALU.mult, r=[pa.b, sg.b, CB1.b], wp=[gts.b])
            k.dma("sp", d["GT"][:, :, 16 + g * 512:16 + (g + 1) * 512], gts.t[:], r=[gts.b], wp=[dGT])
        P.end_phase()


def phase_g1(c):
    nc, P, k, d, cfg = c.nc, c.P, c.k, c.d, c.cfg
    S = cfg.S
    NG = S // 512
    with ExitStack() as ph:
        dwr = sb(ph, nc, [32, 1024], F32, name="dwr")
        DWT = sb(ph, nc, [128, 8, 31], F32, name="dwt")
        dbr = sb(ph, nc, [8, 128], F32, name="dbr")
        DWB = sb(ph, nc, [128, 8], F32, name="dwb")
        DG = sb(ph, nc, [128, 8, 31, 128], BF16, name="dg")
        pD = ps(ph, nc, [128, 8, 32], F32, name="pD")
        pB = ps(ph, nc, [128, 8], F32, name="pB")
        k.dma("sp", dwr.t[0:31, :], d["conv_dw"], w=[dwr.b])
        for cch in range(8):
            k.tr(pD.t[:, cch, 0:31], dwr.t[0:31, cch * 128:(cch + 1) * 128], c.ident_f.t[0:31, 0:31], r=[dwr.b],
                 w=[pD.b] if cch == 0 else (), wp=() if cch == 0 else [pD.b])
        k.cp("dve", DWT.t[:], pD.t[:, :, 0:31], r=[pD.b], w=[DWT.b])
        k.dma("sp", dbr.t[:], d["conv_dw_b"], w=[dbr.b])
        k.tr(pB.t[:], dbr.t[:], c.ident_f.t[0:8, 0:8], r=[dbr.b], w=[pB.b])
        k.cp("dve", DWB.t[:], pB.t[:], r=[pB.b], w=[DWB.b])
        n_ = 0
        for cch in range(8):
            for j in range(31):
                k.ts("dve" if n_ % 2 else "pool", DG.t[:, cch, j, :], c.ident_f.t[:], DWT.t[:, cch, j:j + 1], ALU.mult, r=[DWT.b, c.ident_f.b], wp=[DG.b])
                n_ += 1
        GTw = sb(ph, nc, [128, 8, 544], BF16, n=2, name="gtw")
        Ys = sb(ph, nc, [128, 8, 512], F32, n=2, name="ys")
        pC = ps(ph, nc, [128, 512], F32, n=4, name="pC")
        dYD = Buf("dYD")
        for g in range(NG):
            gtw, ys = GTw[g % 2], Ys[g % 2]
            k.dma("sp", gtw.t[:], d["GT"][:, :, g * 512:g * 512 + 544], w=[gtw.b])
            for cch in range(8):
                pc = pC[cch % 4]
                for j in range(31):
                    k.mm(pc.t[:], DG.t[:, cch, j, :], gtw.t[:, cch, j + 1:j + 513], j == 0, j == 30, r=[DG.b, gtw.b],
                         w=[pc.b] if j == 0 else (), wp=() if j == 0 else [pc.b])
                k.act(ys.t[:, cch, :], pc.t[:], AF.Identity, r=[pc.b, DWB.b], wp=[ys.b], bias=DWB.t[:, cch:cch + 1])
            k.dma("sp", d["YD"][:, :, g * 512:(g + 1) * 512], ys.t[:], r=[ys.b], wp=[dYD])
        P.end_phase()


I32 = mybir.dt.int32


def pool_dma_op(P, fn, reads=(), writes=(), wpart=(), key=None):
    o = Op("pool", fn, P.phase)
    o.is_dma = True
    if key is None:
        key = (list(writes) + list(wpart))[0]
    if key not in P.keymap:
        P.keymap[key] = len(P.keymap)
        assert len(P.keymap) <= NDSEM
    o.key = P.keymap[key]
    P._deps(o, reads, writes, wpart)
    P.ops["pool"].append(o)
    P.order.append(o)
    return o


def phase_route(c, layer, TEi):
    nc, P, k, d, cfg = c.nc, c.P, c.k, c.d, c.cfg
    S, E, NT, NTILE, NSLOT = cfg.S, cfg.E, cfg.NT, cfg.NTILE, cfg.NSLOT
    with ExitStack() as ph:
        MKf = sb(ph, nc, [128, NT, E], F32, name="mkf")
        MKb = sb(ph, nc, [128, NT, E], BF16, name="mkb")
        CMB = sb(ph, nc, [128, NT, E], F32, name="cmb")
        UTf = sb(ph, nc, [128, 128], F32, name="utf")
        UT = sb(ph, nc, [128, 128], BF16, name="ut")
        ONb = sb(ph, nc, [128, 128], BF16, name="onb")
        IOTA = sb(ph, nc, [128, 1], F32, name="iota")
        TH = sb(ph, nc, [1, E * 16], F32, name="th")
        J5 = sb(ph, nc, [1, NTILE * E], F32, name="j5")
        dSLOT, dInit = Buf("dSLOT"), Buf("dInit")
        k.dma("sp", d["SLOT"], d["k_slotinit"], w=[dSLOT, dInit])
        k.dma("sp", MKf.t[:], d["MK"].rearrange("(t p) e -> p t e", p=128), w=[MKf.b])
        k.dma("sp", CMB.t[:], d["COMB"].rearrange("(t p) e -> p t e", p=128), w=[CMB.b])
        k.dma("sp", UTf.t[:], d["k_ut"], w=[UTf.b])
        k.dma("sp", IOTA.t[:], d["k_iota"], w=[IOTA.b])
        k.dma("sp", TH.t[:], d["k_th"], w=[TH.b])
        k.dma("sp", J5.t[:], d["k_j512"], w=[J5.b])
        k.cp("dve", UT.t[:], UTf.t[:], r=[UTf.b], w=[UT.b])
        k.cp("pool", MKb.t[:], MKf.t[:], r=[MKf.b], w=[MKb.b])
        k.memset("dve", ONb.t[:], 1.0, w=[ONb.b])
        pC = ps(ph, nc, [1, E], F32, name="pC")
        pSB = ps(ph, nc, [128, E], F32, name="pSB")
        pR = ps(ph, nc, [128, E], F32, n=2, name="pR")
        V = sb(ph, nc, [1, 8 * E], F32, name="v")
        C16 = sb(ph, nc, [1, E * 16], F32, name="c16")
        CJ = sb(ph, nc, [1, NTILE * E], F32, name="cj")
        TEf = sb(ph, nc, [1, NTILE], F32, name="tef")
        SEGB = sb(ph, nc, [128, E], F32, name="segb")
        for t in range(NT):
            k.mm(pC.t[:], ONb.t[:, 0:1], MKb.t[:, t, :], t == 0, t == NT - 1, r=[ONb.b, MKb.b], w=[pC.b] if t == 0 else (), wp=() if t == 0 else [pC.b])
        cnt, ntl, c512, inc, segs, one = (V.t[:, i * E:(i + 1) * E] for i in range(6))
        k.cp("dve", cnt, pC.t[:], r=[pC.b], wp=[V.b])
        k.tt("dve", C16.t[:].rearrange("o (e m) -> o e m", m=16), cnt.unsqueeze(2).to_broadcast([1, E, 16]), TH.t[:].rearrange("o (e m) -> o e m", m=16), ALU.is_gt,
             r=[V.b, TH.b], w=[C16.b])
        k.red(ntl, C16.t[:].rearrange("o (e m) -> o e m", m=16), r=[C16.b], wp=[V.b])
        k.ts("dve", c512, ntl, 512.0, ALU.mult, r=[V.b], wp=[V.b])
        k.memset("dve", one, 1.0, wp=[V.b])
        P.op("dve", (lambda e_, o=inc, a=one, b_=c512: e_.tensor_tensor_scan(out=o, data0=a, data1=b_, initial=0.0, op0=ALU.mult, op1=ALU.add)),
             reads=[V.b], wpart=[V.b])
        k.tt("dve", segs, inc, c512, ALU.subtract, r=[V.b], wp=[V.b])
        k.tt("dve", CJ.t[:].rearrange("o (j e) -> o j e", e=E), segs.unsqueeze(1).to_broadcast([1, NTILE, E]), J5.t[:].rearrange("o (j e) -> o j e", e=E), ALU.is_le,
             r=[V.b, J5.b], w=[CJ.b])
        k.red(TEf.t[:], CJ.t[:].rearrange("o (j e) -> o j e", e=E), r=[CJ.b], w=[TEf.b])
        k.ts("dve", TEf.t[:], TEf.t[:], -1.0, ALU.add, 0.0, ALU.max, r=[TEf.b], w=[TEf.b])
        IDXW, IDXB = TEi
        KP = sb(ph, nc, [128, 9], F32, name="kp")
        k.dma("sp", KP.t[:], d["k_kp"], w=[KP.b])
        pTE = ps(ph, nc, [128, NTILE], F32, name="pTE")
        TEb = sb(ph, nc, [128, NTILE], F32, name="teb")
        XW = sb(ph, nc, [128, NTILE, 8], F32, name="xw")
        k.mm(pTE.t[:], c.ones_f.t[0:1, :], TEf.t[:], True, True, r=[TEf.b, c.ones_f.b], w=[pTE.b])
        k.cp("dve", TEb.t[:], pTE.t[:], r=[pTE.b], w=[TEb.b])
        for kk in range(8):
            k.ts("dve", XW.t[:, :, kk], TEb.t[:], 1024.0, ALU.mult, KP.t[:, kk:kk + 1], ALU.add, r=[TEb.b, KP.b], wp=[XW.b])
        if layer:
            k.ts("dve", XW.t[:], XW.t[:], float(layer * E * 1024), ALU.add, r=[XW.b], w=[XW.b])
        k.cp("dve", IDXW.t[:], XW.t[:], r=[XW.b], w=[IDXW.b])
        k.ts("dve", TEb.t[:], TEb.t[:], 16.0, ALU.mult, KP.t[:, 8:9], ALU.add, r=[TEb.b, KP.b], w=[TEb.b])
        if layer:
            k.ts("dve", TEb.t[:], TEb.t[:], float(layer * E * 16), ALU.add, r=[TEb.b], w=[TEb.b])
        k.cp("dve", IDXB.t[:], TEb.t[:], r=[TEb.b], w=[IDXB.b])
        k.mm(pSB.t[:], c.ones_f.t[0:1, :], segs, True, True, r=[V.b, c.ones_f.b], w=[pSB.b])
        k.cp("dve", SEGB.t[:], pSB.t[:], r=[pSB.b], w=[SEGB.b])
        POS = sb(ph, nc, [128, E], F32, n=2, name="pos")
        T8 = sb(ph, nc, [128, 8], F32, n=2, name="t8")
        OH = sb(ph, nc, [128, E], F32, n=2, name="oh")
        JK = sb(ph, nc, [128, E], F32, n=2, name="jk")
        P4 = sb(ph, nc, [128, 4], F32, n=2, name="p4")
        P4i = sb(ph, nc, [128, 4], I32, n=2, name="p4i")
        SR = sb(ph, nc, [128, 4, 2], F32, n=2, name="sr")
        for i in range(NT):
            pr = pR[i % 2]
            pos, t8, p4, p4i, sr = POS[i % 2], T8[i % 2], P4[i % 2], P4i[i % 2], SR[i % 2]
            for ip in range(i):
                k.mm(pr.t[:], ONb.t[:], MKb.t[:, ip, :], ip == 0, False, r=[ONb.b, MKb.b], w=[pr.b] if ip == 0 else (), wp=() if ip == 0 else [pr.b])
            k.mm(pr.t[:], UT.t[:], MKb.t[:, i, :], i == 0, True, r=[UT.b, MKb.b], w=[pr.b] if i == 0 else (), wp=() if i == 0 else [pr.b])
            k.tt("dve", pos.t[:], pr.t[:], SEGB.t[:], ALU.add, r=[pr.b, SEGB.b], w=[pos.b])
            P.op("dve", (lambda e_, o=t8.t[:], a=CMB.t[:, i, :]: e_.max(out=o, in_=a)), reads=[CMB.b], writes=[t8.b])
            for kq in range(4):
                oh, jk = OH[kq % 2], JK[kq % 2]
                k.ts("dve", oh.t[:], CMB.t[:, i, :], t8.t[:, kq:kq + 1], ALU.is_equal, r=[CMB.b, t8.b], w=[oh.b])
                k.stt(jk.t[:], oh.t[:], 1.0, pos.t[:], ALU.mult, ALU.mult, r=[oh.b, pos.b], w=[jk.b], wp=[p4.b], accum=p4.t[:, kq:kq + 1])
                k.ts("pool", sr.t[:, kq, 0:1], IOTA.t[:], float(i * 128), ALU.add, r=[IOTA.b], wp=[sr.b])
                k.cp("pool", sr.t[:, kq, 1:2], t8.t[:, kq:kq + 1], r=[t8.b], wp=[sr.b])
            k.ts("dve", p4.t[:], p4.t[:], float(NSLOT - 1), ALU.min, r=[p4.b], w=[p4.b])
            k.cp("dve", p4i.t[:], p4.t[:], r=[p4.b], w=[p4i.b])
            for kq in range(4):
                def sca(e_, off=p4i.t[:, kq:kq + 1], src=sr.t[:, kq, :]):
                    return e_.indirect_dma_start(out=d["SLOT"], out_offset=bass.IndirectOffsetOnAxis(ap=off, axis=0), in_=src, in_offset=None)
                pool_dma_op(P, sca, reads=[p4i.b, sr.b, dInit], wpart=[dSLOT])
        P.end_phase()


def phase_smoe(c, layer, MOD5, TEi):
    nc, P, k, d, cfg = c.nc, c.P, c.k, c.d, c.cfg
    S, E, NTILE = cfg.S, cfg.E, cfg.NTILE
    IDXW, IDXB = TEi
    hin = d["H1"] if layer == 0 else d["H3"]
    hout = d["H2"] if layer == 0 else d["out"]
    w1tab = d["moe_w1"].rearrange("l e k n -> (l e k) n")
    w2tab = d["moe_w2"].rearrange("l e k n -> (l e k) n")
    b1tab = d["moe_b1"].rearrange("l r f -> (l r) f")
    with ExitStack() as ph:
        Z = sb(ph, nc, [128, 1024], BF16, name="z")
        W1 = sb(ph, nc, [128, 8, 2048], BF16, n=2, name="w1")
        W2 = sb(ph, nc, [128, 8, 1024], BF16, name="w2")
        B1r = sb(ph, nc, [128, 128], F32, n=2, name="b1r")
        B1c = sb(ph, nc, [128, 16], F32, n=2, name="b1c")
        SLt = sb(ph, nc, [128, 4, 2], F32, n=2, name="slt")
        TKi = sb(ph, nc, [128, 4], I32, n=2, name="tki")
        XG = sb(ph, nc, [128, 4, 1024], BF16, n=2, name="xg")
        XT = sb(ph, nc, [128, 8, 512], BF16, n=2, name="xt")
        ACTT = sb(ph, nc, [128, 8, 512], BF16, n=2, name="actt")
        G1 = sb(ph, nc, [128, 512], F32, n=2, name="g1")
        S1 = sb(ph, nc, [128, 512], F32, n=2, name="s1")
        L1 = sb(ph, nc, [128, 512], F32, n=2, name="l1")
        L2 = sb(ph, nc, [128, 512], F32, n=2, name="l2")
        GS = sb(ph, nc, [128, 512], F32, n=2, name="gs")
        OS = sb(ph, nc, [128, 1024], F32, n=4, name="os")
        HT = sb(ph, nc, [128, 1024], F32, n=2, name="ht")
        AT = sb(ph, nc, [128, 1024], F32, n=2, name="at")
        pT = ps(ph, nc, [128, 1024], BF16, n=2, name="pT")
        pG = ps(ph, nc, [128, 512], F32, n=2, name="pG")
        pL = ps(ph, nc, [128, 512], F32, n=2, name="pL")
        pO = ps(ph, nc, [128, 512], F32, n=2, name="pO")
        dACC, dXM2z, dOUT = Buf("dACCs"), Buf("dXM2z"), Buf("dOUT")
        k.memset("dve", Z.t[:], 0.0, w=[Z.b])
        k.dma("sp", d["XM2"][S:S + 128, :], Z.t[:], r=[Z.b], w=[dXM2z])

        def gather(out_ap, tab, idx_ap, reads, wslot, part=False):
            def g(e_):
                return e_.indirect_dma_start(out=out_ap, out_offset=None, in_=tab, in_offset=bass.IndirectOffsetOnAxis(ap=idx_ap, axis=0))
            return pool_dma_op(P, g, reads=reads, writes=() if part else [wslot], wpart=[wslot] if part else ())

        def loads(j):
            w1, b1r, slt, tki, xg = W1[j % 2], B1r[j % 2], SLt[j % 2], TKi[j % 2], XG[j % 2]
            k.dma("sp", slt.t[:], d["SLOT"][j * 512:(j + 1) * 512, :].rearrange("(s p) c -> p s c", p=128), w=[slt.b])
            k.cp("dve", tki.t[:], slt.t[:, :, 0], r=[slt.b], w=[tki.b])
            for kk in range(8):
                gather(w1.t[:, kk, :], w1tab, IDXW.t[:, j, kk:kk + 1], [IDXW.b], w1.b, part=True)
            gather(b1r.t[:], b1tab, IDXB.t[:, j:j + 1], [IDXB.b], b1r.b)
            for sub in range(4):
                gather(xg.t[:, sub, :], d["XM2"], tki.t[:, sub:sub + 1], [tki.b, dXM2z], xg.b, part=True)

        io = 0
        loads(0)
        for j in range(NTILE):
            if j + 1 < NTILE:
                loads(j + 1)
            w1, b1r, b1c, slt, tki, xg, xt, actt = (X[j % 2] for X in (W1, B1r, B1c, SLt, TKi, XG, XT, ACTT))
            k.tr(pG[0].t[:, 0:16], b1r.t[0:16, :], c.ident_f.t[0:16, 0:16], r=[b1r.b], w=[pG[0].b])
            k.cp("dve", b1c.t[:], pG[0].t[:, 0:16], r=[pG[0].b], w=[b1c.b])
            for sub in range(4):
                pt = pT[sub % 2]
                for kk in range(8):
                    k.tr(pt.t[:, kk * 128:(kk + 1) * 128], xg.t[:, sub, kk * 128:(kk + 1) * 128], c.ident_b.t[:], r=[xg.b],
                         w=[pt.b] if kk == 0 else (), wp=() if kk == 0 else [pt.b])
                k.cp("act" if sub % 2 else "dve", xt.t[:, :, sub * 128:(sub + 1) * 128], pt.t[:].rearrange("p (k t) -> p k t", k=8), r=[pt.b], wp=[xt.b])
            for pr in range(8):
                pg, pl = pG[pr % 2], pL[pr % 2]
                g1, s1, l1, l2, gs = (X[pr % 2] for X in (G1, S1, L1, L2, GS))
                for kk in range(8):
                    k.mm(pg.t[:], w1.t[:, kk, pr * 128:(pr + 1) * 128], xt.t[:, kk, :], kk == 0, kk == 7,
                         r=[w1.b, xt.b], w=[pg.b] if kk == 0 else (), wp=() if kk == 0 else [pg.b])
                for kk in range(8):
                    k.mm(pl.t[:], w1.t[:, kk, 1024 + pr * 128:1024 + (pr + 1) * 128], xt.t[:, kk, :], kk == 0, kk == 7,
                         r=[w1.b, xt.b], w=[pl.b] if kk == 0 else (), wp=() if kk == 0 else [pl.b])
                k.ts("dve", g1.t[:], pg.t[:], b1c.t[:, pr:pr + 1], ALU.add, 7.0, ALU.min, r=[pg.b, b1c.b], w=[g1.b])
                k.act(s1.t[:], g1.t[:], AF.Sigmoid, r=[g1.b], w=[s1.b], scale=1.702)
                k.act(l1.t[:], pl.t[:], AF.Identity, r=[pl.b, b1c.b], w=[l1.b], bias=b1c.t[:, 8 + pr:9 + pr])
                k.ts("dve", l2.t[:], l1.t[:], 7.0, ALU.min, -7.0, ALU.max, r=[l1.b], w=[l2.b])
                k.tt("dve", gs.t[:], g1.t[:], s1.t[:], ALU.mult, r=[g1.b, s1.b], w=[gs.b])
                k.stt(actt.t[:, pr, :], l2.t[:], 1.0, gs.t[:], ALU.add, ALU.mult, r=[l2.b, gs.b], wp=[actt.b])
            for kk in range(8):
                gather(W2.t[:, kk, :], w2tab, IDXW.t[:, j, kk:kk + 1], [IDXW.b], W2.b, part=True)
            for sub in range(4):
                os_ = OS[(4 * j + sub) % 4]
                for half in range(2):
                    po = pO[io % 2]
                    io += 1
                    for jj in range(8):
                        k.mm(po.t[:], actt.t[:, jj, sub * 128:(sub + 1) * 128], W2.t[:, jj, half * 512:(half + 1) * 512], jj == 0, jj == 7,
                             r=[actt.b, W2.b], w=[po.b] if jj == 0 else (), wp=() if jj == 0 else [po.b])
                    k.act(os_.t[:, half * 512:(half + 1) * 512], po.t[:], AF.Copy, r=[po.b, slt.b], wp=[os_.b], scale=slt.t[:, sub, 1:2])

                def sca(e_, off=tki.t[:, sub:sub + 1], src=os_.t[:]):
                    return e_.indirect_dma_start(out=d["ACCd"], out_offset=bass.IndirectOffsetOnAxis(ap=off, axis=0), in_=src, in_offset=None,
                                                 compute_op=ALU.add)
                pool_dma_op(P, sca, reads=[tki.b, os_.b], writes=[dACC])
        for t in range(S // 128):
            ht_, at_ = HT[t % 2], AT[t % 2]
            rows = slice(t * 128, (t + 1) * 128)
            k.dma("sp", ht_.t[:], hin[rows, :], w=[ht_.b])
            k.dma("sp", at_.t[:], d["ACCd"][rows, :], r=[dACC], w=[at_.b])
            k.tt("dve", at_.t[:], at_.t[:], MOD5.t[:], ALU.mult, r=[at_.b, MOD5.b], w=[at_.b])
            k.tt("pool", ht_.t[:], ht_.t[:], at_.t[:], ALU.add, r=[ht_.b, at_.b], w=[ht_.b])
            k.dma("sp", hout[rows, :], ht_.t[:], r=[ht_.b], wp=[dOUT])
        P.end_phase()
```

```python
from contextlib import ExitStack
import numpy as np
import ml_dtypes
import concourse.bass as bass
import concourse.mybir as mybir
from concourse.bass_utils import run_bass_kernel_spmd

F32 = mybir.dt.float32
BF16 = mybir.dt.bfloat16
AF = mybir.ActivationFunctionType
ALU = mybir.AluOpType
AX = mybir.AxisListType

COMPUTE = ("pe", "act", "dve", "pool")
ALLENG = ("pe", "act", "dve", "pool", "sp")
NDSEM = 72


class Buf:
    __slots__ = ("name", "writers", "readers")

    def __init__(self, name):
        self.name = name
        self.writers = []
        self.readers = []


class Op:
    __slots__ = ("eng", "fn", "raw", "oth", "signal", "tok_sem", "tok_val", "is_dma", "key", "phase")

    def __init__(self, eng, fn, phase):
        self.eng = eng
        self.fn = fn
        self.raw = []
        self.oth = []
        self.signal = False
        self.tok_sem = None
        self.tok_val = 0
        self.is_dma = False
        self.key = None
        self.phase = phase


class Prog:
    def __init__(self, nc, es):
        self.nc = nc
        self.phase = 0
        self.esem = {e: es.enter_context(nc.semaphore("s_" + e)) for e in COMPUTE}
        self.ecnt = {e: 0 for e in COMPUTE}
        self.dsem = [es.enter_context(nc.semaphore("d%d" % i)) for i in range(NDSEM)]
        self.dcnt = [0] * NDSEM
        self.seen = {e: {} for e in ALLENG}
        self._reset()
        self.nops = 0

    def _reset(self):
        self.ops = {e: [] for e in ALLENG}
        self.order = []
        self.keymap = {}
        self.last = {}

    def buf(self, name="b"):
        return Buf(name)

    def _deps(self, op, reads, writes, wpart):
        ph = self.phase
        for b in reads:
            for w in b.writers:
                if w.phase == ph:
                    op.raw.append(w)
        for b in list(writes) + list(wpart):
            for r in b.readers:
                if r.phase == ph:
                    op.oth.append(r)
        for b in writes:
            for w in b.writers:
                if w.phase == ph:
                    op.oth.append(w)
        for b in reads:
            b.readers.append(op)
        for b in writes:
            b.writers = [op]
            b.readers = []
        for b in wpart:
            if b.readers:
                b.writers = [op]
                b.readers = []
            else:
                b.writers.append(op)

    def op(self, eng, fn, reads=(), writes=(), wpart=()):
        o = Op(eng, fn, self.phase)
        self._deps(o, reads, writes, wpart)
        self.ops[eng].append(o)
        self.order.append(o)
        self.last[eng] = o
        return o

    def dma(self, q, out, in_, reads=(), writes=(), wpart=(), key=None, **kw):
        def fn(e, out=out, in_=in_, kw=kw):
            return e.dma_start(out=out, in_=in_, **kw)
        o = Op(q, fn, self.phase)
        o.is_dma = True
        if key is None:
            ws = list(writes) + list(wpart)
            key = ws[0]
        if key not in self.keymap:
            self.keymap[key] = len(self.keymap)
            assert len(self.keymap) <= NDSEM, "too many DMA keys in phase"
        o.key = self.keymap[key]
        self._deps(o, reads, writes, wpart)
        self.ops[q].append(o)
        self.order.append(o)
        return o

    def end_phase(self):
        nc = self.nc
        lasts = [self.last[e] for e in COMPUTE if e in self.last]
        lastd = {}
        for o in self.order:
            if o.is_dma:
                lastd[o.key] = o
        for e in ALLENG:
            o = Op(e, (lambda eng: eng.nop()), self.phase)
            o.raw = list(lasts) + list(lastd.values())
            self.ops[e].append(o)
            self.order.append(o)
        for o in self.order:
            for d in o.raw:
                if d.is_dma:
                    continue
                if d.eng == o.eng and o.eng == "pe" and not o.is_dma:
                    continue
                d.signal = True
            for d in o.oth:
                if d.is_dma:
                    continue
                if d.eng == o.eng and not o.is_dma:
                    continue
                d.signal = True
        for e in COMPUTE:
            for o in self.ops[e]:
                if o.is_dma:
                    continue
                if o.signal:
                    self.ecnt[e] += 1
                    o.tok_sem = self.esem[e]
                    o.tok_val = self.ecnt[e]
        for o in self.order:
            if o.is_dma:
                self.dcnt[o.key] += 16
                o.tok_sem = self.dsem[o.key]
                o.tok_val = self.dcnt[o.key]
        self.nops += len(self.order)

        with nc.Block() as block:
            def run(ename):
                def body(eng):
                    seen = self.seen[ename]
                    for o in self.ops[ename]:
                        need = {}
                        for d in o.raw:
                            if d.tok_sem is None:
                                continue
                            if (not d.is_dma) and d.eng == ename and ename == "pe" and not o.is_dma:
                                continue
                            s = d.tok_sem
                            if need.get(s.num, (None, 0))[1] < d.tok_val:
                                need[s.num] = (s, d.tok_val)
                        for d in o.oth:
                            if d.tok_sem is None:
                                continue
                            if (not d.is_dma) and d.eng == ename and not o.is_dma:
                                continue
                            s = d.tok_sem
                            if need.get(s.num, (None, 0))[1] < d.tok_val:
                                need[s.num] = (s, d.tok_val)
                        for s, v in need.values():
                            if seen.get(s.num, 0) < v:
                                eng.wait_ge(s, v)
                                seen[s.num] = v
                        ins = o.fn(eng)
                        if o.is_dma:
                            ins.then_inc(o.tok_sem, 16)
                        elif o.signal:
                            ins.then_inc(o.tok_sem, 1)
                return body

            block.tensor(run("pe"))
            block.scalar(run("act"))
            block.vector(run("dve"))
            block.gpsimd(run("pool"))
            block.sync(run("sp"))
        self.phase += 1
        self._reset()


class Slot:
    __slots__ = ("t", "b")

    def __init__(self, t, b):
        self.t = t
        self.b = b


class Ctx:
    pass


class K:
    def __init__(self, P):
        self.P = P

    def ts(self, eng, out, in0, s1, op0, s2=None, op1=None, r=(), w=(), wp=(), accum=None):
        if op1 is None:
            if accum is None:
                f = lambda e: e.tensor_scalar(out=out, in0=in0, scalar1=s1, scalar2=None, op0=op0)
            else:
                f = lambda e: e.tensor_scalar(out=out, in0=in0, scalar1=s1, scalar2=None, op0=op0, accum_out=accum)
        else:
            f = lambda e: e.tensor_scalar(out=out, in0=in0, scalar1=s1, scalar2=s2, op0=op0, op1=op1)
        return self.P.op(eng, f, reads=r, writes=w, wpart=wp)

    def tt(self, eng, out, in0, in1, op, r=(), w=(), wp=()):
        return self.P.op(eng, lambda e: e.tensor_tensor(out=out, in0=in0, in1=in1, op=op), reads=r, writes=w, wpart=wp)

    def stt(self, out, in0, scalar, in1, op0, op1, r=(), w=(), wp=(), accum=None):
        if accum is None:
            f = lambda e: e.scalar_tensor_tensor(out=out, in0=in0, scalar=scalar, in1=in1, op0=op0, op1=op1)
        else:
            f = lambda e: e.scalar_tensor_tensor(out=out, in0=in0, scalar=scalar, in1=in1, op0=op0, op1=op1, accum_out=accum)
        return self.P.op("dve", f, reads=r, writes=w, wpart=wp)

    def act(self, out, in_, func, r=(), w=(), wp=(), bias=None, scale=None, accum=None):
        kw = {}
        if bias is not None:
            kw["bias"] = bias
        if scale is not None:
            kw["scale"] = scale
        if accum is not None:
            kw["accum_out"] = accum
        return self.P.op("act", lambda e: e.activation(out=out, in_=in_, func=func, **kw), reads=r, writes=w, wpart=wp)

    def cp(self, eng, out, in_, r=(), w=(), wp=()):
        if eng == "act":
            return self.P.op("act", lambda e: e.copy(out=out, in_=in_), reads=r, writes=w, wpart=wp)
        return self.P.op(eng, lambda e: e.tensor_copy(out=out, in_=in_), reads=r, writes=w, wpart=wp)

    def memset(self, eng, ap, val, w=(), wp=()):
        return self.P.op(eng, lambda e: e.memset(ap, val), writes=w, wpart=wp)

    def mm(self, out, lhsT, rhs, start, stop, r=(), w=(), wp=()):
        return self.P.op("pe", lambda e: e.matmul(out, lhsT, rhs, start=start, stop=stop), reads=r, writes=w, wpart=wp)

    def tr(self, out, in_, ident, r=(), w=(), wp=()):
        return self.P.op("pe", lambda e: e.transpose(out, in_, ident), reads=r, writes=w, wpart=wp)

    def red(self, out, in_, r=(), w=(), wp=()):
        return self.P.op("dve", lambda e: e.tensor_reduce(out=out, in_=in_, axis=AX.X, op=ALU.add), reads=r, writes=w, wpart=wp)

    def recip(self, out, in_, r=(), w=(), wp=()):
        return self.P.op("dve", lambda e: e.reciprocal(out=out, in_=in_), reads=r, writes=w, wpart=wp)

    def dma(self, q, out, in_, r=(), w=(), wp=(), key=None):
        return self.P.dma(q, out, in_, reads=r, writes=w, wpart=wp, key=key)


class Cfg:
    def __init__(self, S=8192, L=256, E=32, debug=False, stop_after=None):
        self.S, self.L, self.E = S, L, E
        self.D = 1024
        self.NT = S // 128
        self.ROWS = S // 64
        self.NCH = S // 64
        self.MB = min(1024, S)
        self.NTILE = (4 * S) // 512 + E
        self.NSLOT = self.NTILE * 512
        self.debug = debug
        self.dense = False
        self.stop_after = stop_after


EPS = 1e-6
NEG = -30000.0


def host_consts(cfg):
    S = cfg.S
    NT = cfg.NT
    cs = {}
    cs["ident_f"] = np.eye(128, dtype=np.float32)
    p = np.arange(128)
    t = np.arange(64)
    s = p % 64
    cs["trif"] = (s[:, None] <= t[None, :]).astype(np.float32)
    cs["trib"] = (s[:, None] >= t[None, :]).astype(np.float32)
    r = np.ones((128, 512), np.float32)
    r[:, ::64] = 0.0
    cs["reset"] = r
    inv = (10000.0 ** (-np.arange(16, dtype=np.float32) / 16.0)).astype(np.float32)
    tt = np.arange(NT)
    row = (2 * tt[None, :] + (p[:, None] // 64)).astype(np.float32)
    col = np.broadcast_to((p % 64).astype(np.float32)[:, None], (128, NT))
    ang = np.stack([row[:, :, None] * inv[None, None, :], col[:, :, None] * inv[None, None, :]], axis=2)
    ang = ang.astype(np.float32)
    E, NTILE = cfg.E, cfg.NTILE
    cs["ut"] = (p[:, None] < p[None, :]).astype(np.float32)
    cs["iota"] = p.astype(np.float32).reshape(128, 1)
    kp = np.zeros((128, 9), np.float32)
    kp[:, :8] = np.arange(8)[None, :] * 128 + p[:, None]
    kp[:, 8] = p % 16
    cs["kp"] = kp
    cs["th"] = np.broadcast_to((512.0 * np.arange(16, dtype=np.float32))[None, None, :], (1, E, 16)).reshape(1, E * 16).copy()
    cs["j512"] = np.broadcast_to((512.0 * np.arange(NTILE, dtype=np.float32))[None, :, None], (1, NTILE, E)).reshape(1, NTILE * E).copy()
    si = np.zeros((cfg.NSLOT, 2), np.float32)
    si[:, 0] = S + (np.arange(cfg.NSLOT) % 128)
    cs["slotinit"] = si
    cs["cos"] = np.cos(ang).astype(np.float32).reshape(128, NT * 32)
    cs["sin"] = np.sin(ang).astype(np.float32).reshape(128, NT * 32)
    return cs


def layout_rpb(rpb):
    H = rpb.shape[0]
    c = np.arange(64)[:, None]
    kc = np.arange(64)[None, :]
    win = np.clip(c - 8, 0, 48)
    valid = (kc >= win) & (kc < win + 16)
    idx = np.clip(kc - c + 15, 0, 30)
    g = rpb[:, :, idx]
    g = np.where(valid[None, None], g, np.float32(NEG)).astype(np.float32)
    return np.ascontiguousarray(g.transpose(2, 0, 1, 3)).reshape(64, H * 15 * 64)


_uid = [0]


def sb(es, nc, shape, dt, n=1, name="t"):
    out = []
    for i in range(n):
        _uid[0] += 1
        t = es.enter_context(nc.sbuf_tensor("%s_%d" % (name, _uid[0]), list(shape), dt))
        out.append(Slot(t, Buf(name)))
    return out if n > 1 else out[0]


def ps(es, nc, shape, dt, n=1, name="p"):
    out = []
    for i in range(n):
        _uid[0] += 1
        t = es.enter_context(nc.psum_tensor("%s_%d" % (name, _uid[0]), list(shape), dt))
        out.append(Slot(t, Buf(name)))
    return out if n > 1 else out[0]


def declare_io(nc, cfg):
    S, L, E = cfg.S, cfg.L, cfg.E
    d = {}

    inputs = set()
    d["_inputs"] = inputs

    def inp(name, shape, dt=F32):
        d[name] = nc.dram_tensor(name, list(shape), dt, kind="ExternalInput").ap()
        inputs.add(name)

    inp("x", [S, 1024]); inp("c", [8, 128]); inp("ctx", [L, 1024]); inp("c_ctx", [8, 128])
    inp("ada_w", [2, 1024, 6144]); inp("ada_b", [2, 6144]); inp("norm1_g", [2, 1024]); inp("norm2_g", [2, 1024])
    inp("ab_w_in", [1024, 4096]); inp("ab_w_out", [1024, 1024]); inp("nat_q_norm", [1, 64]); inp("nat_k_norm", [1, 64])
    inp("rpb_full", [64, 8 * 15 * 64]); inp("hgrn_lb", [16, 128]); inp("hgrn_o_norm", [1, 128])
    inp("conv_w1", [1024, 2048]); inp("conv_b1", [16, 128]); inp("conv_dw", [31, 1024]); inp("conv_dw_b", [8, 128])
    inp("conv_ln_g", [8, 128]); inp("conv_ln_b", [8, 128]); inp("conv_w2", [1024, 1024]); inp("conv_b2", [1, 1024])
    inp("router_w", [2, 1024, E]); inp("router_b", [2, E])
    inp("moe_w1", [2, E, 1024, 2048]); inp("moe_b1", [2, E * 16, 128]); inp("moe_w2", [2, E, 1024, 1024]); inp("moe_b2", [2, E, 1024])
    inp("k_ident_f", [128, 128]); inp("k_trif", [128, 64]); inp("k_trib", [128, 64]); inp("k_reset", [128, 512])
    inp("k_cos", [128, cfg.NT * 32]); inp("k_sin", [128, cfg.NT * 32])
    inp("k_ut", [128, 128]); inp("k_iota", [128, 1]); inp("k_th", [1, E * 16]); inp("k_j512", [1, cfg.NTILE * E])
    inp("k_slotinit", [cfg.NSLOT, 2]); inp("k_kp", [128, 9])
    d["out"] = nc.dram_tensor("out", [S, 1024], F32, kind="ExternalOutput").ap()
    kind = "ExternalOutput" if cfg.debug else "Internal"

    def scr(name, shape, dt):
        d[name] = nc.dram_tensor(name, list(shape), dt, kind=kind).ap()

    scr("XT", [128, 8, S], BF16); scr("XTc", [128, 8, L], BF16)
    scr("QTr", [128, 4, S], BF16); scr("QTf", [128, 4, S], BF16); scr("KTr", [128, 4, S], BF16)
    scr("KcT", [128, 4, L], BF16)
    scr("VA", [S, 520], BF16); scr("VcA", [L, 520], BF16)
    scr("VH", [S, 512], BF16); scr("VHc", [L, 512], BF16)
    scr("G", [S, 512], F32)
    scr("HQ", [2, 128, 4, S], BF16); scr("HK", [2, 128, 4, S], BF16)
    scr("KH", [2, S, 512], BF16); scr("KHc", [2, L, 512], BF16)
    scr("DEC", [2, 128, 4, S // 64], F32); scr("DECc", [2, 128, 4, L // 64], F32)
    scr("OF", [S, 512], F32)
    scr("CAT", [S, 1024], BF16)
    scr("H1", [S, 1024], F32); scr("H2", [S, 1024], F32); scr("H3", [S, 1024], F32)
    scr("XT2", [128, 8, S], BF16)
    scr("COMB", [S, E], F32)
    scr("ACC0", [S, 1024], F32)
    scr("GT", [128, 8, S + 32], BF16)
    scr("YD", [128, 8, S], F32)
    scr("XM2", [S + 128, 1024], BF16)
    scr("MK", [S, E], F32)
    scr("ACCd", [S + 128, 1024], F32)
    scr("SLOT", [cfg.NSLOT, 2], F32)
    return d


def build_program(cfg):
    nc = bass.Bass("TRN2", target_bir_lowering=False, dynamic_dma_scratch_size=32768)
    c = Ctx()
    c.nc, c.cfg = nc, cfg
    c.d = declare_io(nc, cfg)
    c.inputs = c.d.pop("_inputs")
    with ExitStack() as ges:
        P = Prog(nc, ges)
        c.P = P
        c.k = K(P)
        c.ident_f = sb(ges, nc, [128, 128], F32, name="identf")
        c.ident_b = sb(ges, nc, [128, 128], BF16, name="identb")
        c.ones_f = sb(ges, nc, [128, 128], F32, name="onesf")
        c.k.dma("sp", c.ident_f.t[:], c.d["k_ident_f"], w=[c.ident_f.b])
        c.k.cp("dve", c.ident_b.t[:], c.ident_f.t[:], r=[c.ident_f.b], w=[c.ident_b.b])
        c.k.memset("dve", c.ones_f.t[:], 1.0, w=[c.ones_f.b])
        touch = sb(ges, nc, [1, 64], F32, name="touch")
        for i, nm in enumerate(sorted(c.inputs)):
            ap = c.d[nm]
            idx = tuple([0] * (len(ap.shape) - 2) + [slice(0, 1), slice(0, 1)])
            c.k.dma("sp", touch.t[0:1, i:i + 1], ap[idx], wp=[touch.b])
        if cfg.debug:
            tb = sb(ges, nc, [1, 2], BF16, name="touchb")
            c.k.memset("dve", touch.t[0:1, 62:64], 0.0, wp=[touch.b])
            c.k.memset("dve", tb.t[:], 0.0, w=[tb.b])
            for i, (nm, ap) in enumerate(c.d.items()):
                if nm not in c.inputs:
                    idx = tuple([0] * (len(ap.shape) - 2) + [slice(0, 1), slice(0, 1)])
                    src = tb.t[0:1, 0:1] if ap.dtype == BF16 else touch.t[0:1, 63:64]
                    c.k.dma("sp", ap[idx], src, r=[tb.b, touch.b], w=[Buf("o")])
        P.end_phase()
        def dbg_stop(name):
            return cfg.stop_after == name

        done = False
        for layer in (0, 1):
            with ExitStack() as lay:
                c.MOD = sb(lay, nc, [128, 6144], F32, name="MOD")
                if layer == 0:
                    c.CMOD = sb(lay, nc, [128, 2048], F32, name="CMOD")
                    seq = [("mods0", lambda: phase_mods(c, 0)), ("a1", lambda: phase_a1(c)), ("a2", lambda: phase_a2(c)),
                           ("nat", lambda: phase_nat(c)), ("hgrn", lambda: phase_hgrn(c)), ("post0", lambda: phase_post(c, 0))]
                else:
                    seq = [("mods1", lambda: phase_mods(c, 1)), ("f", lambda: phase_f(c)), ("g1", lambda: phase_g1(c)),
                           ("post1", lambda: phase_post(c, 1))]
                for name, fn in seq:
                    fn()
                    if dbg_stop(name):
                        done = True
                        break
            if done:
                break
            with ExitStack() as m5:
                MOD5 = sb(m5, nc, [128, 1024], F32, name="MOD5")
                with ExitStack() as ph:
                    emit_mods(c, ph, layer, [10, 11], lambda ct: MOD5.t[:, (ct - 10) * 512:(ct - 9) * 512], MOD5.b, False)
                    P.end_phase()
                if cfg.dense:
                    phase_moe(c, layer, MOD5)
                else:
                    TEi = (sb(m5, nc, [128, cfg.NTILE, 8], mybir.dt.int32, name="IDXW"), sb(m5, nc, [128, cfg.NTILE], mybir.dt.int32, name="IDXB"))
                    phase_route(c, layer, TEi)
                    if dbg_stop("route%d" % layer):
                        break
                    phase_smoe(c, layer, MOD5, TEi)
            if dbg_stop("moe%d" % layer):
                break
    return nc, c


def emit_mods(c, ph, layer, cts, dst_fn, dst_buf, with_ctx):
    nc, P, k, d = c.nc, c.P, c.k, c.d
    crow = sb(ph, nc, [16, 128], F32, name="crow")
    crow2 = sb(ph, nc, [16, 128], F32, name="crow2")
    ccol = sb(ph, nc, [128, 16], F32, name="ccol")
    CB = sb(ph, nc, [128, 16, 128], F32, name="CB")
    AW = sb(ph, nc, [128, 8, 512], F32, n=2, name="AW")
    ABr = sb(ph, nc, [1, 512], F32, n=2, name="ABr")
    pT = ps(ph, nc, [128, 16], F32, name="pT")
    pM = ps(ph, nc, [128, 512], F32, n=2, name="pM")
    pC = ps(ph, nc, [128, 512], F32, n=2, name="pC")
    k.dma("sp", crow.t[0:8, :], d["c"], wp=[crow.b])
    k.dma("sp", crow.t[8:16, :], d["c_ctx"], wp=[crow.b])
    k.act(crow2.t[:], crow.t[:], AF.Silu, r=[crow.b], w=[crow2.b])
    k.tr(pT.t[:], crow2.t[:], c.ident_f.t[0:16, 0:16], r=[crow2.b], w=[pT.b])
    k.cp("dve", ccol.t[:], pT.t[:], r=[pT.b], w=[ccol.b])
    for j in range(16):
        k.cp("dve" if j % 2 else "pool", CB.t[:, j, :], ccol.t[:, j:j + 1].to_broadcast([128, 128]), r=[ccol.b], wp=[CB.b])
    awv = d["ada_w"][layer].rearrange("(k p) n -> p k n", p=128)
    for i, ct in enumerate(cts):
        aw, ab = AW[i % 2], ABr[i % 2]
        k.dma("sp", aw.t[:], awv[:, :, ct * 512:(ct + 1) * 512], w=[aw.b])
        k.dma("sp", ab.t[:], d["ada_b"][layer:layer + 1, ct * 512:(ct + 1) * 512], w=[ab.b])
        pm = pM[i % 2]
        for kk in range(8):
            k.mm(pm.t[:], CB.t[:, kk, :], aw.t[:, kk, :], kk == 0, False, r=[CB.b, aw.b], w=[pm.b] if kk == 0 else (), wp=() if kk == 0 else [pm.b])
        k.mm(pm.t[:], c.ones_f.t[0:1, :], ab.t[:], False, True, r=[ab.b], wp=[pm.b])
        k.cp("act", dst_fn(ct), pm.t[:], r=[pm.b], wp=[dst_buf])
        if with_ctx and ct < 4:
            pc = pC[i % 2]
            for kk in range(8):
                k.mm(pc.t[:], CB.t[:, 8 + kk, :], aw.t[:, kk, :], kk == 0, False, r=[CB.b, aw.b], w=[pc.b] if kk == 0 else (), wp=() if kk == 0 else [pc.b])
            k.mm(pc.t[:], c.ones_f.t[0:1, :], ab.t[:], False, True, r=[ab.b], wp=[pc.b])
            k.cp("dve", c.CMOD.t[:, ct * 512:(ct + 1) * 512], pc.t[:], r=[pc.b], wp=[c.CMOD.b])


def phase_mods(c, layer):
    with ExitStack() as ph:
        emit_mods(c, ph, layer, list(range(10)), lambda ct: c.MOD.t[:, ct * 512:(ct + 1) * 512], c.MOD.b, layer == 0)
        c.P.end_phase()


def make_A(c, ph, gname, layer, modcols, modt):
    nc, k, d = c.nc, c.k, c.d
    g = sb(ph, nc, [128, 1024], F32, name="gbc")
    A = sb(ph, nc, [128, 1024], F32, name="A")
    k.dma("sp", g.t[:], d[gname][layer:layer + 1, :].partition_broadcast(128), w=[g.b])
    k.stt(A.t[:], modt.t[:, modcols:modcols + 1024], 1.0, g.t[:], ALU.add, ALU.mult, r=[modt.b, g.b], w=[A.b])
    return A


def norm_mod(c, xt, A, SH, shb, outs, tmp, j):
    k = c.k
    ss, t1 = tmp["ss"], tmp["t1"]
    k.stt(t1.t[:], xt.t[:], 1.0, xt.t[:], ALU.mult, ALU.mult, r=[xt.b], w=[t1.b], wp=[ss.b], accum=ss.t[:, 4 * j:4 * j + 1])
    k.ts("dve", ss.t[:, 4 * j + 1:4 * j + 2], ss.t[:, 4 * j:4 * j + 1], 1.0 / 1024, ALU.mult, EPS, ALU.add, r=[ss.b], wp=[ss.b])
    k.act(ss.t[:, 4 * j + 2:4 * j + 3], ss.t[:, 4 * j + 1:4 * j + 2], AF.Sqrt, r=[ss.b], wp=[ss.b])
    k.recip(ss.t[:, 4 * j + 3:4 * j + 4], ss.t[:, 4 * j + 2:4 * j + 3], r=[ss.b], wp=[ss.b])
    k.stt(t1.t[:], xt.t[:], ss.t[:, 4 * j + 3:4 * j + 4], A.t[:], ALU.mult, ALU.mult, r=[xt.b, ss.b, A.b], w=[t1.b])
    for (ap, eng, slot) in outs:
        k.tt(eng, ap, t1.t[:], SH, ALU.add, r=[t1.b, shb], wp=[slot.b])


def phase_a1(c):
    nc, P, k, d, cfg = c.nc, c.P, c.k, c.d, c.cfg
    S, L, NT = cfg.S, cfg.L, cfg.NT
    with ExitStack() as ph:
        W = sb(ph, nc, [128, 8, 2560], BF16, name="Wtok")
        wv = d["ab_w_in"].rearrange("(k p) n -> p k n", p=128)
        for i, c0 in enumerate((0, 512, 1024, 3072, 3584)):
            k.dma("pool", W.t[:, :, i * 512:(i + 1) * 512], wv[:, :, c0:c0 + 512], wp=[W.b])
        A = make_A(c, ph, "norm1_g", 0, 1024, c.MOD)
        Ac = make_A(c, ph, "norm1_g", 0, 1024, c.CMOD)
        g64 = sb(ph, nc, [128, 128], F32, name="g64")
        GQ = sb(ph, nc, [128, 512], F32, name="GQ")
        GK = sb(ph, nc, [128, 512], F32, name="GK")
        k.dma("sp", g64.t[:, 0:64], d["nat_q_norm"].partition_broadcast(128), wp=[g64.b])
        k.dma("sp", g64.t[:, 64:128], d["nat_k_norm"].partition_broadcast(128), wp=[g64.b])
        k.ts("dve", GQ.t[:].rearrange("p (h e) -> p h e", h=8), g64.t[:, 0:64].unsqueeze(1).to_broadcast([128, 8, 64]), 0.125, ALU.mult, r=[g64.b], w=[GQ.b])
        k.ts("dve", GK.t[:].rearrange("p (h e) -> p h e", h=8), g64.t[:, 64:128].unsqueeze(1).to_broadcast([128, 8, 64]), 1.0, ALU.mult, r=[g64.b], w=[GK.b])
        COSG = sb(ph, nc, [128, 128], F32, n=2, name="COS")
        SING = sb(ph, nc, [128, 128], F32, n=2, name="SIN")
        XIN = sb(ph, nc, [128, 1024], F32, n=2, name="xin")
        tmps = [{"ss": sb(ph, nc, [128, 16], F32, name="ss"), "t1": sb(ph, nc, [128, 1024], F32, name="t1")} for _ in range(2)]
        XM = sb(ph, nc, [128, 1024], BF16, n=2, name="xm")
        XTG = sb(ph, nc, [128, 8, 512], BF16, n=2, name="xtg")
        pT = ps(ph, nc, [128, 1024], BF16, name="pT")
        pS = ps(ph, nc, [128, 512], F32, n=5, name="pS")
        pO = ps(ph, nc, [128, 1024], BF16, n=2, name="pO")
        SQs = [sb(ph, nc, [128, 1024], F32, name="sq")] * 2
        STs = sb(ph, nc, [128, 48], F32, n=2, name="st")
        QNs = [sb(ph, nc, [128, 1024], F32, name="qn")] * 2
        R1s = [sb(ph, nc, [128, 1024], F32, name="r1")] * 2
        R2s = [sb(ph, nc, [128, 1024], F32, name="r2")] * 2
        OB = sb(ph, nc, [128, 1536], BF16, n=2, name="ob")
        OTG = sb(ph, nc, [128, 3, 4, 512], BF16, n=2, name="otg")
        VAs = sb(ph, nc, [128, 8, 65], BF16, n=2, name="vas")
        VHs = sb(ph, nc, [128, 512], BF16, n=2, name="vhs")
        Gs = sb(ph, nc, [128, 512], F32, n=2, name="gs")
        for v in VAs:
            k.memset("pool", v.t[:, :, 64:65], 1.0, wp=[v.b])
        dXT, dXTc = Buf("dXT"), Buf("dXTc")
        dQ, dV, dVH, dG = Buf("dQ"), Buf("dV"), Buf("dVH"), Buf("dG")

        def run(src, ntile, Asl, modt, is_ctx):
            ngrp = (ntile + 3) // 4
            it = 0
            for g in range(ngrp):
                nj = min(4, ntile - 4 * g)
                xtg = XTG[g % 2]
                otg = OTG[g % 2]
                COS, SIN = COSG[g % 2], SING[g % 2]
                if not is_ctx:
                    k.dma("sp", COS.t[:, 0:nj * 32], d["k_cos"][:, g * 128:g * 128 + nj * 32], w=[COS.b])
                    k.dma("sp", SIN.t[:, 0:nj * 32], d["k_sin"][:, g * 128:g * 128 + nj * 32], w=[SIN.b])
                states = {}

                def head(j):
                    nonlocal it
                    t = 4 * g + j
                    xin, xm = XIN[it % 2], XM[it % 2]
                    ob, vas, vhs, gs = OB[it % 2], VAs[it % 2], VHs[it % 2], Gs[it % 2]
                    SQ, ST, QN, R1, R2 = SQs[it % 2], STs[it % 2], QNs[it % 2], R1s[it % 2], R2s[it % 2]
                    it += 1
                    k.dma("sp", xin.t[:], src[t * 128:(t + 1) * 128, :], w=[xin.b])
                    norm_mod(c, xin, Asl, modt.t[:, 0:1024], modt.b, [(xm.t[:], "pool", xm)], tmps[it % 2], j)
                    for kk in range(8):
                        k.tr(pT.t[:, kk * 128:(kk + 1) * 128], xm.t[:, kk * 128:(kk + 1) * 128], c.ident_b.t[:], r=[xm.b],
                             w=[pT.b] if kk == 0 else (), wp=() if kk == 0 else [pT.b])
                    k.cp("act", xtg.t[:, :, j * 128:(j + 1) * 128], pT.t[:].rearrange("p (k t) -> p k t", k=8), r=[pT.b], wp=[xtg.b])
                    cols = (1, 2, 3) if is_ctx else (0, 1, 2, 3, 4)
                    for ci in cols:
                        for kk in range(8):
                            k.mm(pS[ci].t[:], xtg.t[:, kk, j * 128:(j + 1) * 128], W.t[:, kk, ci * 512:(ci + 1) * 512], kk == 0, kk == 7,
                                 r=[xtg.b, W.b], w=[pS[ci].b] if kk == 0 else (), wp=() if kk == 0 else [pS[ci].b])
                    k.cp("act", vas.t[:, :, 0:64], pS[2].t[:].rearrange("p (h e) -> p h e", h=8), r=[pS[2].b], wp=[vas.b])
                    k.cp("dve", vhs.t[:], pS[3].t[:], r=[pS[3].b], w=[vhs.b])
                    rows = slice(t * 128, (t + 1) * 128)
                    if is_ctx:
                        k.dma("sp", d["VcA"][rows, :], vas.t[:].rearrange("p h e -> p (h e)"), r=[vas.b], wp=[dV])
                        k.dma("sp", d["VHc"][rows, :], vhs.t[:], r=[vhs.b], wp=[dVH])
                    else:
                        k.act(gs.t[:], pS[4].t[:], AF.Silu, r=[pS[4].b], w=[gs.b])
                        k.dma("sp", d["VA"][rows, :], vas.t[:].rearrange("p h e -> p (h e)"), r=[vas.b], wp=[dV])
                        k.dma("sp", d["VH"][rows, :], vhs.t[:], r=[vhs.b], wp=[dVH])
                        k.dma("sp", d["G"][rows, :], gs.t[:], r=[gs.b], wp=[dG])
                    srcs = ((1, 1),) if is_ctx else ((0, 0), (1, 1))
                    for (ci, slot_i) in srcs:
                        k.act(SQ.t[:, slot_i * 512:(slot_i + 1) * 512], pS[ci].t[:], AF.Square, r=[pS[ci].b], wp=[SQ.b])
                        k.red(ST.t[:, slot_i * 8:(slot_i + 1) * 8], SQ.t[:, slot_i * 512:(slot_i + 1) * 512].rearrange("p (h e) -> p h e", h=8), r=[SQ.b], wp=[ST.b])
                    k.ts("dve", ST.t[:, 16:32], ST.t[:, 0:16], 1.0 / 64, ALU.mult, EPS, ALU.add, r=[ST.b], wp=[ST.b])
                    k.act(ST.t[:, 32:48], ST.t[:, 16:32], AF.Sqrt, r=[ST.b], wp=[ST.b])
                    k.recip(ST.t[:, 16:32], ST.t[:, 32:48], r=[ST.b], wp=[ST.b])
                    for (ci, slot_i) in srcs:
                        qn = QN.t[:, slot_i * 512:(slot_i + 1) * 512]
                        k.tt("dve", qn.rearrange("p (h e) -> p h e", h=8), pS[ci].t[:].rearrange("p (h e) -> p h e", h=8),
                             ST.t[:, 16 + slot_i * 8:16 + slot_i * 8 + 8].unsqueeze(2).to_broadcast([128, 8, 64]), ALU.mult,
                             r=[pS[ci].b, ST.b], wp=[QN.b])
                        Gt = GQ if slot_i == 0 else GK
                        k.tt("pool", qn, qn, Gt.t[:], ALU.mult, r=[QN.b, Gt.b], wp=[QN.b])
                    if is_ctx:
                        k.cp("act", ob.t[:, 1024:1536], QN.t[:, 512:1024], r=[QN.b], wp=[ob.b])
                    else:
                        k.cp("act", ob.t[:, 512:1024], QN.t[:, 0:512], r=[QN.b], wp=[ob.b])
                        qv = QN.t[:].rearrange("p (h a b i) -> p h a b i", h=16, a=2, b=2)
                        cosb = COS.t[:, j * 32:(j + 1) * 32].rearrange("p (a i) -> p a i", a=2).unsqueeze(1).to_broadcast([128, 16, 2, 16])
                        sinb = SIN.t[:, j * 32:(j + 1) * 32].rearrange("p (a i) -> p a i", a=2).unsqueeze(1).to_broadcast([128, 16, 2, 16])
                        r1v = R1.t[:].rearrange("p (h a b i) -> p h a b i", h=16, a=2, b=2)
                        r2v = R2.t[:].rearrange("p (h a b i) -> p h a b i", h=16, a=2, b=2)
                        x1, x2 = qv[:, :, :, 0, :], qv[:, :, :, 1, :]
                        k.tt("dve", r1v[:, :, :, 0, :], x1, cosb, ALU.mult, r=[QN.b, COS.b], wp=[R1.b])
                        k.tt("pool", r2v[:, :, :, 0, :], x2, sinb, ALU.mult, r=[QN.b, SIN.b], wp=[R2.b])
                        k.tt("dve", r1v[:, :, :, 1, :], x2, cosb, ALU.mult, r=[QN.b, COS.b], wp=[R1.b])
                        k.tt("pool", r2v[:, :, :, 1, :], x1, sinb, ALU.mult, r=[QN.b, SIN.b], wp=[R2.b])
                        for slot_i, o0 in ((0, 0), (1, 1024)):
                            ov = ob.t[:, o0:o0 + 512].rearrange("p (h a b i) -> p h a b i", h=8, a=2, b=2)
                            a1 = r1v[:, slot_i * 8:(slot_i + 1) * 8]
                            a2 = r2v[:, slot_i * 8:(slot_i + 1) * 8]
                            k.tt("dve", ov[:, :, :, 0, :], a1[:, :, :, 0, :], a2[:, :, :, 0, :], ALU.subtract, r=[R1.b, R2.b], wp=[ob.b])
                            k.tt("dve", ov[:, :, :, 1, :], a1[:, :, :, 1, :], a2[:, :, :, 1, :], ALU.add, r=[R1.b, R2.b], wp=[ob.b])
                    states[j] = (ob,)

                def tail(j):
                    (ob,) = states[j]
                    which = (2,) if is_ctx else (0, 1, 2)
                    for wi in which:
                        po = pO[0] if wi < 2 else pO[1]
                        for hp in range(4):
                            col = ((wi % 2) * 4 + hp) * 128
                            first = (hp == 0 and wi in (0, 2))
                            k.tr(po.t[:, col:col + 128], ob.t[:, wi * 512 + hp * 128: wi * 512 + (hp + 1) * 128], c.ident_b.t[:], r=[ob.b],
                                 w=[po.b] if first else (), wp=() if first else [po.b])
                    if not is_ctx:
                        k.cp("act", otg.t[:, 0:2, :, j * 128:(j + 1) * 128], pO[0].t[:].rearrange("p (w h t) -> p w h t", w=2, h=4), r=[pO[0].b], wp=[otg.b])
                    k.cp("dve", otg.t[:, 2, :, j * 128:(j + 1) * 128], pO[1].t[:, 0:512].rearrange("p (h t) -> p h t", h=4), r=[pO[1].b], wp=[otg.b])

                head(0)
                for j in range(nj):
                    if j + 1 < nj:
                        head(j + 1)
                    tail(j)
                tok = slice(g * 512, g * 512 + nj * 128)
                w_ = nj * 128
                if is_ctx:
                    k.dma("sp", d["XTc"][:, :, tok], xtg.t[:, :, 0:w_], r=[xtg.b], wp=[dXTc])
                    k.dma("sp", d["KcT"][:, :, tok], otg.t[:, 2, :, 0:w_], r=[otg.b], wp=[dQ])
                else:
                    k.dma("sp", d["XT"][:, :, tok], xtg.t[:, :, 0:w_], r=[xtg.b], wp=[dXT])
                    k.dma("sp", d["QTr"][:, :, tok], otg.t[:, 0, :, 0:w_], r=[otg.b], wp=[dQ])
                    k.dma("sp", d["QTf"][:, :, tok], otg.t[:, 1, :, 0:w_], r=[otg.b], wp=[dQ])
                    k.dma("sp", d["KTr"][:, :, tok], otg.t[:, 2, :, 0:w_], r=[otg.b], wp=[dQ])

        run(d["ctx"], L // 128, Ac, c.CMOD, True)
        run(d["x"], NT, A, c.MOD, False)
        P.end_phase()


def core_inputs(inp, b, cfg, consts):
    f = lambda a: np.ascontiguousarray(np.asarray(a, dtype=np.float32))
    m = {
        "x": f(inp["x"][b]), "c": f(inp["c"][b]).reshape(8, 128), "ctx": f(inp["ctx"][b]),
        "c_ctx": f(inp["c_ctx"]).reshape(8, 128),
        "ada_w": f(inp["ada_w"]), "ada_b": f(inp["ada_b"]), "norm1_g": f(inp["norm1_g"]), "norm2_g": f(inp["norm2_g"]),
        "ab_w_in": f(inp["ab_w_in"][0]), "ab_w_out": f(inp["ab_w_out"][0]),
        "nat_q_norm": f(inp["nat_q_norm"][0]).reshape(1, 64), "nat_k_norm": f(inp["nat_k_norm"][0]).reshape(1, 64),
        "rpb_full": layout_rpb(f(inp["nat_rpb"][0])),
        "hgrn_lb": f(inp["hgrn_lb"]).reshape(16, 128), "hgrn_o_norm": f(inp["hgrn_o_norm"][0]).reshape(1, 128),
        "conv_w1": f(inp["conv_w1"][0]), "conv_b1": f(inp["conv_b1"][0]).reshape(16, 128), "conv_dw": f(inp["conv_dw"][0]),
        "conv_dw_b": f(inp["conv_dw_b"][0]).reshape(8, 128), "conv_ln_g": f(inp["conv_ln_g"][0]).reshape(8, 128),
        "conv_ln_b": f(inp["conv_ln_b"][0]).reshape(8, 128), "conv_w2": f(inp["conv_w2"][0]), "conv_b2": f(inp["conv_b2"][0]).reshape(1, 1024),
        "router_w": f(inp["router_w"]), "router_b": f(inp["router_b"]),
        "moe_w1": f(inp["moe_w1"]), "moe_b1": f(inp["moe_b1"]).reshape(2, cfg.E * 16, 128),
        "moe_w2": f(inp["moe_w2"]), "moe_b2": f(inp["moe_b2"]),
    }
    for kname, v in consts.items():
        m["k_" + kname] = v
    return m


_cache = {}


def kernel(**inputs):
    B = inputs["x"].shape[0]
    S = inputs["x"].shape[1]
    cfg = Cfg(S=S, L=inputs["ctx"].shape[1], E=inputs["moe_w1"].shape[1])
    key = (cfg.S, cfg.L, cfg.E)
    if key not in _cache:
        _cache[key] = build_program(cfg)[0]
    nc = _cache[key]
    consts = host_consts(cfg)
    in_maps = [core_inputs(inputs, b, cfg, consts) for b in range(B)]
    res = run_bass_kernel_spmd(nc, in_maps, core_ids=list(range(B)))
    return np.stack([np.asarray(r["out"], dtype=np.float32) for r in res.results], axis=0)


def phase_a2(c):
    nc, P, k, d, cfg = c.nc, c.P, c.k, c.d, c.cfg
    S, L = cfg.S, cfg.L
    with ExitStack() as ph:
        W = sb(ph, nc, [128, 8, 1536], BF16, name="Wfm")
        wv = d["ab_w_in"].rearrange("(k p) n -> p k n", p=128)
        for i in range(3):
            k.dma("pool", W.t[:, :, i * 512:(i + 1) * 512], wv[:, :, 1536 + i * 512:1536 + (i + 1) * 512], wp=[W.b])
        RESET = sb(ph, nc, [128, 512], F32, name="reset")
        k.dma("sp", RESET.t[:], d["k_reset"], w=[RESET.b])
        lbr = sb(ph, nc, [16, 128], F32, name="lbr")
        Ee = sb(ph, nc, [128, 16], F32, name="Ee")
        LB = sb(ph, nc, [128, 24], F32, name="LB")
        pL = ps(ph, nc, [128, 16], F32, name="pL")
        k.dma("sp", lbr.t[:], d["hgrn_lb"], w=[lbr.b])
        k.tr(pL.t[:], lbr.t[:], c.ident_f.t[0:16, 0:16], r=[lbr.b], w=[pL.b])
        k.act(Ee.t[:], pL.t[:], AF.Exp, r=[pL.b], w=[Ee.b])
        ev = Ee.t[:].rearrange("p (d j h) -> p d j h", d=2, j=2)
        k.tt("dve", LB.t[:, 16:24].rearrange("p (d h) -> p d h", d=2), ev[:, :, 0, :], ev[:, :, 1, :], ALU.add, r=[Ee.b], wp=[LB.b])
        k.recip(LB.t[:, 16:24], LB.t[:, 16:24], r=[LB.b], wp=[LB.b])
        k.tt("dve", LB.t[:, 0:8].rearrange("p (d h) -> p d h", d=2), ev[:, :, 0, :], LB.t[:, 16:24].rearrange("p (d h) -> p d h", d=2), ALU.mult, r=[Ee.b, LB.b], wp=[LB.b])
        k.ts("dve", LB.t[:, 8:16], LB.t[:, 0:8], -1.0, ALU.mult, 1.0, ALU.add, r=[LB.b], wp=[LB.b])

        XTG = sb(ph, nc, [128, 8, 512], BF16, n=2, name="xtg")
        names = ("q32", "sg", "f", "lf", "kk", "B", "e1", "e2", "t1", "r", "e3")
        TM = {n_: sb(ph, nc, [128, 512], F32, n=2, name=n_) for n_ in names}
        HQs = sb(ph, nc, [128, 2, 4, 512], BF16, n=2, name="hqs")
        HKs = sb(ph, nc, [128, 2, 4, 512], BF16, n=2, name="hks")
        KHT = sb(ph, nc, [128, 512], BF16, n=2, name="kht")
        KHs = sb(ph, nc, [128, 4, 2, 512], BF16, n=2, name="khs")
        DECs = sb(ph, nc, [128, 2, 4, 8], F32, n=2, name="decs")
        pQ = ps(ph, nc, [128, 512], F32, n=2, name="pQ")
        pF = ps(ph, nc, [128, 512], F32, n=3, name="pF")
        pK = ps(ph, nc, [128, 512], BF16, n=2, name="pK")
        dHQ, dHK, dKH, dDEC = Buf("dHQ"), Buf("dHK"), Buf("dKH"), Buf("dDEC")
        QS = 128.0 ** -0.5

        def run(src, ntok, is_ctx):
            ngrp = (ntok + 511) // 512
            it = 0
            for g in range(ngrp):
                n = min(512, ntok - g * 512)
                nch = n // 64
                nsub = n // 128
                xtg, hqs, hks, khs, decs = XTG[g % 2], HQs[g % 2], HKs[g % 2], KHs[g % 2], DECs[g % 2]
                k.dma("sp", xtg.t[:, :, 0:n], src[:, :, g * 512:g * 512 + n], w=[xtg.b])
                for h in range(4):
                    pq = pQ[h % 2]
                    if not is_ctx:
                        for kk_ in range(8):
                            k.mm(pq.t[:, 0:n], W.t[:, kk_, h * 128:(h + 1) * 128], xtg.t[:, kk_, 0:n], kk_ == 0, kk_ == 7,
                                 r=[W.b, xtg.b], w=[pq.b] if kk_ == 0 else (), wp=() if kk_ == 0 else [pq.b])
                        q32 = TM["q32"][h % 2]
                        k.act(q32.t[:, 0:n], pq.t[:, 0:n], AF.Silu, r=[pq.b], w=[q32.b])
                    for dd in range(2):
                        pf = pF[(2 * h + dd) % 3]
                        c0 = 512 + dd * 512 + h * 128
                        for kk_ in range(8):
                            k.mm(pf.t[:, 0:n], W.t[:, kk_, c0:c0 + 128], xtg.t[:, kk_, 0:n], kk_ == 0, kk_ == 7,
                                 r=[W.b, xtg.b], w=[pf.b] if kk_ == 0 else (), wp=() if kk_ == 0 else [pf.b])
                        tm = {n_: TM[n_][it % 2] for n_ in names}
                        kht = KHT[it % 2]
                        pk = pK[it % 2]
                        it += 1
                        sg, f, lf, kk, Bc, e1, e2, t1, rr, e3 = (tm[x] for x in ("sg", "f", "lf", "kk", "B", "e1", "e2", "t1", "r", "e3"))
                        li = dd * 4 + h
                        k.act(sg.t[:, 0:n], pf.t[:, 0:n], AF.Sigmoid, r=[pf.b], w=[sg.b])
                        k.ts("dve", f.t[:, 0:n], sg.t[:, 0:n], LB.t[:, 8 + li:9 + li], ALU.mult, LB.t[:, li:li + 1], ALU.add, r=[sg.b, LB.b], w=[f.b])
                        k.act(lf.t[:, 0:n], f.t[:, 0:n], AF.Ln, r=[f.b], w=[lf.b])
                        k.ts("pool", kk.t[:, 0:n], f.t[:, 0:n], -1.0, ALU.mult, 1.0, ALU.add, r=[f.b], w=[kk.b])
                        P.op("dve", (lambda e, o=Bc.t[:, 0:n], a=RESET.t[:, 0:n], b_=lf.t[:, 0:n]:
                                     e.tensor_tensor_scan(out=o, data0=a, data1=b_, initial=0.0, op0=ALU.mult, op1=ALU.add)),
                             reads=[RESET.b, lf.b], writes=[Bc.b])
                        Bv = Bc.t[:, 0:n].rearrange("p (c t) -> p c t", t=64)
                        Bend = Bv[:, :, 63:64].to_broadcast([128, nch, 64])
                        v3 = lambda s_: s_.t[:, 0:n].rearrange("p (c t) -> p c t", t=64)
                        if dd == 0:
                            k.act(e1.t[:, 0:n], Bc.t[:, 0:n], AF.Exp, r=[Bc.b], w=[e1.b])
                            k.act(e2.t[:, 0:n], Bc.t[:, 0:n], AF.Exp, r=[Bc.b], w=[e2.b], scale=-1.0)
                            k.tt("dve", v3(t1), Bend, Bv, ALU.subtract, r=[Bc.b], w=[t1.b])
                            k.act(e3.t[:, 0:n], t1.t[:, 0:n], AF.Exp, r=[t1.b], w=[e3.b])
                        else:
                            k.tt("dve", t1.t[:, 0:n], lf.t[:, 0:n], Bc.t[:, 0:n], ALU.subtract, r=[lf.b, Bc.b], w=[t1.b])
                            k.tt("dve", v3(rr), v3(t1), Bend, ALU.add, r=[t1.b, Bc.b], w=[rr.b])
                            k.act(e1.t[:, 0:n], rr.t[:, 0:n], AF.Exp, r=[rr.b], w=[e1.b])
                            k.act(e2.t[:, 0:n], rr.t[:, 0:n], AF.Exp, r=[rr.b], w=[e2.b], scale=-1.0)
                            k.act(e3.t[:, 0:n], t1.t[:, 0:n], AF.Exp, r=[t1.b], w=[e3.b], scale=-1.0)
                        k.act(decs.t[:, dd, h, 0:nch], Bv[:, :, 63], AF.Exp, r=[Bc.b], wp=[decs.b])
                        if not is_ctx:
                            q32 = TM["q32"][h % 2]
                            k.stt(hqs.t[:, dd, h, 0:n], q32.t[:, 0:n], QS, e1.t[:, 0:n], ALU.mult, ALU.mult, r=[q32.b, e1.b], wp=[hqs.b])
                            k.tt("pool", hks.t[:, dd, h, 0:n], kk.t[:, 0:n], e2.t[:, 0:n], ALU.mult, r=[kk.b, e2.b], wp=[hks.b])
                        k.tt("pool", kht.t[:, 0:n], kk.t[:, 0:n], e3.t[:, 0:n], ALU.mult, r=[kk.b, e3.b], w=[kht.b])
                        for sub in range(nsub):
                            k.tr(pk.t[:, sub * 128:(sub + 1) * 128], kht.t[:, sub * 128:(sub + 1) * 128], c.ident_b.t[:], r=[kht.b],
                                 w=[pk.b] if sub == 0 else (), wp=() if sub == 0 else [pk.b])
                        k.cp("act", khs.t[:, 0:nsub, dd, h * 128:(h + 1) * 128], pk.t[:, 0:n].rearrange("p (s e) -> p s e", e=128), r=[pk.b], wp=[khs.b])
                tok = slice(g * 512, g * 512 + n)
                for dd in range(2):
                    if is_ctx:
                        k.dma("sp", d["KHc"][dd, tok, :].rearrange("(s p) e -> p s e", p=128), khs.t[:, 0:nsub, dd, :], r=[khs.b], wp=[dKH])
                        k.dma("sp", d["DECc"][dd, :, :, g * 8:g * 8 + nch], decs.t[:, dd, :, 0:nch], r=[decs.b], wp=[dDEC])
                    else:
                        k.dma("sp", d["HQ"][dd, :, :, tok], hqs.t[:, dd, :, 0:n], r=[hqs.b], wp=[dHQ])
                        k.dma("sp", d["HK"][dd, :, :, tok], hks.t[:, dd, :, 0:n], r=[hks.b], wp=[dHK])
                        k.dma("sp", d["KH"][dd, tok, :].rearrange("(s p) e -> p s e", p=128), khs.t[:, 0:nsub, dd, :], r=[khs.b], wp=[dKH])
                        k.dma("sp", d["DEC"][dd, :, :, g * 8:g * 8 + nch], decs.t[:, dd, :, 0:nch], r=[decs.b], wp=[dDEC])

        run(d["XTc"], L, True)
        run(d["XT"], S, False)
        P.end_phase()


def phase_nat(c):
    nc, P, k, d, cfg = c.nc, c.P, c.k, c.d, c.cfg
    S, L, ROWS = cfg.S, cfg.L, cfg.ROWS
    NCC = L // 128
    NCH = 4 + NCC
    with ExitStack() as ph:
        BFf = sb(ph, nc, [128, 7680], F32, name="bff")
        BFb = sb(ph, nc, [128, 8, 960], BF16, name="bfb")
        k.dma("sp", BFf.t[0:64, :], d["rpb_full"], wp=[BFf.b])
        k.dma("sp", BFf.t[64:128, :], d["rpb_full"], wp=[BFf.b])
        k.cp("dve", BFb.t[:].rearrange("p h e -> p (h e)"), BFf.t[:], r=[BFf.b], w=[BFb.b])
        KcT = sb(ph, nc, [128, 4, L], BF16, name="kct")
        VcA = sb(ph, nc, [128, NCC, 520], BF16, name="vca")
        k.dma("sp", KcT.t[:], d["KcT"], w=[KcT.b])
        k.dma("sp", VcA.t[:], d["VcA"].rearrange("(c p) f -> p c f", p=128), w=[VcA.b])
        QR = sb(ph, nc, [128, 4, 512], BF16, n=2, name="qr")
        QF = sb(ph, nc, [128, 4, 512], BF16, n=2, name="qf")
        KW = sb(ph, nc, [128, 4, 512], BF16, n=3, name="kw")
        VW = sb(ph, nc, [128, 4, 520], BF16, n=3, name="vw")
        PT = sb(ph, nc, [128, NCH * 64], BF16, n=3, name="pt")
        NS = sb(ph, nc, [64, 512], BF16, n=2, name="ns")
        RD = sb(ph, nc, [64, 8], F32, n=2, name="rd")
        pS = ps(ph, nc, [128, 512], F32, n=4, name="pS")
        pO = ps(ph, nc, [64, 4, 65], F32, n=4, name="pO")
        dCAT = Buf("dCATn")
        it = 0
        for r in range(ROWS):
            g8, ro = r // 8, (r % 8) * 64
            qr, qf = QR[g8 % 2], QF[g8 % 2]
            if r % 8 == 0:
                k.dma("sp", qr.t[:], d["QTr"][:, :, g8 * 512:(g8 + 1) * 512], w=[qr.b])
                k.dma("sp", qf.t[:], d["QTf"][:, :, g8 * 512:(g8 + 1) * 512], w=[qf.b])
            rs = min(max(r - 4, 0), ROWS - 8)
            dr0 = rs - r + 7
            kw, vw = KW[r % 3], VW[r % 3]
            k.dma("sp", kw.t[:], d["KTr"][:, :, rs * 64:rs * 64 + 512], w=[kw.b])
            k.dma("sp", vw.t[:], d["VA"][rs * 64:rs * 64 + 512, :].rearrange("(c p) f -> p c f", p=128), w=[vw.b])
            ns, rd = NS[r % 2], RD[r % 2]
            po2 = (pO[(2 * r) % 4], pO[(2 * r + 1) % 4])
            def scores(h):
                nonlocal it
                hp, pb = h // 2, (h % 2) * 64
                psx, pt = pS[it % 4], PT[it % 3]
                it += 1
                first = True
                for kc in range(4):
                    k.mm(psx.t[:, kc * 64:(kc + 1) * 64], kw.t[pb:pb + 64, hp, kc * 128:(kc + 1) * 128], qr.t[pb:pb + 64, hp, ro:ro + 64], True, False,
                         r=[kw.b, qr.b], w=[psx.b] if first else (), wp=() if first else [psx.b])
                    first = False
                    k.mm(psx.t[:, kc * 64:(kc + 1) * 64], BFb.t[pb:pb + 64, h, (dr0 + 2 * kc) * 64:(dr0 + 2 * kc) * 64 + 128], c.ident_b.t[pb:pb + 64, pb:pb + 64], False, True,
                         r=[BFb.b], wp=[psx.b])
                for cc in range(NCC):
                    k.mm(psx.t[:, (4 + cc) * 64:(5 + cc) * 64], KcT.t[pb:pb + 64, hp, cc * 128:(cc + 1) * 128], qf.t[pb:pb + 64, hp, ro:ro + 64], True, True,
                         r=[KcT.b, qf.b], wp=[psx.b])
                k.act(pt.t[:], psx.t[:, 0:NCH * 64], AF.Exp, r=[psx.b], w=[pt.b])
                return pt

            def pv(h, pt):
                po = po2[h // 4]
                hh = h % 4
                for ch in range(NCH):
                    rhs = vw.t[:, ch, h * 65:(h + 1) * 65] if ch < 4 else VcA.t[:, ch - 4, h * 65:(h + 1) * 65]
                    k.mm(po.t[:, hh, :], pt.t[:, ch * 64:(ch + 1) * 64], rhs, ch == 0, ch == NCH - 1,
                         r=[pt.b, vw.b, VcA.b], w=[po.b] if (ch == 0 and hh == 0) else (), wp=() if (ch == 0 and hh == 0) else [po.b])

            pts = [scores(0)]
            for h in range(8):
                if h + 1 < 8:
                    pts.append(scores(h + 1))
                pv(h, pts[h])
            for half in range(2):
                po = po2[half]
                k.recip(rd.t[:, half * 4:(half + 1) * 4], po.t[:, :, 64], r=[po.b], wp=[rd.b])
                k.tt("dve", ns.t[:, half * 256:(half + 1) * 256].rearrange("p (h e) -> p h e", h=4), po.t[:, :, 0:64],
                     rd.t[:, half * 4:(half + 1) * 4].unsqueeze(2).to_broadcast([64, 4, 64]), ALU.mult, r=[po.b, rd.b], wp=[ns.b])
            k.dma("sp", d["CAT"][r * 64:(r + 1) * 64, 0:512], ns.t[:], r=[ns.b], wp=[dCAT])
        P.end_phase()


def phase_hgrn(c):
    nc, P, k, d, cfg = c.nc, c.P, c.k, c.d, c.cfg
    S, L = cfg.S, cfg.L
    NG = S // 512
    NCHT = S // 64
    LC = L // 64
    with ExitStack() as ph:
        TRI = sb(ph, nc, [128, 2, 64], F32, name="tri")
        k.dma("sp", TRI.t[:, 0, :], d["k_trif"], wp=[TRI.b])
        k.dma("sp", TRI.t[:, 1, :], d["k_trib"], wp=[TRI.b])
        og = sb(ph, nc, [128, 128], F32, name="og")
        ONG = sb(ph, nc, [128, 512], F32, name="ong")
        k.dma("sp", og.t[:], d["hgrn_o_norm"].partition_broadcast(128), w=[og.b])
        k.ts("dve", ONG.t[:].rearrange("p (h e) -> p h e", h=4), og.t[:].unsqueeze(1).to_broadcast([128, 4, 128]), 1.0, ALU.mult, r=[og.b], w=[ONG.b])
        S32s = sb(ph, nc, [128, 4, 128], F32, n=2, name="s32")
        Sbfs = sb(ph, nc, [128, 4, 128], BF16, n=2, name="sbf")
        DECt = sb(ph, nc, [128, 4, NCHT], F32, name="dect")
        DECc = sb(ph, nc, [128, 4, LC], F32, name="decc")
        KHc = sb(ph, nc, [128, L // 128, 512], BF16, name="khc")
        VHc = sb(ph, nc, [128, L // 128, 512], BF16, name="vhc")
        HQg = sb(ph, nc, [128, 4, 512], BF16, n=2, name="hqg")
        HKg = sb(ph, nc, [128, 4, 512], BF16, n=2, name="hkg")
        KHg = sb(ph, nc, [128, 4, 512], BF16, n=2, name="khg")
        VHg = sb(ph, nc, [128, 4, 512], BF16, n=2, name="vhg")
        SC = [sb(ph, nc, [128, 256], BF16, n=2, name="sc%d" % i) for i in range(2)]
        for i in range(2):
            for s_ in SC[i]:
                k.memset("pool", s_.t[:], 0.0, w=[s_.b])
        OFs = sb(ph, nc, [64, 512], F32, n=2, name="ofs")
        OFc = sb(ph, nc, [64, 512], F32, n=2, name="ofc")
        Gc = sb(ph, nc, [64, 512], F32, n=2, name="gc")
        O32 = sb(ph, nc, [64, 512], F32, n=2, name="o32")
        SQ = sb(ph, nc, [64, 512], F32, n=2, name="sq")
        ST = sb(ph, nc, [64, 16], F32, n=2, name="st")
        Y1 = sb(ph, nc, [64, 512], F32, n=2, name="y1")
        YB = sb(ph, nc, [64, 512], BF16, n=2, name="yb")
        pSs = ps(ph, nc, [128, 512], F32, n=2, name="pSs")
        pSo = ps(ph, nc, [128, 512], F32, n=2, name="pSo")
        pSt = ps(ph, nc, [128, 512], F32, n=2, name="pSt")
        dOF, dCAT = Buf("dOF"), Buf("dCATh")
        k.dma("sp", VHc.t[:], d["VHc"].rearrange("(s p) e -> p s e", p=128), w=[VHc.b])
        it = 0
        for dd in range(2):
            k.memset("dve", S32s[0].t[:], 0.0, w=[S32s[0].b])
            k.dma("sp", DECt.t[:], d["DEC"][dd], w=[DECt.b])
            k.dma("sp", DECc.t[:], d["DECc"][dd], w=[DECc.b])
            k.dma("sp", KHc.t[:], d["KHc"][dd].rearrange("(s p) e -> p s e", p=128), w=[KHc.b])

            sn = 0

            def state_update(khs, vhs, tl, pb, dec_ap_fn):
                nonlocal it, sn
                pst = pSt[it % 2]
                for h in range(4):
                    k.mm(pst.t[:, h * 128:(h + 1) * 128], khs.t[pb:pb + 64, tl, h * 128:(h + 1) * 128], vhs.t[pb:pb + 64, tl, h * 128:(h + 1) * 128], True, True,
                         r=[khs.b, vhs.b], w=[pst.b] if h == 0 else (), wp=() if h == 0 else [pst.b])
                so, sw = S32s[sn % 2], S32s[(sn + 1) % 2]
                for h in range(4):
                    k.stt(sw.t[:, h, :], so.t[:, h, :], dec_ap_fn(h), pst.t[:, h * 128:(h + 1) * 128], ALU.mult, ALU.add,
                          r=[so.b, pst.b, DECt.b, DECc.b], wp=[sw.b])
                nb = Sbfs[(sn + 1) % 2]
                k.cp("act", nb.t[:], sw.t[:], r=[sw.b], w=[nb.b])
                sn += 1

            chs = range(LC) if dd == 0 else range(LC - 1, -1, -1)
            for ch in chs:
                state_update(KHc, VHc, ch // 2, (ch % 2) * 64, lambda h, ch=ch: DECc.t[:, h, ch:ch + 1])
                it += 1
            groups = list(range(NG)) if dd == 0 else list(range(NG - 1, -1, -1))
            order = []
            for gi, g in enumerate(groups):
                for tl in (range(4) if dd == 0 else range(3, -1, -1)):
                    for cc in ((0, 1) if dd == 0 else (1, 0)):
                        order.append((gi, g, tl, cc))
            loaded = set()

            def ensure(gi, g):
                if gi in loaded:
                    return
                loaded.add(gi)
                hq, hk, kh, vh = HQg[gi % 2], HKg[gi % 2], KHg[gi % 2], VHg[gi % 2]
                tok = slice(g * 512, (g + 1) * 512)
                k.dma("sp", hq.t[:], d["HQ"][dd, :, :, tok], w=[hq.b])
                k.dma("sp", hk.t[:], d["HK"][dd, :, :, tok], w=[hk.b])
                k.dma("sp", kh.t[:], d["KH"][dd, tok, :].rearrange("(s p) e -> p s e", p=128), w=[kh.b])
                k.dma("sp", vh.t[:], d["VH"][tok, :].rearrange("(s p) e -> p s e", p=128), w=[vh.b])

            def scores(n):
                gi, g, tl, cc = order[n]
                ensure(gi, g)
                hq, hk = HQg[gi % 2], HKg[gi % 2]
                pb = cc * 64
                toff, qoff = tl * 128, tl * 128 + cc * 64
                pss = pSs[n % 2]
                sc = SC[cc][(n // 2) % 2]
                for h in range(4):
                    k.mm(pss.t[:, h * 64:(h + 1) * 64], hk.t[:, h, toff:toff + 128], hq.t[:, h, qoff:qoff + 64], True, True,
                         r=[hk.b, hq.b], w=[pss.b] if h == 0 else (), wp=() if h == 0 else [pss.b])
                k.tt("dve", sc.t[pb:pb + 64, :].rearrange("p (h t) -> p h t", h=4), pss.t[pb:pb + 64, 0:256].rearrange("p (h t) -> p h t", h=4),
                     TRI.t[pb:pb + 64, dd, :].unsqueeze(1).to_broadcast([64, 4, 64]), ALU.mult, r=[pss.b, TRI.b], wp=[sc.b])
                return sc

            def rest(n, sc):
                nonlocal it
                gi, g, tl, cc = order[n]
                hq, hk, kh, vh = HQg[gi % 2], HKg[gi % 2], KHg[gi % 2], VHg[gi % 2]
                ch = g * 8 + tl * 2 + cc
                pb = cc * 64
                qoff = tl * 128 + cc * 64
                pso = pSo[n % 2]
                Sbf = Sbfs[sn % 2]
                state_update(kh, vh, tl, pb, lambda h, ch=ch: DECt.t[:, h, ch:ch + 1])
                it += 1
                for h in range(4):
                    k.mm(pso.t[0:64, h * 128:(h + 1) * 128], sc.t[:, h * 64:(h + 1) * 64], vh.t[:, tl, h * 128:(h + 1) * 128], True, False,
                         r=[sc.b, vh.b], w=[pso.b] if h == 0 else (), wp=() if h == 0 else [pso.b])
                    k.mm(pso.t[0:64, h * 128:(h + 1) * 128], hq.t[:, h, qoff:qoff + 64], Sbf.t[:, h, :], False, True,
                         r=[hq.b, Sbf.b], wp=[pso.b])
                rows = slice(ch * 64, (ch + 1) * 64)
                if dd == 0:
                    ofs = OFs[n % 2]
                    k.cp("act", ofs.t[:], pso.t[0:64, :], r=[pso.b], w=[ofs.b])
                    k.dma("sp", d["OF"][rows, :], ofs.t[:], r=[ofs.b], wp=[dOF])
                else:
                    ofc, gc, o32, sq, st, y1, yb = (X[n % 2] for X in (OFc, Gc, O32, SQ, ST, Y1, YB))
                    k.dma("sp", ofc.t[:], d["OF"][rows, :], r=[dOF], w=[ofc.b])
                    k.dma("sp", gc.t[:], d["G"][rows, :], w=[gc.b])
                    k.tt("dve", o32.t[:], pso.t[0:64, :], ofc.t[:], ALU.add, r=[pso.b, ofc.b], w=[o32.b])
                    k.act(sq.t[:], o32.t[:], AF.Square, r=[o32.b], w=[sq.b])
                    k.red(st.t[:, 0:4], sq.t[:].rearrange("p (h e) -> p h e", h=4), r=[sq.b], wp=[st.b])
                    k.ts("dve", st.t[:, 4:8], st.t[:, 0:4], 1.0 / 128, ALU.mult, EPS, ALU.add, r=[st.b], wp=[st.b])
                    k.act(st.t[:, 8:12], st.t[:, 4:8], AF.Sqrt, r=[st.b], wp=[st.b])
                    k.recip(st.t[:, 12:16], st.t[:, 8:12], r=[st.b], wp=[st.b])
                    k.tt("dve", y1.t[:].rearrange("p (h e) -> p h e", h=4), o32.t[:].rearrange("p (h e) -> p h e", h=4),
                         st.t[:, 12:16].unsqueeze(2).to_broadcast([64, 4, 128]), ALU.mult, r=[o32.b, st.b], w=[y1.b])
                    k.tt("pool", y1.t[:], y1.t[:], ONG.t[0:64, :], ALU.mult, r=[y1.b, ONG.b], w=[y1.b])
                    k.tt("pool", yb.t[:], y1.t[:], gc.t[:], ALU.mult, r=[y1.b, gc.b], w=[yb.b])
                    k.dma("sp", d["CAT"][rows, 512:1024], yb.t[:], r=[yb.b], wp=[dCAT])

            N = len(order)
            cur = scores(0)
            for n in range(N):
                nxt = scores(n + 1) if n + 1 < N else None
                rest(n, cur)
                cur = nxt
        P.end_phase()


def phase_post(c, layer):
    nc, P, k, d, cfg = c.nc, c.P, c.k, c.d, c.cfg
    S, E, NT = cfg.S, cfg.E, cfg.NT
    NG = S // 512
    hin = d["x"] if layer == 0 else d["H2"]
    hout = d["H1"] if layer == 0 else d["H3"]
    with ExitStack() as ph:
        Wo = sb(ph, nc, [128, 8, 1024], BF16, name="Wo")
        wsrc = d["ab_w_out"] if layer == 0 else d["conv_w2"]
        k.dma("pool", Wo.t[:], wsrc.rearrange("(k p) n -> p k n", p=128), w=[Wo.b])
        RW = sb(ph, nc, [128, 8, E], F32, name="RW")
        k.dma("sp", RW.t[:], d["router_w"][layer].rearrange("(k p) e -> p k e", p=128), w=[RW.b])
        RB = sb(ph, nc, [128, E], F32, name="RB")
        k.dma("sp", RB.t[:], d["router_b"][layer:layer + 1, :].partition_broadcast(128), w=[RB.b])
        B2t = sb(ph, nc, [E, 1024], F32, name="B2t")
        k.dma("sp", B2t.t[:], d["moe_b2"][layer], w=[B2t.b])
        A2 = make_A(c, ph, "norm2_g", layer, 4096, c.MOD)
        XIN = sb(ph, nc, [128, 1024], F32, n=2, name="xin")
        Hs = sb(ph, nc, [128, 1024], F32, n=2, name="hs")
        V1s = sb(ph, nc, [128, 1024], F32, n=2, name="v1")
        tmps = [{"ss": sb(ph, nc, [128, 16], F32, name="ss"), "t1": sb(ph, nc, [128, 1024], F32, name="t1")} for _ in range(2)]
        XFs = sb(ph, nc, [128, 1024], F32, n=2, name="xf")
        XB = sb(ph, nc, [128, 1024], BF16, n=2, name="xb")
        XT2g = sb(ph, nc, [128, 8, 512], BF16, n=2, name="xt2g")
        XFTs = sb(ph, nc, [128, 8, 128], F32, n=2, name="xft")
        LG = sb(ph, nc, [128, 4 * E + 32], F32, n=2, name="lg")
        CTs = sb(ph, nc, [E, 128], F32, n=2, name="ct")
        ACss = sb(ph, nc, [128, 1024], F32, n=2, name="acs")
        pT = ps(ph, nc, [128, 1024], BF16, name="pT")
        pT2 = ps(ph, nc, [128, 1024], BF16, name="pT2")
        pY = ps(ph, nc, [128, 512], F32, n=2, name="pY")
        pTf = ps(ph, nc, [128, 512], F32, n=2, name="pTf")
        pLg = ps(ph, nc, [128, E], F32, name="pLg")
        pCT = ps(ph, nc, [E, 128], F32, name="pCT")
        dH, dXT2, dCOMB, dACC = Buf("dH"), Buf("dXT2"), Buf("dCOMB"), Buf("dACC")
        if layer == 0:
            CATt = sb(ph, nc, [128, 1024], BF16, n=2, name="catt")
            catT = sb(ph, nc, [128, 8, 128], BF16, n=2, name="catT")
        else:
            YG = sb(ph, nc, [128, 8, 512], F32, name="yg")
            YSQ = sb(ph, nc, [128, 512], F32, n=2, name="ysq")
            Mm = sb(ph, nc, [128, 512], F32, name="mm_")
            MSQ = sb(ph, nc, [128, 512], F32, name="msq")
            RS = sb(ph, nc, [128, 512], F32, name="rs")
            TA = sb(ph, nc, [128, 512], F32, n=2, name="ta")
            TB = sb(ph, nc, [128, 512], F32, n=2, name="tb")
            HN = sb(ph, nc, [128, 8, 512], BF16, name="hn")
            pSum = pTf[0]
            pSq = pTf[1]
            lnr = sb(ph, nc, [16, 128], F32, name="lnr")
            LNP = sb(ph, nc, [128, 16], F32, name="lnp")
            k.dma("sp", lnr.t[0:8, :], d["conv_ln_g"], wp=[lnr.b])
            k.dma("sp", lnr.t[8:16, :], d["conv_ln_b"], wp=[lnr.b])
            k.tr(pLg.t[:, 0:16] if E >= 16 else pTf[0].t[:, 0:16], lnr.t[:], c.ident_f.t[0:16, 0:16], r=[lnr.b], w=[pLg.b if E >= 16 else pTf[0].b])
            k.cp("dve", LNP.t[:], pLg.t[:, 0:16] if E >= 16 else pTf[0].t[:, 0:16], r=[pLg.b if E >= 16 else pTf[0].b], w=[LNP.b])
            b2bc = sb(ph, nc, [128, 1024], F32, name="b2bc")
            B2M = sb(ph, nc, [128, 1024], F32, name="b2m")
            k.dma("sp", b2bc.t[:], d["conv_b2"].partition_broadcast(128), w=[b2bc.b])
            k.tt("dve", B2M.t[:], b2bc.t[:], c.MOD.t[:, 2048:3072], ALU.mult, r=[b2bc.b, c.MOD.b], w=[B2M.b])

        it = 0
        for g in range(NG):
            xt2g = XT2g[g % 2]
            if layer == 1:
                k.dma("sp", YG.t[:], d["YD"][:, :, g * 512:(g + 1) * 512], w=[YG.b])
                for cch in range(8):
                    ysq = YSQ[cch % 2]
                    k.act(ysq.t[:], YG.t[:, cch, :], AF.Square, r=[YG.b], w=[ysq.b])
                    k.mm(pSum.t[:], c.ones_f.t[:], YG.t[:, cch, :], cch == 0, cch == 7, r=[YG.b, c.ones_f.b], w=[pSum.b] if cch == 0 else (), wp=() if cch == 0 else [pSum.b])
                    k.mm(pSq.t[:], c.ones_f.t[:], ysq.t[:], cch == 0, cch == 7, r=[ysq.b, c.ones_f.b], w=[pSq.b] if cch == 0 else (), wp=() if cch == 0 else [pSq.b])
                k.act(Mm.t[:], pSum.t[:], AF.Copy, r=[pSum.b], w=[Mm.b], scale=1.0 / 1024)
                k.tt("pool", MSQ.t[:], Mm.t[:], Mm.t[:], ALU.mult, r=[Mm.b], w=[MSQ.b])
                k.stt(RS.t[:], pSq.t[:], 1.0 / 1024, MSQ.t[:], ALU.mult, ALU.subtract, r=[pSq.b, MSQ.b], w=[RS.b])
                k.ts("dve", RS.t[:], RS.t[:], EPS, ALU.add, r=[RS.b], w=[RS.b])
                k.act(RS.t[:], RS.t[:], AF.Sqrt, r=[RS.b], w=[RS.b])
                k.recip(RS.t[:], RS.t[:], r=[RS.b], w=[RS.b])
                for cch in range(8):
                    ta, tb = TA[cch % 2], TB[cch % 2]
                    k.tt("dve", ta.t[:], YG.t[:, cch, :], Mm.t[:], ALU.subtract, r=[YG.b, Mm.b], w=[ta.b])
                    k.tt("pool", tb.t[:], ta.t[:], RS.t[:], ALU.mult, r=[ta.b, RS.b], w=[tb.b])
                    k.act(HN.t[:, cch, :], tb.t[:], AF.Silu, r=[tb.b, LNP.b], wp=[HN.b], scale=LNP.t[:, cch:cch + 1], bias=LNP.t[:, 8 + cch:9 + cch])
            states = {}

            def front(j):
                nonlocal it
                t = g * 4 + j
                rows = slice(t * 128, (t + 1) * 128)
                xin, hs, xb = XIN[it % 2], Hs[it % 2], XB[it % 2]
                lg = LG[it % 2]
                V1, XF = V1s[it % 2], XFs[it % 2]
                k.dma("sp", xin.t[:], hin[rows, :], w=[xin.b])
                if layer == 0:
                    cat, ctT = CATt[it % 2], catT[it % 2]
                    k.dma("sp", cat.t[:], d["CAT"][rows, :], w=[cat.b])
                    for kk in range(8):
                        k.tr(pT.t[:, kk * 128:(kk + 1) * 128], cat.t[:, kk * 128:(kk + 1) * 128], c.ident_b.t[:], r=[cat.b],
                             w=[pT.b] if kk == 0 else (), wp=() if kk == 0 else [pT.b])
                    k.cp("act", ctT.t[:].rearrange("p k t -> p (k t)"), pT.t[:], r=[pT.b], w=[ctT.b])
                    lhs = lambda kk: ctT.t[:, kk, :]
                    lb_ = ctT.b
                else:
                    lhs = lambda kk: HN.t[:, kk, j * 128:(j + 1) * 128]
                    lb_ = HN.b
                it += 1
                for half in range(2):
                    for kk in range(8):
                        k.mm(pY[half].t[:], lhs(kk), Wo.t[:, kk, half * 512:(half + 1) * 512], kk == 0, kk == 7,
                             r=[lb_, Wo.b], w=[pY[half].b] if kk == 0 else (), wp=() if kk == 0 else [pY[half].b])
                    k.tt("dve", V1.t[:, half * 512:(half + 1) * 512], pY[half].t[:], c.MOD.t[:, 2048 + half * 512:2048 + (half + 1) * 512], ALU.mult,
                         r=[pY[half].b, c.MOD.b], wp=[V1.b])
                if layer == 1:
                    k.tt("pool", xin.t[:], xin.t[:], B2M.t[:], ALU.add, r=[xin.b, B2M.b], w=[xin.b])
                k.tt("pool", hs.t[:], V1.t[:], xin.t[:], ALU.add, r=[V1.b, xin.b], w=[hs.b])
                k.dma("sp", hout[rows, :], hs.t[:], r=[hs.b], wp=[dH])
                norm_mod(c, hs, A2, c.MOD.t[:, 3072:4096], c.MOD.b, [(XF.t[:], "dve", XF), (xb.t[:], "pool", xb)], tmps[it % 2], j)

                states[j] = (t, rows, hs, xb, lg, XF)

            def back(j):
                t, rows, hs, xb, lg, XF = states[j]
                XFT_, CT_, ACs_ = XFTs[t % 2], CTs[t % 2], ACss[t % 2]
                for kk in range(8):
                    k.tr(pT2.t[:, kk * 128:(kk + 1) * 128], xb.t[:, kk * 128:(kk + 1) * 128], c.ident_b.t[:], r=[xb.b],
                         w=[pT2.b] if kk == 0 else (), wp=() if kk == 0 else [pT2.b])
                k.cp("act", xt2g.t[:, :, j * 128:(j + 1) * 128], pT2.t[:].rearrange("p (k t) -> p k t", k=8), r=[pT2.b], wp=[xt2g.b])
                for kk in range(8):
                    pf = pTf[kk // 4]
                    k.tr(pf.t[:, (kk % 4) * 128:(kk % 4 + 1) * 128], XF.t[:, kk * 128:(kk + 1) * 128], c.ident_f.t[:], r=[XF.b],
                         w=[pf.b] if kk % 4 == 0 else (), wp=() if kk % 4 == 0 else [pf.b])
                for hf in range(2):
                    k.cp("act" if hf else "dve", XFT_.t[:, hf * 4:(hf + 1) * 4, :], pTf[hf].t[:].rearrange("p (k t) -> p k t", k=4), r=[pTf[hf].b], wp=[XFT_.b])
                for kk in range(8):
                    k.mm(pLg.t[:], XFT_.t[:, kk, :], RW.t[:, kk, :], kk == 0, kk == 7, r=[XFT_.b, RW.b], w=[pLg.b] if kk == 0 else (), wp=() if kk == 0 else [pLg.b])
                L0, MK, EX, EXM, MS = (lg.t[:, 0:E], lg.t[:, E:2 * E], lg.t[:, 2 * E:3 * E], lg.t[:, 3 * E:4 * E], lg.t[:, 4 * E:4 * E + 32])
                k.tt("dve", L0, pLg.t[:], RB.t[:], ALU.add, r=[pLg.b, RB.b], wp=[lg.b])
                P.op("dve", (lambda e, o=MS[:, 0:8], i_=L0: e.max(out=o, in_=i_)), reads=[lg.b], wpart=[lg.b])
                k.ts("dve", MK, L0, MS[:, 3:4], ALU.is_ge, r=[lg.b], wp=[lg.b])
                k.ts("dve", MS[:, 8:9], MS[:, 0:1], -1.0, ALU.mult, r=[lg.b], wp=[lg.b])
                k.act(EX, L0, AF.Exp, r=[lg.b], wp=[lg.b], bias=MS[:, 8:9])
                k.stt(EXM, EX, 1.0, MK, ALU.mult, ALU.mult, r=[lg.b], wp=[lg.b], accum=MS[:, 9:10])
                k.recip(MS[:, 10:11], MS[:, 9:10], r=[lg.b], wp=[lg.b])
                k.ts("dve", EXM, EXM, MS[:, 10:11], ALU.mult, r=[lg.b], wp=[lg.b])
                k.dma("sp", d["COMB"][rows, :], EXM, r=[lg.b], wp=[dCOMB])
                k.dma("sp", d["MK"][rows, :], MK, r=[lg.b], wp=[dCOMB])
                k.dma("sp", d["XM2"][rows, :], xb.t[:], r=[xb.b], wp=[dXT2])
                k.tr(pCT.t[:], EXM, c.ident_f.t[:], r=[lg.b], w=[pCT.b])
                k.cp("act", CT_.t[:], pCT.t[:], r=[pCT.b], w=[CT_.b])
                for half in range(2):
                    k.mm(pTf[half].t[:], CT_.t[:], B2t.t[:, half * 512:(half + 1) * 512], True, True, r=[CT_.b, B2t.b], w=[pTf[half].b])
                    k.cp("act" if half else "dve", ACs_.t[:, half * 512:(half + 1) * 512], pTf[half].t[:], r=[pTf[half].b], wp=[ACs_.b])
                k.dma("sp", d["ACCd"][rows, :], ACs_.t[:], r=[ACs_.b], wp=[dACC])

            front(0)
            for j in range(4):
                if j + 1 < 4:
                    front(j + 1)
                back(j)
            k.dma("sp", d["XT2"][:, :, g * 512:(g + 1) * 512], xt2g.t[:], r=[xt2g.b], wp=[dXT2])
        P.end_phase()


def phase_moe(c, layer, MOD5):
    nc, P, k, d, cfg = c.nc, c.P, c.k, c.d, c.cfg
    S, E, MB = cfg.S, cfg.E, cfg.MB
    NTB = MB // 128
    NH = MB // 512
    hin = d["H1"] if layer == 0 else d["H3"]
    hout = d["H2"] if layer == 0 else d["out"]
    with ExitStack() as ph:
        nb1 = (E * 16) // 128
        B1T = sb(ph, nc, [128, E * 16], F32, name="b1t")
        b1r = sb(ph, nc, [128, 128], F32, n=2, name="b1r")
        ACC = sb(ph, nc, [128, NTB, 1024], F32, name="acc")
        XT2b = sb(ph, nc, [128, 8, MB], BF16, name="xt2b")
        CMB = sb(ph, nc, [128, NTB, E], F32, name="cmb")
        W1 = sb(ph, nc, [128, 8, 2048], BF16, n=2, name="w1")
        W2 = sb(ph, nc, [128, 8, 1024], BF16, name="w2")
        ACTT = sb(ph, nc, [128, 8, 512], BF16, n=2, name="actt")
        G1 = sb(ph, nc, [128, 512], F32, n=2, name="g1")
        S1 = sb(ph, nc, [128, 512], F32, n=2, name="s1")
        L1 = sb(ph, nc, [128, 512], F32, n=2, name="l1")
        L2 = sb(ph, nc, [128, 512], F32, n=2, name="l2")
        GS = sb(ph, nc, [128, 512], F32, n=2, name="gs")
        HT = sb(ph, nc, [128, 1024], F32, n=2, name="ht")
        pG = ps(ph, nc, [128, 512], F32, n=2, name="pG")
        pL = ps(ph, nc, [128, 512], F32, n=2, name="pL")
        pO = ps(ph, nc, [128, 512], F32, n=4, name="pO")
        dOUT = Buf("dOUT")
        for i in range(nb1):
            br = b1r[i % 2]
            k.dma("sp", br.t[:], d["moe_b1"][layer, i * 128:(i + 1) * 128, :], w=[br.b])
            k.tr(pG[i % 2].t[:, 0:128], br.t[:], c.ident_f.t[:], r=[br.b], w=[pG[i % 2].b])
            k.cp("dve", B1T.t[:, i * 128:(i + 1) * 128], pG[i % 2].t[:, 0:128], r=[pG[i % 2].b], wp=[B1T.b])
        wi = 0
        io = 0
        for blk in range(S // MB):
            t0 = blk * NTB
            rows = slice(blk * MB, (blk + 1) * MB)
            k.dma("sp", ACC.t[:], d["ACCd"][rows, :].rearrange("(t p) n -> p t n", p=128), w=[ACC.b])
            k.dma("sp", XT2b.t[:], d["XT2"][:, :, rows], w=[XT2b.b])
            k.dma("sp", CMB.t[:], d["COMB"][rows, :].rearrange("(t p) e -> p t e", p=128), w=[CMB.b])
            for e in range(E):
                w1 = W1[wi % 2]
                wi += 1
                w1v = d["moe_w1"][layer, e].rearrange("(k p) n -> p k n", p=128)
                k.dma("pool", w1.t[:, 0:4, :], w1v[:, 0:4, :], wp=[w1.b])
                k.dma("pool", w1.t[:, 4:8, :], w1v[:, 4:8, :], wp=[w1.b])
                w2_loaded = False
                for ht in range(NH):
                    actt = ACTT[io % 2]
                    for pr in range(8):
                        pg, pl = pG[pr % 2], pL[pr % 2]
                        g1, s1, l1, l2, gs = (X[pr % 2] for X in (G1, S1, L1, L2, GS))
                        for kk in range(8):
                            k.mm(pg.t[:], w1.t[:, kk, pr * 128:(pr + 1) * 128], XT2b.t[:, kk, ht * 512:(ht + 1) * 512], kk == 0, kk == 7,
                                 r=[w1.b, XT2b.b], w=[pg.b] if kk == 0 else (), wp=() if kk == 0 else [pg.b])
                        for kk in range(8):
                            k.mm(pl.t[:], w1.t[:, kk, 1024 + pr * 128:1024 + (pr + 1) * 128], XT2b.t[:, kk, ht * 512:(ht + 1) * 512], kk == 0, kk == 7,
                                 r=[w1.b, XT2b.b], w=[pl.b] if kk == 0 else (), wp=() if kk == 0 else [pl.b])
                        bg = B1T.t[:, e * 16 + pr:e * 16 + pr + 1]
                        bl = B1T.t[:, e * 16 + 8 + pr:e * 16 + 8 + pr + 1]
                        k.ts("dve", g1.t[:], pg.t[:], bg, ALU.add, 7.0, ALU.min, r=[pg.b, B1T.b], w=[g1.b])
                        k.act(s1.t[:], g1.t[:], AF.Sigmoid, r=[g1.b], w=[s1.b], scale=1.702)
                        k.act(l1.t[:], pl.t[:], AF.Identity, r=[pl.b, B1T.b], w=[l1.b], bias=bl)
                        k.ts("dve", l2.t[:], l1.t[:], 7.0, ALU.min, -7.0, ALU.max, r=[l1.b], w=[l2.b])
                        k.tt("pool", gs.t[:], g1.t[:], s1.t[:], ALU.mult, r=[g1.b, s1.b], w=[gs.b])
                        k.stt(actt.t[:, pr, :], l2.t[:], 1.0, gs.t[:], ALU.add, ALU.mult, r=[l2.b, gs.b], wp=[actt.b])
                    if not w2_loaded:
                        k.dma("pool", W2.t[:], d["moe_w2"][layer, e].rearrange("(k p) n -> p k n", p=128), w=[W2.b])
                        w2_loaded = True
                    for sub in range(4):
                        tl = ht * 4 + sub
                        for half in range(2):
                            po = pO[io % 4]
                            io += 1
                            for jj in range(8):
                                k.mm(po.t[:], actt.t[:, jj, sub * 128:(sub + 1) * 128], W2.t[:, jj, half * 512:(half + 1) * 512], jj == 0, jj == 7,
                                     r=[actt.b, W2.b], w=[po.b] if jj == 0 else (), wp=() if jj == 0 else [po.b])
                            acc = ACC.t[:, tl, half * 512:(half + 1) * 512]
                            k.stt(acc, po.t[:], CMB.t[:, tl, e:e + 1], acc, ALU.mult, ALU.add, r=[po.b, CMB.b, ACC.b], wp=[ACC.b])
            for tl in range(NTB):
                t = t0 + tl
                ht_ = HT[tl % 2]
                k.dma("sp", ht_.t[:], hin[t * 128:(t + 1) * 128, :], w=[ht_.b])
                k.tt("dve", ACC.t[:, tl, :], ACC.t[:, tl, :], MOD5.t[:], ALU.mult, r=[ACC.b, MOD5.b], wp=[ACC.b])
                k.tt("pool", ht_.t[:], ht_.t[:], ACC.t[:, tl, :], ALU.add, r=[ht_.b, ACC.b], w=[ht_.b])
                k.dma("sp", hout[t * 128:(t + 1) * 128, :], ht_.t[:], r=[ht_.b], wp=[dOUT])
        P.end_phase()


def phase_f(c):
    nc, P, k, d, cfg = c.nc, c.P, c.k, c.d, c.cfg
    S = cfg.S
    NG = S // 512
    with ExitStack() as ph:
        W = sb(ph, nc, [128, 8, 2048], BF16, name="Wc1")
        wv = d["conv_w1"].rearrange("(k p) n -> p k n", p=128)
        k.dma("pool", W.t[:, 0:4, :], wv[:, 0:4, :], wp=[W.b])
        k.dma("pool", W.t[:, 4:8, :], wv[:, 4:8, :], wp=[W.b])
        A = make_A(c, ph, "norm1_g", 1, 1024, c.MOD)
        b1r = sb(ph, nc, [16, 128], F32, name="b1r")
        CB1 = sb(ph, nc, [128, 16], F32, name="cb1")
        pB = ps(ph, nc, [128, 16], F32, name="pB")
        k.dma("sp", b1r.t[:], d["conv_b1"], w=[b1r.b])
        k.tr(pB.t[:], b1r.t[:], c.ident_f.t[0:16, 0:16], r=[b1r.b], w=[pB.b])
        k.cp("dve", CB1.t[:], pB.t[:], r=[pB.b], w=[CB1.b])
        Z = sb(ph, nc, [128, 8, 16], BF16, name="z")
        k.memset("dve", Z.t[:], 0.0, w=[Z.b])
        dGT = Buf("dGT")
        k.dma("sp", d["GT"][:, :, 0:16], Z.t[:], r=[Z.b], wp=[dGT])
        k.dma("sp", d["GT"][:, :, S + 16:S + 32], Z.t[:], r=[Z.b], wp=[dGT])
        XIN = sb(ph, nc, [128, 1024], F32, n=2, name="xin")
        tmps = [{"ss": sb(ph, nc, [128, 16], F32, name="ss"), "t1": sb(ph, nc, [128, 1024], F32, name="t1")} for _ in range(2)]
        XM = sb(ph, nc, [128, 1024], BF16, n=2, name="xm")
        XTG = sb(ph, nc, [128, 8, 512], BF16, n=2, name="xtg")
        SG = sb(ph, nc, [128, 512], F32, n=2, name="sg")
        GTs = sb(ph, nc, [128, 8, 512], BF16, n=2, name="gts")
        pT = ps(ph, nc, [128, 1024], BF16, name="pT")
        pA = ps(ph, nc, [128, 512], F32, n=2, name="pA")
        pGt = ps(ph, nc, [128, 512], F32, n=2, name="pGt")
        it = 0
        for g in range(NG):
            xtg, gts = XTG[g % 2], GTs[g % 2]
            for j in range(4):
                t = 4 * g + j
                xin, xm = XIN[it % 2], XM[it % 2]
                it += 1
                k.dma("sp", xin.t[:], d["H2"][t * 128:(t + 1) * 128, :], w=[xin.b])
                norm_mod(c, xin, A, c.MOD.t[:, 0:1024], c.MOD.b, [(xm.t[:], "pool", xm)], tmps[it % 2], j)
                for kk in range(8):
                    k.tr(pT.t[:, kk * 128:(kk + 1) * 128], xm.t[:, kk * 128:(kk + 1) * 128], c.ident_b.t[:], r=[xm.b],
                         w=[pT.b] if kk == 0 else (), wp=() if kk == 0 else [pT.b])
                k.cp("act", xtg.t[:, :, j * 128:(j + 1) * 128], pT.t[:].rearrange("p (k t) -> p k t", k=8), r=[pT.b], wp=[xtg.b])
            for cp_ in range(8):
                pa, pg, sg = pA[cp_ % 2], pGt[cp_ % 2], SG[cp_ % 2]
                for kk in range(8):
                    k.mm(pa.t[:], W.t[:, kk, cp_ * 128:(cp_ + 1) * 128], xtg.t[:, kk, :], kk == 0, kk == 7, r=[W.b, xtg.b],
                         w=[pa.b] if kk == 0 else (), wp=() if kk == 0 else [pa.b])
                for kk in range(8):
                    k.mm(pg.t[:], W.t[:, kk, 1024 + cp_ * 128:1024 + (cp_ + 1) * 128], xtg.t[:, kk, :], kk == 0, kk == 7, r=[W.b, xtg.b],
                         w=[pg.b] if kk == 0 else (), wp=() if kk == 0 else [pg.b])
                k.act(sg.t[:], pg.t[:], AF.Sigmoid, r=[pg.b, CB1.b], w=[sg.b], bias=CB1.t[:, 8 + cp_:9 + cp_])
                k.stt(gts.t[:, cp_, :], pa.t[:], CB1.t[:, cp_:cp_ + 1], sg.t[:], ALU.add, ALU.mult, r=[pa.b, sg.b, CB1.b], wp=[gts.b])
            k.dma("sp", d["GT"][:, :, 16 + g * 512:16 + (g + 1) * 512], gts.t[:], r=[gts.b], wp=[dGT])
        P.end_phase()


def phase_g1(c):
    nc, P, k, d, cfg = c.nc, c.P, c.k, c.d, c.cfg
    S = cfg.S
    NG = S // 512
    with ExitStack() as ph:
        dwr = sb(ph, nc, [32, 1024], F32, name="dwr")
        DWT = sb(ph, nc, [128, 8, 31], F32, name="dwt")
        dbr = sb(ph, nc, [8, 128], F32, name="dbr")
        DWB = sb(ph, nc, [128, 8], F32, name="dwb")
        DG = sb(ph, nc, [128, 8, 31, 128], BF16, name="dg")
        pD = ps(ph, nc, [128, 8, 32], F32, name="pD")
        pB = ps(ph, nc, [128, 8], F32, name="pB")
        k.dma("sp", dwr.t[0:31, :], d["conv_dw"], w=[dwr.b])
        for cch in range(8):
            k.tr(pD.t[:, cch, 0:31], dwr.t[0:31, cch * 128:(cch + 1) * 128], c.ident_f.t[0:31, 0:31], r=[dwr.b],
                 w=[pD.b] if cch == 0 else (), wp=() if cch == 0 else [pD.b])
        k.cp("dve", DWT.t[:], pD.t[:, :, 0:31], r=[pD.b], w=[DWT.b])
        k.dma("sp", dbr.t[:], d["conv_dw_b"], w=[dbr.b])
        k.tr(pB.t[:], dbr.t[:], c.ident_f.t[0:8, 0:8], r=[dbr.b], w=[pB.b])
        k.cp("dve", DWB.t[:], pB.t[:], r=[pB.b], w=[DWB.b])
        n_ = 0
        for cch in range(8):
            for j in range(31):
                k.ts("dve" if n_ % 2 else "pool", DG.t[:, cch, j, :], c.ident_f.t[:], DWT.t[:, cch, j:j + 1], ALU.mult, r=[DWT.b, c.ident_f.b], wp=[DG.b])
                n_ += 1
        GTw = sb(ph, nc, [128, 8, 544], BF16, n=2, name="gtw")
        Ys = sb(ph, nc, [128, 8, 512], F32, n=2, name="ys")
        pC = ps(ph, nc, [128, 512], F32, n=4, name="pC")
        dYD = Buf("dYD")
        for g in range(NG):
            gtw, ys = GTw[g % 2], Ys[g % 2]
            k.dma("sp", gtw.t[:], d["GT"][:, :, g * 512:g * 512 + 544], w=[gtw.b])
            for cch in range(8):
                pc = pC[cch % 4]
                for j in range(31):
                    k.mm(pc.t[:], DG.t[:, cch, j, :], gtw.t[:, cch, j + 1:j + 513], j == 0, j == 30, r=[DG.b, gtw.b],
                         w=[pc.b] if j == 0 else (), wp=() if j == 0 else [pc.b])
                k.act(ys.t[:, cch, :], pc.t[:], AF.Identity, r=[pc.b, DWB.b], wp=[ys.b], bias=DWB.t[:, cch:cch + 1])
            k.dma("sp", d["YD"][:, :, g * 512:(g + 1) * 512], ys.t[:], r=[ys.b], wp=[dYD])
        P.end_phase()


I32 = mybir.dt.int32


def pool_dma_op(P, fn, reads=(), writes=(), wpart=(), key=None):
    o = Op("pool", fn, P.phase)
    o.is_dma = True
    if key is None:
        key = (list(writes) + list(wpart))[0]
    if key not in P.keymap:
        P.keymap[key] = len(P.keymap)
        assert len(P.keymap) <= NDSEM
    o.key = P.keymap[key]
    P._deps(o, reads, writes, wpart)
    P.ops["pool"].append(o)
    P.order.append(o)
    return o


def phase_route(c, layer, TEi):
    nc, P, k, d, cfg = c.nc, c.P, c.k, c.d, c.cfg
    S, E, NT, NTILE, NSLOT = cfg.S, cfg.E, cfg.NT, cfg.NTILE, cfg.NSLOT
    with ExitStack() as ph:
        MKf = sb(ph, nc, [128, NT, E], F32, name="mkf")
        MKb = sb(ph, nc, [128, NT, E], BF16, name="mkb")
        CMB = sb(ph, nc, [128, NT, E], F32, name="cmb")
        UTf = sb(ph, nc, [128, 128], F32, name="utf")
        UT = sb(ph, nc, [128, 128], BF16, name="ut")
        ONb = sb(ph, nc, [128, 128], BF16, name="onb")
        IOTA = sb(ph, nc, [128, 1], F32, name="iota")
        TH = sb(ph, nc, [1, E * 16], F32, name="th")
        J5 = sb(ph, nc, [1, NTILE * E], F32, name="j5")
        dSLOT, dInit = Buf("dSLOT"), Buf("dInit")
        k.dma("sp", d["SLOT"], d["k_slotinit"], w=[dSLOT, dInit])
        k.dma("sp", MKf.t[:], d["MK"].rearrange("(t p) e -> p t e", p=128), w=[MKf.b])
        k.dma("sp", CMB.t[:], d["COMB"].rearrange("(t p) e -> p t e", p=128), w=[CMB.b])
        k.dma("sp", UTf.t[:], d["k_ut"], w=[UTf.b])
        k.dma("sp", IOTA.t[:], d["k_iota"], w=[IOTA.b])
        k.dma("sp", TH.t[:], d["k_th"], w=[TH.b])
        k.dma("sp", J5.t[:], d["k_j512"], w=[J5.b])
        k.cp("dve", UT.t[:], UTf.t[:], r=[UTf.b], w=[UT.b])
        k.cp("pool", MKb.t[:], MKf.t[:], r=[MKf.b], w=[MKb.b])
        k.memset("dve", ONb.t[:], 1.0, w=[ONb.b])
        pC = ps(ph, nc, [1, E], F32, name="pC")
        pSB = ps(ph, nc, [128, E], F32, name="pSB")
        pR = ps(ph, nc, [128, E], F32, n=2, name="pR")
        V = sb(ph, nc, [1, 8 * E], F32, name="v")
        C16 = sb(ph, nc, [1, E * 16], F32, name="c16")
        CJ = sb(ph, nc, [1, NTILE * E], F32, name="cj")
        TEf = sb(ph, nc, [1, NTILE], F32, name="tef")
        SEGB = sb(ph, nc, [128, E], F32, name="segb")
        for t in range(NT):
            k.mm(pC.t[:], ONb.t[:, 0:1], MKb.t[:, t, :], t == 0, t == NT - 1, r=[ONb.b, MKb.b], w=[pC.b] if t == 0 else (), wp=() if t == 0 else [pC.b])
        cnt, ntl, c512, inc, segs, one = (V.t[:, i * E:(i + 1) * E] for i in range(6))
        k.cp("dve", cnt, pC.t[:], r=[pC.b], wp=[V.b])
        k.tt("dve", C16.t[:].rearrange("o (e m) -> o e m", m=16), cnt.unsqueeze(2).to_broadcast([1, E, 16]), TH.t[:].rearrange("o (e m) -> o e m", m=16), ALU.is_gt,
             r=[V.b, TH.b], w=[C16.b])
        k.red(ntl, C16.t[:].rearrange("o (e m) -> o e m", m=16), r=[C16.b], wp=[V.b])
        k.ts("dve", c512, ntl, 512.0, ALU.mult, r=[V.b], wp=[V.b])
        k.memset("dve", one, 1.0, wp=[V.b])
        P.op("dve", (lambda e_, o=inc, a=one, b_=c512: e_.tensor_tensor_scan(out=o, data0=a, data1=b_, initial=0.0, op0=ALU.mult, op1=ALU.add)),
             reads=[V.b], wpart=[V.b])
        k.tt("dve", segs, inc, c512, ALU.subtract, r=[V.b], wp=[V.b])
        k.tt("dve", CJ.t[:].rearrange("o (j e) -> o j e", e=E), segs.unsqueeze(1).to_broadcast([1, NTILE, E]), J5.t[:].rearrange("o (j e) -> o j e", e=E), ALU.is_le,
             r=[V.b, J5.b], w=[CJ.b])
        k.red(TEf.t[:], CJ.t[:].rearrange("o (j e) -> o j e", e=E), r=[CJ.b], w=[TEf.b])
        k.ts("dve", TEf.t[:], TEf.t[:], -1.0, ALU.add, 0.0, ALU.max, r=[TEf.b], w=[TEf.b])
        IDXW, IDXB = TEi
        KP = sb(ph, nc, [128, 9], F32, name="kp")
        k.dma("sp", KP.t[:], d["k_kp"], w=[KP.b])
        pTE = ps(ph, nc, [128, NTILE], F32, name="pTE")
        TEb = sb(ph, nc, [128, NTILE], F32, name="teb")
        XW = sb(ph, nc, [128, NTILE, 8], F32, name="xw")
        k.mm(pTE.t[:], c.ones_f.t[0:1, :], TEf.t[:], True, True, r=[TEf.b, c.ones_f.b], w=[pTE.b])
        k.cp("dve", TEb.t[:], pTE.t[:], r=[pTE.b], w=[TEb.b])
        for kk in range(8):
            k.ts("dve", XW.t[:, :, kk], TEb.t[:], 1024.0, ALU.mult, KP.t[:, kk:kk + 1], ALU.add, r=[TEb.b, KP.b], wp=[XW.b])
        if layer:
            k.ts("dve", XW.t[:], XW.t[:], float(layer * E * 1024), ALU.add, r=[XW.b], w=[XW.b])
        k.cp("dve", IDXW.t[:], XW.t[:], r=[XW.b], w=[IDXW.b])
        k.ts("dve", TEb.t[:], TEb.t[:], 16.0, ALU.mult, KP.t[:, 8:9], ALU.add, r=[TEb.b, KP.b], w=[TEb.b])
        if layer:
            k.ts("dve", TEb.t[:], TEb.t[:], float(layer * E * 16), ALU.add, r=[TEb.b], w=[TEb.b])
        k.cp("dve", IDXB.t[:], TEb.t[:], r=[TEb.b], w=[IDXB.b])
        k.mm(pSB.t[:], c.ones_f.t[0:1, :], segs, True, True, r=[V.b, c.ones_f.b], w=[pSB.b])
        k.cp("dve", SEGB.t[:], pSB.t[:], r=[pSB.b], w=[SEGB.b])
        POS = sb(ph, nc, [128, E], F32, n=2, name="pos")
        T8 = sb(ph, nc, [128, 8], F32, n=2, name="t8")
        OH = sb(ph, nc, [128, E], F32, n=2, name="oh")
        JK = sb(ph, nc, [128, E], F32, n=2, name="jk")
        P4 = sb(ph, nc, [128, 4], F32, n=2, name="p4")
        P4i = sb(ph, nc, [128, 4], I32, n=2, name="p4i")
        SR = sb(ph, nc, [128, 4, 2], F32, n=2, name="sr")
        for i in range(NT):
            pr = pR[i % 2]
            pos, t8, p4, p4i, sr = POS[i % 2], T8[i % 2], P4[i % 2], P4i[i % 2], SR[i % 2]
            for ip in range(i):
                k.mm(pr.t[:], ONb.t[:], MKb.t[:, ip, :], ip == 0, False, r=[ONb.b, MKb.b], w=[pr.b] if ip == 0 else (), wp=() if ip == 0 else [pr.b])
            k.mm(pr.t[:], UT.t[:], MKb.t[:, i, :], i == 0, True, r=[UT.b, MKb.b], w=[pr.b] if i == 0 else (), wp=() if i == 0 else [pr.b])
            k.tt("dve", pos.t[:], pr.t[:], SEGB.t[:], ALU.add, r=[pr.b, SEGB.b], w=[pos.b])
            P.op("dve", (lambda e_, o=t8.t[:], a=CMB.t[:, i, :]: e_.max(out=o, in_=a)), reads=[CMB.b], writes=[t8.b])
            for kq in range(4):
                oh, jk = OH[kq % 2], JK[kq % 2]
                k.ts("dve", oh.t[:], CMB.t[:, i, :], t8.t[:, kq:kq + 1], ALU.is_equal, r=[CMB.b, t8.b], w=[oh.b])
                k.stt(jk.t[:], oh.t[:], 1.0, pos.t[:], ALU.mult, ALU.mult, r=[oh.b, pos.b], w=[jk.b], wp=[p4.b], accum=p4.t[:, kq:kq + 1])
                k.ts("pool", sr.t[:, kq, 0:1], IOTA.t[:], float(i * 128), ALU.add, r=[IOTA.b], wp=[sr.b])
                k.cp("pool", sr.t[:, kq, 1:2], t8.t[:, kq:kq + 1], r=[t8.b], wp=[sr.b])
            k.ts("dve", p4.t[:], p4.t[:], float(NSLOT - 1), ALU.min, r=[p4.b], w=[p4.b])
            k.cp("dve", p4i.t[:], p4.t[:], r=[p4.b], w=[p4i.b])
            for kq in range(4):
                def sca(e_, off=p4i.t[:, kq:kq + 1], src=sr.t[:, kq, :]):
                    return e_.indirect_dma_start(out=d["SLOT"], out_offset=bass.IndirectOffsetOnAxis(ap=off, axis=0), in_=src, in_offset=None)
                pool_dma_op(P, sca, reads=[p4i.b, sr.b, dInit], wpart=[dSLOT])
        P.end_phase()


def phase_smoe(c, layer, MOD5, TEi):
    nc, P, k, d, cfg = c.nc, c.P, c.k, c.d, c.cfg
    S, E, NTILE = cfg.S, cfg.E, cfg.NTILE
    IDXW, IDXB = TEi
    hin = d["H1"] if layer == 0 else d["H3"]
    hout = d["H2"] if layer == 0 else d["out"]
    w1tab = d["moe_w1"].rearrange("l e k n -> (l e k) n")
    w2tab = d["moe_w2"].rearrange("l e k n -> (l e k) n")
    b1tab = d["moe_b1"].rearrange("l r f -> (l r) f")
    with ExitStack() as ph:
        Z = sb(ph, nc, [128, 1024], BF16, name="z")
        W1 = sb(ph, nc, [128, 8, 2048], BF16, n=2, name="w1")
        W2 = sb(ph, nc, [128, 8, 1024], BF16, name="w2")
        B1r = sb(ph, nc, [128, 128], F32, n=2, name="b1r")
        B1c = sb(ph, nc, [128, 16], F32, n=2, name="b1c")
        SLt = sb(ph, nc, [128, 4, 2], F32, n=2, name="slt")
        TKi = sb(ph, nc, [128, 4], I32, n=2, name="tki")
        XG = sb(ph, nc, [128, 4, 1024], BF16, n=2, name="xg")
        XT = sb(ph, nc, [128, 8, 512], BF16, n=2, name="xt")
        ACTT = sb(ph, nc, [128, 8, 512], BF16, n=2, name="actt")
        G1 = sb(ph, nc, [128, 512], F32, n=3, name="g1")
        S1 = sb(ph, nc, [128, 512], F32, n=3, name="s1")
        L2 = sb(ph, nc, [128, 512], F32, n=3, name="l2")
        GS = sb(ph, nc, [128, 512], F32, n=3, name="gs")
        OS = sb(ph, nc, [128, 1024], F32, n=4, name="os")
        pT = ps(ph, nc, [128, 1024], BF16, n=2, name="pT")
        pG = ps(ph, nc, [128, 512], F32, n=2, name="pG")
        pL = ps(ph, nc, [128, 512], F32, n=2, name="pL")
        pO = ps(ph, nc, [128, 512], F32, n=2, name="pO")
        dACC, dXM2z, dOUT = Buf("dACCs"), Buf("dXM2z"), Buf("dOUT")
        k.memset("dve", Z.t[:], 0.0, w=[Z.b])
        k.dma("sp", d["XM2"][S:S + 128, :], Z.t[:], r=[Z.b], w=[dXM2z])

        def gather(out_ap, tab, idx_ap, reads, wslot, part=False):
            def g(e_):
                return e_.indirect_dma_start(out=out_ap, out_offset=None, in_=tab, in_offset=bass.IndirectOffsetOnAxis(ap=idx_ap, axis=0))
            return pool_dma_op(P, g, reads=reads, writes=() if part else [wslot], wpart=[wslot] if part else ())

        def loads(j):
            w1, b1r, slt, tki, xg = W1[j % 2], B1r[j % 2], SLt[j % 2], TKi[j % 2], XG[j % 2]
            k.dma("sp", slt.t[:], d["SLOT"][j * 512:(j + 1) * 512, :].rearrange("(s p) c -> p s c", p=128), w=[slt.b])
            k.cp("dve", tki.t[:], slt.t[:, :, 0], r=[slt.b], w=[tki.b])
            for kk in range(8):
                gather(w1.t[:, kk, :], w1tab, IDXW.t[:, j, kk:kk + 1], [IDXW.b], w1.b, part=True)
            gather(b1r.t[:], b1tab, IDXB.t[:, j:j + 1], [IDXB.b], b1r.b)
            for sub in range(4):
                gather(xg.t[:, sub, :], d["XM2"], tki.t[:, sub:sub + 1], [tki.b, dXM2z], xg.b, part=True)

        io = 0
        loads(0)
        for j in range(NTILE):
            if j + 1 < NTILE:
                loads(j + 1)
            w1, b1r, b1c, slt, tki, xg, xt, actt = (X[j % 2] for X in (W1, B1r, B1c, SLt, TKi, XG, XT, ACTT))
            k.tr(pG[0].t[:, 0:16], b1r.t[0:16, :], c.ident_f.t[0:16, 0:16], r=[b1r.b], w=[pG[0].b])
            k.cp("dve", b1c.t[:, 0:8], pG[0].t[:, 0:8], r=[pG[0].b], wp=[b1c.b])
            k.ts("dve", b1c.t[:, 8:16], pG[0].t[:, 8:16], 1.0, ALU.add, r=[pG[0].b], wp=[b1c.b])
            for sub in range(4):
                pt = pT[sub % 2]
                for kk in range(8):
                    k.tr(pt.t[:, kk * 128:(kk + 1) * 128], xg.t[:, sub, kk * 128:(kk + 1) * 128], c.ident_b.t[:], r=[xg.b],
                         w=[pt.b] if kk == 0 else (), wp=() if kk == 0 else [pt.b])
                k.cp("act" if sub % 2 else "dve", xt.t[:, :, sub * 128:(sub + 1) * 128], pt.t[:].rearrange("p (k t) -> p k t", k=8), r=[pt.b], wp=[xt.b])
            for pr in range(8):
                pg, pl = pG[pr % 2], pL[pr % 2]
                g1, s1, l2, gs = (X[pr % 3] for X in (G1, S1, L2, GS))
                for kk in range(8):
                    k.mm(pg.t[:], w1.t[:, kk, pr * 128:(pr + 1) * 128], xt.t[:, kk, :], kk == 0, kk == 7,
                         r=[w1.b, xt.b], w=[pg.b] if kk == 0 else (), wp=() if kk == 0 else [pg.b])
                for kk in range(8):
                    k.mm(pl.t[:], w1.t[:, kk, 1024 + pr * 128:1024 + (pr + 1) * 128], xt.t[:, kk, :], kk == 0, kk == 7,
                         r=[w1.b, xt.b], w=[pl.b] if kk == 0 else (), wp=() if kk == 0 else [pl.b])
                k.ts("dve", g1.t[:], pg.t[:], b1c.t[:, pr:pr + 1], ALU.add, 7.0, ALU.min, r=[pg.b, b1c.b], w=[g1.b])
                k.act(s1.t[:], g1.t[:], AF.Sigmoid, r=[g1.b], w=[s1.b], scale=1.702)
                k.ts("dve", l2.t[:], pl.t[:], b1c.t[:, 8 + pr:9 + pr], ALU.add, 8.0, ALU.min, r=[pl.b, b1c.b], w=[l2.b])
                k.tt("dve", gs.t[:], g1.t[:], s1.t[:], ALU.mult, r=[g1.b, s1.b], w=[gs.b])
                k.stt(actt.t[:, pr, :], l2.t[:], -6.0, gs.t[:], ALU.max, ALU.mult, r=[l2.b, gs.b], wp=[actt.b])
            for kk in range(8):
                gather(W2.t[:, kk, :], w2tab, IDXW.t[:, j, kk:kk + 1], [IDXW.b], W2.b, part=True)
            for sub in range(4):
                os_ = OS[(4 * j + sub) % 4]
                for half in range(2):
                    po = pO[io % 2]
                    io += 1
                    for jj in range(8):
                        k.mm(po.t[:], actt.t[:, jj, sub * 128:(sub + 1) * 128], W2.t[:, jj, half * 512:(half + 1) * 512], jj == 0, jj == 7,
                             r=[actt.b, W2.b], w=[po.b] if jj == 0 else (), wp=() if jj == 0 else [po.b])
                    k.act(os_.t[:, half * 512:(half + 1) * 512], po.t[:], AF.Copy, r=[po.b, slt.b], wp=[os_.b], scale=slt.t[:, sub, 1:2])

                def sca(e_, off=tki.t[:, sub:sub + 1], src=os_.t[:]):
                    return e_.indirect_dma_start(out=d["ACCd"], out_offset=bass.IndirectOffsetOnAxis(ap=off, axis=0), in_=src, in_offset=None,
                                                 compute_op=ALU.add)
                pool_dma_op(P, sca, reads=[tki.b, os_.b], writes=[dACC])
        for t in range(S // 128):
            ht_, at_ = OS[t % 2], OS[2 + t % 2]
            rows = slice(t * 128, (t + 1) * 128)
            k.dma("sp", ht_.t[:], hin[rows, :], w=[ht_.b])
            k.dma("sp", at_.t[:], d["ACCd"][rows, :], r=[dACC], w=[at_.b])
            k.tt("dve", at_.t[:], at_.t[:], MOD5.t[:], ALU.mult, r=[at_.b, MOD5.b], w=[at_.b])
            k.tt("pool", ht_.t[:], ht_.t[:], at_.t[:], ALU.add, r=[ht_.b, at_.b], w=[ht_.b])
            k.dma("sp", hout[rows, :], ht_.t[:], r=[ht_.b], wp=[dOUT])
        P.end_phase()
```

```python
from contextlib import ExitStack
import numpy as np
import ml_dtypes
import concourse.bass as bass
import concourse.mybir as mybir
from concourse.bass_utils import run_bass_kernel_spmd

F32 = mybir.dt.float32
BF16 = mybir.dt.bfloat16
AF = mybir.ActivationFunctionType
ALU = mybir.AluOpType
AX = mybir.AxisListType

COMPUTE = ("pe", "act", "dve", "pool")
ALLENG = ("pe", "act", "dve", "pool", "sp")
NDSEM = 72


class Buf:
    __slots__ = ("name", "writers", "readers")

    def __init__(self, name):
        self.name = name
        self.writers = []
        self.readers = []


class Op:
    __slots__ = ("eng", "fn", "raw", "oth", "signal", "tok_sem", "tok_val", "is_dma", "key", "phase")

    def __init__(self, eng, fn, phase):
        self.eng = eng
        self.fn = fn
        self.raw = []
        self.oth = []
        self.signal = False
        self.tok_sem = None
        self.tok_val = 0
        self.is_dma = False
        self.key = None
        self.phase = phase


class Prog:
    def __init__(self, nc, es):
        self.nc = nc
        self.phase = 0
        self.esem = {e: es.enter_context(nc.semaphore("s_" + e)) for e in COMPUTE}
        self.ecnt = {e: 0 for e in COMPUTE}
        self.dsem = [es.enter_context(nc.semaphore("d%d" % i)) for i in range(NDSEM)]
        self.dcnt = [0] * NDSEM
        self.seen = {e: {} for e in ALLENG}
        self._reset()
        self.nops = 0

    def _reset(self):
        self.ops = {e: [] for e in ALLENG}
        self.order = []
        self.keymap = {}
        self.last = {}

    def buf(self, name="b"):
        return Buf(name)

    def _deps(self, op, reads, writes, wpart):
        ph = self.phase
        for b in reads:
            for w in b.writers:
                if w.phase == ph:
                    op.raw.append(w)
        for b in list(writes) + list(wpart):
            for r in b.readers:
                if r.phase == ph:
                    op.oth.append(r)
        for b in writes:
            for w in b.writers:
                if w.phase == ph:
                    op.oth.append(w)
        for b in reads:
            b.readers.append(op)
        for b in writes:
            b.writers = [op]
            b.readers = []
        for b in wpart:
            if b.readers:
                b.writers = [op]
                b.readers = []
            else:
                b.writers.append(op)

    def op(self, eng, fn, reads=(), writes=(), wpart=()):
        o = Op(eng, fn, self.phase)
        self._deps(o, reads, writes, wpart)
        self.ops[eng].append(o)
        self.order.append(o)
        self.last[eng] = o
        return o

    def dma(self, q, out, in_, reads=(), writes=(), wpart=(), key=None, **kw):
        def fn(e, out=out, in_=in_, kw=kw):
            return e.dma_start(out=out, in_=in_, **kw)
        o = Op(q, fn, self.phase)
        o.is_dma = True
        if key is None:
            ws = list(writes) + list(wpart)
            key = ws[0]
        if key not in self.keymap:
            self.keymap[key] = len(self.keymap)
            assert len(self.keymap) <= NDSEM, "too many DMA keys in phase"
        o.key = self.keymap[key]
        self._deps(o, reads, writes, wpart)
        self.ops[q].append(o)
        self.order.append(o)
        return o

    def end_phase(self):
        nc = self.nc
        lasts = [self.last[e] for e in COMPUTE if e in self.last]
        lastd = {}
        for o in self.order:
            if o.is_dma:
                lastd[o.key] = o
        for e in ALLENG:
            o = Op(e, (lambda eng: eng.nop()), self.phase)
            o.raw = list(lasts) + list(lastd.values())
            self.ops[e].append(o)
            self.order.append(o)
        for o in self.order:
            for d in o.raw:
                if d.is_dma:
                    continue
                if d.eng == o.eng and o.eng == "pe" and not o.is_dma:
                    continue
                d.signal = True
            for d in o.oth:
                if d.is_dma:
                    continue
                if d.eng == o.eng and not o.is_dma:
                    continue
                d.signal = True
        for e in COMPUTE:
            for o in self.ops[e]:
                if o.is_dma:
                    continue
                if o.signal:
                    self.ecnt[e] += 1
                    o.tok_sem = self.esem[e]
                    o.tok_val = self.ecnt[e]
        for o in self.order:
            if o.is_dma:
                self.dcnt[o.key] += 16
                o.tok_sem = self.dsem[o.key]
                o.tok_val = self.dcnt[o.key]
        self.nops += len(self.order)

        with nc.Block() as block:
            def run(ename):
                def body(eng):
                    seen = self.seen[ename]
                    for o in self.ops[ename]:
                        need = {}
                        for d in o.raw:
                            if d.tok_sem is None:
                                continue
                            if (not d.is_dma) and d.eng == ename and ename == "pe" and not o.is_dma:
                                continue
                            s = d.tok_sem
                            if need.get(s.num, (None, 0))[1] < d.tok_val:
                                need[s.num] = (s, d.tok_val)
                        for d in o.oth:
                            if d.tok_sem is None:
                                continue
                            if (not d.is_dma) and d.eng == ename and not o.is_dma:
                                continue
                            s = d.tok_sem
                            if need.get(s.num, (None, 0))[1] < d.tok_val:
                                need[s.num] = (s, d.tok_val)
                        for s, v in need.values():
                            if seen.get(s.num, 0) < v:
                                eng.wait_ge(s, v)
                                seen[s.num] = v
                        ins = o.fn(eng)
                        if o.is_dma:
                            ins.then_inc(o.tok_sem, 16)
                        elif o.signal:
                            ins.then_inc(o.tok_sem, 1)
                return body

            block.tensor(run("pe"))
            block.scalar(run("act"))
            block.vector(run("dve"))
            block.gpsimd(run("pool"))
            block.sync(run("sp"))
        self.phase += 1
        self._reset()


class Slot:
    __slots__ = ("t", "b")

    def __init__(self, t, b):
        self.t = t
        self.b = b


class Ctx:
    pass


class K:
    def __init__(self, P):
        self.P = P

    def ts(self, eng, out, in0, s1, op0, s2=None, op1=None, r=(), w=(), wp=(), accum=None):
        if op1 is None:
            if accum is None:
                f = lambda e: e.tensor_scalar(out=out, in0=in0, scalar1=s1, scalar2=None, op0=op0)
            else:
                f = lambda e: e.tensor_scalar(out=out, in0=in0, scalar1=s1, scalar2=None, op0=op0, accum_out=accum)
        else:
            f = lambda e: e.tensor_scalar(out=out, in0=in0, scalar1=s1, scalar2=s2, op0=op0, op1=op1)
        return self.P.op(eng, f, reads=r, writes=w, wpart=wp)

    def tt(self, eng, out, in0, in1, op, r=(), w=(), wp=()):
        return self.P.op(eng, lambda e: e.tensor_tensor(out=out, in0=in0, in1=in1, op=op), reads=r, writes=w, wpart=wp)

    def stt(self, out, in0, scalar, in1, op0, op1, r=(), w=(), wp=(), accum=None):
        if accum is None:
            f = lambda e: e.scalar_tensor_tensor(out=out, in0=in0, scalar=scalar, in1=in1, op0=op0, op1=op1)
        else:
            f = lambda e: e.scalar_tensor_tensor(out=out, in0=in0, scalar=scalar, in1=in1, op0=op0, op1=op1, accum_out=accum)
        return self.P.op("dve", f, reads=r, writes=w, wpart=wp)

    def act(self, out, in_, func, r=(), w=(), wp=(), bias=None, scale=None, accum=None):
        kw = {}
        if bias is not None:
            kw["bias"] = bias
        if scale is not None:
            kw["scale"] = scale
        if accum is not None:
            kw["accum_out"] = accum
        return self.P.op("act", lambda e: e.activation(out=out, in_=in_, func=func, **kw), reads=r, writes=w, wpart=wp)

    def cp(self, eng, out, in_, r=(), w=(), wp=()):
        if eng == "act":
            return self.P.op("act", lambda e: e.copy(out=out, in_=in_), reads=r, writes=w, wpart=wp)
        return self.P.op(eng, lambda e: e.tensor_copy(out=out, in_=in_), reads=r, writes=w, wpart=wp)

    def memset(self, eng, ap, val, w=(), wp=()):
        return self.P.op(eng, lambda e: e.memset(ap, val), writes=w, wpart=wp)

    def mm(self, out, lhsT, rhs, start, stop, r=(), w=(), wp=()):
        return self.P.op("pe", lambda e: e.matmul(out, lhsT, rhs, start=start, stop=stop), reads=r, writes=w, wpart=wp)

    def tr(self, out, in_, ident, r=(), w=(), wp=()):
        return self.P.op("pe", lambda e: e.transpose(out, in_, ident), reads=r, writes=w, wpart=wp)

    def red(self, out, in_, r=(), w=(), wp=()):
        return self.P.op("dve", lambda e: e.tensor_reduce(out=out, in_=in_, axis=AX.X, op=ALU.add), reads=r, writes=w, wpart=wp)

    def recip(self, out, in_, r=(), w=(), wp=()):
        return self.P.op("dve", lambda e: e.reciprocal(out=out, in_=in_), reads=r, writes=w, wpart=wp)

    def dma(self, q, out, in_, r=(), w=(), wp=(), key=None):
        return self.P.dma(q, out, in_, reads=r, writes=w, wpart=wp, key=key)


class Cfg:
    def __init__(self, S=8192, L=256, E=32, debug=False, stop_after=None):
        self.S, self.L, self.E = S, L, E
        self.D = 1024
        self.NT = S // 128
        self.ROWS = S // 64
        self.NCH = S // 64
        self.MB = min(1024, S)
        self.NTILE = (4 * S) // 512 + E
        self.NSLOT = self.NTILE * 512
        self.debug = debug
        self.dense = False
        self.stop_after = stop_after


EPS = 1e-6
NEG = -30000.0


def host_consts(cfg):
    S = cfg.S
    NT = cfg.NT
    cs = {}
    cs["ident_f"] = np.eye(128, dtype=np.float32)
    p = np.arange(128)
    t = np.arange(64)
    s = p % 64
    cs["trif"] = (s[:, None] <= t[None, :]).astype(np.float32)
    cs["trib"] = (s[:, None] >= t[None, :]).astype(np.float32)
    r = np.ones((128, 512), np.float32)
    r[:, ::64] = 0.0
    cs["reset"] = r
    inv = (10000.0 ** (-np.arange(16, dtype=np.float32) / 16.0)).astype(np.float32)
    tt = np.arange(NT)
    row = (2 * tt[None, :] + (p[:, None] // 64)).astype(np.float32)
    col = np.broadcast_to((p % 64).astype(np.float32)[:, None], (128, NT))
    ang = np.stack([row[:, :, None] * inv[None, None, :], col[:, :, None] * inv[None, None, :]], axis=2)
    ang = ang.astype(np.float32)
    E, NTILE = cfg.E, cfg.NTILE
    cs["ut"] = (p[:, None] < p[None, :]).astype(np.float32)
    cs["iota"] = p.astype(np.float32).reshape(128, 1)
    kp = np.zeros((128, 9), np.float32)
    kp[:, :8] = np.arange(8)[None, :] * 128 + p[:, None]
    kp[:, 8] = p % 16
    cs["kp"] = kp
    cs["th"] = np.broadcast_to((512.0 * np.arange(16, dtype=np.float32))[None, None, :], (1, E, 16)).reshape(1, E * 16).copy()
    cs["j512"] = np.broadcast_to((512.0 * np.arange(NTILE, dtype=np.float32))[None, :, None], (1, NTILE, E)).reshape(1, NTILE * E).copy()
    si = np.zeros((cfg.NSLOT, 2), np.float32)
    si[:, 0] = S + (np.arange(cfg.NSLOT) % 128)
    cs["slotinit"] = si
    cs["cos"] = np.cos(ang).astype(np.float32).reshape(128, NT * 32)
    cs["sin"] = np.sin(ang).astype(np.float32).reshape(128, NT * 32)
    return cs


def layout_rpb(rpb):
    H = rpb.shape[0]
    c = np.arange(64)[:, None]
    kc = np.arange(64)[None, :]
    win = np.clip(c - 8, 0, 48)
    valid = (kc >= win) & (kc < win + 16)
    idx = np.clip(kc - c + 15, 0, 30)
    g = rpb[:, :, idx]
    g = np.where(valid[None, None], g, np.float32(NEG)).astype(np.float32)
    return np.ascontiguousarray(g.transpose(2, 0, 1, 3)).reshape(64, H * 15 * 64)


_uid = [0]


def sb(es, nc, shape, dt, n=1, name="t"):
    out = []
    for i in range(n):
        _uid[0] += 1
        t = es.enter_context(nc.sbuf_tensor("%s_%d" % (name, _uid[0]), list(shape), dt))
        out.append(Slot(t, Buf(name)))
    return out if n > 1 else out[0]


def ps(es, nc, shape, dt, n=1, name="p"):
    out = []
    for i in range(n):
        _uid[0] += 1
        t = es.enter_context(nc.psum_tensor("%s_%d" % (name, _uid[0]), list(shape), dt))
        out.append(Slot(t, Buf(name)))
    return out if n > 1 else out[0]


def declare_io(nc, cfg):
    S, L, E = cfg.S, cfg.L, cfg.E
    d = {}

    inputs = set()
    d["_inputs"] = inputs

    def inp(name, shape, dt=F32):
        d[name] = nc.dram_tensor(name, list(shape), dt, kind="ExternalInput").ap()
        inputs.add(name)

    inp("x", [S, 1024]); inp("c", [8, 128]); inp("ctx", [L, 1024]); inp("c_ctx", [8, 128])
    inp("ada_w", [2, 1024, 6144]); inp("ada_b", [2, 6144]); inp("norm1_g", [2, 1024]); inp("norm2_g", [2, 1024])
    inp("ab_w_in", [1024, 4096]); inp("ab_w_out", [1024, 1024]); inp("nat_q_norm", [1, 64]); inp("nat_k_norm", [1, 64])
    inp("rpb_full", [64, 8 * 15 * 64]); inp("hgrn_lb", [16, 128]); inp("hgrn_o_norm", [1, 128])
    inp("conv_w1", [1024, 2048]); inp("conv_b1", [16, 128]); inp("conv_dw", [31, 1024]); inp("conv_dw_b", [8, 128])
    inp("conv_ln_g", [8, 128]); inp("conv_ln_b", [8, 128]); inp("conv_w2", [1024, 1024]); inp("conv_b2", [1, 1024])
    inp("router_w", [2, 1024, E]); inp("router_b", [2, E])
    inp("moe_w1", [2, E, 1024, 2048]); inp("moe_b1", [2, E * 16, 128]); inp("moe_w2", [2, E, 1024, 1024]); inp("moe_b2", [2, E, 1024])
    inp("k_ident_f", [128, 128]); inp("k_trif", [128, 64]); inp("k_trib", [128, 64]); inp("k_reset", [128, 512])
    inp("k_cos", [128, cfg.NT * 32]); inp("k_sin", [128, cfg.NT * 32])
    inp("k_ut", [128, 128]); inp("k_iota", [128, 1]); inp("k_th", [1, E * 16]); inp("k_j512", [1, cfg.NTILE * E])
    inp("k_slotinit", [cfg.NSLOT, 2]); inp("k_kp", [128, 9])
    d["out"] = nc.dram_tensor("out", [S, 1024], F32, kind="ExternalOutput").ap()
    kind = "ExternalOutput" if cfg.debug else "Internal"

    def scr(name, shape, dt):
        d[name] = nc.dram_tensor(name, list(shape), dt, kind=kind).ap()

    scr("XT", [128, 8, S], BF16); scr("XTc", [128, 8, L], BF16)
    scr("QTr", [128, 4, S], BF16); scr("QTf", [128, 4, S], BF16); scr("KTr", [128, 4, S], BF16)
    scr("KcT", [128, 4, L], BF16)
    scr("VA", [S, 520], BF16); scr("VcA", [L, 520], BF16)
    scr("VH", [S, 512], BF16); scr("VHc", [L, 512], BF16)
    scr("G", [S, 512], F32)
    scr("HQ", [2, 128, 4, S], BF16); scr("HK", [2, 128, 4, S], BF16)
    scr("KH", [2, S, 512], BF16); scr("KHc", [2, L, 512], BF16)
    scr("DEC", [2, 128, 4, S // 64], F32); scr("DECc", [2, 128, 4, L // 64], F32)
    scr("OF", [S, 512], F32)
    scr("CAT", [S, 1024], BF16)
    scr("H1", [S, 1024], F32); scr("H2", [S, 1024], F32); scr("H3", [S, 1024], F32)
    scr("XT2", [128, 8, S], BF16)
    scr("COMB", [S, E], F32)
    scr("ACC0", [S, 1024], F32)
    scr("GT", [128, 8, S + 32], BF16)
    scr("YD", [128, 8, S], F32)
    scr("XM2", [S + 128, 1024], BF16)
    scr("MK", [S, E], F32)
    scr("ACCd", [S + 128, 1024], F32)
    scr("SLOT", [cfg.NSLOT, 2], F32)
    scr("OUTS", [cfg.NSLOT, 1024], F32)
    return d


def build_program(cfg):
    nc = bass.Bass("TRN2", target_bir_lowering=False, dynamic_dma_scratch_size=32768)
    c = Ctx()
    c.nc, c.cfg = nc, cfg
    c.d = declare_io(nc, cfg)
    c.inputs = c.d.pop("_inputs")
    with ExitStack() as ges:
        P = Prog(nc, ges)
        c.P = P
        c.k = K(P)
        c.ident_f = sb(ges, nc, [128, 128], F32, name="identf")
        c.ident_b = sb(ges, nc, [128, 128], BF16, name="identb")
        c.ones_f = sb(ges, nc, [128, 128], F32, name="onesf")
        c.k.dma("sp", c.ident_f.t[:], c.d["k_ident_f"], w=[c.ident_f.b])
        c.k.cp("dve", c.ident_b.t[:], c.ident_f.t[:], r=[c.ident_f.b], w=[c.ident_b.b])
        c.k.memset("dve", c.ones_f.t[:], 1.0, w=[c.ones_f.b])
        touch = sb(ges, nc, [1, 64], F32, name="touch")
        for i, nm in enumerate(sorted(c.inputs)):
            ap = c.d[nm]
            idx = tuple([0] * (len(ap.shape) - 2) + [slice(0, 1), slice(0, 1)])
            c.k.dma("sp", touch.t[0:1, i:i + 1], ap[idx], wp=[touch.b])
        if cfg.debug:
            tb = sb(ges, nc, [1, 2], BF16, name="touchb")
            c.k.memset("dve", touch.t[0:1, 62:64], 0.0, wp=[touch.b])
            c.k.memset("dve", tb.t[:], 0.0, w=[tb.b])
            for i, (nm, ap) in enumerate(c.d.items()):
                if nm not in c.inputs:
                    idx = tuple([0] * (len(ap.shape) - 2) + [slice(0, 1), slice(0, 1)])
                    src = tb.t[0:1, 0:1] if ap.dtype == BF16 else touch.t[0:1, 63:64]
                    c.k.dma("sp", ap[idx], src, r=[tb.b, touch.b], w=[Buf("o")])
        P.end_phase()
        def dbg_stop(name):
            return cfg.stop_after == name

        done = False
        for layer in (0, 1):
            with ExitStack() as lay:
                c.MOD = sb(lay, nc, [128, 6144], F32, name="MOD")
                if layer == 0:
                    c.CMOD = sb(lay, nc, [128, 2048], F32, name="CMOD")
                    seq = [("mods0", lambda: phase_mods(c, 0)), ("a1", lambda: phase_a1(c)), ("a2", lambda: phase_a2(c)),
                           ("nat", lambda: phase_nat(c)), ("hgrn", lambda: phase_hgrn(c)), ("post0", lambda: phase_post(c, 0))]
                else:
                    seq = [("mods1", lambda: phase_mods(c, 1)), ("f", lambda: phase_f(c)), ("g1", lambda: phase_g1(c)),
                           ("post1", lambda: phase_post(c, 1))]
                for name, fn in seq:
                    fn()
                    if dbg_stop(name):
                        done = True
                        break
            if done:
                break
            with ExitStack() as m5:
                MOD5 = sb(m5, nc, [128, 1024], F32, name="MOD5")
                with ExitStack() as ph:
                    emit_mods(c, ph, layer, [10, 11], lambda ct: MOD5.t[:, (ct - 10) * 512:(ct - 9) * 512], MOD5.b, False)
                    P.end_phase()
                if cfg.dense:
                    phase_moe(c, layer, MOD5)
                else:
                    TEi = (sb(m5, nc, [128, cfg.NTILE, 8], mybir.dt.int32, name="IDXW"), sb(m5, nc, [128, cfg.NTILE], mybir.dt.int32, name="IDXB"),
                           sb(m5, nc, [128, cfg.NT, 4], mybir.dt.int32, name="P4all"))
                    phase_route(c, layer, TEi)
                    if dbg_stop("route%d" % layer):
                        break
                    phase_smoe(c, layer, MOD5, TEi)
            if dbg_stop("moe%d" % layer):
                break
    return nc, c


def emit_mods(c, ph, layer, cts, dst_fn, dst_buf, with_ctx):
    nc, P, k, d = c.nc, c.P, c.k, c.d
    crow = sb(ph, nc, [16, 128], F32, name="crow")
    crow2 = sb(ph, nc, [16, 128], F32, name="crow2")
    ccol = sb(ph, nc, [128, 16], F32, name="ccol")
    CB = sb(ph, nc, [128, 16, 128], F32, name="CB")
    AW = sb(ph, nc, [128, 8, 512], F32, n=2, name="AW")
    ABr = sb(ph, nc, [1, 512], F32, n=2, name="ABr")
    pT = ps(ph, nc, [128, 16], F32, name="pT")
    pM = ps(ph, nc, [128, 512], F32, n=2, name="pM")
    pC = ps(ph, nc, [128, 512], F32, n=2, name="pC")
    k.dma("sp", crow.t[0:8, :], d["c"], wp=[crow.b])
    k.dma("sp", crow.t[8:16, :], d["c_ctx"], wp=[crow.b])
    k.act(crow2.t[:], crow.t[:], AF.Silu, r=[crow.b], w=[crow2.b])
    k.tr(pT.t[:], crow2.t[:], c.ident_f.t[0:16, 0:16], r=[crow2.b], w=[pT.b])
    k.cp("dve", ccol.t[:], pT.t[:], r=[pT.b], w=[ccol.b])
    for j in range(16):
        k.cp("dve" if j % 2 else "pool", CB.t[:, j, :], ccol.t[:, j:j + 1].to_broadcast([128, 128]), r=[ccol.b], wp=[CB.b])
    awv = d["ada_w"][layer].rearrange("(k p) n -> p k n", p=128)
    for i, ct in enumerate(cts):
        aw, ab = AW[i % 2], ABr[i % 2]
        k.dma("sp", aw.t[:], awv[:, :, ct * 512:(ct + 1) * 512], w=[aw.b])
        k.dma("sp", ab.t[:], d["ada_b"][layer:layer + 1, ct * 512:(ct + 1) * 512], w=[ab.b])
        pm = pM[i % 2]
        for kk in range(8):
            k.mm(pm.t[:], CB.t[:, kk, :], aw.t[:, kk, :], kk == 0, False, r=[CB.b, aw.b], w=[pm.b] if kk == 0 else (), wp=() if kk == 0 else [pm.b])
        k.mm(pm.t[:], c.ones_f.t[0:1, :], ab.t[:], False, True, r=[ab.b], wp=[pm.b])
        k.cp("act", dst_fn(ct), pm.t[:], r=[pm.b], wp=[dst_buf])
        if with_ctx and ct < 4:
            pc = pC[i % 2]
            for kk in range(8):
                k.mm(pc.t[:], CB.t[:, 8 + kk, :], aw.t[:, kk, :], kk == 0, False, r=[CB.b, aw.b], w=[pc.b] if kk == 0 else (), wp=() if kk == 0 else [pc.b])
            k.mm(pc.t[:], c.ones_f.t[0:1, :], ab.t[:], False, True, r=[ab.b], wp=[pc.b])
            k.cp("dve", c.CMOD.t[:, ct * 512:(ct + 1) * 512], pc.t[:], r=[pc.b], wp=[c.CMOD.b])


def phase_mods(c, layer):
    with ExitStack() as ph:
        emit_mods(c, ph, layer, list(range(10)), lambda ct: c.MOD.t[:, ct * 512:(ct + 1) * 512], c.MOD.b, layer == 0)
        c.P.end_phase()


def make_A(c, ph, gname, layer, modcols, modt):
    nc, k, d = c.nc, c.k, c.d
    g = sb(ph, nc, [128, 1024], F32, name="gbc")
    A = sb(ph, nc, [128, 1024], F32, name="A")
    k.dma("sp", g.t[:], d[gname][layer:layer + 1, :].partition_broadcast(128), w=[g.b])
    k.stt(A.t[:], modt.t[:, modcols:modcols + 1024], 1.0, g.t[:], ALU.add, ALU.mult, r=[modt.b, g.b], w=[A.b])
    return A


def norm_mod(c, xt, A, SH, shb, outs, tmp, j):
    k = c.k
    ss, t1 = tmp["ss"], tmp["t1"]
    k.stt(t1.t[:], xt.t[:], 1.0, xt.t[:], ALU.mult, ALU.mult, r=[xt.b], w=[t1.b], wp=[ss.b], accum=ss.t[:, 4 * j:4 * j + 1])
    k.ts("dve", ss.t[:, 4 * j + 1:4 * j + 2], ss.t[:, 4 * j:4 * j + 1], 1.0 / 1024, ALU.mult, EPS, ALU.add, r=[ss.b], wp=[ss.b])
    k.act(ss.t[:, 4 * j + 2:4 * j + 3], ss.t[:, 4 * j + 1:4 * j + 2], AF.Sqrt, r=[ss.b], wp=[ss.b])
    k.recip(ss.t[:, 4 * j + 3:4 * j + 4], ss.t[:, 4 * j + 2:4 * j + 3], r=[ss.b], wp=[ss.b])
    k.stt(t1.t[:], xt.t[:], ss.t[:, 4 * j + 3:4 * j + 4], A.t[:], ALU.mult, ALU.mult, r=[xt.b, ss.b, A.b], w=[t1.b])
    for (ap, eng, slot) in outs:
        k.tt(eng, ap, t1.t[:], SH, ALU.add, r=[t1.b, shb], wp=[slot.b])


def phase_a1(c):
    nc, P, k, d, cfg = c.nc, c.P, c.k, c.d, c.cfg
    S, L, NT = cfg.S, cfg.L, cfg.NT
    with ExitStack() as ph:
        W = sb(ph, nc, [128, 8, 2560], BF16, name="Wtok")
        wv = d["ab_w_in"].rearrange("(k p) n -> p k n", p=128)
        for i, c0 in enumerate((0, 512, 1024, 3072, 3584)):
            k.dma("pool", W.t[:, :, i * 512:(i + 1) * 512], wv[:, :, c0:c0 + 512], wp=[W.b])
        A = make_A(c, ph, "norm1_g", 0, 1024, c.MOD)
        Ac = make_A(c, ph, "norm1_g", 0, 1024, c.CMOD)
        g64 = sb(ph, nc, [128, 128], F32, name="g64")
        GQ = sb(ph, nc, [128, 512], F32, name="GQ")
        GK = sb(ph, nc, [128, 512], F32, name="GK")
        k.dma("sp", g64.t[:, 0:64], d["nat_q_norm"].partition_broadcast(128), wp=[g64.b])
        k.dma("sp", g64.t[:, 64:128], d["nat_k_norm"].partition_broadcast(128), wp=[g64.b])
        k.ts("dve", GQ.t[:].rearrange("p (h e) -> p h e", h=8), g64.t[:, 0:64].unsqueeze(1).to_broadcast([128, 8, 64]), 0.125, ALU.mult, r=[g64.b], w=[GQ.b])
        k.ts("dve", GK.t[:].rearrange("p (h e) -> p h e", h=8), g64.t[:, 64:128].unsqueeze(1).to_broadcast([128, 8, 64]), 1.0, ALU.mult, r=[g64.b], w=[GK.b])
        COSG = sb(ph, nc, [128, 128], F32, n=2, name="COS")
        SING = sb(ph, nc, [128, 128], F32, n=2, name="SIN")
        XIN = sb(ph, nc, [128, 1024], F32, n=2, name="xin")
        tmps = [{"ss": sb(ph, nc, [128, 16], F32, name="ss"), "t1": sb(ph, nc, [128, 1024], F32, name="t1")} for _ in range(2)]
        XM = sb(ph, nc, [128, 1024], BF16, n=2, name="xm")
        XTG = sb(ph, nc, [128, 8, 512], BF16, n=2, name="xtg")
        pT = ps(ph, nc, [128, 1024], BF16, name="pT")
        pS = ps(ph, nc, [128, 512], F32, n=5, name="pS")
        pO = ps(ph, nc, [128, 1024], BF16, n=2, name="pO")
        SQs = [sb(ph, nc, [128, 1024], F32, name="sq")] * 2
        STs = sb(ph, nc, [128, 48], F32, n=2, name="st")
        QNs = [sb(ph, nc, [128, 1024], F32, name="qn")] * 2
        R1s = [sb(ph, nc, [128, 1024], F32, name="r1")] * 2
        R2s = [sb(ph, nc, [128, 1024], F32, name="r2")] * 2
        OB = sb(ph, nc, [128, 1536], BF16, n=2, name="ob")
        OTG = sb(ph, nc, [128, 3, 4, 512], BF16, n=2, name="otg")
        VAs = sb(ph, nc, [128, 8, 65], BF16, n=2, name="vas")
        VHs = sb(ph, nc, [128, 512], BF16, n=2, name="vhs")
        Gs = sb(ph, nc, [128, 512], F32, n=2, name="gs")
        for v in VAs:
            k.memset("pool", v.t[:, :, 64:65], 1.0, wp=[v.b])
        dXT, dXTc = Buf("dXT"), Buf("dXTc")
        dQ, dV, dVH, dG = Buf("dQ"), Buf("dV"), Buf("dVH"), Buf("dG")

        def run(src, ntile, Asl, modt, is_ctx):
            ngrp = (ntile + 3) // 4
            it = 0
            for g in range(ngrp):
                nj = min(4, ntile - 4 * g)
                xtg = XTG[g % 2]
                otg = OTG[g % 2]
                COS, SIN = COSG[g % 2], SING[g % 2]
                if not is_ctx:
                    k.dma("sp", COS.t[:, 0:nj * 32], d["k_cos"][:, g * 128:g * 128 + nj * 32], w=[COS.b])
                    k.dma("sp", SIN.t[:, 0:nj * 32], d["k_sin"][:, g * 128:g * 128 + nj * 32], w=[SIN.b])
                states = {}

                def head(j):
                    nonlocal it
                    t = 4 * g + j
                    xin, xm = XIN[it % 2], XM[it % 2]
                    ob, vas, vhs, gs = OB[it % 2], VAs[it % 2], VHs[it % 2], Gs[it % 2]
                    SQ, ST, QN, R1, R2 = SQs[it % 2], STs[it % 2], QNs[it % 2], R1s[it % 2], R2s[it % 2]
                    it += 1
                    k.dma("sp", xin.t[:], src[t * 128:(t + 1) * 128, :], w=[xin.b])
                    norm_mod(c, xin, Asl, modt.t[:, 0:1024], modt.b, [(xm.t[:], "pool", xm)], tmps[it % 2], j)
                    for kk in range(8):
                        k.tr(pT.t[:, kk * 128:(kk + 1) * 128], xm.t[:, kk * 128:(kk + 1) * 128], c.ident_b.t[:], r=[xm.b],
                             w=[pT.b] if kk == 0 else (), wp=() if kk == 0 else [pT.b])
                    k.cp("act", xtg.t[:, :, j * 128:(j + 1) * 128], pT.t[:].rearrange("p (k t) -> p k t", k=8), r=[pT.b], wp=[xtg.b])
                    cols = (1, 2, 3) if is_ctx else (0, 1, 2, 3, 4)
                    for ci in cols:
                        for kk in range(8):
                            k.mm(pS[ci].t[:], xtg.t[:, kk, j * 128:(j + 1) * 128], W.t[:, kk, ci * 512:(ci + 1) * 512], kk == 0, kk == 7,
                                 r=[xtg.b, W.b], w=[pS[ci].b] if kk == 0 else (), wp=() if kk == 0 else [pS[ci].b])
                    k.cp("act", vas.t[:, :, 0:64], pS[2].t[:].rearrange("p (h e) -> p h e", h=8), r=[pS[2].b], wp=[vas.b])
                    k.cp("dve", vhs.t[:], pS[3].t[:], r=[pS[3].b], w=[vhs.b])
                    rows = slice(t * 128, (t + 1) * 128)
                    if is_ctx:
                        k.dma("sp", d["VcA"][rows, :], vas.t[:].rearrange("p h e -> p (h e)"), r=[vas.b], wp=[dV])
                        k.dma("sp", d["VHc"][rows, :], vhs.t[:], r=[vhs.b], wp=[dVH])
                    else:
                        k.act(gs.t[:], pS[4].t[:], AF.Silu, r=[pS[4].b], w=[gs.b])
                        k.dma("sp", d["VA"][rows, :], vas.t[:].rearrange("p h e -> p (h e)"), r=[vas.b], wp=[dV])
                        k.dma("sp", d["VH"][rows, :], vhs.t[:], r=[vhs.b], wp=[dVH])
                        k.dma("sp", d["G"][rows, :], gs.t[:], r=[gs.b], wp=[dG])
                    srcs = ((1, 1),) if is_ctx else ((0, 0), (1, 1))
                    for (ci, slot_i) in srcs:
                        k.act(SQ.t[:, slot_i * 512:(slot_i + 1) * 512], pS[ci].t[:], AF.Square, r=[pS[ci].b], wp=[SQ.b])
                        k.red(ST.t[:, slot_i * 8:(slot_i + 1) * 8], SQ.t[:, slot_i * 512:(slot_i + 1) * 512].rearrange("p (h e) -> p h e", h=8), r=[SQ.b], wp=[ST.b])
                    k.ts("dve", ST.t[:, 16:32], ST.t[:, 0:16], 1.0 / 64, ALU.mult, EPS, ALU.add, r=[ST.b], wp=[ST.b])
                    k.act(ST.t[:, 32:48], ST.t[:, 16:32], AF.Sqrt, r=[ST.b], wp=[ST.b])
                    k.recip(ST.t[:, 16:32], ST.t[:, 32:48], r=[ST.b], wp=[ST.b])
                    for (ci, slot_i) in srcs:
                        qn = QN.t[:, slot_i * 512:(slot_i + 1) * 512]
                        k.tt("dve", qn.rearrange("p (h e) -> p h e", h=8), pS[ci].t[:].rearrange("p (h e) -> p h e", h=8),
                             ST.t[:, 16 + slot_i * 8:16 + slot_i * 8 + 8].unsqueeze(2).to_broadcast([128, 8, 64]), ALU.mult,
                             r=[pS[ci].b, ST.b], wp=[QN.b])
                        Gt = GQ if slot_i == 0 else GK
                        k.tt("pool", qn, qn, Gt.t[:], ALU.mult, r=[QN.b, Gt.b], wp=[QN.b])
                    if is_ctx:
                        k.cp("act", ob.t[:, 1024:1536], QN.t[:, 512:1024], r=[QN.b], wp=[ob.b])
                    else:
                        k.cp("act", ob.t[:, 512:1024], QN.t[:, 0:512], r=[QN.b], wp=[ob.b])
                        qv = QN.t[:].rearrange("p (h a b i) -> p h a b i", h=16, a=2, b=2)
                        cosb = COS.t[:, j * 32:(j + 1) * 32].rearrange("p (a i) -> p a i", a=2).unsqueeze(1).to_broadcast([128, 16, 2, 16])
                        sinb = SIN.t[:, j * 32:(j + 1) * 32].rearrange("p (a i) -> p a i", a=2).unsqueeze(1).to_broadcast([128, 16, 2, 16])
                        r1v = R1.t[:].rearrange("p (h a b i) -> p h a b i", h=16, a=2, b=2)
                        r2v = R2.t[:].rearrange("p (h a b i) -> p h a b i", h=16, a=2, b=2)
                        x1, x2 = qv[:, :, :, 0, :], qv[:, :, :, 1, :]
                        k.tt("dve", r1v[:, :, :, 0, :], x1, cosb, ALU.mult, r=[QN.b, COS.b], wp=[R1.b])
                        k.tt("pool", r2v[:, :, :, 0, :], x2, sinb, ALU.mult, r=[QN.b, SIN.b], wp=[R2.b])
                        k.tt("dve", r1v[:, :, :, 1, :], x2, cosb, ALU.mult, r=[QN.b, COS.b], wp=[R1.b])
                        k.tt("pool", r2v[:, :, :, 1, :], x1, sinb, ALU.mult, r=[QN.b, SIN.b], wp=[R2.b])
                        for slot_i, o0 in ((0, 0), (1, 1024)):
                            ov = ob.t[:, o0:o0 + 512].rearrange("p (h a b i) -> p h a b i", h=8, a=2, b=2)
                            a1 = r1v[:, slot_i * 8:(slot_i + 1) * 8]
                            a2 = r2v[:, slot_i * 8:(slot_i + 1) * 8]
                            k.tt("dve", ov[:, :, :, 0, :], a1[:, :, :, 0, :], a2[:, :, :, 0, :], ALU.subtract, r=[R1.b, R2.b], wp=[ob.b])
                            k.tt("dve", ov[:, :, :, 1, :], a1[:, :, :, 1, :], a2[:, :, :, 1, :], ALU.add, r=[R1.b, R2.b], wp=[ob.b])
                    states[j] = (ob,)

                def tail(j):
                    (ob,) = states[j]
                    which = (2,) if is_ctx else (0, 1, 2)
                    for wi in which:
                        po = pO[0] if wi < 2 else pO[1]
                        for hp in range(4):
                            col = ((wi % 2) * 4 + hp) * 128
                            first = (hp == 0 and wi in (0, 2))
                            k.tr(po.t[:, col:col + 128], ob.t[:, wi * 512 + hp * 128: wi * 512 + (hp + 1) * 128], c.ident_b.t[:], r=[ob.b],
                                 w=[po.b] if first else (), wp=() if first else [po.b])
                    if not is_ctx:
                        k.cp("act", otg.t[:, 0:2, :, j * 128:(j + 1) * 128], pO[0].t[:].rearrange("p (w h t) -> p w h t", w=2, h=4), r=[pO[0].b], wp=[otg.b])
                    k.cp("dve", otg.t[:, 2, :, j * 128:(j + 1) * 128], pO[1].t[:, 0:512].rearrange("p (h t) -> p h t", h=4), r=[pO[1].b], wp=[otg.b])

                head(0)
                for j in range(nj):
                    if j + 1 < nj:
                        head(j + 1)
                    tail(j)
                tok = slice(g * 512, g * 512 + nj * 128)
                w_ = nj * 128
                if is_ctx:
                    k.dma("sp", d["XTc"][:, :, tok], xtg.t[:, :, 0:w_], r=[xtg.b], wp=[dXTc])
                    k.dma("sp", d["KcT"][:, :, tok], otg.t[:, 2, :, 0:w_], r=[otg.b], wp=[dQ])
                else:
                    k.dma("sp", d["XT"][:, :, tok], xtg.t[:, :, 0:w_], r=[xtg.b], wp=[dXT])
                    k.dma("sp", d["QTr"][:, :, tok], otg.t[:, 0, :, 0:w_], r=[otg.b], wp=[dQ])
                    k.dma("sp", d["QTf"][:, :, tok], otg.t[:, 1, :, 0:w_], r=[otg.b], wp=[dQ])
                    k.dma("sp", d["KTr"][:, :, tok], otg.t[:, 2, :, 0:w_], r=[otg.b], wp=[dQ])

        run(d["ctx"], L // 128, Ac, c.CMOD, True)
        run(d["x"], NT, A, c.MOD, False)
        P.end_phase()


def core_inputs(inp, b, cfg, consts):
    f = lambda a: np.ascontiguousarray(np.asarray(a, dtype=np.float32))
    m = {
        "x": f(inp["x"][b]), "c": f(inp["c"][b]).reshape(8, 128), "ctx": f(inp["ctx"][b]),
        "c_ctx": f(inp["c_ctx"]).reshape(8, 128),
        "ada_w": f(inp["ada_w"]), "ada_b": f(inp["ada_b"]), "norm1_g": f(inp["norm1_g"]), "norm2_g": f(inp["norm2_g"]),
        "ab_w_in": f(inp["ab_w_in"][0]), "ab_w_out": f(inp["ab_w_out"][0]),
        "nat_q_norm": f(inp["nat_q_norm"][0]).reshape(1, 64), "nat_k_norm": f(inp["nat_k_norm"][0]).reshape(1, 64),
        "rpb_full": layout_rpb(f(inp["nat_rpb"][0])),
        "hgrn_lb": f(inp["hgrn_lb"]).reshape(16, 128), "hgrn_o_norm": f(inp["hgrn_o_norm"][0]).reshape(1, 128),
        "conv_w1": f(inp["conv_w1"][0]), "conv_b1": f(inp["conv_b1"][0]).reshape(16, 128), "conv_dw": f(inp["conv_dw"][0]),
        "conv_dw_b": f(inp["conv_dw_b"][0]).reshape(8, 128), "conv_ln_g": f(inp["conv_ln_g"][0]).reshape(8, 128),
        "conv_ln_b": f(inp["conv_ln_b"][0]).reshape(8, 128), "conv_w2": f(inp["conv_w2"][0]), "conv_b2": f(inp["conv_b2"][0]).reshape(1, 1024),
        "router_w": f(inp["router_w"]), "router_b": f(inp["router_b"]),
        "moe_w1": f(inp["moe_w1"]), "moe_b1": f(inp["moe_b1"]).reshape(2, cfg.E * 16, 128),
        "moe_w2": f(inp["moe_w2"]), "moe_b2": f(inp["moe_b2"]),
    }
    for kname, v in consts.items():
        m["k_" + kname] = v
    return m


_cache = {}


def kernel(**inputs):
    B = inputs["x"].shape[0]
    S = inputs["x"].shape[1]
    cfg = Cfg(S=S, L=inputs["ctx"].shape[1], E=inputs["moe_w1"].shape[1])
    key = (cfg.S, cfg.L, cfg.E)
    if key not in _cache:
        _cache[key] = build_program(cfg)[0]
    nc = _cache[key]
    consts = host_consts(cfg)
    in_maps = [core_inputs(inputs, b, cfg, consts) for b in range(B)]
    res = run_bass_kernel_spmd(nc, in_maps, core_ids=list(range(B)))
    return np.stack([np.asarray(r["out"], dtype=np.float32) for r in res.results], axis=0)


def phase_a2(c):
    nc, P, k, d, cfg = c.nc, c.P, c.k, c.d, c.cfg
    S, L = cfg.S, cfg.L
    with ExitStack() as ph:
        W = sb(ph, nc, [128, 8, 1536], BF16, name="Wfm")
        wv = d["ab_w_in"].rearrange("(k p) n -> p k n", p=128)
        for i in range(3):
            k.dma("pool", W.t[:, :, i * 512:(i + 1) * 512], wv[:, :, 1536 + i * 512:1536 + (i + 1) * 512], wp=[W.b])
        RESET = sb(ph, nc, [128, 512], F32, name="reset")
        k.dma("sp", RESET.t[:], d["k_reset"], w=[RESET.b])
        lbr = sb(ph, nc, [16, 128], F32, name="lbr")
        Ee = sb(ph, nc, [128, 16], F32, name="Ee")
        LB = sb(ph, nc, [128, 24], F32, name="LB")
        pL = ps(ph, nc, [128, 16], F32, name="pL")
        k.dma("sp", lbr.t[:], d["hgrn_lb"], w=[lbr.b])
        k.tr(pL.t[:], lbr.t[:], c.ident_f.t[0:16, 0:16], r=[lbr.b], w=[pL.b])
        k.act(Ee.t[:], pL.t[:], AF.Exp, r=[pL.b], w=[Ee.b])
        ev = Ee.t[:].rearrange("p (d j h) -> p d j h", d=2, j=2)
        k.tt("dve", LB.t[:, 16:24].rearrange("p (d h) -> p d h", d=2), ev[:, :, 0, :], ev[:, :, 1, :], ALU.add, r=[Ee.b], wp=[LB.b])
        k.recip(LB.t[:, 16:24], LB.t[:, 16:24], r=[LB.b], wp=[LB.b])
        k.tt("dve", LB.t[:, 0:8].rearrange("p (d h) -> p d h", d=2), ev[:, :, 0, :], LB.t[:, 16:24].rearrange("p (d h) -> p d h", d=2), ALU.mult, r=[Ee.b, LB.b], wp=[LB.b])
        k.ts("dve", LB.t[:, 8:16], LB.t[:, 0:8], -1.0, ALU.mult, 1.0, ALU.add, r=[LB.b], wp=[LB.b])

        XTG = sb(ph, nc, [128, 8, 512], BF16, n=2, name="xtg")
        names = ("q32", "sg", "f", "lf", "kk", "B", "e1", "e2", "t1", "r", "e3")
        TM = {n_: sb(ph, nc, [128, 512], F32, n=2, name=n_) for n_ in names}
        HQs = sb(ph, nc, [128, 2, 4, 512], BF16, n=2, name="hqs")
        HKs = sb(ph, nc, [128, 2, 4, 512], BF16, n=2, name="hks")
        KHT = sb(ph, nc, [128, 512], BF16, n=2, name="kht")
        KHs = sb(ph, nc, [128, 4, 2, 512], BF16, n=2, name="khs")
        DECs = sb(ph, nc, [128, 2, 4, 8], F32, n=2, name="decs")
        pQ = ps(ph, nc, [128, 512], F32, n=2, name="pQ")
        pF = ps(ph, nc, [128, 512], F32, n=3, name="pF")
        pK = ps(ph, nc, [128, 512], BF16, n=2, name="pK")
        dHQ, dHK, dKH, dDEC = Buf("dHQ"), Buf("dHK"), Buf("dKH"), Buf("dDEC")
        QS = 128.0 ** -0.5

        def run(src, ntok, is_ctx):
            ngrp = (ntok + 511) // 512
            it = 0
            for g in range(ngrp):
                n = min(512, ntok - g * 512)
                nch = n // 64
                nsub = n // 128
                xtg, hqs, hks, khs, decs = XTG[g % 2], HQs[g % 2], HKs[g % 2], KHs[g % 2], DECs[g % 2]
                k.dma("sp", xtg.t[:, :, 0:n], src[:, :, g * 512:g * 512 + n], w=[xtg.b])
                for h in range(4):
                    pq = pQ[h % 2]
                    if not is_ctx:
                        for kk_ in range(8):
                            k.mm(pq.t[:, 0:n], W.t[:, kk_, h * 128:(h + 1) * 128], xtg.t[:, kk_, 0:n], kk_ == 0, kk_ == 7,
                                 r=[W.b, xtg.b], w=[pq.b] if kk_ == 0 else (), wp=() if kk_ == 0 else [pq.b])
                        q32 = TM["q32"][h % 2]
                        k.act(q32.t[:, 0:n], pq.t[:, 0:n], AF.Silu, r=[pq.b], w=[q32.b])
                    for dd in range(2):
                        pf = pF[(2 * h + dd) % 3]
                        c0 = 512 + dd * 512 + h * 128
                        for kk_ in range(8):
                            k.mm(pf.t[:, 0:n], W.t[:, kk_, c0:c0 + 128], xtg.t[:, kk_, 0:n], kk_ == 0, kk_ == 7,
                                 r=[W.b, xtg.b], w=[pf.b] if kk_ == 0 else (), wp=() if kk_ == 0 else [pf.b])
                        tm = {n_: TM[n_][it % 2] for n_ in names}
                        kht = KHT[it % 2]
                        pk = pK[it % 2]
                        it += 1
                        sg, f, lf, kk, Bc, e1, e2, t1, rr, e3 = (tm[x] for x in ("sg", "f", "lf", "kk", "B", "e1", "e2", "t1", "r", "e3"))
                        li = dd * 4 + h
                        k.act(sg.t[:, 0:n], pf.t[:, 0:n], AF.Sigmoid, r=[pf.b], w=[sg.b])
                        k.ts("dve", f.t[:, 0:n], sg.t[:, 0:n], LB.t[:, 8 + li:9 + li], ALU.mult, LB.t[:, li:li + 1], ALU.add, r=[sg.b, LB.b], w=[f.b])
                        k.act(lf.t[:, 0:n], f.t[:, 0:n], AF.Ln, r=[f.b], w=[lf.b])
                        k.ts("pool", kk.t[:, 0:n], f.t[:, 0:n], -1.0, ALU.mult, 1.0, ALU.add, r=[f.b], w=[kk.b])
                        P.op("dve", (lambda e, o=Bc.t[:, 0:n], a=RESET.t[:, 0:n], b_=lf.t[:, 0:n]:
                                     e.tensor_tensor_scan(out=o, data0=a, data1=b_, initial=0.0, op0=ALU.mult, op1=ALU.add)),
                             reads=[RESET.b, lf.b], writes=[Bc.b])
                        Bv = Bc.t[:, 0:n].rearrange("p (c t) -> p c t", t=64)
                        Bend = Bv[:, :, 63:64].to_broadcast([128, nch, 64])
                        v3 = lambda s_: s_.t[:, 0:n].rearrange("p (c t) -> p c t", t=64)
                        if dd == 0:
                            k.act(e1.t[:, 0:n], Bc.t[:, 0:n], AF.Exp, r=[Bc.b], w=[e1.b])
                            k.act(e2.t[:, 0:n], Bc.t[:, 0:n], AF.Exp, r=[Bc.b], w=[e2.b], scale=-1.0)
                            k.tt("dve", v3(t1), Bend, Bv, ALU.subtract, r=[Bc.b], w=[t1.b])
                            k.act(e3.t[:, 0:n], t1.t[:, 0:n], AF.Exp, r=[t1.b], w=[e3.b])
                        else:
                            k.tt("dve", t1.t[:, 0:n], lf.t[:, 0:n], Bc.t[:, 0:n], ALU.subtract, r=[lf.b, Bc.b], w=[t1.b])
                            k.tt("dve", v3(rr), v3(t1), Bend, ALU.add, r=[t1.b, Bc.b], w=[rr.b])
                            k.act(e1.t[:, 0:n], rr.t[:, 0:n], AF.Exp, r=[rr.b], w=[e1.b])
                            k.act(e2.t[:, 0:n], rr.t[:, 0:n], AF.Exp, r=[rr.b], w=[e2.b], scale=-1.0)
                            k.act(e3.t[:, 0:n], t1.t[:, 0:n], AF.Exp, r=[t1.b], w=[e3.b], scale=-1.0)
                        k.act(decs.t[:, dd, h, 0:nch], Bv[:, :, 63], AF.Exp, r=[Bc.b], wp=[decs.b])
                        if not is_ctx:
                            q32 = TM["q32"][h % 2]
                            k.stt(hqs.t[:, dd, h, 0:n], q32.t[:, 0:n], QS, e1.t[:, 0:n], ALU.mult, ALU.mult, r=[q32.b, e1.b], wp=[hqs.b])
                            k.tt("pool", hks.t[:, dd, h, 0:n], kk.t[:, 0:n], e2.t[:, 0:n], ALU.mult, r=[kk.b, e2.b], wp=[hks.b])
                        k.tt("pool", kht.t[:, 0:n], kk.t[:, 0:n], e3.t[:, 0:n], ALU.mult, r=[kk.b, e3.b], w=[kht.b])
                        for sub in range(nsub):
                            k.tr(pk.t[:, sub * 128:(sub + 1) * 128], kht.t[:, sub * 128:(sub + 1) * 128], c.ident_b.t[:], r=[kht.b],
                                 w=[pk.b] if sub == 0 else (), wp=() if sub == 0 else [pk.b])
                        k.cp("act", khs.t[:, 0:nsub, dd, h * 128:(h + 1) * 128], pk.t[:, 0:n].rearrange("p (s e) -> p s e", e=128), r=[pk.b], wp=[khs.b])
                tok = slice(g * 512, g * 512 + n)
                for dd in range(2):
                    if is_ctx:
                        k.dma("sp", d["KHc"][dd, tok, :].rearrange("(s p) e -> p s e", p=128), khs.t[:, 0:nsub, dd, :], r=[khs.b], wp=[dKH])
                        k.dma("sp", d["DECc"][dd, :, :, g * 8:g * 8 + nch], decs.t[:, dd, :, 0:nch], r=[decs.b], wp=[dDEC])
                    else:
                        k.dma("sp", d["HQ"][dd, :, :, tok], hqs.t[:, dd, :, 0:n], r=[hqs.b], wp=[dHQ])
                        k.dma("sp", d["HK"][dd, :, :, tok], hks.t[:, dd, :, 0:n], r=[hks.b], wp=[dHK])
                        k.dma("sp", d["KH"][dd, tok, :].rearrange("(s p) e -> p s e", p=128), khs.t[:, 0:nsub, dd, :], r=[khs.b], wp=[dKH])
                        k.dma("sp", d["DEC"][dd, :, :, g * 8:g * 8 + nch], decs.t[:, dd, :, 0:nch], r=[decs.b], wp=[dDEC])

        run(d["XTc"], L, True)
        run(d["XT"], S, False)
        P.end_phase()


def phase_nat(c):
    nc, P, k, d, cfg = c.nc, c.P, c.k, c.d, c.cfg
    S, L, ROWS = cfg.S, cfg.L, cfg.ROWS
    NCC = L // 128
    NCH = 4 + NCC
    with ExitStack() as ph:
        BFf = sb(ph, nc, [128, 7680], F32, name="bff")
        BFb = sb(ph, nc, [128, 8, 960], BF16, name="bfb")
        k.dma("sp", BFf.t[0:64, :], d["rpb_full"], wp=[BFf.b])
        k.dma("sp", BFf.t[64:128, :], d["rpb_full"], wp=[BFf.b])
        k.cp("dve", BFb.t[:].rearrange("p h e -> p (h e)"), BFf.t[:], r=[BFf.b], w=[BFb.b])
        KcT = sb(ph, nc, [128, 4, L], BF16, name="kct")
        VcA = sb(ph, nc, [128, NCC, 520], BF16, name="vca")
        k.dma("sp", KcT.t[:], d["KcT"], w=[KcT.b])
        k.dma("sp", VcA.t[:], d["VcA"].rearrange("(c p) f -> p c f", p=128), w=[VcA.b])
        QR = sb(ph, nc, [128, 4, 512], BF16, n=2, name="qr")
        QF = sb(ph, nc, [128, 4, 512], BF16, n=2, name="qf")
        KW = sb(ph, nc, [128, 4, 512], BF16, n=3, name="kw")
        VW = sb(ph, nc, [128, 4, 520], BF16, n=3, name="vw")
        PT = sb(ph, nc, [128, NCH * 64], BF16, n=3, name="pt")
        NS = sb(ph, nc, [64, 512], BF16, n=2, name="ns")
        RD = sb(ph, nc, [64, 8], F32, n=2, name="rd")
        pS = ps(ph, nc, [128, 512], F32, n=4, name="pS")
        pO = ps(ph, nc, [64, 4, 65], F32, n=4, name="pO")
        dCAT = Buf("dCATn")
        it = 0
        for r in range(ROWS):
            g8, ro = r // 8, (r % 8) * 64
            qr, qf = QR[g8 % 2], QF[g8 % 2]
            if r % 8 == 0:
                k.dma("sp", qr.t[:], d["QTr"][:, :, g8 * 512:(g8 + 1) * 512], w=[qr.b])
                k.dma("sp", qf.t[:], d["QTf"][:, :, g8 * 512:(g8 + 1) * 512], w=[qf.b])
            rs = min(max(r - 4, 0), ROWS - 8)
            dr0 = rs - r + 7
            kw, vw = KW[r % 3], VW[r % 3]
            k.dma("sp", kw.t[:], d["KTr"][:, :, rs * 64:rs * 64 + 512], w=[kw.b])
            k.dma("sp", vw.t[:], d["VA"][rs * 64:rs * 64 + 512, :].rearrange("(c p) f -> p c f", p=128), w=[vw.b])
            ns, rd = NS[r % 2], RD[r % 2]
            po2 = (pO[(2 * r) % 4], pO[(2 * r + 1) % 4])
            def scores(h):
                nonlocal it
                hp, pb = h // 2, (h % 2) * 64
                psx, pt = pS[it % 4], PT[it % 3]
                it += 1
                first = True
                for kc in range(4):
                    k.mm(psx.t[:, kc * 64:(kc + 1) * 64], kw.t[pb:pb + 64, hp, kc * 128:(kc + 1) * 128], qr.t[pb:pb + 64, hp, ro:ro + 64], True, False,
                         r=[kw.b, qr.b], w=[psx.b] if first else (), wp=() if first else [psx.b])
                    first = False
                    k.mm(psx.t[:, kc * 64:(kc + 1) * 64], BFb.t[pb:pb + 64, h, (dr0 + 2 * kc) * 64:(dr0 + 2 * kc) * 64 + 128], c.ident_b.t[pb:pb + 64, pb:pb + 64], False, True,
                         r=[BFb.b], wp=[psx.b])
                for cc in range(NCC):
                    k.mm(psx.t[:, (4 + cc) * 64:(5 + cc) * 64], KcT.t[pb:pb + 64, hp, cc * 128:(cc + 1) * 128], qf.t[pb:pb + 64, hp, ro:ro + 64], True, True,
                         r=[KcT.b, qf.b], wp=[psx.b])
                k.act(pt.t[:], psx.t[:, 0:NCH * 64], AF.Exp, r=[psx.b], w=[pt.b])
                return pt

            def pv(h, pt):
                po = po2[h // 4]
                hh = h % 4
                for ch in range(NCH):
                    rhs = vw.t[:, ch, h * 65:(h + 1) * 65] if ch < 4 else VcA.t[:, ch - 4, h * 65:(h + 1) * 65]
                    k.mm(po.t[:, hh, :], pt.t[:, ch * 64:(ch + 1) * 64], rhs, ch == 0, ch == NCH - 1,
                         r=[pt.b, vw.b, VcA.b], w=[po.b] if (ch == 0 and hh == 0) else (), wp=() if (ch == 0 and hh == 0) else [po.b])

            pts = [scores(0)]
            for h in range(8):
                if h + 1 < 8:
                    pts.append(scores(h + 1))
                pv(h, pts[h])
            for half in range(2):
                po = po2[half]
                k.recip(rd.t[:, half * 4:(half + 1) * 4], po.t[:, :, 64], r=[po.b], wp=[rd.b])
                k.tt("dve", ns.t[:, half * 256:(half + 1) * 256].rearrange("p (h e) -> p h e", h=4), po.t[:, :, 0:64],
                     rd.t[:, half * 4:(half + 1) * 4].unsqueeze(2).to_broadcast([64, 4, 64]), ALU.mult, r=[po.b, rd.b], wp=[ns.b])
            k.dma("sp", d["CAT"][r * 64:(r + 1) * 64, 0:512], ns.t[:], r=[ns.b], wp=[dCAT])
        P.end_phase()


def phase_hgrn(c):
    nc, P, k, d, cfg = c.nc, c.P, c.k, c.d, c.cfg
    S, L = cfg.S, cfg.L
    NG = S // 512
    NCHT = S // 64
    LC = L // 64
    with ExitStack() as ph:
        TRI = sb(ph, nc, [128, 2, 64], F32, name="tri")
        k.dma("sp", TRI.t[:, 0, :], d["k_trif"], wp=[TRI.b])
        k.dma("sp", TRI.t[:, 1, :], d["k_trib"], wp=[TRI.b])
        og = sb(ph, nc, [128, 128], F32, name="og")
        ONG = sb(ph, nc, [128, 512], F32, name="ong")
        k.dma("sp", og.t[:], d["hgrn_o_norm"].partition_broadcast(128), w=[og.b])
        k.ts("dve", ONG.t[:].rearrange("p (h e) -> p h e", h=4), og.t[:].unsqueeze(1).to_broadcast([128, 4, 128]), 1.0, ALU.mult, r=[og.b], w=[ONG.b])
        S32s = sb(ph, nc, [128, 4, 128], F32, n=2, name="s32")
        Sbfs = sb(ph, nc, [128, 4, 128], BF16, n=2, name="sbf")
        DECt = sb(ph, nc, [128, 4, NCHT], F32, name="dect")
        DECc = sb(ph, nc, [128, 4, LC], F32, name="decc")
        KHc = sb(ph, nc, [128, L // 128, 512], BF16, name="khc")
        VHc = sb(ph, nc, [128, L // 128, 512], BF16, name="vhc")
        HQg = sb(ph, nc, [128, 4, 512], BF16, n=2, name="hqg")
        HKg = sb(ph, nc, [128, 4, 512], BF16, n=2, name="hkg")
        KHg = sb(ph, nc, [128, 4, 512], BF16, n=2, name="khg")
        VHg = sb(ph, nc, [128, 4, 512], BF16, n=2, name="vhg")
        SC = [sb(ph, nc, [128, 256], BF16, n=2, name="sc%d" % i) for i in range(2)]
        for i in range(2):
            for s_ in SC[i]:
                k.memset("pool", s_.t[:], 0.0, w=[s_.b])
        OFs = sb(ph, nc, [64, 512], F32, n=2, name="ofs")
        OFc = sb(ph, nc, [64, 512], F32, n=2, name="ofc")
        Gc = sb(ph, nc, [64, 512], F32, n=2, name="gc")
        O32 = sb(ph, nc, [64, 512], F32, n=2, name="o32")
        SQ = sb(ph, nc, [64, 512], F32, n=2, name="sq")
        ST = sb(ph, nc, [64, 16], F32, n=2, name="st")
        Y1 = sb(ph, nc, [64, 512], F32, n=2, name="y1")
        YB = sb(ph, nc, [64, 512], BF16, n=2, name="yb")
        pSs = ps(ph, nc, [128, 512], F32, n=2, name="pSs")
        pSo = ps(ph, nc, [128, 512], F32, n=2, name="pSo")
        pSt = ps(ph, nc, [128, 512], F32, n=2, name="pSt")
        dOF, dCAT = Buf("dOF"), Buf("dCATh")
        k.dma("sp", VHc.t[:], d["VHc"].rearrange("(s p) e -> p s e", p=128), w=[VHc.b])
        it = 0
        for dd in range(2):
            k.memset("dve", S32s[0].t[:], 0.0, w=[S32s[0].b])
            k.dma("sp", DECt.t[:], d["DEC"][dd], w=[DECt.b])
            k.dma("sp", DECc.t[:], d["DECc"][dd], w=[DECc.b])
            k.dma("sp", KHc.t[:], d["KHc"][dd].rearrange("(s p) e -> p s e", p=128), w=[KHc.b])

            sn = 0

            def state_update(khs, vhs, tl, pb, dec_ap_fn):
                nonlocal it, sn
                pst = pSt[it % 2]
                for h in range(4):
                    k.mm(pst.t[:, h * 128:(h + 1) * 128], khs.t[pb:pb + 64, tl, h * 128:(h + 1) * 128], vhs.t[pb:pb + 64, tl, h * 128:(h + 1) * 128], True, True,
                         r=[khs.b, vhs.b], w=[pst.b] if h == 0 else (), wp=() if h == 0 else [pst.b])
                so, sw = S32s[sn % 2], S32s[(sn + 1) % 2]
                for h in range(4):
                    k.stt(sw.t[:, h, :], so.t[:, h, :], dec_ap_fn(h), pst.t[:, h * 128:(h + 1) * 128], ALU.mult, ALU.add,
                          r=[so.b, pst.b, DECt.b, DECc.b], wp=[sw.b])
                nb = Sbfs[(sn + 1) % 2]
                k.cp("act", nb.t[:], sw.t[:], r=[sw.b], w=[nb.b])
                sn += 1

            chs = range(LC) if dd == 0 else range(LC - 1, -1, -1)
            for ch in chs:
                state_update(KHc, VHc, ch // 2, (ch % 2) * 64, lambda h, ch=ch: DECc.t[:, h, ch:ch + 1])
                it += 1
            groups = list(range(NG)) if dd == 0 else list(range(NG - 1, -1, -1))
            order = []
            for gi, g in enumerate(groups):
                for tl in (range(4) if dd == 0 else range(3, -1, -1)):
                    for cc in ((0, 1) if dd == 0 else (1, 0)):
                        order.append((gi, g, tl, cc))
            loaded = set()

            def ensure(gi, g):
                if gi in loaded:
                    return
                loaded.add(gi)
                hq, hk, kh, vh = HQg[gi % 2], HKg[gi % 2], KHg[gi % 2], VHg[gi % 2]
                tok = slice(g * 512, (g + 1) * 512)
                k.dma("sp", hq.t[:], d["HQ"][dd, :, :, tok], w=[hq.b])
                k.dma("sp", hk.t[:], d["HK"][dd, :, :, tok], w=[hk.b])
                k.dma("sp", kh.t[:], d["KH"][dd, tok, :].rearrange("(s p) e -> p s e", p=128), w=[kh.b])
                k.dma("sp", vh.t[:], d["VH"][tok, :].rearrange("(s p) e -> p s e", p=128), w=[vh.b])

            def scores(n):
                gi, g, tl, cc = order[n]
                ensure(gi, g)
                hq, hk = HQg[gi % 2], HKg[gi % 2]
                pb = cc * 64
                toff, qoff = tl * 128, tl * 128 + cc * 64
                pss = pSs[n % 2]
                sc = SC[cc][(n // 2) % 2]
                for h in range(4):
                    k.mm(pss.t[:, h * 64:(h + 1) * 64], hk.t[:, h, toff:toff + 128], hq.t[:, h, qoff:qoff + 64], True, True,
                         r=[hk.b, hq.b], w=[pss.b] if h == 0 else (), wp=() if h == 0 else [pss.b])
                k.tt("dve", sc.t[pb:pb + 64, :].rearrange("p (h t) -> p h t", h=4), pss.t[pb:pb + 64, 0:256].rearrange("p (h t) -> p h t", h=4),
                     TRI.t[pb:pb + 64, dd, :].unsqueeze(1).to_broadcast([64, 4, 64]), ALU.mult, r=[pss.b, TRI.b], wp=[sc.b])
                return sc

            def rest(n, sc):
                nonlocal it
                gi, g, tl, cc = order[n]
                hq, hk, kh, vh = HQg[gi % 2], HKg[gi % 2], KHg[gi % 2], VHg[gi % 2]
                ch = g * 8 + tl * 2 + cc
                pb = cc * 64
                qoff = tl * 128 + cc * 64
                pso = pSo[n % 2]
                Sbf = Sbfs[sn % 2]
                state_update(kh, vh, tl, pb, lambda h, ch=ch: DECt.t[:, h, ch:ch + 1])
                it += 1
                for h in range(4):
                    k.mm(pso.t[0:64, h * 128:(h + 1) * 128], sc.t[:, h * 64:(h + 1) * 64], vh.t[:, tl, h * 128:(h + 1) * 128], True, False,
                         r=[sc.b, vh.b], w=[pso.b] if h == 0 else (), wp=() if h == 0 else [pso.b])
                    k.mm(pso.t[0:64, h * 128:(h + 1) * 128], hq.t[:, h, qoff:qoff + 64], Sbf.t[:, h, :], False, True,
                         r=[hq.b, Sbf.b], wp=[pso.b])
                rows = slice(ch * 64, (ch + 1) * 64)
                if dd == 0:
                    ofs = OFs[n % 2]
                    k.cp("act", ofs.t[:], pso.t[0:64, :], r=[pso.b], w=[ofs.b])
                    k.dma("sp", d["OF"][rows, :], ofs.t[:], r=[ofs.b], wp=[dOF])
                else:
                    ofc, gc, o32, sq, st, y1, yb = (X[n % 2] for X in (OFc, Gc, O32, SQ, ST, Y1, YB))
                    k.dma("sp", ofc.t[:], d["OF"][rows, :], r=[dOF], w=[ofc.b])
                    k.dma("sp", gc.t[:], d["G"][rows, :], w=[gc.b])
                    k.tt("dve", o32.t[:], pso.t[0:64, :], ofc.t[:], ALU.add, r=[pso.b, ofc.b], w=[o32.b])
                    k.act(sq.t[:], o32.t[:], AF.Square, r=[o32.b], w=[sq.b])
                    k.red(st.t[:, 0:4], sq.t[:].rearrange("p (h e) -> p h e", h=4), r=[sq.b], wp=[st.b])
                    k.ts("dve", st.t[:, 4:8], st.t[:, 0:4], 1.0 / 128, ALU.mult, EPS, ALU.add, r=[st.b], wp=[st.b])
                    k.act(st.t[:, 8:12], st.t[:, 4:8], AF.Sqrt, r=[st.b], wp=[st.b])
                    k.recip(st.t[:, 12:16], st.t[:, 8:12], r=[st.b], wp=[st.b])
                    k.tt("dve", y1.t[:].rearrange("p (h e) -> p h e", h=4), o32.t[:].rearrange("p (h e) -> p h e", h=4),
                         st.t[:, 12:16].unsqueeze(2).to_broadcast([64, 4, 128]), ALU.mult, r=[o32.b, st.b], w=[y1.b])
                    k.tt("pool", y1.t[:], y1.t[:], ONG.t[0:64, :], ALU.mult, r=[y1.b, ONG.b], w=[y1.b])
                    k.tt("pool", yb.t[:], y1.t[:], gc.t[:], ALU.mult, r=[y1.b, gc.b], w=[yb.b])
                    k.dma("sp", d["CAT"][rows, 512:1024], yb.t[:], r=[yb.b], wp=[dCAT])

            N = len(order)
            cur = scores(0)
            for n in range(N):
                nxt = scores(n + 1) if n + 1 < N else None
                rest(n, cur)
                cur = nxt
        P.end_phase()


def phase_post(c, layer):
    nc, P, k, d, cfg = c.nc, c.P, c.k, c.d, c.cfg
    S, E, NT = cfg.S, cfg.E, cfg.NT
    NG = S // 512
    hin = d["x"] if layer == 0 else d["H2"]
    hout = d["H1"] if layer == 0 else d["H3"]
    with ExitStack() as ph:
        Wo = sb(ph, nc, [128, 8, 1024], BF16, name="Wo")
        wsrc = d["ab_w_out"] if layer == 0 else d["conv_w2"]
        k.dma("pool", Wo.t[:], wsrc.rearrange("(k p) n -> p k n", p=128), w=[Wo.b])
        RW = sb(ph, nc, [128, 8, E], F32, name="RW")
        k.dma("sp", RW.t[:], d["router_w"][layer].rearrange("(k p) e -> p k e", p=128), w=[RW.b])
        RB = sb(ph, nc, [128, E], F32, name="RB")
        k.dma("sp", RB.t[:], d["router_b"][layer:layer + 1, :].partition_broadcast(128), w=[RB.b])
        B2t = sb(ph, nc, [E, 1024], F32, name="B2t")
        k.dma("sp", B2t.t[:], d["moe_b2"][layer], w=[B2t.b])
        A2 = make_A(c, ph, "norm2_g", layer, 4096, c.MOD)
        XIN = sb(ph, nc, [128, 1024], F32, n=2, name="xin")
        Hs = sb(ph, nc, [128, 1024], F32, n=2, name="hs")
        V1s = sb(ph, nc, [128, 1024], F32, n=2, name="v1")
        tmps = [{"ss": sb(ph, nc, [128, 16], F32, name="ss"), "t1": sb(ph, nc, [128, 1024], F32, name="t1")} for _ in range(2)]
        XFs = sb(ph, nc, [128, 1024], F32, n=2, name="xf")
        XB = sb(ph, nc, [128, 1024], BF16, n=2, name="xb")
        XT2g = sb(ph, nc, [128, 8, 512], BF16, n=2, name="xt2g")
        XFTs = sb(ph, nc, [128, 8, 128], F32, n=2, name="xft")
        LG = sb(ph, nc, [128, 4 * E + 32], F32, n=2, name="lg")
        CTs = sb(ph, nc, [E, 128], F32, n=2, name="ct")
        ACss = sb(ph, nc, [128, 1024], F32, n=2, name="acs")
        pT = ps(ph, nc, [128, 1024], BF16, name="pT")
        pT2 = ps(ph, nc, [128, 1024], BF16, name="pT2")
        pY = ps(ph, nc, [128, 512], F32, n=2, name="pY")
        pTf = ps(ph, nc, [128, 512], F32, n=2, name="pTf")
        pLg = ps(ph, nc, [128, E], F32, name="pLg")
        pCT = ps(ph, nc, [E, 128], F32, name="pCT")
        dH, dXT2, dCOMB, dACC = Buf("dH"), Buf("dXT2"), Buf("dCOMB"), Buf("dACC")
        if layer == 0:
            CATt = sb(ph, nc, [128, 1024], BF16, n=2, name="catt")
            catT = sb(ph, nc, [128, 8, 128], BF16, n=2, name="catT")
        else:
            YG = sb(ph, nc, [128, 8, 512], F32, name="yg")
            YSQ = sb(ph, nc, [128, 512], F32, n=2, name="ysq")
            Mm = sb(ph, nc, [128, 512], F32, name="mm_")
            MSQ = sb(ph, nc, [128, 512], F32, name="msq")
            RS = sb(ph, nc, [128, 512], F32, name="rs")
            TA = sb(ph, nc, [128, 512], F32, n=2, name="ta")
            TB = sb(ph, nc, [128, 512], F32, n=2, name="tb")
            HN = sb(ph, nc, [128, 8, 512], BF16, name="hn")
            pSum = pTf[0]
            pSq = pTf[1]
            lnr = sb(ph, nc, [16, 128], F32, name="lnr")
            LNP = sb(ph, nc, [128, 16], F32, name="lnp")
            k.dma("sp", lnr.t[0:8, :], d["conv_ln_g"], wp=[lnr.b])
            k.dma("sp", lnr.t[8:16, :], d["conv_ln_b"], wp=[lnr.b])
            k.tr(pLg.t[:, 0:16] if E >= 16 else pTf[0].t[:, 0:16], lnr.t[:], c.ident_f.t[0:16, 0:16], r=[lnr.b], w=[pLg.b if E >= 16 else pTf[0].b])
            k.cp("dve", LNP.t[:], pLg.t[:, 0:16] if E >= 16 else pTf[0].t[:, 0:16], r=[pLg.b if E >= 16 else pTf[0].b], w=[LNP.b])
            b2bc = sb(ph, nc, [128, 1024], F32, name="b2bc")
            B2M = sb(ph, nc, [128, 1024], F32, name="b2m")
            k.dma("sp", b2bc.t[:], d["conv_b2"].partition_broadcast(128), w=[b2bc.b])
            k.tt("dve", B2M.t[:], b2bc.t[:], c.MOD.t[:, 2048:3072], ALU.mult, r=[b2bc.b, c.MOD.b], w=[B2M.b])

        it = 0
        for g in range(NG):
            xt2g = XT2g[g % 2]
            if layer == 1:
                k.dma("sp", YG.t[:], d["YD"][:, :, g * 512:(g + 1) * 512], w=[YG.b])
                for cch in range(8):
                    ysq = YSQ[cch % 2]
                    k.act(ysq.t[:], YG.t[:, cch, :], AF.Square, r=[YG.b], w=[ysq.b])
                    k.mm(pSum.t[:], c.ones_f.t[:], YG.t[:, cch, :], cch == 0, cch == 7, r=[YG.b, c.ones_f.b], w=[pSum.b] if cch == 0 else (), wp=() if cch == 0 else [pSum.b])
                    k.mm(pSq.t[:], c.ones_f.t[:], ysq.t[:], cch == 0, cch == 7, r=[ysq.b, c.ones_f.b], w=[pSq.b] if cch == 0 else (), wp=() if cch == 0 else [pSq.b])
                k.act(Mm.t[:], pSum.t[:], AF.Copy, r=[pSum.b], w=[Mm.b], scale=1.0 / 1024)
                k.tt("pool", MSQ.t[:], Mm.t[:], Mm.t[:], ALU.mult, r=[Mm.b], w=[MSQ.b])
                k.stt(RS.t[:], pSq.t[:], 1.0 / 1024, MSQ.t[:], ALU.mult, ALU.subtract, r=[pSq.b, MSQ.b], w=[RS.b])
                k.ts("dve", RS.t[:], RS.t[:], EPS, ALU.add, r=[RS.b], w=[RS.b])
                k.act(RS.t[:], RS.t[:], AF.Sqrt, r=[RS.b], w=[RS.b])
                k.recip(RS.t[:], RS.t[:], r=[RS.b], w=[RS.b])
                for cch in range(8):
                    ta, tb = TA[cch % 2], TB[cch % 2]
                    k.tt("dve", ta.t[:], YG.t[:, cch, :], Mm.t[:], ALU.subtract, r=[YG.b, Mm.b], w=[ta.b])
                    k.tt("pool", tb.t[:], ta.t[:], RS.t[:], ALU.mult, r=[ta.b, RS.b], w=[tb.b])
                    k.act(HN.t[:, cch, :], tb.t[:], AF.Silu, r=[tb.b, LNP.b], wp=[HN.b], scale=LNP.t[:, cch:cch + 1], bias=LNP.t[:, 8 + cch:9 + cch])
            states = {}

            def front(j):
                nonlocal it
                t = g * 4 + j
                rows = slice(t * 128, (t + 1) * 128)
                xin, hs, xb = XIN[it % 2], Hs[it % 2], XB[it % 2]
                lg = LG[it % 2]
                V1, XF = V1s[it % 2], XFs[it % 2]
                k.dma("sp", xin.t[:], hin[rows, :], w=[xin.b])
                if layer == 0:
                    cat, ctT = CATt[it % 2], catT[it % 2]
                    k.dma("sp", cat.t[:], d["CAT"][rows, :], w=[cat.b])
                    for kk in range(8):
                        k.tr(pT.t[:, kk * 128:(kk + 1) * 128], cat.t[:, kk * 128:(kk + 1) * 128], c.ident_b.t[:], r=[cat.b],
                             w=[pT.b] if kk == 0 else (), wp=() if kk == 0 else [pT.b])
                    k.cp("act", ctT.t[:].rearrange("p k t -> p (k t)"), pT.t[:], r=[pT.b], w=[ctT.b])
                    lhs = lambda kk: ctT.t[:, kk, :]
                    lb_ = ctT.b
                else:
                    lhs = lambda kk: HN.t[:, kk, j * 128:(j + 1) * 128]
                    lb_ = HN.b
                it += 1
                for half in range(2):
                    for kk in range(8):
                        k.mm(pY[half].t[:], lhs(kk), Wo.t[:, kk, half * 512:(half + 1) * 512], kk == 0, kk == 7,
                             r=[lb_, Wo.b], w=[pY[half].b] if kk == 0 else (), wp=() if kk == 0 else [pY[half].b])
                    k.tt("dve", V1.t[:, half * 512:(half + 1) * 512], pY[half].t[:], c.MOD.t[:, 2048 + half * 512:2048 + (half + 1) * 512], ALU.mult,
                         r=[pY[half].b, c.MOD.b], wp=[V1.b])
                if layer == 1:
                    k.tt("pool", xin.t[:], xin.t[:], B2M.t[:], ALU.add, r=[xin.b, B2M.b], w=[xin.b])
                k.tt("pool", hs.t[:], V1.t[:], xin.t[:], ALU.add, r=[V1.b, xin.b], w=[hs.b])
                k.dma("sp", hout[rows, :], hs.t[:], r=[hs.b], wp=[dH])
                norm_mod(c, hs, A2, c.MOD.t[:, 3072:4096], c.MOD.b, [(XF.t[:], "dve", XF), (xb.t[:], "pool", xb)], tmps[it % 2], j)

                states[j] = (t, rows, hs, xb, lg, XF)

            def back(j):
                t, rows, hs, xb, lg, XF = states[j]
                XFT_, CT_, ACs_ = XFTs[t % 2], CTs[t % 2], ACss[t % 2]
                for kk in range(8):
                    k.tr(pT2.t[:, kk * 128:(kk + 1) * 128], xb.t[:, kk * 128:(kk + 1) * 128], c.ident_b.t[:], r=[xb.b],
                         w=[pT2.b] if kk == 0 else (), wp=() if kk == 0 else [pT2.b])
                k.cp("act", xt2g.t[:, :, j * 128:(j + 1) * 128], pT2.t[:].rearrange("p (k t) -> p k t", k=8), r=[pT2.b], wp=[xt2g.b])
                for kk in range(8):
                    pf = pTf[kk // 4]
                    k.tr(pf.t[:, (kk % 4) * 128:(kk % 4 + 1) * 128], XF.t[:, kk * 128:(kk + 1) * 128], c.ident_f.t[:], r=[XF.b],
                         w=[pf.b] if kk % 4 == 0 else (), wp=() if kk % 4 == 0 else [pf.b])
                for hf in range(2):
                    k.cp("act" if hf else "dve", XFT_.t[:, hf * 4:(hf + 1) * 4, :], pTf[hf].t[:].rearrange("p (k t) -> p k t", k=4), r=[pTf[hf].b], wp=[XFT_.b])
                for kk in range(8):
                    k.mm(pLg.t[:], XFT_.t[:, kk, :], RW.t[:, kk, :], kk == 0, kk == 7, r=[XFT_.b, RW.b], w=[pLg.b] if kk == 0 else (), wp=() if kk == 0 else [pLg.b])
                L0, MK, EX, EXM, MS = (lg.t[:, 0:E], lg.t[:, E:2 * E], lg.t[:, 2 * E:3 * E], lg.t[:, 3 * E:4 * E], lg.t[:, 4 * E:4 * E + 32])
                k.tt("dve", L0, pLg.t[:], RB.t[:], ALU.add, r=[pLg.b, RB.b], wp=[lg.b])
                P.op("dve", (lambda e, o=MS[:, 0:8], i_=L0: e.max(out=o, in_=i_)), reads=[lg.b], wpart=[lg.b])
                k.ts("dve", MK, L0, MS[:, 3:4], ALU.is_ge, r=[lg.b], wp=[lg.b])
                k.ts("dve", MS[:, 8:9], MS[:, 0:1], -1.0, ALU.mult, r=[lg.b], wp=[lg.b])
                k.act(EX, L0, AF.Exp, r=[lg.b], wp=[lg.b], bias=MS[:, 8:9])
                k.stt(EXM, EX, 1.0, MK, ALU.mult, ALU.mult, r=[lg.b], wp=[lg.b], accum=MS[:, 9:10])
                k.recip(MS[:, 10:11], MS[:, 9:10], r=[lg.b], wp=[lg.b])
                k.ts("dve", EXM, EXM, MS[:, 10:11], ALU.mult, r=[lg.b], wp=[lg.b])
                k.dma("sp", d["COMB"][rows, :], EXM, r=[lg.b], wp=[dCOMB])
                k.dma("sp", d["MK"][rows, :], MK, r=[lg.b], wp=[dCOMB])
                k.dma("sp", d["XM2"][rows, :], xb.t[:], r=[xb.b], wp=[dXT2])
                k.tr(pCT.t[:], EXM, c.ident_f.t[:], r=[lg.b], w=[pCT.b])
                k.cp("act", CT_.t[:], pCT.t[:], r=[pCT.b], w=[CT_.b])
                for half in range(2):
                    k.mm(pTf[half].t[:], CT_.t[:], B2t.t[:, half * 512:(half + 1) * 512], True, True, r=[CT_.b, B2t.b], w=[pTf[half].b])
                    k.cp("act" if half else "dve", ACs_.t[:, half * 512:(half + 1) * 512], pTf[half].t[:], r=[pTf[half].b], wp=[ACs_.b])
                k.dma("sp", d["ACCd"][rows, :], ACs_.t[:], r=[ACs_.b], wp=[dACC])

            front(0)
            for j in range(4):
                if j + 1 < 4:
                    front(j + 1)
                back(j)
            k.dma("sp", d["XT2"][:, :, g * 512:(g + 1) * 512], xt2g.t[:], r=[xt2g.b], wp=[dXT2])
        P.end_phase()


def phase_moe(c, layer, MOD5):
    nc, P, k, d, cfg = c.nc, c.P, c.k, c.d, c.cfg
    S, E, MB = cfg.S, cfg.E, cfg.MB
    NTB = MB // 128
    NH = MB // 512
    hin = d["H1"] if layer == 0 else d["H3"]
    hout = d["H2"] if layer == 0 else d["out"]
    with ExitStack() as ph:
        nb1 = (E * 16) // 128
        B1T = sb(ph, nc, [128, E * 16], F32, name="b1t")
        b1r = sb(ph, nc, [128, 128], F32, n=2, name="b1r")
        ACC = sb(ph, nc, [128, NTB, 1024], F32, name="acc")
        XT2b = sb(ph, nc, [128, 8, MB], BF16, name="xt2b")
        CMB = sb(ph, nc, [128, NTB, E], F32, name="cmb")
        W1 = sb(ph, nc, [128, 8, 2048], BF16, n=2, name="w1")
        W2 = sb(ph, nc, [128, 8, 1024], BF16, name="w2")
        ACTT = sb(ph, nc, [128, 8, 512], BF16, n=2, name="actt")
        G1 = sb(ph, nc, [128, 512], F32, n=2, name="g1")
        S1 = sb(ph, nc, [128, 512], F32, n=2, name="s1")
        L1 = sb(ph, nc, [128, 512], F32, n=2, name="l1")
        L2 = sb(ph, nc, [128, 512], F32, n=2, name="l2")
        GS = sb(ph, nc, [128, 512], F32, n=2, name="gs")
        HT = sb(ph, nc, [128, 1024], F32, n=2, name="ht")
        pG = ps(ph, nc, [128, 512], F32, n=2, name="pG")
        pL = ps(ph, nc, [128, 512], F32, n=2, name="pL")
        pO = ps(ph, nc, [128, 512], F32, n=4, name="pO")
        dOUT = Buf("dOUT")
        for i in range(nb1):
            br = b1r[i % 2]
            k.dma("sp", br.t[:], d["moe_b1"][layer, i * 128:(i + 1) * 128, :], w=[br.b])
            k.tr(pG[i % 2].t[:, 0:128], br.t[:], c.ident_f.t[:], r=[br.b], w=[pG[i % 2].b])
            k.cp("dve", B1T.t[:, i * 128:(i + 1) * 128], pG[i % 2].t[:, 0:128], r=[pG[i % 2].b], wp=[B1T.b])
        wi = 0
        io = 0
        for blk in range(S // MB):
            t0 = blk * NTB
            rows = slice(blk * MB, (blk + 1) * MB)
            k.dma("sp", ACC.t[:], d["ACCd"][rows, :].rearrange("(t p) n -> p t n", p=128), w=[ACC.b])
            k.dma("sp", XT2b.t[:], d["XT2"][:, :, rows], w=[XT2b.b])
            k.dma("sp", CMB.t[:], d["COMB"][rows, :].rearrange("(t p) e -> p t e", p=128), w=[CMB.b])
            for e in range(E):
                w1 = W1[wi % 2]
                wi += 1
                w1v = d["moe_w1"][layer, e].rearrange("(k p) n -> p k n", p=128)
                k.dma("pool", w1.t[:, 0:4, :], w1v[:, 0:4, :], wp=[w1.b])
                k.dma("pool", w1.t[:, 4:8, :], w1v[:, 4:8, :], wp=[w1.b])
                w2_loaded = False
                for ht in range(NH):
                    actt = ACTT[io % 2]
                    for pr in range(8):
                        pg, pl = pG[pr % 2], pL[pr % 2]
                        g1, s1, l1, l2, gs = (X[pr % 2] for X in (G1, S1, L1, L2, GS))
                        for kk in range(8):
                            k.mm(pg.t[:], w1.t[:, kk, pr * 128:(pr + 1) * 128], XT2b.t[:, kk, ht * 512:(ht + 1) * 512], kk == 0, kk == 7,
                                 r=[w1.b, XT2b.b], w=[pg.b] if kk == 0 else (), wp=() if kk == 0 else [pg.b])
                        for kk in range(8):
                            k.mm(pl.t[:], w1.t[:, kk, 1024 + pr * 128:1024 + (pr + 1) * 128], XT2b.t[:, kk, ht * 512:(ht + 1) * 512], kk == 0, kk == 7,
                                 r=[w1.b, XT2b.b], w=[pl.b] if kk == 0 else (), wp=() if kk == 0 else [pl.b])
                        bg = B1T.t[:, e * 16 + pr:e * 16 + pr + 1]
                        bl = B1T.t[:, e * 16 + 8 + pr:e * 16 + 8 + pr + 1]
                        k.ts("dve", g1.t[:], pg.t[:], bg, ALU.add, 7.0, ALU.min, r=[pg.b, B1T.b], w=[g1.b])
                        k.act(s1.t[:], g1.t[:], AF.Sigmoid, r=[g1.b], w=[s1.b], scale=1.702)
                        k.act(l1.t[:], pl.t[:], AF.Identity, r=[pl.b, B1T.b], w=[l1.b], bias=bl)
                        k.ts("dve", l2.t[:], l1.t[:], 7.0, ALU.min, -7.0, ALU.max, r=[l1.b], w=[l2.b])
                        k.tt("pool", gs.t[:], g1.t[:], s1.t[:], ALU.mult, r=[g1.b, s1.b], w=[gs.b])
                        k.stt(actt.t[:, pr, :], l2.t[:], 1.0, gs.t[:], ALU.add, ALU.mult, r=[l2.b, gs.b], wp=[actt.b])
                    if not w2_loaded:
                        k.dma("pool", W2.t[:], d["moe_w2"][layer, e].rearrange("(k p) n -> p k n", p=128), w=[W2.b])
                        w2_loaded = True
                    for sub in range(4):
                        tl = ht * 4 + sub
                        for half in range(2):
                            po = pO[io % 4]
                            io += 1
                            for jj in range(8):
                                k.mm(po.t[:], actt.t[:, jj, sub * 128:(sub + 1) * 128], W2.t[:, jj, half * 512:(half + 1) * 512], jj == 0, jj == 7,
                                     r=[actt.b, W2.b], w=[po.b] if jj == 0 else (), wp=() if jj == 0 else [po.b])
                            acc = ACC.t[:, tl, half * 512:(half + 1) * 512]
                            k.stt(acc, po.t[:], CMB.t[:, tl, e:e + 1], acc, ALU.mult, ALU.add, r=[po.b, CMB.b, ACC.b], wp=[ACC.b])
            for tl in range(NTB):
                t = t0 + tl
                ht_ = HT[tl % 2]
                k.dma("sp", ht_.t[:], hin[t * 128:(t + 1) * 128, :], w=[ht_.b])
                k.tt("dve", ACC.t[:, tl, :], ACC.t[:, tl, :], MOD5.t[:], ALU.mult, r=[ACC.b, MOD5.b], wp=[ACC.b])
                k.tt("pool", ht_.t[:], ht_.t[:], ACC.t[:, tl, :], ALU.add, r=[ht_.b, ACC.b], w=[ht_.b])
                k.dma("sp", hout[t * 128:(t + 1) * 128, :], ht_.t[:], r=[ht_.b], wp=[dOUT])
        P.end_phase()


def phase_f(c):
    nc, P, k, d, cfg = c.nc, c.P, c.k, c.d, c.cfg
    S = cfg.S
    NG = S // 512
    with ExitStack() as ph:
        W = sb(ph, nc, [128, 8, 2048], BF16, name="Wc1")
        wv = d["conv_w1"].rearrange("(k p) n -> p k n", p=128)
        k.dma("pool", W.t[:, 0:4, :], wv[:, 0:4, :], wp=[W.b])
        k.dma("pool", W.t[:, 4:8, :], wv[:, 4:8, :], wp=[W.b])
        A = make_A(c, ph, "norm1_g", 1, 1024, c.MOD)
        b1r = sb(ph, nc, [16, 128], F32, name="b1r")
        CB1 = sb(ph, nc, [128, 16], F32, name="cb1")
        pB = ps(ph, nc, [128, 16], F32, name="pB")
        k.dma("sp", b1r.t[:], d["conv_b1"], w=[b1r.b])
        k.tr(pB.t[:], b1r.t[:], c.ident_f.t[0:16, 0:16], r=[b1r.b], w=[pB.b])
        k.cp("dve", CB1.t[:], pB.t[:], r=[pB.b], w=[CB1.b])
        Z = sb(ph, nc, [128, 8, 16], BF16, name="z")
        k.memset("dve", Z.t[:], 0.0, w=[Z.b])
        dGT = Buf("dGT")
        k.dma("sp", d["GT"][:, :, 0:16], Z.t[:], r=[Z.b], wp=[dGT])
        k.dma("sp", d["GT"][:, :, S + 16:S + 32], Z.t[:], r=[Z.b], wp=[dGT])
        XIN = sb(ph, nc, [128, 1024], F32, n=2, name="xin")
        tmps = [{"ss": sb(ph, nc, [128, 16], F32, name="ss"), "t1": sb(ph, nc, [128, 1024], F32, name="t1")} for _ in range(2)]
        XM = sb(ph, nc, [128, 1024], BF16, n=2, name="xm")
        XTG = sb(ph, nc, [128, 8, 512], BF16, n=2, name="xtg")
        SG = sb(ph, nc, [128, 512], F32, n=2, name="sg")
        GTs = sb(ph, nc, [128, 8, 512], BF16, n=2, name="gts")
        pT = ps(ph, nc, [128, 1024], BF16, name="pT")
        pA = ps(ph, nc, [128, 512], F32, n=2, name="pA")
        pGt = ps(ph, nc, [128, 512], F32, n=2, name="pGt")
        it = 0
        for g in range(NG):
            xtg, gts = XTG[g % 2], GTs[g % 2]
            for j in range(4):
                t = 4 * g + j
                xin, xm = XIN[it % 2], XM[it % 2]
                it += 1
                k.dma("sp", xin.t[:], d["H2"][t * 128:(t + 1) * 128, :], w=[xin.b])
                norm_mod(c, xin, A, c.MOD.t[:, 0:1024], c.MOD.b, [(xm.t[:], "pool", xm)], tmps[it % 2], j)
                for kk in range(8):
                    k.tr(pT.t[:, kk * 128:(kk + 1) * 128], xm.t[:, kk * 128:(kk + 1) * 128], c.ident_b.t[:], r=[xm.b],
                         w=[pT.b] if kk == 0 else (), wp=() if kk == 0 else [pT.b])
                k.cp("act", xtg.t[:, :, j * 128:(j + 1) * 128], pT.t[:].rearrange("p (k t) -> p k t", k=8), r=[pT.b], wp=[xtg.b])
            for cp_ in range(8):
                pa, pg, sg = pA[cp_ % 2], pGt[cp_ % 2], SG[cp_ % 2]
                for kk in range(8):
                    k.mm(pa.t[:], W.t[:, kk, cp_ * 128:(cp_ + 1) * 128], xtg.t[:, kk, :], kk == 0, kk == 7, r=[W.b, xtg.b],
                         w=[pa.b] if kk == 0 else (), wp=() if kk == 0 else [pa.b])
                for kk in range(8):
                    k.mm(pg.t[:], W.t[:, kk, 1024 + cp_ * 128:1024 + (cp_ + 1) * 128], xtg.t[:, kk, :], kk == 0, kk == 7, r=[W.b, xtg.b],
                         w=[pg.b] if kk == 0 else (), wp=() if kk == 0 else [pg.b])
                k.act(sg.t[:], pg.t[:], AF.Sigmoid, r=[pg.b, CB1.b], w=[sg.b], bias=CB1.t[:, 8 + cp_:9 + cp_])
                k.stt(gts.t[:, cp_, :], pa.t[:], CB1.t[:, cp_:cp_ + 1], sg.t[:], ALU.add, ALU.mult, r=[pa.b, sg.b, CB1.b], wp=[gts.b])
            k.dma("sp", d["GT"][:, :, 16 + g * 512:16 + (g + 1) * 512], gts.t[:], r=[gts.b], wp=[dGT])
        P.end_phase()


def phase_g1(c):
    nc, P, k, d, cfg = c.nc, c.P, c.k, c.d, c.cfg
    S = cfg.S
    NG = S // 512
    with ExitStack() as ph:
        dwr = sb(ph, nc, [32, 1024], F32, name="dwr")
        DWT = sb(ph, nc, [128, 8, 31], F32, name="dwt")
        dbr = sb(ph, nc, [8, 128], F32, name="dbr")
        DWB = sb(ph, nc, [128, 8], F32, name="dwb")
        DG = sb(ph, nc, [128, 8, 31, 128], BF16, name="dg")
        pD = ps(ph, nc, [128, 8, 32], F32, name="pD")
        pB = ps(ph, nc, [128, 8], F32, name="pB")
        k.dma("sp", dwr.t[0:31, :], d["conv_dw"], w=[dwr.b])
        for cch in range(8):
            k.tr(pD.t[:, cch, 0:31], dwr.t[0:31, cch * 128:(cch + 1) * 128], c.ident_f.t[0:31, 0:31], r=[dwr.b],
                 w=[pD.b] if cch == 0 else (), wp=() if cch == 0 else [pD.b])
        k.cp("dve", DWT.t[:], pD.t[:, :, 0:31], r=[pD.b], w=[DWT.b])
        k.dma("sp", dbr.t[:], d["conv_dw_b"], w=[dbr.b])
        k.tr(pB.t[:], dbr.t[:], c.ident_f.t[0:8, 0:8], r=[dbr.b], w=[pB.b])
        k.cp("dve", DWB.t[:], pB.t[:], r=[pB.b], w=[DWB.b])
        n_ = 0
        for cch in range(8):
            for j in range(31):
                k.ts("dve" if n_ % 2 else "pool", DG.t[:, cch, j, :], c.ident_f.t[:], DWT.t[:, cch, j:j + 1], ALU.mult, r=[DWT.b, c.ident_f.b], wp=[DG.b])
                n_ += 1
        GTw = sb(ph, nc, [128, 8, 544], BF16, n=2, name="gtw")
        Ys = sb(ph, nc, [128, 8, 512], F32, n=2, name="ys")
        pC = ps(ph, nc, [128, 512], F32, n=4, name="pC")
        dYD = Buf("dYD")
        for g in range(NG):
            gtw, ys = GTw[g % 2], Ys[g % 2]
            k.dma("sp", gtw.t[:], d["GT"][:, :, g * 512:g * 512 + 544], w=[gtw.b])
            for cch in range(8):
                pc = pC[cch % 4]
                for j in range(31):
                    k.mm(pc.t[:], DG.t[:, cch, j, :], gtw.t[:, cch, j + 1:j + 513], j == 0, j == 30, r=[DG.b, gtw.b],
                         w=[pc.b] if j == 0 else (), wp=() if j == 0 else [pc.b])
                k.act(ys.t[:, cch, :], pc.t[:], AF.Identity, r=[pc.b, DWB.b], wp=[ys.b], bias=DWB.t[:, cch:cch + 1])
            k.dma("sp", d["YD"][:, :, g * 512:(g + 1) * 512], ys.t[:], r=[ys.b], wp=[dYD])
        P.end_phase()


I32 = mybir.dt.int32


def pool_dma_op(P, fn, reads=(), writes=(), wpart=(), key=None):
    o = Op("pool", fn, P.phase)
    o.is_dma = True
    if key is None:
        key = (list(writes) + list(wpart))[0]
    if key not in P.keymap:
        P.keymap[key] = len(P.keymap)
        assert len(P.keymap) <= NDSEM
    o.key = P.keymap[key]
    P._deps(o, reads, writes, wpart)
    P.ops["pool"].append(o)
    P.order.append(o)
    return o


def phase_route(c, layer, TEi):
    nc, P, k, d, cfg = c.nc, c.P, c.k, c.d, c.cfg
    S, E, NT, NTILE, NSLOT = cfg.S, cfg.E, cfg.NT, cfg.NTILE, cfg.NSLOT
    with ExitStack() as ph:
        MKf = sb(ph, nc, [128, NT, E], F32, name="mkf")
        MKb = sb(ph, nc, [128, NT, E], BF16, name="mkb")
        CMB = sb(ph, nc, [128, NT, E], F32, name="cmb")
        UTf = sb(ph, nc, [128, 128], F32, name="utf")
        UT = sb(ph, nc, [128, 128], BF16, name="ut")
        ONb = sb(ph, nc, [128, 128], BF16, name="onb")
        IOTA = sb(ph, nc, [128, 1], F32, name="iota")
        TH = sb(ph, nc, [1, E * 16], F32, name="th")
        J5 = sb(ph, nc, [1, NTILE * E], F32, name="j5")
        dSLOT, dInit = Buf("dSLOT"), Buf("dInit")
        k.dma("sp", d["SLOT"], d["k_slotinit"], w=[dSLOT, dInit])
        k.dma("sp", MKf.t[:], d["MK"].rearrange("(t p) e -> p t e", p=128), w=[MKf.b])
        k.dma("sp", CMB.t[:], d["COMB"].rearrange("(t p) e -> p t e", p=128), w=[CMB.b])
        k.dma("sp", UTf.t[:], d["k_ut"], w=[UTf.b])
        k.dma("sp", IOTA.t[:], d["k_iota"], w=[IOTA.b])
        k.dma("sp", TH.t[:], d["k_th"], w=[TH.b])
        k.dma("sp", J5.t[:], d["k_j512"], w=[J5.b])
        k.cp("dve", UT.t[:], UTf.t[:], r=[UTf.b], w=[UT.b])
        k.cp("pool", MKb.t[:], MKf.t[:], r=[MKf.b], w=[MKb.b])
        k.memset("dve", ONb.t[:], 1.0, w=[ONb.b])
        pC = ps(ph, nc, [1, E], F32, name="pC")
        pSB = ps(ph, nc, [128, E], F32, name="pSB")
        pR = ps(ph, nc, [128, E], F32, n=2, name="pR")
        V = sb(ph, nc, [1, 8 * E], F32, name="v")
        C16 = sb(ph, nc, [1, E * 16], F32, name="c16")
        CJ = sb(ph, nc, [1, NTILE * E], F32, name="cj")
        TEf = sb(ph, nc, [1, NTILE], F32, name="tef")
        SEGB = sb(ph, nc, [128, E], F32, name="segb")
        for t in range(NT):
            k.mm(pC.t[:], ONb.t[:, 0:1], MKb.t[:, t, :], t == 0, t == NT - 1, r=[ONb.b, MKb.b], w=[pC.b] if t == 0 else (), wp=() if t == 0 else [pC.b])
        cnt, ntl, c512, inc, segs, one = (V.t[:, i * E:(i + 1) * E] for i in range(6))
        k.cp("dve", cnt, pC.t[:], r=[pC.b], wp=[V.b])
        k.tt("dve", C16.t[:].rearrange("o (e m) -> o e m", m=16), cnt.unsqueeze(2).to_broadcast([1, E, 16]), TH.t[:].rearrange("o (e m) -> o e m", m=16), ALU.is_gt,
             r=[V.b, TH.b], w=[C16.b])
        k.red(ntl, C16.t[:].rearrange("o (e m) -> o e m", m=16), r=[C16.b], wp=[V.b])
        k.ts("dve", c512, ntl, 512.0, ALU.mult, r=[V.b], wp=[V.b])
        k.memset("dve", one, 1.0, wp=[V.b])
        P.op("dve", (lambda e_, o=inc, a=one, b_=c512: e_.tensor_tensor_scan(out=o, data0=a, data1=b_, initial=0.0, op0=ALU.mult, op1=ALU.add)),
             reads=[V.b], wpart=[V.b])
        k.tt("dve", segs, inc, c512, ALU.subtract, r=[V.b], wp=[V.b])
        k.tt("dve", CJ.t[:].rearrange("o (j e) -> o j e", e=E), segs.unsqueeze(1).to_broadcast([1, NTILE, E]), J5.t[:].rearrange("o (j e) -> o j e", e=E), ALU.is_le,
             r=[V.b, J5.b], w=[CJ.b])
        k.red(TEf.t[:], CJ.t[:].rearrange("o (j e) -> o j e", e=E), r=[CJ.b], w=[TEf.b])
        k.ts("dve", TEf.t[:], TEf.t[:], -1.0, ALU.add, 0.0, ALU.max, r=[TEf.b], w=[TEf.b])
        IDXW, IDXB, P4all = TEi
        KP = sb(ph, nc, [128, 9], F32, name="kp")
        k.dma("sp", KP.t[:], d["k_kp"], w=[KP.b])
        pTE = ps(ph, nc, [128, NTILE], F32, name="pTE")
        TEb = sb(ph, nc, [128, NTILE], F32, name="teb")
        XW = sb(ph, nc, [128, NTILE, 8], F32, name="xw")
        k.mm(pTE.t[:], c.ones_f.t[0:1, :], TEf.t[:], True, True, r=[TEf.b, c.ones_f.b], w=[pTE.b])
        k.cp("dve", TEb.t[:], pTE.t[:], r=[pTE.b], w=[TEb.b])
        for kk in range(8):
            k.ts("dve", XW.t[:, :, kk], TEb.t[:], 1024.0, ALU.mult, KP.t[:, kk:kk + 1], ALU.add, r=[TEb.b, KP.b], wp=[XW.b])
        if layer:
            k.ts("dve", XW.t[:], XW.t[:], float(layer * E * 1024), ALU.add, r=[XW.b], w=[XW.b])
        k.cp("dve", IDXW.t[:], XW.t[:], r=[XW.b], w=[IDXW.b])
        k.ts("dve", TEb.t[:], TEb.t[:], 16.0, ALU.mult, KP.t[:, 8:9], ALU.add, r=[TEb.b, KP.b], w=[TEb.b])
        if layer:
            k.ts("dve", TEb.t[:], TEb.t[:], float(layer * E * 16), ALU.add, r=[TEb.b], w=[TEb.b])
        k.cp("dve", IDXB.t[:], TEb.t[:], r=[TEb.b], w=[IDXB.b])
        k.mm(pSB.t[:], c.ones_f.t[0:1, :], segs, True, True, r=[V.b, c.ones_f.b], w=[pSB.b])
        k.cp("dve", SEGB.t[:], pSB.t[:], r=[pSB.b], w=[SEGB.b])
        POS = sb(ph, nc, [128, E], F32, n=2, name="pos")
        T8 = sb(ph, nc, [128, 8], F32, n=2, name="t8")
        OH = sb(ph, nc, [128, E], F32, n=2, name="oh")
        JK = sb(ph, nc, [128, E], F32, n=2, name="jk")
        P4 = sb(ph, nc, [128, 4], F32, n=2, name="p4")
        P4i = sb(ph, nc, [128, 4], I32, n=2, name="p4i")
        SR = sb(ph, nc, [128, 4, 2], F32, n=2, name="sr")
        for i in range(NT):
            pr = pR[i % 2]
            pos, t8, p4, p4i, sr = POS[i % 2], T8[i % 2], P4[i % 2], P4i[i % 2], SR[i % 2]
            for ip in range(i):
                k.mm(pr.t[:], ONb.t[:], MKb.t[:, ip, :], ip == 0, False, r=[ONb.b, MKb.b], w=[pr.b] if ip == 0 else (), wp=() if ip == 0 else [pr.b])
            k.mm(pr.t[:], UT.t[:], MKb.t[:, i, :], i == 0, True, r=[UT.b, MKb.b], w=[pr.b] if i == 0 else (), wp=() if i == 0 else [pr.b])
            k.tt("dve", pos.t[:], pr.t[:], SEGB.t[:], ALU.add, r=[pr.b, SEGB.b], w=[pos.b])
            P.op("dve", (lambda e_, o=t8.t[:], a=CMB.t[:, i, :]: e_.max(out=o, in_=a)), reads=[CMB.b], writes=[t8.b])
            for kq in range(4):
                oh, jk = OH[kq % 2], JK[kq % 2]
                k.ts("dve", oh.t[:], CMB.t[:, i, :], t8.t[:, kq:kq + 1], ALU.is_equal, r=[CMB.b, t8.b], w=[oh.b])
                k.stt(jk.t[:], oh.t[:], 1.0, pos.t[:], ALU.mult, ALU.mult, r=[oh.b, pos.b], w=[jk.b], wp=[p4.b], accum=p4.t[:, kq:kq + 1])
                k.ts("pool", sr.t[:, kq, 0:1], IOTA.t[:], float(i * 128), ALU.add, r=[IOTA.b], wp=[sr.b])
                k.cp("pool", sr.t[:, kq, 1:2], t8.t[:, kq:kq + 1], r=[t8.b], wp=[sr.b])
            k.ts("dve", p4.t[:], p4.t[:], float(NSLOT - 1), ALU.min, r=[p4.b], w=[p4.b])
            k.cp("dve", p4i.t[:], p4.t[:], r=[p4.b], w=[p4i.b])
            k.cp("dve", P4all.t[:, i, :], p4.t[:], r=[p4.b], wp=[P4all.b])
            for kq in range(4):
                def sca(e_, off=p4i.t[:, kq:kq + 1], src=sr.t[:, kq, :]):
                    return e_.indirect_dma_start(out=d["SLOT"], out_offset=bass.IndirectOffsetOnAxis(ap=off, axis=0), in_=src, in_offset=None)
                pool_dma_op(P, sca, reads=[p4i.b, sr.b, dInit], wpart=[dSLOT])
        P.end_phase()


def phase_smoe(c, layer, MOD5, TEi):
    nc, P, k, d, cfg = c.nc, c.P, c.k, c.d, c.cfg
    S, E, NTILE = cfg.S, cfg.E, cfg.NTILE
    IDXW, IDXB, P4all = TEi
    hin = d["H1"] if layer == 0 else d["H3"]
    hout = d["H2"] if layer == 0 else d["out"]
    w1tab = d["moe_w1"].rearrange("l e k n -> (l e k) n")
    w2tab = d["moe_w2"].rearrange("l e k n -> (l e k) n")
    b1tab = d["moe_b1"].rearrange("l r f -> (l r) f")
    with ExitStack() as ph:
        Z = sb(ph, nc, [128, 1024], BF16, name="z")
        W1 = sb(ph, nc, [128, 8, 2048], BF16, n=2, name="w1")
        W2 = sb(ph, nc, [128, 8, 1024], BF16, name="w2")
        B1r = sb(ph, nc, [128, 128], F32, n=2, name="b1r")
        B1c = sb(ph, nc, [128, 16], F32, n=2, name="b1c")
        SLt = sb(ph, nc, [128, 4, 2], F32, n=2, name="slt")
        TKi = sb(ph, nc, [128, 4], I32, n=2, name="tki")
        XG = sb(ph, nc, [128, 4, 1024], BF16, n=2, name="xg")
        XT = sb(ph, nc, [128, 8, 512], BF16, n=2, name="xt")
        ACTT = sb(ph, nc, [128, 8, 512], BF16, n=2, name="actt")
        G1 = sb(ph, nc, [128, 512], F32, n=3, name="g1")
        S1 = sb(ph, nc, [128, 512], F32, n=3, name="s1")
        L2 = sb(ph, nc, [128, 512], F32, n=3, name="l2")
        GS = sb(ph, nc, [128, 512], F32, n=3, name="gs")
        OS = sb(ph, nc, [128, 1024], F32, n=4, name="os")
        pT = ps(ph, nc, [128, 1024], BF16, n=2, name="pT")
        pG = ps(ph, nc, [128, 512], F32, n=2, name="pG")
        pL = ps(ph, nc, [128, 512], F32, n=2, name="pL")
        pO = ps(ph, nc, [128, 512], F32, n=2, name="pO")
        dACC, dXM2z, dOUT = Buf("dACCs"), Buf("dXM2z"), Buf("dOUT")
        k.memset("dve", Z.t[:], 0.0, w=[Z.b])
        k.dma("sp", d["XM2"][S:S + 128, :], Z.t[:], r=[Z.b], w=[dXM2z])

        def gather(out_ap, tab, idx_ap, reads, wslot, part=False):
            def g(e_):
                return e_.indirect_dma_start(out=out_ap, out_offset=None, in_=tab, in_offset=bass.IndirectOffsetOnAxis(ap=idx_ap, axis=0))
            return pool_dma_op(P, g, reads=reads, writes=() if part else [wslot], wpart=[wslot] if part else ())

        def loads(j):
            w1, b1r, slt, tki, xg = W1[j % 2], B1r[j % 2], SLt[j % 2], TKi[j % 2], XG[j % 2]
            k.dma("sp", slt.t[:], d["SLOT"][j * 512:(j + 1) * 512, :].rearrange("(s p) c -> p s c", p=128), w=[slt.b])
            k.cp("dve", tki.t[:], slt.t[:, :, 0], r=[slt.b], w=[tki.b])
            for kk in range(8):
                gather(w1.t[:, kk, :], w1tab, IDXW.t[:, j, kk:kk + 1], [IDXW.b], w1.b, part=True)
            gather(b1r.t[:], b1tab, IDXB.t[:, j:j + 1], [IDXB.b], b1r.b)
            for sub in range(4):
                gather(xg.t[:, sub, :], d["XM2"], tki.t[:, sub:sub + 1], [tki.b, dXM2z], xg.b, part=True)

        io = 0
        loads(0)
        for j in range(NTILE):
            if j + 1 < NTILE:
                loads(j + 1)
            w1, b1r, b1c, slt, tki, xg, xt, actt = (X[j % 2] for X in (W1, B1r, B1c, SLt, TKi, XG, XT, ACTT))
            k.tr(pG[0].t[:, 0:16], b1r.t[0:16, :], c.ident_f.t[0:16, 0:16], r=[b1r.b], w=[pG[0].b])
            k.cp("dve", b1c.t[:, 0:8], pG[0].t[:, 0:8], r=[pG[0].b], wp=[b1c.b])
            k.ts("dve", b1c.t[:, 8:16], pG[0].t[:, 8:16], 1.0, ALU.add, r=[pG[0].b], wp=[b1c.b])
            for sub in range(4):
                pt = pT[sub % 2]
                for kk in range(8):
                    k.tr(pt.t[:, kk * 128:(kk + 1) * 128], xg.t[:, sub, kk * 128:(kk + 1) * 128], c.ident_b.t[:], r=[xg.b],
                         w=[pt.b] if kk == 0 else (), wp=() if kk == 0 else [pt.b])
                k.cp("act" if sub % 2 else "dve", xt.t[:, :, sub * 128:(sub + 1) * 128], pt.t[:].rearrange("p (k t) -> p k t", k=8), r=[pt.b], wp=[xt.b])
            for pr in range(8):
                pg, pl = pG[pr % 2], pL[pr % 2]
                g1, s1, l2, gs = (X[pr % 3] for X in (G1, S1, L2, GS))
                for kk in range(8):
                    k.mm(pg.t[:], w1.t[:, kk, pr * 128:(pr + 1) * 128], xt.t[:, kk, :], kk == 0, kk == 7,
                         r=[w1.b, xt.b], w=[pg.b] if kk == 0 else (), wp=() if kk == 0 else [pg.b])
                for kk in range(8):
                    k.mm(pl.t[:], w1.t[:, kk, 1024 + pr * 128:1024 + (pr + 1) * 128], xt.t[:, kk, :], kk == 0, kk == 7,
                         r=[w1.b, xt.b], w=[pl.b] if kk == 0 else (), wp=() if kk == 0 else [pl.b])
                k.ts("dve", g1.t[:], pg.t[:], b1c.t[:, pr:pr + 1], ALU.add, 7.0, ALU.min, r=[pg.b, b1c.b], w=[g1.b])
                k.act(s1.t[:], g1.t[:], AF.Sigmoid, r=[g1.b], w=[s1.b], scale=1.702)
                k.ts("dve", l2.t[:], pl.t[:], b1c.t[:, 8 + pr:9 + pr], ALU.add, 8.0, ALU.min, r=[pl.b, b1c.b], w=[l2.b])
                k.tt("dve", gs.t[:], g1.t[:], s1.t[:], ALU.mult, r=[g1.b, s1.b], w=[gs.b])
                k.stt(actt.t[:, pr, :], l2.t[:], -6.0, gs.t[:], ALU.max, ALU.mult, r=[l2.b, gs.b], wp=[actt.b])
            for kk in range(8):
                gather(W2.t[:, kk, :], w2tab, IDXW.t[:, j, kk:kk + 1], [IDXW.b], W2.b, part=True)
            for sub in range(4):
                os_ = OS[(4 * j + sub) % 4]
                for half in range(2):
                    po = pO[io % 2]
                    io += 1
                    for jj in range(8):
                        k.mm(po.t[:], actt.t[:, jj, sub * 128:(sub + 1) * 128], W2.t[:, jj, half * 512:(half + 1) * 512], jj == 0, jj == 7,
                             r=[actt.b, W2.b], w=[po.b] if jj == 0 else (), wp=() if jj == 0 else [po.b])
                    k.act(os_.t[:, half * 512:(half + 1) * 512], po.t[:], AF.Copy, r=[po.b, slt.b], wp=[os_.b], scale=slt.t[:, sub, 1:2])

                k.dma("sp", d["OUTS"][j * 512 + sub * 128:j * 512 + (sub + 1) * 128, :], os_.t[:], r=[os_.b], wp=[dACC])
        GA = sb(ph, nc, [128, 1024], F32, n=2, name="ga")
        for t in range(S // 128):
            ht_, at_ = OS[t % 2], OS[2 + t % 2]
            rows = slice(t * 128, (t + 1) * 128)
            k.dma("sp", ht_.t[:], hin[rows, :], w=[ht_.b])
            k.dma("sp", at_.t[:], d["ACCd"][rows, :], w=[at_.b])
            for kq in range(4):
                ga = GA[kq % 2]
                gather(ga.t[:], d["OUTS"], P4all.t[:, t, kq:kq + 1], [P4all.b, dACC], ga.b)
                k.tt("dve", at_.t[:], at_.t[:], ga.t[:], ALU.add, r=[at_.b, ga.b], w=[at_.b])
            k.tt("dve", at_.t[:], at_.t[:], MOD5.t[:], ALU.mult, r=[at_.b, MOD5.b], w=[at_.b])
            k.tt("pool", ht_.t[:], ht_.t[:], at_.t[:], ALU.add, r=[ht_.b, at_.b], w=[ht_.b])
            k.dma("sp", hout[rows, :], ht_.t[:], r=[ht_.b], wp=[dOUT])
        P.end_phase()
```

```python
from contextlib import ExitStack
import numpy as np
import ml_dtypes
import concourse.bass as bass
import concourse.mybir as mybir
from concourse.bass_utils import run_bass_kernel_spmd

F32 = mybir.dt.float32
BF16 = mybir.dt.bfloat16
AF = mybir.ActivationFunctionType
ALU = mybir.AluOpType
AX = mybir.AxisListType

COMPUTE = ("pe", "act", "dve", "pool")
ALLENG = ("pe", "act", "dve", "pool", "sp")
NDSEM = 72


class Buf:
    __slots__ = ("name", "writers", "readers")

    def __init__(self, name):
        self.name = name
        self.writers = []
        self.readers = []


class Op:
    __slots__ = ("eng", "fn", "raw", "oth", "signal", "tok_sem", "tok_val", "is_dma", "key", "phase")

    def __init__(self, eng, fn, phase):
        self.eng = eng
        self.fn = fn
        self.raw = []
        self.oth = []
        self.signal = False
        self.tok_sem = None
        self.tok_val = 0
        self.is_dma = False
        self.key = None
        self.phase = phase


class Prog:
    def __init__(self, nc, es):
        self.nc = nc
        self.phase = 0
        self.esem = {e: es.enter_context(nc.semaphore("s_" + e)) for e in COMPUTE}
        self.ecnt = {e: 0 for e in COMPUTE}
        self.dsem = [es.enter_context(nc.semaphore("d%d" % i)) for i in range(NDSEM)]
        self.dcnt = [0] * NDSEM
        self.seen = {e: {} for e in ALLENG}
        self._reset()
        self.nops = 0

    def _reset(self):
        self.ops = {e: [] for e in ALLENG}
        self.order = []
        self.keymap = {}
        self.last = {}

    def buf(self, name="b"):
        return Buf(name)

    def _deps(self, op, reads, writes, wpart):
        ph = self.phase
        for b in reads:
            for w in b.writers:
                if w.phase == ph:
                    op.raw.append(w)
        for b in list(writes) + list(wpart):
            for r in b.readers:
                if r.phase == ph:
                    op.oth.append(r)
        for b in writes:
            for w in b.writers:
                if w.phase == ph:
                    op.oth.append(w)
        for b in reads:
            b.readers.append(op)
        for b in writes:
            b.writers = [op]
            b.readers = []
        for b in wpart:
            if b.readers:
                b.writers = [op]
                b.readers = []
            else:
                b.writers.append(op)

    def op(self, eng, fn, reads=(), writes=(), wpart=()):
        o = Op(eng, fn, self.phase)
        self._deps(o, reads, writes, wpart)
        self.ops[eng].append(o)
        self.order.append(o)
        self.last[eng] = o
        return o

    def dma(self, q, out, in_, reads=(), writes=(), wpart=(), key=None, **kw):
        def fn(e, out=out, in_=in_, kw=kw):
            return e.dma_start(out=out, in_=in_, **kw)
        o = Op(q, fn, self.phase)
        o.is_dma = True
        if key is None:
            ws = list(writes) + list(wpart)
            key = ws[0]
        if key not in self.keymap:
            self.keymap[key] = len(self.keymap)
            assert len(self.keymap) <= NDSEM, "too many DMA keys in phase"
        o.key = self.keymap[key]
        self._deps(o, reads, writes, wpart)
        self.ops[q].append(o)
        self.order.append(o)
        return o

    def end_phase(self):
        nc = self.nc
        lasts = [self.last[e] for e in COMPUTE if e in self.last]
        lastd = {}
        for o in self.order:
            if o.is_dma:
                lastd[o.key] = o
        for e in ALLENG:
            o = Op(e, (lambda eng: eng.nop()), self.phase)
            o.raw = list(lasts) + list(lastd.values())
            self.ops[e].append(o)
            self.order.append(o)
        for o in self.order:
            for d in o.raw:
                if d.is_dma:
                    continue
                if d.eng == o.eng and o.eng == "pe" and not o.is_dma:
                    continue
                d.signal = True
            for d in o.oth:
                if d.is_dma:
                    continue
                if d.eng == o.eng and not o.is_dma:
                    continue
                d.signal = True
        for e in COMPUTE:
            for o in self.ops[e]:
                if o.is_dma:
                    continue
                if o.signal:
                    self.ecnt[e] += 1
                    o.tok_sem = self.esem[e]
                    o.tok_val = self.ecnt[e]
        for o in self.order:
            if o.is_dma:
                self.dcnt[o.key] += 16
                o.tok_sem = self.dsem[o.key]
                o.tok_val = self.dcnt[o.key]
        self.nops += len(self.order)

        with nc.Block() as block:
            def run(ename):
                def body(eng):
                    seen = self.seen[ename]
                    for o in self.ops[ename]:
                        need = {}
                        for d in o.raw:
                            if d.tok_sem is None:
                                continue
                            if (not d.is_dma) and d.eng == ename and ename == "pe" and not o.is_dma:
                                continue
                            s = d.tok_sem
                            if need.get(s.num, (None, 0))[1] < d.tok_val:
                                need[s.num] = (s, d.tok_val)
                        for d in o.oth:
                            if d.tok_sem is None:
                                continue
                            if (not d.is_dma) and d.eng == ename and not o.is_dma:
                                continue
                            s = d.tok_sem
                            if need.get(s.num, (None, 0))[1] < d.tok_val:
                                need[s.num] = (s, d.tok_val)
                        for s, v in need.values():
                            if seen.get(s.num, 0) < v:
                                eng.wait_ge(s, v)
                                seen[s.num] = v
                        ins = o.fn(eng)
                        if o.is_dma:
                            ins.then_inc(o.tok_sem, 16)
                        elif o.signal:
                            ins.then_inc(o.tok_sem, 1)
                return body

            block.tensor(run("pe"))
            block.scalar(run("act"))
            block.vector(run("dve"))
            block.gpsimd(run("pool"))
            block.sync(run("sp"))
        self.phase += 1
        self._reset()


class Slot:
    __slots__ = ("t", "b")

    def __init__(self, t, b):
        self.t = t
        self.b = b


class Ctx:
    pass


class K:
    def __init__(self, P):
        self.P = P

    def ts(self, eng, out, in0, s1, op0, s2=None, op1=None, r=(), w=(), wp=(), accum=None):
        if op1 is None:
            if accum is None:
                f = lambda e: e.tensor_scalar(out=out, in0=in0, scalar1=s1, scalar2=None, op0=op0)
            else:
                f = lambda e: e.tensor_scalar(out=out, in0=in0, scalar1=s1, scalar2=None, op0=op0, accum_out=accum)
        else:
            f = lambda e: e.tensor_scalar(out=out, in0=in0, scalar1=s1, scalar2=s2, op0=op0, op1=op1)
        return self.P.op(eng, f, reads=r, writes=w, wpart=wp)

    def tt(self, eng, out, in0, in1, op, r=(), w=(), wp=()):
        return self.P.op(eng, lambda e: e.tensor_tensor(out=out, in0=in0, in1=in1, op=op), reads=r, writes=w, wpart=wp)

    def stt(self, out, in0, scalar, in1, op0, op1, r=(), w=(), wp=(), accum=None):
        if accum is None:
            f = lambda e: e.scalar_tensor_tensor(out=out, in0=in0, scalar=scalar, in1=in1, op0=op0, op1=op1)
        else:
            f = lambda e: e.scalar_tensor_tensor(out=out, in0=in0, scalar=scalar, in1=in1, op0=op0, op1=op1, accum_out=accum)
        return self.P.op("dve", f, reads=r, writes=w, wpart=wp)

    def act(self, out, in_, func, r=(), w=(), wp=(), bias=None, scale=None, accum=None):
        kw = {}
        if bias is not None:
            kw["bias"] = bias
        if scale is not None:
            kw["scale"] = scale
        if accum is not None:
            kw["accum_out"] = accum
        return self.P.op("act", lambda e: e.activation(out=out, in_=in_, func=func, **kw), reads=r, writes=w, wpart=wp)

    def cp(self, eng, out, in_, r=(), w=(), wp=()):
        if eng == "act":
            return self.P.op("act", lambda e: e.copy(out=out, in_=in_), reads=r, writes=w, wpart=wp)
        return self.P.op(eng, lambda e: e.tensor_copy(out=out, in_=in_), reads=r, writes=w, wpart=wp)

    def memset(self, eng, ap, val, w=(), wp=()):
        return self.P.op(eng, lambda e: e.memset(ap, val), writes=w, wpart=wp)

    def mm(self, out, lhsT, rhs, start, stop, r=(), w=(), wp=()):
        return self.P.op("pe", lambda e: e.matmul(out, lhsT, rhs, start=start, stop=stop), reads=r, writes=w, wpart=wp)

    def tr(self, out, in_, ident, r=(), w=(), wp=()):
        return self.P.op("pe", lambda e: e.transpose(out, in_, ident), reads=r, writes=w, wpart=wp)

    def red(self, out, in_, r=(), w=(), wp=()):
        return self.P.op("dve", lambda e: e.tensor_reduce(out=out, in_=in_, axis=AX.X, op=ALU.add), reads=r, writes=w, wpart=wp)

    def recip(self, out, in_, r=(), w=(), wp=()):
        return self.P.op("dve", lambda e: e.reciprocal(out=out, in_=in_), reads=r, writes=w, wpart=wp)

    def dma(self, q, out, in_, r=(), w=(), wp=(), key=None):
        return self.P.dma(q, out, in_, reads=r, writes=w, wpart=wp, key=key)


class Cfg:
    def __init__(self, S=8192, L=256, E=32, debug=False, stop_after=None):
        self.S, self.L, self.E = S, L, E
        self.D = 1024
        self.NT = S // 128
        self.ROWS = S // 64
        self.NCH = S // 64
        self.MB = min(1024, S)
        self.NTILE = (4 * S) // 512 + E
        self.NSLOT = self.NTILE * 512
        self.debug = debug
        self.dense = False
        self.stop_after = stop_after


EPS = 1e-6
NEG = -30000.0


def host_consts(cfg):
    S = cfg.S
    NT = cfg.NT
    cs = {}
    cs["ident_f"] = np.eye(128, dtype=np.float32)
    p = np.arange(128)
    t = np.arange(64)
    s = p % 64
    cs["trif"] = (s[:, None] <= t[None, :]).astype(np.float32)
    cs["trib"] = (s[:, None] >= t[None, :]).astype(np.float32)
    r = np.ones((128, 512), np.float32)
    r[:, ::64] = 0.0
    cs["reset"] = r
    inv = (10000.0 ** (-np.arange(16, dtype=np.float32) / 16.0)).astype(np.float32)
    tt = np.arange(NT)
    row = (2 * tt[None, :] + (p[:, None] // 64)).astype(np.float32)
    col = np.broadcast_to((p % 64).astype(np.float32)[:, None], (128, NT))
    ang = np.stack([row[:, :, None] * inv[None, None, :], col[:, :, None] * inv[None, None, :]], axis=2)
    ang = ang.astype(np.float32)
    E, NTILE = cfg.E, cfg.NTILE
    cs["ut"] = (p[:, None] < p[None, :]).astype(np.float32)
    cs["iota"] = p.astype(np.float32).reshape(128, 1)
    kp = np.zeros((128, 9), np.float32)
    kp[:, :8] = np.arange(8)[None, :] * 128 + p[:, None]
    kp[:, 8] = p % 16
    cs["kp"] = kp
    cs["th"] = np.broadcast_to((512.0 * np.arange(16, dtype=np.float32))[None, None, :], (1, E, 16)).reshape(1, E * 16).copy()
    cs["j512"] = np.broadcast_to((512.0 * np.arange(NTILE, dtype=np.float32))[None, :, None], (1, NTILE, E)).reshape(1, NTILE * E).copy()
    si = np.zeros((cfg.NSLOT, 2), np.float32)
    si[:, 0] = S + (np.arange(cfg.NSLOT) % 128)
    cs["slotinit"] = si
    cs["cos"] = np.cos(ang).astype(np.float32).reshape(128, NT * 32)
    cs["sin"] = np.sin(ang).astype(np.float32).reshape(128, NT * 32)
    return cs


def layout_rpb(rpb):
    H = rpb.shape[0]
    c = np.arange(64)[:, None]
    kc = np.arange(64)[None, :]
    win = np.clip(c - 8, 0, 48)
    valid = (kc >= win) & (kc < win + 16)
    idx = np.clip(kc - c + 15, 0, 30)
    g = rpb[:, :, idx]
    g = np.where(valid[None, None], g, np.float32(NEG)).astype(np.float32)
    return np.ascontiguousarray(g.transpose(2, 0, 1, 3)).reshape(64, H * 15 * 64)


_uid = [0]


def sb(es, nc, shape, dt, n=1, name="t"):
    out = []
    for i in range(n):
        _uid[0] += 1
        t = es.enter_context(nc.sbuf_tensor("%s_%d" % (name, _uid[0]), list(shape), dt))
        out.append(Slot(t, Buf(name)))
    return out if n > 1 else out[0]


def ps(es, nc, shape, dt, n=1, name="p"):
    out = []
    for i in range(n):
        _uid[0] += 1
        t = es.enter_context(nc.psum_tensor("%s_%d" % (name, _uid[0]), list(shape), dt))
        out.append(Slot(t, Buf(name)))
    return out if n > 1 else out[0]


def declare_io(nc, cfg):
    S, L, E = cfg.S, cfg.L, cfg.E
    d = {}

    inputs = set()
    d["_inputs"] = inputs

    def inp(name, shape, dt=F32):
        d[name] = nc.dram_tensor(name, list(shape), dt, kind="ExternalInput").ap()
        inputs.add(name)

    inp("x", [S, 1024]); inp("c", [8, 128]); inp("ctx", [L, 1024]); inp("c_ctx", [8, 128])
    inp("ada_w", [2, 1024, 6144]); inp("ada_b", [2, 6144]); inp("norm1_g", [2, 1024]); inp("norm2_g", [2, 1024])
    inp("ab_w_in", [1024, 4096]); inp("ab_w_out", [1024, 1024]); inp("nat_q_norm", [1, 64]); inp("nat_k_norm", [1, 64])
    inp("rpb_full", [64, 8 * 15 * 64]); inp("hgrn_lb", [16, 128]); inp("hgrn_o_norm", [1, 128])
    inp("conv_w1", [1024, 2048]); inp("conv_b1", [16, 128]); inp("conv_dw", [31, 1024]); inp("conv_dw_b", [8, 128])
    inp("conv_ln_g", [8, 128]); inp("conv_ln_b", [8, 128]); inp("conv_w2", [1024, 1024]); inp("conv_b2", [1, 1024])
    inp("router_w", [2, 1024, E]); inp("router_b", [2, E])
    inp("moe_w1", [2, E, 1024, 2048]); inp("moe_b1", [2, E * 16, 128]); inp("moe_w2", [2, E, 1024, 1024]); inp("moe_b2", [2, E, 1024])
    inp("k_ident_f", [128, 128]); inp("k_trif", [128, 64]); inp("k_trib", [128, 64]); inp("k_reset", [128, 512])
    inp("k_cos", [128, cfg.NT * 32]); inp("k_sin", [128, cfg.NT * 32])
    inp("k_ut", [128, 128]); inp("k_iota", [128, 1]); inp("k_th", [1, E * 16]); inp("k_j512", [1, cfg.NTILE * E])
    inp("k_slotinit", [cfg.NSLOT, 2]); inp("k_kp", [128, 9])
    d["out"] = nc.dram_tensor("out", [S, 1024], F32, kind="ExternalOutput").ap()
    kind = "ExternalOutput" if cfg.debug else "Internal"

    def scr(name, shape, dt):
        d[name] = nc.dram_tensor(name, list(shape), dt, kind=kind).ap()

    scr("XT", [128, 8, S], BF16); scr("XTc", [128, 8, L], BF16)
    scr("QTr", [128, 4, S], BF16); scr("QTf", [128, 4, S], BF16); scr("KTr", [128, 4, S], BF16)
    scr("KcT", [128, 4, L], BF16)
    scr("VA", [S, 520], BF16); scr("VcA", [L, 520], BF16)
    scr("VH", [S, 512], BF16); scr("VHc", [L, 512], BF16)
    scr("G", [S, 512], F32)
    scr("HQ", [2, 128, 4, S], BF16); scr("HK", [2, 128, 4, S], BF16)
    scr("KH", [2, S, 512], BF16); scr("KHc", [2, L, 512], BF16)
    scr("DEC", [2, 128, 4, S // 64], F32); scr("DECc", [2, 128, 4, L // 64], F32)
    scr("OF", [S, 512], F32)
    scr("CAT", [S, 1024], BF16)
    scr("H1", [S, 1024], F32); scr("H2", [S, 1024], F32); scr("H3", [S, 1024], F32)
    scr("XT2", [128, 8, S], BF16)
    scr("COMB", [S, E], F32)
    scr("ACC0", [S, 1024], F32)
    scr("GT", [128, 8, S + 32], BF16)
    scr("YD", [128, 8, S], F32)
    scr("XM2", [S + 128, 1024], BF16)
    scr("MK", [S, E], F32)
    scr("ACCd", [S + 128, 1024], F32)
    scr("SLOT", [cfg.NSLOT, 2], F32)
    scr("OUTS", [cfg.NSLOT, 1024], F32)
    return d


def build_program(cfg):
    nc = bass.Bass("TRN2", target_bir_lowering=False, dynamic_dma_scratch_size=32768)
    c = Ctx()
    c.nc, c.cfg = nc, cfg
    c.d = declare_io(nc, cfg)
    c.inputs = c.d.pop("_inputs")
    with ExitStack() as ges:
        P = Prog(nc, ges)
        c.P = P
        c.k = K(P)
        c.ident_f = sb(ges, nc, [128, 128], F32, name="identf")
        c.ident_b = sb(ges, nc, [128, 128], BF16, name="identb")
        c.ones_f = sb(ges, nc, [128, 128], F32, name="onesf")
        c.k.dma("sp", c.ident_f.t[:], c.d["k_ident_f"], w=[c.ident_f.b])
        c.k.cp("dve", c.ident_b.t[:], c.ident_f.t[:], r=[c.ident_f.b], w=[c.ident_b.b])
        c.k.memset("dve", c.ones_f.t[:], 1.0, w=[c.ones_f.b])
        touch = sb(ges, nc, [1, 64], F32, name="touch")
        for i, nm in enumerate(sorted(c.inputs)):
            ap = c.d[nm]
            idx = tuple([0] * (len(ap.shape) - 2) + [slice(0, 1), slice(0, 1)])
            c.k.dma("sp", touch.t[0:1, i:i + 1], ap[idx], wp=[touch.b])
        if cfg.debug:
            tb = sb(ges, nc, [1, 2], BF16, name="touchb")
            c.k.memset("dve", touch.t[0:1, 62:64], 0.0, wp=[touch.b])
            c.k.memset("dve", tb.t[:], 0.0, w=[tb.b])
            for i, (nm, ap) in enumerate(c.d.items()):
                if nm not in c.inputs:
                    idx = tuple([0] * (len(ap.shape) - 2) + [slice(0, 1), slice(0, 1)])
                    src = tb.t[0:1, 0:1] if ap.dtype == BF16 else touch.t[0:1, 63:64]
                    c.k.dma("sp", ap[idx], src, r=[tb.b, touch.b], w=[Buf("o")])
        P.end_phase()
        def dbg_stop(name):
            return cfg.stop_after == name

        done = False
        for layer in (0, 1):
            with ExitStack() as lay:
                c.MOD = sb(lay, nc, [128, 6144], F32, name="MOD")
                if layer == 0:
                    c.CMOD = sb(lay, nc, [128, 2048], F32, name="CMOD")
                    seq = [("mods0", lambda: phase_mods(c, 0)), ("a1", lambda: phase_a1(c)), ("a2", lambda: phase_a2(c)),
                           ("nat", lambda: phase_nat(c)), ("hgrn", lambda: phase_hgrn(c)), ("post0", lambda: phase_post(c, 0))]
                else:
                    seq = [("mods1", lambda: phase_mods(c, 1)), ("f", lambda: phase_f(c)), ("g1", lambda: phase_g1(c)),
                           ("post1", lambda: phase_post(c, 1))]
                for name, fn in seq:
                    fn()
                    if dbg_stop(name):
                        done = True
                        break
            if done:
                break
            with ExitStack() as m5:
                MOD5 = sb(m5, nc, [128, 1024], F32, name="MOD5")
                with ExitStack() as ph:
                    emit_mods(c, ph, layer, [10, 11], lambda ct: MOD5.t[:, (ct - 10) * 512:(ct - 9) * 512], MOD5.b, False)
                    P.end_phase()
                if cfg.dense:
                    phase_moe(c, layer, MOD5)
                else:
                    TEi = (sb(m5, nc, [128, cfg.NTILE, 8], mybir.dt.int32, name="IDXW"), sb(m5, nc, [128, cfg.NTILE], mybir.dt.int32, name="IDXB"),
                           sb(m5, nc, [128, cfg.NT, 4], mybir.dt.int32, name="P4all"))
                    phase_route(c, layer, TEi)
                    if dbg_stop("route%d" % layer):
                        break
                    phase_smoe(c, layer, MOD5, TEi)
            if dbg_stop("moe%d" % layer):
                break
    return nc, c


def emit_mods(c, ph, layer, cts, dst_fn, dst_buf, with_ctx):
    nc, P, k, d = c.nc, c.P, c.k, c.d
    crow = sb(ph, nc, [16, 128], F32, name="crow")
    crow2 = sb(ph, nc, [16, 128], F32, name="crow2")
    ccol = sb(ph, nc, [128, 16], F32, name="ccol")
    CB = sb(ph, nc, [128, 16, 128], F32, name="CB")
    AW = sb(ph, nc, [128, 8, 512], F32, n=2, name="AW")
    ABr = sb(ph, nc, [1, 512], F32, n=2, name="ABr")
    pT = ps(ph, nc, [128, 16], F32, name="pT")
    pM = ps(ph, nc, [128, 512], F32, n=2, name="pM")
    pC = ps(ph, nc, [128, 512], F32, n=2, name="pC")
    k.dma("sp", crow.t[0:8, :], d["c"], wp=[crow.b])
    k.dma("sp", crow.t[8:16, :], d["c_ctx"], wp=[crow.b])
    k.act(crow2.t[:], crow.t[:], AF.Silu, r=[crow.b], w=[crow2.b])
    k.tr(pT.t[:], crow2.t[:], c.ident_f.t[0:16, 0:16], r=[crow2.b], w=[pT.b])
    k.cp("dve", ccol.t[:], pT.t[:], r=[pT.b], w=[ccol.b])
    for j in range(16):
        k.cp("dve" if j % 2 else "pool", CB.t[:, j, :], ccol.t[:, j:j + 1].to_broadcast([128, 128]), r=[ccol.b], wp=[CB.b])
    awv = d["ada_w"][layer].rearrange("(k p) n -> p k n", p=128)
    for i, ct in enumerate(cts):
        aw, ab = AW[i % 2], ABr[i % 2]
        k.dma("sp", aw.t[:], awv[:, :, ct * 512:(ct + 1) * 512], w=[aw.b])
        k.dma("sp", ab.t[:], d["ada_b"][layer:layer + 1, ct * 512:(ct + 1) * 512], w=[ab.b])
        pm = pM[i % 2]
        for kk in range(8):
            k.mm(pm.t[:], CB.t[:, kk, :], aw.t[:, kk, :], kk == 0, False, r=[CB.b, aw.b], w=[pm.b] if kk == 0 else (), wp=() if kk == 0 else [pm.b])
        k.mm(pm.t[:], c.ones_f.t[0:1, :], ab.t[:], False, True, r=[ab.b], wp=[pm.b])
        k.cp("act", dst_fn(ct), pm.t[:], r=[pm.b], wp=[dst_buf])
        if with_ctx and ct < 4:
            pc = pC[i % 2]
            for kk in range(8):
                k.mm(pc.t[:], CB.t[:, 8 + kk, :], aw.t[:, kk, :], kk == 0, False, r=[CB.b, aw.b], w=[pc.b] if kk == 0 else (), wp=() if kk == 0 else [pc.b])
            k.mm(pc.t[:], c.ones_f.t[0:1, :], ab.t[:], False, True, r=[ab.b], wp=[pc.b])
            k.cp("dve", c.CMOD.t[:, ct * 512:(ct + 1) * 512], pc.t[:], r=[pc.b], wp=[c.CMOD.b])


def phase_mods(c, layer):
    with ExitStack() as ph:
        emit_mods(c, ph, layer, list(range(10)), lambda ct: c.MOD.t[:, ct * 512:(ct + 1) * 512], c.MOD.b, layer == 0)
        c.P.end_phase()


def make_A(c, ph, gname, layer, modcols, modt):
    nc, k, d = c.nc, c.k, c.d
    g = sb(ph, nc, [128, 1024], F32, name="gbc")
    A = sb(ph, nc, [128, 1024], F32, name="A")
    k.dma("sp", g.t[:], d[gname][layer:layer + 1, :].partition_broadcast(128), w=[g.b])
    k.stt(A.t[:], modt.t[:, modcols:modcols + 1024], 1.0, g.t[:], ALU.add, ALU.mult, r=[modt.b, g.b], w=[A.b])
    return A


def norm_mod(c, xt, A, SH, shb, outs, tmp, j):
    k = c.k
    ss, t1 = tmp["ss"], tmp["t1"]
    k.stt(t1.t[:], xt.t[:], 1.0, xt.t[:], ALU.mult, ALU.mult, r=[xt.b], w=[t1.b], wp=[ss.b], accum=ss.t[:, 4 * j:4 * j + 1])
    k.ts("dve", ss.t[:, 4 * j + 1:4 * j + 2], ss.t[:, 4 * j:4 * j + 1], 1.0 / 1024, ALU.mult, EPS, ALU.add, r=[ss.b], wp=[ss.b])
    k.act(ss.t[:, 4 * j + 2:4 * j + 3], ss.t[:, 4 * j + 1:4 * j + 2], AF.Sqrt, r=[ss.b], wp=[ss.b])
    k.recip(ss.t[:, 4 * j + 3:4 * j + 4], ss.t[:, 4 * j + 2:4 * j + 3], r=[ss.b], wp=[ss.b])
    k.stt(t1.t[:], xt.t[:], ss.t[:, 4 * j + 3:4 * j + 4], A.t[:], ALU.mult, ALU.mult, r=[xt.b, ss.b, A.b], w=[t1.b])
    for (ap, eng, slot) in outs:
        k.tt(eng, ap, t1.t[:], SH, ALU.add, r=[t1.b, shb], wp=[slot.b])


def phase_a1(c):
    nc, P, k, d, cfg = c.nc, c.P, c.k, c.d, c.cfg
    S, L, NT = cfg.S, cfg.L, cfg.NT
    with ExitStack() as ph:
        W = sb(ph, nc, [128, 8, 2560], BF16, name="Wtok")
        wv = d["ab_w_in"].rearrange("(k p) n -> p k n", p=128)
        for i, c0 in enumerate((0, 512, 1024, 3072, 3584)):
            k.dma("pool", W.t[:, :, i * 512:(i + 1) * 512], wv[:, :, c0:c0 + 512], wp=[W.b])
        A = make_A(c, ph, "norm1_g", 0, 1024, c.MOD)
        Ac = make_A(c, ph, "norm1_g", 0, 1024, c.CMOD)
        g64 = sb(ph, nc, [128, 128], F32, name="g64")
        GQ = sb(ph, nc, [128, 512], F32, name="GQ")
        GK = sb(ph, nc, [128, 512], F32, name="GK")
        k.dma("sp", g64.t[:, 0:64], d["nat_q_norm"].partition_broadcast(128), wp=[g64.b])
        k.dma("sp", g64.t[:, 64:128], d["nat_k_norm"].partition_broadcast(128), wp=[g64.b])
        k.ts("dve", GQ.t[:].rearrange("p (h e) -> p h e", h=8), g64.t[:, 0:64].unsqueeze(1).to_broadcast([128, 8, 64]), 0.125, ALU.mult, r=[g64.b], w=[GQ.b])
        k.ts("dve", GK.t[:].rearrange("p (h e) -> p h e", h=8), g64.t[:, 64:128].unsqueeze(1).to_broadcast([128, 8, 64]), 1.0, ALU.mult, r=[g64.b], w=[GK.b])
        COSG = sb(ph, nc, [128, 128], F32, n=2, name="COS")
        SING = sb(ph, nc, [128, 128], F32, n=2, name="SIN")
        XIN = sb(ph, nc, [128, 1024], F32, n=2, name="xin")
        tmps = [{"ss": sb(ph, nc, [128, 16], F32, name="ss"), "t1": sb(ph, nc, [128, 1024], F32, name="t1")} for _ in range(2)]
        XM = sb(ph, nc, [128, 1024], BF16, n=2, name="xm")
        XTG = sb(ph, nc, [128, 8, 512], BF16, n=2, name="xtg")
        pT = ps(ph, nc, [128, 1024], BF16, name="pT")
        pS = ps(ph, nc, [128, 512], F32, n=5, name="pS")
        pO = ps(ph, nc, [128, 1024], BF16, n=2, name="pO")
        SQs = [sb(ph, nc, [128, 1024], F32, name="sq")] * 2
        STs = sb(ph, nc, [128, 48], F32, n=2, name="st")
        QNs = [sb(ph, nc, [128, 1024], F32, name="qn")] * 2
        R1s = [sb(ph, nc, [128, 1024], F32, name="r1")] * 2
        R2s = [sb(ph, nc, [128, 1024], F32, name="r2")] * 2
        OB = sb(ph, nc, [128, 1536], BF16, n=2, name="ob")
        OTG = sb(ph, nc, [128, 3, 4, 512], BF16, n=2, name="otg")
        VAs = sb(ph, nc, [128, 8, 65], BF16, n=2, name="vas")
        VHs = sb(ph, nc, [128, 512], BF16, n=2, name="vhs")
        Gs = sb(ph, nc, [128, 512], F32, n=2, name="gs")
        for v in VAs:
            k.memset("pool", v.t[:, :, 64:65], 1.0, wp=[v.b])
        dXT, dXTc = Buf("dXT"), Buf("dXTc")
        dQ, dV, dVH, dG = Buf("dQ"), Buf("dV"), Buf("dVH"), Buf("dG")

        def run(src, ntile, Asl, modt, is_ctx):
            ngrp = (ntile + 3) // 4
            it = 0
            for g in range(ngrp):
                nj = min(4, ntile - 4 * g)
                xtg = XTG[g % 2]
                otg = OTG[g % 2]
                COS, SIN = COSG[g % 2], SING[g % 2]
                if not is_ctx:
                    k.dma("sp", COS.t[:, 0:nj * 32], d["k_cos"][:, g * 128:g * 128 + nj * 32], w=[COS.b])
                    k.dma("sp", SIN.t[:, 0:nj * 32], d["k_sin"][:, g * 128:g * 128 + nj * 32], w=[SIN.b])
                states = {}

                def head(j):
                    nonlocal it
                    t = 4 * g + j
                    xin, xm = XIN[it % 2], XM[it % 2]
                    ob, vas, vhs, gs = OB[it % 2], VAs[it % 2], VHs[it % 2], Gs[it % 2]
                    SQ, ST, QN, R1, R2 = SQs[it % 2], STs[it % 2], QNs[it % 2], R1s[it % 2], R2s[it % 2]
                    it += 1
                    k.dma("sp", xin.t[:], src[t * 128:(t + 1) * 128, :], w=[xin.b])
                    norm_mod(c, xin, Asl, modt.t[:, 0:1024], modt.b, [(xm.t[:], "pool", xm)], tmps[it % 2], j)
                    for kk in range(8):
                        k.tr(pT.t[:, kk * 128:(kk + 1) * 128], xm.t[:, kk * 128:(kk + 1) * 128], c.ident_b.t[:], r=[xm.b],
                             w=[pT.b] if kk == 0 else (), wp=() if kk == 0 else [pT.b])
                    k.cp("act", xtg.t[:, :, j * 128:(j + 1) * 128], pT.t[:].rearrange("p (k t) -> p k t", k=8), r=[pT.b], wp=[xtg.b])
                    cols = (1, 2, 3) if is_ctx else (0, 1, 2, 3, 4)
                    for ci in cols:
                        for kk in range(8):
                            k.mm(pS[ci].t[:], xtg.t[:, kk, j * 128:(j + 1) * 128], W.t[:, kk, ci * 512:(ci + 1) * 512], kk == 0, kk == 7,
                                 r=[xtg.b, W.b], w=[pS[ci].b] if kk == 0 else (), wp=() if kk == 0 else [pS[ci].b])
                    k.cp("act", vas.t[:, :, 0:64], pS[2].t[:].rearrange("p (h e) -> p h e", h=8), r=[pS[2].b], wp=[vas.b])
                    k.cp("dve", vhs.t[:], pS[3].t[:], r=[pS[3].b], w=[vhs.b])
                    rows = slice(t * 128, (t + 1) * 128)
                    if is_ctx:
                        k.dma("sp", d["VcA"][rows, :], vas.t[:].rearrange("p h e -> p (h e)"), r=[vas.b], wp=[dV])
                        k.dma("sp", d["VHc"][rows, :], vhs.t[:], r=[vhs.b], wp=[dVH])
                    else:
                        k.act(gs.t[:], pS[4].t[:], AF.Silu, r=[pS[4].b], w=[gs.b])
                        k.dma("sp", d["VA"][rows, :], vas.t[:].rearrange("p h e -> p (h e)"), r=[vas.b], wp=[dV])
                        k.dma("sp", d["VH"][rows, :], vhs.t[:], r=[vhs.b], wp=[dVH])
                        k.dma("sp", d["G"][rows, :], gs.t[:], r=[gs.b], wp=[dG])
                    srcs = ((1, 1),) if is_ctx else ((0, 0), (1, 1))
                    for (ci, slot_i) in srcs:
                        k.act(SQ.t[:, slot_i * 512:(slot_i + 1) * 512], pS[ci].t[:], AF.Square, r=[pS[ci].b], wp=[SQ.b])
                        k.red(ST.t[:, slot_i * 8:(slot_i + 1) * 8], SQ.t[:, slot_i * 512:(slot_i + 1) * 512].rearrange("p (h e) -> p h e", h=8), r=[SQ.b], wp=[ST.b])
                    k.ts("dve", ST.t[:, 16:32], ST.t[:, 0:16], 1.0 / 64, ALU.mult, EPS, ALU.add, r=[ST.b], wp=[ST.b])
                    k.act(ST.t[:, 32:48], ST.t[:, 16:32], AF.Sqrt, r=[ST.b], wp=[ST.b])
                    k.recip(ST.t[:, 16:32], ST.t[:, 32:48], r=[ST.b], wp=[ST.b])
                    for (ci, slot_i) in srcs:
                        qn = QN.t[:, slot_i * 512:(slot_i + 1) * 512]
                        k.tt("dve", qn.rearrange("p (h e) -> p h e", h=8), pS[ci].t[:].rearrange("p (h e) -> p h e", h=8),
                             ST.t[:, 16 + slot_i * 8:16 + slot_i * 8 + 8].unsqueeze(2).to_broadcast([128, 8, 64]), ALU.mult,
                             r=[pS[ci].b, ST.b], wp=[QN.b])
                        Gt = GQ if slot_i == 0 else GK
                        k.tt("pool", qn, qn, Gt.t[:], ALU.mult, r=[QN.b, Gt.b], wp=[QN.b])
                    if is_ctx:
                        k.cp("act", ob.t[:, 1024:1536], QN.t[:, 512:1024], r=[QN.b], wp=[ob.b])
                    else:
                        k.cp("act", ob.t[:, 512:1024], QN.t[:, 0:512], r=[QN.b], wp=[ob.b])
                        qv = QN.t[:].rearrange("p (h a b i) -> p h a b i", h=16, a=2, b=2)
                        cosb = COS.t[:, j * 32:(j + 1) * 32].rearrange("p (a i) -> p a i", a=2).unsqueeze(1).to_broadcast([128, 16, 2, 16])
                        sinb = SIN.t[:, j * 32:(j + 1) * 32].rearrange("p (a i) -> p a i", a=2).unsqueeze(1).to_broadcast([128, 16, 2, 16])
                        r1v = R1.t[:].rearrange("p (h a b i) -> p h a b i", h=16, a=2, b=2)
                        r2v = R2.t[:].rearrange("p (h a b i) -> p h a b i", h=16, a=2, b=2)
                        x1, x2 = qv[:, :, :, 0, :], qv[:, :, :, 1, :]
                        k.tt("dve", r1v[:, :, :, 0, :], x1, cosb, ALU.mult, r=[QN.b, COS.b], wp=[R1.b])
                        k.tt("pool", r2v[:, :, :, 0, :], x2, sinb, ALU.mult, r=[QN.b, SIN.b], wp=[R2.b])
                        k.tt("dve", r1v[:, :, :, 1, :], x2, cosb, ALU.mult, r=[QN.b, COS.b], wp=[R1.b])
                        k.tt("pool", r2v[:, :, :, 1, :], x1, sinb, ALU.mult, r=[QN.b, SIN.b], wp=[R2.b])
                        for slot_i, o0 in ((0, 0), (1, 1024)):
                            ov = ob.t[:, o0:o0 + 512].rearrange("p (h a b i) -> p h a b i", h=8, a=2, b=2)
                            a1 = r1v[:, slot_i * 8:(slot_i + 1) * 8]
                            a2 = r2v[:, slot_i * 8:(slot_i + 1) * 8]
                            k.tt("dve", ov[:, :, :, 0, :], a1[:, :, :, 0, :], a2[:, :, :, 0, :], ALU.subtract, r=[R1.b, R2.b], wp=[ob.b])
                            k.tt("dve", ov[:, :, :, 1, :], a1[:, :, :, 1, :], a2[:, :, :, 1, :], ALU.add, r=[R1.b, R2.b], wp=[ob.b])
                    states[j] = (ob,)

                def tail(j):
                    (ob,) = states[j]
                    which = (2,) if is_ctx else (0, 1, 2)
                    for wi in which:
                        po = pO[0] if wi < 2 else pO[1]
                        for hp in range(4):
                            col = ((wi % 2) * 4 + hp) * 128
                            first = (hp == 0 and wi in (0, 2))
                            k.tr(po.t[:, col:col + 128], ob.t[:, wi * 512 + hp * 128: wi * 512 + (hp + 1) * 128], c.ident_b.t[:], r=[ob.b],
                                 w=[po.b] if first else (), wp=() if first else [po.b])
                    if not is_ctx:
                        k.cp("act", otg.t[:, 0:2, :, j * 128:(j + 1) * 128], pO[0].t[:].rearrange("p (w h t) -> p w h t", w=2, h=4), r=[pO[0].b], wp=[otg.b])
                    k.cp("dve", otg.t[:, 2, :, j * 128:(j + 1) * 128], pO[1].t[:, 0:512].rearrange("p (h t) -> p h t", h=4), r=[pO[1].b], wp=[otg.b])

                head(0)
                for j in range(nj):
                    if j + 1 < nj:
                        head(j + 1)
                    tail(j)
                tok = slice(g * 512, g * 512 + nj * 128)
                w_ = nj * 128
                if is_ctx:
                    k.dma("sp", d["XTc"][:, :, tok], xtg.t[:, :, 0:w_], r=[xtg.b], wp=[dXTc])
                    k.dma("sp", d["KcT"][:, :, tok], otg.t[:, 2, :, 0:w_], r=[otg.b], wp=[dQ])
                else:
                    k.dma("sp", d["XT"][:, :, tok], xtg.t[:, :, 0:w_], r=[xtg.b], wp=[dXT])
                    k.dma("sp", d["QTr"][:, :, tok], otg.t[:, 0, :, 0:w_], r=[otg.b], wp=[dQ])
                    k.dma("sp", d["QTf"][:, :, tok], otg.t[:, 1, :, 0:w_], r=[otg.b], wp=[dQ])
                    k.dma("sp", d["KTr"][:, :, tok], otg.t[:, 2, :, 0:w_], r=[otg.b], wp=[dQ])

        run(d["ctx"], L // 128, Ac, c.CMOD, True)
        run(d["x"], NT, A, c.MOD, False)
        P.end_phase()


def core_inputs(inp, b, cfg, consts):
    f = lambda a: np.ascontiguousarray(np.asarray(a, dtype=np.float32))
    m = {
        "x": f(inp["x"][b]), "c": f(inp["c"][b]).reshape(8, 128), "ctx": f(inp["ctx"][b]),
        "c_ctx": f(inp["c_ctx"]).reshape(8, 128),
        "ada_w": f(inp["ada_w"]), "ada_b": f(inp["ada_b"]), "norm1_g": f(inp["norm1_g"]), "norm2_g": f(inp["norm2_g"]),
        "ab_w_in": f(inp["ab_w_in"][0]), "ab_w_out": f(inp["ab_w_out"][0]),
        "nat_q_norm": f(inp["nat_q_norm"][0]).reshape(1, 64), "nat_k_norm": f(inp["nat_k_norm"][0]).reshape(1, 64),
        "rpb_full": layout_rpb(f(inp["nat_rpb"][0])),
        "hgrn_lb": f(inp["hgrn_lb"]).reshape(16, 128), "hgrn_o_norm": f(inp["hgrn_o_norm"][0]).reshape(1, 128),
        "conv_w1": f(inp["conv_w1"][0]), "conv_b1": f(inp["conv_b1"][0]).reshape(16, 128), "conv_dw": f(inp["conv_dw"][0]),
        "conv_dw_b": f(inp["conv_dw_b"][0]).reshape(8, 128), "conv_ln_g": f(inp["conv_ln_g"][0]).reshape(8, 128),
        "conv_ln_b": f(inp["conv_ln_b"][0]).reshape(8, 128), "conv_w2": f(inp["conv_w2"][0]), "conv_b2": f(inp["conv_b2"][0]).reshape(1, 1024),
        "router_w": f(inp["router_w"]), "router_b": f(inp["router_b"]),
        "moe_w1": f(inp["moe_w1"]), "moe_b1": f(inp["moe_b1"]).reshape(2, cfg.E * 16, 128),
        "moe_w2": f(inp["moe_w2"]), "moe_b2": f(inp["moe_b2"]),
    }
    for kname, v in consts.items():
        m["k_" + kname] = v
    return m


_cache = {}


def kernel(**inputs):
    B = inputs["x"].shape[0]
    S = inputs["x"].shape[1]
    cfg = Cfg(S=S, L=inputs["ctx"].shape[1], E=inputs["moe_w1"].shape[1])
    key = (cfg.S, cfg.L, cfg.E)
    if key not in _cache:
        _cache[key] = build_program(cfg)[0]
    nc = _cache[key]
    consts = host_consts(cfg)
    in_maps = [core_inputs(inputs, b, cfg, consts) for b in range(B)]
    res = run_bass_kernel_spmd(nc, in_maps, core_ids=list(range(B)))
    return np.stack([np.asarray(r["out"], dtype=np.float32) for r in res.results], axis=0)


def phase_a2(c):
    nc, P, k, d, cfg = c.nc, c.P, c.k, c.d, c.cfg
    S, L = cfg.S, cfg.L
    with ExitStack() as ph:
        W = sb(ph, nc, [128, 8, 1536], BF16, name="Wfm")
        wv = d["ab_w_in"].rearrange("(k p) n -> p k n", p=128)
        for i in range(3):
            k.dma("pool", W.t[:, :, i * 512:(i + 1) * 512], wv[:, :, 1536 + i * 512:1536 + (i + 1) * 512], wp=[W.b])
        RESET = sb(ph, nc, [128, 512], F32, name="reset")
        k.dma("sp", RESET.t[:], d["k_reset"], w=[RESET.b])
        lbr = sb(ph, nc, [16, 128], F32, name="lbr")
        Ee = sb(ph, nc, [128, 16], F32, name="Ee")
        LB = sb(ph, nc, [128, 24], F32, name="LB")
        pL = ps(ph, nc, [128, 16], F32, name="pL")
        k.dma("sp", lbr.t[:], d["hgrn_lb"], w=[lbr.b])
        k.tr(pL.t[:], lbr.t[:], c.ident_f.t[0:16, 0:16], r=[lbr.b], w=[pL.b])
        k.act(Ee.t[:], pL.t[:], AF.Exp, r=[pL.b], w=[Ee.b])
        ev = Ee.t[:].rearrange("p (d j h) -> p d j h", d=2, j=2)
        k.tt("dve", LB.t[:, 16:24].rearrange("p (d h) -> p d h", d=2), ev[:, :, 0, :], ev[:, :, 1, :], ALU.add, r=[Ee.b], wp=[LB.b])
        k.recip(LB.t[:, 16:24], LB.t[:, 16:24], r=[LB.b], wp=[LB.b])
        k.tt("dve", LB.t[:, 0:8].rearrange("p (d h) -> p d h", d=2), ev[:, :, 0, :], LB.t[:, 16:24].rearrange("p (d h) -> p d h", d=2), ALU.mult, r=[Ee.b, LB.b], wp=[LB.b])
        k.ts("dve", LB.t[:, 8:16], LB.t[:, 0:8], -1.0, ALU.mult, 1.0, ALU.add, r=[LB.b], wp=[LB.b])

        XTG = sb(ph, nc, [128, 8, 512], BF16, n=2, name="xtg")
        names = ("q32", "sg", "f", "lf", "kk", "B", "e1", "e2", "t1", "r", "e3")
        TM = {n_: sb(ph, nc, [128, 512], F32, n=2, name=n_) for n_ in names}
        HQs = sb(ph, nc, [128, 2, 4, 512], BF16, n=2, name="hqs")
        HKs = sb(ph, nc, [128, 2, 4, 512], BF16, n=2, name="hks")
        KHT = sb(ph, nc, [128, 512], BF16, n=2, name="kht")
        KHs = sb(ph, nc, [128, 4, 2, 512], BF16, n=2, name="khs")
        DECs = sb(ph, nc, [128, 2, 4, 8], F32, n=2, name="decs")
        pQ = ps(ph, nc, [128, 512], F32, n=2, name="pQ")
        pF = ps(ph, nc, [128, 512], F32, n=3, name="pF")
        pK = ps(ph, nc, [128, 512], BF16, n=2, name="pK")
        dHQ, dHK, dKH, dDEC = Buf("dHQ"), Buf("dHK"), Buf("dKH"), Buf("dDEC")
        QS = 128.0 ** -0.5

        def run(src, ntok, is_ctx):
            ngrp = (ntok + 511) // 512
            it = 0
            for g in range(ngrp):
                n = min(512, ntok - g * 512)
                nch = n // 64
                nsub = n // 128
                xtg, hqs, hks, khs, decs = XTG[g % 2], HQs[g % 2], HKs[g % 2], KHs[g % 2], DECs[g % 2]
                k.dma("sp", xtg.t[:, :, 0:n], src[:, :, g * 512:g * 512 + n], w=[xtg.b])
                for h in range(4):
                    pq = pQ[h % 2]
                    if not is_ctx:
                        for kk_ in range(8):
                            k.mm(pq.t[:, 0:n], W.t[:, kk_, h * 128:(h + 1) * 128], xtg.t[:, kk_, 0:n], kk_ == 0, kk_ == 7,
                                 r=[W.b, xtg.b], w=[pq.b] if kk_ == 0 else (), wp=() if kk_ == 0 else [pq.b])
                        q32 = TM["q32"][h % 2]
                        k.act(q32.t[:, 0:n], pq.t[:, 0:n], AF.Silu, r=[pq.b], w=[q32.b])
                    for dd in range(2):
                        pf = pF[(2 * h + dd) % 3]
                        c0 = 512 + dd * 512 + h * 128
                        for kk_ in range(8):
                            k.mm(pf.t[:, 0:n], W.t[:, kk_, c0:c0 + 128], xtg.t[:, kk_, 0:n], kk_ == 0, kk_ == 7,
                                 r=[W.b, xtg.b], w=[pf.b] if kk_ == 0 else (), wp=() if kk_ == 0 else [pf.b])
                        tm = {n_: TM[n_][it % 2] for n_ in names}
                        kht = KHT[it % 2]
                        pk = pK[it % 2]
                        it += 1
                        sg, f, lf, kk, Bc, e1, e2, t1, rr, e3 = (tm[x] for x in ("sg", "f", "lf", "kk", "B", "e1", "e2", "t1", "r", "e3"))
                        li = dd * 4 + h
                        k.act(sg.t[:, 0:n], pf.t[:, 0:n], AF.Sigmoid, r=[pf.b], w=[sg.b])
                        k.ts("dve", f.t[:, 0:n], sg.t[:, 0:n], LB.t[:, 8 + li:9 + li], ALU.mult, LB.t[:, li:li + 1], ALU.add, r=[sg.b, LB.b], w=[f.b])
                        k.act(lf.t[:, 0:n], f.t[:, 0:n], AF.Ln, r=[f.b], w=[lf.b])
                        k.ts("pool", kk.t[:, 0:n], f.t[:, 0:n], -1.0, ALU.mult, 1.0, ALU.add, r=[f.b], w=[kk.b])
                        P.op("dve", (lambda e, o=Bc.t[:, 0:n], a=RESET.t[:, 0:n], b_=lf.t[:, 0:n]:
                                     e.tensor_tensor_scan(out=o, data0=a, data1=b_, initial=0.0, op0=ALU.mult, op1=ALU.add)),
                             reads=[RESET.b, lf.b], writes=[Bc.b])
                        Bv = Bc.t[:, 0:n].rearrange("p (c t) -> p c t", t=64)
                        Bend = Bv[:, :, 63:64].to_broadcast([128, nch, 64])
                        v3 = lambda s_: s_.t[:, 0:n].rearrange("p (c t) -> p c t", t=64)
                        if dd == 0:
                            k.act(e1.t[:, 0:n], Bc.t[:, 0:n], AF.Exp, r=[Bc.b], w=[e1.b])
                            k.act(e2.t[:, 0:n], Bc.t[:, 0:n], AF.Exp, r=[Bc.b], w=[e2.b], scale=-1.0)
                            k.tt("dve", v3(t1), Bend, Bv, ALU.subtract, r=[Bc.b], w=[t1.b])
                            k.act(e3.t[:, 0:n], t1.t[:, 0:n], AF.Exp, r=[t1.b], w=[e3.b])
                        else:
                            k.tt("dve", t1.t[:, 0:n], lf.t[:, 0:n], Bc.t[:, 0:n], ALU.subtract, r=[lf.b, Bc.b], w=[t1.b])
                            k.tt("dve", v3(rr), v3(t1), Bend, ALU.add, r=[t1.b, Bc.b], w=[rr.b])
                            k.act(e1.t[:, 0:n], rr.t[:, 0:n], AF.Exp, r=[rr.b], w=[e1.b])
                            k.act(e2.t[:, 0:n], rr.t[:, 0:n], AF.Exp, r=[rr.b], w=[e2.b], scale=-1.0)
                            k.act(e3.t[:, 0:n], t1.t[:, 0:n], AF.Exp, r=[t1.b], w=[e3.b], scale=-1.0)
                        k.act(decs.t[:, dd, h, 0:nch], Bv[:, :, 63], AF.Exp, r=[Bc.b], wp=[decs.b])
                        if not is_ctx:
                            q32 = TM["q32"][h % 2]
                            k.stt(hqs.t[:, dd, h, 0:n], q32.t[:, 0:n], QS, e1.t[:, 0:n], ALU.mult, ALU.mult, r=[q32.b, e1.b], wp=[hqs.b])
                            k.tt("pool", hks.t[:, dd, h, 0:n], kk.t[:, 0:n], e2.t[:, 0:n], ALU.mult, r=[kk.b, e2.b], wp=[hks.b])
                        k.tt("pool", kht.t[:, 0:n], kk.t[:, 0:n], e3.t[:, 0:n], ALU.mult, r=[kk.b, e3.b], w=[kht.b])
                        for sub in range(nsub):
                            k.tr(pk.t[:, sub * 128:(sub + 1) * 128], kht.t[:, sub * 128:(sub + 1) * 128], c.ident_b.t[:], r=[kht.b],
                                 w=[pk.b] if sub == 0 else (), wp=() if sub == 0 else [pk.b])
                        k.cp("act", khs.t[:, 0:nsub, dd, h * 128:(h + 1) * 128], pk.t[:, 0:n].rearrange("p (s e) -> p s e", e=128), r=[pk.b], wp=[khs.b])
                tok = slice(g * 512, g * 512 + n)
                for dd in range(2):
                    if is_ctx:
                        k.dma("sp", d["KHc"][dd, tok, :].rearrange("(s p) e -> p s e", p=128), khs.t[:, 0:nsub, dd, :], r=[khs.b], wp=[dKH])
                        k.dma("sp", d["DECc"][dd, :, :, g * 8:g * 8 + nch], decs.t[:, dd, :, 0:nch], r=[decs.b], wp=[dDEC])
                    else:
                        k.dma("sp", d["HQ"][dd, :, :, tok], hqs.t[:, dd, :, 0:n], r=[hqs.b], wp=[dHQ])
                        k.dma("sp", d["HK"][dd, :, :, tok], hks.t[:, dd, :, 0:n], r=[hks.b], wp=[dHK])
                        k.dma("sp", d["KH"][dd, tok, :].rearrange("(s p) e -> p s e", p=128), khs.t[:, 0:nsub, dd, :], r=[khs.b], wp=[dKH])
                        k.dma("sp", d["DEC"][dd, :, :, g * 8:g * 8 + nch], decs.t[:, dd, :, 0:nch], r=[decs.b], wp=[dDEC])

        run(d["XTc"], L, True)
        run(d["XT"], S, False)
        P.end_phase()


def phase_nat(c):
    nc, P, k, d, cfg = c.nc, c.P, c.k, c.d, c.cfg
    S, L, ROWS = cfg.S, cfg.L, cfg.ROWS
    NCC = L // 128
    NCH = 4 + NCC
    with ExitStack() as ph:
        BFf = sb(ph, nc, [128, 7680], F32, name="bff")
        BFb = sb(ph, nc, [128, 8, 960], BF16, name="bfb")
        k.dma("sp", BFf.t[0:64, :], d["rpb_full"], wp=[BFf.b])
        k.dma("sp", BFf.t[64:128, :], d["rpb_full"], wp=[BFf.b])
        k.cp("dve", BFb.t[:].rearrange("p h e -> p (h e)"), BFf.t[:], r=[BFf.b], w=[BFb.b])
        KcT = sb(ph, nc, [128, 4, L], BF16, name="kct")
        VcA = sb(ph, nc, [128, NCC, 520], BF16, name="vca")
        k.dma("sp", KcT.t[:], d["KcT"], w=[KcT.b])
        k.dma("sp", VcA.t[:], d["VcA"].rearrange("(c p) f -> p c f", p=128), w=[VcA.b])
        QR = sb(ph, nc, [128, 4, 512], BF16, n=2, name="qr")
        QF = sb(ph, nc, [128, 4, 512], BF16, n=2, name="qf")
        KW = sb(ph, nc, [128, 4, 512], BF16, n=3, name="kw")
        VW = sb(ph, nc, [128, 4, 520], BF16, n=3, name="vw")
        PT = sb(ph, nc, [128, NCH * 64], BF16, n=3, name="pt")
        NS = sb(ph, nc, [64, 512], BF16, n=2, name="ns")
        RD = sb(ph, nc, [64, 8], F32, n=2, name="rd")
        pS = ps(ph, nc, [128, 512], F32, n=4, name="pS")
        pO = ps(ph, nc, [64, 4, 65], F32, n=4, name="pO")
        dCAT = Buf("dCATn")
        it = 0
        for r in range(ROWS):
            g8, ro = r // 8, (r % 8) * 64
            qr, qf = QR[g8 % 2], QF[g8 % 2]
            if r % 8 == 0:
                k.dma("sp", qr.t[:], d["QTr"][:, :, g8 * 512:(g8 + 1) * 512], w=[qr.b])
                k.dma("sp", qf.t[:], d["QTf"][:, :, g8 * 512:(g8 + 1) * 512], w=[qf.b])
            rs = min(max(r - 4, 0), ROWS - 8)
            dr0 = rs - r + 7
            kw, vw = KW[r % 3], VW[r % 3]
            k.dma("sp", kw.t[:], d["KTr"][:, :, rs * 64:rs * 64 + 512], w=[kw.b])
            k.dma("sp", vw.t[:], d["VA"][rs * 64:rs * 64 + 512, :].rearrange("(c p) f -> p c f", p=128), w=[vw.b])
            ns, rd = NS[r % 2], RD[r % 2]
            po2 = (pO[(2 * r) % 4], pO[(2 * r + 1) % 4])
            def scores(h):
                nonlocal it
                hp, pb = h // 2, (h % 2) * 64
                psx, pt = pS[it % 4], PT[it % 3]
                it += 1
                first = True
                for kc in range(4):
                    k.mm(psx.t[:, kc * 64:(kc + 1) * 64], kw.t[pb:pb + 64, hp, kc * 128:(kc + 1) * 128], qr.t[pb:pb + 64, hp, ro:ro + 64], True, False,
                         r=[kw.b, qr.b], w=[psx.b] if first else (), wp=() if first else [psx.b])
                    first = False
                    k.mm(psx.t[:, kc * 64:(kc + 1) * 64], BFb.t[pb:pb + 64, h, (dr0 + 2 * kc) * 64:(dr0 + 2 * kc) * 64 + 128], c.ident_b.t[pb:pb + 64, pb:pb + 64], False, True,
                         r=[BFb.b], wp=[psx.b])
                for cc in range(NCC):
                    k.mm(psx.t[:, (4 + cc) * 64:(5 + cc) * 64], KcT.t[pb:pb + 64, hp, cc * 128:(cc + 1) * 128], qf.t[pb:pb + 64, hp, ro:ro + 64], True, True,
                         r=[KcT.b, qf.b], wp=[psx.b])
                k.act(pt.t[:], psx.t[:, 0:NCH * 64], AF.Exp, r=[psx.b], w=[pt.b])
                return pt

            def pv(h, pt):
                po = po2[h // 4]
                hh = h % 4
                for ch in range(NCH):
                    rhs = vw.t[:, ch, h * 65:(h + 1) * 65] if ch < 4 else VcA.t[:, ch - 4, h * 65:(h + 1) * 65]
                    k.mm(po.t[:, hh, :], pt.t[:, ch * 64:(ch + 1) * 64], rhs, ch == 0, ch == NCH - 1,
                         r=[pt.b, vw.b, VcA.b], w=[po.b] if (ch == 0 and hh == 0) else (), wp=() if (ch == 0 and hh == 0) else [po.b])

            pts = [scores(0)]
            for h in range(8):
                if h + 1 < 8:
                    pts.append(scores(h + 1))
                pv(h, pts[h])
            for half in range(2):
                po = po2[half]
                k.recip(rd.t[:, half * 4:(half + 1) * 4], po.t[:, :, 64], r=[po.b], wp=[rd.b])
                k.tt("dve", ns.t[:, half * 256:(half + 1) * 256].rearrange("p (h e) -> p h e", h=4), po.t[:, :, 0:64],
                     rd.t[:, half * 4:(half + 1) * 4].unsqueeze(2).to_broadcast([64, 4, 64]), ALU.mult, r=[po.b, rd.b], wp=[ns.b])
            k.dma("sp", d["CAT"][r * 64:(r + 1) * 64, 0:512], ns.t[:], r=[ns.b], wp=[dCAT])
        P.end_phase()


def phase_hgrn(c):
    nc, P, k, d, cfg = c.nc, c.P, c.k, c.d, c.cfg
    S, L = cfg.S, cfg.L
    NG = S // 512
    NCHT = S // 64
    LC = L // 64
    with ExitStack() as ph:
        TRI = sb(ph, nc, [128, 2, 64], F32, name="tri")
        k.dma("sp", TRI.t[:, 0, :], d["k_trif"], wp=[TRI.b])
        k.dma("sp", TRI.t[:, 1, :], d["k_trib"], wp=[TRI.b])
        og = sb(ph, nc, [128, 128], F32, name="og")
        ONG = sb(ph, nc, [128, 512], F32, name="ong")
        k.dma("sp", og.t[:], d["hgrn_o_norm"].partition_broadcast(128), w=[og.b])
        k.ts("dve", ONG.t[:].rearrange("p (h e) -> p h e", h=4), og.t[:].unsqueeze(1).to_broadcast([128, 4, 128]), 1.0, ALU.mult, r=[og.b], w=[ONG.b])
        S32s = sb(ph, nc, [128, 4, 128], F32, n=2, name="s32")
        Sbfs = sb(ph, nc, [128, 4, 128], BF16, n=2, name="sbf")
        DECt = sb(ph, nc, [128, 4, NCHT], F32, name="dect")
        DECc = sb(ph, nc, [128, 4, LC], F32, name="decc")
        KHc = sb(ph, nc, [128, L // 128, 512], BF16, name="khc")
        VHc = sb(ph, nc, [128, L // 128, 512], BF16, name="vhc")
        HQg = sb(ph, nc, [128, 4, 512], BF16, n=2, name="hqg")
        HKg = sb(ph, nc, [128, 4, 512], BF16, n=2, name="hkg")
        KHg = sb(ph, nc, [128, 4, 512], BF16, n=2, name="khg")
        VHg = sb(ph, nc, [128, 4, 512], BF16, n=2, name="vhg")
        SC = [sb(ph, nc, [128, 256], BF16, n=2, name="sc%d" % i) for i in range(2)]
        for i in range(2):
            for s_ in SC[i]:
                k.memset("pool", s_.t[:], 0.0, w=[s_.b])
        OFs = sb(ph, nc, [64, 512], F32, n=2, name="ofs")
        OFc = sb(ph, nc, [64, 512], F32, n=2, name="ofc")
        Gc = sb(ph, nc, [64, 512], F32, n=2, name="gc")
        O32 = sb(ph, nc, [64, 512], F32, n=2, name="o32")
        SQ = sb(ph, nc, [64, 512], F32, n=2, name="sq")
        ST = sb(ph, nc, [64, 16], F32, n=2, name="st")
        Y1 = sb(ph, nc, [64, 512], F32, n=2, name="y1")
        YB = sb(ph, nc, [64, 512], BF16, n=2, name="yb")
        pSs = ps(ph, nc, [128, 512], F32, n=2, name="pSs")
        pSo = ps(ph, nc, [128, 512], F32, n=2, name="pSo")
        pSt = ps(ph, nc, [128, 512], F32, n=2, name="pSt")
        dOF, dCAT = Buf("dOF"), Buf("dCATh")
        k.dma("sp", VHc.t[:], d["VHc"].rearrange("(s p) e -> p s e", p=128), w=[VHc.b])
        it = 0
        for dd in range(2):
            k.memset("dve", S32s[0].t[:], 0.0, w=[S32s[0].b])
            k.dma("sp", DECt.t[:], d["DEC"][dd], w=[DECt.b])
            k.dma("sp", DECc.t[:], d["DECc"][dd], w=[DECc.b])
            k.dma("sp", KHc.t[:], d["KHc"][dd].rearrange("(s p) e -> p s e", p=128), w=[KHc.b])

            sn = 0

            def state_update(khs, vhs, tl, pb, dec_ap_fn):
                nonlocal it, sn
                pst = pSt[it % 2]
                for h in range(4):
                    k.mm(pst.t[:, h * 128:(h + 1) * 128], khs.t[pb:pb + 64, tl, h * 128:(h + 1) * 128], vhs.t[pb:pb + 64, tl, h * 128:(h + 1) * 128], True, True,
                         r=[khs.b, vhs.b], w=[pst.b] if h == 0 else (), wp=() if h == 0 else [pst.b])
                so, sw = S32s[sn % 2], S32s[(sn + 1) % 2]
                for h in range(4):
                    k.stt(sw.t[:, h, :], so.t[:, h, :], dec_ap_fn(h), pst.t[:, h * 128:(h + 1) * 128], ALU.mult, ALU.add,
                          r=[so.b, pst.b, DECt.b, DECc.b], wp=[sw.b])
                nb = Sbfs[(sn + 1) % 2]
                k.cp("act", nb.t[:], sw.t[:], r=[sw.b], w=[nb.b])
                sn += 1

            chs = range(LC) if dd == 0 else range(LC - 1, -1, -1)
            for ch in chs:
                state_update(KHc, VHc, ch // 2, (ch % 2) * 64, lambda h, ch=ch: DECc.t[:, h, ch:ch + 1])
                it += 1
            groups = list(range(NG)) if dd == 0 else list(range(NG - 1, -1, -1))
            order = []
            for gi, g in enumerate(groups):
                for tl in (range(4) if dd == 0 else range(3, -1, -1)):
                    for cc in ((0, 1) if dd == 0 else (1, 0)):
                        order.append((gi, g, tl, cc))
            loaded = set()

            def ensure(gi, g):
                if gi in loaded:
                    return
                loaded.add(gi)
                hq, hk, kh, vh = HQg[gi % 2], HKg[gi % 2], KHg[gi % 2], VHg[gi % 2]
                tok = slice(g * 512, (g + 1) * 512)
                k.dma("sp", hq.t[:], d["HQ"][dd, :, :, tok], w=[hq.b])
                k.dma("sp", hk.t[:], d["HK"][dd, :, :, tok], w=[hk.b])
                k.dma("sp", kh.t[:], d["KH"][dd, tok, :].rearrange("(s p) e -> p s e", p=128), w=[kh.b])
                k.dma("sp", vh.t[:], d["VH"][tok, :].rearrange("(s p) e -> p s e", p=128), w=[vh.b])

            def scores(n):
                gi, g, tl, cc = order[n]
                ensure(gi, g)
                hq, hk = HQg[gi % 2], HKg[gi % 2]
                pb = cc * 64
                toff, qoff = tl * 128, tl * 128 + cc * 64
                pss = pSs[n % 2]
                sc = SC[cc][(n // 2) % 2]
                if dd == 1:
                    rows_ = slice((g * 8 + tl * 2 + cc) * 64, (g * 8 + tl * 2 + cc + 1) * 64)
                    k.dma("sp", OFc[n % 2].t[:], d["OF"][rows_, :], r=[dOF], w=[OFc[n % 2].b])
                    k.dma("sp", Gc[n % 2].t[:], d["G"][rows_, :], w=[Gc[n % 2].b])
                for h in range(4):
                    k.mm(pss.t[:, h * 64:(h + 1) * 64], hk.t[:, h, toff:toff + 128], hq.t[:, h, qoff:qoff + 64], True, True,
                         r=[hk.b, hq.b], w=[pss.b] if h == 0 else (), wp=() if h == 0 else [pss.b])
                k.tt("dve", sc.t[pb:pb + 64, :].rearrange("p (h t) -> p h t", h=4), pss.t[pb:pb + 64, 0:256].rearrange("p (h t) -> p h t", h=4),
                     TRI.t[pb:pb + 64, dd, :].unsqueeze(1).to_broadcast([64, 4, 64]), ALU.mult, r=[pss.b, TRI.b], wp=[sc.b])
                return sc

            def rest(n, sc):
                nonlocal it
                gi, g, tl, cc = order[n]
                hq, hk, kh, vh = HQg[gi % 2], HKg[gi % 2], KHg[gi % 2], VHg[gi % 2]
                ch = g * 8 + tl * 2 + cc
                pb = cc * 64
                qoff = tl * 128 + cc * 64
                pso = pSo[n % 2]
                Sbf = Sbfs[sn % 2]
                state_update(kh, vh, tl, pb, lambda h, ch=ch: DECt.t[:, h, ch:ch + 1])
                it += 1
                for h in range(4):
                    k.mm(pso.t[0:64, h * 128:(h + 1) * 128], sc.t[:, h * 64:(h + 1) * 64], vh.t[:, tl, h * 128:(h + 1) * 128], True, False,
                         r=[sc.b, vh.b], w=[pso.b] if h == 0 else (), wp=() if h == 0 else [pso.b])
                    k.mm(pso.t[0:64, h * 128:(h + 1) * 128], hq.t[:, h, qoff:qoff + 64], Sbf.t[:, h, :], False, True,
                         r=[hq.b, Sbf.b], wp=[pso.b])
                rows = slice(ch * 64, (ch + 1) * 64)
                if dd == 0:
                    ofs = OFs[n % 2]
                    k.cp("act", ofs.t[:], pso.t[0:64, :], r=[pso.b], w=[ofs.b])
                    k.dma("sp", d["OF"][rows, :], ofs.t[:], r=[ofs.b], wp=[dOF])
                else:
                    ofc, gc, o32, sq, st, y1, yb = (X[n % 2] for X in (OFc, Gc, O32, SQ, ST, Y1, YB))
                    k.tt("dve", o32.t[:], pso.t[0:64, :], ofc.t[:], ALU.add, r=[pso.b, ofc.b], w=[o32.b])
                    k.act(sq.t[:], o32.t[:], AF.Square, r=[o32.b], w=[sq.b])
                    k.red(st.t[:, 0:4], sq.t[:].rearrange("p (h e) -> p h e", h=4), r=[sq.b], wp=[st.b])
                    k.ts("dve", st.t[:, 4:8], st.t[:, 0:4], 1.0 / 128, ALU.mult, EPS, ALU.add, r=[st.b], wp=[st.b])
                    k.act(st.t[:, 8:12], st.t[:, 4:8], AF.Sqrt, r=[st.b], wp=[st.b])
                    k.recip(st.t[:, 12:16], st.t[:, 8:12], r=[st.b], wp=[st.b])
                    k.tt("dve", y1.t[:].rearrange("p (h e) -> p h e", h=4), o32.t[:].rearrange("p (h e) -> p h e", h=4),
                         st.t[:, 12:16].unsqueeze(2).to_broadcast([64, 4, 128]), ALU.mult, r=[o32.b, st.b], w=[y1.b])
                    k.tt("pool", y1.t[:], y1.t[:], ONG.t[0:64, :], ALU.mult, r=[y1.b, ONG.b], w=[y1.b])
                    k.tt("pool", yb.t[:], y1.t[:], gc.t[:], ALU.mult, r=[y1.b, gc.b], w=[yb.b])
                    k.dma("sp", d["CAT"][rows, 512:1024], yb.t[:], r=[yb.b], wp=[dCAT])

            N = len(order)
            cur = scores(0)
            for n in range(N):
                nxt = scores(n + 1) if n + 1 < N else None
                rest(n, cur)
                cur = nxt
        P.end_phase()


def phase_post(c, layer):
    nc, P, k, d, cfg = c.nc, c.P, c.k, c.d, c.cfg
    S, E, NT = cfg.S, cfg.E, cfg.NT
    NG = S // 512
    hin = d["x"] if layer == 0 else d["H2"]
    hout = d["H1"] if layer == 0 else d["H3"]
    with ExitStack() as ph:
        Wo = sb(ph, nc, [128, 8, 1024], BF16, name="Wo")
        wsrc = d["ab_w_out"] if layer == 0 else d["conv_w2"]
        k.dma("pool", Wo.t[:], wsrc.rearrange("(k p) n -> p k n", p=128), w=[Wo.b])
        RW = sb(ph, nc, [128, 8, E], F32, name="RW")
        k.dma("sp", RW.t[:], d["router_w"][layer].rearrange("(k p) e -> p k e", p=128), w=[RW.b])
        RB = sb(ph, nc, [128, E], F32, name="RB")
        k.dma("sp", RB.t[:], d["router_b"][layer:layer + 1, :].partition_broadcast(128), w=[RB.b])
        B2t = sb(ph, nc, [E, 1024], F32, name="B2t")
        k.dma("sp", B2t.t[:], d["moe_b2"][layer], w=[B2t.b])
        A2 = make_A(c, ph, "norm2_g", layer, 4096, c.MOD)
        XIN = sb(ph, nc, [128, 1024], F32, n=2, name="xin")
        Hs = sb(ph, nc, [128, 1024], F32, n=2, name="hs")
        V1s = sb(ph, nc, [128, 1024], F32, n=2, name="v1")
        tmps = [{"ss": sb(ph, nc, [128, 16], F32, name="ss"), "t1": sb(ph, nc, [128, 1024], F32, name="t1")} for _ in range(2)]
        XFs = sb(ph, nc, [128, 1024], F32, n=2, name="xf")
        XB = sb(ph, nc, [128, 1024], BF16, n=2, name="xb")
        XT2g = sb(ph, nc, [128, 8, 512], BF16, n=2, name="xt2g")
        XFTs = sb(ph, nc, [128, 8, 128], F32, n=2, name="xft")
        LG = sb(ph, nc, [128, 4 * E + 32], F32, n=2, name="lg")
        CTs = sb(ph, nc, [E, 128], F32, n=2, name="ct")
        ACss = sb(ph, nc, [128, 1024], F32, n=2, name="acs")
        pT = ps(ph, nc, [128, 1024], BF16, name="pT")
        pT2 = ps(ph, nc, [128, 1024], BF16, name="pT2")
        pY = ps(ph, nc, [128, 512], F32, n=2, name="pY")
        pTf = ps(ph, nc, [128, 512], F32, n=2, name="pTf")
        pLg = ps(ph, nc, [128, E], F32, name="pLg")
        pCT = ps(ph, nc, [E, 128], F32, name="pCT")
        dH, dXT2, dCOMB, dACC = Buf("dH"), Buf("dXT2"), Buf("dCOMB"), Buf("dACC")
        if layer == 0:
            CATt = sb(ph, nc, [128, 1024], BF16, n=2, name="catt")
            catT = sb(ph, nc, [128, 8, 128], BF16, n=2, name="catT")
        else:
            YG = sb(ph, nc, [128, 8, 512], F32, name="yg")
            YSQ = sb(ph, nc, [128, 512], F32, n=2, name="ysq")
            Mm = sb(ph, nc, [128, 512], F32, name="mm_")
            MSQ = sb(ph, nc, [128, 512], F32, name="msq")
            RS = sb(ph, nc, [128, 512], F32, name="rs")
            TA = sb(ph, nc, [128, 512], F32, n=2, name="ta")
            TB = sb(ph, nc, [128, 512], F32, n=2, name="tb")
            HN = sb(ph, nc, [128, 8, 512], BF16, name="hn")
            pSum = pTf[0]
            pSq = pTf[1]
            lnr = sb(ph, nc, [16, 128], F32, name="lnr")
            LNP = sb(ph, nc, [128, 16], F32, name="lnp")
            k.dma("sp", lnr.t[0:8, :], d["conv_ln_g"], wp=[lnr.b])
            k.dma("sp", lnr.t[8:16, :], d["conv_ln_b"], wp=[lnr.b])
            k.tr(pLg.t[:, 0:16] if E >= 16 else pTf[0].t[:, 0:16], lnr.t[:], c.ident_f.t[0:16, 0:16], r=[lnr.b], w=[pLg.b if E >= 16 else pTf[0].b])
            k.cp("dve", LNP.t[:], pLg.t[:, 0:16] if E >= 16 else pTf[0].t[:, 0:16], r=[pLg.b if E >= 16 else pTf[0].b], w=[LNP.b])
            b2bc = sb(ph, nc, [128, 1024], F32, name="b2bc")
            B2M = sb(ph, nc, [128, 1024], F32, name="b2m")
            k.dma("sp", b2bc.t[:], d["conv_b2"].partition_broadcast(128), w=[b2bc.b])
            k.tt("dve", B2M.t[:], b2bc.t[:], c.MOD.t[:, 2048:3072], ALU.mult, r=[b2bc.b, c.MOD.b], w=[B2M.b])

        it = 0
        for g in range(NG):
            xt2g = XT2g[g % 2]
            if layer == 1:
                k.dma("sp", YG.t[:], d["YD"][:, :, g * 512:(g + 1) * 512], w=[YG.b])
                for cch in range(8):
                    ysq = YSQ[cch % 2]
                    k.act(ysq.t[:], YG.t[:, cch, :], AF.Square, r=[YG.b], w=[ysq.b])
                    k.mm(pSum.t[:], c.ones_f.t[:], YG.t[:, cch, :], cch == 0, cch == 7, r=[YG.b, c.ones_f.b], w=[pSum.b] if cch == 0 else (), wp=() if cch == 0 else [pSum.b])
                    k.mm(pSq.t[:], c.ones_f.t[:], ysq.t[:], cch == 0, cch == 7, r=[ysq.b, c.ones_f.b], w=[pSq.b] if cch == 0 else (), wp=() if cch == 0 else [pSq.b])
                k.act(Mm.t[:], pSum.t[:], AF.Copy, r=[pSum.b], w=[Mm.b], scale=1.0 / 1024)
                k.tt("pool", MSQ.t[:], Mm.t[:], Mm.t[:], ALU.mult, r=[Mm.b], w=[MSQ.b])
                k.stt(RS.t[:], pSq.t[:], 1.0 / 1024, MSQ.t[:], ALU.mult, ALU.subtract, r=[pSq.b, MSQ.b], w=[RS.b])
                k.ts("dve", RS.t[:], RS.t[:], EPS, ALU.add, r=[RS.b], w=[RS.b])
                k.act(RS.t[:], RS.t[:], AF.Sqrt, r=[RS.b], w=[RS.b])
                k.recip(RS.t[:], RS.t[:], r=[RS.b], w=[RS.b])
                for cch in range(8):
                    ta, tb = TA[cch % 2], TB[cch % 2]
                    k.tt("dve", ta.t[:], YG.t[:, cch, :], Mm.t[:], ALU.subtract, r=[YG.b, Mm.b], w=[ta.b])
                    k.tt("pool", tb.t[:], ta.t[:], RS.t[:], ALU.mult, r=[ta.b, RS.b], w=[tb.b])
                    k.act(HN.t[:, cch, :], tb.t[:], AF.Silu, r=[tb.b, LNP.b], wp=[HN.b], scale=LNP.t[:, cch:cch + 1], bias=LNP.t[:, 8 + cch:9 + cch])
            states = {}

            def front(j):
                nonlocal it
                t = g * 4 + j
                rows = slice(t * 128, (t + 1) * 128)
                xin, hs, xb = XIN[it % 2], Hs[it % 2], XB[it % 2]
                lg = LG[it % 2]
                V1, XF = V1s[it % 2], XFs[it % 2]
                k.dma("sp", xin.t[:], hin[rows, :], w=[xin.b])
                if layer == 0:
                    cat, ctT = CATt[it % 2], catT[it % 2]
                    k.dma("sp", cat.t[:], d["CAT"][rows, :], w=[cat.b])
                    for kk in range(8):
                        k.tr(pT.t[:, kk * 128:(kk + 1) * 128], cat.t[:, kk * 128:(kk + 1) * 128], c.ident_b.t[:], r=[cat.b],
                             w=[pT.b] if kk == 0 else (), wp=() if kk == 0 else [pT.b])
                    k.cp("act", ctT.t[:].rearrange("p k t -> p (k t)"), pT.t[:], r=[pT.b], w=[ctT.b])
                    lhs = lambda kk: ctT.t[:, kk, :]
                    lb_ = ctT.b
                else:
                    lhs = lambda kk: HN.t[:, kk, j * 128:(j + 1) * 128]
                    lb_ = HN.b
                it += 1
                for half in range(2):
                    for kk in range(8):
                        k.mm(pY[half].t[:], lhs(kk), Wo.t[:, kk, half * 512:(half + 1) * 512], kk == 0, kk == 7,
                             r=[lb_, Wo.b], w=[pY[half].b] if kk == 0 else (), wp=() if kk == 0 else [pY[half].b])
                    k.tt("dve", V1.t[:, half * 512:(half + 1) * 512], pY[half].t[:], c.MOD.t[:, 2048 + half * 512:2048 + (half + 1) * 512], ALU.mult,
                         r=[pY[half].b, c.MOD.b], wp=[V1.b])
                if layer == 1:
                    k.tt("pool", xin.t[:], xin.t[:], B2M.t[:], ALU.add, r=[xin.b, B2M.b], w=[xin.b])
                k.tt("pool", hs.t[:], V1.t[:], xin.t[:], ALU.add, r=[V1.b, xin.b], w=[hs.b])
                k.dma("sp", hout[rows, :], hs.t[:], r=[hs.b], wp=[dH])
                norm_mod(c, hs, A2, c.MOD.t[:, 3072:4096], c.MOD.b, [(XF.t[:], "dve", XF), (xb.t[:], "pool", xb)], tmps[it % 2], j)

                states[j] = (t, rows, hs, xb, lg, XF)

            def back(j):
                t, rows, hs, xb, lg, XF = states[j]
                XFT_, CT_, ACs_ = XFTs[t % 2], CTs[t % 2], ACss[t % 2]
                for kk in range(8):
                    k.tr(pT2.t[:, kk * 128:(kk + 1) * 128], xb.t[:, kk * 128:(kk + 1) * 128], c.ident_b.t[:], r=[xb.b],
                         w=[pT2.b] if kk == 0 else (), wp=() if kk == 0 else [pT2.b])
                k.cp("act", xt2g.t[:, :, j * 128:(j + 1) * 128], pT2.t[:].rearrange("p (k t) -> p k t", k=8), r=[pT2.b], wp=[xt2g.b])
                for kk in range(8):
                    pf = pTf[kk // 4]
                    k.tr(pf.t[:, (kk % 4) * 128:(kk % 4 + 1) * 128], XF.t[:, kk * 128:(kk + 1) * 128], c.ident_f.t[:], r=[XF.b],
                         w=[pf.b] if kk % 4 == 0 else (), wp=() if kk % 4 == 0 else [pf.b])
                for hf in range(2):
                    k.cp("act" if hf else "dve", XFT_.t[:, hf * 4:(hf + 1) * 4, :], pTf[hf].t[:].rearrange("p (k t) -> p k t", k=4), r=[pTf[hf].b], wp=[XFT_.b])
                for kk in range(8):
                    k.mm(pLg.t[:], XFT_.t[:, kk, :], RW.t[:, kk, :], kk == 0, kk == 7, r=[XFT_.b, RW.b], w=[pLg.b] if kk == 0 else (), wp=() if kk == 0 else [pLg.b])
                L0, MK, EX, EXM, MS = (lg.t[:, 0:E], lg.t[:, E:2 * E], lg.t[:, 2 * E:3 * E], lg.t[:, 3 * E:4 * E], lg.t[:, 4 * E:4 * E + 32])
                k.tt("dve", L0, pLg.t[:], RB.t[:], ALU.add, r=[pLg.b, RB.b], wp=[lg.b])
                P.op("dve", (lambda e, o=MS[:, 0:8], i_=L0: e.max(out=o, in_=i_)), reads=[lg.b], wpart=[lg.b])
                k.ts("dve", MK, L0, MS[:, 3:4], ALU.is_ge, r=[lg.b], wp=[lg.b])
                k.ts("dve", MS[:, 8:9], MS[:, 0:1], -1.0, ALU.mult, r=[lg.b], wp=[lg.b])
                k.act(EX, L0, AF.Exp, r=[lg.b], wp=[lg.b], bias=MS[:, 8:9])
                k.stt(EXM, EX, 1.0, MK, ALU.mult, ALU.mult, r=[lg.b], wp=[lg.b], accum=MS[:, 9:10])
                k.recip(MS[:, 10:11], MS[:, 9:10], r=[lg.b], wp=[lg.b])
                k.ts("dve", EXM, EXM, MS[:, 10:11], ALU.mult, r=[lg.b], wp=[lg.b])
                k.dma("sp", d["COMB"][rows, :], EXM, r=[lg.b], wp=[dCOMB])
                k.dma("sp", d["MK"][rows, :], MK, r=[lg.b], wp=[dCOMB])
                k.dma("sp", d["XM2"][rows, :], xb.t[:], r=[xb.b], wp=[dXT2])
                k.tr(pCT.t[:], EXM, c.ident_f.t[:], r=[lg.b], w=[pCT.b])
                k.cp("act", CT_.t[:], pCT.t[:], r=[pCT.b], w=[CT_.b])
                for half in range(2):
                    k.mm(pTf[half].t[:], CT_.t[:], B2t.t[:, half * 512:(half + 1) * 512], True, True, r=[CT_.b, B2t.b], w=[pTf[half].b])
                    k.cp("act" if half else "dve", ACs_.t[:, half * 512:(half + 1) * 512], pTf[half].t[:], r=[pTf[half].b], wp=[ACs_.b])
                k.dma("sp", d["ACCd"][rows, :], ACs_.t[:], r=[ACs_.b], wp=[dACC])

            front(0)
            for j in range(4):
                if j + 1 < 4:
                    front(j + 1)
                back(j)
            k.dma("sp", d["XT2"][:, :, g * 512:(g + 1) * 512], xt2g.t[:], r=[xt2g.b], wp=[dXT2])
        P.end_phase()


def phase_moe(c, layer, MOD5):
    nc, P, k, d, cfg = c.nc, c.P, c.k, c.d, c.cfg
    S, E, MB = cfg.S, cfg.E, cfg.MB
    NTB = MB // 128
    NH = MB // 512
    hin = d["H1"] if layer == 0 else d["H3"]
    hout = d["H2"] if layer == 0 else d["out"]
    with ExitStack() as ph:
        nb1 = (E * 16) // 128
        B1T = sb(ph, nc, [128, E * 16], F32, name="b1t")
        b1r = sb(ph, nc, [128, 128], F32, n=2, name="b1r")
        ACC = sb(ph, nc, [128, NTB, 1024], F32, name="acc")
        XT2b = sb(ph, nc, [128, 8, MB], BF16, name="xt2b")
        CMB = sb(ph, nc, [128, NTB, E], F32, name="cmb")
        W1 = sb(ph, nc, [128, 8, 2048], BF16, n=2, name="w1")
        W2 = sb(ph, nc, [128, 8, 1024], BF16, name="w2")
        ACTT = sb(ph, nc, [128, 8, 512], BF16, n=2, name="actt")
        G1 = sb(ph, nc, [128, 512], F32, n=2, name="g1")
        S1 = sb(ph, nc, [128, 512], F32, n=2, name="s1")
        L1 = sb(ph, nc, [128, 512], F32, n=2, name="l1")
        L2 = sb(ph, nc, [128, 512], F32, n=2, name="l2")
        GS = sb(ph, nc, [128, 512], F32, n=2, name="gs")
        HT = sb(ph, nc, [128, 1024], F32, n=2, name="ht")
        pG = ps(ph, nc, [128, 512], F32, n=2, name="pG")
        pL = ps(ph, nc, [128, 512], F32, n=2, name="pL")
        pO = ps(ph, nc, [128, 512], F32, n=4, name="pO")
        dOUT = Buf("dOUT")
        for i in range(nb1):
            br = b1r[i % 2]
            k.dma("sp", br.t[:], d["moe_b1"][layer, i * 128:(i + 1) * 128, :], w=[br.b])
            k.tr(pG[i % 2].t[:, 0:128], br.t[:], c.ident_f.t[:], r=[br.b], w=[pG[i % 2].b])
            k.cp("dve", B1T.t[:, i * 128:(i + 1) * 128], pG[i % 2].t[:, 0:128], r=[pG[i % 2].b], wp=[B1T.b])
        wi = 0
        io = 0
        for blk in range(S // MB):
            t0 = blk * NTB
            rows = slice(blk * MB, (blk + 1) * MB)
            k.dma("sp", ACC.t[:], d["ACCd"][rows, :].rearrange("(t p) n -> p t n", p=128), w=[ACC.b])
            k.dma("sp", XT2b.t[:], d["XT2"][:, :, rows], w=[XT2b.b])
            k.dma("sp", CMB.t[:], d["COMB"][rows, :].rearrange("(t p) e -> p t e", p=128), w=[CMB.b])
            for e in range(E):
                w1 = W1[wi % 2]
                wi += 1
                w1v = d["moe_w1"][layer, e].rearrange("(k p) n -> p k n", p=128)
                k.dma("pool", w1.t[:, 0:4, :], w1v[:, 0:4, :], wp=[w1.b])
                k.dma("pool", w1.t[:, 4:8, :], w1v[:, 4:8, :], wp=[w1.b])
                w2_loaded = False
                for ht in range(NH):
                    actt = ACTT[io % 2]
                    for pr in range(8):
                        pg, pl = pG[pr % 2], pL[pr % 2]
                        g1, s1, l1, l2, gs = (X[pr % 2] for X in (G1, S1, L1, L2, GS))
                        for kk in range(8):
                            k.mm(pg.t[:], w1.t[:, kk, pr * 128:(pr + 1) * 128], XT2b.t[:, kk, ht * 512:(ht + 1) * 512], kk == 0, kk == 7,
                                 r=[w1.b, XT2b.b], w=[pg.b] if kk == 0 else (), wp=() if kk == 0 else [pg.b])
                        for kk in range(8):
                            k.mm(pl.t[:], w1.t[:, kk, 1024 + pr * 128:1024 + (pr + 1) * 128], XT2b.t[:, kk, ht * 512:(ht + 1) * 512], kk == 0, kk == 7,
                                 r=[w1.b, XT2b.b], w=[pl.b] if kk == 0 else (), wp=() if kk == 0 else [pl.b])
                        bg = B1T.t[:, e * 16 + pr:e * 16 + pr + 1]
                        bl = B1T.t[:, e * 16 + 8 + pr:e * 16 + 8 + pr + 1]
                        k.ts("dve", g1.t[:], pg.t[:], bg, ALU.add, 7.0, ALU.min, r=[pg.b, B1T.b], w=[g1.b])
                        k.act(s1.t[:], g1.t[:], AF.Sigmoid, r=[g1.b], w=[s1.b], scale=1.702)
                        k.act(l1.t[:], pl.t[:], AF.Identity, r=[pl.b, B1T.b], w=[l1.b], bias=bl)
                        k.ts("dve", l2.t[:], l1.t[:], 7.0, ALU.min, -7.0, ALU.max, r=[l1.b], w=[l2.b])
                        k.tt("pool", gs.t[:], g1.t[:], s1.t[:], ALU.mult, r=[g1.b, s1.b], w=[gs.b])
                        k.stt(actt.t[:, pr, :], l2.t[:], 1.0, gs.t[:], ALU.add, ALU.mult, r=[l2.b, gs.b], wp=[actt.b])
                    if not w2_loaded:
                        k.dma("pool", W2.t[:], d["moe_w2"][layer, e].rearrange("(k p) n -> p k n", p=128), w=[W2.b])
                        w2_loaded = True
                    for sub in range(4):
                        tl = ht * 4 + sub
                        for half in range(2):
                            po = pO[io % 4]
                            io += 1
                            for jj in range(8):
                                k.mm(po.t[:], actt.t[:, jj, sub * 128:(sub + 1) * 128], W2.t[:, jj, half * 512:(half + 1) * 512], jj == 0, jj == 7,
                                     r=[actt.b, W2.b], w=[po.b] if jj == 0 else (), wp=() if jj == 0 else [po.b])
                            acc = ACC.t[:, tl, half * 512:(half + 1) * 512]
                            k.stt(acc, po.t[:], CMB.t[:, tl, e:e + 1], acc, ALU.mult, ALU.add, r=[po.b, CMB.b, ACC.b], wp=[ACC.b])
            for tl in range(NTB):
                t = t0 + tl
                ht_ = HT[tl % 2]
                k.dma("sp", ht_.t[:], hin[t * 128:(t + 1) * 128, :], w=[ht_.b])
                k.tt("dve", ACC.t[:, tl, :], ACC.t[:, tl, :], MOD5.t[:], ALU.mult, r=[ACC.b, MOD5.b], wp=[ACC.b])
                k.tt("pool", ht_.t[:], ht_.t[:], ACC.t[:, tl, :], ALU.add, r=[ht_.b, ACC.b], w=[ht_.b])
                k.dma("sp", hout[t * 128:(t + 1) * 128, :], ht_.t[:], r=[ht_.b], wp=[dOUT])
        P.end_phase()


def phase_f(c):
    nc, P, k, d, cfg = c.nc, c.P, c.k, c.d, c.cfg
    S = cfg.S
    NG = S // 512
    with ExitStack() as ph:
        W = sb(ph, nc, [128, 8, 2048], BF16, name="Wc1")
        wv = d["conv_w1"].rearrange("(k p) n -> p k n", p=128)
        k.dma("pool", W.t[:, 0:4, :], wv[:, 0:4, :], wp=[W.b])
        k.dma("pool", W.t[:, 4:8, :], wv[:, 4:8, :], wp=[W.b])
        A = make_A(c, ph, "norm1_g", 1, 1024, c.MOD)
        b1r = sb(ph, nc, [16, 128], F32, name="b1r")
        CB1 = sb(ph, nc, [128, 16], F32, name="cb1")
        pB = ps(ph, nc, [128, 16], F32, name="pB")
        k.dma("sp", b1r.t[:], d["conv_b1"], w=[b1r.b])
        k.tr(pB.t[:], b1r.t[:], c.ident_f.t[0:16, 0:16], r=[b1r.b], w=[pB.b])
        k.cp("dve", CB1.t[:], pB.t[:], r=[pB.b], w=[CB1.b])
        Z = sb(ph, nc, [128, 8, 16], BF16, name="z")
        k.memset("dve", Z.t[:], 0.0, w=[Z.b])
        dGT = Buf("dGT")
        k.dma("sp", d["GT"][:, :, 0:16], Z.t[:], r=[Z.b], wp=[dGT])
        k.dma("sp", d["GT"][:, :, S + 16:S + 32], Z.t[:], r=[Z.b], wp=[dGT])
        XIN = sb(ph, nc, [128, 1024], F32, n=2, name="xin")
        tmps = [{"ss": sb(ph, nc, [128, 16], F32, name="ss"), "t1": sb(ph, nc, [128, 1024], F32, name="t1")} for _ in range(2)]
        XM = sb(ph, nc, [128, 1024], BF16, n=2, name="xm")
        XTG = sb(ph, nc, [128, 8, 512], BF16, n=2, name="xtg")
        SG = sb(ph, nc, [128, 512], F32, n=2, name="sg")
        GTs = sb(ph, nc, [128, 8, 512], BF16, n=2, name="gts")
        pT = ps(ph, nc, [128, 1024], BF16, name="pT")
        pA = ps(ph, nc, [128, 512], F32, n=2, name="pA")
        pGt = ps(ph, nc, [128, 512], F32, n=2, name="pGt")
        it = 0
        for g in range(NG):
            xtg, gts = XTG[g % 2], GTs[g % 2]
            for j in range(4):
                t = 4 * g + j
                xin, xm = XIN[it % 2], XM[it % 2]
                it += 1
                k.dma("sp", xin.t[:], d["H2"][t * 128:(t + 1) * 128, :], w=[xin.b])
                norm_mod(c, xin, A, c.MOD.t[:, 0:1024], c.MOD.b, [(xm.t[:], "pool", xm)], tmps[it % 2], j)
                for kk in range(8):
                    k.tr(pT.t[:, kk * 128:(kk + 1) * 128], xm.t[:, kk * 128:(kk + 1) * 128], c.ident_b.t[:], r=[xm.b],
                         w=[pT.b] if kk == 0 else (), wp=() if kk == 0 else [pT.b])
                k.cp("act", xtg.t[:, :, j * 128:(j + 1) * 128], pT.t[:].rearrange("p (k t) -> p k t", k=8), r=[pT.b], wp=[xtg.b])
            for cp_ in range(8):
                pa, pg, sg = pA[cp_ % 2], pGt[cp_ % 2], SG[cp_ % 2]
                for kk in range(8):
                    k.mm(pa.t[:], W.t[:, kk, cp_ * 128:(cp_ + 1) * 128], xtg.t[:, kk, :], kk == 0, kk == 7, r=[W.b, xtg.b],
                         w=[pa.b] if kk == 0 else (), wp=() if kk == 0 else [pa.b])
                for kk in range(8):
                    k.mm(pg.t[:], W.t[:, kk, 1024 + cp_ * 128:1024 + (cp_ + 1) * 128], xtg.t[:, kk, :], kk == 0, kk == 7, r=[W.b, xtg.b],
                         w=[pg.b] if kk == 0 else (), wp=() if kk == 0 else [pg.b])
                k.act(sg.t[:], pg.t[:], AF.Sigmoid, r=[pg.b, CB1.b], w=[sg.b], bias=CB1.t[:, 8 + cp_:9 + cp_])
                k.stt(gts.t[:, cp_, :], pa.t[:], CB1.t[:, cp_:cp_ + 1], sg.t[:], ALU.add, ALU.mult, r=[pa.b, sg.b, CB1.b], wp=[gts.b])
            k.dma("sp", d["GT"][:, :, 16 + g * 512:16 + (g + 1) * 512], gts.t[:], r=[gts.b], wp=[dGT])
        P.end_phase()


def phase_g1(c):
    nc, P, k, d, cfg = c.nc, c.P, c.k, c.d, c.cfg
    S = cfg.S
    NG = S // 512
    with ExitStack() as ph:
        dwr = sb(ph, nc, [32, 1024], F32, name="dwr")
        DWT = sb(ph, nc, [128, 8, 31], F32, name="dwt")
        dbr = sb(ph, nc, [8, 128], F32, name="dbr")
        DWB = sb(ph, nc, [128, 8], F32, name="dwb")
        DG = sb(ph, nc, [128, 8, 31, 128], BF16, name="dg")
        pD = ps(ph, nc, [128, 8, 32], F32, name="pD")
        pB = ps(ph, nc, [128, 8], F32, name="pB")
        k.dma("sp", dwr.t[0:31, :], d["conv_dw"], w=[dwr.b])
        for cch in range(8):
            k.tr(pD.t[:, cch, 0:31], dwr.t[0:31, cch * 128:(cch + 1) * 128], c.ident_f.t[0:31, 0:31], r=[dwr.b],
                 w=[pD.b] if cch == 0 else (), wp=() if cch == 0 else [pD.b])
        k.cp("dve", DWT.t[:], pD.t[:, :, 0:31], r=[pD.b], w=[DWT.b])
        k.dma("sp", dbr.t[:], d["conv_dw_b"], w=[dbr.b])
        k.tr(pB.t[:], dbr.t[:], c.ident_f.t[0:8, 0:8], r=[dbr.b], w=[pB.b])
        k.cp("dve", DWB.t[:], pB.t[:], r=[pB.b], w=[DWB.b])
        n_ = 0
        for cch in range(8):
            for j in range(31):
                k.ts("dve" if n_ % 2 else "pool", DG.t[:, cch, j, :], c.ident_f.t[:], DWT.t[:, cch, j:j + 1], ALU.mult, r=[DWT.b, c.ident_f.b], wp=[DG.b])
                n_ += 1
        GTw = sb(ph, nc, [128, 8, 544], BF16, n=2, name="gtw")
        Ys = sb(ph, nc, [128, 8, 512], F32, n=2, name="ys")
        pC = ps(ph, nc, [128, 512], F32, n=4, name="pC")
        dYD = Buf("dYD")
        for g in range(NG):
            gtw, ys = GTw[g % 2], Ys[g % 2]
            k.dma("sp", gtw.t[:], d["GT"][:, :, g * 512:g * 512 + 544], w=[gtw.b])
            for cch in range(8):
                pc = pC[cch % 4]
                for j in range(31):
                    k.mm(pc.t[:], DG.t[:, cch, j, :], gtw.t[:, cch, j + 1:j + 513], j == 0, j == 30, r=[DG.b, gtw.b],
                         w=[pc.b] if j == 0 else (), wp=() if j == 0 else [pc.b])
                k.act(ys.t[:, cch, :], pc.t[:], AF.Identity, r=[pc.b, DWB.b], wp=[ys.b], bias=DWB.t[:, cch:cch + 1])
            k.dma("sp", d["YD"][:, :, g * 512:(g + 1) * 512], ys.t[:], r=[ys.b], wp=[dYD])
        P.end_phase()


I32 = mybir.dt.int32


def pool_dma_op(P, fn, reads=(), writes=(), wpart=(), key=None):
    o = Op("pool", fn, P.phase)
    o.is_dma = True
    if key is None:
        key = (list(writes) + list(wpart))[0]
    if key not in P.keymap:
        P.keymap[key] = len(P.keymap)
        assert len(P.keymap) <= NDSEM
    o.key = P.keymap[key]
    P._deps(o, reads, writes, wpart)
    P.ops["pool"].append(o)
    P.order.append(o)
    return o


def phase_route(c, layer, TEi):
    nc, P, k, d, cfg = c.nc, c.P, c.k, c.d, c.cfg
    S, E, NT, NTILE, NSLOT = cfg.S, cfg.E, cfg.NT, cfg.NTILE, cfg.NSLOT
    with ExitStack() as ph:
        MKf = sb(ph, nc, [128, NT, E], F32, name="mkf")
        MKb = sb(ph, nc, [128, NT, E], BF16, name="mkb")
        CMB = sb(ph, nc, [128, NT, E], F32, name="cmb")
        UTf = sb(ph, nc, [128, 128], F32, name="utf")
        UT = sb(ph, nc, [128, 128], BF16, name="ut")
        ONb = sb(ph, nc, [128, 128], BF16, name="onb")
        IOTA = sb(ph, nc, [128, 1], F32, name="iota")
        TH = sb(ph, nc, [1, E * 16], F32, name="th")
        J5 = sb(ph, nc, [1, NTILE * E], F32, name="j5")
        dSLOT, dInit = Buf("dSLOT"), Buf("dInit")
        k.dma("sp", d["SLOT"], d["k_slotinit"], w=[dSLOT, dInit])
        k.dma("sp", MKf.t[:], d["MK"].rearrange("(t p) e -> p t e", p=128), w=[MKf.b])
        k.dma("sp", CMB.t[:], d["COMB"].rearrange("(t p) e -> p t e", p=128), w=[CMB.b])
        k.dma("sp", UTf.t[:], d["k_ut"], w=[UTf.b])
        k.dma("sp", IOTA.t[:], d["k_iota"], w=[IOTA.b])
        k.dma("sp", TH.t[:], d["k_th"], w=[TH.b])
        k.dma("sp", J5.t[:], d["k_j512"], w=[J5.b])
        k.cp("dve", UT.t[:], UTf.t[:], r=[UTf.b], w=[UT.b])
        k.cp("pool", MKb.t[:], MKf.t[:], r=[MKf.b], w=[MKb.b])
        k.memset("dve", ONb.t[:], 1.0, w=[ONb.b])
        pC = ps(ph, nc, [1, E], F32, name="pC")
        pSB = ps(ph, nc, [128, E], F32, name="pSB")
        pR = ps(ph, nc, [128, E], F32, n=2, name="pR")
        V = sb(ph, nc, [1, 8 * E], F32, name="v")
        C16 = sb(ph, nc, [1, E * 16], F32, name="c16")
        CJ = sb(ph, nc, [1, NTILE * E], F32, name="cj")
        TEf = sb(ph, nc, [1, NTILE], F32, name="tef")
        SEGB = sb(ph, nc, [128, E], F32, name="segb")
        for t in range(NT):
            k.mm(pC.t[:], ONb.t[:, 0:1], MKb.t[:, t, :], t == 0, t == NT - 1, r=[ONb.b, MKb.b], w=[pC.b] if t == 0 else (), wp=() if t == 0 else [pC.b])
        cnt, ntl, c512, inc, segs, one = (V.t[:, i * E:(i + 1) * E] for i in range(6))
        k.cp("dve", cnt, pC.t[:], r=[pC.b], wp=[V.b])
        k.tt("dve", C16.t[:].rearrange("o (e m) -> o e m", m=16), cnt.unsqueeze(2).to_broadcast([1, E, 16]), TH.t[:].rearrange("o (e m) -> o e m", m=16), ALU.is_gt,
             r=[V.b, TH.b], w=[C16.b])
        k.red(ntl, C16.t[:].rearrange("o (e m) -> o e m", m=16), r=[C16.b], wp=[V.b])
        k.ts("dve", c512, ntl, 512.0, ALU.mult, r=[V.b], wp=[V.b])
        k.memset("dve", one, 1.0, wp=[V.b])
        P.op("dve", (lambda e_, o=inc, a=one, b_=c512: e_.tensor_tensor_scan(out=o, data0=a, data1=b_, initial=0.0, op0=ALU.mult, op1=ALU.add)),
             reads=[V.b], wpart=[V.b])
        k.tt("dve", segs, inc, c512, ALU.subtract, r=[V.b], wp=[V.b])
        k.tt("dve", CJ.t[:].rearrange("o (j e) -> o j e", e=E), segs.unsqueeze(1).to_broadcast([1, NTILE, E]), J5.t[:].rearrange("o (j e) -> o j e", e=E), ALU.is_le,
             r=[V.b, J5.b], w=[CJ.b])
        k.red(TEf.t[:], CJ.t[:].rearrange("o (j e) -> o j e", e=E), r=[CJ.b], w=[TEf.b])
        k.ts("dve", TEf.t[:], TEf.t[:], -1.0, ALU.add, 0.0, ALU.max, r=[TEf.b], w=[TEf.b])
        IDXW, IDXB, P4all = TEi
        KP = sb(ph, nc, [128, 9], F32, name="kp")
        k.dma("sp", KP.t[:], d["k_kp"], w=[KP.b])
        pTE = ps(ph, nc, [128, NTILE], F32, name="pTE")
        TEb = sb(ph, nc, [128, NTILE], F32, name="teb")
        XW = sb(ph, nc, [128, NTILE, 8], F32, name="xw")
        k.mm(pTE.t[:], c.ones_f.t[0:1, :], TEf.t[:], True, True, r=[TEf.b, c.ones_f.b], w=[pTE.b])
        k.cp("dve", TEb.t[:], pTE.t[:], r=[pTE.b], w=[TEb.b])
        for kk in range(8):
            k.ts("dve", XW.t[:, :, kk], TEb.t[:], 1024.0, ALU.mult, KP.t[:, kk:kk + 1], ALU.add, r=[TEb.b, KP.b], wp=[XW.b])
        if layer:
            k.ts("dve", XW.t[:], XW.t[:], float(layer * E * 1024), ALU.add, r=[XW.b], w=[XW.b])
        k.cp("dve", IDXW.t[:], XW.t[:], r=[XW.b], w=[IDXW.b])
        k.ts("dve", TEb.t[:], TEb.t[:], 16.0, ALU.mult, KP.t[:, 8:9], ALU.add, r=[TEb.b, KP.b], w=[TEb.b])
        if layer:
            k.ts("dve", TEb.t[:], TEb.t[:], float(layer * E * 16), ALU.add, r=[TEb.b], w=[TEb.b])
        k.cp("dve", IDXB.t[:], TEb.t[:], r=[TEb.b], w=[IDXB.b])
        k.mm(pSB.t[:], c.ones_f.t[0:1, :], segs, True, True, r=[V.b, c.ones_f.b], w=[pSB.b])
        k.cp("dve", SEGB.t[:], pSB.t[:], r=[pSB.b], w=[SEGB.b])
        POS = sb(ph, nc, [128, E], F32, n=2, name="pos")
        T8 = sb(ph, nc, [128, 8], F32, n=2, name="t8")
        OH = sb(ph, nc, [128, E], F32, n=2, name="oh")
        JK = sb(ph, nc, [128, E], F32, n=2, name="jk")
        P4 = sb(ph, nc, [128, 4], F32, n=2, name="p4")
        P4i = sb(ph, nc, [128, 4], I32, n=2, name="p4i")
        SR = sb(ph, nc, [128, 4, 2], F32, n=2, name="sr")
        for i in range(NT):
            pr = pR[i % 2]
            pos, t8, p4, p4i, sr = POS[i % 2], T8[i % 2], P4[i % 2], P4i[i % 2], SR[i % 2]
            for ip in range(i):
                k.mm(pr.t[:], ONb.t[:], MKb.t[:, ip, :], ip == 0, False, r=[ONb.b, MKb.b], w=[pr.b] if ip == 0 else (), wp=() if ip == 0 else [pr.b])
            k.mm(pr.t[:], UT.t[:], MKb.t[:, i, :], i == 0, True, r=[UT.b, MKb.b], w=[pr.b] if i == 0 else (), wp=() if i == 0 else [pr.b])
            k.tt("dve", pos.t[:], pr.t[:], SEGB.t[:], ALU.add, r=[pr.b, SEGB.b], w=[pos.b])
            P.op("dve", (lambda e_, o=t8.t[:], a=CMB.t[:, i, :]: e_.max(out=o, in_=a)), reads=[CMB.b], writes=[t8.b])
            for kq in range(4):
                oh, jk = OH[kq % 2], JK[kq % 2]
                k.ts("dve", oh.t[:], CMB.t[:, i, :], t8.t[:, kq:kq + 1], ALU.is_equal, r=[CMB.b, t8.b], w=[oh.b])
                k.stt(jk.t[:], oh.t[:], 1.0, pos.t[:], ALU.mult, ALU.mult, r=[oh.b, pos.b], w=[jk.b], wp=[p4.b], accum=p4.t[:, kq:kq + 1])
                k.ts("pool", sr.t[:, kq, 0:1], IOTA.t[:], float(i * 128), ALU.add, r=[IOTA.b], wp=[sr.b])
                k.cp("pool", sr.t[:, kq, 1:2], t8.t[:, kq:kq + 1], r=[t8.b], wp=[sr.b])
            k.ts("dve", p4.t[:], p4.t[:], float(NSLOT - 1), ALU.min, r=[p4.b], w=[p4.b])
            k.cp("dve", p4i.t[:], p4.t[:], r=[p4.b], w=[p4i.b])
            k.cp("dve", P4all.t[:, i, :], p4.t[:], r=[p4.b], wp=[P4all.b])
            for kq in range(4):
                def sca(e_, off=p4i.t[:, kq:kq + 1], src=sr.t[:, kq, :]):
                    return e_.indirect_dma_start(out=d["SLOT"], out_offset=bass.IndirectOffsetOnAxis(ap=off, axis=0), in_=src, in_offset=None)
                pool_dma_op(P, sca, reads=[p4i.b, sr.b, dInit], wpart=[dSLOT])
        P.end_phase()


def phase_smoe(c, layer, MOD5, TEi):
    nc, P, k, d, cfg = c.nc, c.P, c.k, c.d, c.cfg
    S, E, NTILE = cfg.S, cfg.E, cfg.NTILE
    IDXW, IDXB, P4all = TEi
    hin = d["H1"] if layer == 0 else d["H3"]
    hout = d["H2"] if layer == 0 else d["out"]
    w1tab = d["moe_w1"].rearrange("l e k n -> (l e k) n")
    w2tab = d["moe_w2"].rearrange("l e k n -> (l e k) n")
    b1tab = d["moe_b1"].rearrange("l r f -> (l r) f")
    with ExitStack() as ph:
        Z = sb(ph, nc, [128, 1024], BF16, name="z")
        W1 = sb(ph, nc, [128, 8, 2048], BF16, n=2, name="w1")
        W2 = sb(ph, nc, [128, 8, 1024], BF16, name="w2")
        B1r = sb(ph, nc, [128, 128], F32, n=2, name="b1r")
        B1c = sb(ph, nc, [128, 16], F32, n=2, name="b1c")
        SLt = sb(ph, nc, [128, 4, 2], F32, n=2, name="slt")
        TKi = sb(ph, nc, [128, 4], I32, n=2, name="tki")
        XG = sb(ph, nc, [128, 4, 1024], BF16, n=2, name="xg")
        XT = sb(ph, nc, [128, 8, 512], BF16, n=2, name="xt")
        ACTT = sb(ph, nc, [128, 8, 512], BF16, n=2, name="actt")
        G1 = sb(ph, nc, [128, 512], F32, n=3, name="g1")
        S1 = sb(ph, nc, [128, 512], F32, n=3, name="s1")
        L2 = sb(ph, nc, [128, 512], F32, n=3, name="l2")
        GS = sb(ph, nc, [128, 512], F32, n=3, name="gs")
        OS = sb(ph, nc, [128, 1024], F32, n=4, name="os")
        pT = ps(ph, nc, [128, 1024], BF16, n=2, name="pT")
        pG = ps(ph, nc, [128, 512], F32, n=2, name="pG")
        pL = ps(ph, nc, [128, 512], F32, n=2, name="pL")
        pO = ps(ph, nc, [128, 512], F32, n=2, name="pO")
        dACC, dXM2z, dOUT = Buf("dACCs"), Buf("dXM2z"), Buf("dOUT")
        k.memset("dve", Z.t[:], 0.0, w=[Z.b])
        k.dma("sp", d["XM2"][S:S + 128, :], Z.t[:], r=[Z.b], w=[dXM2z])

        def gather(out_ap, tab, idx_ap, reads, wslot, part=False):
            def g(e_):
                return e_.indirect_dma_start(out=out_ap, out_offset=None, in_=tab, in_offset=bass.IndirectOffsetOnAxis(ap=idx_ap, axis=0))
            return pool_dma_op(P, g, reads=reads, writes=() if part else [wslot], wpart=[wslot] if part else ())

        def loads(j):
            w1, b1r, slt, tki, xg = W1[j % 2], B1r[j % 2], SLt[j % 2], TKi[j % 2], XG[j % 2]
            k.dma("sp", slt.t[:], d["SLOT"][j * 512:(j + 1) * 512, :].rearrange("(s p) c -> p s c", p=128), w=[slt.b])
            k.cp("dve", tki.t[:], slt.t[:, :, 0], r=[slt.b], w=[tki.b])
            for kk in range(8):
                gather(w1.t[:, kk, :], w1tab, IDXW.t[:, j, kk:kk + 1], [IDXW.b], w1.b, part=True)
            gather(b1r.t[:], b1tab, IDXB.t[:, j:j + 1], [IDXB.b], b1r.b)
            for sub in range(4):
                gather(xg.t[:, sub, :], d["XM2"], tki.t[:, sub:sub + 1], [tki.b, dXM2z], xg.b, part=True)

        io = 0
        loads(0)
        for j in range(NTILE):
            if j + 1 < NTILE:
                loads(j + 1)
            w1, b1r, b1c, slt, tki, xg, xt, actt = (X[j % 2] for X in (W1, B1r, B1c, SLt, TKi, XG, XT, ACTT))
            k.tr(pG[0].t[:, 0:16], b1r.t[0:16, :], c.ident_f.t[0:16, 0:16], r=[b1r.b], w=[pG[0].b])
            k.cp("dve", b1c.t[:, 0:8], pG[0].t[:, 0:8], r=[pG[0].b], wp=[b1c.b])
            k.ts("dve", b1c.t[:, 8:16], pG[0].t[:, 8:16], 1.0, ALU.add, r=[pG[0].b], wp=[b1c.b])
            for sub in range(4):
                pt = pT[sub % 2]
                for kk in range(8):
                    k.tr(pt.t[:, kk * 128:(kk + 1) * 128], xg.t[:, sub, kk * 128:(kk + 1) * 128], c.ident_b.t[:], r=[xg.b],
                         w=[pt.b] if kk == 0 else (), wp=() if kk == 0 else [pt.b])
                k.cp("act" if sub % 2 else "dve", xt.t[:, :, sub * 128:(sub + 1) * 128], pt.t[:].rearrange("p (k t) -> p k t", k=8), r=[pt.b], wp=[xt.b])
            for pr in range(8):
                pg, pl = pG[pr % 2], pL[pr % 2]
                g1, s1, l2, gs = (X[pr % 3] for X in (G1, S1, L2, GS))
                for kk in range(8):
                    k.mm(pg.t[:], w1.t[:, kk, pr * 128:(pr + 1) * 128], xt.t[:, kk, :], kk == 0, kk == 7,
                         r=[w1.b, xt.b], w=[pg.b] if kk == 0 else (), wp=() if kk == 0 else [pg.b])
                for kk in range(8):
                    k.mm(pl.t[:], w1.t[:, kk, 1024 + pr * 128:1024 + (pr + 1) * 128], xt.t[:, kk, :], kk == 0, kk == 7,
                         r=[w1.b, xt.b], w=[pl.b] if kk == 0 else (), wp=() if kk == 0 else [pl.b])
                k.ts("dve", g1.t[:], pg.t[:], b1c.t[:, pr:pr + 1], ALU.add, 7.0, ALU.min, r=[pg.b, b1c.b], w=[g1.b])
                k.act(s1.t[:], g1.t[:], AF.Sigmoid, r=[g1.b], w=[s1.b], scale=1.702)
                k.ts("dve", l2.t[:], pl.t[:], b1c.t[:, 8 + pr:9 + pr], ALU.add, 8.0, ALU.min, r=[pl.b, b1c.b], w=[l2.b])
                k.tt("dve", gs.t[:], g1.t[:], s1.t[:], ALU.mult, r=[g1.b, s1.b], w=[gs.b])
                k.stt(actt.t[:, pr, :], l2.t[:], -6.0, gs.t[:], ALU.max, ALU.mult, r=[l2.b, gs.b], wp=[actt.b])
            for kk in range(8):
                gather(W2.t[:, kk, :], w2tab, IDXW.t[:, j, kk:kk + 1], [IDXW.b], W2.b, part=True)
            for sub in range(4):
                os_ = OS[(4 * j + sub) % 4]
                for half in range(2):
                    po = pO[io % 2]
                    io += 1
                    for jj in range(8):
                        k.mm(po.t[:], actt.t[:, jj, sub * 128:(sub + 1) * 128], W2.t[:, jj, half * 512:(half + 1) * 512], jj == 0, jj == 7,
                             r=[actt.b, W2.b], w=[po.b] if jj == 0 else (), wp=() if jj == 0 else [po.b])
                    k.act(os_.t[:, half * 512:(half + 1) * 512], po.t[:], AF.Copy, r=[po.b, slt.b], wp=[os_.b], scale=slt.t[:, sub, 1:2])

                k.dma("sp", d["OUTS"][j * 512 + sub * 128:j * 512 + (sub + 1) * 128, :], os_.t[:], r=[os_.b], wp=[dACC])
        GA = sb(ph, nc, [128, 1024], F32, n=2, name="ga")
        for t in range(S // 128):
            ht_, at_ = OS[t % 2], OS[2 + t % 2]
            rows = slice(t * 128, (t + 1) * 128)
            k.dma("sp", ht_.t[:], hin[rows, :], w=[ht_.b])
            k.dma("sp", at_.t[:], d["ACCd"][rows, :], w=[at_.b])
            for kq in range(4):
                ga = GA[kq % 2]
                gather(ga.t[:], d["OUTS"], P4all.t[:, t, kq:kq + 1], [P4all.b, dACC], ga.b)
                k.tt("dve", at_.t[:], at_.t[:], ga.t[:], ALU.add, r=[at_.b, ga.b], w=[at_.b])
            k.tt("dve", at_.t[:], at_.t[:], MOD5.t[:], ALU.mult, r=[at_.b, MOD5.b], w=[at_.b])
            k.tt("pool", ht_.t[:], ht_.t[:], at_.t[:], ALU.add, r=[ht_.b, at_.b], w=[ht_.b])
            k.dma("sp", hout[rows, :], ht_.t[:], r=[ht_.b], wp=[dOUT])
        P.end_phase()
```
